# Optimizing a Trainium2 kernel written in Bass

```python
import math
import jax
import jax.numpy as jnp
from jax import lax
import numpy as np

D_MODEL = 1024
BATCH = 2
SEQ = 8192
DEPTH = 2

GRID_W = 64
CTX_LEN = 256
BRANCH_W = D_MODEL // 2
N_BRANCH = 3
GLA_HEADS = 4
GLA_DV = BRANCH_W // GLA_HEADS
GLA_DK = GLA_DV // 2
GLA_RANK = 16
GLA_TAU = 16.0
GLA_CHUNK = 16
MLA_HEADS = 8
MLA_DV = BRANCH_W // MLA_HEADS
MLA_NOPE = MLA_DV
MLA_ROPE = MLA_NOPE // 2
MLA_Q_RANK = 3 * D_MODEL // 8
MLA_KV_RANK = D_MODEL // 4
ROPE_BASE = 10000.0
Q_BLOCK = 128
HY_W = BRANCH_W
HY_ORDER = 2
HY_DIRS = 2
HY_BANDS = 16
HY_EMB = 2 * HY_BANDS + 1
HY_FFN = 64
HY_DECAY_TARGET = 1e-2
HY_FAST_PCT = 0.3
HY_SLOW_PCT = 1.5
N_EXPERTS = 32
TOP_K = 4
D_FF = D_MODEL
SWIGLU_LIMIT = 7.0
SWIGLU_ALPHA = 1.702
MOE_BLOCK = 128
LN_EPS = 1e-5
RMS_EPS = 1e-6
DEEPNORM_ALPHA = (2 * DEPTH) ** 0.25
DEEPNORM_BETA = (8 * DEPTH) ** -0.25
IN_SPLITS = (
    GLA_HEADS * GLA_DK,
    GLA_HEADS * GLA_DV,
    GLA_RANK,
    GLA_RANK,
    MLA_KV_RANK,
    MLA_ROPE,
    GLA_HEADS * GLA_DK,
    GLA_HEADS * GLA_DV,
    MLA_Q_RANK,
    3 * HY_W,
    N_BRANCH * D_MODEL,
)
N_KEY_GROUPS = 6
KEY_COLS = sum(IN_SPLITS[:N_KEY_GROUPS])
IN_OFFSETS = tuple(int(o) for o in np.cumsum(IN_SPLITS)[:-1])
IN_TOTAL = sum(IN_SPLITS)
F32 = jnp.float32

kernel_name = 'hybrid_gla_mla_hyena_moe_diffusion_block'


def layer_norm(x, gain=None, bias=None):
    xf = x.astype(F32)
    xc = xf - xf.mean(-1, keepdims=True)
    y = xc * lax.rsqrt((xc * xc).mean(-1, keepdims=True) + LN_EPS)
    if gain is not None:
        y = y * gain.astype(F32) + bias.astype(F32)
    return y.astype(x.dtype)


def rms_norm(x, gain):
    xf = x.astype(F32)
    y = xf * lax.rsqrt(jnp.mean(xf * xf, -1, keepdims=True) + RMS_EPS) * gain.astype(F32)
    return y.astype(x.dtype)


def modulate(x, shift, scale):
    return layer_norm(x) * (1 + scale) + shift


def to_heads(t, n):
    B, L, _ = t.shape
    return t.reshape(B, L, n, -1).transpose(0, 2, 1, 3)


def from_heads(t):
    B, H, L, d = t.shape
    return t.transpose(0, 2, 1, 3).reshape(B, L, H * d)


def _rev(t):
    return None if t is None else jnp.flip(t, axis=2)


def axial_rope_angles(L):
    rows = L // GRID_W
    row = jnp.repeat(jnp.arange(rows, dtype=F32), GRID_W)
    col = jnp.tile(jnp.arange(GRID_W, dtype=F32), rows)
    n_freq = MLA_ROPE // 4
    inv = ROPE_BASE ** (-jnp.arange(n_freq, dtype=F32) / n_freq)
    ang = jnp.concatenate([row[:, None] * inv, col[:, None] * inv], -1)
    return jnp.cos(ang), jnp.sin(ang)


def apply_rope(x, cos, sin):
    half = x.shape[-1] // 2
    x1, x2 = x[..., :half].astype(F32), x[..., half:].astype(F32)
    return jnp.concatenate([x1 * cos - x2 * sin, x2 * cos + x1 * sin], -1).astype(x.dtype)


def attend_blocks(q, k, v):
    B, H, Lq, dq = q.shape
    nb = Lq // Q_BLOCK
    scale = dq ** -0.5
    qb = q.reshape(B, H, nb, Q_BLOCK, dq).transpose(2, 0, 1, 3, 4)

    def one_block(qi):
        s = jnp.einsum('bhqd,bhkd->bhqk', qi, k).astype(F32) * scale
        p = jax.nn.softmax(s, axis=-1)
        return jnp.einsum('bhqk,bhkd->bhqd', p.astype(v.dtype), v)

    o = lax.map(one_block, qb)
    return o.transpose(1, 2, 0, 3, 4).reshape(B, H, Lq, v.shape[-1])


def _chunk(t):
    B, H, L, d = t.shape
    return t.reshape(B, H, L // GLA_CHUNK, GLA_CHUNK, d)


def gla_states(k, v, b, s0):
    b_last = b[:, :, :, -1:, :]
    delta = jnp.einsum('bhncd,bhnce->bhnde', k * jnp.exp(b_last - b), v)
    dec = jnp.exp(b_last[:, :, :, 0, :])

    def step(s, inp):
        d_n, delta_n = inp
        return d_n[..., None] * s + delta_n, s

    s_final, s_prev = lax.scan(step, s0, (jnp.moveaxis(dec, 2, 0), jnp.moveaxis(delta, 2, 0)))
    return jnp.moveaxis(s_prev, 0, 2), s_final


def gla_outputs(q, k, v, b, s_prev):
    C = q.shape[3]
    lower = jnp.tril(jnp.ones((C, C), bool))
    diff = b[:, :, :, :, None, :] - b[:, :, :, None, :, :]
    decay = jnp.exp(jnp.where(lower[:, :, None], diff, -jnp.inf))
    scores = jnp.einsum('bhnid,bhnjd,bhnijd->bhnij', q, k, decay)
    o = (jnp.einsum('bhnij,bhnje->bhnie', scores, v)
         + jnp.einsum('bhnid,bhnde->bhnie', q * jnp.exp(b), s_prev))
    B, H, N, _, dv = o.shape
    return o.reshape(B, H, N * C, dv)


def gla_direction(q, k, v, g, qc, kc, vc, gc):
    B, H, _, dk = k.shape
    s0 = jnp.zeros((B, H, dk, v.shape[-1]), F32)
    kc_, vc_ = _chunk(kc), _chunk(vc)
    bc = jnp.cumsum(_chunk(gc), axis=3)
    sc_prev, sc_final = gla_states(kc_, vc_, bc, s0)
    oc = None if qc is None else gla_outputs(_chunk(qc), kc_, vc_, bc, sc_prev)
    k_, v_ = _chunk(k), _chunk(v)
    b = jnp.cumsum(_chunk(g), axis=3)
    s_prev, _ = gla_states(k_, v_, b, sc_final)
    return gla_outputs(_chunk(q), k_, v_, b, s_prev), oc


def gla_keys(pk, pv, paf, pab, wa2_f, ba_f, wa2_b, ba_b):
    k = to_heads(pk, GLA_HEADS)
    v = to_heads(pv, GLA_HEADS)
    g_f = to_heads(jax.nn.log_sigmoid((paf @ wa2_f + ba_f).astype(F32)) / GLA_TAU, GLA_HEADS)
    g_b = to_heads(jax.nn.log_sigmoid((pab @ wa2_b + ba_b).astype(F32)) / GLA_TAU, GLA_HEADS)
    return k, v, g_f, g_b


def gla_query(pq):
    return to_heads(pq, GLA_HEADS) * GLA_DK ** -0.5


def gla_finish(o, r, norm_w):
    return from_heads(rms_norm(o, norm_w)).astype(r.dtype) * jax.nn.silu(r)


def mla_keys(pkva, pkr, kv_norm, w_ukv, rope):
    kv = to_heads(rms_norm(pkva, kv_norm) @ w_ukv, MLA_HEADS)
    k_nope, v = kv[..., :MLA_NOPE], kv[..., MLA_NOPE:]
    k_rope = pkr[:, None]
    if rope is not None:
        k_rope = apply_rope(k_rope, *rope)
    k_rope = jnp.broadcast_to(k_rope, k_nope.shape[:-1] + (MLA_ROPE,))
    return jnp.concatenate([k_nope, k_rope], -1), v


def mla_query(pqa, q_norm, w_uq, rope):
    q = to_heads(rms_norm(pqa, q_norm) @ w_uq, MLA_HEADS)
    if rope is not None:
        q = jnp.concatenate([q[..., :MLA_NOPE], apply_rope(q[..., MLA_NOPE:], *rope)], -1)
    return q


def short_conv3(x, w, b):
    xp = jnp.pad(x, ((0, 0), (1, 1), (0, 0)))
    return xp[:, :-2] * w[0] + xp[:, 1:-1] * w[1] + xp[:, 2:] * w[2] + b


def hyena_filters(L, w1, b1, w2, b2, w3, freq):
    t = jnp.linspace(0.0, 1.0, L, dtype=F32)[:, None]
    w = 2 * math.pi * jnp.arange(L, dtype=F32)[:, None] / L
    f = jnp.linspace(1e-4, HY_BANDS - 1, HY_BANDS, dtype=F32)
    z = jnp.concatenate([t, jnp.cos(f * w), -jnp.sin(f * w)], -1)
    fr = freq.astype(F32)
    h = jnp.sin(fr * (z @ w1.astype(F32) + b1.astype(F32)))
    h = jnp.sin(fr * (h @ w2.astype(F32) + b2.astype(F32)))
    h = (h @ w3.astype(F32)).reshape(L, HY_ORDER, HY_DIRS, HY_W)
    deltas = jnp.abs(jnp.linspace(math.log(HY_DECAY_TARGET) / HY_SLOW_PCT,
                                  math.log(HY_DECAY_TARGET) / HY_FAST_PCT, HY_W, dtype=F32))
    h = h * jnp.exp(-t * deltas)[:, None, None, :]
    return h / jnp.sum(jnp.abs(h), axis=(0, 2), keepdims=True)


def two_sided_fftconv(z, h_fwd, h_bwd, bias):
    B, L, C = z.shape
    k = jnp.concatenate([h_fwd, jnp.zeros((1, C), F32), jnp.flip(h_bwd[1:], 0)], 0)
    zf = jnp.fft.rfft(z.astype(F32), n=2 * L, axis=1)
    kf = jnp.fft.rfft(k, n=2 * L, axis=0)
    y = jnp.fft.irfft(zf * kf[None], n=2 * L, axis=1)[:, :L]
    return (y + z.astype(F32) * bias.astype(F32)).astype(z.dtype)


def hyena_branch(p, conv_w, conv_b, w1, b1, w2, b2, w3, freq, hbias):
    v, x1, x2 = jnp.split(short_conv3(p, conv_w, conv_b), 3, axis=-1)
    filt = hyena_filters(p.shape[1], w1, b1, w2, b2, w3, freq)
    z = x1 * two_sided_fftconv(v, filt[:, 0, 0], filt[:, 0, 1], hbias[0])
    return x2 * two_sided_fftconv(z, filt[:, 1, 0], filt[:, 1, 1], hbias[1])


def merge_branches(o_gla, o_mla, o_hy, gates, w_br_gla, w_br_mla, w_br_hy, w_out):
    g1, g2, g3 = jnp.split(jax.nn.sigmoid(gates.astype(F32)).astype(o_gla.dtype), 3, axis=-1)
    y = g1 * (o_gla @ w_br_gla) + g2 * (o_mla @ w_br_mla) + g3 * (o_hy @ w_br_hy)
    return y @ w_out


def token_mixer(h, hc, rope, with_ctx_out, w_in, gla_wa2_f, gla_ba_f, gla_wa2_b, gla_ba_b, gla_norm,
                mla_q_norm, mla_w_uq, mla_kv_norm, mla_w_ukv,
                hy_conv_w, hy_conv_b, hy_w1, hy_b1, hy_w2, hy_b2, hy_w3, hy_freq, hy_bias,
                w_br_gla, w_br_mla, w_br_hy, w_out):
    gk, gv, gaf, gab, mkva, mkr, gq, gr, mqa, hy, gates = jnp.split(h @ w_in, IN_OFFSETS, axis=-1)
    if with_ctx_out:
        ck, cv, caf, cab, ckva, ckr, cq, cr, cqa, chy, cgates = jnp.split(hc @ w_in, IN_OFFSETS, axis=-1)
    else:
        ck, cv, caf, cab, ckva, ckr = jnp.split(hc @ w_in[:, :KEY_COLS], IN_OFFSETS[:N_KEY_GROUPS - 1], axis=-1)
    gate_w = (gla_wa2_f, gla_ba_f, gla_wa2_b, gla_ba_b)
    k, v, g_f, g_b = gla_keys(gk, gv, gaf, gab, *gate_w)
    kc, vc, gc_f, gc_b = gla_keys(ck, cv, caf, cab, *gate_w)
    q = gla_query(gq)
    qc = gla_query(cq) if with_ctx_out else None
    o_f, oc_f = gla_direction(q, k, v, g_f, qc, kc, vc, gc_f)
    o_b, oc_b = gla_direction(_rev(q), _rev(k), _rev(v), _rev(g_b), _rev(qc), _rev(kc), _rev(vc), _rev(gc_b))
    o_gla = gla_finish(o_f + _rev(o_b), gr, gla_norm)
    mk, mv = mla_keys(mkva, mkr, mla_kv_norm, mla_w_ukv, rope)
    mkc, mvc = mla_keys(ckva, ckr, mla_kv_norm, mla_w_ukv, None)
    mq = mla_query(mqa, mla_q_norm, mla_w_uq, rope)
    o_mla = from_heads(attend_blocks(mq, jnp.concatenate([mkc, mk], 2), jnp.concatenate([mvc, mv], 2)))
    hy_w = (hy_conv_w, hy_conv_b, hy_w1, hy_b1, hy_w2, hy_b2, hy_w3, hy_freq, hy_bias)
    o_hy = hyena_branch(hy, *hy_w)
    br_w = (w_br_gla, w_br_mla, w_br_hy, w_out)
    y = merge_branches(o_gla, o_mla, o_hy, gates, *br_w)
    if not with_ctx_out:
        return y, None
    oc_gla = gla_finish(oc_f + _rev(oc_b), cr, gla_norm)
    oc_mla = from_heads(attend_blocks(mla_query(cqa, mla_q_norm, mla_w_uq, None), mkc, mvc))
    oc_hy = hyena_branch(chy, *hy_w)
    yc = merge_branches(oc_gla, oc_mla, oc_hy, cgates, *br_w)
    return y, yc


def moe_ffn(h, router_w, router_b, w1, b1, w2, b2):
    N, D = h.shape
    logits = (h @ router_w + router_b).astype(F32)
    top_val, top_idx = lax.top_k(logits, TOP_K)
    gates = jax.nn.softmax(top_val, axis=-1)
    NK = N * TOP_K
    flat_e = top_idx.reshape(NK)
    order = jnp.argsort(flat_e)
    sorted_e = flat_e[order]
    sorted_tok = (order // TOP_K).astype(jnp.int32)
    counts = jnp.bincount(flat_e, length=N_EXPERTS)
    padded = (counts + MOE_BLOCK - 1) // MOE_BLOCK * MOE_BLOCK
    pad_end = jnp.cumsum(padded)
    pad_start = pad_end - padded
    start = jnp.cumsum(counts) - counts
    dest = pad_start[sorted_e] + (jnp.arange(NK) - start[sorted_e])
    n_blocks = -(-NK // MOE_BLOCK) + N_EXPERTS
    buf_tok = jnp.zeros((n_blocks * MOE_BLOCK,), jnp.int32).at[dest].set(sorted_tok)
    block_e = jnp.minimum(jnp.searchsorted(pad_end, jnp.arange(n_blocks) * MOE_BLOCK, side='right'), N_EXPERTS - 1)
    xb = h[buf_tok].reshape(n_blocks, MOE_BLOCK, D)

    def expert_block(args):
        xi, e = args
        glu, lin = jnp.split(xi @ w1[e] + b1[e], 2, axis=-1)
        glu = jnp.minimum(glu, SWIGLU_LIMIT)
        lin = jnp.clip(lin, -SWIGLU_LIMIT, SWIGLU_LIMIT)
        act = glu * jax.nn.sigmoid(SWIGLU_ALPHA * glu) * (lin + 1)
        return act @ w2[e] + b2[e]

    yb = lax.map(expert_block, (xb, block_e)).reshape(-1, D)
    y_assign = yb[dest] * gates.reshape(NK)[order][:, None].astype(yb.dtype)
    return jax.ops.segment_sum(y_assign, sorted_tok, num_segments=N)


def setup_inputs(seed: int = 0) -> dict:
    key = jax.random.key(seed)
    ks = iter(jax.random.split(key, 48))

    def nrm(shape, scale):
        return jax.random.normal(next(ks), shape, F32) * scale

    def gain(shape):
        return 1.0 + nrm(shape, 0.05)

    L_, D = DEPTH, D_MODEL
    return {
        'x': nrm((BATCH, SEQ, D), 1.0),
        'c': nrm((BATCH, D), 1.0),
        'ctx': nrm((BATCH, CTX_LEN, D), 1.0),
        'c_ctx': nrm((D,), 1.0),
        'ada_w': nrm((L_, D, 6 * D), 0.5 * D ** -0.5),
        'ada_b': nrm((L_, 6 * D), 0.02),
        'w_in': nrm((L_, D, IN_TOTAL), D ** -0.5),
        'gla_wa2_f': nrm((L_, GLA_RANK, GLA_HEADS * GLA_DK), GLA_RANK ** -0.5),
        'gla_ba_f': nrm((L_, GLA_HEADS * GLA_DK), 0.1),
        'gla_wa2_b': nrm((L_, GLA_RANK, GLA_HEADS * GLA_DK), GLA_RANK ** -0.5),
        'gla_ba_b': nrm((L_, GLA_HEADS * GLA_DK), 0.1),
        'gla_norm': gain((L_, GLA_DV)),
        'mla_q_norm': gain((L_, MLA_Q_RANK)),
        'mla_w_uq': nrm((L_, MLA_Q_RANK, MLA_HEADS * (MLA_NOPE + MLA_ROPE)), MLA_Q_RANK ** -0.5),
        'mla_kv_norm': gain((L_, MLA_KV_RANK)),
        'mla_w_ukv': nrm((L_, MLA_KV_RANK, MLA_HEADS * (MLA_NOPE + MLA_DV)), MLA_KV_RANK ** -0.5),
        'hy_conv_w': nrm((L_, 3, 3 * HY_W), 3 ** -0.5),
        'hy_conv_b': nrm((L_, 3 * HY_W), 0.02),
        'hy_w1': nrm((L_, HY_EMB, HY_FFN), HY_EMB ** -0.5),
        'hy_b1': nrm((L_, HY_FFN), 0.1),
        'hy_w2': nrm((L_, HY_FFN, HY_FFN), HY_FFN ** -0.5),
        'hy_b2': nrm((L_, HY_FFN), 0.1),
        'hy_w3': nrm((L_, HY_FFN, HY_ORDER * HY_DIRS * HY_W), HY_FFN ** -0.5),
        'hy_freq': gain((L_, HY_FFN)),
        'hy_bias': nrm((L_, HY_ORDER, HY_W), 1.0),
        'w_br_gla': nrm((L_, BRANCH_W, D), BRANCH_W ** -0.5 * DEEPNORM_BETA),
        'w_br_mla': nrm((L_, BRANCH_W, D), BRANCH_W ** -0.5 * DEEPNORM_BETA),
        'w_br_hy': nrm((L_, BRANCH_W, D), BRANCH_W ** -0.5 * DEEPNORM_BETA),
        'w_out': nrm((L_, D, D), D ** -0.5 * DEEPNORM_BETA),
        'ln1_g': gain((L_, D)),
        'ln1_b': nrm((L_, D), 0.02),
        'ln2_g': gain((L_, D)),
        'ln2_b': nrm((L_, D), 0.02),
        'router_w': nrm((L_, D, N_EXPERTS), D ** -0.5),
        'router_b': nrm((L_, N_EXPERTS), 0.01),
        'moe_w1': nrm((L_, N_EXPERTS, D, 2 * D_FF), D ** -0.5),
        'moe_b1': nrm((L_, N_EXPERTS, 2 * D_FF), 0.02),
        'moe_w2': nrm((L_, N_EXPERTS, D_FF, D), D_FF ** -0.5 * DEEPNORM_BETA),
        'moe_b2': nrm((L_, N_EXPERTS, D), 0.02),
    }


def reference(x, c, ctx, c_ctx, ada_w, ada_b, w_in, gla_wa2_f, gla_ba_f, gla_wa2_b, gla_ba_b, gla_norm,
              mla_q_norm, mla_w_uq, mla_kv_norm, mla_w_ukv, hy_conv_w, hy_conv_b, hy_w1, hy_b1, hy_w2, hy_b2,
              hy_w3, hy_freq, hy_bias, w_br_gla, w_br_mla, w_br_hy, w_out, ln1_g, ln1_b, ln2_g, ln2_b,
              router_w, router_b, moe_w1, moe_b1, moe_w2, moe_b2):
    B, L, D = x.shape
    CL = ctx.shape[1]
    rope = axial_rope_angles(L)
    xc = ctx
    c_act = jax.nn.silu(c)
    cc_act = jax.nn.silu(c_ctx)
    for l in range(DEPTH):
        last = l == DEPTH - 1
        sh1, sc1, g1, sh2, sc2, g2 = jnp.split((c_act @ ada_w[l] + ada_b[l])[:, None, :], 6, axis=-1)
        csh1, csc1, cg1, csh2, csc2, cg2 = jnp.split(cc_act @ ada_w[l] + ada_b[l], 6, axis=-1)
        y, yc = token_mixer(modulate(x, sh1, sc1), modulate(xc, csh1, csc1), rope, not last,
                            w_in[l], gla_wa2_f[l], gla_ba_f[l], gla_wa2_b[l], gla_ba_b[l], gla_norm[l],
                            mla_q_norm[l], mla_w_uq[l], mla_kv_norm[l], mla_w_ukv[l],
                            hy_conv_w[l], hy_conv_b[l], hy_w1[l], hy_b1[l], hy_w2[l], hy_b2[l], hy_w3[l],
                            hy_freq[l], hy_bias[l], w_br_gla[l], w_br_mla[l], w_br_hy[l], w_out[l])
        x = layer_norm(DEEPNORM_ALPHA * x + g1 * y, ln1_g[l], ln1_b[l])
        h = modulate(x, sh2, sc2).reshape(B * L, D)
        moe_w = (router_w[l], router_b[l], moe_w1[l], moe_b1[l], moe_w2[l], moe_b2[l])
        if last:
            f = moe_ffn(h, *moe_w).reshape(B, L, D)
            x = layer_norm(DEEPNORM_ALPHA * x + g2 * f, ln2_g[l], ln2_b[l])
        else:
            xc = layer_norm(DEEPNORM_ALPHA * xc + cg1 * yc, ln1_g[l], ln1_b[l])
            hc = modulate(xc, csh2, csc2).reshape(B * CL, D)
            f = moe_ffn(jnp.concatenate([h, hc], 0), *moe_w)
            x = layer_norm(DEEPNORM_ALPHA * x + g2 * f[:B * L].reshape(B, L, D), ln2_g[l], ln2_b[l])
            xc = layer_norm(DEEPNORM_ALPHA * xc + cg2 * f[B * L:].reshape(B, CL, D), ln2_g[l], ln2_b[l])
    return x
```

```python
import math
from contextlib import ExitStack

import numpy as np
import concourse.bass as bass
import concourse.mybir as mybir
from concourse.bass_utils import run_bass_kernel_spmd

F32 = mybir.dt.float32
BF16 = mybir.dt.bfloat16
AF = mybir.ActivationFunctionType
ALU = mybir.AluOpType
AX = mybir.AxisListType

D = 1024
NCTX = 256
NLAT = 8192
NT = NCTX + NLAT
NTILE = NT // 128
DEPTH = 2
IN_TOTAL = 6848
LN_EPS = 1e-5
RMS_EPS = 1e-6
ALPHA = (2 * DEPTH) ** 0.25
O_GK, O_GV, O_GAF, O_GAB, O_MKVA, O_MKR, O_GQ, O_GR, O_MQA, O_HY, O_GATES = (
    0, 256, 768, 784, 800, 1056, 1088, 1344, 1856, 2240, 3776)


class Prog:
    def __init__(self, nc, es):
        self.nc = nc
        self.es = es
        self.E = dict(pe=nc.tensor, act=nc.scalar, dve=nc.vector, pool=nc.gpsimd, sp=nc.sync)
        self.sems = {}
        self.cnt = {}
        self.seen = {e: {} for e in self.E}
        self.st = {}
        self.n_ops = 0
        self.alias = {}
        self.free_slots = []
        self.n_slots = 0
        self.persistent = set(['wcast', 'xbzero'])

    def sem(self, key):
        if key not in self.sems:
            self.sems[key] = self.es.enter_context(self.nc.semaphore("s%d" % len(self.sems)))
            self.cnt[key] = 0
        return self.sems[key]

    def _deps(self, reads, writes):
        deps = {}

        def add(k, v):
            if deps.get(k, 0) < v:
                deps[k] = v

        for r in reads:
            s = self.st.get(r)
            if s and s[0]:
                add(*s[0])
        for w in writes:
            s = self.st.get(w)
            if s:
                if s[0]:
                    add(*s[0])
                for k, v in s[1].items():
                    add(k, v)
        return deps

    def _wait(self, eng, deps):
        E = self.E[eng]
        seen = self.seen[eng]
        for k, v in deps.items():
            if k == 'pe' and eng == 'pe':
                continue
            if seen.get(k, 0) < v:
                E.wait_ge(self.sems[k], v)
                seen[k] = v

    def _commit(self, ev, reads, writes):
        k, v = ev
        for r in reads:
            s = self.st.setdefault(r, [None, {}])
            if s[1].get(k, 0) < v:
                s[1][k] = v
        for w in writes:
            self.st[w] = [ev, {}]

    def op(self, eng, reads, writes, fn):
        reads = list(reads)
        writes = list(writes)
        for r in reads:
            if isinstance(r, tuple) and r[0] == 'bank' and r not in writes:
                writes.append(r)
        self._wait(eng, self._deps(reads, writes))
        sem = self.sem(eng)
        ins = fn(self.E[eng])
        self.cnt[eng] += 1
        ins.then_inc(sem, 1)
        self._commit((eng, self.cnt[eng]), reads, writes)
        self.n_ops += 1

    def _slot(self, semkey):
        if semkey in self.persistent:
            return semkey
        if semkey not in self.alias:
            if self.free_slots:
                self.alias[semkey] = self.free_slots.pop()
            else:
                self.alias[semkey] = ('dsem', self.n_slots)
                self.n_slots += 1
        return self.alias[semkey]

    def idma(self, semkey, reads, writes, calls):
        reads = list(reads)
        writes = list(writes)
        self._wait('pool', self._deps(reads, writes))
        key = self._slot(semkey)
        sem = self.sem(key)
        for kw in calls:
            self.nc.gpsimd.indirect_dma_start(**kw).then_inc(sem, 16)
            self.cnt[key] += 16
        self._commit((key, self.cnt[key]), reads, writes)
        self.n_ops += len(calls)

    def dma(self, eng, semkey, reads, writes, pairs, slow=False):
        reads = list(reads)
        writes = list(writes)
        self._wait(eng, self._deps(reads, writes))
        semkey = self._slot(semkey)
        sem = self.sem(semkey)
        for o, i in pairs:
            if slow:
                self.E[eng].dma_start(out=o, in_=i, allow_slow_non_contiguous=True).then_inc(sem, 16)
            else:
                self.E[eng].dma_start(out=o, in_=i).then_inc(sem, 16)
            self.cnt[semkey] += 16
        self._commit((semkey, self.cnt[semkey]), reads, writes)
        self.n_ops += len(pairs)

    def pe_fence(self, ins):
        sem = self.sem('pe')
        self.cnt['pe'] += 1
        ins.then_inc(sem, 1)
        self.E['pe'].wait_ge(sem, self.cnt['pe'])
        self.seen['pe']['pe'] = self.cnt['pe']

    def barrier(self):
        deps = {k: v for k, v in self.cnt.items() if v > 0 and k not in self.persistent}
        for eng in self.E:
            self._wait(eng, dict(deps))
        self.free_slots = [('dsem', i) for i in range(self.n_slots)]
        self.alias = {}

    def finish(self, eng='sp'):
        deps = {}
        for k, c in self.cnt.items():
            if c > 0:
                deps[k] = c
        self._wait(eng, deps)


class Ctx:
    pass


_uid = [0]


def sb(c, name, shape, dt):
    _uid[0] += 1
    return c.es.enter_context(c.nc.sbuf_tensor("%s_u%d" % (name, _uid[0]), list(shape), dt))


def ps(c, name, shape, dt):
    return c.es.enter_context(c.nc.psum_tensor(name, list(shape), dt))


def bank16(c, i):
    return c.bank[i][:, :].bitcast(BF16).rearrange("p (a b) -> p a b", a=8)


def dram(c, name, shape, dt, out=False):
    kind = "ExternalOutput" if (out or name in c.dbg) else "Internal"
    return c.nc.dram_tensor(name, list(shape), dt, kind=kind).ap()


def stage_adaln(c, l):
    with ExitStack() as es:
        c.es = es
        c.adaw = [sb(c, 'adaw%d' % i, [128, 8, 512], F32) for i in range(2)]
        c.adab = [sb(c, 'adab%d' % i, [2, 512], F32) for i in range(2)]
        c.modrow = [sb(c, 'modrow%d' % i, [2, 512], F32) for i in range(2)]
        c.psA = [c.bank[0], c.bank[1]]
        _stage_adaln(c, l)
        c.p.barrier()


def _stage_adaln(c, l):
    p, nc = c.p, c.nc
    I = c.inp
    cT = c.cT
    modv = c.modv[l]
    for cb in range(12):
        slot = cb % 2
        wt = c.adaw[slot]
        p.dma('sp', 'adaw%d' % slot, [], [('adaw', slot)],
              [(wt[:, :, :], I['ada_w'][l, :, cb * 512:(cb + 1) * 512].rearrange("(k p) n -> p k n", p=128))])
        bt = c.adab[slot]
        p.dma('sp', 'adab%d' % slot, [], [('adab', slot)],
              [(bt[0:1, :], I['ada_b'][l:l + 1, cb * 512:(cb + 1) * 512]),
               (bt[1:2, :], I['ada_b'][l:l + 1, cb * 512:(cb + 1) * 512])])
        pt = c.psA[cb % 2]

        def mm(E, wt=wt, pt=pt):
            ins = None
            for k in range(8):
                ins = E.matmul(pt[0:2, :], lhsT=cT[:, k, :], rhs=wt[:, k, :], start=(k == 0), stop=(k == 7))
            return ins
        p.op('pe', [('adaw', slot), 'cT'], [('bank', cb % 2)], mm)
        mt = c.modrow[slot]
        p.op('dve', [('bank', cb % 2), ('adab', slot)], [('modrow', slot)],
             lambda E, mt=mt, pt=pt, bt=bt: E.tensor_tensor(out=mt[0:2, :], in0=pt[0:2, :], in1=bt[0:2, :], op=ALU.add))
        p.dma('sp', 'modrow%d' % slot, [('modrow', slot)], [('modv', l)],
              [(modv[0:2, cb * 512:(cb + 1) * 512], mt[0:2, :])])


def load_mod(c, l):
    p = c.p
    modv = c.modv[l]
    pairs = []
    for r in range(2):
        for g in range(6):
            pairs.append((c.modT[:, r, g, :], modv[r, g * 1024:(g + 1) * 1024].rearrange("(k p) -> p k", p=128)))
    p.dma('sp', 'modT', [('modv', l)], ['modT'], pairs, slow=True)
    p.op('dve', ['modT'], ['modT'],
         lambda E: E.tensor_scalar(out=c.modT[:, :, 1, :], in0=c.modT[:, :, 1, :], scalar1=1.0, scalar2=None, op0=ALU.add))
    p.op('dve', ['modT'], ['modT'],
         lambda E: E.tensor_scalar(out=c.modT[:, :, 4, :], in0=c.modT[:, :, 4, :], scalar1=1.0, scalar2=None, op0=ALU.add))


def ln_mod_tile(c, src_ap, r, gsh, gsc, hT, col0, hkey):
    p = c.p
    i = c.ln_i
    c.ln_i += 1
    s = i % 3
    xt = c.xt[s]
    st = c.lnst[s]
    p.dma('sp', 'xt%d' % s, [], [('xt', s)], [(xt[:, :], src_ap)])
    junk = c.junk[i % 2]
    p.op('act', [('xt', s)], [('junk', i % 2), ('lnst', s, 0)],
         lambda E: E.activation(out=junk[:, :], in_=xt[:, :], func=AF.Copy, accum_out=st[:, 0:1]))
    p.op('dve', [('lnst', s, 0)], [('lnst', s, 1)],
         lambda E: E.tensor_scalar(out=st[:, 1:2], in0=st[:, 0:1], scalar1=-1.0 / D, scalar2=None, op0=ALU.mult))
    p.op('act', [('xt', s), ('lnst', s, 1)], [('junk', i % 2), ('lnst', s, 2)],
         lambda E: E.activation(out=junk[:, :], in_=xt[:, :], func=AF.Square, bias=st[:, 1:2], scale=1.0,
                                accum_out=st[:, 2:3]))
    p.op('act', [('lnst', s, 2)], [('lnst', s, 3)],
         lambda E: E.activation(out=st[:, 3:4], in_=st[:, 2:3], func=AF.Ln, scale=1.0 / D, bias=LN_EPS))
    p.op('act', [('lnst', s, 3)], [('lnst', s, 4)],
         lambda E: E.activation(out=st[:, 4:5], in_=st[:, 3:4], func=AF.Exp, scale=-0.5))
    if c.cfg.get('lnsteps', 9) < 2:
        return
    xh = c.xh[i % 2]
    p.op('dve', [('xt', s), ('lnst', s, 1), ('lnst', s, 4)], [('xh', i % 2)],
         lambda E: E.tensor_scalar(out=xh[:, :], in0=xt[:, :], scalar1=st[:, 1:2], scalar2=st[:, 4:5],
                                   op0=ALU.add, op1=ALU.mult))
    if c.cfg.get('lnsteps', 9) < 3:
        return
    tp = c.tp[i % 2]

    def tr(E):
        ins = None
        for k in range(8):
            ins = E.transpose(tp[:, k, :], xh[:, k * 128:(k + 1) * 128], c.ident[:, :])
        return ins
    p.op('pe', [('xh', i % 2), 'ident'], [c.tpk[i % 2]], tr)
    if c.cfg.get('lnsteps', 9) < 4:
        return
    for k in range(8):
        eng = c.cfg.get('evac_eng') or ('act' if i % 2 == 0 else 'dve')
        if eng == 'act':
            p.op('act', [c.tpk[i % 2], 'modT'], [hkey + (k,)],
                 lambda E, k=k: E.activation(out=hT[:, k, col0:col0 + 128], in_=tp[:, k, :], func=AF.Identity,
                                             bias=c.modT[:, r, gsh, k:k + 1], scale=c.modT[:, r, gsc, k:k + 1]))
        else:
            p.op('dve', [c.tpk[i % 2], 'modT'], [hkey + (k,)],
                 lambda E, k=k: E.tensor_scalar(out=hT[:, k, col0:col0 + 128], in0=tp[:, k, :],
                                                scalar1=c.modT[:, r, gsc, k:k + 1],
                                                scalar2=c.modT[:, r, gsh, k:k + 1],
                                                op0=ALU.mult, op1=ALU.add))


FM_GROUPS = [
    ('KT', O_GK, 256), ('QT', O_GQ, 256), ('AFT', O_GAF, 16), ('ABT', O_GAB, 16),
    ('MKVAT', O_MKVA, 256), ('MQAT', O_MQA, 384),
]
TM_GROUPS = [
    ('V', O_GV, 512), ('MKR', O_MKR, 32), ('GR', O_GR, 512), ('HY', O_HY, 1536), ('G3', O_GATES, 3072),
]


def stage_proj(c, l, xsrc):
    with ExitStack() as es:
        c.es = es
        c.win = sb(c, 'win', [128, 8, IN_TOTAL], BF16)
        c.hT = [sb(c, 'hT%d' % i, [128, 8, 512], BF16) for i in range(2)]
        alloc_ln(c)
        c.o16 = [sb(c, 'o16_%d' % i, [128, 512], BF16) for i in range(4)]
        c.o32 = [sb(c, 'o32_%d' % i, [128, 512], F32) for i in range(4)]
        c.psB = [c.bank[i] for i in range(4)]
        load_mod(c, l)
        _stage_proj(c, l, xsrc)
        c.p.barrier()


def alloc_ln(c):
    c.xt = [sb(c, 'xt%d' % i, [128, D], F32) for i in range(3)]
    c.lnst = [sb(c, 'lnst%d' % i, [128, 8], F32) for i in range(3)]
    c.junk = [sb(c, 'junk%d' % i, [128, D], BF16) for i in range(2)]
    c.xh = [sb(c, 'xh%d' % i, [128, D], BF16) for i in range(2)]
    c.tp = [bank16(c, 4), bank16(c, 5)]
    c.tpk = [('bank', 4), ('bank', 5)]
    c.ln_i = 0


def _stage_proj(c, l, xsrc):
    p, nc = c.p, c.nc
    I = c.inp
    win = c.win
    for k in range(8):
        p.dma('pool', 'win', [], [('win', k)],
              [(win[:, k, :], I['w_in'][l, k * 128:(k + 1) * 128, :])])
    S = c.scr[l]
    blocks = [(0, 2)] + [(2 + 4 * i, 4) for i in range(16)]
    blocks = blocks[:c.cfg.get('nblk', 17)]
    ev = 0
    for bi, (t0, ntl) in enumerate(blocks):
        T = ntl * 128
        tok0 = t0 * 128
        hb = bi % 2
        hT = c.hT[hb]
        r = 1 if bi == 0 else 0
        for j in range(ntl):
            ln_mod_tile(c, xsrc(t0 + j), r, 0, 1, hT, j * 128, ('hT', hb))
        hkeys = [('hT', hb, k) for k in range(8)]
        wkeys = [('win', k) for k in range(8)]
        if c.cfg.get('nomm'):
            continue
        for name, off, ncols in FM_GROUPS:
            for m0 in range(0, ncols, 128):
                M = min(128, ncols - m0)
                pb = ev % 4
                pt = c.psB[pb]

                def mm(E, pt=pt, off=off, m0=m0, M=M, T=T):
                    ins = None
                    for k in range(8):
                        ins = E.matmul(pt[0:M, 0:T], lhsT=win[:, k, off + m0:off + m0 + M], rhs=hT[:, k, 0:T],
                                       start=(k == 0), stop=(k == 7))
                    return ins
                p.op('pe', hkeys + wkeys, [('bank', pb)], mm)
                fp32 = name in ('AFT', 'ABT')
                ob = ev % 4
                ot = (c.o32 if fp32 else c.o16)[ob]
                okey = ('o32' if fp32 else 'o16', ob)
                eng = 'act' if ev % 2 == 0 else 'dve'
                if eng == 'act':
                    p.op('act', [('bank', pb)], [okey],
                         lambda E, ot=ot, pt=pt, M=M, T=T: E.activation(out=ot[0:M, 0:T], in_=pt[0:M, 0:T], func=AF.Copy))
                else:
                    p.op('dve', [('bank', pb)], [okey],
                         lambda E, ot=ot, pt=pt, M=M, T=T: E.tensor_copy(out=ot[0:M, 0:T], in_=pt[0:M, 0:T]))
                p.dma('sp', 'o%s%d' % ('32' if fp32 else '16', ob), [okey], [(name, l)],
                      [(S[name][m0:m0 + M, tok0:tok0 + T], ot[0:M, 0:T])])
                ev += 1
        for name, off, ncols in TM_GROUPS:
            for n0 in range(0, ncols, 512):
                N = min(512, ncols - n0)
                for j in range(ntl):
                    pb = ev % 4
                    pt = c.psB[pb]

                    def mm(E, pt=pt, off=off, n0=n0, N=N, j=j):
                        ins = None
                        for k in range(8):
                            ins = E.matmul(pt[:, 0:N], lhsT=hT[:, k, j * 128:(j + 1) * 128],
                                           rhs=win[:, k, off + n0:off + n0 + N], start=(k == 0), stop=(k == 7))
                        return ins
                    p.op('pe', hkeys + wkeys, [('bank', pb)], mm)
                    fp32 = name == 'MKR'
                    ob = ev % 4
                    ot = (c.o32 if fp32 else c.o16)[ob]
                    okey = ('o32' if fp32 else 'o16', ob)
                    if name == 'G3':
                        p.op('act', [('bank', pb)], [okey],
                             lambda E, ot=ot, pt=pt, N=N: E.activation(out=ot[:, 0:N], in_=pt[:, 0:N], func=AF.Sigmoid))
                    elif ev % 2 == 0:
                        p.op('act', [('bank', pb)], [okey],
                             lambda E, ot=ot, pt=pt, N=N: E.activation(out=ot[:, 0:N], in_=pt[:, 0:N], func=AF.Copy))
                    else:
                        p.op('dve', [('bank', pb)], [okey],
                             lambda E, ot=ot, pt=pt, N=N: E.tensor_copy(out=ot[:, 0:N], in_=pt[:, 0:N]))
                    p.dma('sp', 'o%s%d' % ('32' if fp32 else '16', ob), [okey], [(name, l)],
                          [(S[name][tok0 + j * 128:tok0 + (j + 1) * 128, n0:n0 + N], ot[:, 0:N])])
                    ev += 1


def stage_gla(c, l):
    with ExitStack() as es:
        c.es = es
        _stage_gla(c, l)
        c.p.barrier()


def _stage_gla(c, l):
    p, nc = c.p, c.nc
    I = c.inp
    S = c.scr[l]
    NCH = NTILE
    qT = sb(c, 'g_qT', [128, NT], BF16)
    kT = sb(c, 'g_kT', [128, NT], BF16)
    v = sb(c, 'g_v', [128, NCH, 256], BF16)
    oacc = sb(c, 'g_oacc', [128, NCH, 256], F32)
    wa2 = sb(c, 'g_wa2', [16, 2, 256], F32)
    ba = sb(c, 'g_ba', [1, 2, 256], F32)
    normw = sb(c, 'g_normw', [128, 128], F32)
    aft = [sb(c, 'g_aft%d' % i, [16, 128], F32) for i in range(3)]
    g1 = [sb(c, 'g_g1%d' % i, [128, 128], F32) for i in range(2)]
    g2 = [sb(c, 'g_g2%d' % i, [128, 128], F32) for i in range(2)]
    e1 = [sb(c, 'g_e1%d' % i, [128, 128], F32) for i in range(2)]
    e2 = [sb(c, 'g_e2%d' % i, [128, 128], F32) for i in range(2)]
    qb = [sb(c, 'g_qb%d' % i, [128, 128], BF16) for i in range(2)]
    kb = [sb(c, 'g_kb%d' % i, [128, 128], BF16) for i in range(2)]
    kd = [sb(c, 'g_kd%d' % i, [128, 128], BF16) for i in range(2)]
    kdt = [sb(c, 'g_kdt%d' % i, [128, 128], BF16) for i in range(2)]
    am = [sb(c, 'g_am%d' % i, [128, 256], BF16) for i in range(2)]
    St = sb(c, 'g_S', [128, 128], F32)
    Sb = sb(c, 'g_Sb', [128, 128], BF16)
    rst = sb(c, 'g_rst', [128, NCH * 2], F32)
    rst2 = sb(c, 'g_rst2', [128, NCH * 2], F32)
    junk = sb(c, 'g_junk', [128, 128], BF16)
    osb = [sb(c, 'g_osb%d' % i, [128, 512], BF16) for i in range(2)]
    psG = c.bank[0][:, 0:128]
    psBt = [c.bank[1][:, 0:128], c.bank[2][:, 0:128]]
    psA = c.bank[3][:, 0:256]
    psK = c.bank[4][:, :].bitcast(BF16)[:, 0:128]
    psO = [c.bank[5][:, 0:256], c.bank[6][:, 0:256]]
    psS = c.bank[7][:, 0:128]
    c.tp = [bank16(c, 1), bank16(c, 2)]
    cst = c.cst
    U16 = [cst[:, 128:256], cst[:, 256:384]]
    MSK = [cst[:, 384:512], cst[:, 512:640]]
    ones = cst[:, 640:768]

    p.dma('sp', 'g_w', [], ['g_w'],
          [(wa2[:, 0, :], I['gla_wa2_f'][l]), (wa2[:, 1, :], I['gla_wa2_b'][l]),
           (ba[0:1, 0, :], I['gla_ba_f'][l:l + 1, :]), (ba[0:1, 1, :], I['gla_ba_b'][l:l + 1, :]),
           (normw[:, :], I['gla_norm'][l, :].partition_broadcast(128))])
    gr = v
    step = 0
    for hp in range(2):
        p.dma('sp', 'g_q', [], ['g_qT'], [(qT[:, :], S['QT'][hp * 128:(hp + 1) * 128, :])])
        p.dma('sp', 'g_k', [], ['g_kT'], [(kT[:, :], S['KT'][hp * 128:(hp + 1) * 128, :])])
        p.dma('sp', 'g_v', [], ['g_v'],
              [(v[:, :, :], S['V'][:, hp * 256:(hp + 1) * 256].rearrange("(n p) c -> p n c", p=128))])
        for dirn in range(2):
            p.op('dve', [], ['g_S'], lambda E: E.memset(St[:, :], 0.0))
            p.op('dve', [], ['g_Sb'], lambda E: E.memset(Sb[:, :], 0.0))
            order = list(range(NCH)) if dirn == 0 else [1, 0] + list(range(NCH - 1, 1, -1))
            last = 127 if dirn == 0 else 0
            gsrc = S['AFT'] if dirn == 0 else S['ABT']
            for n in order[:c.cfg.get('gsteps', 999)]:
                t0 = n * 128
                s2 = step % 2
                s3 = step % 3
                step += 1
                a_t = aft[s3]
                p.dma('sp', 'g_aft%d' % s3, [], [('g_aft', s3)], [(a_t[:, :], gsrc[:, t0:t0 + 128])])

                def mmg(E, a_t=a_t, dirn=dirn, hp=hp):
                    E.matmul(psG[:, :], lhsT=a_t[0:16, :], rhs=wa2[0:16, dirn, hp * 128:(hp + 1) * 128],
                             start=True, stop=False)
                    return E.matmul(psG[:, :], lhsT=ones[0:1, :], rhs=ba[0:1, dirn, hp * 128:(hp + 1) * 128],
                                    start=False, stop=True)
                p.op('pe', [('g_aft', s3), 'g_w', 'cst'], [('bank', 0)], mmg)
                if c.cfg.get('gsub', 99) < 1:
                    continue
                p.op('act', [('bank', 0)], [('g_g1', s2)],
                     lambda E, s2=s2: E.activation(out=g1[s2][:, :], in_=psG[:, :], func=AF.Exp, scale=-1.0))
                p.op('act', [('g_g1', s2)], [('g_g2', s2)],
                     lambda E, s2=s2: E.activation(out=g2[s2][:, :], in_=g1[s2][:, :], func=AF.Ln, bias=1.0, scale=1.0))
                if c.cfg.get('gsub', 99) < 2:
                    continue
                pB = psBt[s2]
                p.op('pe', [('g_g2', s2), 'cst'], [('bank', 1 + s2)],
                     lambda E, s2=s2, pB=pB, dirn=dirn: E.matmul(pB[:, :], lhsT=g2[s2][:, :], rhs=U16[dirn],
                                                                 start=True, stop=True))
                p.op('act', [('bank', 1 + s2)], [('g_e1', s2)],
                     lambda E, s2=s2, pB=pB: E.activation(out=e1[s2][:, :], in_=pB[:, :], func=AF.Exp, scale=-1.0))
                p.op('act', [('bank', 1 + s2)], [('g_e2', s2)],
                     lambda E, s2=s2, pB=pB: E.activation(out=e2[s2][:, :], in_=pB[:, :], func=AF.Exp, scale=1.0))
                if c.cfg.get('gsub', 99) < 3:
                    continue
                p.op('dve', ['g_qT', ('g_e1', s2)], [('g_qb', s2)],
                     lambda E, s2=s2, t0=t0: E.scalar_tensor_tensor(out=qb[s2][:, :], in0=qT[:, t0:t0 + 128], scalar=0.125,
                                                                    in1=e1[s2][:, :], op0=ALU.mult, op1=ALU.mult))
                p.op('dve', ['g_kT', ('g_e2', s2)], [('g_kb', s2)],
                     lambda E, s2=s2, t0=t0: E.tensor_tensor(out=kb[s2][:, :], in0=kT[:, t0:t0 + 128], in1=e2[s2][:, :],
                                                             op=ALU.mult))
                p.op('dve', [('g_kb', s2), ('g_e1', s2)], [('g_kd', s2)],
                     lambda E, s2=s2, last=last: E.tensor_scalar(out=kd[s2][:, :], in0=kb[s2][:, :],
                                                                 scalar1=e1[s2][:, last:last + 1], scalar2=None,
                                                                 op0=ALU.mult))

                if c.cfg.get('gsub', 99) < 4:
                    continue
                def mma(E, s2=s2):
                    ins = None
                    for h in range(2):
                        if h == 1:
                            p.pe_fence(ins)
                        ins = E.matmul(psA[:, h * 128:(h + 1) * 128], lhsT=kb[s2][h * 64:(h + 1) * 64, :],
                                       rhs=qb[s2][h * 64:(h + 1) * 64, :], start=True, stop=True)
                    return ins
                p.op('pe', [('g_kb', s2), ('g_qb', s2)], [('bank', 3)], mma)
                if c.cfg.get('gsub', 99) < 5:
                    continue
                msk = MSK[dirn]
                p.op('dve', [('bank', 3), 'cst'], [('g_am', s2)],
                     lambda E, s2=s2, msk=msk: E.tensor_tensor(
                         out=am[s2][:, :].rearrange("p (h i) -> p h i", h=2),
                         in0=psA[:, :].rearrange("p (h i) -> p h i", h=2),
                         in1=msk.unsqueeze(1).to_broadcast([128, 2, 128]), op=ALU.mult))
                if c.cfg.get('gsub', 99) < 6:
                    continue
                p.op('pe', [('g_kd', s2), 'ident'], [('bank', 4)],
                     lambda E, s2=s2: E.transpose(psK[:, :], kd[s2][:, :], c.ident[:, :]))
                p.op('act', [('bank', 4)], [('g_kdt', s2)],
                     lambda E, s2=s2: E.activation(out=kdt[s2][:, :], in_=psK[:, :], func=AF.Copy))
                if c.cfg.get('gsub', 99) < 7:
                    continue
                pO = psO[s2]

                def mmo(E, s2=s2, pO=pO, n=n):
                    ins = None
                    for h in range(2):
                        if h == 1:
                            p.pe_fence(ins)
                        E.matmul(pO[:, h * 128:(h + 1) * 128], lhsT=am[s2][:, h * 128:(h + 1) * 128],
                                 rhs=v[:, n, h * 128:(h + 1) * 128], start=True, stop=False)
                        ins = E.matmul(pO[:, h * 128:(h + 1) * 128], lhsT=qb[s2][h * 64:(h + 1) * 64, :],
                                       rhs=Sb[h * 64:(h + 1) * 64, :], start=False, stop=True)
                    return ins
                p.op('pe', [('g_am', s2), 'g_v', ('g_qb', s2), 'g_Sb'], [('bank', 5 + s2)], mmo)
                if dirn == 0:
                    p.op('act', [('bank', 5 + s2)], [('g_oacc', n)],
                         lambda E, pO=pO, n=n: E.activation(out=oacc[:, n, :], in_=pO[:, :], func=AF.Copy))
                else:
                    p.op('dve', [('bank', 5 + s2), ('g_oacc', n)], [('g_oacc', n)],
                         lambda E, pO=pO, n=n: E.tensor_tensor(out=oacc[:, n, :], in0=pO[:, :], in1=oacc[:, n, :],
                                                               op=ALU.add))

                if c.cfg.get('gsub', 99) < 8:
                    continue
                def mms(E, s2=s2, n=n):
                    ins = None
                    for h in range(2):
                        if h == 1:
                            p.pe_fence(ins)
                        ins = E.matmul(psS[h * 64:(h + 1) * 64, :], lhsT=kdt[s2][:, h * 64:(h + 1) * 64],
                                       rhs=v[:, n, h * 128:(h + 1) * 128], start=True, stop=True)
                    return ins
                p.op('pe', [('g_kdt', s2), 'g_v'], [('bank', 7)], mms)
                if c.cfg.get('gsub', 99) < 9:
                    continue
                p.op('dve', [('bank', 7), ('g_e1', s2), 'g_S'], ['g_S'],
                     lambda E, s2=s2, last=last: E.scalar_tensor_tensor(out=St[:, :], in0=St[:, :],
                                                                        scalar=e1[s2][:, last:last + 1], in1=psS[:, :],
                                                                        op0=ALU.mult, op1=ALU.add))
                p.op('act', ['g_S'], ['g_Sb'], lambda E: E.activation(out=Sb[:, :], in_=St[:, :], func=AF.Copy))
        if c.cfg.get('gnofin'):
            continue
        okeys = [('g_oacc', n) for n in range(NCH)]
        for n in range(NCH):
            for h in range(2):
                p.op('act', [('g_oacc', n)], ['g_junk', ('g_rst', n, h)],
                     lambda E, n=n, h=h: E.activation(out=junk[:, :], in_=oacc[:, n, h * 128:(h + 1) * 128],
                                                      func=AF.Square, accum_out=rst[:, n * 2 + h:n * 2 + h + 1]))
        rkeys = [('g_rst', n, h) for n in range(NCH) for h in range(2)]
        p.op('act', rkeys, ['g_rst2'],
             lambda E: E.activation(out=rst2[:, :], in_=rst[:, :], func=AF.Ln, scale=1.0 / 128, bias=RMS_EPS))
        p.op('act', ['g_rst2'], ['g_rst2'],
             lambda E: E.activation(out=rst2[:, :], in_=rst2[:, :], func=AF.Exp, scale=-0.5))
        p.dma('sp', 'g_v', [], ['g_v'],
              [(gr[:, :, :], S['GR'][:, hp * 256:(hp + 1) * 256].rearrange("(n p) c -> p n c", p=128))])
        p.op('act', ['g_v'], ['g_v'], lambda E: E.activation(out=gr[:, :, :], in_=gr[:, :, :], func=AF.Silu))
        o4 = oacc[:, :, :].rearrange("p n (h d) -> p (n h) d", h=2)
        p.op('dve', okeys + ['g_rst2'], okeys,
             lambda E: E.tensor_tensor(out=o4, in0=o4, in1=rst2[:, :].unsqueeze(2).to_broadcast([128, NCH * 2, 128]),
                                       op=ALU.mult))
        p.op('dve', okeys + ['g_w'], okeys,
             lambda E: E.tensor_tensor(out=o4, in0=o4, in1=normw[:, :].unsqueeze(1).to_broadcast([128, NCH * 2, 128]),
                                       op=ALU.mult))
        p.op('dve', okeys + ['g_v'], ['g_v'],
             lambda E: E.tensor_tensor(out=gr[:, :, :], in0=oacc[:, :, :], in1=gr[:, :, :], op=ALU.mult))
        groups = [(0, 2)] + [(2 + 4 * i, 4) for i in range(16)]
        for gi, (n0, cnt) in enumerate(groups):
            for h in range(2):
                tb = (gi * 2 + h) % 2
                tpt = c.tp[tb]

                def tr(E, n0=n0, cnt=cnt, h=h, tpt=tpt):
                    ins = None
                    for j in range(cnt):
                        ins = E.transpose(tpt[:, j, :], gr[:, n0 + j, h * 128:(h + 1) * 128], c.ident[:, :])
                    return ins
                p.op('pe', ['g_v', 'ident'], [('bank', 1 + tb)], tr)
                ot = osb[tb]
                T = cnt * 128
                if tb == 0:
                    p.op('act', [('bank', 1 + tb)], [('g_osb', tb)],
                         lambda E, ot=ot, tpt=tpt, T=T: E.activation(out=ot[:, 0:T], in_=tpt[:, :, :].rearrange("p a b -> p (a b)")[:, 0:T], func=AF.Copy))
                else:
                    p.op('dve', [('bank', 1 + tb)], [('g_osb', tb)],
                         lambda E, ot=ot, tpt=tpt, T=T: E.tensor_copy(out=ot[:, 0:T], in_=tpt[:, :, :].rearrange("p a b -> p (a b)")[:, 0:T]))
                row0 = (hp * 2 + h) * 128
                p.dma('sp', 'g_osb%d' % tb, [('g_osb', tb)], [('OGT', l)],
                      [(S['OGT'][row0:row0 + 128, n0 * 128:n0 * 128 + T], ot[:, 0:T])])


MLA_SCALE = 96 ** -0.5


def stage_mla(c, l, ctx_q):
    with ExitStack() as es:
        c.es = es
        _stage_mla(c, l, ctx_q)
        c.p.barrier()


def _rms_rstd(c, psq, rs, nfeat, key_ps, key_rs):
    p = c.p
    p.op('act', [key_ps], [key_rs],
         lambda E: E.activation(out=rs, in_=psq, func=AF.Ln, scale=1.0 / nfeat, bias=RMS_EPS))
    p.op('act', [key_rs], [key_rs], lambda E: E.activation(out=rs, in_=rs, func=AF.Exp, scale=-0.5))


def _stage_mla(c, l, ctx_q):
    p, nc = c.p, c.nc
    I = c.inp
    S = c.scr[l]
    KpT, VpD, QpT = c.KpT, c.VpD, c.QpT
    cst = c.cst
    ones32 = cst[:, 640:768]
    wkv32 = sb(c, 'm_wkv32', [128, 2, 1024], F32)
    wq32 = sb(c, 'm_wq32', [128, 3, 768], F32)
    wkv = sb(c, 'm_wkv', [128, 2, 1024], BF16)
    wq = sb(c, 'm_wq', [128, 3, 768], BF16)
    gn = sb(c, 'm_gn', [128, 5], F32)
    onesb = sb(c, 'm_onesb', [128, 8], BF16)
    p.dma('sp', 'm_w', [], ['m_w32'],
          [(wkv32[:, :, :], I['mla_w_ukv'][l].rearrange("(k p) n -> p k n", p=128)),
           (wq32[:, :, :], I['mla_w_uq'][l].rearrange("(k p) n -> p k n", p=128))])
    p.dma('sp', 'm_g', [], ['m_gn'],
          [(gn[:, 0:2], I['mla_kv_norm'][l, :].rearrange("(k p) -> p k", p=128)),
           (gn[:, 2:5], I['mla_q_norm'][l, :].rearrange("(k p) -> p k", p=128))], slow=True)
    p.op('dve', [], ['m_onesb'], lambda E: E.memset(onesb[:, :], 1.0))
    for k in range(2):
        p.op('dve', ['m_w32', 'm_gn'], [('m_wkv', k)],
             lambda E, k=k: E.tensor_scalar(out=wkv[:, k, :], in0=wkv32[:, k, :], scalar1=gn[:, k:k + 1], scalar2=None,
                                            op0=ALU.mult))
    for k in range(3):
        p.op('dve', ['m_w32', 'm_gn'], [('m_wq', k)],
             lambda E, k=k: E.tensor_scalar(out=wq[:, k, :], in0=wq32[:, k, :], scalar1=gn[:, 2 + k:3 + k], scalar2=None,
                                            op0=ALU.mult))
    wkvk = [('m_wkv', k) for k in range(2)]
    wqk = [('m_wq', k) for k in range(3)]
    src = [sb(c, 'm_src%d' % i, [128, 3, 512], BF16) for i in range(2)]
    sq = [sb(c, 'm_sq%d' % i, [128, 3, 512], BF16) for i in range(2)]
    rs = [sb(c, 'm_rs%d' % i, [128, 2], F32) for i in range(2)]
    kr = [sb(c, 'm_kr%d' % i, [128, 32], F32) for i in range(2)]
    krr = [sb(c, 'm_krr%d' % i, [128, 32], F32) for i in range(2)]
    rtab = [sb(c, 'm_rtab%d' % i, [128, 32], F32) for i in range(2)]
    tmp16 = [sb(c, 'm_tmp%d' % i, [128, 16], F32) for i in range(2)]
    kp = [sb(c, 'm_kp%d' % i, [128, 8, 97], BF16) for i in range(2)]
    vp = [sb(c, 'm_vp%d' % i, [128, 8, 65], BF16) for i in range(2)]
    kpt = [sb(c, 'm_kpt%d' % i, [97, 8, 128], BF16) for i in range(2)]
    qf = [sb(c, 'm_qf%d' % i, [128, 8, 96], F32) for i in range(2)]
    qsq = sb(c, 'm_qsq', [128, 8, 96], F32)
    ks = sb(c, 'm_ks', [128, 8], F32)
    kmx = sb(c, 'm_kmx', [128, 8], F32)
    kmT = sb(c, 'm_kmT', [8, 1], F32)
    kdiag = sb(c, 'm_kdiag', [8, 8], F32)
    kbc = sb(c, 'm_kbc', [128, 8], F32)
    qn = [sb(c, 'm_qn%d' % i, [128, 8], F32) for i in range(2)]
    B = c.bank
    bk = lambda i: ('bank', i)
    for i in range(2):
        p.op('dve', [], [('m_kp', i)], lambda E, i=i: E.memset(kp[i][:, :, :], 1.0))
        p.op('dve', [], [('m_vp', i)], lambda E, i=i: E.memset(vp[i][:, :, :], 1.0))
    p.op('dve', [], ['m_kmx'], lambda E: E.memset(kmx[:, :], 0.0))

    blocks = [(0, 2)] + [(2 + 4 * i, 4) for i in range(16)]
    it = 0

    def load_src(name, nk, t0, ntl, sslot):
        T = ntl * 128
        p.dma('sp', 'm_src%d' % sslot, [], [('m_src', sslot)],
              [(src[sslot][:, 0:nk, 0:T], S[name][:, t0 * 128:t0 * 128 + T].rearrange("(k p) t -> p k t", p=128))])
        p.op('act', [('m_src', sslot)], [('m_sq', sslot)],
             lambda E: E.activation(out=sq[sslot][:, 0:nk, 0:T], in_=src[sslot][:, 0:nk, 0:T], func=AF.Square))

    def rope_rows(t, s2):
        p.dma('sp', 'm_rtab%d' % s2, [], [('m_rtab', s2)], [(rtab[s2][:, :], I['rope'][t * 128:(t + 1) * 128, :])])

    for bi, (tb0, ntl) in enumerate(blocks):
        sslot = bi % 2
        load_src('MKVAT', 2, tb0, ntl, sslot)
        for j in range(ntl):
            t = tb0 + j
            s2 = it % 2
            it += 1
            cols = slice(j * 128, (j + 1) * 128)

            def mmq(E, sslot=sslot, cols=cols):
                ins = None
                for k in range(2):
                    ins = E.matmul(B[0][:, 0:1], lhsT=sq[sslot][:, k, cols], rhs=onesb[:, 0:1], start=(k == 0), stop=(k == 1))
                return ins
            p.op('pe', [('m_sq', sslot), 'm_onesb'], [bk(0)], mmq)
            _rms_rstd(c, B[0][:, 0:1], rs[s2][:, 0:1], 256, bk(0), ('m_rs', s2))
            for half in range(2):
                def mmkv(E, sslot=sslot, cols=cols, half=half):
                    ins = None
                    for k in range(2):
                        ins = E.matmul(B[1 + half][:, :], lhsT=src[sslot][:, k, cols],
                                       rhs=wkv[:, k, half * 512:(half + 1) * 512], start=(k == 0), stop=(k == 1))
                    return ins
                p.op('pe', [('m_src', sslot)] + wkvk, [bk(1 + half)], mmkv)
            for half in range(2):
                pv = B[1 + half][:, :].rearrange("p (h e) -> p h e", h=4)
                p.op('dve', [bk(1 + half), ('m_rs', s2)], [('m_kp', s2)],
                     lambda E, pv=pv, half=half, s2=s2: E.tensor_scalar(out=kp[s2][:, half * 4:(half + 1) * 4, 0:64], in0=pv[:, :, 0:64],
                                                                        scalar1=rs[s2][:, 0:1], scalar2=None, op0=ALU.mult))
                p.op('act', [bk(1 + half), ('m_rs', s2)], [('m_vp', s2)],
                     lambda E, pv=pv, half=half, s2=s2: E.activation(out=vp[s2][:, half * 4:(half + 1) * 4, 0:64], in_=pv[:, :, 64:128],
                                                                     func=AF.Copy, scale=rs[s2][:, 0:1]))
            p.dma('sp', 'm_kr%d' % s2, [], [('m_kr', s2)], [(kr[s2][:, :], S['MKR'][t * 128:(t + 1) * 128, :])])
            if t >= 2:
                rope_rows(t - 2, s2)
                x1, x2 = kr[s2][:, 0:16], kr[s2][:, 16:32]
                cs, sn = rtab[s2][:, 0:16], rtab[s2][:, 16:32]
                o1, o2 = krr[s2][:, 0:16], krr[s2][:, 16:32]
                tm = tmp16[s2]
                rk = [('m_kr', s2), ('m_rtab', s2)]
                p.op('dve', rk, [('m_krr', s2)], lambda E, o1=o1, x1=x1, cs=cs: E.tensor_tensor(out=o1, in0=x1, in1=cs, op=ALU.mult))
                p.op('dve', rk, [('m_tmp', s2)], lambda E, tm=tm, x2=x2, sn=sn: E.tensor_tensor(out=tm[:, :], in0=x2, in1=sn, op=ALU.mult))
                p.op('dve', [('m_krr', s2), ('m_tmp', s2)], [('m_krr', s2)],
                     lambda E, o1=o1, tm=tm: E.tensor_tensor(out=o1, in0=o1, in1=tm[:, :], op=ALU.subtract))
                p.op('dve', rk + [('m_krr', s2)], [('m_krr', s2)], lambda E, o2=o2, x2=x2, cs=cs: E.tensor_tensor(out=o2, in0=x2, in1=cs, op=ALU.mult))
                p.op('dve', rk + [('m_tmp', s2)], [('m_tmp', s2)], lambda E, tm=tm, x1=x1, sn=sn: E.tensor_tensor(out=tm[:, :], in0=x1, in1=sn, op=ALU.mult))
                p.op('dve', [('m_krr', s2), ('m_tmp', s2)], [('m_krr', s2)],
                     lambda E, o2=o2, tm=tm: E.tensor_tensor(out=o2, in0=o2, in1=tm[:, :], op=ALU.add))
                rsrc, rkey = krr[s2], ('m_krr', s2)
            else:
                rsrc, rkey = kr[s2], ('m_kr', s2)
            p.op('dve', [rkey, ('m_kp', s2)], [('m_kp', s2)],
                 lambda E, rsrc=rsrc, s2=s2: E.tensor_copy(out=kp[s2][:, :, 64:96],
                                                           in_=rsrc[:, :].unsqueeze(1).to_broadcast([128, 8, 32])))
            p.op('dve', [('m_kp', s2)], ['m_qsq'],
                 lambda E, s2=s2: E.tensor_tensor(out=qsq[:, :, :], in0=kp[s2][:, :, 0:96], in1=kp[s2][:, :, 0:96], op=ALU.mult))
            p.op('dve', ['m_qsq'], ['m_ks'], lambda E: E.tensor_reduce(out=ks[:, :], in_=qsq[:, :, :], axis=AX.X, op=ALU.add))
            p.op('dve', ['m_ks', 'm_kmx'], ['m_kmx'], lambda E: E.tensor_tensor(out=kmx[:, :], in0=kmx[:, :], in1=ks[:, :], op=ALU.max))
            tpb = 3 + s2
            tpv = B[tpb][:, :].bitcast(BF16).rearrange("p (h t) -> p h t", h=8)

            def trk(E, s2=s2, tpv=tpv):
                ins = None
                for h in range(8):
                    ins = E.transpose(tpv[0:97, h, :], kp[s2][:, h, :], c.ident[:, :])
                return ins
            p.op('pe', [('m_kp', s2), 'ident'], [bk(tpb)], trk)
            p.op('act', [bk(tpb)], [('m_kpt', s2)],
                 lambda E, s2=s2, tpv=tpv: E.activation(out=kpt[s2][:, :, :], in_=tpv[0:97, :, :], func=AF.Copy))
            p.dma('sp', 'm_kpt%d' % s2, [('m_kpt', s2)], ['KpT'],
                  [(KpT[:, :, t * 128:(t + 1) * 128].rearrange("h d t -> d h t"), kpt[s2][:, :, :])])
            p.dma('sp', 'm_vp%d' % s2, [('m_vp', s2)], ['VpD'],
                  [(VpD[:, t * 128:(t + 1) * 128, :].rearrange("h p e -> p h e"), vp[s2][:, :, :])])
    p.op('pe', ['m_kmx', 'cst'], [bk(0)], lambda E: E.transpose(B[0][0:8, 0:128], kmx[:, :], cst[:, 0:128]))
    p.op('dve', [bk(0)], ['m_kmT'], lambda E: E.tensor_reduce(out=kmT[:, :], in_=B[0][0:8, 0:128], axis=AX.X, op=ALU.max))
    p.op('dve', ['m_kmT', 'cst'], ['m_kdiag'],
         lambda E: E.tensor_scalar(out=kdiag[:, :], in0=cst[0:8, 0:8], scalar1=kmT[:, 0:1], scalar2=None, op0=ALU.mult))
    p.op('pe', ['m_kdiag', 'cst'], [bk(0)],
         lambda E: E.matmul(B[0][:, 0:8], lhsT=ones32[0:8, :], rhs=kdiag[:, :], start=True, stop=True))
    p.op('act', [bk(0)], ['m_kbc'], lambda E: E.activation(out=kbc[:, :], in_=B[0][:, 0:8], func=AF.Copy))
    qblocks = ([(0, 2)] if ctx_q else []) + [(2 + 4 * i, 4) for i in range(16)]
    for bi, (tb0, ntl) in enumerate(qblocks):
        sslot = bi % 2
        load_src('MQAT', 3, tb0, ntl, sslot)
        for j in range(ntl):
            t = tb0 + j
            s2 = it % 2
            it += 1
            cols = slice(j * 128, (j + 1) * 128)

            def mmq(E, sslot=sslot, cols=cols):
                ins = None
                for k in range(3):
                    ins = E.matmul(B[0][:, 0:1], lhsT=sq[sslot][:, k, cols], rhs=onesb[:, 0:1], start=(k == 0), stop=(k == 2))
                return ins
            p.op('pe', [('m_sq', sslot), 'm_onesb'], [bk(0)], mmq)
            _rms_rstd(c, B[0][:, 0:1], rs[s2][:, 0:1], 384, bk(0), ('m_rs', s2))
            p.op('dve', [('m_rs', s2)], [('m_rs', s2)],
                 lambda E, s2=s2: E.tensor_scalar(out=rs[s2][:, 0:1], in0=rs[s2][:, 0:1], scalar1=MLA_SCALE, scalar2=None, op0=ALU.mult))
            for half, (n0, nn) in enumerate([(0, 512), (512, 256)]):
                def mmqq(E, sslot=sslot, cols=cols, half=half, n0=n0, nn=nn):
                    ins = None
                    for k in range(3):
                        ins = E.matmul(B[1 + half][:, 0:nn], lhsT=src[sslot][:, k, cols], rhs=wq[:, k, n0:n0 + nn],
                                       start=(k == 0), stop=(k == 2))
                    return ins
                p.op('pe', [('m_src', sslot)] + wqk, [bk(1 + half)], mmqq)
            qv = qf[s2][:, :, :].rearrange("p h e -> p (h e)")
            p.op('dve', [bk(1), ('m_rs', s2)], [('m_qf', s2, 0)],
                 lambda E, qv=qv, s2=s2: E.tensor_scalar(out=qv[:, 0:512], in0=B[1][:, 0:512], scalar1=rs[s2][:, 0:1], scalar2=None, op0=ALU.mult))
            p.op('act', [bk(2), ('m_rs', s2)], [('m_qf', s2, 1)],
                 lambda E, qv=qv, s2=s2: E.activation(out=qv[:, 512:768], in_=B[2][:, 0:256], func=AF.Copy, scale=rs[s2][:, 0:1]))
            qk = [('m_qf', s2, 0), ('m_qf', s2, 1)]
            kpq = kp[s2]
            if t >= 2:
                rope_rows(t - 2, s2)
                x1, x2 = qf[s2][:, :, 64:80], qf[s2][:, :, 80:96]
                cs = rtab[s2][:, 0:16].unsqueeze(1).to_broadcast([128, 8, 16])
                sn = rtab[s2][:, 16:32].unsqueeze(1).to_broadcast([128, 8, 16])
                ta, tb_ = qsq[:, :, 0:16], qsq[:, :, 16:32]
                tc_, td = qsq[:, :, 32:48], qsq[:, :, 48:64]
                rk = qk + [('m_rtab', s2)]
                p.op('dve', rk, ['m_qsq'], lambda E, ta=ta, x1=x1, cs=cs: E.tensor_tensor(out=ta, in0=x1, in1=cs, op=ALU.mult))
                p.op('dve', rk + ['m_qsq'], ['m_qsq'], lambda E, tb_=tb_, x2=x2, sn=sn: E.tensor_tensor(out=tb_, in0=x2, in1=sn, op=ALU.mult))
                p.op('dve', rk + ['m_qsq'], ['m_qsq'], lambda E, tc_=tc_, x2=x2, cs=cs: E.tensor_tensor(out=tc_, in0=x2, in1=cs, op=ALU.mult))
                p.op('dve', rk + ['m_qsq'], ['m_qsq'], lambda E, td=td, x1=x1, sn=sn: E.tensor_tensor(out=td, in0=x1, in1=sn, op=ALU.mult))
                p.op('dve', ['m_qsq'] + qk, qk, lambda E, x1=x1, ta=ta, tb_=tb_: E.tensor_tensor(out=x1, in0=ta, in1=tb_, op=ALU.subtract))
                p.op('dve', ['m_qsq'] + qk, qk, lambda E, x2=x2, tc_=tc_, td=td: E.tensor_tensor(out=x2, in0=tc_, in1=td, op=ALU.add))
            p.op('dve', qk, ['m_qsq'],
                 lambda E, s2=s2: E.tensor_tensor(out=qsq[:, :, :], in0=qf[s2][:, :, :], in1=qf[s2][:, :, :], op=ALU.mult))
            p.op('dve', ['m_qsq'], [('m_qn', s2)], lambda E, s2=s2: E.tensor_reduce(out=qn[s2][:, :], in_=qsq[:, :, :], axis=AX.X, op=ALU.add))
            p.op('dve', [('m_qn', s2), 'm_kbc'], [('m_qn', s2)],
                 lambda E, s2=s2: E.tensor_tensor(out=qn[s2][:, :], in0=qn[s2][:, :], in1=kbc[:, :], op=ALU.mult))
            p.op('act', [('m_qn', s2)], [('m_qn', s2)], lambda E, s2=s2: E.activation(out=qn[s2][:, :], in_=qn[s2][:, :], func=AF.Sqrt))
            p.op('dve', qk + [('m_kp', s2)], [('m_kp', s2)],
                 lambda E, s2=s2: E.tensor_copy(out=kpq[:, :, 0:96], in_=qf[s2][:, :, :]))
            p.op('dve', [('m_qn', s2), ('m_kp', s2)], [('m_kp', s2)],
                 lambda E, s2=s2: E.tensor_scalar(out=kpq[:, :, 96:97], in0=qn[s2][:, :].unsqueeze(2), scalar1=-1.0, scalar2=None, op0=ALU.mult))
            tpb = 3 + s2
            tpv = B[tpb][:, :].bitcast(BF16).rearrange("p (h t) -> p h t", h=8)

            def trq(E, s2=s2, tpv=tpv):
                ins = None
                for h in range(8):
                    ins = E.transpose(tpv[0:97, h, :], kp[s2][:, h, :], c.ident[:, :])
                return ins
            p.op('pe', [('m_kp', s2), 'ident'], [bk(tpb)], trq)
            p.op('act', [bk(tpb)], [('m_kpt', s2)],
                 lambda E, s2=s2, tpv=tpv: E.activation(out=kpt[s2][:, :, :], in_=tpv[0:97, :, :], func=AF.Copy))
            p.dma('sp', 'm_kpt%d' % s2, [('m_kpt', s2)], ['QpT'],
                  [(QpT[:, :, t * 128:(t + 1) * 128].rearrange("h d t -> d h t"), kpt[s2][:, :, :])])
    p.barrier()
    kh = [sb(c, 'm_kh%d' % i, [97, NT], BF16) for i in range(2)]
    qh = [sb(c, 'm_qh%d' % i, [97, NT], BF16) for i in range(2)]
    vh = [sb(c, 'm_vh%d' % i, [128, NTILE, 65], BF16) for i in range(2)]
    pt = [sb(c, 'm_pt%d' % i, [128, 512], BF16) for i in range(3)]
    osb = [sb(c, 'm_osb%d' % i, [65, 512], F32) for i in range(2)]
    on = [sb(c, 'm_on%d' % i, [64, 512], BF16) for i in range(2)]
    ei = 0
    ci = 0
    for h in range(8):
        hs = h % 2
        p.dma('sp', 'm_kh%d' % hs, ['KpT'], [('m_kh', hs)], [(kh[hs][:, :], KpT[h, :, :])])
        p.dma('sp', 'm_qh%d' % hs, ['QpT'], [('m_qh', hs)], [(qh[hs][:, :], QpT[h, :, :])])
        p.dma('sp', 'm_vh%d' % hs, ['VpD'], [('m_vh', hs)],
              [(vh[hs][:, :, :], VpD[h, :, :].rearrange("(n p) e -> p n e", p=128))])
        chunks = ([(0, 256, 2)] if ctx_q else []) + [(256 + 512 * i, 512, NTILE) for i in range(16)]
        for (q0, nq, nkt) in chunks:
            ob = 6 + ci % 2
            cs2 = ci % 2
            ci += 1
            def emit_qk(kt):
                sbk = (ei0 + kt) % 3
                p.op('pe', [('m_kh', hs), ('m_qh', hs)], [bk(sbk)],
                     lambda E: E.matmul(B[sbk][:, 0:nq], lhsT=kh[hs][:, kt * 128:(kt + 1) * 128],
                                        rhs=qh[hs][:, q0:q0 + nq], start=True, stop=True))
            ei0 = ei
            ei += nkt
            for kt in range(min(2, nkt)):
                emit_qk(kt)
            for kt in range(nkt):
                sbk = (ei0 + kt) % 3
                p.op('act', [bk(sbk)], [('m_pt', sbk)],
                     lambda E: E.activation(out=pt[sbk][:, 0:nq], in_=B[sbk][:, 0:nq], func=AF.Exp))
                if kt + 2 < nkt:
                    emit_qk(kt + 2)
                p.op('pe', [('m_vh', hs), ('m_pt', sbk)], [bk(ob)],
                     lambda E: E.matmul(B[ob][0:65, 0:nq], lhsT=vh[hs][:, kt, :], rhs=pt[sbk][:, 0:nq],
                                        start=(kt == 0), stop=(kt == nkt - 1)))
            o_t = osb[cs2]
            p.op('dve', [bk(ob)], [('m_osb', cs2)], lambda E, o_t=o_t, ob=ob, nq=nq: E.tensor_copy(out=o_t[:, 0:nq], in_=B[ob][0:65, 0:nq]))
            p.op('dve', [('m_osb', cs2)], [('m_osb', cs2)],
                 lambda E, o_t=o_t, nq=nq: E.reciprocal(out=o_t[64:65, 0:nq], in_=o_t[64:65, 0:nq]))
            p.op('pe', [('m_osb', cs2), 'cst'], [bk(5)],
                 lambda E, o_t=o_t, nq=nq: E.matmul(B[5][0:64, 0:nq], lhsT=ones32[64:65, 0:64], rhs=o_t[64:65, 0:nq], start=True, stop=True))
            p.op('dve', [bk(5), ('m_osb', cs2)], [('m_on', cs2)],
                 lambda E, o_t=o_t, nq=nq, cs2=cs2: E.tensor_tensor(out=on[cs2][:, 0:nq], in0=o_t[0:64, 0:nq], in1=B[5][0:64, 0:nq], op=ALU.mult))
            p.dma('sp', 'm_on%d' % cs2, [('m_on', cs2)], [('OMT', l)],
                  [(S['OMT'][h * 64:(h + 1) * 64, q0:q0 + nq], on[cs2][:, 0:nq])])


NFFT = 16384


def hy_conv3(c, l):
    p = c.p
    I = c.inp
    S = c.scr[l]
    with ExitStack() as es:
        c.es = es
        wb32 = sb(c, 'h3_w32', [64, 4, 1536], F32)
        wb = sb(c, 'h3_w', [64, 4, 1536], BF16)
        zin = [sb(c, 'h3_zin%d' % i, [64, 10, 512], BF16) for i in range(2)]
        t0_ = [sb(c, 'h3_t0%d' % i, [64, 8, 512], BF16) for i in range(2)]
        t1_ = [sb(c, 'h3_t1%d' % i, [64, 8, 512], BF16) for i in range(2)]
        zo = [sb(c, 'h3_zo%d' % i, [64, 8, 512], BF16) for i in range(2)]
        p.dma('sp', 'h3_w', [], ['h3_w32'],
              [(wb32[:, k, :], I['hy_conv_w'][l, k, :].partition_broadcast(64)) for k in range(3)] +
              [(wb32[:, 3, :], I['hy_conv_b'][l, :].partition_broadcast(64))])
        p.op('dve', ['h3_w32'], ['h3_w'], lambda E: E.tensor_copy(out=wb[:, :, :], in_=wb32[:, :, :]))
        it = 0
        for (tok0, na) in ((0, 2), (NCTX, 64)):
            src = S['HY'][tok0:tok0 + na * 128, :].rearrange("(a b) c -> a b c", b=128)
            dst = c.HYC[tok0:tok0 + na * 128, :].rearrange("(a b) c -> a b c", b=128)
            for cs in range(3):
                c0 = cs * 512
                for bc in range(16):
                    b0 = bc * 8
                    s2 = it % 2
                    it += 1
                    z = zin[s2]
                    pairs = []
                    pre = []
                    lo, hi = b0 - 1, b0 + 9
                    if bc == 0:
                        pre.append(lambda E, z=z: E.memset(z[0:1, 0:1, :], 0.0))
                        if na > 1:
                            pairs.append((z[1:na, 0:1, :], src[0:na - 1, 127:128, c0:c0 + 512]))
                        pairs.append((z[0:na, 1:10, :], src[0:na, 0:9, c0:c0 + 512]))
                    elif bc == 15:
                        pre.append(lambda E, z=z, na=na: E.memset(z[0:na, 9:10, :], 0.0))
                        if na > 1:
                            pairs.append((z[0:na - 1, 9:10, :], src[1:na, 0:1, c0:c0 + 512]))
                        pairs.append((z[0:na, 0:9, :], src[0:na, lo:128, c0:c0 + 512]))
                    else:
                        pairs.append((z[0:na, 0:10, :], src[0:na, lo:hi, c0:c0 + 512]))
                    for f in pre:
                        p.op('pool', [], [('h3_zin', s2)], f)
                    p.dma('sp', 'h3_zin%d' % s2, [('HY', l)], [('h3_zin', s2)], pairs)
                    w = lambda k, c0=c0, na=na: wb[0:na, k, c0:c0 + 512].unsqueeze(1).to_broadcast([na, 8, 512])
                    a0, a1, oz = t0_[s2], t1_[s2], zo[s2]
                    zk = ('h3_zin', s2)
                    p.op('dve', [zk, 'h3_w'], [('h3_t0', s2)],
                         lambda E, z=z, a0=a0, w=w, na=na: E.tensor_tensor(out=a0[0:na], in0=z[0:na, 0:8, :], in1=w(0), op=ALU.mult))
                    p.op('pool', [zk, 'h3_w'], [('h3_t1', s2)],
                         lambda E, z=z, a1=a1, w=w, na=na: E.tensor_tensor(out=a1[0:na], in0=z[0:na, 1:9, :], in1=w(1), op=ALU.mult))
                    p.op('dve', [zk, 'h3_w'], [('h3_zo', s2)],
                         lambda E, z=z, oz=oz, w=w, na=na: E.tensor_tensor(out=oz[0:na], in0=z[0:na, 2:10, :], in1=w(2), op=ALU.mult))
                    p.op('dve', [('h3_t0', s2), 'h3_w'], [('h3_t0', s2)],
                         lambda E, a0=a0, w=w, na=na: E.tensor_tensor(out=a0[0:na], in0=a0[0:na], in1=w(3), op=ALU.add))
                    p.op('dve', [('h3_t0', s2), ('h3_zo', s2)], [('h3_zo', s2)],
                         lambda E, a0=a0, oz=oz, na=na: E.tensor_tensor(out=oz[0:na], in0=a0[0:na], in1=oz[0:na], op=ALU.add))
                    p.op('dve', [('h3_t1', s2), ('h3_zo', s2)], [('h3_zo', s2)],
                         lambda E, a1=a1, oz=oz, na=na: E.tensor_tensor(out=oz[0:na], in0=a1[0:na], in1=oz[0:na], op=ALU.add))
                    p.dma('sp', 'h3_zo%d' % s2, [('h3_zo', s2)], ['HYC'],
                          [(dst[0:na, b0:b0 + 8, c0:c0 + 512], oz[0:na, :, :])])
        p.barrier()


def hy_filters(c, l, job):
    p = c.p
    I = c.inp
    nt = 128 if job == 0 else 4
    feat = I['hy_feat%d' % job]
    tvec = I['hy_tvec%d' % job]
    KTD = c.KTD[job]
    B = c.bank
    bk = lambda i: ('bank', i)
    with ExitStack() as es:
        c.es = es
        w1 = sb(c, 'hf_w1', [33, 64], F32)
        w2 = sb(c, 'hf_w2', [64, 64], F32)
        w3 = sb(c, 'hf_w3', [64, 2048], F32)
        pb = sb(c, 'hf_pb', [64, 8], F32)
        ft = [sb(c, 'hf_ft%d' % i, [33, 512], F32) for i in range(2)]
        tv = [sb(c, 'hf_tv%d' % i, [1, 512], F32) for i in range(2)]
        u = [sb(c, 'hf_u%d' % i, [64, 512], F32) for i in range(2)]
        ui = [sb(c, 'hf_ui%d' % i, [64, 512], mybir.dt.int32) for i in range(2)]
        uf = [sb(c, 'hf_uf%d' % i, [64, 512], F32) for i in range(2)]
        h1 = [sb(c, 'hf_h1%d' % i, [64, 512], F32) for i in range(2)]
        h2 = [sb(c, 'hf_h2%d' % i, [64, 512], F32) for i in range(2)]
        dec = [sb(c, 'hf_dec%d' % i, [128, 512], F32) for i in range(2)]
        hd = [sb(c, 'hf_hd%d' % i, [128, 2, 512], F32) for i in range(2)]
        ha = [sb(c, 'hf_ha%d' % i, [128, 2, 512], F32) for i in range(2)]
        hb = [sb(c, 'hf_hb%d' % i, [128, 2, 512], BF16) for i in range(2)]
        nd = sb(c, 'hf_nd', [1, 512], F32)
        l1 = sb(c, 'hf_l1', [1, 1024], F32)
        cst = c.cst
        ones = cst[:, 640:768]
        p.dma('sp', 'hf_w', [], ['hf_w'],
              [(w1[:, :], I['hy_w1'][l]), (w2[:, :], I['hy_w2'][l]), (w3[:, :], I['hy_w3'][l]),
               (nd[:, :], I['hy_negdelta'][0:1, :])])
        p.dma('sp', 'hf_pb', [], ['hf_pb'],
              [(pb[:, 0:1], I['hy_b1'][l, :].rearrange("(p o) -> p o", o=1)),
               (pb[:, 1:2], I['hy_b2'][l, :].rearrange("(p o) -> p o", o=1)),
               (pb[:, 2:3], I['hy_freq'][l, :].rearrange("(p o) -> p o", o=1))], slow=True)
        p.op('dve', ['hf_pb'], ['hf_pb2'],
             lambda E: E.tensor_scalar(out=pb[:, 3:4], in0=pb[:, 2:3], scalar1=1.0 / (2 * math.pi), scalar2=None, op0=ALU.mult))
        p.op('dve', ['hf_pb', 'hf_pb2'], ['hf_pb3'],
             lambda E: E.tensor_scalar(out=pb[:, 4:6], in0=pb[:, 0:2], scalar1=pb[:, 3:4], scalar2=None, op0=ALU.mult))
        pbk = ['hf_pb', 'hf_pb2', 'hf_pb3']

        def sin_layer(src_ps, srck, bcol, dst, dstk, s2):
            p.op('dve', [srck] + pbk, [('hf_u', s2)],
                 lambda E: E.tensor_scalar(out=u[s2][:, :], in0=src_ps, scalar1=pb[:, 3:4], scalar2=pb[:, bcol:bcol + 1],
                                           op0=ALU.mult, op1=ALU.add))
            p.op('dve', [('hf_u', s2)], [('hf_ui', s2)], lambda E: E.tensor_copy(out=ui[s2][:, :], in_=u[s2][:, :]))
            p.op('dve', [('hf_ui', s2)], [('hf_uf', s2)], lambda E: E.tensor_copy(out=uf[s2][:, :], in_=ui[s2][:, :]))
            p.op('dve', [('hf_u', s2), ('hf_uf', s2)], [('hf_u', s2)],
                 lambda E: E.tensor_tensor(out=u[s2][:, :], in0=u[s2][:, :], in1=uf[s2][:, :], op=ALU.subtract))
            p.op('act', [('hf_u', s2)], [dstk], lambda E: E.activation(out=dst, in_=u[s2][:, :], func=AF.Sin, scale=2 * math.pi))

        nchunk = nt // 4
        first_bwd_tile = 64 if job == 0 else 2
        for ch in range(nchunk):
            s2 = ch % 2
            p.dma('sp', 'hf_ft%d' % s2, [], [('hf_ft', s2)],
                  [(ft[s2][:, :], feat[:, ch * 512:(ch + 1) * 512]), (tv[s2][:, :], tvec[:, ch * 512:(ch + 1) * 512])])
            p.op('pe', [('hf_ft', s2), 'hf_w'], [bk(0)],
                 lambda E, s2=s2: E.matmul(B[0][0:64, :], lhsT=w1[:, :], rhs=ft[s2][:, :], start=True, stop=True))
            sin_layer(B[0][0:64, :], bk(0), 4, h1[s2][:, :], ('hf_h1', s2), s2)
            p.op('pe', [('hf_h1', s2), 'hf_w'], [bk(1)],
                 lambda E, s2=s2: E.matmul(B[1][0:64, :], lhsT=w2[:, :], rhs=h1[s2][:, :], start=True, stop=True))
            sin_layer(B[1][0:64, :], bk(1), 5, h2[s2][:, :], ('hf_h2', s2), s2)
            for j in range(4):
                tile = ch * 4 + j
                d = 0 if tile < first_bwd_tile else 1
                j2 = tile % 2
                cols = slice(j * 128, (j + 1) * 128)
                p.op('pe', [('hf_ft', s2), 'hf_w'], [bk(2)],
                     lambda E, s2=s2, cols=cols: E.matmul(B[2][:, :], lhsT=tv[s2][0:1, cols], rhs=nd[0:1, :], start=True, stop=True))
                p.op('act', [bk(2)], [('hf_dec', j2)], lambda E, j2=j2: E.activation(out=dec[j2][:, :], in_=B[2][:, :], func=AF.Exp))
                for o in range(2):
                    c0 = o * 1024 + d * 512
                    p.op('pe', [('hf_h2', s2), 'hf_w'], [bk(3 + o)],
                         lambda E, s2=s2, cols=cols, c0=c0, o=o: E.matmul(B[3 + o][:, :], lhsT=h2[s2][:, cols], rhs=w3[:, c0:c0 + 512],
                                                                        start=True, stop=True))
                    p.op('dve', [bk(3 + o), ('hf_dec', j2)], [('hf_hd', j2, o)],
                         lambda E, j2=j2, o=o: E.tensor_tensor(out=hd[j2][:, o, :], in0=B[3 + o][:, :], in1=dec[j2][:, :], op=ALU.mult))
                p.op('act', [('hf_hd', j2, 0), ('hf_hd', j2, 1)], [('hf_ha', j2)],
                     lambda E, j2=j2: E.activation(out=ha[j2][:, :, :], in_=hd[j2][:, :, :], func=AF.Abs))
                for o in range(2):
                    p.op('pe', [('hf_ha', j2), 'cst'], [bk(5 + o)],
                         lambda E, j2=j2, o=o, tile=tile: E.matmul(B[5 + o][0:1, :], lhsT=ones[:, 0:1], rhs=ha[j2][:, o, :],
                                                                   start=(tile == 0), stop=(tile == nt - 1)))
                p.op('pool', [('hf_hd', j2, 0), ('hf_hd', j2, 1)], [('hf_hb', j2)],
                     lambda E, j2=j2: E.tensor_copy(out=hb[j2][:, :, :], in_=hd[j2][:, :, :]))
                if tile == first_bwd_tile:
                    p.op('pool', [('hf_hb', j2)], [('hf_hb', j2)], lambda E, j2=j2: E.memset(hb[j2][0:1, :, :], 0.0))
                p.dma('sp', 'hf_hb%d' % j2, [('hf_hb', j2)], [('KTD', job)],
                      [(KTD[tile * 128:(tile + 1) * 128, :].rearrange("p (o c) -> p o c", o=2), hb[j2][:, :, :])])
        for o in range(2):
            p.op('dve', [bk(5 + o)], ['hf_l1'],
                 lambda E, o=o: E.tensor_scalar(out=l1[0:1, o * 512:(o + 1) * 512], in0=B[5 + o][0:1, :], scalar1=float(NFFT if job == 0 else 512), scalar2=None,
                                                op0=ALU.mult))
        p.op('dve', ['hf_l1'], ['hf_l1'], lambda E: E.reciprocal(out=l1[:, :], in_=l1[:, :]))
        p.dma('sp', 'hf_l1', ['hf_l1'], [('SCL', job)], [(c.SCL[job][:, :], l1[:, :])])
        p.barrier()


def hy_fwd1(c, src, K, tab, X1D, NF1):
    p = c.p
    B = c.bank
    bk = lambda i: ('bank', i)
    zt = c.hy_zt
    xo = c.hy_xo
    for bc in range(16):
        s2 = bc % 2
        p.dma('sp', 'hy_zt%d' % s2, ['HYC', 'Z2', ('KTD', 0), ('KTD', 1)], [('hy_zt', s2)], [(zt[s2][0:K, :, :], src(bc * 8, 8))])
        for j in range(8):
            b = bc * 8 + j
            e2 = b % 2
            for ri in range(2):
                p.op('pe', [('hy_zt', s2), 'hy_tab'], [bk(e2 * 2 + ri)],
                     lambda E, s2=s2, j=j, ri=ri, e2=e2: E.matmul(B[e2 * 2 + ri][0:NF1, :], lhsT=tab[0:K, ri, 0:NF1], rhs=zt[s2][0:K, j, :],
                                                                  start=True, stop=True))
            p.op('act', [bk(e2 * 2)], [('hy_xo', e2, 0)],
                 lambda E, e2=e2: E.activation(out=xo[e2][0:NF1, 0, :], in_=B[e2 * 2][0:NF1, :], func=AF.Copy))
            p.op('dve', [bk(e2 * 2 + 1)], [('hy_xo', e2, 1)],
                 lambda E, e2=e2: E.tensor_copy(out=xo[e2][0:NF1, 1, :], in_=B[e2 * 2 + 1][0:NF1, :]))
            p.dma('sp', 'hy_xo%d' % e2, [('hy_xo', e2, 0), ('hy_xo', e2, 1)], ['X1D'],
                  [(X1D[b, 0:NF1, :, :], xo[e2][0:NF1, :, :])])


def hy_stage2(c, X1D, mode, KS, QD, NF1=128, tw2name='hy_tw2'):
    p = c.p
    I = c.inp
    B = c.bank
    bk = lambda i: ('bank', i)
    xin, tw, ksb, pr, t4, qo = c.hy_xin, c.hy_tw, c.hy_ksb, c.hy_pr, c.hy_t4, c.hy_qo
    E3 = c.hy_E3

    def front(f1):
        s2 = f1 % 2
        p.dma('sp', 'hy_xin%d' % s2, ['X1D'], [('hy_xin', s2)],
              [(xin[s2][:, :, :], X1D[:, f1, :, :])])
        p.dma('sp', 'hy_tw%d' % s2, [], [('hy_tw', s2)], [(tw[s2][:, :, :], I[tw2name][f1])])
        if mode != 'filter':
            p.dma('sp', 'hy_ksb%d' % s2, ['KS'], [('hy_ksb', s2)], [(ksb[s2][:, :, :], KS[:, f1, :, :])])
        zr, zi = s2 * 2, s2 * 2 + 1

        def mmz(E):
            E.matmul(B[zr][:, :], lhsT=tw[s2][:, 0, :], rhs=xin[s2][:, 0, :], start=True, stop=False)
            E.matmul(B[zr][:, :], lhsT=tw[s2][:, 2, :], rhs=xin[s2][:, 1, :], start=False, stop=True)
            E.matmul(B[zi][:, :], lhsT=tw[s2][:, 0, :], rhs=xin[s2][:, 1, :], start=True, stop=False)
            return E.matmul(B[zi][:, :], lhsT=tw[s2][:, 1, :], rhs=xin[s2][:, 0, :], start=False, stop=True)
        p.op('pe', [('hy_xin', s2), ('hy_tw', s2)], [bk(zr), bk(zi)], mmz)

    front(0)
    for f1 in range(NF1):
        s2 = f1 % 2
        zr, zi = s2 * 2, s2 * 2 + 1
        if mode == 'filter':
            p.op('act', [bk(zr)], [('hy_pr', s2, 0)], lambda E: E.activation(out=pr[s2][:, 0, :], in_=B[zr][:, :], func=AF.Copy))
            p.op('dve', [bk(zi)], [('hy_pr', s2, 1)], lambda E: E.tensor_copy(out=pr[s2][:, 1, :], in_=B[zi][:, :]))
            if f1 + 1 < NF1:
                front(f1 + 1)
            p.dma('sp', 'hy_pr%d' % s2, [('hy_pr', s2, 0), ('hy_pr', s2, 1)], ['KS'],
                  [(KS[:, f1, :, :], pr[s2][:, :, :])])
            continue
        kk = ('hy_ksb', s2)
        tt = t4[s2]
        p.op('dve', [bk(zr), kk], [('hy_t4', s2, 0)], lambda E: E.tensor_tensor(out=tt[:, 0, :], in0=B[zr][:, :], in1=ksb[s2][:, 0, :], op=ALU.mult))
        p.op('dve', [bk(zi), kk], [('hy_t4', s2, 1)], lambda E: E.tensor_tensor(out=tt[:, 1, :], in0=B[zi][:, :], in1=ksb[s2][:, 1, :], op=ALU.mult))
        p.op('dve', [bk(zr), kk], [('hy_t4', s2, 2)], lambda E: E.tensor_tensor(out=tt[:, 2, :], in0=B[zr][:, :], in1=ksb[s2][:, 1, :], op=ALU.mult))
        p.op('dve', [bk(zi), kk], [('hy_t4', s2, 3)], lambda E: E.tensor_tensor(out=tt[:, 3, :], in0=B[zi][:, :], in1=ksb[s2][:, 0, :], op=ALU.mult))
        if f1 + 1 < NF1:
            front(f1 + 1)
        p.op('pool', [('hy_t4', s2, 0), ('hy_t4', s2, 1)], [('hy_pr', s2, 0)],
             lambda E: E.tensor_tensor(out=pr[s2][:, 0, :], in0=tt[:, 0, :], in1=tt[:, 1, :], op=ALU.subtract))
        p.op('pool', [('hy_t4', s2, 2), ('hy_t4', s2, 3)], [('hy_pr', s2, 1)],
             lambda E: E.tensor_tensor(out=pr[s2][:, 1, :], in0=tt[:, 2, :], in1=tt[:, 3, :], op=ALU.add))

        def mmq(E):
            E.matmul(B[4][:, :], lhsT=E3[:, 0, :], rhs=pr[s2][:, 0, :], start=True, stop=False)
            E.matmul(B[4][:, :], lhsT=E3[:, 2, :], rhs=pr[s2][:, 1, :], start=False, stop=True)
            E.matmul(B[5][:, :], lhsT=E3[:, 1, :], rhs=pr[s2][:, 0, :], start=True, stop=False)
            return E.matmul(B[5][:, :], lhsT=E3[:, 0, :], rhs=pr[s2][:, 1, :], start=False, stop=True)
        p.op('pe', [('hy_pr', s2, 0), ('hy_pr', s2, 1), 'hy_tab'], [bk(4), bk(5)], mmq)
        p.op('act', [bk(4)], [('hy_qo', s2, 0)], lambda E: E.activation(out=qo[s2][:, 0, :], in_=B[4][:, :], func=AF.Copy))
        p.op('act', [bk(5)], [('hy_qo', s2, 1)], lambda E: E.activation(out=qo[s2][:, 1, :], in_=B[5][:, :], func=AF.Copy))
        p.dma('sp', 'hy_qo%d' % s2, [('hy_qo', s2, 0), ('hy_qo', s2, 1)], ['QD'],
              [(QD[:, f1, :, :], qo[s2][:, :, :])])


def hy_final(c, QD, na, scl, bias, zsrc, gsrc, dst, NF1=128, twfname='hy_twf', Mm=64):
    p = c.p
    I = c.inp
    B = c.bank
    bk = lambda i: ('bank', i)
    qin, twf, zg, ya, yb, yo = c.hy_qin, c.hy_twf, c.hy_zg, c.hy_ya, c.hy_yb, c.hy_yo
    for b in range(128):
        s2 = b % 2
        p.dma('sp', 'hy_qin%d' % s2, ['QD'], [('hy_qin', s2)], [(qin[s2][0:NF1, :, :], QD[b, 0:NF1, :, :])])
        p.dma('sp', 'hy_twf%d' % s2, [], [('hy_twf', s2)], [(twf[s2][0:NF1, :, 0:Mm], I[twfname][b])])
        p.dma('sp', 'hy_zg%d' % s2, ['HYC', 'Z2'], [('hy_zg', s2)],
              [(zg[s2][0:na, 0, :], zsrc[:, b, :]), (zg[s2][0:na, 1, :], gsrc[:, b, :])])

        def mmy(E, s2=s2):
            E.matmul(B[s2][0:Mm, :], lhsT=twf[s2][0:NF1, 0, 0:Mm], rhs=qin[s2][0:NF1, 0, :], start=True, stop=False)
            return E.matmul(B[s2][0:Mm, :], lhsT=twf[s2][0:NF1, 1, 0:Mm], rhs=qin[s2][0:NF1, 1, :], start=False, stop=True)
        p.op('pe', [('hy_qin', s2), ('hy_twf', s2)], [bk(s2)], mmy)
        p.op('dve', [bk(s2), 'hy_scl'], [('hy_ya', s2)],
             lambda E, s2=s2: E.tensor_tensor(out=ya[s2][0:na, :], in0=B[s2][0:na, :], in1=scl[0:na, :], op=ALU.mult))
        p.op('pool', [('hy_zg', s2), 'hy_scl'], [('hy_yb', s2)],
             lambda E, s2=s2: E.tensor_tensor(out=yb[s2][0:na, :], in0=zg[s2][0:na, 0, :], in1=bias[0:na, :], op=ALU.mult))
        p.op('pool', [('hy_ya', s2), ('hy_yb', s2)], [('hy_ya', s2)],
             lambda E, s2=s2: E.tensor_tensor(out=ya[s2][0:na, :], in0=ya[s2][0:na, :], in1=yb[s2][0:na, :], op=ALU.add))
        p.op('dve', [('hy_ya', s2), ('hy_zg', s2)], [('hy_yo', s2)],
             lambda E, s2=s2: E.tensor_tensor(out=yo[s2][0:na, :], in0=ya[s2][0:na, :], in1=zg[s2][0:na, 1, :], op=ALU.mult))
        p.dma('sp', 'hy_yo%d' % s2, [('hy_yo', s2)], ['Z2', 'OH'], [(dst[:, b, :], yo[s2][0:na, :])])


def stage_hyena(c, l, with_ctx):
    p = c.p
    I = c.inp
    hy_conv3(c, l)
    jobs = [0, 1] if with_ctx else [0]
    hp = c.cfg.get('hy_parts', 9)
    if hp < 1:
        return
    for job in jobs:
        hy_filters(c, l, job)
    if hp < 2:
        return
    with ExitStack() as es:
        c.es = es
        c.hy_zt = [sb(c, 'hy_zt%d' % i, [128, 8, 512], BF16) for i in range(2)]
        c.hy_xo = [sb(c, 'hy_xo%d' % i, [128, 2, 512], BF16) for i in range(2)]
        c.hy_xin = [sb(c, 'hy_xin%d' % i, [128, 2, 512], BF16) for i in range(2)]
        c.hy_tw = [sb(c, 'hy_tw%d' % i, [128, 3, 128], BF16) for i in range(2)]
        c.hy_ksb = [sb(c, 'hy_ksb%d' % i, [128, 2, 512], BF16) for i in range(2)]
        c.hy_pr = [sb(c, 'hy_pr%d' % i, [128, 2, 512], BF16) for i in range(2)]
        c.hy_t4 = [sb(c, 'hy_t4%d' % i, [128, 4, 512], F32) for i in range(2)]
        c.hy_qo = [sb(c, 'hy_qo%d' % i, [128, 2, 512], BF16) for i in range(2)]
        c.hy_qin = [sb(c, 'hy_qin%d' % i, [128, 2, 512], BF16) for i in range(2)]
        c.hy_twf = [sb(c, 'hy_twf%d' % i, [128, 2, 64], BF16) for i in range(2)]
        c.hy_zg = [sb(c, 'hy_zg%d' % i, [64, 2, 512], BF16) for i in range(2)]
        c.hy_ya = [sb(c, 'hy_ya%d' % i, [64, 512], F32) for i in range(2)]
        c.hy_yb = [sb(c, 'hy_yb%d' % i, [64, 512], F32) for i in range(2)]
        c.hy_yo = [sb(c, 'hy_yo%d' % i, [64, 512], BF16) for i in range(2)]
        tabs = sb(c, 'hy_tabs', [128, 2, 2, 128], BF16)
        c.hy_E3 = sb(c, 'hy_E3', [128, 3, 128], BF16)
        scl = sb(c, 'hy_scl', [64, 2, 512], F32)
        bias = sb(c, 'hy_bias', [64, 2, 512], F32)
        p.dma('sp', 'hy_tab', [], ['hy_tab'],
              [(tabs[:, :, :, :], I['hy_dft1'][:, :, :, :]), (c.hy_E3[:, :, :], I['hy_e3'][:, :, :])])
        for job in jobs:
            na = 64 if job == 0 else 2
            tok0 = NCTX if job == 0 else 0
            nt = 128 if job == 0 else 4
            ftab = tabs[:, job, :, :]
            NF1 = 128 if job == 0 else 4
            tw2n = 'hy_tw2' if job == 0 else 'hy_tw2c'
            twfn = 'hy_twf' if job == 0 else 'hy_twfc'
            Mm = 64 if job == 0 else 2
            KTD, KS, X1D, QD = c.KTD[job], c.KS, c.X1D, c.QD
            for o in range(2):
                ksrc = KTD[:, o * 512:(o + 1) * 512].rearrange("(a b) c -> a b c", b=128)
                hy_fwd1(c, lambda b0, nb, ksrc=ksrc: ksrc[:, b0:b0 + nb, :], nt, ftab, X1D, NF1)
                if hp >= 4:
                    hy_stage2(c, X1D, 'filter', KS[o], None, NF1, tw2n)
            if hp < 5:
                continue
            p.dma('sp', 'hy_scl', [('SCL', job)], ['hy_scl'],
                  [(scl[:, o, :], c.SCL[job][0, o * 512:(o + 1) * 512].partition_broadcast(64)) for o in range(2)] +
                  [(bias[:, o, :], I['hy_bias'][l, o, :].partition_broadcast(64)) for o in range(2)])
            hyc = c.HYC[tok0:tok0 + na * 128, :].rearrange("(a b) c -> a b c", b=128)
            z2 = c.Z2[tok0:tok0 + na * 128, :].rearrange("(a b) c -> a b c", b=128)
            oh = c.OH[tok0:tok0 + na * 128, :].rearrange("(a b) c -> a b c", b=128)
            vsrc = hyc[:, :, 0:512]
            hy_fwd1(c, lambda b0, nb: vsrc[:, b0:b0 + nb, :], na, ftab, X1D, NF1)
            hy_stage2(c, X1D, 'conv', KS[0], QD, NF1, tw2n)
            if hp < 6:
                continue
            hy_final(c, QD, na, scl[:, 0, :], bias[:, 0, :], vsrc, hyc[:, :, 512:1024], z2, NF1, twfn, Mm)
            if hp < 7:
                continue
            hy_fwd1(c, lambda b0, nb: z2[:, b0:b0 + nb, :], na, ftab, X1D, NF1)
            hy_stage2(c, X1D, 'conv', KS[1], QD, NF1, tw2n)
            hy_final(c, QD, na, scl[:, 1, :], bias[:, 1, :], z2, hyc[:, :, 1024:1536], oh, NF1, twfn, Mm)
        p.barrier()


def ln_affine_store(c, r, rkey, gb, gbkey, dsts, slot):
    p = c.p
    st = c.e_st[slot]
    junk = c.e_junk
    p.op('act', [rkey], ['e_junk', ('e_st', slot, 0)],
         lambda E: E.activation(out=junk[:, :], in_=r[:, :], func=AF.Copy, accum_out=st[:, 0:1]))
    p.op('dve', [('e_st', slot, 0)], [('e_st', slot, 1)],
         lambda E: E.tensor_scalar(out=st[:, 1:2], in0=st[:, 0:1], scalar1=-1.0 / D, scalar2=None, op0=ALU.mult))
    p.op('act', [rkey, ('e_st', slot, 1)], ['e_junk', ('e_st', slot, 2)],
         lambda E: E.activation(out=junk[:, :], in_=r[:, :], func=AF.Square, bias=st[:, 1:2], scale=1.0, accum_out=st[:, 2:3]))
    p.op('act', [('e_st', slot, 2)], [('e_st', slot, 3)],
         lambda E: E.activation(out=st[:, 3:4], in_=st[:, 2:3], func=AF.Ln, scale=1.0 / D, bias=LN_EPS))
    p.op('act', [('e_st', slot, 3)], [('e_st', slot, 4)],
         lambda E: E.activation(out=st[:, 4:5], in_=st[:, 3:4], func=AF.Exp, scale=-0.5))
    p.op('dve', [rkey, ('e_st', slot, 1), ('e_st', slot, 4)], [rkey],
         lambda E: E.tensor_scalar(out=r[:, :], in0=r[:, :], scalar1=st[:, 1:2], scalar2=st[:, 4:5], op0=ALU.add, op1=ALU.mult))
    p.op('pool', [rkey, gbkey], [rkey], lambda E: E.tensor_tensor(out=r[:, :], in0=r[:, :], in1=gb[:, 0, :], op=ALU.mult))
    p.op('pool', [rkey, gbkey], [rkey], lambda E: E.tensor_tensor(out=r[:, :], in0=r[:, :], in1=gb[:, 1, :], op=ALU.add))
    p.dma('sp', 'e_r%d' % slot, [rkey], ['XOUT'], [(d, r[:, :]) for d in dsts])


def load_bc_rows(c, l, which):
    p = c.p
    I = c.inp
    g = 2 if which == 1 else 5
    lg, lb = ('ln1_g', 'ln1_b') if which == 1 else ('ln2_g', 'ln2_b')
    p.dma('sp', 'e_bc', [('modv', l)], ['e_bc'],
          [(c.e_ag[:, r, :], c.modv[l][r, g * 1024:(g + 1) * 1024].partition_broadcast(128)) for r in range(2)] +
          [(c.e_gb[:, 0, :], I[lg][l, :].partition_broadcast(128)), (c.e_gb[:, 1, :], I[lb][l, :].partition_broadcast(128))])


def stage_merge(c, l, xsrc, tiles):
    p = c.p
    I = c.inp
    S = c.scr[l]
    B = c.bank
    bk = lambda i: ('bank', i)
    with ExitStack() as es:
        c.es = es
        wbr = sb(c, 'mg_wbr', [128, 3, 4, 1024], BF16)
        wout = sb(c, 'mg_wout', [128, 8, 1024], BF16)
        c.e_ag = sb(c, 'e_ag', [128, 2, 1024], F32)
        c.e_gb = sb(c, 'e_gb', [128, 2, 1024], F32)
        c.e_st = [sb(c, 'e_st%d' % i, [128, 8], F32) for i in range(2)]
        c.e_junk = sb(c, 'e_junk', [128, 1024], BF16)
        oT = [sb(c, 'mg_oT%d' % i, [128, 3, 4, 128], BF16) for i in range(2)]
        oh = [sb(c, 'mg_oh%d' % i, [128, 512], BF16) for i in range(2)]
        g3 = [sb(c, 'mg_g3%d' % i, [128, 3072], BF16) for i in range(2)]
        y = [sb(c, 'mg_y%d' % i, [128, 1024], F32) for i in range(2)]
        tt = [sb(c, 'mg_t%d' % i, [128, 1024], F32) for i in range(2)]
        yb = [sb(c, 'mg_yb%d' % i, [128, 1024], BF16) for i in range(2)]
        yT = [sb(c, 'mg_yT%d' % i, [128, 8, 128], BF16) for i in range(2)]
        xt = [sb(c, 'mg_xt%d' % i, [128, 1024], F32) for i in range(2)]
        for br, nm in enumerate(['w_br_gla', 'w_br_mla', 'w_br_hy']):
            p.dma('pool', 'mg_w', [], ['mg_w'], [(wbr[:, br, :, :], I[nm][l].rearrange("(k p) n -> p k n", p=128))])
        p.dma('pool', 'mg_w', [], ['mg_w'], [(wout[:, :, :], I['w_out'][l].rearrange("(k p) n -> p k n", p=128))])
        load_bc_rows(c, l, 1)
        for i, t in enumerate(tiles):
            s2 = i % 2
            r = 1 if t < 2 else 0
            tok = slice(t * 128, (t + 1) * 128)
            p.dma('sp', 'mg_oT%d' % s2, [('OGT', l), ('OMT', l)], [('mg_oT', s2)],
                  [(oT[s2][:, 0, :, :], S['OGT'][:, tok].rearrange("(k p) t -> p k t", p=128)),
                   (oT[s2][:, 1, :, :], S['OMT'][:, tok].rearrange("(k p) t -> p k t", p=128))])
            p.dma('sp', 'mg_oh%d' % s2, ['OH'], [('mg_oh', s2)], [(oh[s2][:, :], c.OH[tok, :])])
            p.dma('sp', 'mg_g3%d' % s2, [('G3', l)], [('mg_g3', s2)], [(g3[s2][:, :], S['G3'][tok, :])])
            p.dma('sp', 'mg_xt%d' % s2, ['XOUT'], [('mg_xt', s2)], [(xt[s2][:, :], xsrc(t))])
            tpv = bank16(c, 6)

            def tro(E):
                ins = None
                for k in range(4):
                    ins = E.transpose(tpv[:, k, :], oh[s2][:, k * 128:(k + 1) * 128], c.ident[:, :])
                return ins
            p.op('pe', [('mg_oh', s2), 'ident'], [bk(6)], tro)
            p.op('act', [bk(6)], [('mg_oT', s2)], lambda E: E.activation(out=oT[s2][:, 2, :, :], in_=tpv[:, 0:4, :], func=AF.Copy))
            for br in range(3):
                for half in range(2):
                    bb = (br * 2 + half) % 4

                    def mmb(E):
                        ins = None
                        for k in range(4):
                            ins = E.matmul(B[bb][:, :], lhsT=oT[s2][:, br, k, :], rhs=wbr[:, br, k, half * 512:(half + 1) * 512],
                                           start=(k == 0), stop=(k == 3))
                        return ins
                    p.op('pe', [('mg_oT', s2), 'mg_w'], [bk(bb)], mmb)
                    hs = slice(half * 512, (half + 1) * 512)
                    gs = slice(br * 1024 + half * 512, br * 1024 + (half + 1) * 512)
                    if br == 0:
                        p.op('dve', [bk(bb), ('mg_g3', s2)], [('mg_y', s2, half)],
                             lambda E: E.tensor_tensor(out=y[s2][:, hs], in0=B[bb][:, :], in1=g3[s2][:, gs], op=ALU.mult))
                    else:
                        p.op('dve', [bk(bb), ('mg_g3', s2)], [('mg_t', s2, half)],
                             lambda E: E.tensor_tensor(out=tt[s2][:, hs], in0=B[bb][:, :], in1=g3[s2][:, gs], op=ALU.mult))
                        p.op('pool', [('mg_t', s2, half), ('mg_y', s2, half)], [('mg_y', s2, half)],
                             lambda E: E.tensor_tensor(out=y[s2][:, hs], in0=y[s2][:, hs], in1=tt[s2][:, hs], op=ALU.add))
            p.op('act', [('mg_y', s2, 0), ('mg_y', s2, 1)], [('mg_yb', s2)],
                 lambda E: E.activation(out=yb[s2][:, :], in_=y[s2][:, :], func=AF.Copy))
            tp7 = bank16(c, 7)

            def try_(E):
                ins = None
                for k in range(8):
                    ins = E.transpose(tp7[:, k, :], yb[s2][:, k * 128:(k + 1) * 128], c.ident[:, :])
                return ins
            p.op('pe', [('mg_yb', s2), 'ident'], [bk(7)], try_)
            p.op('act', [bk(7)], [('mg_yT', s2)], lambda E: E.activation(out=yT[s2][:, :, :], in_=tp7[:, :, :], func=AF.Copy))
            for half in range(2):
                bb = 4 + half

                def mmo(E):
                    ins = None
                    for k in range(8):
                        ins = E.matmul(B[bb][:, :], lhsT=yT[s2][:, k, :], rhs=wout[:, k, half * 512:(half + 1) * 512],
                                       start=(k == 0), stop=(k == 7))
                    return ins
                p.op('pe', [('mg_yT', s2), 'mg_w'], [bk(bb)], mmo)
                hs = slice(half * 512, (half + 1) * 512)
                p.op('dve', [bk(bb), 'e_bc'], [('mg_t', s2, half)],
                     lambda E: E.tensor_tensor(out=tt[s2][:, hs], in0=B[bb][:, :], in1=c.e_ag[:, r, hs], op=ALU.mult))
                p.op('dve', [('mg_t', s2, half), ('mg_xt', s2)], [('mg_xt', s2)],
                     lambda E: E.scalar_tensor_tensor(out=xt[s2][:, hs], in0=xt[s2][:, hs], scalar=ALPHA, in1=tt[s2][:, hs],
                                                      op0=ALU.mult, op1=ALU.add))
            ln_affine_store(c, xt[s2], ('mg_xt', s2), c.e_gb, 'e_bc', [c.X1[tok, :]], s2)
        p.barrier()


def moe_precast(c, l):
    p = c.p
    I = c.inp
    for e in range(32):
        if c.cfg.get('moe_mode', 'sparse') == 'sparse':
            p.dma('pool', 'wcast', [], [('WB', l, e)],
                  [(c.WB1[l][e].rearrange("(p k) n -> p k n", k=8), I['moe_w1'][l, e].rearrange("(k p) n -> p k n", p=128)),
                   (c.WB2[l][e].rearrange("(p k) n -> p k n", k=8), I['moe_w2'][l, e].rearrange("(k p) n -> p k n", p=128))])
        else:
            p.dma('pool', 'wcast', [], [('WB', l, e)],
                  [(c.WB1[l][e], I['moe_w1'][l, e]), (c.WB2[l][e], I['moe_w2'][l, e])])


def stage_moe(c, l, tiles, dst_fn):
    p = c.p
    I = c.inp
    B = c.bank
    bk = lambda i: ('bank', i)
    cst = c.cst
    with ExitStack() as es:
        c.es = es
        c.e_ag = sb(c, 'e_ag', [128, 2, 1024], F32)
        c.e_gb = sb(c, 'e_gb', [128, 2, 1024], F32)
        c.e_st = [sb(c, 'e_st%d' % i, [128, 8], F32) for i in range(2)]
        c.e_junk = sb(c, 'e_junk', [128, 1024], BF16)
        w1 = [sb(c, 'mo_w1%d' % i, [128, 8, 2048], BF16) for i in range(2)]
        w2 = [sb(c, 'mo_w2%d' % i, [128, 8, 1024], BF16) for i in range(1)] * 2
        hT = sb(c, 'mo_hT', [128, 8, 1024], BF16)
        aT = sb(c, 'mo_aT', [128, 8, 1024], BF16)
        yacc = sb(c, 'mo_yacc', [128, 8, 1024], F32)
        rw = sb(c, 'mo_rw', [128, 8, 32], F32)
        rb = sb(c, 'mo_rb', [1, 32], F32)
        b1 = sb(c, 'mo_b1', [128, 32, 16], F32)
        b2 = sb(c, 'mo_b2', [32, 1024], F32)
        G = sb(c, 'mo_G', [128, 8, 32], F32)
        GT = sb(c, 'mo_GT', [32, 8, 128], F32)
        xt = [sb(c, 'mo_xt%d' % i, [128, 1024], F32) for i in range(2)]
        xh = [sb(c, 'mo_xh%d' % i, [128, 1024], F32) for i in range(1)] * 2
        h32 = [sb(c, 'mo_h32%d' % i, [128, 8, 128], F32) for i in range(1)] * 2
        lg = [sb(c, 'mo_lg%d' % i, [128, 32], F32) for i in range(2)]
        mx = [sb(c, 'mo_mx%d' % i, [128, 8], F32) for i in range(2)]
        ex = [sb(c, 'mo_ex%d' % i, [128, 32], F32) for i in range(2)]
        sm = [sb(c, 'mo_sm%d' % i, [128, 2], F32) for i in range(2)]
        gg = [sb(c, 'mo_gg%d' % i, [128, 512], F32) for i in range(2)]
        sg = [sb(c, 'mo_sg%d' % i, [128, 512], F32) for i in range(2)]
        ll = [sb(c, 'mo_ll%d' % i, [128, 512], F32) for i in range(2)]
        st = [sb(c, 'mo_st%d' % i, [128, 8], F32) for i in range(2)]
        p.dma('sp', 'mo_c', [], ['mo_c'],
              [(rw[:, :, :], I['router_w'][l].rearrange("(k p) e -> p k e", p=128)),
               (rb[:, :], I['router_b'][l:l + 1, :]), (b2[:, :], I['moe_b2'][l])])
        p.dma('sp', 'mo_b1', [], ['mo_b1'],
              [(b1[:, e, :], I['moe_b1'][l, e, :].rearrange("(j p) -> p j", p=128)) for e in range(32)], slow=True)
        load_bc_rows(c, l, 2)
        ones = cst[:, 640:768]
        ident32 = cst[:, 0:128]
        groups = [tiles[i:i + 8] for i in range(0, len(tiles), 8)][:c.cfg.get('moe_groups', 99)]
        wi = 0
        for gi, gt in enumerate(groups):
            ng = len(gt)
            T = ng * 128
            for j, t in enumerate(gt):
                s2 = j % 2
                r = 1 if t < 2 else 0
                tok = slice(t * 128, (t + 1) * 128)
                p.dma('sp', 'mo_xt%d' % s2, ['XOUT'], [('mo_xt', s2)], [(xt[s2][:, :], c.X1[tok, :])])
                s_ = st[s2]
                p.op('act', [('mo_xt', s2)], ['e_junk', ('mo_st', s2, 0)],
                     lambda E: E.activation(out=c.e_junk[:, :], in_=xt[s2][:, :], func=AF.Copy, accum_out=s_[:, 0:1]))
                p.op('dve', [('mo_st', s2, 0)], [('mo_st', s2, 1)],
                     lambda E: E.tensor_scalar(out=s_[:, 1:2], in0=s_[:, 0:1], scalar1=-1.0 / D, scalar2=None, op0=ALU.mult))
                p.op('act', [('mo_xt', s2), ('mo_st', s2, 1)], ['e_junk', ('mo_st', s2, 2)],
                     lambda E: E.activation(out=c.e_junk[:, :], in_=xt[s2][:, :], func=AF.Square, bias=s_[:, 1:2], scale=1.0,
                                            accum_out=s_[:, 2:3]))
                p.op('act', [('mo_st', s2, 2)], [('mo_st', s2, 3)],
                     lambda E: E.activation(out=s_[:, 3:4], in_=s_[:, 2:3], func=AF.Ln, scale=1.0 / D, bias=LN_EPS))
                p.op('act', [('mo_st', s2, 3)], [('mo_st', s2, 4)],
                     lambda E: E.activation(out=s_[:, 4:5], in_=s_[:, 3:4], func=AF.Exp, scale=-0.5))
                p.op('dve', [('mo_xt', s2), ('mo_st', s2, 1), ('mo_st', s2, 4)], [('mo_xh', 0)],
                     lambda E: E.tensor_scalar(out=xh[s2][:, :], in0=xt[s2][:, :], scalar1=s_[:, 1:2], scalar2=s_[:, 4:5],
                                               op0=ALU.add, op1=ALU.mult))
                for hh in range(2):
                    bb = hh
                    def tr32(E):
                        ins = None
                        for k in range(4):
                            kk = hh * 4 + k
                            ins = E.transpose(B[bb][:, k * 128:(k + 1) * 128], xh[s2][:, kk * 128:(kk + 1) * 128], ident32)
                        return ins
                    p.op('pe', [('mo_xh', 0), 'cst'], [bk(bb)], tr32)
                    for k in range(4):
                        kk = hh * 4 + k
                        p.op('dve', [bk(bb), 'modT'], [('mo_h32', 0, kk)],
                             lambda E: E.tensor_scalar(out=h32[s2][:, kk, :], in0=B[bb][:, k * 128:(k + 1) * 128],
                                                       scalar1=c.modT[:, r, 4, kk:kk + 1], scalar2=c.modT[:, r, 3, kk:kk + 1],
                                                       op0=ALU.mult, op1=ALU.add))
                hk = [('mo_h32', 0, kk) for kk in range(8)]
                p.op('pool', hk, [('mo_hT', j)], lambda E: E.tensor_copy(out=hT[:, :, j * 128:(j + 1) * 128], in_=h32[s2][:, :, :]))

                def mml(E):
                    for kk in range(8):
                        E.matmul(B[2][:, 0:32], lhsT=h32[s2][:, kk, :], rhs=rw[:, kk, :], start=(kk == 0), stop=False)
                    return E.matmul(B[2][:, 0:32], lhsT=ones[0:1, :], rhs=rb[0:1, :], start=False, stop=True)
                p.op('pe', hk + ['mo_c', 'cst'], [bk(2)], mml)
                p.op('dve', [bk(2)], [('mo_lg', s2)], lambda E: E.tensor_copy(out=lg[s2][:, :], in_=B[2][:, 0:32]))
                p.op('dve', [('mo_lg', s2)], [('mo_mx', s2)], lambda E: E.max(out=mx[s2][:, :], in_=lg[s2][:, :]))
                p.op('dve', [('mo_mx', s2)], [('mo_sm', s2, 0)],
                     lambda E: E.tensor_scalar(out=sm[s2][:, 0:1], in0=mx[s2][:, 0:1], scalar1=-1.0, scalar2=None, op0=ALU.mult))
                p.op('act', [('mo_lg', s2), ('mo_sm', s2, 0)], [('mo_ex', s2)],
                     lambda E: E.activation(out=ex[s2][:, :], in_=lg[s2][:, :], func=AF.Exp, bias=sm[s2][:, 0:1], scale=1.0))
                p.op('dve', [('mo_lg', s2), ('mo_mx', s2)], [('mo_lg', s2)],
                     lambda E: E.tensor_scalar(out=lg[s2][:, :], in0=lg[s2][:, :], scalar1=mx[s2][:, 3:4], scalar2=None, op0=ALU.is_ge))
                p.op('dve', [('mo_lg', s2), ('mo_ex', s2)], [('mo_ex', s2)],
                     lambda E: E.tensor_tensor(out=ex[s2][:, :], in0=ex[s2][:, :], in1=lg[s2][:, :], op=ALU.mult))
                p.op('dve', [('mo_ex', s2)], [('mo_sm', s2, 1)],
                     lambda E: E.tensor_reduce(out=sm[s2][:, 1:2], in_=ex[s2][:, :], axis=AX.X, op=ALU.add))
                p.op('dve', [('mo_sm', s2, 1)], [('mo_sm', s2, 1)], lambda E: E.reciprocal(out=sm[s2][:, 1:2], in_=sm[s2][:, 1:2]))
                p.op('dve', [('mo_ex', s2), ('mo_sm', s2, 1)], [('mo_G', j)],
                     lambda E: E.tensor_scalar(out=G[:, j, :], in0=ex[s2][:, :], scalar1=sm[s2][:, 1:2], scalar2=None, op0=ALU.mult))
                p.op('pe', [('mo_G', j), 'cst'], [bk(3)], lambda E: E.transpose(B[3][0:32, 0:128], G[:, j, :], ident32))
                p.op('act', [bk(3)], [('mo_GT', j)], lambda E: E.activation(out=GT[:, j, :], in_=B[3][0:32, 0:128], func=AF.Copy))
                for half in range(2):
                    p.op('pe', [('mo_GT', j), 'mo_c'], [bk(4 + half)],
                         lambda E: E.matmul(B[4 + half][:, :], lhsT=GT[:, j, :], rhs=b2[:, half * 512:(half + 1) * 512], start=True, stop=True))
                    p.op('act', [bk(4 + half)], [('mo_yacc', j, half)],
                         lambda E: E.activation(out=yacc[:, j, half * 512:(half + 1) * 512], in_=B[4 + half][:, :], func=AF.Copy))
            hTk = [('mo_hT', j) for j in range(ng)]
            ei = 0
            for e in range(32):
                ws = wi % 2
                wi += 1
                p.dma('sp', 'mo_w1%d' % ws, [('WB', l, e)], [('mo_w1', ws)],
                      [(w1[ws][:, :, :], c.WB1[l][e].rearrange("(k p) n -> p k n", p=128))])
                p.dma('sp', 'mo_w2', [('WB', l, e)], [('mo_w2', 0)],
                      [(w2[ws][:, :, :], c.WB2[l][e].rearrange("(k p) n -> p k n", p=128))])
                for th in range((T + 511) // 512):
                    c0 = th * 512
                    n = min(512, T - c0)
                    for fc in range(8):
                        s2 = ei % 2
                        ei += 1
                        bg, bl = s2 * 2, s2 * 2 + 1

                        def mm1(E):
                            ins = None
                            for k in range(8):
                                E.matmul(B[bg][:, 0:n], lhsT=w1[ws][:, k, fc * 128:(fc + 1) * 128], rhs=hT[:, k, c0:c0 + n],
                                         start=(k == 0), stop=(k == 7))
                            for k in range(8):
                                ins = E.matmul(B[bl][:, 0:n], lhsT=w1[ws][:, k, 1024 + fc * 128:1024 + (fc + 1) * 128],
                                               rhs=hT[:, k, c0:c0 + n], start=(k == 0), stop=(k == 7))
                            return ins
                        p.op('pe', hTk + [('mo_w1', ws)], [bk(bg), bk(bl)], mm1)
                        p.op('dve', [bk(bg), 'mo_b1'], [('mo_gg', s2)],
                             lambda E: E.tensor_scalar(out=gg[s2][:, 0:n], in0=B[bg][:, 0:n], scalar1=b1[:, e, fc:fc + 1], scalar2=7.0,
                                                       op0=ALU.add, op1=ALU.min))
                        p.op('act', [('mo_gg', s2)], [('mo_sg', s2)],
                             lambda E: E.activation(out=sg[s2][:, 0:n], in_=gg[s2][:, 0:n], func=AF.Sigmoid, scale=1.702))
                        p.op('dve', [bk(bl), 'mo_b1'], [('mo_ll', s2)],
                             lambda E: E.tensor_scalar(out=ll[s2][:, 0:n], in0=B[bl][:, 0:n], scalar1=b1[:, e, 8 + fc:9 + fc], scalar2=7.0,
                                                       op0=ALU.add, op1=ALU.min))
                        p.op('pool', [('mo_ll', s2)], [('mo_ll', s2)],
                             lambda E: E.tensor_scalar(out=ll[s2][:, 0:n], in0=ll[s2][:, 0:n], scalar1=-7.0, scalar2=1.0,
                                                       op0=ALU.max, op1=ALU.add))
                        p.op('pool', [('mo_gg', s2), ('mo_sg', s2)], [('mo_gg', s2)],
                             lambda E: E.tensor_tensor(out=gg[s2][:, 0:n], in0=gg[s2][:, 0:n], in1=sg[s2][:, 0:n], op=ALU.mult))
                        p.op('dve', [('mo_gg', s2), ('mo_ll', s2)], [('mo_aT', fc, th)],
                             lambda E: E.tensor_tensor(out=aT[:, fc, c0:c0 + n], in0=gg[s2][:, 0:n], in1=ll[s2][:, 0:n], op=ALU.mult))
                aTk = [('mo_aT', fc, th) for fc in range(8) for th in range((T + 511) // 512)]
                for j in range(ng):
                    for half in range(2):
                        bb = 4 + (j * 2 + half) % 4

                        def mm2(E):
                            ins = None
                            for k in range(8):
                                ins = E.matmul(B[bb][:, :], lhsT=aT[:, k, j * 128:(j + 1) * 128], rhs=w2[ws][:, k, half * 512:(half + 1) * 512],
                                               start=(k == 0), stop=(k == 7))
                            return ins
                        p.op('pe', aTk + [('mo_w2', 0)], [bk(bb)], mm2)
                        hs = slice(half * 512, (half + 1) * 512)
                        p.op('dve', [bk(bb), ('mo_G', j), ('mo_yacc', j, half)], [('mo_yacc', j, half)],
                             lambda E: E.scalar_tensor_tensor(out=yacc[:, j, hs], in0=B[bb][:, :], scalar=G[:, j, e:e + 1], in1=yacc[:, j, hs],
                                                              op0=ALU.mult, op1=ALU.add))
            for j, t in enumerate(gt):
                s2 = j % 2
                r = 1 if t < 2 else 0
                tok = slice(t * 128, (t + 1) * 128)
                p.dma('sp', 'mo_xt%d' % s2, ['XOUT'], [('mo_xt', s2)], [(xt[s2][:, :], c.X1[tok, :])])
                yk = [('mo_yacc', j, 0), ('mo_yacc', j, 1)]
                p.op('pool', yk + ['e_bc'], yk, lambda E: E.tensor_tensor(out=yacc[:, j, :], in0=yacc[:, j, :], in1=c.e_ag[:, r, :], op=ALU.mult))
                p.op('dve', yk + [('mo_xt', s2)], [('mo_xt', s2)],
                     lambda E: E.scalar_tensor_tensor(out=xt[s2][:, :], in0=xt[s2][:, :], scalar=ALPHA, in1=yacc[:, j, :],
                                                      op0=ALU.mult, op1=ALU.add))
                ln_affine_store(c, xt[s2], ('mo_xt', s2), c.e_gb, 'e_bc', dst_fn(t), s2)
        p.barrier()


def stage_moe2(c, l, tiles, dst_fn):
    p = c.p
    I = c.inp
    B = c.bank
    bk = lambda i: ('bank', i)
    cst = c.cst
    ones = cst[:, 640:768]
    ident32 = cst[:, 0:128]
    iota_f = cst[:, 768:800]
    iota_p = cst[:, 800:801]
    blk512 = cst[:, 896:1024]
    ntl = len(tiles)
    NB = (ntl * 128 * 4) // 512 + 32
    XB, YB, HB = c.XB, c.YB, c.HB
    W1f = c.WB1[l].rearrange("e (p k) n -> (e p) (k n)", k=8)
    W2f = c.WB2[l].rearrange("e (p k) n -> (e p) (k n)", k=8)
    I32 = mybir.dt.int32
    with ExitStack() as es_outer:
        c.es = es_outer
        slot_i = sb(c, 'ms_slot', [128, NTILE, 4], I32)
        gk = sb(c, 'ms_gk', [128, NTILE, 4], F32)
        idxw = sb(c, 'ms_idxw', [128, NB, 8], I32)
        be_bc = sb(c, 'ms_bebc', [128, NB], F32)
        OHall = sb(c, 'ms_oh', [32, NB], F32)
        c.e_ag = sb(c, 'e_ag', [128, 2, 1024], F32)
        c.e_gb = sb(c, 'e_gb', [128, 2, 1024], F32)
        c.e_st = [sb(c, 'e_st%d' % i, [128, 8], F32) for i in range(2)]
        c.e_junk = sb(c, 'e_junk', [128, 1024], BF16)
        load_bc_rows(c, l, 2)
        with ExitStack() as es:
            c.es = es
            Mall = sb(c, 'ms_M', [128, NTILE, 32], F32)
            Gall = sb(c, 'ms_G', [128, NTILE, 32], F32)
            Lall = sb(c, 'ms_L', [128, NTILE, 32], F32)
            mxall = sb(c, 'ms_mx', [128, NTILE, 8], F32)
            rw = sb(c, 'ms_rw', [128, 8, 32], F32)
            rb = sb(c, 'ms_rb', [1, 32], F32)
            modbc = sb(c, 'ms_modbc', [128, 2, 2, 1024], F32)
            xt = [sb(c, 'ms_xt%d' % i, [128, 1024], F32) for i in range(2)]
            xh = sb(c, 'ms_xh', [128, 1024], F32)
            hb = [sb(c, 'ms_hb%d' % i, [128, 1024], BF16) for i in range(2)]
            h32 = sb(c, 'ms_h32', [128, 8, 128], F32)
            ex = [sb(c, 'ms_ex%d' % i, [128, 32], F32) for i in range(2)]
            sm = [sb(c, 'ms_sm%d' % i, [128, 2], F32) for i in range(2)]
            st = [sb(c, 'ms_st%d' % i, [128, 8], F32) for i in range(2)]
            Lst = sb(c, 'ms_Lst', [128, 128], F32)
            Msum = sb(c, 'ms_Msum', [128, 32], F32)
            cnt = sb(c, 'ms_cnt', [1, 32], F32)
            cnti = sb(c, 'ms_cnti', [1, 32], I32)
            pcol = sb(c, 'ms_pcol', [32, 4], F32)
            psbc = sb(c, 'ms_psbc', [128, 32], F32)
            cmp_ = sb(c, 'ms_cmp', [32, 128], F32)
            berow = sb(c, 'ms_berow', [1, 128], F32)
            tmpf = sb(c, 'ms_tmpf', [128, 128], F32)
            pos = [sb(c, 'ms_pos%d' % i, [128, 32], F32) for i in range(2)]
            oh = [sb(c, 'ms_ohk%d' % i, [128, 32], F32) for i in range(2)]
            tq = [sb(c, 'ms_tq%d' % i, [128, 32], F32) for i in range(2)]
            sl = [sb(c, 'ms_sl%d' % i, [128, 4], F32) for i in range(2)]
            p.dma('sp', 'ms_c', [], ['ms_c'],
                  [(rw[:, :, :], I['router_w'][l].rearrange("(k p) e -> p k e", p=128)), (rb[:, :], I['router_b'][l:l + 1, :])])
            p.dma('sp', 'ms_modbc', [('modv', l)], ['ms_modbc'],
                  [(modbc[:, r, q, :], c.modv[l][r, (4 - q) * 1024:(5 - q) * 1024].partition_broadcast(128))
                   for r in range(2) for q in range(2)])
            for r in range(2):
                p.op('dve', ['ms_modbc'], ['ms_modbc'],
                     lambda E: E.tensor_scalar(out=modbc[:, r, 0, :], in0=modbc[:, r, 0, :], scalar1=1.0, scalar2=None, op0=ALU.add))
            p.op('dve', ['cst'], ['ms_Lst'], lambda E: E.tensor_tensor(out=Lst[:, :], in0=cst[:, 384:512], in1=ident32, op=ALU.subtract))
            for jj, t in enumerate(tiles):
                s2 = jj % 2
                r = 1 if t < 2 else 0
                tok = slice(t * 128, (t + 1) * 128)
                p.dma('sp', 'ms_xt%d' % s2, ['XOUT'], [('ms_xt', s2)], [(xt[s2][:, :], c.X1[tok, :])])
                s_ = st[s2]
                p.op('act', [('ms_xt', s2)], ['e_junk', ('ms_st', s2, 0)],
                     lambda E: E.activation(out=c.e_junk[:, :], in_=xt[s2][:, :], func=AF.Copy, accum_out=s_[:, 0:1]))
                p.op('dve', [('ms_st', s2, 0)], [('ms_st', s2, 1)],
                     lambda E: E.tensor_scalar(out=s_[:, 1:2], in0=s_[:, 0:1], scalar1=-1.0 / D, scalar2=None, op0=ALU.mult))
                p.op('act', [('ms_xt', s2), ('ms_st', s2, 1)], ['e_junk', ('ms_st', s2, 2)],
                     lambda E: E.activation(out=c.e_junk[:, :], in_=xt[s2][:, :], func=AF.Square, bias=s_[:, 1:2], scale=1.0,
                                            accum_out=s_[:, 2:3]))
                p.op('act', [('ms_st', s2, 2)], [('ms_st', s2, 3)],
                     lambda E: E.activation(out=s_[:, 3:4], in_=s_[:, 2:3], func=AF.Ln, scale=1.0 / D, bias=LN_EPS))
                p.op('act', [('ms_st', s2, 3)], [('ms_st', s2, 4)],
                     lambda E: E.activation(out=s_[:, 4:5], in_=s_[:, 3:4], func=AF.Exp, scale=-0.5))
                p.op('dve', [('ms_xt', s2), ('ms_st', s2, 1), ('ms_st', s2, 4)], ['ms_xh'],
                     lambda E: E.tensor_scalar(out=xh[:, :], in0=xt[s2][:, :], scalar1=s_[:, 1:2], scalar2=s_[:, 4:5],
                                               op0=ALU.add, op1=ALU.mult))
                p.op('pool', ['ms_xh', 'ms_modbc'], [('ms_xt', s2)],
                     lambda E: E.tensor_tensor(out=xt[s2][:, :], in0=xh[:, :], in1=modbc[:, r, 0, :], op=ALU.mult))
                p.op('pool', [('ms_xt', s2), 'ms_modbc'], [('ms_hb', s2)],
                     lambda E: E.tensor_tensor(out=hb[s2][:, :], in0=xt[s2][:, :], in1=modbc[:, r, 1, :], op=ALU.add))
                p.dma('sp', 'ms_hb%d' % s2, [('ms_hb', s2)], [('HB', t)], [(HB[tok, :], hb[s2][:, :])])
                for hh in range(2):
                    def tr32(E):
                        ins = None
                        for k in range(4):
                            kk = hh * 4 + k
                            ins = E.transpose(B[hh][:, k * 128:(k + 1) * 128], xh[:, kk * 128:(kk + 1) * 128], ident32)
                        return ins
                    p.op('pe', ['ms_xh', 'cst'], [bk(hh)], tr32)
                    for k in range(4):
                        kk = hh * 4 + k
                        p.op('dve', [bk(hh), 'modT'], [('ms_h32', kk)],
                             lambda E: E.tensor_scalar(out=h32[:, kk, :], in0=B[hh][:, k * 128:(k + 1) * 128],
                                                       scalar1=c.modT[:, r, 4, kk:kk + 1], scalar2=c.modT[:, r, 3, kk:kk + 1],
                                                       op0=ALU.mult, op1=ALU.add))
                hk = [('ms_h32', kk) for kk in range(8)]

                def mml(E):
                    for kk in range(8):
                        E.matmul(B[2][:, 0:32], lhsT=h32[:, kk, :], rhs=rw[:, kk, :], start=(kk == 0), stop=False)
                    return E.matmul(B[2][:, 0:32], lhsT=ones[0:1, :], rhs=rb[0:1, :], start=False, stop=True)
                p.op('pe', hk + ['ms_c', 'cst'], [bk(2)], mml)
                lgj, mxj, Mj, Gj = Lall[:, t, :], mxall[:, t, :], Mall[:, t, :], Gall[:, t, :]
                p.op('dve', [bk(2)], [('ms_L', t)], lambda E: E.tensor_copy(out=lgj, in_=B[2][:, 0:32]))
                p.op('dve', [('ms_L', t)], [('ms_mx', t)], lambda E: E.max(out=mxj, in_=lgj))
                p.op('dve', [('ms_mx', t)], [('ms_sm', s2, 0)],
                     lambda E: E.tensor_scalar(out=sm[s2][:, 0:1], in0=mxall[:, t, 0:1], scalar1=-1.0, scalar2=None, op0=ALU.mult))
                p.op('act', [('ms_L', t), ('ms_sm', s2, 0)], [('ms_ex', s2)],
                     lambda E: E.activation(out=ex[s2][:, :], in_=lgj, func=AF.Exp, bias=sm[s2][:, 0:1], scale=1.0))
                p.op('dve', [('ms_L', t), ('ms_mx', t)], [('ms_M', t)],
                     lambda E: E.tensor_scalar(out=Mj, in0=lgj, scalar1=mxall[:, t, 3:4], scalar2=None, op0=ALU.is_ge))
                p.op('dve', [('ms_M', t), ('ms_ex', s2)], [('ms_ex', s2)],
                     lambda E: E.tensor_tensor(out=ex[s2][:, :], in0=ex[s2][:, :], in1=Mj, op=ALU.mult))
                p.op('dve', [('ms_ex', s2)], [('ms_sm', s2, 1)],
                     lambda E: E.tensor_reduce(out=sm[s2][:, 1:2], in_=ex[s2][:, :], axis=AX.X, op=ALU.add))
                p.op('dve', [('ms_sm', s2, 1)], [('ms_sm', s2, 1)], lambda E: E.reciprocal(out=sm[s2][:, 1:2], in_=sm[s2][:, 1:2]))
                p.op('dve', [('ms_ex', s2), ('ms_sm', s2, 1)], [('ms_G', t)],
                     lambda E: E.tensor_scalar(out=Gj, in0=ex[s2][:, :], scalar1=sm[s2][:, 1:2], scalar2=None, op0=ALU.mult))
                p.op('pe', [('ms_M', t), 'cst'], [bk(7)],
                     lambda E: E.matmul(B[7][0:1, 0:32], lhsT=ones[:, 0:1], rhs=Mj, start=(jj == 0), stop=(jj == ntl - 1)))
            p.op('dve', [bk(7)], ['ms_cnt'], lambda E: E.tensor_scalar(out=cnt[:, :], in0=B[7][0:1, 0:32], scalar1=1.0 / 512, scalar2=255.5 / 512,
                                                                       op0=ALU.mult, op1=ALU.add))
            p.op('dve', ['ms_cnt'], ['ms_cnti'], lambda E: E.tensor_copy(out=cnti[:, :], in_=cnt[:, :]))
            p.op('dve', ['ms_cnti'], ['ms_cnt'], lambda E: E.tensor_copy(out=cnt[:, :], in_=cnti[:, :]))
            p.op('dve', ['ms_cnt'], ['ms_cnt'], lambda E: E.tensor_scalar(out=cnt[:, :], in0=cnt[:, :], scalar1=512.0, scalar2=None, op0=ALU.mult))
            p.op('pe', ['ms_cnt', 'cst'], [bk(0)], lambda E: E.transpose(B[0][0:32, 0:1], cnt[0:1, :], ident32[0:1, 0:1]))
            p.op('dve', [bk(0)], ['ms_pcol'], lambda E: E.tensor_copy(out=pcol[:, 0:1], in_=B[0][0:32, 0:1]))
            p.op('pe', ['ms_pcol', 'ms_Lst'], [bk(1)],
                 lambda E: E.matmul(B[1][0:1, 0:32], lhsT=pcol[:, 0:1], rhs=Lst[0:32, 0:32], start=True, stop=True))
            p.op('dve', [bk(1)], ['ms_cnt'], lambda E: E.tensor_copy(out=cnt[:, :], in_=B[1][0:1, 0:32]))
            p.op('pe', ['ms_cnt', 'cst'], [bk(1)],
                 lambda E: E.matmul(B[1][:, 0:32], lhsT=ones[0:1, :], rhs=cnt[0:1, :], start=True, stop=True))
            p.op('dve', [bk(1)], ['ms_psbc'], lambda E: E.tensor_copy(out=psbc[:, :], in_=B[1][:, 0:32]))
            p.op('pe', ['ms_pcol', 'cst'], [bk(0)],
                 lambda E: E.matmul(B[0][0:32, 0:1], lhsT=cst[0:32, 384:416], rhs=pcol[:, 0:1], start=True, stop=True))
            p.op('dve', [bk(0)], ['ms_pcol2'], lambda E: E.tensor_copy(out=pcol[:, 1:2], in_=B[0][0:32, 0:1]))
            p.op('dve', ['ms_pcol2', 'cst'], ['ms_cmp'],
                 lambda E: E.tensor_scalar(out=cmp_[:, :], in0=blk512[0:32, :], scalar1=pcol[:, 1:2], scalar2=None, op0=ALU.is_ge))
            p.op('pe', ['ms_cmp', 'cst'], [bk(0)],
                 lambda E: E.matmul(B[0][0:1, 0:128], lhsT=ones[0:32, 0:1], rhs=cmp_[:, :], start=True, stop=True))
            p.op('dve', [bk(0)], ['ms_berow'],
                 lambda E: E.tensor_scalar(out=berow[:, :], in0=B[0][0:1, 0:128], scalar1=31.0, scalar2=None, op0=ALU.min))
            p.op('pe', ['ms_berow', 'cst'], [bk(0)],
                 lambda E: E.matmul(B[0][:, 0:128], lhsT=ones[0:1, :], rhs=berow[0:1, :], start=True, stop=True))
            p.op('dve', [bk(0)], ['ms_bebc'], lambda E: E.tensor_copy(out=be_bc[:, :], in_=B[0][:, 0:NB]))
            p.op('dve', ['ms_bebc', 'cst'], ['ms_oh'],
                 lambda E: E.tensor_scalar(out=OHall[:, :], in0=be_bc[0:32, :], scalar1=iota_p[0:32, :], scalar2=None, op0=ALU.is_equal))
            p.op('dve', ['ms_bebc', 'cst'], ['ms_tmpf'],
                 lambda E: E.tensor_scalar(out=tmpf[:, 0:NB], in0=be_bc[:, :], scalar1=128.0, scalar2=iota_p[:, :], op0=ALU.mult, op1=ALU.add))
            p.op('dve', ['ms_tmpf'], ['ms_idxw'], lambda E: E.tensor_copy(out=idxw[:, :, 0], in_=tmpf[:, 0:NB]))
            p.op('dve', [], ['ms_Msum'], lambda E: E.memset(Msum[:, :], 0.0))
            for jj, t in enumerate(tiles):
                s2 = jj % 2
                tok = slice(t * 128, (t + 1) * 128)
                lgj, Mj, Gj = Lall[:, t, :], Mall[:, t, :], Gall[:, t, :]

                def mmp(E):
                    E.matmul(B[3 + s2][:, 0:32], lhsT=Lst[:, :], rhs=Mj, start=True, stop=False)
                    return E.matmul(B[3 + s2][:, 0:32], lhsT=ones[:, :], rhs=Msum[:, :], start=False, stop=True)
                p.op('pe', [('ms_M', t), 'ms_Msum', 'ms_Lst', 'cst'], [bk(3 + s2)], mmp)
                p.op('dve', [bk(3 + s2), 'ms_psbc'], [('ms_pos', s2)],
                     lambda E: E.tensor_tensor(out=pos[s2][:, :], in0=B[3 + s2][:, 0:32], in1=psbc[:, :], op=ALU.add))
                p.op('pool', [('ms_M', t), 'ms_Msum'], ['ms_Msum'],
                     lambda E: E.tensor_tensor(out=Msum[:, :], in0=Msum[:, :], in1=Mj, op=ALU.add))
                for k in range(4):
                    p.op('dve', [('ms_L', t), ('ms_mx', t)], [('ms_ohk', s2)],
                         lambda E: E.tensor_scalar(out=oh[s2][:, :], in0=lgj, scalar1=mxall[:, t, k:k + 1], scalar2=None, op0=ALU.is_equal))
                    p.op('dve', [('ms_ohk', s2), ('ms_pos', s2)], [('ms_tq', s2)],
                         lambda E: E.tensor_tensor(out=tq[s2][:, :], in0=oh[s2][:, :], in1=pos[s2][:, :], op=ALU.mult))
                    p.op('dve', [('ms_tq', s2)], [('ms_sl', s2, k)],
                         lambda E: E.tensor_reduce(out=sl[s2][:, k:k + 1], in_=tq[s2][:, :], axis=AX.X, op=ALU.add))
                    p.op('dve', [('ms_ohk', s2), ('ms_G', t)], [('ms_tq', s2)],
                         lambda E: E.tensor_tensor(out=tq[s2][:, :], in0=oh[s2][:, :], in1=Gj, op=ALU.mult))
                    p.op('dve', [('ms_tq', s2)], [('ms_gk', t, k)],
                         lambda E: E.tensor_reduce(out=gk[:, t, k:k + 1], in_=tq[s2][:, :], axis=AX.X, op=ALU.add))
                p.op('dve', [('ms_sl', s2, k) for k in range(4)], [('ms_slot', t)],
                     lambda E: E.tensor_copy(out=slot_i[:, t, :], in_=sl[s2][:, :]))
                p.dma('sp', 'ms_hbl%d' % s2, [('HB', t)], [('ms_hb', s2)], [(hb[s2][:, :], HB[tok, :])])
                p.idma('scat', [('ms_hb', s2), ('ms_slot', t), 'XBZ'], [('XB', t)],
                       [dict(out=XB[:, :], out_offset=bass.IndirectOffsetOnAxis(ap=slot_i[:, t, k:k + 1], axis=0),
                             in_=hb[s2][:, :], in_offset=None) for k in range(4)])
            p.barrier()
        if c.cfg.get('moe_phases', 3) < 2:
            return
        with ExitStack() as es:
            c.es = es
            w1 = [sb(c, 'me_w1%d' % i, [128, 8, 2048], BF16) for i in range(2)]
            w2 = [sb(c, 'me_w2%d' % i, [128, 8, 1024], BF16) for i in range(2)]
            xb = [sb(c, 'me_xb%d' % i, [128, 4, 1024], BF16) for i in range(2)]
            xT = [sb(c, 'me_xT%d' % i, [128, 8, 512], BF16) for i in range(2)]
            aT = sb(c, 'me_aT', [128, 8, 512], BF16)
            yb = [sb(c, 'me_yb%d' % i, [128, 1024], BF16) for i in range(2)]
            b1 = sb(c, 'me_b1', [128, 32, 16], F32)
            b2 = sb(c, 'me_b2', [32, 1024], BF16)
            b2f = sb(c, 'me_b2f', [32, 1024], F32)
            ohr = [sb(c, 'me_ohr%d' % i, [128, 32], F32) for i in range(2)]
            b1t = sb(c, 'me_b1t', [128, 32, 16], F32)
            b1s = [sb(c, 'me_b1s%d' % i, [128, 16], F32) for i in range(2)]
            ohb = [sb(c, 'me_ohb%d' % i, [32, 128], BF16) for i in range(2)]
            gg = [sb(c, 'me_gg%d' % i, [128, 512], F32) for i in range(2)]
            sg = [sb(c, 'me_sg%d' % i, [128, 512], F32) for i in range(2)]
            ll = [sb(c, 'me_ll%d' % i, [128, 512], F32) for i in range(2)]
            p.dma('sp', 'me_b2', [], ['me_b2f'], [(b2f[:, :], I['moe_b2'][l])])
            p.op('dve', ['me_b2f'], ['me_b2'], lambda E: E.tensor_copy(out=b2[:, :], in_=b2f[:, :]))
            p.dma('sp', 'me_b1', [], ['me_b1'],
                  [(b1[:, e, :], I['moe_b1'][l, e, :].rearrange("(j p) -> p j", p=128)) for e in range(32)], slow=True)
            wbk = [('WB', l, e) for e in range(32)]
            ei = 0
            for i in range(NB):
                ws = i % 2
                wdeps = (wbk if i < 2 else []) + ['ms_idxw']
                p.idma('gw1%d' % ws, wdeps, [('me_w1', ws)],
                       [dict(out=w1[ws][:, :, :].rearrange("p k n -> p (k n)"), out_offset=None, in_=W1f[:, :],
                             in_offset=bass.IndirectOffsetOnAxis(ap=idxw[:, i, 0:1], axis=0))])
                p.idma('gw2%d' % ws, wdeps, [('me_w2', ws)],
                       [dict(out=w2[ws][:, :, :].rearrange("p k n -> p (k n)"), out_offset=None, in_=W2f[:, :],
                             in_offset=bass.IndirectOffsetOnAxis(ap=idxw[:, i, 0:1], axis=0))])
                xbk = [('XB', t) for t in tiles] if i == 0 else []
                p.dma('sp', 'me_xb%d' % ws, xbk, [('me_xb', ws)],
                      [(xb[ws][:, :, :], XB[i * 512:(i + 1) * 512, :].rearrange("(a p) f -> p a f", p=128))])
                for a in range(4):
                    tb = 4 + (i * 4 + a) % 2
                    tpv = bank16(c, tb)

                    def trx(E):
                        ins = None
                        for k in range(8):
                            ins = E.transpose(tpv[:, k, :], xb[ws][:, a, k * 128:(k + 1) * 128], c.ident[:, :])
                        return ins
                    p.op('pe', [('me_xb', ws), 'ident'], [bk(tb)], trx)
                    if a % 2 == 0:
                        p.op('act', [bk(tb)], [('me_xT', ws, a)],
                             lambda E: E.activation(out=xT[ws][:, :, a * 128:(a + 1) * 128], in_=tpv[:, :, :], func=AF.Copy))
                    else:
                        p.op('dve', [bk(tb)], [('me_xT', ws, a)],
                             lambda E: E.tensor_copy(out=xT[ws][:, :, a * 128:(a + 1) * 128], in_=tpv[:, :, :]))
                xTk = [('me_xT', ws, a) for a in range(4)]
                p.op('dve', ['ms_bebc', 'cst'], [('me_ohr', ws)],
                     lambda E: E.tensor_scalar(out=ohr[ws][:, :], in0=iota_f, scalar1=be_bc[:, i:i + 1], scalar2=None, op0=ALU.is_equal))
                p.op('dve', [('me_ohr', ws), 'me_b1'], ['me_b1t'],
                     lambda E: E.tensor_tensor(out=b1t[:, :, :], in0=b1[:, :, :], in1=ohr[ws][:, :].unsqueeze(2).to_broadcast([128, 32, 16]),
                                               op=ALU.mult))
                p.op('dve', ['me_b1t'], [('me_b1s', ws)],
                     lambda E: E.tensor_reduce(out=b1s[ws][:, :], in_=b1t[:, :, :].rearrange("p e j -> p j e"), axis=AX.X, op=ALU.add))
                p.op('dve', ['ms_oh', 'cst'], [('me_ohb', ws)],
                     lambda E: E.tensor_scalar(out=ohb[ws][:, :], in0=ones[0:32, :], scalar1=OHall[:, i:i + 1], scalar2=None, op0=ALU.mult))
                for fc in range(8):
                    s2 = ei % 2
                    ei += 1
                    bg, bl = s2 * 2, s2 * 2 + 1

                    def mm1(E):
                        ins = None
                        for k in range(8):
                            E.matmul(B[bg][:, :], lhsT=w1[ws][:, k, fc * 128:(fc + 1) * 128], rhs=xT[ws][:, k, :], start=(k == 0), stop=(k == 7))
                        for k in range(8):
                            ins = E.matmul(B[bl][:, :], lhsT=w1[ws][:, k, 1024 + fc * 128:1024 + (fc + 1) * 128], rhs=xT[ws][:, k, :],
                                           start=(k == 0), stop=(k == 7))
                        return ins
                    p.op('pe', xTk + [('me_w1', ws)], [bk(bg), bk(bl)], mm1)
                    p.op('dve', [bk(bg), ('me_b1s', ws)], [('me_gg', s2)],
                         lambda E: E.tensor_scalar(out=gg[s2][:, :], in0=B[bg][:, :], scalar1=b1s[ws][:, fc:fc + 1], scalar2=7.0,
                                                   op0=ALU.add, op1=ALU.min))
                    p.op('act', [('me_gg', s2)], [('me_sg', s2)],
                         lambda E: E.activation(out=sg[s2][:, :], in_=gg[s2][:, :], func=AF.Sigmoid, scale=1.702))
                    p.op('dve', [bk(bl), ('me_b1s', ws)], [('me_ll', s2)],
                         lambda E: E.tensor_scalar(out=ll[s2][:, :], in0=B[bl][:, :], scalar1=b1s[ws][:, 8 + fc:9 + fc], scalar2=7.0,
                                                   op0=ALU.add, op1=ALU.min))
                    p.op('pool', [('me_ll', s2)], [('me_ll', s2)],
                         lambda E: E.tensor_scalar(out=ll[s2][:, :], in0=ll[s2][:, :], scalar1=-7.0, scalar2=1.0, op0=ALU.max, op1=ALU.add))
                    p.op('pool', [('me_gg', s2), ('me_sg', s2)], [('me_gg', s2)],
                         lambda E: E.tensor_tensor(out=gg[s2][:, :], in0=gg[s2][:, :], in1=sg[s2][:, :], op=ALU.mult))
                    p.op('dve', [('me_gg', s2), ('me_ll', s2)], [('me_aT', fc)],
                         lambda E: E.tensor_tensor(out=aT[:, fc, :], in0=gg[s2][:, :], in1=ll[s2][:, :], op=ALU.mult))
                aTk = [('me_aT', fc) for fc in range(8)]
                for a in range(4):
                    y2 = (i * 4 + a) % 2
                    for half in range(2):
                        bb = 6 + half

                        def mm2(E):
                            for k in range(8):
                                E.matmul(B[bb][:, :], lhsT=aT[:, k, a * 128:(a + 1) * 128], rhs=w2[ws][:, k, half * 512:(half + 1) * 512],
                                         start=(k == 0), stop=False)
                            return E.matmul(B[bb][:, :], lhsT=ohb[ws][:, :], rhs=b2[:, half * 512:(half + 1) * 512], start=False, stop=True)
                        p.op('pe', aTk + [('me_w2', ws), ('me_ohb', ws), 'me_b2'], [bk(bb)], mm2)
                        if half == 0:
                            p.op('act', [bk(bb)], [('me_yb', y2, half)],
                                 lambda E: E.activation(out=yb[y2][:, 0:512], in_=B[bb][:, :], func=AF.Copy))
                        else:
                            p.op('dve', [bk(bb)], [('me_yb', y2, half)], lambda E: E.tensor_copy(out=yb[y2][:, 512:1024], in_=B[bb][:, :]))
                    r0 = i * 512 + a * 128
                    p.dma('sp', 'me_yb%d' % y2, [('me_yb', y2, 0), ('me_yb', y2, 1)], [('YB', i, a)], [(YB[r0:r0 + 128, :], yb[y2][:, :])])
            p.barrier()
        if c.cfg.get('moe_phases', 3) < 3:
            return
        with ExitStack() as es:
            c.es = es
            yg = [[sb(c, 'mc_yg%d_%d' % (i, k), [128, 1024], BF16) for k in range(4)] for i in range(2)]
            xt = [sb(c, 'mc_xt%d' % i, [128, 1024], F32) for i in range(2)]
            ff = [sb(c, 'mc_ff%d' % i, [128, 1024], F32) for i in range(2)]
            for jj, t in enumerate(tiles):
                s2 = jj % 2
                r = 1 if t < 2 else 0
                tok = slice(t * 128, (t + 1) * 128)
                p.dma('sp', 'mc_xt%d' % s2, ['XOUT'], [('mc_xt', s2)], [(xt[s2][:, :], c.X1[tok, :])])
                ybk = [('YB', i, a) for i in range(NB) for a in range(4)] if jj < 2 else []
                p.idma('gy%d' % s2, [('ms_slot', t)] + ybk, [('mc_yg', s2)],
                       [dict(out=yg[s2][k][:, :], out_offset=None, in_=YB[:, :],
                             in_offset=bass.IndirectOffsetOnAxis(ap=slot_i[:, t, k:k + 1], axis=0)) for k in range(4)])
                p.op('dve', [('mc_yg', s2), ('ms_gk', t, 0)], [('mc_ff', s2)],
                     lambda E: E.tensor_scalar(out=ff[s2][:, :], in0=yg[s2][0][:, :], scalar1=gk[:, t, 0:1], scalar2=None, op0=ALU.mult))
                for k in range(1, 4):
                    p.op('dve', [('mc_yg', s2), ('ms_gk', t, k), ('mc_ff', s2)], [('mc_ff', s2)],
                         lambda E: E.scalar_tensor_tensor(out=ff[s2][:, :], in0=yg[s2][k][:, :], scalar=gk[:, t, k:k + 1], in1=ff[s2][:, :],
                                                          op0=ALU.mult, op1=ALU.add))
                p.op('pool', [('mc_ff', s2), 'e_bc'], [('mc_ff', s2)],
                     lambda E: E.tensor_tensor(out=ff[s2][:, :], in0=ff[s2][:, :], in1=c.e_ag[:, r, :], op=ALU.mult))
                p.op('dve', [('mc_ff', s2), ('mc_xt', s2)], [('mc_xt', s2)],
                     lambda E: E.scalar_tensor_tensor(out=xt[s2][:, :], in0=xt[s2][:, :], scalar=ALPHA, in1=ff[s2][:, :],
                                                      op0=ALU.mult, op1=ALU.add))
                ln_affine_store(c, xt[s2], ('mc_xt', s2), c.e_gb, 'e_bc', dst_fn(t), s2)
            p.barrier()


def make_rope():
    rows = NLAT // 64
    row = np.repeat(np.arange(rows, dtype=np.float32), 64)
    col = np.tile(np.arange(64, dtype=np.float32), rows)
    inv = (np.float32(10000.0) ** (-np.arange(8, dtype=np.float32) / np.float32(8))).astype(np.float32)
    ang = np.concatenate([row[:, None] * inv, col[:, None] * inv], -1).astype(np.float32)
    return np.concatenate([np.cos(ang), np.sin(ang)], -1).astype(np.float32)


def make_hyena_consts():
    import ml_dtypes
    bf = ml_dtypes.bfloat16
    N = 16384
    out = {}
    a = np.arange(128, dtype=np.float64)
    f = np.arange(128, dtype=np.float64)
    ang = 2 * np.pi * np.outer(a, f) / 128
    d1 = np.zeros((128, 2, 2, 128), np.float64)
    d1[:, 0, 0, :] = np.cos(ang)
    d1[:, 0, 1, :] = -np.sin(ang)
    for aa in range(4):
        for ff_ in range(4):
            d1[aa, 1, 0, ff_] = np.cos(2 * np.pi * aa * ff_ / 4)
            d1[aa, 1, 1, ff_] = -np.sin(2 * np.pi * aa * ff_ / 4)
    out['hy_dft1'] = d1.astype(np.float32).astype(bf)
    e3 = np.zeros((128, 3, 128), np.float64)
    e3[:, 0, :] = np.cos(ang)
    e3[:, 1, :] = np.sin(ang)
    e3[:, 2, :] = -np.sin(ang)
    out['hy_e3'] = e3.astype(np.float32).astype(bf)
    f1 = np.arange(128)[:, None, None]
    b = np.arange(128)[None, :, None]
    f2 = np.arange(128)[None, None, :]
    th = 2 * np.pi * ((b * (f1 + 128 * f2)) % N) / N
    tw2 = np.stack([np.cos(th), -np.sin(th), np.sin(th)], axis=2)
    out['hy_tw2'] = tw2.astype(np.float32).astype(bf)
    bb = np.arange(128)[:, None, None]
    ff = np.arange(128)[None, :, None]
    aa = np.arange(64)[None, None, :]
    ps_ = 2 * np.pi * (((128 * aa + bb) * ff) % N) / N
    twf = np.stack([np.cos(ps_), -np.sin(ps_)], axis=2)
    out['hy_twf'] = twf.astype(np.float32).astype(bf)
    f1c = np.arange(4)[:, None, None]
    thc = 2 * np.pi * ((b * (f1c + 4 * f2)) % 512) / 512
    out['hy_tw2c'] = np.stack([np.cos(thc), -np.sin(thc), np.sin(thc)], axis=2).astype(np.float32).astype(bf)
    ffc = np.arange(4)[None, :, None]
    aac = np.arange(2)[None, None, :]
    psc = 2 * np.pi * (((128 * aac + bb) * ffc) % 512) / 512
    out['hy_twfc'] = np.stack([np.cos(psc), -np.sin(psc)], axis=2).astype(np.float32).astype(bf)
    deltas = np.abs(np.linspace(math.log(1e-2) / 1.5, math.log(1e-2) / 0.3, 512, dtype=np.float32))
    out['hy_negdelta'] = (-deltas).astype(np.float32)[None, :]

    def feats(L, s):
        s = np.asarray(s)
        t = np.linspace(0.0, 1.0, L, dtype=np.float32)[s][:, None]
        w = (np.float32(2 * math.pi) * np.arange(L, dtype=np.float32) / np.float32(L))[s][:, None]
        fq = np.linspace(1e-4, 15, 16, dtype=np.float32)
        z = np.concatenate([t, np.cos(fq * w), -np.sin(fq * w)], -1).astype(np.float32)
        return z, t[:, 0]
    BIG = 1e4
    L = 8192
    tau = np.arange(N)
    s = np.where(tau < L, tau, N - tau)
    s[L] = 0
    z, t = feats(L, s)
    out['hy_feat0'] = np.ascontiguousarray(z.T)
    out['hy_tvec0'] = t[None, :].astype(np.float32)
    L = 256
    tau = np.arange(512)
    s = np.where(tau < L, tau, 512 - tau)
    s[256] = 0
    z, t = feats(L, s)
    out['hy_feat1'] = np.ascontiguousarray(z.T)
    out['hy_tvec1'] = t[None, :].astype(np.float32)
    return out


def make_consts():
    cst = np.zeros((128, 1024), np.float32)
    cst[:, 0:128] = np.eye(128)
    U = (np.arange(128)[:, None] <= np.arange(128)[None, :]).astype(np.float32)
    cst[:, 128:256] = U / 16.0
    cst[:, 256:384] = U.T / 16.0
    cst[:, 384:512] = U
    cst[:, 512:640] = U.T
    cst[:, 640:768] = 1.0
    cst[:, 768:800] = np.arange(32, dtype=np.float32)[None, :]
    cst[:, 800] = np.arange(128, dtype=np.float32)
    cst[:, 896:1024] = 512.0 * np.arange(128, dtype=np.float32)[None, :]
    return cst


def build_program(cfg):
    nc = bass.Bass("TRN2", target_bir_lowering=False)
    es = ExitStack()
    c = Ctx()
    c.nc, c.es, c.cfg = nc, es, cfg
    c.dbg = set(cfg.get('dbg', []))
    c.p = Prog(nc, es)
    p = c.p
    layers = cfg.get('layers', [0, 1])
    stages = cfg.get('stages', ['adaln', 'proj'])

    def ext(name, shape, dt=F32):
        return nc.dram_tensor(name, list(shape), dt, kind="ExternalInput").ap()

    I = {}
    I['x'] = ext('x', [NLAT, D])
    I['ctx'] = ext('ctx', [NCTX, D])
    I['cc'] = ext('cc', [2, D])
    I['ada_w'] = ext('ada_w', [DEPTH, D, 6 * D])
    I['ada_b'] = ext('ada_b', [DEPTH, 6 * D])
    I['w_in'] = ext('w_in', [DEPTH, D, IN_TOTAL])
    I['cst'] = ext('cst', [128, 1024])
    for nm, shp in [('gla_wa2_f', [DEPTH, 16, 256]), ('gla_ba_f', [DEPTH, 256]), ('gla_wa2_b', [DEPTH, 16, 256]),
                    ('gla_ba_b', [DEPTH, 256]), ('gla_norm', [DEPTH, 128]),
                    ('mla_q_norm', [DEPTH, 384]), ('mla_w_uq', [DEPTH, 384, 768]), ('mla_kv_norm', [DEPTH, 256]),
                    ('mla_w_ukv', [DEPTH, 256, 1024]), ('rope', [NLAT, 32]),
                    ('hy_conv_w', [DEPTH, 3, 1536]), ('hy_conv_b', [DEPTH, 1536]), ('hy_w1', [DEPTH, 33, 64]),
                    ('hy_b1', [DEPTH, 64]), ('hy_w2', [DEPTH, 64, 64]), ('hy_b2', [DEPTH, 64]), ('hy_w3', [DEPTH, 64, 2048]),
                    ('hy_freq', [DEPTH, 64]), ('hy_bias', [DEPTH, 2, 512]),
                    ('hy_feat0', [33, 16384]), ('hy_tvec0', [1, 16384]), ('hy_feat1', [33, 512]), ('hy_tvec1', [1, 512]),
                    ('hy_negdelta', [1, 512]),
                    ('w_br_gla', [DEPTH, 512, D]), ('w_br_mla', [DEPTH, 512, D]), ('w_br_hy', [DEPTH, 512, D]),
                    ('w_out', [DEPTH, D, D]), ('ln1_g', [DEPTH, D]), ('ln1_b', [DEPTH, D]), ('ln2_g', [DEPTH, D]),
                    ('ln2_b', [DEPTH, D]), ('router_w', [DEPTH, D, 32]), ('router_b', [DEPTH, 32]),
                    ('moe_b1', [DEPTH, 32, 2048]), ('moe_b2', [DEPTH, 32, D])]:
        I[nm] = ext(nm, shp)
    for nm, shp in [('hy_dft1', [128, 2, 2, 128]), ('hy_e3', [128, 3, 128]), ('hy_tw2', [128, 128, 3, 128]),
                    ('hy_twf', [128, 128, 2, 64]), ('hy_tw2c', [4, 128, 3, 128]), ('hy_twfc', [128, 4, 2, 2])]:
        I[nm] = ext(nm, shp, BF16)
    if 'moe' in stages:
        I['moe_w1'] = ext('moe_w1', [DEPTH, 32, D, 2048])
        I['moe_w2'] = ext('moe_w2', [DEPTH, 32, D, D])
        c.WB1 = [nc.dram_tensor('WB1_%d' % l, [32, D, 2048], BF16).ap() for l in range(DEPTH)]
        c.WB2 = [nc.dram_tensor('WB2_%d' % l, [32, D, D], BF16).ap() for l in range(DEPTH)]
        c.XB = nc.dram_tensor('XB', [98 * 512, D], BF16).ap()
        c.YB = nc.dram_tensor('YB', [98 * 512, D], BF16).ap()
        c.HB = nc.dram_tensor('HB', [NT, D], BF16).ap()
    c.inp = I
    c.out = nc.dram_tensor('out', [NLAT, D], F32, kind="ExternalOutput").ap()

    c.modv = [dram(c, 'modv%d' % l, [2, 6 * D], F32) for l in range(DEPTH)]
    c.scr = []
    for l in range(DEPTH):
        S = {}
        for name, off, ncols in FM_GROUPS:
            S[name] = dram(c, '%s%d' % (name, l), [ncols, NT], F32 if name in ('AFT', 'ABT') else BF16)
        for name, off, ncols in TM_GROUPS:
            S[name] = dram(c, '%s%d' % (name, l), [NT, ncols], F32 if name == 'MKR' else BF16)
        for name in ('OGT', 'OMT', 'OHT'):
            S[name] = dram(c, '%s%d' % (name, l), [512, NT], BF16)
        c.scr.append(S)
    c.X1 = dram(c, 'X1', [NT, D], F32)
    c.X2 = dram(c, 'X2', [NT, D], F32)
    c.KpT = dram(c, 'KpT', [8, 97, NT], BF16)
    c.QpT = dram(c, 'QpT', [8, 97, NT], BF16)
    c.VpD = dram(c, 'VpD', [8, NT, 65], BF16)
    c.HYC = dram(c, 'HYC', [NT, 1536], BF16)
    c.Z2 = dram(c, 'Z2', [NT, 512], BF16)
    c.OH = dram(c, 'OH', [NT, 512], BF16)
    c.KTD = [dram(c, 'KTD0', [16384, 1024], BF16), dram(c, 'KTD1', [512, 1024], BF16)]
    c.KS = [dram(c, 'KS%d' % o, [128, 128, 2, 512], BF16) for o in range(2)]
    c.X1D = dram(c, 'X1D', [128, 128, 2, 512], BF16)
    c.QD = dram(c, 'QD', [128, 128, 2, 512], BF16)
    c.SCL = [dram(c, 'SCL%d' % j, [1, 1024], F32) for j in range(2)]

    c.cst = sb(c, 'cst_sb', [128, 1024], F32)
    c.ident = sb(c, 'ident16', [128, 128], BF16)
    c.cT = sb(c, 'cTsb', [128, 8, 2], F32)
    c.modT = sb(c, 'modT', [128, 2, 6, 8], F32)
    c.bank = [ps(c, 'bank%d' % i, [128, 512], F32) for i in range(8)]

    p.dma('sp', 'cst', [], ['cst'], [(c.cst[:, :], I['cst'][:, :])])
    p.op('dve', ['cst'], ['ident'], lambda E: E.tensor_copy(out=c.ident[:, :], in_=c.cst[:, 0:128]))
    p.dma('sp', 'cT', [], ['cT'],
          [(c.cT[:, :, r], I['cc'][r, :].rearrange("(k p) -> p k", p=128)) for r in range(2)], slow=True)
    p.op('act', ['cT'], ['cT'], lambda E: E.activation(out=c.cT[:, :, :], in_=c.cT[:, :, :], func=AF.Silu))

    for l in layers:
        if 'adaln' in stages:
            stage_adaln(c, l)
    if 'moe' in stages:
        for l in layers:
            moe_precast(c, l)
        if cfg.get('moe_mode', 'sparse') == 'sparse':
            c.zt = sb(c, 'zero_t', [128, 2048], BF16)
            p.op('pool', [], ['zero_t'], lambda E: E.memset(c.zt[:, :], 0.0))
            p.dma('sp', 'xbzero', ['zero_t'], ['XBZ'],
                  [(c.XB[i * 256:(i + 1) * 256, :].rearrange("(p a) f -> p (a f)", p=128), c.zt[:, :]) for i in range(196)])
    for l in layers:
        last = (l == DEPTH - 1)

        def xsrc(t, l=l):
            if l == 0:
                if t < 2:
                    return I['ctx'][t * 128:(t + 1) * 128, :]
                return I['x'][(t - 2) * 128:(t - 1) * 128, :]
            return c.X2[t * 128:(t + 1) * 128, :]
        if 'proj' in stages:
            stage_proj(c, l, xsrc)
        if 'gla' in stages:
            stage_gla(c, l)
        if 'mla' in stages:
            stage_mla(c, l, ctx_q=not last)
        if 'hyena' in stages:
            stage_hyena(c, l, with_ctx=not last)
        tiles = list(range(2, NTILE)) if last else list(range(NTILE))
        if 'merge' in stages:
            load_mod(c, l)
            stage_merge(c, l, xsrc, tiles)
        if 'moe' in stages:
            load_mod(c, l)

            def dst_fn(t, last=last):
                if last:
                    return [c.out[(t - 2) * 128:(t - 1) * 128, :]]
                return [c.X2[t * 128:(t + 1) * 128, :]]
            if cfg.get('moe_mode', 'sparse') == 'sparse':
                stage_moe2(c, l, tiles, dst_fn)
            else:
                stage_moe(c, l, tiles, dst_fn)
    p.finish('sp')
    return nc, c


ALL_STAGES = ['adaln', 'proj', 'gla', 'mla', 'hyena', 'merge', 'moe']
WEIGHT_KEYS = ['ada_w', 'ada_b', 'w_in', 'gla_wa2_f', 'gla_ba_f', 'gla_wa2_b', 'gla_ba_b', 'gla_norm', 'mla_q_norm', 'mla_w_uq',
               'mla_kv_norm', 'mla_w_ukv', 'hy_conv_w', 'hy_conv_b', 'hy_w1', 'hy_b1', 'hy_w2', 'hy_b2', 'hy_w3', 'hy_freq',
               'hy_bias', 'w_br_gla', 'w_br_mla', 'w_br_hy', 'w_out', 'ln1_g', 'ln1_b', 'ln2_g', 'ln2_b', 'router_w', 'router_b',
               'moe_w1', 'moe_b1', 'moe_w2', 'moe_b2']


def make_in_map(inputs, b, with_moe=True):
    f32 = lambda a: np.ascontiguousarray(np.asarray(a, dtype=np.float32))
    im = dict(x=f32(inputs['x'][b]), ctx=f32(inputs['ctx'][b]),
              cc=f32(np.stack([np.asarray(inputs['c'])[b], np.asarray(inputs['c_ctx'])])),
              cst=make_consts(), rope=make_rope())
    im.update(make_hyena_consts())
    for k in WEIGHT_KEYS:
        if not with_moe and k in ('moe_w1', 'moe_w2'):
            continue
        im[k] = f32(inputs[k])
    return im


def kernel(**inputs):
    nc, c = build_program(dict(layers=[0, 1], stages=ALL_STAGES))
    nb = np.asarray(inputs['x']).shape[0]
    in_maps = [make_in_map(inputs, b) for b in range(nb)]
    res = run_bass_kernel_spmd(nc, in_maps, core_ids=list(range(nb)))
    out = np.stack([np.asarray(res.results[b]['out']) for b in range(nb)], 0)
    return out.astype(np.float32)
```

```python
import math
from contextlib import ExitStack

import numpy as np
import concourse.bass as bass
import concourse.mybir as mybir
from concourse.bass_utils import run_bass_kernel_spmd

F32 = mybir.dt.float32
BF16 = mybir.dt.bfloat16
AF = mybir.ActivationFunctionType
ALU = mybir.AluOpType
AX = mybir.AxisListType

D = 1024
NCTX = 256
NLAT = 8192
NT = NCTX + NLAT
NTILE = NT // 128
DEPTH = 2
IN_TOTAL = 6848
LN_EPS = 1e-5
RMS_EPS = 1e-6
ALPHA = (2 * DEPTH) ** 0.25
O_GK, O_GV, O_GAF, O_GAB, O_MKVA, O_MKR, O_GQ, O_GR, O_MQA, O_HY, O_GATES = (
    0, 256, 768, 784, 800, 1056, 1088, 1344, 1856, 2240, 3776)


class Prog:
    def __init__(self, nc, es):
        self.nc = nc
        self.es = es
        self.E = dict(pe=nc.tensor, act=nc.scalar, dve=nc.vector, pool=nc.gpsimd, sp=nc.sync)
        self.sems = {}
        self.cnt = {}
        self.seen = {e: {} for e in self.E}
        self.st = {}
        self.n_ops = 0
        self.alias = {}
        self.free_slots = []
        self.n_slots = 0
        self.persistent = set(['wcast', 'xbzero'])

    def sem(self, key):
        if key not in self.sems:
            self.sems[key] = self.es.enter_context(self.nc.semaphore("s%d" % len(self.sems)))
            self.cnt[key] = 0
        return self.sems[key]

    def _deps(self, reads, writes):
        deps = {}

        def add(k, v):
            if deps.get(k, 0) < v:
                deps[k] = v

        for r in reads:
            s = self.st.get(r)
            if s and s[0]:
                add(*s[0])
        for w in writes:
            s = self.st.get(w)
            if s:
                if s[0]:
                    add(*s[0])
                for k, v in s[1].items():
                    add(k, v)
        return deps

    def _wait(self, eng, deps):
        E = self.E[eng]
        seen = self.seen[eng]
        for k, v in deps.items():
            if k == 'pe' and eng == 'pe':
                continue
            if seen.get(k, 0) < v:
                E.wait_ge(self.sems[k], v)
                seen[k] = v

    def _commit(self, ev, reads, writes):
        k, v = ev
        for r in reads:
            s = self.st.setdefault(r, [None, {}])
            if s[1].get(k, 0) < v:
                s[1][k] = v
        for w in writes:
            self.st[w] = [ev, {}]

    def op(self, eng, reads, writes, fn):
        reads = list(reads)
        writes = list(writes)
        for r in reads:
            if isinstance(r, tuple) and r[0] == 'bank' and r not in writes:
                writes.append(r)
        self._wait(eng, self._deps(reads, writes))
        sem = self.sem(eng)
        ins = fn(self.E[eng])
        self.cnt[eng] += 1
        ins.then_inc(sem, 1)
        self._commit((eng, self.cnt[eng]), reads, writes)
        self.n_ops += 1

    def _slot(self, semkey):
        if semkey in self.persistent:
            return semkey
        if semkey not in self.alias:
            if self.free_slots:
                self.alias[semkey] = self.free_slots.pop()
            else:
                self.alias[semkey] = ('dsem', self.n_slots)
                self.n_slots += 1
        return self.alias[semkey]

    def idma(self, semkey, reads, writes, calls):
        reads = list(reads)
        writes = list(writes)
        self._wait('pool', self._deps(reads, writes))
        key = self._slot(semkey)
        sem = self.sem(key)
        for kw in calls:
            self.nc.gpsimd.indirect_dma_start(**kw).then_inc(sem, 16)
            self.cnt[key] += 16
        self._commit((key, self.cnt[key]), reads, writes)
        self.n_ops += len(calls)

    def dma(self, eng, semkey, reads, writes, pairs, slow=False):
        reads = list(reads)
        writes = list(writes)
        self._wait(eng, self._deps(reads, writes))
        semkey = self._slot(semkey)
        sem = self.sem(semkey)
        for o, i in pairs:
            if slow:
                self.E[eng].dma_start(out=o, in_=i, allow_slow_non_contiguous=True).then_inc(sem, 16)
            else:
                self.E[eng].dma_start(out=o, in_=i).then_inc(sem, 16)
            self.cnt[semkey] += 16
        self._commit((semkey, self.cnt[semkey]), reads, writes)
        self.n_ops += len(pairs)

    def pe_fence(self, ins):
        sem = self.sem('pe')
        self.cnt['pe'] += 1
        ins.then_inc(sem, 1)
        self.E['pe'].wait_ge(sem, self.cnt['pe'])
        self.seen['pe']['pe'] = self.cnt['pe']

    def barrier(self):
        deps = {k: v for k, v in self.cnt.items() if v > 0 and k not in self.persistent}
        for eng in self.E:
            self._wait(eng, dict(deps))
        self.free_slots = [('dsem', i) for i in range(self.n_slots)]
        self.alias = {}

    def finish(self, eng='sp'):
        deps = {}
        for k, c in self.cnt.items():
            if c > 0:
                deps[k] = c
        self._wait(eng, deps)


class Ctx:
    pass


_uid = [0]


def sb(c, name, shape, dt):
    _uid[0] += 1
    return c.es.enter_context(c.nc.sbuf_tensor("%s_u%d" % (name, _uid[0]), list(shape), dt))


def ps(c, name, shape, dt):
    return c.es.enter_context(c.nc.psum_tensor(name, list(shape), dt))


def bank16(c, i):
    return c.bank[i][:, :].bitcast(BF16).rearrange("p (a b) -> p a b", a=8)


def dram(c, name, shape, dt, out=False):
    kind = "ExternalOutput" if (out or name in c.dbg) else "Internal"
    return c.nc.dram_tensor(name, list(shape), dt, kind=kind).ap()


def stage_adaln(c, l):
    with ExitStack() as es:
        c.es = es
        c.adaw = [sb(c, 'adaw%d' % i, [128, 8, 512], F32) for i in range(2)]
        c.adab = [sb(c, 'adab%d' % i, [2, 512], F32) for i in range(2)]
        c.modrow = [sb(c, 'modrow%d' % i, [2, 512], F32) for i in range(2)]
        c.psA = [c.bank[0], c.bank[1]]
        _stage_adaln(c, l)
        c.p.barrier()


def _stage_adaln(c, l):
    p, nc = c.p, c.nc
    I = c.inp
    cT = c.cT
    modv = c.modv[l]
    for cb in range(12):
        slot = cb % 2
        wt = c.adaw[slot]
        p.dma('sp', 'adaw%d' % slot, [], [('adaw', slot)],
              [(wt[:, :, :], I['ada_w'][l, :, cb * 512:(cb + 1) * 512].rearrange("(k p) n -> p k n", p=128))])
        bt = c.adab[slot]
        p.dma('sp', 'adab%d' % slot, [], [('adab', slot)],
              [(bt[0:1, :], I['ada_b'][l:l + 1, cb * 512:(cb + 1) * 512]),
               (bt[1:2, :], I['ada_b'][l:l + 1, cb * 512:(cb + 1) * 512])])
        pt = c.psA[cb % 2]

        def mm(E, wt=wt, pt=pt):
            ins = None
            for k in range(8):
                ins = E.matmul(pt[0:2, :], lhsT=cT[:, k, :], rhs=wt[:, k, :], start=(k == 0), stop=(k == 7))
            return ins
        p.op('pe', [('adaw', slot), 'cT'], [('bank', cb % 2)], mm)
        mt = c.modrow[slot]
        p.op('dve', [('bank', cb % 2), ('adab', slot)], [('modrow', slot)],
             lambda E, mt=mt, pt=pt, bt=bt: E.tensor_tensor(out=mt[0:2, :], in0=pt[0:2, :], in1=bt[0:2, :], op=ALU.add))
        p.dma('sp', 'modrow%d' % slot, [('modrow', slot)], [('modv', l)],
              [(modv[0:2, cb * 512:(cb + 1) * 512], mt[0:2, :])])


def load_mod(c, l):
    p = c.p
    modv = c.modv[l]
    pairs = []
    for r in range(2):
        for g in range(6):
            pairs.append((c.modT[:, r, g, :], modv[r, g * 1024:(g + 1) * 1024].rearrange("(k p) -> p k", p=128)))
    p.dma('sp', 'modT', [('modv', l)], ['modT'], pairs, slow=True)
    p.op('dve', ['modT'], ['modT'],
         lambda E: E.tensor_scalar(out=c.modT[:, :, 1, :], in0=c.modT[:, :, 1, :], scalar1=1.0, scalar2=None, op0=ALU.add))
    p.op('dve', ['modT'], ['modT'],
         lambda E: E.tensor_scalar(out=c.modT[:, :, 4, :], in0=c.modT[:, :, 4, :], scalar1=1.0, scalar2=None, op0=ALU.add))


def ln_mod_tile(c, src_ap, r, gsh, gsc, hT, col0, hkey):
    p = c.p
    i = c.ln_i
    c.ln_i += 1
    s = i % 3
    xt = c.xt[s]
    st = c.lnst[s]
    p.dma('sp', 'xt%d' % s, [], [('xt', s)], [(xt[:, :], src_ap)])
    junk = c.junk[i % 2]
    p.op('act', [('xt', s)], [('junk', i % 2), ('lnst', s, 0)],
         lambda E: E.activation(out=junk[:, :], in_=xt[:, :], func=AF.Copy, accum_out=st[:, 0:1]))
    p.op('dve', [('lnst', s, 0)], [('lnst', s, 1)],
         lambda E: E.tensor_scalar(out=st[:, 1:2], in0=st[:, 0:1], scalar1=-1.0 / D, scalar2=None, op0=ALU.mult))
    p.op('act', [('xt', s), ('lnst', s, 1)], [('junk', i % 2), ('lnst', s, 2)],
         lambda E: E.activation(out=junk[:, :], in_=xt[:, :], func=AF.Square, bias=st[:, 1:2], scale=1.0,
                                accum_out=st[:, 2:3]))
    p.op('act', [('lnst', s, 2)], [('lnst', s, 3)],
         lambda E: E.activation(out=st[:, 3:4], in_=st[:, 2:3], func=AF.Ln, scale=1.0 / D, bias=LN_EPS))
    p.op('act', [('lnst', s, 3)], [('lnst', s, 4)],
         lambda E: E.activation(out=st[:, 4:5], in_=st[:, 3:4], func=AF.Exp, scale=-0.5))
    if c.cfg.get('lnsteps', 9) < 2:
        return
    xh = c.xh[i % 2]
    p.op('dve', [('xt', s), ('lnst', s, 1), ('lnst', s, 4)], [('xh', i % 2)],
         lambda E: E.tensor_scalar(out=xh[:, :], in0=xt[:, :], scalar1=st[:, 1:2], scalar2=st[:, 4:5],
                                   op0=ALU.add, op1=ALU.mult))
    if c.cfg.get('lnsteps', 9) < 3:
        return
    tp = c.tp[i % 2]

    def tr(E):
        ins = None
        for k in range(8):
            ins = E.transpose(tp[:, k, :], xh[:, k * 128:(k + 1) * 128], c.ident[:, :])
        return ins
    p.op('pe', [('xh', i % 2), 'ident'], [c.tpk[i % 2]], tr)
    if c.cfg.get('lnsteps', 9) < 4:
        return
    for k in range(8):
        eng = c.cfg.get('evac_eng') or ('act' if i % 2 == 0 else 'dve')
        if eng == 'act':
            p.op('act', [c.tpk[i % 2], 'modT'], [hkey + (k,)],
                 lambda E, k=k: E.activation(out=hT[:, k, col0:col0 + 128], in_=tp[:, k, :], func=AF.Identity,
                                             bias=c.modT[:, r, gsh, k:k + 1], scale=c.modT[:, r, gsc, k:k + 1]))
        else:
            p.op('dve', [c.tpk[i % 2], 'modT'], [hkey + (k,)],
                 lambda E, k=k: E.tensor_scalar(out=hT[:, k, col0:col0 + 128], in0=tp[:, k, :],
                                                scalar1=c.modT[:, r, gsc, k:k + 1],
                                                scalar2=c.modT[:, r, gsh, k:k + 1],
                                                op0=ALU.mult, op1=ALU.add))


FM_GROUPS = [
    ('KT', O_GK, 256), ('QT', O_GQ, 256), ('AFT', O_GAF, 16), ('ABT', O_GAB, 16),
    ('MKVAT', O_MKVA, 256), ('MQAT', O_MQA, 384),
]
TM_GROUPS = [
    ('V', O_GV, 512), ('MKR', O_MKR, 32), ('GR', O_GR, 512), ('HY', O_HY, 1536), ('G3', O_GATES, 3072),
]


def stage_proj(c, l, xsrc):
    with ExitStack() as es:
        c.es = es
        c.win = sb(c, 'win', [128, 8, IN_TOTAL], BF16)
        c.hT = [sb(c, 'hT%d' % i, [128, 8, 512], BF16) for i in range(2)]
        alloc_ln(c)
        c.o16 = [sb(c, 'o16_%d' % i, [128, 512], BF16) for i in range(4)]
        c.o32 = [sb(c, 'o32_%d' % i, [128, 512], F32) for i in range(4)]
        c.psB = [c.bank[i] for i in range(4)]
        load_mod(c, l)
        _stage_proj(c, l, xsrc)
        c.p.barrier()


def alloc_ln(c):
    c.xt = [sb(c, 'xt%d' % i, [128, D], F32) for i in range(3)]
    c.lnst = [sb(c, 'lnst%d' % i, [128, 8], F32) for i in range(3)]
    c.junk = [sb(c, 'junk%d' % i, [128, D], BF16) for i in range(2)]
    c.xh = [sb(c, 'xh%d' % i, [128, D], BF16) for i in range(2)]
    c.tp = [bank16(c, 4), bank16(c, 5)]
    c.tpk = [('bank', 4), ('bank', 5)]
    c.ln_i = 0


def _stage_proj(c, l, xsrc):
    p, nc = c.p, c.nc
    I = c.inp
    win = c.win
    for k in range(8):
        p.dma('pool', 'win', [], [('win', k)],
              [(win[:, k, :], I['w_in'][l, k * 128:(k + 1) * 128, :])])
    S = c.scr[l]
    blocks = [(0, 2)] + [(2 + 4 * i, 4) for i in range(16)]
    blocks = blocks[:c.cfg.get('nblk', 17)]
    ev = 0
    for bi, (t0, ntl) in enumerate(blocks):
        T = ntl * 128
        tok0 = t0 * 128
        hb = bi % 2
        hT = c.hT[hb]
        r = 1 if bi == 0 else 0
        for j in range(ntl):
            ln_mod_tile(c, xsrc(t0 + j), r, 0, 1, hT, j * 128, ('hT', hb))
        hkeys = [('hT', hb, k) for k in range(8)]
        wkeys = [('win', k) for k in range(8)]
        if c.cfg.get('nomm'):
            continue
        for name, off, ncols in FM_GROUPS:
            for m0 in range(0, ncols, 128):
                M = min(128, ncols - m0)
                pb = ev % 4
                pt = c.psB[pb]

                def mm(E, pt=pt, off=off, m0=m0, M=M, T=T):
                    ins = None
                    for k in range(8):
                        ins = E.matmul(pt[0:M, 0:T], lhsT=win[:, k, off + m0:off + m0 + M], rhs=hT[:, k, 0:T],
                                       start=(k == 0), stop=(k == 7))
                    return ins
                p.op('pe', hkeys + wkeys, [('bank', pb)], mm)
                fp32 = name in ('AFT', 'ABT')
                ob = ev % 4
                ot = (c.o32 if fp32 else c.o16)[ob]
                okey = ('o32' if fp32 else 'o16', ob)
                eng = 'act' if ev % 2 == 0 else 'dve'
                if eng == 'act':
                    p.op('act', [('bank', pb)], [okey],
                         lambda E, ot=ot, pt=pt, M=M, T=T: E.activation(out=ot[0:M, 0:T], in_=pt[0:M, 0:T], func=AF.Copy))
                else:
                    p.op('dve', [('bank', pb)], [okey],
                         lambda E, ot=ot, pt=pt, M=M, T=T: E.tensor_copy(out=ot[0:M, 0:T], in_=pt[0:M, 0:T]))
                p.dma('sp', 'o%s%d' % ('32' if fp32 else '16', ob), [okey], [(name, l)],
                      [(S[name][m0:m0 + M, tok0:tok0 + T], ot[0:M, 0:T])])
                ev += 1
        for name, off, ncols in TM_GROUPS:
            for n0 in range(0, ncols, 512):
                N = min(512, ncols - n0)
                for j in range(ntl):
                    pb = ev % 4
                    pt = c.psB[pb]

                    def mm(E, pt=pt, off=off, n0=n0, N=N, j=j):
                        ins = None
                        for k in range(8):
                            ins = E.matmul(pt[:, 0:N], lhsT=hT[:, k, j * 128:(j + 1) * 128],
                                           rhs=win[:, k, off + n0:off + n0 + N], start=(k == 0), stop=(k == 7))
                        return ins
                    p.op('pe', hkeys + wkeys, [('bank', pb)], mm)
                    fp32 = name == 'MKR'
                    ob = ev % 4
                    ot = (c.o32 if fp32 else c.o16)[ob]
                    okey = ('o32' if fp32 else 'o16', ob)
                    if name == 'G3':
                        p.op('act', [('bank', pb)], [okey],
                             lambda E, ot=ot, pt=pt, N=N: E.activation(out=ot[:, 0:N], in_=pt[:, 0:N], func=AF.Sigmoid))
                    elif ev % 2 == 0:
                        p.op('act', [('bank', pb)], [okey],
                             lambda E, ot=ot, pt=pt, N=N: E.activation(out=ot[:, 0:N], in_=pt[:, 0:N], func=AF.Copy))
                    else:
                        p.op('dve', [('bank', pb)], [okey],
                             lambda E, ot=ot, pt=pt, N=N: E.tensor_copy(out=ot[:, 0:N], in_=pt[:, 0:N]))
                    p.dma('sp', 'o%s%d' % ('32' if fp32 else '16', ob), [okey], [(name, l)],
                          [(S[name][tok0 + j * 128:tok0 + (j + 1) * 128, n0:n0 + N], ot[:, 0:N])])
                    ev += 1


def stage_gla(c, l):
    with ExitStack() as es:
        c.es = es
        _stage_gla(c, l)
        c.p.barrier()


def _stage_gla(c, l):
    p, nc = c.p, c.nc
    I = c.inp
    S = c.scr[l]
    NCH = NTILE
    qT = sb(c, 'g_qT', [128, NT], BF16)
    kT = sb(c, 'g_kT', [128, NT], BF16)
    v = sb(c, 'g_v', [128, NCH, 256], BF16)
    oacc = sb(c, 'g_oacc', [128, NCH, 256], F32)
    wa2 = sb(c, 'g_wa2', [16, 2, 256], F32)
    ba = sb(c, 'g_ba', [1, 2, 256], F32)
    normw = sb(c, 'g_normw', [128, 128], F32)
    aft = [sb(c, 'g_aft%d' % i, [16, 128], F32) for i in range(3)]
    g1 = [sb(c, 'g_g1%d' % i, [128, 128], F32) for i in range(2)]
    g2 = [sb(c, 'g_g2%d' % i, [128, 128], F32) for i in range(2)]
    e1 = [sb(c, 'g_e1%d' % i, [128, 128], F32) for i in range(2)]
    e2 = [sb(c, 'g_e2%d' % i, [128, 128], F32) for i in range(2)]
    qb = [sb(c, 'g_qb%d' % i, [128, 128], BF16) for i in range(2)]
    kb = [sb(c, 'g_kb%d' % i, [128, 128], BF16) for i in range(2)]
    kd = [sb(c, 'g_kd%d' % i, [128, 128], BF16) for i in range(2)]
    kdt = [sb(c, 'g_kdt%d' % i, [128, 128], BF16) for i in range(2)]
    am = [sb(c, 'g_am%d' % i, [128, 256], BF16) for i in range(2)]
    St = sb(c, 'g_S', [128, 128], F32)
    Sb = sb(c, 'g_Sb', [128, 128], BF16)
    rst = sb(c, 'g_rst', [128, NCH * 2], F32)
    rst2 = sb(c, 'g_rst2', [128, NCH * 2], F32)
    junk = sb(c, 'g_junk', [128, 128], BF16)
    osb = [sb(c, 'g_osb%d' % i, [128, 512], BF16) for i in range(2)]
    psG = c.bank[0][:, 0:128]
    psBt = [c.bank[1][:, 0:128], c.bank[2][:, 0:128]]
    psA = c.bank[3][:, 0:256]
    psK = c.bank[4][:, :].bitcast(BF16)[:, 0:128]
    psO = [c.bank[5][:, 0:256], c.bank[6][:, 0:256]]
    psS = c.bank[7][:, 0:128]
    c.tp = [bank16(c, 1), bank16(c, 2)]
    cst = c.cst
    U16 = [cst[:, 128:256], cst[:, 256:384]]
    MSK = [cst[:, 384:512], cst[:, 512:640]]
    ones = cst[:, 640:768]

    p.dma('sp', 'g_w', [], ['g_w'],
          [(wa2[:, 0, :], I['gla_wa2_f'][l]), (wa2[:, 1, :], I['gla_wa2_b'][l]),
           (ba[0:1, 0, :], I['gla_ba_f'][l:l + 1, :]), (ba[0:1, 1, :], I['gla_ba_b'][l:l + 1, :]),
           (normw[:, :], I['gla_norm'][l, :].partition_broadcast(128))])
    gr = v
    step = 0
    for hp in range(2):
        p.dma('sp', 'g_q', [], ['g_qT'], [(qT[:, :], S['QT'][hp * 128:(hp + 1) * 128, :])])
        p.dma('sp', 'g_k', [], ['g_kT'], [(kT[:, :], S['KT'][hp * 128:(hp + 1) * 128, :])])
        p.dma('sp', 'g_v', [], ['g_v'],
              [(v[:, :, :], S['V'][:, hp * 256:(hp + 1) * 256].rearrange("(n p) c -> p n c", p=128))])
        for dirn in range(2):
            p.op('dve', [], ['g_S'], lambda E: E.memset(St[:, :], 0.0))
            p.op('dve', [], ['g_Sb'], lambda E: E.memset(Sb[:, :], 0.0))
            order = list(range(NCH)) if dirn == 0 else [1, 0] + list(range(NCH - 1, 1, -1))
            last = 127 if dirn == 0 else 0
            gsrc = S['AFT'] if dirn == 0 else S['ABT']
            for n in order[:c.cfg.get('gsteps', 999)]:
                t0 = n * 128
                s2 = step % 2
                s3 = step % 3
                step += 1
                a_t = aft[s3]
                p.dma('sp', 'g_aft%d' % s3, [], [('g_aft', s3)], [(a_t[:, :], gsrc[:, t0:t0 + 128])])

                def mmg(E, a_t=a_t, dirn=dirn, hp=hp):
                    E.matmul(psG[:, :], lhsT=a_t[0:16, :], rhs=wa2[0:16, dirn, hp * 128:(hp + 1) * 128],
                             start=True, stop=False)
                    return E.matmul(psG[:, :], lhsT=ones[0:1, :], rhs=ba[0:1, dirn, hp * 128:(hp + 1) * 128],
                                    start=False, stop=True)
                p.op('pe', [('g_aft', s3), 'g_w', 'cst'], [('bank', 0)], mmg)
                if c.cfg.get('gsub', 99) < 1:
                    continue
                p.op('act', [('bank', 0)], [('g_g1', s2)],
                     lambda E, s2=s2: E.activation(out=g1[s2][:, :], in_=psG[:, :], func=AF.Exp, scale=-1.0))
                p.op('act', [('g_g1', s2)], [('g_g2', s2)],
                     lambda E, s2=s2: E.activation(out=g2[s2][:, :], in_=g1[s2][:, :], func=AF.Ln, bias=1.0, scale=1.0))
                if c.cfg.get('gsub', 99) < 2:
                    continue
                pB = psBt[s2]
                p.op('pe', [('g_g2', s2), 'cst'], [('bank', 1 + s2)],
                     lambda E, s2=s2, pB=pB, dirn=dirn: E.matmul(pB[:, :], lhsT=g2[s2][:, :], rhs=U16[dirn],
                                                                 start=True, stop=True))
                p.op('act', [('bank', 1 + s2)], [('g_e1', s2)],
                     lambda E, s2=s2, pB=pB: E.activation(out=e1[s2][:, :], in_=pB[:, :], func=AF.Exp, scale=-1.0))
                p.op('act', [('bank', 1 + s2)], [('g_e2', s2)],
                     lambda E, s2=s2, pB=pB: E.activation(out=e2[s2][:, :], in_=pB[:, :], func=AF.Exp, scale=1.0))
                if c.cfg.get('gsub', 99) < 3:
                    continue
                p.op('dve', ['g_qT', ('g_e1', s2)], [('g_qb', s2)],
                     lambda E, s2=s2, t0=t0: E.scalar_tensor_tensor(out=qb[s2][:, :], in0=qT[:, t0:t0 + 128], scalar=0.125,
                                                                    in1=e1[s2][:, :], op0=ALU.mult, op1=ALU.mult))
                p.op('dve', ['g_kT', ('g_e2', s2)], [('g_kb', s2)],
                     lambda E, s2=s2, t0=t0: E.tensor_tensor(out=kb[s2][:, :], in0=kT[:, t0:t0 + 128], in1=e2[s2][:, :],
                                                             op=ALU.mult))
                p.op('dve', [('g_kb', s2), ('g_e1', s2)], [('g_kd', s2)],
                     lambda E, s2=s2, last=last: E.tensor_scalar(out=kd[s2][:, :], in0=kb[s2][:, :],
                                                                 scalar1=e1[s2][:, last:last + 1], scalar2=None,
                                                                 op0=ALU.mult))

                if c.cfg.get('gsub', 99) < 4:
                    continue
                def mma(E, s2=s2):
                    ins = None
                    for h in range(2):
                        if h == 1:
                            p.pe_fence(ins)
                        ins = E.matmul(psA[:, h * 128:(h + 1) * 128], lhsT=kb[s2][h * 64:(h + 1) * 64, :],
                                       rhs=qb[s2][h * 64:(h + 1) * 64, :], start=True, stop=True)
                    return ins
                p.op('pe', [('g_kb', s2), ('g_qb', s2)], [('bank', 3)], mma)
                if c.cfg.get('gsub', 99) < 5:
                    continue
                msk = MSK[dirn]
                p.op('dve', [('bank', 3), 'cst'], [('g_am', s2)],
                     lambda E, s2=s2, msk=msk: E.tensor_tensor(
                         out=am[s2][:, :].rearrange("p (h i) -> p h i", h=2),
                         in0=psA[:, :].rearrange("p (h i) -> p h i", h=2),
                         in1=msk.unsqueeze(1).to_broadcast([128, 2, 128]), op=ALU.mult))
                if c.cfg.get('gsub', 99) < 6:
                    continue
                p.op('pe', [('g_kd', s2), 'ident'], [('bank', 4)],
                     lambda E, s2=s2: E.transpose(psK[:, :], kd[s2][:, :], c.ident[:, :]))
                p.op('act', [('bank', 4)], [('g_kdt', s2)],
                     lambda E, s2=s2: E.activation(out=kdt[s2][:, :], in_=psK[:, :], func=AF.Copy))
                if c.cfg.get('gsub', 99) < 7:
                    continue
                pO = psO[s2]

                def mmo(E, s2=s2, pO=pO, n=n):
                    ins = None
                    for h in range(2):
                        if h == 1:
                            p.pe_fence(ins)
                        E.matmul(pO[:, h * 128:(h + 1) * 128], lhsT=am[s2][:, h * 128:(h + 1) * 128],
                                 rhs=v[:, n, h * 128:(h + 1) * 128], start=True, stop=False)
                        ins = E.matmul(pO[:, h * 128:(h + 1) * 128], lhsT=qb[s2][h * 64:(h + 1) * 64, :],
                                       rhs=Sb[h * 64:(h + 1) * 64, :], start=False, stop=True)
                    return ins
                p.op('pe', [('g_am', s2), 'g_v', ('g_qb', s2), 'g_Sb'], [('bank', 5 + s2)], mmo)
                if dirn == 0:
                    p.op('act', [('bank', 5 + s2)], [('g_oacc', n)],
                         lambda E, pO=pO, n=n: E.activation(out=oacc[:, n, :], in_=pO[:, :], func=AF.Copy))
                else:
                    p.op('dve', [('bank', 5 + s2), ('g_oacc', n)], [('g_oacc', n)],
                         lambda E, pO=pO, n=n: E.tensor_tensor(out=oacc[:, n, :], in0=pO[:, :], in1=oacc[:, n, :],
                                                               op=ALU.add))

                if c.cfg.get('gsub', 99) < 8:
                    continue
                def mms(E, s2=s2, n=n):
                    ins = None
                    for h in range(2):
                        if h == 1:
                            p.pe_fence(ins)
                        ins = E.matmul(psS[h * 64:(h + 1) * 64, :], lhsT=kdt[s2][:, h * 64:(h + 1) * 64],
                                       rhs=v[:, n, h * 128:(h + 1) * 128], start=True, stop=True)
                    return ins
                p.op('pe', [('g_kdt', s2), 'g_v'], [('bank', 7)], mms)
                if c.cfg.get('gsub', 99) < 9:
                    continue
                p.op('dve', [('bank', 7), ('g_e1', s2), 'g_S'], ['g_S'],
                     lambda E, s2=s2, last=last: E.scalar_tensor_tensor(out=St[:, :], in0=St[:, :],
                                                                        scalar=e1[s2][:, last:last + 1], in1=psS[:, :],
                                                                        op0=ALU.mult, op1=ALU.add))
                p.op('act', ['g_S'], ['g_Sb'], lambda E: E.activation(out=Sb[:, :], in_=St[:, :], func=AF.Copy))
        if c.cfg.get('gnofin'):
            continue
        okeys = [('g_oacc', n) for n in range(NCH)]
        for n in range(NCH):
            for h in range(2):
                p.op('act', [('g_oacc', n)], ['g_junk', ('g_rst', n, h)],
                     lambda E, n=n, h=h: E.activation(out=junk[:, :], in_=oacc[:, n, h * 128:(h + 1) * 128],
                                                      func=AF.Square, accum_out=rst[:, n * 2 + h:n * 2 + h + 1]))
        rkeys = [('g_rst', n, h) for n in range(NCH) for h in range(2)]
        p.op('act', rkeys, ['g_rst2'],
             lambda E: E.activation(out=rst2[:, :], in_=rst[:, :], func=AF.Ln, scale=1.0 / 128, bias=RMS_EPS))
        p.op('act', ['g_rst2'], ['g_rst2'],
             lambda E: E.activation(out=rst2[:, :], in_=rst2[:, :], func=AF.Exp, scale=-0.5))
        p.dma('sp', 'g_v', [], ['g_v'],
              [(gr[:, :, :], S['GR'][:, hp * 256:(hp + 1) * 256].rearrange("(n p) c -> p n c", p=128))])
        p.op('act', ['g_v'], ['g_v'], lambda E: E.activation(out=gr[:, :, :], in_=gr[:, :, :], func=AF.Silu))
        o4 = oacc[:, :, :].rearrange("p n (h d) -> p (n h) d", h=2)
        p.op('dve', okeys + ['g_rst2'], okeys,
             lambda E: E.tensor_tensor(out=o4, in0=o4, in1=rst2[:, :].unsqueeze(2).to_broadcast([128, NCH * 2, 128]),
                                       op=ALU.mult))
        p.op('dve', okeys + ['g_w'], okeys,
             lambda E: E.tensor_tensor(out=o4, in0=o4, in1=normw[:, :].unsqueeze(1).to_broadcast([128, NCH * 2, 128]),
                                       op=ALU.mult))
        p.op('dve', okeys + ['g_v'], ['g_v'],
             lambda E: E.tensor_tensor(out=gr[:, :, :], in0=oacc[:, :, :], in1=gr[:, :, :], op=ALU.mult))
        groups = [(0, 2)] + [(2 + 4 * i, 4) for i in range(16)]
        for gi, (n0, cnt) in enumerate(groups):
            for h in range(2):
                tb = (gi * 2 + h) % 2
                tpt = c.tp[tb]

                def tr(E, n0=n0, cnt=cnt, h=h, tpt=tpt):
                    ins = None
                    for j in range(cnt):
                        ins = E.transpose(tpt[:, j, :], gr[:, n0 + j, h * 128:(h + 1) * 128], c.ident[:, :])
                    return ins
                p.op('pe', ['g_v', 'ident'], [('bank', 1 + tb)], tr)
                ot = osb[tb]
                T = cnt * 128
                if tb == 0:
                    p.op('act', [('bank', 1 + tb)], [('g_osb', tb)],
                         lambda E, ot=ot, tpt=tpt, T=T: E.activation(out=ot[:, 0:T], in_=tpt[:, :, :].rearrange("p a b -> p (a b)")[:, 0:T], func=AF.Copy))
                else:
                    p.op('dve', [('bank', 1 + tb)], [('g_osb', tb)],
                         lambda E, ot=ot, tpt=tpt, T=T: E.tensor_copy(out=ot[:, 0:T], in_=tpt[:, :, :].rearrange("p a b -> p (a b)")[:, 0:T]))
                row0 = (hp * 2 + h) * 128
                p.dma('sp', 'g_osb%d' % tb, [('g_osb', tb)], [('OGT', l)],
                      [(S['OGT'][row0:row0 + 128, n0 * 128:n0 * 128 + T], ot[:, 0:T])])


MLA_SCALE = 96 ** -0.5


def stage_mla(c, l, ctx_q):
    with ExitStack() as es:
        c.es = es
        _stage_mla(c, l, ctx_q)
        c.p.barrier()


def _rms_rstd(c, psq, rs, nfeat, key_ps, key_rs):
    p = c.p
    p.op('act', [key_ps], [key_rs],
         lambda E: E.activation(out=rs, in_=psq, func=AF.Ln, scale=1.0 / nfeat, bias=RMS_EPS))
    p.op('act', [key_rs], [key_rs], lambda E: E.activation(out=rs, in_=rs, func=AF.Exp, scale=-0.5))


def _stage_mla(c, l, ctx_q):
    p, nc = c.p, c.nc
    I = c.inp
    S = c.scr[l]
    KpT, VpD, QpT = c.KpT, c.VpD, c.QpT
    cst = c.cst
    ones32 = cst[:, 640:768]
    wkv32 = sb(c, 'm_wkv32', [128, 2, 1024], F32)
    wq32 = sb(c, 'm_wq32', [128, 3, 768], F32)
    wkv = sb(c, 'm_wkv', [128, 2, 1024], BF16)
    wq = sb(c, 'm_wq', [128, 3, 768], BF16)
    gn = sb(c, 'm_gn', [128, 5], F32)
    onesb = sb(c, 'm_onesb', [128, 8], BF16)
    p.dma('sp', 'm_w', [], ['m_w32'],
          [(wkv32[:, :, :], I['mla_w_ukv'][l].rearrange("(k p) n -> p k n", p=128)),
           (wq32[:, :, :], I['mla_w_uq'][l].rearrange("(k p) n -> p k n", p=128))])
    p.dma('sp', 'm_g', [], ['m_gn'],
          [(gn[:, 0:2], I['mla_kv_norm'][l, :].rearrange("(k p) -> p k", p=128)),
           (gn[:, 2:5], I['mla_q_norm'][l, :].rearrange("(k p) -> p k", p=128))], slow=True)
    p.op('dve', [], ['m_onesb'], lambda E: E.memset(onesb[:, :], 1.0))
    for k in range(2):
        p.op('dve', ['m_w32', 'm_gn'], [('m_wkv', k)],
             lambda E, k=k: E.tensor_scalar(out=wkv[:, k, :], in0=wkv32[:, k, :], scalar1=gn[:, k:k + 1], scalar2=None,
                                            op0=ALU.mult))
    for k in range(3):
        p.op('dve', ['m_w32', 'm_gn'], [('m_wq', k)],
             lambda E, k=k: E.tensor_scalar(out=wq[:, k, :], in0=wq32[:, k, :], scalar1=gn[:, 2 + k:3 + k], scalar2=None,
                                            op0=ALU.mult))
    wkvk = [('m_wkv', k) for k in range(2)]
    wqk = [('m_wq', k) for k in range(3)]
    src = [sb(c, 'm_src%d' % i, [128, 3, 512], BF16) for i in range(2)]
    sq = [sb(c, 'm_sq%d' % i, [128, 3, 512], BF16) for i in range(2)]
    rs = [sb(c, 'm_rs%d' % i, [128, 2], F32) for i in range(2)]
    kr = [sb(c, 'm_kr%d' % i, [128, 32], F32) for i in range(2)]
    krr = [sb(c, 'm_krr%d' % i, [128, 32], F32) for i in range(2)]
    rtab = [sb(c, 'm_rtab%d' % i, [128, 32], F32) for i in range(2)]
    tmp16 = [sb(c, 'm_tmp%d' % i, [128, 16], F32) for i in range(2)]
    kp = [sb(c, 'm_kp%d' % i, [128, 8, 97], BF16) for i in range(2)]
    vp = [sb(c, 'm_vp%d' % i, [128, 8, 65], BF16) for i in range(2)]
    kpt = [sb(c, 'm_kpt%d' % i, [97, 8, 128], BF16) for i in range(2)]
    qf = [sb(c, 'm_qf%d' % i, [128, 8, 96], F32) for i in range(2)]
    qsq = sb(c, 'm_qsq', [128, 8, 96], F32)
    ks = sb(c, 'm_ks', [128, 8], F32)
    kmx = sb(c, 'm_kmx', [128, 8], F32)
    kmT = sb(c, 'm_kmT', [8, 1], F32)
    kdiag = sb(c, 'm_kdiag', [8, 8], F32)
    kbc = sb(c, 'm_kbc', [128, 8], F32)
    qn = [sb(c, 'm_qn%d' % i, [128, 8], F32) for i in range(2)]
    B = c.bank
    bk = lambda i: ('bank', i)
    for i in range(2):
        p.op('dve', [], [('m_kp', i)], lambda E, i=i: E.memset(kp[i][:, :, :], 1.0))
        p.op('dve', [], [('m_vp', i)], lambda E, i=i: E.memset(vp[i][:, :, :], 1.0))
    p.op('dve', [], ['m_kmx'], lambda E: E.memset(kmx[:, :], 0.0))

    blocks = [(0, 2)] + [(2 + 4 * i, 4) for i in range(16)]
    it = 0

    def load_src(name, nk, t0, ntl, sslot):
        T = ntl * 128
        p.dma('sp', 'm_src%d' % sslot, [], [('m_src', sslot)],
              [(src[sslot][:, 0:nk, 0:T], S[name][:, t0 * 128:t0 * 128 + T].rearrange("(k p) t -> p k t", p=128))])
        p.op('act', [('m_src', sslot)], [('m_sq', sslot)],
             lambda E: E.activation(out=sq[sslot][:, 0:nk, 0:T], in_=src[sslot][:, 0:nk, 0:T], func=AF.Square))

    def rope_rows(t, s2):
        p.dma('sp', 'm_rtab%d' % s2, [], [('m_rtab', s2)], [(rtab[s2][:, :], I['rope'][t * 128:(t + 1) * 128, :])])

    for bi, (tb0, ntl) in enumerate(blocks):
        sslot = bi % 2
        load_src('MKVAT', 2, tb0, ntl, sslot)
        for j in range(ntl):
            t = tb0 + j
            s2 = it % 2
            it += 1
            cols = slice(j * 128, (j + 1) * 128)

            def mmq(E, sslot=sslot, cols=cols):
                ins = None
                for k in range(2):
                    ins = E.matmul(B[0][:, 0:1], lhsT=sq[sslot][:, k, cols], rhs=onesb[:, 0:1], start=(k == 0), stop=(k == 1))
                return ins
            p.op('pe', [('m_sq', sslot), 'm_onesb'], [bk(0)], mmq)
            _rms_rstd(c, B[0][:, 0:1], rs[s2][:, 0:1], 256, bk(0), ('m_rs', s2))
            for half in range(2):
                def mmkv(E, sslot=sslot, cols=cols, half=half):
                    ins = None
                    for k in range(2):
                        ins = E.matmul(B[1 + half][:, :], lhsT=src[sslot][:, k, cols],
                                       rhs=wkv[:, k, half * 512:(half + 1) * 512], start=(k == 0), stop=(k == 1))
                    return ins
                p.op('pe', [('m_src', sslot)] + wkvk, [bk(1 + half)], mmkv)
            for half in range(2):
                pv = B[1 + half][:, :].rearrange("p (h e) -> p h e", h=4)
                p.op('dve', [bk(1 + half), ('m_rs', s2)], [('m_kp', s2)],
                     lambda E, pv=pv, half=half, s2=s2: E.tensor_scalar(out=kp[s2][:, half * 4:(half + 1) * 4, 0:64], in0=pv[:, :, 0:64],
                                                                        scalar1=rs[s2][:, 0:1], scalar2=None, op0=ALU.mult))
                p.op('act', [bk(1 + half), ('m_rs', s2)], [('m_vp', s2)],
                     lambda E, pv=pv, half=half, s2=s2: E.activation(out=vp[s2][:, half * 4:(half + 1) * 4, 0:64], in_=pv[:, :, 64:128],
                                                                     func=AF.Copy, scale=rs[s2][:, 0:1]))
            p.dma('sp', 'm_kr%d' % s2, [], [('m_kr', s2)], [(kr[s2][:, :], S['MKR'][t * 128:(t + 1) * 128, :])])
            if t >= 2:
                rope_rows(t - 2, s2)
                x1, x2 = kr[s2][:, 0:16], kr[s2][:, 16:32]
                cs, sn = rtab[s2][:, 0:16], rtab[s2][:, 16:32]
                o1, o2 = krr[s2][:, 0:16], krr[s2][:, 16:32]
                tm = tmp16[s2]
                rk = [('m_kr', s2), ('m_rtab', s2)]
                p.op('dve', rk, [('m_krr', s2)], lambda E, o1=o1, x1=x1, cs=cs: E.tensor_tensor(out=o1, in0=x1, in1=cs, op=ALU.mult))
                p.op('dve', rk, [('m_tmp', s2)], lambda E, tm=tm, x2=x2, sn=sn: E.tensor_tensor(out=tm[:, :], in0=x2, in1=sn, op=ALU.mult))
                p.op('dve', [('m_krr', s2), ('m_tmp', s2)], [('m_krr', s2)],
                     lambda E, o1=o1, tm=tm: E.tensor_tensor(out=o1, in0=o1, in1=tm[:, :], op=ALU.subtract))
                p.op('dve', rk + [('m_krr', s2)], [('m_krr', s2)], lambda E, o2=o2, x2=x2, cs=cs: E.tensor_tensor(out=o2, in0=x2, in1=cs, op=ALU.mult))
                p.op('dve', rk + [('m_tmp', s2)], [('m_tmp', s2)], lambda E, tm=tm, x1=x1, sn=sn: E.tensor_tensor(out=tm[:, :], in0=x1, in1=sn, op=ALU.mult))
                p.op('dve', [('m_krr', s2), ('m_tmp', s2)], [('m_krr', s2)],
                     lambda E, o2=o2, tm=tm: E.tensor_tensor(out=o2, in0=o2, in1=tm[:, :], op=ALU.add))
                rsrc, rkey = krr[s2], ('m_krr', s2)
            else:
                rsrc, rkey = kr[s2], ('m_kr', s2)
            p.op('dve', [rkey, ('m_kp', s2)], [('m_kp', s2)],
                 lambda E, rsrc=rsrc, s2=s2: E.tensor_copy(out=kp[s2][:, :, 64:96],
                                                           in_=rsrc[:, :].unsqueeze(1).to_broadcast([128, 8, 32])))
            p.op('dve', [('m_kp', s2)], ['m_qsq'],
                 lambda E, s2=s2: E.tensor_tensor(out=qsq[:, :, :], in0=kp[s2][:, :, 0:96], in1=kp[s2][:, :, 0:96], op=ALU.mult))
            p.op('dve', ['m_qsq'], ['m_ks'], lambda E: E.tensor_reduce(out=ks[:, :], in_=qsq[:, :, :], axis=AX.X, op=ALU.add))
            p.op('dve', ['m_ks', 'm_kmx'], ['m_kmx'], lambda E: E.tensor_tensor(out=kmx[:, :], in0=kmx[:, :], in1=ks[:, :], op=ALU.max))
            tpb = 3 + s2
            tpv = B[tpb][:, :].bitcast(BF16).rearrange("p (h t) -> p h t", h=8)

            def trk(E, s2=s2, tpv=tpv):
                ins = None
                for h in range(8):
                    ins = E.transpose(tpv[0:97, h, :], kp[s2][:, h, :], c.ident[:, :])
                return ins
            p.op('pe', [('m_kp', s2), 'ident'], [bk(tpb)], trk)
            p.op('act', [bk(tpb)], [('m_kpt', s2)],
                 lambda E, s2=s2, tpv=tpv: E.activation(out=kpt[s2][:, :, :], in_=tpv[0:97, :, :], func=AF.Copy))
            p.dma('sp', 'm_kpt%d' % s2, [('m_kpt', s2)], ['KpT'],
                  [(KpT[:, :, t * 128:(t + 1) * 128].rearrange("h d t -> d h t"), kpt[s2][:, :, :])])
            p.dma('sp', 'm_vp%d' % s2, [('m_vp', s2)], ['VpD'],
                  [(VpD[:, t * 128:(t + 1) * 128, :].rearrange("h p e -> p h e"), vp[s2][:, :, :])])
    p.op('pe', ['m_kmx', 'cst'], [bk(0)], lambda E: E.transpose(B[0][0:8, 0:128], kmx[:, :], cst[:, 0:128]))
    p.op('dve', [bk(0)], ['m_kmT'], lambda E: E.tensor_reduce(out=kmT[:, :], in_=B[0][0:8, 0:128], axis=AX.X, op=ALU.max))
    p.op('dve', ['m_kmT', 'cst'], ['m_kdiag'],
         lambda E: E.tensor_scalar(out=kdiag[:, :], in0=cst[0:8, 0:8], scalar1=kmT[:, 0:1], scalar2=None, op0=ALU.mult))
    p.op('pe', ['m_kdiag', 'cst'], [bk(0)],
         lambda E: E.matmul(B[0][:, 0:8], lhsT=ones32[0:8, :], rhs=kdiag[:, :], start=True, stop=True))
    p.op('act', [bk(0)], ['m_kbc'], lambda E: E.activation(out=kbc[:, :], in_=B[0][:, 0:8], func=AF.Copy))
    qblocks = ([(0, 2)] if ctx_q else []) + [(2 + 4 * i, 4) for i in range(16)]
    for bi, (tb0, ntl) in enumerate(qblocks):
        sslot = bi % 2
        load_src('MQAT', 3, tb0, ntl, sslot)
        for j in range(ntl):
            t = tb0 + j
            s2 = it % 2
            it += 1
            cols = slice(j * 128, (j + 1) * 128)

            def mmq(E, sslot=sslot, cols=cols):
                ins = None
                for k in range(3):
                    ins = E.matmul(B[0][:, 0:1], lhsT=sq[sslot][:, k, cols], rhs=onesb[:, 0:1], start=(k == 0), stop=(k == 2))
                return ins
            p.op('pe', [('m_sq', sslot), 'm_onesb'], [bk(0)], mmq)
            _rms_rstd(c, B[0][:, 0:1], rs[s2][:, 0:1], 384, bk(0), ('m_rs', s2))
            p.op('dve', [('m_rs', s2)], [('m_rs', s2)],
                 lambda E, s2=s2: E.tensor_scalar(out=rs[s2][:, 0:1], in0=rs[s2][:, 0:1], scalar1=MLA_SCALE, scalar2=None, op0=ALU.mult))
            for half, (n0, nn) in enumerate([(0, 512), (512, 256)]):
                def mmqq(E, sslot=sslot, cols=cols, half=half, n0=n0, nn=nn):
                    ins = None
                    for k in range(3):
                        ins = E.matmul(B[1 + half][:, 0:nn], lhsT=src[sslot][:, k, cols], rhs=wq[:, k, n0:n0 + nn],
                                       start=(k == 0), stop=(k == 2))
                    return ins
                p.op('pe', [('m_src', sslot)] + wqk, [bk(1 + half)], mmqq)
            qv = qf[s2][:, :, :].rearrange("p h e -> p (h e)")
            p.op('dve', [bk(1), ('m_rs', s2)], [('m_qf', s2, 0)],
                 lambda E, qv=qv, s2=s2: E.tensor_scalar(out=qv[:, 0:512], in0=B[1][:, 0:512], scalar1=rs[s2][:, 0:1], scalar2=None, op0=ALU.mult))
            p.op('act', [bk(2), ('m_rs', s2)], [('m_qf', s2, 1)],
                 lambda E, qv=qv, s2=s2: E.activation(out=qv[:, 512:768], in_=B[2][:, 0:256], func=AF.Copy, scale=rs[s2][:, 0:1]))
            qk = [('m_qf', s2, 0), ('m_qf', s2, 1)]
            kpq = kp[s2]
            if t >= 2:
                rope_rows(t - 2, s2)
                x1, x2 = qf[s2][:, :, 64:80], qf[s2][:, :, 80:96]
                cs = rtab[s2][:, 0:16].unsqueeze(1).to_broadcast([128, 8, 16])
                sn = rtab[s2][:, 16:32].unsqueeze(1).to_broadcast([128, 8, 16])
                ta, tb_ = qsq[:, :, 0:16], qsq[:, :, 16:32]
                tc_, td = qsq[:, :, 32:48], qsq[:, :, 48:64]
                rk = qk + [('m_rtab', s2)]
                p.op('dve', rk, ['m_qsq'], lambda E, ta=ta, x1=x1, cs=cs: E.tensor_tensor(out=ta, in0=x1, in1=cs, op=ALU.mult))
                p.op('dve', rk + ['m_qsq'], ['m_qsq'], lambda E, tb_=tb_, x2=x2, sn=sn: E.tensor_tensor(out=tb_, in0=x2, in1=sn, op=ALU.mult))
                p.op('dve', rk + ['m_qsq'], ['m_qsq'], lambda E, tc_=tc_, x2=x2, cs=cs: E.tensor_tensor(out=tc_, in0=x2, in1=cs, op=ALU.mult))
                p.op('dve', rk + ['m_qsq'], ['m_qsq'], lambda E, td=td, x1=x1, sn=sn: E.tensor_tensor(out=td, in0=x1, in1=sn, op=ALU.mult))
                p.op('dve', ['m_qsq'] + qk, qk, lambda E, x1=x1, ta=ta, tb_=tb_: E.tensor_tensor(out=x1, in0=ta, in1=tb_, op=ALU.subtract))
                p.op('dve', ['m_qsq'] + qk, qk, lambda E, x2=x2, tc_=tc_, td=td: E.tensor_tensor(out=x2, in0=tc_, in1=td, op=ALU.add))
            p.op('dve', qk, ['m_qsq'],
                 lambda E, s2=s2: E.tensor_tensor(out=qsq[:, :, :], in0=qf[s2][:, :, :], in1=qf[s2][:, :, :], op=ALU.mult))
            p.op('dve', ['m_qsq'], [('m_qn', s2)], lambda E, s2=s2: E.tensor_reduce(out=qn[s2][:, :], in_=qsq[:, :, :], axis=AX.X, op=ALU.add))
            p.op('dve', [('m_qn', s2), 'm_kbc'], [('m_qn', s2)],
                 lambda E, s2=s2: E.tensor_tensor(out=qn[s2][:, :], in0=qn[s2][:, :], in1=kbc[:, :], op=ALU.mult))
            p.op('act', [('m_qn', s2)], [('m_qn', s2)], lambda E, s2=s2: E.activation(out=qn[s2][:, :], in_=qn[s2][:, :], func=AF.Sqrt))
            p.op('dve', qk + [('m_kp', s2)], [('m_kp', s2)],
                 lambda E, s2=s2: E.tensor_copy(out=kpq[:, :, 0:96], in_=qf[s2][:, :, :]))
            p.op('dve', [('m_qn', s2), ('m_kp', s2)], [('m_kp', s2)],
                 lambda E, s2=s2: E.tensor_scalar(out=kpq[:, :, 96:97], in0=qn[s2][:, :].unsqueeze(2), scalar1=-1.0, scalar2=None, op0=ALU.mult))
            tpb = 3 + s2
            tpv = B[tpb][:, :].bitcast(BF16).rearrange("p (h t) -> p h t", h=8)

            def trq(E, s2=s2, tpv=tpv):
                ins = None
                for h in range(8):
                    ins = E.transpose(tpv[0:97, h, :], kp[s2][:, h, :], c.ident[:, :])
                return ins
            p.op('pe', [('m_kp', s2), 'ident'], [bk(tpb)], trq)
            p.op('act', [bk(tpb)], [('m_kpt', s2)],
                 lambda E, s2=s2, tpv=tpv: E.activation(out=kpt[s2][:, :, :], in_=tpv[0:97, :, :], func=AF.Copy))
            p.dma('sp', 'm_kpt%d' % s2, [('m_kpt', s2)], ['QpT'],
                  [(QpT[:, :, t * 128:(t + 1) * 128].rearrange("h d t -> d h t"), kpt[s2][:, :, :])])
    p.barrier()
    kh = [sb(c, 'm_kh%d' % i, [97, NT], BF16) for i in range(2)]
    qh = [sb(c, 'm_qh%d' % i, [97, NT], BF16) for i in range(2)]
    vh = [sb(c, 'm_vh%d' % i, [128, NTILE, 65], BF16) for i in range(2)]
    pt = [sb(c, 'm_pt%d' % i, [128, 512], BF16) for i in range(3)]
    osb = [sb(c, 'm_osb%d' % i, [65, 512], F32) for i in range(2)]
    on = [sb(c, 'm_on%d' % i, [64, 512], BF16) for i in range(2)]
    ei = 0
    ci = 0
    for h in range(8):
        hs = h % 2
        p.dma('sp', 'm_kh%d' % hs, ['KpT'], [('m_kh', hs)], [(kh[hs][:, :], KpT[h, :, :])])
        p.dma('sp', 'm_qh%d' % hs, ['QpT'], [('m_qh', hs)], [(qh[hs][:, :], QpT[h, :, :])])
        p.dma('sp', 'm_vh%d' % hs, ['VpD'], [('m_vh', hs)],
              [(vh[hs][:, :, :], VpD[h, :, :].rearrange("(n p) e -> p n e", p=128))])
        chunks = ([(0, 256, 2)] if ctx_q else []) + [(256 + 512 * i, 512, NTILE) for i in range(16)]
        for (q0, nq, nkt) in chunks:
            ob = 6 + ci % 2
            cs2 = ci % 2
            ci += 1
            def emit_qk(kt):
                sbk = (ei0 + kt) % 3
                p.op('pe', [('m_kh', hs), ('m_qh', hs)], [bk(sbk)],
                     lambda E: E.matmul(B[sbk][:, 0:nq], lhsT=kh[hs][:, kt * 128:(kt + 1) * 128],
                                        rhs=qh[hs][:, q0:q0 + nq], start=True, stop=True))
            ei0 = ei
            ei += nkt
            for kt in range(min(2, nkt)):
                emit_qk(kt)
            for kt in range(nkt):
                sbk = (ei0 + kt) % 3
                p.op('act', [bk(sbk)], [('m_pt', sbk)],
                     lambda E: E.activation(out=pt[sbk][:, 0:nq], in_=B[sbk][:, 0:nq], func=AF.Exp))
                if kt + 2 < nkt:
                    emit_qk(kt + 2)
                p.op('pe', [('m_vh', hs), ('m_pt', sbk)], [bk(ob)],
                     lambda E: E.matmul(B[ob][0:65, 0:nq], lhsT=vh[hs][:, kt, :], rhs=pt[sbk][:, 0:nq],
                                        start=(kt == 0), stop=(kt == nkt - 1)))
            o_t = osb[cs2]
            p.op('dve', [bk(ob)], [('m_osb', cs2)], lambda E, o_t=o_t, ob=ob, nq=nq: E.tensor_copy(out=o_t[:, 0:nq], in_=B[ob][0:65, 0:nq]))
            p.op('dve', [('m_osb', cs2)], [('m_osb', cs2)],
                 lambda E, o_t=o_t, nq=nq: E.reciprocal(out=o_t[64:65, 0:nq], in_=o_t[64:65, 0:nq]))
            p.op('pe', [('m_osb', cs2), 'cst'], [bk(5)],
                 lambda E, o_t=o_t, nq=nq: E.matmul(B[5][0:64, 0:nq], lhsT=ones32[64:65, 0:64], rhs=o_t[64:65, 0:nq], start=True, stop=True))
            p.op('dve', [bk(5), ('m_osb', cs2)], [('m_on', cs2)],
                 lambda E, o_t=o_t, nq=nq, cs2=cs2: E.tensor_tensor(out=on[cs2][:, 0:nq], in0=o_t[0:64, 0:nq], in1=B[5][0:64, 0:nq], op=ALU.mult))
            p.dma('sp', 'm_on%d' % cs2, [('m_on', cs2)], [('OMT', l)],
                  [(S['OMT'][h * 64:(h + 1) * 64, q0:q0 + nq], on[cs2][:, 0:nq])])


NFFT = 16384


def hy_conv3(c, l):
    p = c.p
    I = c.inp
    S = c.scr[l]
    with ExitStack() as es:
        c.es = es
        wb32 = sb(c, 'h3_w32', [64, 4, 1536], F32)
        wb = sb(c, 'h3_w', [64, 4, 1536], BF16)
        zin = [sb(c, 'h3_zin%d' % i, [64, 10, 512], BF16) for i in range(2)]
        t0_ = [sb(c, 'h3_t0%d' % i, [64, 8, 512], BF16) for i in range(2)]
        t1_ = [sb(c, 'h3_t1%d' % i, [64, 8, 512], BF16) for i in range(2)]
        zo = [sb(c, 'h3_zo%d' % i, [64, 8, 512], BF16) for i in range(2)]
        p.dma('sp', 'h3_w', [], ['h3_w32'],
              [(wb32[:, k, :], I['hy_conv_w'][l, k, :].partition_broadcast(64)) for k in range(3)] +
              [(wb32[:, 3, :], I['hy_conv_b'][l, :].partition_broadcast(64))])
        p.op('dve', ['h3_w32'], ['h3_w'], lambda E: E.tensor_copy(out=wb[:, :, :], in_=wb32[:, :, :]))
        it = 0
        for (tok0, na) in ((0, 2), (NCTX, 64)):
            src = S['HY'][tok0:tok0 + na * 128, :].rearrange("(a b) c -> a b c", b=128)
            dst = c.HYC[tok0:tok0 + na * 128, :].rearrange("(a b) c -> a b c", b=128)
            for cs in range(3):
                c0 = cs * 512
                for bc in range(16):
                    b0 = bc * 8
                    s2 = it % 2
                    it += 1
                    z = zin[s2]
                    pairs = []
                    pre = []
                    lo, hi = b0 - 1, b0 + 9
                    if bc == 0:
                        pre.append(lambda E, z=z: E.memset(z[0:1, 0:1, :], 0.0))
                        if na > 1:
                            pairs.append((z[1:na, 0:1, :], src[0:na - 1, 127:128, c0:c0 + 512]))
                        pairs.append((z[0:na, 1:10, :], src[0:na, 0:9, c0:c0 + 512]))
                    elif bc == 15:
                        pre.append(lambda E, z=z, na=na: E.memset(z[0:na, 9:10, :], 0.0))
                        if na > 1:
                            pairs.append((z[0:na - 1, 9:10, :], src[1:na, 0:1, c0:c0 + 512]))
                        pairs.append((z[0:na, 0:9, :], src[0:na, lo:128, c0:c0 + 512]))
                    else:
                        pairs.append((z[0:na, 0:10, :], src[0:na, lo:hi, c0:c0 + 512]))
                    for f in pre:
                        p.op('pool', [], [('h3_zin', s2)], f)
                    p.dma('sp', 'h3_zin%d' % s2, [('HY', l)], [('h3_zin', s2)], pairs)
                    w = lambda k, c0=c0, na=na: wb[0:na, k, c0:c0 + 512].unsqueeze(1).to_broadcast([na, 8, 512])
                    a0, a1, oz = t0_[s2], t1_[s2], zo[s2]
                    zk = ('h3_zin', s2)
                    p.op('dve', [zk, 'h3_w'], [('h3_t0', s2)],
                         lambda E, z=z, a0=a0, w=w, na=na: E.tensor_tensor(out=a0[0:na], in0=z[0:na, 0:8, :], in1=w(0), op=ALU.mult))
                    p.op('pool', [zk, 'h3_w'], [('h3_t1', s2)],
                         lambda E, z=z, a1=a1, w=w, na=na: E.tensor_tensor(out=a1[0:na], in0=z[0:na, 1:9, :], in1=w(1), op=ALU.mult))
                    p.op('dve', [zk, 'h3_w'], [('h3_zo', s2)],
                         lambda E, z=z, oz=oz, w=w, na=na: E.tensor_tensor(out=oz[0:na], in0=z[0:na, 2:10, :], in1=w(2), op=ALU.mult))
                    p.op('dve', [('h3_t0', s2), 'h3_w'], [('h3_t0', s2)],
                         lambda E, a0=a0, w=w, na=na: E.tensor_tensor(out=a0[0:na], in0=a0[0:na], in1=w(3), op=ALU.add))
                    p.op('dve', [('h3_t0', s2), ('h3_zo', s2)], [('h3_zo', s2)],
                         lambda E, a0=a0, oz=oz, na=na: E.tensor_tensor(out=oz[0:na], in0=a0[0:na], in1=oz[0:na], op=ALU.add))
                    p.op('dve', [('h3_t1', s2), ('h3_zo', s2)], [('h3_zo', s2)],
                         lambda E, a1=a1, oz=oz, na=na: E.tensor_tensor(out=oz[0:na], in0=a1[0:na], in1=oz[0:na], op=ALU.add))
                    p.dma('sp', 'h3_zo%d' % s2, [('h3_zo', s2)], ['HYC'],
                          [(dst[0:na, b0:b0 + 8, c0:c0 + 512], oz[0:na, :, :])])
        p.barrier()


def hy_filters(c, l, job):
    p = c.p
    I = c.inp
    nt = 128 if job == 0 else 4
    feat = I['hy_feat%d' % job]
    tvec = I['hy_tvec%d' % job]
    KTD = c.KTD[job]
    B = c.bank
    bk = lambda i: ('bank', i)
    with ExitStack() as es:
        c.es = es
        w1 = sb(c, 'hf_w1', [33, 64], F32)
        w2 = sb(c, 'hf_w2', [64, 64], F32)
        w3 = sb(c, 'hf_w3', [64, 2048], F32)
        pb = sb(c, 'hf_pb', [64, 8], F32)
        ft = [sb(c, 'hf_ft%d' % i, [33, 512], F32) for i in range(2)]
        tv = [sb(c, 'hf_tv%d' % i, [1, 512], F32) for i in range(2)]
        u = [sb(c, 'hf_u%d' % i, [64, 512], F32) for i in range(2)]
        ui = [sb(c, 'hf_ui%d' % i, [64, 512], mybir.dt.int32) for i in range(2)]
        uf = [sb(c, 'hf_uf%d' % i, [64, 512], F32) for i in range(2)]
        h1 = [sb(c, 'hf_h1%d' % i, [64, 512], F32) for i in range(2)]
        h2 = [sb(c, 'hf_h2%d' % i, [64, 512], F32) for i in range(2)]
        dec = [sb(c, 'hf_dec%d' % i, [128, 512], F32) for i in range(2)]
        hd = [sb(c, 'hf_hd%d' % i, [128, 2, 512], F32) for i in range(2)]
        ha = [sb(c, 'hf_ha%d' % i, [128, 2, 512], F32) for i in range(2)]
        hb = [sb(c, 'hf_hb%d' % i, [128, 2, 512], BF16) for i in range(2)]
        nd = sb(c, 'hf_nd', [1, 512], F32)
        l1 = sb(c, 'hf_l1', [1, 1024], F32)
        cst = c.cst
        ones = cst[:, 640:768]
        p.dma('sp', 'hf_w', [], ['hf_w'],
              [(w1[:, :], I['hy_w1'][l]), (w2[:, :], I['hy_w2'][l]), (w3[:, :], I['hy_w3'][l]),
               (nd[:, :], I['hy_negdelta'][0:1, :])])
        p.dma('sp', 'hf_pb', [], ['hf_pb'],
              [(pb[:, 0:1], I['hy_b1'][l, :].rearrange("(p o) -> p o", o=1)),
               (pb[:, 1:2], I['hy_b2'][l, :].rearrange("(p o) -> p o", o=1)),
               (pb[:, 2:3], I['hy_freq'][l, :].rearrange("(p o) -> p o", o=1))], slow=True)
        p.op('dve', ['hf_pb'], ['hf_pb2'],
             lambda E: E.tensor_scalar(out=pb[:, 3:4], in0=pb[:, 2:3], scalar1=1.0 / (2 * math.pi), scalar2=None, op0=ALU.mult))
        p.op('dve', ['hf_pb', 'hf_pb2'], ['hf_pb3'],
             lambda E: E.tensor_scalar(out=pb[:, 4:6], in0=pb[:, 0:2], scalar1=pb[:, 3:4], scalar2=None, op0=ALU.mult))
        pbk = ['hf_pb', 'hf_pb2', 'hf_pb3']

        def sin_layer(src_ps, srck, bcol, dst, dstk, s2):
            p.op('dve', [srck] + pbk, [('hf_u', s2)],
                 lambda E: E.tensor_scalar(out=u[s2][:, :], in0=src_ps, scalar1=pb[:, 3:4], scalar2=pb[:, bcol:bcol + 1],
                                           op0=ALU.mult, op1=ALU.add))
            p.op('dve', [('hf_u', s2)], [('hf_ui', s2)], lambda E: E.tensor_copy(out=ui[s2][:, :], in_=u[s2][:, :]))
            p.op('dve', [('hf_ui', s2)], [('hf_uf', s2)], lambda E: E.tensor_copy(out=uf[s2][:, :], in_=ui[s2][:, :]))
            p.op('dve', [('hf_u', s2), ('hf_uf', s2)], [('hf_u', s2)],
                 lambda E: E.tensor_tensor(out=u[s2][:, :], in0=u[s2][:, :], in1=uf[s2][:, :], op=ALU.subtract))
            p.op('act', [('hf_u', s2)], [dstk], lambda E: E.activation(out=dst, in_=u[s2][:, :], func=AF.Sin, scale=2 * math.pi))

        nchunk = nt // 4
        first_bwd_tile = 64 if job == 0 else 2
        for ch in range(nchunk):
            s2 = ch % 2
            p.dma('sp', 'hf_ft%d' % s2, [], [('hf_ft', s2)],
                  [(ft[s2][:, :], feat[:, ch * 512:(ch + 1) * 512]), (tv[s2][:, :], tvec[:, ch * 512:(ch + 1) * 512])])
            p.op('pe', [('hf_ft', s2), 'hf_w'], [bk(0)],
                 lambda E, s2=s2: E.matmul(B[0][0:64, :], lhsT=w1[:, :], rhs=ft[s2][:, :], start=True, stop=True))
            sin_layer(B[0][0:64, :], bk(0), 4, h1[s2][:, :], ('hf_h1', s2), s2)
            p.op('pe', [('hf_h1', s2), 'hf_w'], [bk(1)],
                 lambda E, s2=s2: E.matmul(B[1][0:64, :], lhsT=w2[:, :], rhs=h1[s2][:, :], start=True, stop=True))
            sin_layer(B[1][0:64, :], bk(1), 5, h2[s2][:, :], ('hf_h2', s2), s2)
            for j in range(4):
                tile = ch * 4 + j
                d = 0 if tile < first_bwd_tile else 1
                j2 = tile % 2
                cols = slice(j * 128, (j + 1) * 128)
                p.op('pe', [('hf_ft', s2), 'hf_w'], [bk(2)],
                     lambda E, s2=s2, cols=cols: E.matmul(B[2][:, :], lhsT=tv[s2][0:1, cols], rhs=nd[0:1, :], start=True, stop=True))
                p.op('act', [bk(2)], [('hf_dec', j2)], lambda E, j2=j2: E.activation(out=dec[j2][:, :], in_=B[2][:, :], func=AF.Exp))
                for o in range(2):
                    c0 = o * 1024 + d * 512
                    p.op('pe', [('hf_h2', s2), 'hf_w'], [bk(3 + o)],
                         lambda E, s2=s2, cols=cols, c0=c0, o=o: E.matmul(B[3 + o][:, :], lhsT=h2[s2][:, cols], rhs=w3[:, c0:c0 + 512],
                                                                        start=True, stop=True))
                    p.op('dve', [bk(3 + o), ('hf_dec', j2)], [('hf_hd', j2, o)],
                         lambda E, j2=j2, o=o: E.tensor_tensor(out=hd[j2][:, o, :], in0=B[3 + o][:, :], in1=dec[j2][:, :], op=ALU.mult))
                p.op('act', [('hf_hd', j2, 0), ('hf_hd', j2, 1)], [('hf_ha', j2)],
                     lambda E, j2=j2: E.activation(out=ha[j2][:, :, :], in_=hd[j2][:, :, :], func=AF.Abs))
                for o in range(2):
                    p.op('pe', [('hf_ha', j2), 'cst'], [bk(5 + o)],
                         lambda E, j2=j2, o=o, tile=tile: E.matmul(B[5 + o][0:1, :], lhsT=ones[:, 0:1], rhs=ha[j2][:, o, :],
                                                                   start=(tile == 0), stop=(tile == nt - 1)))
                p.op('pool', [('hf_hd', j2, 0), ('hf_hd', j2, 1)], [('hf_hb', j2)],
                     lambda E, j2=j2: E.tensor_copy(out=hb[j2][:, :, :], in_=hd[j2][:, :, :]))
                if tile == first_bwd_tile:
                    p.op('pool', [('hf_hb', j2)], [('hf_hb', j2)], lambda E, j2=j2: E.memset(hb[j2][0:1, :, :], 0.0))
                p.dma('sp', 'hf_hb%d' % j2, [('hf_hb', j2)], [('KTD', job)],
                      [(KTD[tile * 128:(tile + 1) * 128, :].rearrange("p (o c) -> p o c", o=2), hb[j2][:, :, :])])
        for o in range(2):
            p.op('dve', [bk(5 + o)], ['hf_l1'],
                 lambda E, o=o: E.tensor_scalar(out=l1[0:1, o * 512:(o + 1) * 512], in0=B[5 + o][0:1, :], scalar1=float(NFFT if job == 0 else 512), scalar2=None,
                                                op0=ALU.mult))
        p.op('dve', ['hf_l1'], ['hf_l1'], lambda E: E.reciprocal(out=l1[:, :], in_=l1[:, :]))
        p.dma('sp', 'hf_l1', ['hf_l1'], [('SCL', job)], [(c.SCL[job][:, :], l1[:, :])])
        p.barrier()


def hy_fwd1(c, src, K, tab, X1D, NF1):
    p = c.p
    B = c.bank
    bk = lambda i: ('bank', i)
    zt = c.hy_zt
    xo = c.hy_xo
    for bc in range(16):
        s2 = bc % 2
        p.dma('sp', 'hy_zt%d' % s2, ['HYC', 'Z2', ('KTD', 0), ('KTD', 1)], [('hy_zt', s2)], [(zt[s2][0:K, :, :], src(bc * 8, 8))])
        for j in range(8):
            b = bc * 8 + j
            e2 = b % 2
            for ri in range(2):
                p.op('pe', [('hy_zt', s2), 'hy_tab'], [bk(e2 * 2 + ri)],
                     lambda E, s2=s2, j=j, ri=ri, e2=e2: E.matmul(B[e2 * 2 + ri][0:NF1, :], lhsT=tab[0:K, ri, 0:NF1], rhs=zt[s2][0:K, j, :],
                                                                  start=True, stop=True))
            p.op('act', [bk(e2 * 2)], [('hy_xo', e2, 0)],
                 lambda E, e2=e2: E.activation(out=xo[e2][0:NF1, 0, :], in_=B[e2 * 2][0:NF1, :], func=AF.Copy))
            p.op('dve', [bk(e2 * 2 + 1)], [('hy_xo', e2, 1)],
                 lambda E, e2=e2: E.tensor_copy(out=xo[e2][0:NF1, 1, :], in_=B[e2 * 2 + 1][0:NF1, :]))
            p.dma('sp', 'hy_xo%d' % e2, [('hy_xo', e2, 0), ('hy_xo', e2, 1)], ['X1D'],
                  [(X1D[b, 0:NF1, :, :], xo[e2][0:NF1, :, :])])


def hy_stage2(c, X1D, mode, KS, QD, NF1=128, tw2name='hy_tw2', twres=None):
    p = c.p
    I = c.inp
    B = c.bank
    bk = lambda i: ('bank', i)
    xin, tw, ksb, pr, t4, qo = c.hy_xin, c.hy_tw, c.hy_ksb, c.hy_pr, c.hy_t4, c.hy_qo
    E3 = c.hy_E3

    def front(f1):
        s2 = f1 % 2
        p.dma('sp', 'hy_xin%d' % s2, ['X1D'], [('hy_xin', s2)],
              [(xin[s2][:, :, :], X1D[:, f1, :, :])])
        if twres is None:
            p.dma('sp', 'hy_tw%d' % s2, [], [('hy_tw', s2)], [(tw[s2][:, :, :], I[tw2name][f1])])
            twv, twk = tw[s2], ('hy_tw', s2)
        else:
            twv, twk = twres[:, f1, :, :], 'hy_twres'
        if mode != 'filter':
            p.dma('sp', 'hy_ksb%d' % s2, ['KS'], [('hy_ksb', s2)], [(ksb[s2][:, :, :], KS[:, f1, :, :])])
        zr, zi = s2 * 2, s2 * 2 + 1

        def mmz(E):
            E.matmul(B[zr][:, :], lhsT=twv[:, 0, :], rhs=xin[s2][:, 0, :], start=True, stop=False)
            E.matmul(B[zr][:, :], lhsT=twv[:, 2, :], rhs=xin[s2][:, 1, :], start=False, stop=True)
            E.matmul(B[zi][:, :], lhsT=twv[:, 0, :], rhs=xin[s2][:, 1, :], start=True, stop=False)
            return E.matmul(B[zi][:, :], lhsT=twv[:, 1, :], rhs=xin[s2][:, 0, :], start=False, stop=True)
        p.op('pe', [('hy_xin', s2), twk], [bk(zr), bk(zi)], mmz)

    front(0)
    for f1 in range(NF1):
        s2 = f1 % 2
        zr, zi = s2 * 2, s2 * 2 + 1
        if mode == 'filter':
            p.op('act', [bk(zr)], [('hy_pr', s2, 0)], lambda E: E.activation(out=pr[s2][:, 0, :], in_=B[zr][:, :], func=AF.Copy))
            p.op('dve', [bk(zi)], [('hy_pr', s2, 1)], lambda E: E.tensor_copy(out=pr[s2][:, 1, :], in_=B[zi][:, :]))
            if f1 + 1 < NF1:
                front(f1 + 1)
            p.dma('sp', 'hy_pr%d' % s2, [('hy_pr', s2, 0), ('hy_pr', s2, 1)], ['KS'],
                  [(KS[:, f1, :, :], pr[s2][:, :, :])])
            continue
        kk = ('hy_ksb', s2)
        tt = t4[s2]
        p.op('dve', [bk(zr), kk], [('hy_t4', s2, 0)], lambda E: E.tensor_tensor(out=tt[:, 0, :], in0=B[zr][:, :], in1=ksb[s2][:, 0, :], op=ALU.mult))
        p.op('dve', [bk(zi), kk], [('hy_t4', s2, 1)], lambda E: E.tensor_tensor(out=tt[:, 1, :], in0=B[zi][:, :], in1=ksb[s2][:, 1, :], op=ALU.mult))
        p.op('dve', [bk(zr), kk], [('hy_t4', s2, 2)], lambda E: E.tensor_tensor(out=tt[:, 2, :], in0=B[zr][:, :], in1=ksb[s2][:, 1, :], op=ALU.mult))
        p.op('dve', [bk(zi), kk], [('hy_t4', s2, 3)], lambda E: E.tensor_tensor(out=tt[:, 3, :], in0=B[zi][:, :], in1=ksb[s2][:, 0, :], op=ALU.mult))
        if f1 + 1 < NF1:
            front(f1 + 1)
        p.op('pool', [('hy_t4', s2, 0), ('hy_t4', s2, 1)], [('hy_pr', s2, 0)],
             lambda E: E.tensor_tensor(out=pr[s2][:, 0, :], in0=tt[:, 0, :], in1=tt[:, 1, :], op=ALU.subtract))
        p.op('pool', [('hy_t4', s2, 2), ('hy_t4', s2, 3)], [('hy_pr', s2, 1)],
             lambda E: E.tensor_tensor(out=pr[s2][:, 1, :], in0=tt[:, 2, :], in1=tt[:, 3, :], op=ALU.add))

        def mmq(E):
            E.matmul(B[4][:, :], lhsT=E3[:, 0, :], rhs=pr[s2][:, 0, :], start=True, stop=False)
            E.matmul(B[4][:, :], lhsT=E3[:, 2, :], rhs=pr[s2][:, 1, :], start=False, stop=True)
            E.matmul(B[5][:, :], lhsT=E3[:, 1, :], rhs=pr[s2][:, 0, :], start=True, stop=False)
            return E.matmul(B[5][:, :], lhsT=E3[:, 0, :], rhs=pr[s2][:, 1, :], start=False, stop=True)
        p.op('pe', [('hy_pr', s2, 0), ('hy_pr', s2, 1), 'hy_tab'], [bk(4), bk(5)], mmq)
        p.op('act', [bk(4)], [('hy_qo', s2, 0)], lambda E: E.activation(out=qo[s2][:, 0, :], in_=B[4][:, :], func=AF.Copy))
        p.op('act', [bk(5)], [('hy_qo', s2, 1)], lambda E: E.activation(out=qo[s2][:, 1, :], in_=B[5][:, :], func=AF.Copy))
        p.dma('sp', 'hy_qo%d' % s2, [('hy_qo', s2, 0), ('hy_qo', s2, 1)], ['QD'],
              [(QD[:, f1, :, :], qo[s2][:, :, :])])


def hy_final(c, QD, na, scl, bias, zsrc, gsrc, dst, NF1=128, twfname='hy_twf', Mm=64):
    p = c.p
    I = c.inp
    B = c.bank
    bk = lambda i: ('bank', i)
    qin, twf, zg, ya, yb, yo = c.hy_qin, c.hy_twf, c.hy_zg, c.hy_ya, c.hy_yb, c.hy_yo
    for b in range(128):
        s2 = b % 2
        p.dma('sp', 'hy_qin%d' % s2, ['QD'], [('hy_qin', s2)], [(qin[s2][0:NF1, :, :], QD[b, 0:NF1, :, :])])
        p.dma('sp', 'hy_twf%d' % s2, [], [('hy_twf', s2)], [(twf[s2][0:NF1, :, 0:Mm], I[twfname][b])])
        p.dma('sp', 'hy_zg%d' % s2, ['HYC', 'Z2'], [('hy_zg', s2)],
              [(zg[s2][0:na, 0, :], zsrc[:, b, :]), (zg[s2][0:na, 1, :], gsrc[:, b, :])])

        def mmy(E, s2=s2):
            E.matmul(B[s2][0:Mm, :], lhsT=twf[s2][0:NF1, 0, 0:Mm], rhs=qin[s2][0:NF1, 0, :], start=True, stop=False)
            return E.matmul(B[s2][0:Mm, :], lhsT=twf[s2][0:NF1, 1, 0:Mm], rhs=qin[s2][0:NF1, 1, :], start=False, stop=True)
        p.op('pe', [('hy_qin', s2), ('hy_twf', s2)], [bk(s2)], mmy)
        p.op('dve', [bk(s2), 'hy_scl'], [('hy_ya', s2)],
             lambda E, s2=s2: E.tensor_tensor(out=ya[s2][0:na, :], in0=B[s2][0:na, :], in1=scl[0:na, :], op=ALU.mult))
        p.op('pool', [('hy_zg', s2), 'hy_scl'], [('hy_yb', s2)],
             lambda E, s2=s2: E.tensor_tensor(out=yb[s2][0:na, :], in0=zg[s2][0:na, 0, :], in1=bias[0:na, :], op=ALU.mult))
        p.op('pool', [('hy_ya', s2), ('hy_yb', s2)], [('hy_ya', s2)],
             lambda E, s2=s2: E.tensor_tensor(out=ya[s2][0:na, :], in0=ya[s2][0:na, :], in1=yb[s2][0:na, :], op=ALU.add))
        p.op('dve', [('hy_ya', s2), ('hy_zg', s2)], [('hy_yo', s2)],
             lambda E, s2=s2: E.tensor_tensor(out=yo[s2][0:na, :], in0=ya[s2][0:na, :], in1=zg[s2][0:na, 1, :], op=ALU.mult))
        p.dma('sp', 'hy_yo%d' % s2, [('hy_yo', s2)], ['Z2', 'OH'], [(dst[:, b, :], yo[s2][0:na, :])])


def stage_hyena(c, l, with_ctx):
    p = c.p
    I = c.inp
    hy_conv3(c, l)
    jobs = [0, 1] if with_ctx else [0]
    hp = c.cfg.get('hy_parts', 9)
    if hp < 1:
        return
    for job in jobs:
        hy_filters(c, l, job)
    if hp < 2:
        return
    with ExitStack() as es:
        c.es = es
        c.hy_zt = [sb(c, 'hy_zt%d' % i, [128, 8, 512], BF16) for i in range(2)]
        c.hy_xo = [sb(c, 'hy_xo%d' % i, [128, 2, 512], BF16) for i in range(2)]
        c.hy_xin = [sb(c, 'hy_xin%d' % i, [128, 2, 512], BF16) for i in range(2)]
        c.hy_tw = [sb(c, 'hy_tw%d' % i, [128, 3, 128], BF16) for i in range(2)]
        c.hy_ksb = [sb(c, 'hy_ksb%d' % i, [128, 2, 512], BF16) for i in range(2)]
        c.hy_pr = [sb(c, 'hy_pr%d' % i, [128, 2, 512], BF16) for i in range(2)]
        c.hy_t4 = [sb(c, 'hy_t4%d' % i, [128, 4, 512], F32) for i in range(2)]
        c.hy_qo = [sb(c, 'hy_qo%d' % i, [128, 2, 512], BF16) for i in range(2)]
        c.hy_qin = [sb(c, 'hy_qin%d' % i, [128, 2, 512], BF16) for i in range(2)]
        c.hy_twf = [sb(c, 'hy_twf%d' % i, [128, 2, 64], BF16) for i in range(2)]
        c.hy_zg = [sb(c, 'hy_zg%d' % i, [64, 2, 512], BF16) for i in range(2)]
        c.hy_ya = [sb(c, 'hy_ya%d' % i, [64, 512], F32) for i in range(2)]
        c.hy_yb = [sb(c, 'hy_yb%d' % i, [64, 512], F32) for i in range(2)]
        c.hy_yo = [sb(c, 'hy_yo%d' % i, [64, 512], BF16) for i in range(2)]
        tabs = sb(c, 'hy_tabs', [128, 2, 2, 128], BF16)
        c.hy_E3 = sb(c, 'hy_E3', [128, 3, 128], BF16)
        scl = sb(c, 'hy_scl', [64, 2, 512], F32)
        bias = sb(c, 'hy_bias', [64, 2, 512], F32)
        p.dma('sp', 'hy_tab', [], ['hy_tab'],
              [(tabs[:, :, :, :], I['hy_dft1'][:, :, :, :]), (c.hy_E3[:, :, :], I['hy_e3'][:, :, :])])
        twres = sb(c, 'hy_twres', [128, 128, 3, 128], BF16)
        p.dma('sp', 'hy_twres', [], ['hy_twres'],
              [(twres[:, g * 16:(g + 1) * 16, :, :], I['hy_tw2'][g * 16:(g + 1) * 16].rearrange("f b k g -> b f k g")) for g in range(8)])
        for job in jobs:
            na = 64 if job == 0 else 2
            tok0 = NCTX if job == 0 else 0
            nt = 128 if job == 0 else 4
            ftab = tabs[:, job, :, :]
            NF1 = 128 if job == 0 else 4
            tw2n = 'hy_tw2' if job == 0 else 'hy_tw2c'
            twfn = 'hy_twf' if job == 0 else 'hy_twfc'
            Mm = 64 if job == 0 else 2
            KTD, KS, X1D, QD = c.KTD[job], c.KS, c.X1D, c.QD
            for o in range(2):
                ksrc = KTD[:, o * 512:(o + 1) * 512].rearrange("(a b) c -> a b c", b=128)
                hy_fwd1(c, lambda b0, nb, ksrc=ksrc: ksrc[:, b0:b0 + nb, :], nt, ftab, X1D, NF1)
                if hp >= 4:
                    hy_stage2(c, X1D, 'filter', KS[o], None, NF1, tw2n, twres if job == 0 else None)
            if hp < 5:
                continue
            p.dma('sp', 'hy_scl', [('SCL', job)], ['hy_scl'],
                  [(scl[:, o, :], c.SCL[job][0, o * 512:(o + 1) * 512].partition_broadcast(64)) for o in range(2)] +
                  [(bias[:, o, :], I['hy_bias'][l, o, :].partition_broadcast(64)) for o in range(2)])
            hyc = c.HYC[tok0:tok0 + na * 128, :].rearrange("(a b) c -> a b c", b=128)
            z2 = c.Z2[tok0:tok0 + na * 128, :].rearrange("(a b) c -> a b c", b=128)
            oh = c.OH[tok0:tok0 + na * 128, :].rearrange("(a b) c -> a b c", b=128)
            vsrc = hyc[:, :, 0:512]
            hy_fwd1(c, lambda b0, nb: vsrc[:, b0:b0 + nb, :], na, ftab, X1D, NF1)
            hy_stage2(c, X1D, 'conv', KS[0], QD, NF1, tw2n, twres if job == 0 else None)
            if hp < 6:
                continue
            hy_final(c, QD, na, scl[:, 0, :], bias[:, 0, :], vsrc, hyc[:, :, 512:1024], z2, NF1, twfn, Mm)
            if hp < 7:
                continue
            hy_fwd1(c, lambda b0, nb: z2[:, b0:b0 + nb, :], na, ftab, X1D, NF1)
            hy_stage2(c, X1D, 'conv', KS[1], QD, NF1, tw2n, twres if job == 0 else None)
            hy_final(c, QD, na, scl[:, 1, :], bias[:, 1, :], z2, hyc[:, :, 1024:1536], oh, NF1, twfn, Mm)
        p.barrier()


def ln_affine_store(c, r, rkey, gb, gbkey, dsts, slot):
    p = c.p
    st = c.e_st[slot]
    junk = c.e_junk
    p.op('act', [rkey], ['e_junk', ('e_st', slot, 0)],
         lambda E: E.activation(out=junk[:, :], in_=r[:, :], func=AF.Copy, accum_out=st[:, 0:1]))
    p.op('dve', [('e_st', slot, 0)], [('e_st', slot, 1)],
         lambda E: E.tensor_scalar(out=st[:, 1:2], in0=st[:, 0:1], scalar1=-1.0 / D, scalar2=None, op0=ALU.mult))
    p.op('act', [rkey, ('e_st', slot, 1)], ['e_junk', ('e_st', slot, 2)],
         lambda E: E.activation(out=junk[:, :], in_=r[:, :], func=AF.Square, bias=st[:, 1:2], scale=1.0, accum_out=st[:, 2:3]))
    p.op('act', [('e_st', slot, 2)], [('e_st', slot, 3)],
         lambda E: E.activation(out=st[:, 3:4], in_=st[:, 2:3], func=AF.Ln, scale=1.0 / D, bias=LN_EPS))
    p.op('act', [('e_st', slot, 3)], [('e_st', slot, 4)],
         lambda E: E.activation(out=st[:, 4:5], in_=st[:, 3:4], func=AF.Exp, scale=-0.5))
    p.op('dve', [rkey, ('e_st', slot, 1), ('e_st', slot, 4)], [rkey],
         lambda E: E.tensor_scalar(out=r[:, :], in0=r[:, :], scalar1=st[:, 1:2], scalar2=st[:, 4:5], op0=ALU.add, op1=ALU.mult))
    p.op('pool', [rkey, gbkey], [rkey], lambda E: E.tensor_tensor(out=r[:, :], in0=r[:, :], in1=gb[:, 0, :], op=ALU.mult))
    p.op('pool', [rkey, gbkey], [rkey], lambda E: E.tensor_tensor(out=r[:, :], in0=r[:, :], in1=gb[:, 1, :], op=ALU.add))
    p.dma('sp', 'e_r%d' % slot, [rkey], ['XOUT'], [(d, r[:, :]) for d in dsts])


def load_bc_rows(c, l, which):
    p = c.p
    I = c.inp
    g = 2 if which == 1 else 5
    lg, lb = ('ln1_g', 'ln1_b') if which == 1 else ('ln2_g', 'ln2_b')
    p.dma('sp', 'e_bc', [('modv', l)], ['e_bc'],
          [(c.e_ag[:, r, :], c.modv[l][r, g * 1024:(g + 1) * 1024].partition_broadcast(128)) for r in range(2)] +
          [(c.e_gb[:, 0, :], I[lg][l, :].partition_broadcast(128)), (c.e_gb[:, 1, :], I[lb][l, :].partition_broadcast(128))])


def stage_merge(c, l, xsrc, tiles):
    p = c.p
    I = c.inp
    S = c.scr[l]
    B = c.bank
    bk = lambda i: ('bank', i)
    with ExitStack() as es:
        c.es = es
        wbr = sb(c, 'mg_wbr', [128, 3, 4, 1024], BF16)
        wout = sb(c, 'mg_wout', [128, 8, 1024], BF16)
        c.e_ag = sb(c, 'e_ag', [128, 2, 1024], F32)
        c.e_gb = sb(c, 'e_gb', [128, 2, 1024], F32)
        c.e_st = [sb(c, 'e_st%d' % i, [128, 8], F32) for i in range(2)]
        c.e_junk = sb(c, 'e_junk', [128, 1024], BF16)
        oT = [sb(c, 'mg_oT%d' % i, [128, 3, 4, 128], BF16) for i in range(2)]
        oh = [sb(c, 'mg_oh%d' % i, [128, 512], BF16) for i in range(2)]
        g3 = [sb(c, 'mg_g3%d' % i, [128, 3072], BF16) for i in range(2)]
        y = [sb(c, 'mg_y%d' % i, [128, 1024], F32) for i in range(2)]
        tt = [sb(c, 'mg_t%d' % i, [128, 1024], F32) for i in range(2)]
        yb = [sb(c, 'mg_yb%d' % i, [128, 1024], BF16) for i in range(2)]
        yT = [sb(c, 'mg_yT%d' % i, [128, 8, 128], BF16) for i in range(2)]
        xt = [sb(c, 'mg_xt%d' % i, [128, 1024], F32) for i in range(2)]
        for br, nm in enumerate(['w_br_gla', 'w_br_mla', 'w_br_hy']):
            p.dma('pool', 'mg_w', [], ['mg_w'], [(wbr[:, br, :, :], I[nm][l].rearrange("(k p) n -> p k n", p=128))])
        p.dma('pool', 'mg_w', [], ['mg_w'], [(wout[:, :, :], I['w_out'][l].rearrange("(k p) n -> p k n", p=128))])
        load_bc_rows(c, l, 1)
        for i, t in enumerate(tiles):
            s2 = i % 2
            r = 1 if t < 2 else 0
            tok = slice(t * 128, (t + 1) * 128)
            p.dma('sp', 'mg_oT%d' % s2, [('OGT', l), ('OMT', l)], [('mg_oT', s2)],
                  [(oT[s2][:, 0, :, :], S['OGT'][:, tok].rearrange("(k p) t -> p k t", p=128)),
                   (oT[s2][:, 1, :, :], S['OMT'][:, tok].rearrange("(k p) t -> p k t", p=128))])
            p.dma('sp', 'mg_oh%d' % s2, ['OH'], [('mg_oh', s2)], [(oh[s2][:, :], c.OH[tok, :])])
            p.dma('sp', 'mg_g3%d' % s2, [('G3', l)], [('mg_g3', s2)], [(g3[s2][:, :], S['G3'][tok, :])])
            p.dma('sp', 'mg_xt%d' % s2, ['XOUT'], [('mg_xt', s2)], [(xt[s2][:, :], xsrc(t))])
            tpv = bank16(c, 6)

            def tro(E):
                ins = None
                for k in range(4):
                    ins = E.transpose(tpv[:, k, :], oh[s2][:, k * 128:(k + 1) * 128], c.ident[:, :])
                return ins
            p.op('pe', [('mg_oh', s2), 'ident'], [bk(6)], tro)
            p.op('act', [bk(6)], [('mg_oT', s2)], lambda E: E.activation(out=oT[s2][:, 2, :, :], in_=tpv[:, 0:4, :], func=AF.Copy))
            for br in range(3):
                for half in range(2):
                    bb = (br * 2 + half) % 4

                    def mmb(E):
                        ins = None
                        for k in range(4):
                            ins = E.matmul(B[bb][:, :], lhsT=oT[s2][:, br, k, :], rhs=wbr[:, br, k, half * 512:(half + 1) * 512],
                                           start=(k == 0), stop=(k == 3))
                        return ins
                    p.op('pe', [('mg_oT', s2), 'mg_w'], [bk(bb)], mmb)
                    hs = slice(half * 512, (half + 1) * 512)
                    gs = slice(br * 1024 + half * 512, br * 1024 + (half + 1) * 512)
                    if br == 0:
                        p.op('dve', [bk(bb), ('mg_g3', s2)], [('mg_y', s2, half)],
                             lambda E: E.tensor_tensor(out=y[s2][:, hs], in0=B[bb][:, :], in1=g3[s2][:, gs], op=ALU.mult))
                    else:
                        p.op('dve', [bk(bb), ('mg_g3', s2)], [('mg_t', s2, half)],
                             lambda E: E.tensor_tensor(out=tt[s2][:, hs], in0=B[bb][:, :], in1=g3[s2][:, gs], op=ALU.mult))
                        p.op('pool', [('mg_t', s2, half), ('mg_y', s2, half)], [('mg_y', s2, half)],
                             lambda E: E.tensor_tensor(out=y[s2][:, hs], in0=y[s2][:, hs], in1=tt[s2][:, hs], op=ALU.add))
            p.op('act', [('mg_y', s2, 0), ('mg_y', s2, 1)], [('mg_yb', s2)],
                 lambda E: E.activation(out=yb[s2][:, :], in_=y[s2][:, :], func=AF.Copy))
            tp7 = bank16(c, 7)

            def try_(E):
                ins = None
                for k in range(8):
                    ins = E.transpose(tp7[:, k, :], yb[s2][:, k * 128:(k + 1) * 128], c.ident[:, :])
                return ins
            p.op('pe', [('mg_yb', s2), 'ident'], [bk(7)], try_)
            p.op('act', [bk(7)], [('mg_yT', s2)], lambda E: E.activation(out=yT[s2][:, :, :], in_=tp7[:, :, :], func=AF.Copy))
            for half in range(2):
                bb = 4 + half

                def mmo(E):
                    ins = None
                    for k in range(8):
                        ins = E.matmul(B[bb][:, :], lhsT=yT[s2][:, k, :], rhs=wout[:, k, half * 512:(half + 1) * 512],
                                       start=(k == 0), stop=(k == 7))
                    return ins
                p.op('pe', [('mg_yT', s2), 'mg_w'], [bk(bb)], mmo)
                hs = slice(half * 512, (half + 1) * 512)
                p.op('dve', [bk(bb), 'e_bc'], [('mg_t', s2, half)],
                     lambda E: E.tensor_tensor(out=tt[s2][:, hs], in0=B[bb][:, :], in1=c.e_ag[:, r, hs], op=ALU.mult))
                p.op('dve', [('mg_t', s2, half), ('mg_xt', s2)], [('mg_xt', s2)],
                     lambda E: E.scalar_tensor_tensor(out=xt[s2][:, hs], in0=xt[s2][:, hs], scalar=ALPHA, in1=tt[s2][:, hs],
                                                      op0=ALU.mult, op1=ALU.add))
            ln_affine_store(c, xt[s2], ('mg_xt', s2), c.e_gb, 'e_bc', [c.X1[tok, :]], s2)
        p.barrier()


def moe_precast(c, l):
    p = c.p
    I = c.inp
    for e in range(32):
        if c.cfg.get('moe_mode', 'sparse') == 'sparse':
            p.dma('pool', 'wcast', [], [('WB', l, e)],
                  [(c.WB1[l][e].rearrange("(p k) n -> p k n", k=8), I['moe_w1'][l, e].rearrange("(k p) n -> p k n", p=128)),
                   (c.WB2[l][e].rearrange("(p k) n -> p k n", k=8), I['moe_w2'][l, e].rearrange("(k p) n -> p k n", p=128))])
        else:
            p.dma('pool', 'wcast', [], [('WB', l, e)],
                  [(c.WB1[l][e], I['moe_w1'][l, e]), (c.WB2[l][e], I['moe_w2'][l, e])])


def stage_moe(c, l, tiles, dst_fn):
    p = c.p
    I = c.inp
    B = c.bank
    bk = lambda i: ('bank', i)
    cst = c.cst
    with ExitStack() as es:
        c.es = es
        c.e_ag = sb(c, 'e_ag', [128, 2, 1024], F32)
        c.e_gb = sb(c, 'e_gb', [128, 2, 1024], F32)
        c.e_st = [sb(c, 'e_st%d' % i, [128, 8], F32) for i in range(2)]
        c.e_junk = sb(c, 'e_junk', [128, 1024], BF16)
        w1 = [sb(c, 'mo_w1%d' % i, [128, 8, 2048], BF16) for i in range(2)]
        w2 = [sb(c, 'mo_w2%d' % i, [128, 8, 1024], BF16) for i in range(1)] * 2
        hT = sb(c, 'mo_hT', [128, 8, 1024], BF16)
        aT = sb(c, 'mo_aT', [128, 8, 1024], BF16)
        yacc = sb(c, 'mo_yacc', [128, 8, 1024], F32)
        rw = sb(c, 'mo_rw', [128, 8, 32], F32)
        rb = sb(c, 'mo_rb', [1, 32], F32)
        b1 = sb(c, 'mo_b1', [128, 32, 16], F32)
        b2 = sb(c, 'mo_b2', [32, 1024], F32)
        G = sb(c, 'mo_G', [128, 8, 32], F32)
        GT = sb(c, 'mo_GT', [32, 8, 128], F32)
        xt = [sb(c, 'mo_xt%d' % i, [128, 1024], F32) for i in range(2)]
        xh = [sb(c, 'mo_xh%d' % i, [128, 1024], F32) for i in range(1)] * 2
        h32 = [sb(c, 'mo_h32%d' % i, [128, 8, 128], F32) for i in range(1)] * 2
        lg = [sb(c, 'mo_lg%d' % i, [128, 32], F32) for i in range(2)]
        mx = [sb(c, 'mo_mx%d' % i, [128, 8], F32) for i in range(2)]
        ex = [sb(c, 'mo_ex%d' % i, [128, 32], F32) for i in range(2)]
        sm = [sb(c, 'mo_sm%d' % i, [128, 2], F32) for i in range(2)]
        gg = [sb(c, 'mo_gg%d' % i, [128, 512], F32) for i in range(2)]
        sg = [sb(c, 'mo_sg%d' % i, [128, 512], F32) for i in range(2)]
        ll = [sb(c, 'mo_ll%d' % i, [128, 512], F32) for i in range(2)]
        st = [sb(c, 'mo_st%d' % i, [128, 8], F32) for i in range(2)]
        p.dma('sp', 'mo_c', [], ['mo_c'],
              [(rw[:, :, :], I['router_w'][l].rearrange("(k p) e -> p k e", p=128)),
               (rb[:, :], I['router_b'][l:l + 1, :]), (b2[:, :], I['moe_b2'][l])])
        p.dma('sp', 'mo_b1', [], ['mo_b1'],
              [(b1[:, e, :], I['moe_b1'][l, e, :].rearrange("(j p) -> p j", p=128)) for e in range(32)], slow=True)
        load_bc_rows(c, l, 2)
        ones = cst[:, 640:768]
        ident32 = cst[:, 0:128]
        groups = [tiles[i:i + 8] for i in range(0, len(tiles), 8)][:c.cfg.get('moe_groups', 99)]
        wi = 0
        for gi, gt in enumerate(groups):
            ng = len(gt)
            T = ng * 128
            for j, t in enumerate(gt):
                s2 = j % 2
                r = 1 if t < 2 else 0
                tok = slice(t * 128, (t + 1) * 128)
                p.dma('sp', 'mo_xt%d' % s2, ['XOUT'], [('mo_xt', s2)], [(xt[s2][:, :], c.X1[tok, :])])
                s_ = st[s2]
                p.op('act', [('mo_xt', s2)], ['e_junk', ('mo_st', s2, 0)],
                     lambda E: E.activation(out=c.e_junk[:, :], in_=xt[s2][:, :], func=AF.Copy, accum_out=s_[:, 0:1]))
                p.op('dve', [('mo_st', s2, 0)], [('mo_st', s2, 1)],
                     lambda E: E.tensor_scalar(out=s_[:, 1:2], in0=s_[:, 0:1], scalar1=-1.0 / D, scalar2=None, op0=ALU.mult))
                p.op('act', [('mo_xt', s2), ('mo_st', s2, 1)], ['e_junk', ('mo_st', s2, 2)],
                     lambda E: E.activation(out=c.e_junk[:, :], in_=xt[s2][:, :], func=AF.Square, bias=s_[:, 1:2], scale=1.0,
                                            accum_out=s_[:, 2:3]))
                p.op('act', [('mo_st', s2, 2)], [('mo_st', s2, 3)],
                     lambda E: E.activation(out=s_[:, 3:4], in_=s_[:, 2:3], func=AF.Ln, scale=1.0 / D, bias=LN_EPS))
                p.op('act', [('mo_st', s2, 3)], [('mo_st', s2, 4)],
                     lambda E: E.activation(out=s_[:, 4:5], in_=s_[:, 3:4], func=AF.Exp, scale=-0.5))
                p.op('dve', [('mo_xt', s2), ('mo_st', s2, 1), ('mo_st', s2, 4)], [('mo_xh', 0)],
                     lambda E: E.tensor_scalar(out=xh[s2][:, :], in0=xt[s2][:, :], scalar1=s_[:, 1:2], scalar2=s_[:, 4:5],
                                               op0=ALU.add, op1=ALU.mult))
                for hh in range(2):
                    bb = hh
                    def tr32(E):
                        ins = None
                        for k in range(4):
                            kk = hh * 4 + k
                            ins = E.transpose(B[bb][:, k * 128:(k + 1) * 128], xh[s2][:, kk * 128:(kk + 1) * 128], ident32)
                        return ins
                    p.op('pe', [('mo_xh', 0), 'cst'], [bk(bb)], tr32)
                    for k in range(4):
                        kk = hh * 4 + k
                        p.op('dve', [bk(bb), 'modT'], [('mo_h32', 0, kk)],
                             lambda E: E.tensor_scalar(out=h32[s2][:, kk, :], in0=B[bb][:, k * 128:(k + 1) * 128],
                                                       scalar1=c.modT[:, r, 4, kk:kk + 1], scalar2=c.modT[:, r, 3, kk:kk + 1],
                                                       op0=ALU.mult, op1=ALU.add))
                hk = [('mo_h32', 0, kk) for kk in range(8)]
                p.op('pool', hk, [('mo_hT', j)], lambda E: E.tensor_copy(out=hT[:, :, j * 128:(j + 1) * 128], in_=h32[s2][:, :, :]))

                def mml(E):
                    for kk in range(8):
                        E.matmul(B[2][:, 0:32], lhsT=h32[s2][:, kk, :], rhs=rw[:, kk, :], start=(kk == 0), stop=False)
                    return E.matmul(B[2][:, 0:32], lhsT=ones[0:1, :], rhs=rb[0:1, :], start=False, stop=True)
                p.op('pe', hk + ['mo_c', 'cst'], [bk(2)], mml)
                p.op('dve', [bk(2)], [('mo_lg', s2)], lambda E: E.tensor_copy(out=lg[s2][:, :], in_=B[2][:, 0:32]))
                p.op('dve', [('mo_lg', s2)], [('mo_mx', s2)], lambda E: E.max(out=mx[s2][:, :], in_=lg[s2][:, :]))
                p.op('dve', [('mo_mx', s2)], [('mo_sm', s2, 0)],
                     lambda E: E.tensor_scalar(out=sm[s2][:, 0:1], in0=mx[s2][:, 0:1], scalar1=-1.0, scalar2=None, op0=ALU.mult))
                p.op('act', [('mo_lg', s2), ('mo_sm', s2, 0)], [('mo_ex', s2)],
                     lambda E: E.activation(out=ex[s2][:, :], in_=lg[s2][:, :], func=AF.Exp, bias=sm[s2][:, 0:1], scale=1.0))
                p.op('dve', [('mo_lg', s2), ('mo_mx', s2)], [('mo_lg', s2)],
                     lambda E: E.tensor_scalar(out=lg[s2][:, :], in0=lg[s2][:, :], scalar1=mx[s2][:, 3:4], scalar2=None, op0=ALU.is_ge))
                p.op('dve', [('mo_lg', s2), ('mo_ex', s2)], [('mo_ex', s2)],
                     lambda E: E.tensor_tensor(out=ex[s2][:, :], in0=ex[s2][:, :], in1=lg[s2][:, :], op=ALU.mult))
                p.op('dve', [('mo_ex', s2)], [('mo_sm', s2, 1)],
                     lambda E: E.tensor_reduce(out=sm[s2][:, 1:2], in_=ex[s2][:, :], axis=AX.X, op=ALU.add))
                p.op('dve', [('mo_sm', s2, 1)], [('mo_sm', s2, 1)], lambda E: E.reciprocal(out=sm[s2][:, 1:2], in_=sm[s2][:, 1:2]))
                p.op('dve', [('mo_ex', s2), ('mo_sm', s2, 1)], [('mo_G', j)],
                     lambda E: E.tensor_scalar(out=G[:, j, :], in0=ex[s2][:, :], scalar1=sm[s2][:, 1:2], scalar2=None, op0=ALU.mult))
                p.op('pe', [('mo_G', j), 'cst'], [bk(3)], lambda E: E.transpose(B[3][0:32, 0:128], G[:, j, :], ident32))
                p.op('act', [bk(3)], [('mo_GT', j)], lambda E: E.activation(out=GT[:, j, :], in_=B[3][0:32, 0:128], func=AF.Copy))
                for half in range(2):
                    p.op('pe', [('mo_GT', j), 'mo_c'], [bk(4 + half)],
                         lambda E: E.matmul(B[4 + half][:, :], lhsT=GT[:, j, :], rhs=b2[:, half * 512:(half + 1) * 512], start=True, stop=True))
                    p.op('act', [bk(4 + half)], [('mo_yacc', j, half)],
                         lambda E: E.activation(out=yacc[:, j, half * 512:(half + 1) * 512], in_=B[4 + half][:, :], func=AF.Copy))
            hTk = [('mo_hT', j) for j in range(ng)]
            ei = 0
            for e in range(32):
                ws = wi % 2
                wi += 1
                p.dma('sp', 'mo_w1%d' % ws, [('WB', l, e)], [('mo_w1', ws)],
                      [(w1[ws][:, :, :], c.WB1[l][e].rearrange("(k p) n -> p k n", p=128))])
                p.dma('sp', 'mo_w2', [('WB', l, e)], [('mo_w2', 0)],
                      [(w2[ws][:, :, :], c.WB2[l][e].rearrange("(k p) n -> p k n", p=128))])
                for th in range((T + 511) // 512):
                    c0 = th * 512
                    n = min(512, T - c0)
                    for fc in range(8):
                        s2 = ei % 2
                        ei += 1
                        bg, bl = s2 * 2, s2 * 2 + 1

                        def mm1(E):
                            ins = None
                            for k in range(8):
                                E.matmul(B[bg][:, 0:n], lhsT=w1[ws][:, k, fc * 128:(fc + 1) * 128], rhs=hT[:, k, c0:c0 + n],
                                         start=(k == 0), stop=(k == 7))
                            for k in range(8):
                                ins = E.matmul(B[bl][:, 0:n], lhsT=w1[ws][:, k, 1024 + fc * 128:1024 + (fc + 1) * 128],
                                               rhs=hT[:, k, c0:c0 + n], start=(k == 0), stop=(k == 7))
                            return ins
                        p.op('pe', hTk + [('mo_w1', ws)], [bk(bg), bk(bl)], mm1)
                        p.op('dve', [bk(bg), 'mo_b1'], [('mo_gg', s2)],
                             lambda E: E.tensor_scalar(out=gg[s2][:, 0:n], in0=B[bg][:, 0:n], scalar1=b1[:, e, fc:fc + 1], scalar2=7.0,
                                                       op0=ALU.add, op1=ALU.min))
                        p.op('act', [('mo_gg', s2)], [('mo_sg', s2)],
                             lambda E: E.activation(out=sg[s2][:, 0:n], in_=gg[s2][:, 0:n], func=AF.Sigmoid, scale=1.702))
                        p.op('dve', [bk(bl), 'mo_b1'], [('mo_ll', s2)],
                             lambda E: E.tensor_scalar(out=ll[s2][:, 0:n], in0=B[bl][:, 0:n], scalar1=b1[:, e, 8 + fc:9 + fc], scalar2=7.0,
                                                       op0=ALU.add, op1=ALU.min))
                        p.op('pool', [('mo_ll', s2)], [('mo_ll', s2)],
                             lambda E: E.tensor_scalar(out=ll[s2][:, 0:n], in0=ll[s2][:, 0:n], scalar1=-7.0, scalar2=1.0,
                                                       op0=ALU.max, op1=ALU.add))
                        p.op('pool', [('mo_gg', s2), ('mo_sg', s2)], [('mo_gg', s2)],
                             lambda E: E.tensor_tensor(out=gg[s2][:, 0:n], in0=gg[s2][:, 0:n], in1=sg[s2][:, 0:n], op=ALU.mult))
                        p.op('dve', [('mo_gg', s2), ('mo_ll', s2)], [('mo_aT', fc, th)],
                             lambda E: E.tensor_tensor(out=aT[:, fc, c0:c0 + n], in0=gg[s2][:, 0:n], in1=ll[s2][:, 0:n], op=ALU.mult))
                aTk = [('mo_aT', fc, th) for fc in range(8) for th in range((T + 511) // 512)]
                for j in range(ng):
                    for half in range(2):
                        bb = 4 + (j * 2 + half) % 4

                        def mm2(E):
                            ins = None
                            for k in range(8):
                                ins = E.matmul(B[bb][:, :], lhsT=aT[:, k, j * 128:(j + 1) * 128], rhs=w2[ws][:, k, half * 512:(half + 1) * 512],
                                               start=(k == 0), stop=(k == 7))
                            return ins
                        p.op('pe', aTk + [('mo_w2', 0)], [bk(bb)], mm2)
                        hs = slice(half * 512, (half + 1) * 512)
                        p.op('dve', [bk(bb), ('mo_G', j), ('mo_yacc', j, half)], [('mo_yacc', j, half)],
                             lambda E: E.scalar_tensor_tensor(out=yacc[:, j, hs], in0=B[bb][:, :], scalar=G[:, j, e:e + 1], in1=yacc[:, j, hs],
                                                              op0=ALU.mult, op1=ALU.add))
            for j, t in enumerate(gt):
                s2 = j % 2
                r = 1 if t < 2 else 0
                tok = slice(t * 128, (t + 1) * 128)
                p.dma('sp', 'mo_xt%d' % s2, ['XOUT'], [('mo_xt', s2)], [(xt[s2][:, :], c.X1[tok, :])])
                yk = [('mo_yacc', j, 0), ('mo_yacc', j, 1)]
                p.op('pool', yk + ['e_bc'], yk, lambda E: E.tensor_tensor(out=yacc[:, j, :], in0=yacc[:, j, :], in1=c.e_ag[:, r, :], op=ALU.mult))
                p.op('dve', yk + [('mo_xt', s2)], [('mo_xt', s2)],
                     lambda E: E.scalar_tensor_tensor(out=xt[s2][:, :], in0=xt[s2][:, :], scalar=ALPHA, in1=yacc[:, j, :],
                                                      op0=ALU.mult, op1=ALU.add))
                ln_affine_store(c, xt[s2], ('mo_xt', s2), c.e_gb, 'e_bc', dst_fn(t), s2)
        p.barrier()


def stage_moe2(c, l, tiles, dst_fn):
    p = c.p
    I = c.inp
    B = c.bank
    bk = lambda i: ('bank', i)
    cst = c.cst
    ones = cst[:, 640:768]
    ident32 = cst[:, 0:128]
    iota_f = cst[:, 768:800]
    iota_p = cst[:, 800:801]
    blk512 = cst[:, 896:1024]
    ntl = len(tiles)
    NB = (ntl * 128 * 4) // 512 + 32
    XB, YB, HB = c.XB, c.YB, c.HB
    W1f = c.WB1[l].rearrange("e (p k) n -> (e p) (k n)", k=8)
    W2f = c.WB2[l].rearrange("e (p k) n -> (e p) (k n)", k=8)
    I32 = mybir.dt.int32
    with ExitStack() as es_outer:
        c.es = es_outer
        slot_i = sb(c, 'ms_slot', [128, NTILE, 4], I32)
        gk = sb(c, 'ms_gk', [128, NTILE, 4], F32)
        idxw = sb(c, 'ms_idxw', [128, NB, 8], I32)
        be_bc = sb(c, 'ms_bebc', [128, NB], F32)
        OHall = sb(c, 'ms_oh', [32, NB], F32)
        c.e_ag = sb(c, 'e_ag', [128, 2, 1024], F32)
        c.e_gb = sb(c, 'e_gb', [128, 2, 1024], F32)
        c.e_st = [sb(c, 'e_st%d' % i, [128, 8], F32) for i in range(2)]
        c.e_junk = sb(c, 'e_junk', [128, 1024], BF16)
        load_bc_rows(c, l, 2)
        with ExitStack() as es:
            c.es = es
            Mall = sb(c, 'ms_M', [128, NTILE, 32], F32)
            Gall = sb(c, 'ms_G', [128, NTILE, 32], F32)
            Lall = sb(c, 'ms_L', [128, NTILE, 32], F32)
            mxall = sb(c, 'ms_mx', [128, NTILE, 8], F32)
            rw = sb(c, 'ms_rw', [128, 8, 32], F32)
            rb = sb(c, 'ms_rb', [1, 32], F32)
            modbc = sb(c, 'ms_modbc', [128, 2, 2, 1024], F32)
            xt = [sb(c, 'ms_xt%d' % i, [128, 1024], F32) for i in range(2)]
            xh = sb(c, 'ms_xh', [128, 1024], F32)
            hb = [sb(c, 'ms_hb%d' % i, [128, 1024], BF16) for i in range(2)]
            h32 = sb(c, 'ms_h32', [128, 8, 128], F32)
            ex = [sb(c, 'ms_ex%d' % i, [128, 32], F32) for i in range(2)]
            sm = [sb(c, 'ms_sm%d' % i, [128, 2], F32) for i in range(2)]
            st = [sb(c, 'ms_st%d' % i, [128, 8], F32) for i in range(2)]
            Lst = sb(c, 'ms_Lst', [128, 128], F32)
            Msum = sb(c, 'ms_Msum', [128, 32], F32)
            cnt = sb(c, 'ms_cnt', [1, 32], F32)
            cnti = sb(c, 'ms_cnti', [1, 32], I32)
            pcol = sb(c, 'ms_pcol', [32, 4], F32)
            psbc = sb(c, 'ms_psbc', [128, 32], F32)
            cmp_ = sb(c, 'ms_cmp', [32, 128], F32)
            berow = sb(c, 'ms_berow', [1, 128], F32)
            tmpf = sb(c, 'ms_tmpf', [128, 128], F32)
            pos = [sb(c, 'ms_pos%d' % i, [128, 32], F32) for i in range(2)]
            oh = [sb(c, 'ms_ohk%d' % i, [128, 32], F32) for i in range(2)]
            tq = [sb(c, 'ms_tq%d' % i, [128, 32], F32) for i in range(2)]
            sl = [sb(c, 'ms_sl%d' % i, [128, 4], F32) for i in range(2)]
            p.dma('sp', 'ms_c', [], ['ms_c'],
                  [(rw[:, :, :], I['router_w'][l].rearrange("(k p) e -> p k e", p=128)), (rb[:, :], I['router_b'][l:l + 1, :])])
            p.dma('sp', 'ms_modbc', [('modv', l)], ['ms_modbc'],
                  [(modbc[:, r, q, :], c.modv[l][r, (4 - q) * 1024:(5 - q) * 1024].partition_broadcast(128))
                   for r in range(2) for q in range(2)])
            for r in range(2):
                p.op('dve', ['ms_modbc'], ['ms_modbc'],
                     lambda E: E.tensor_scalar(out=modbc[:, r, 0, :], in0=modbc[:, r, 0, :], scalar1=1.0, scalar2=None, op0=ALU.add))
            p.op('dve', ['cst'], ['ms_Lst'], lambda E: E.tensor_tensor(out=Lst[:, :], in0=cst[:, 384:512], in1=ident32, op=ALU.subtract))
            for jj, t in enumerate(tiles):
                s2 = jj % 2
                r = 1 if t < 2 else 0
                tok = slice(t * 128, (t + 1) * 128)
                p.dma('sp', 'ms_xt%d' % s2, ['XOUT'], [('ms_xt', s2)], [(xt[s2][:, :], c.X1[tok, :])])
                s_ = st[s2]
                p.op('act', [('ms_xt', s2)], ['e_junk', ('ms_st', s2, 0)],
                     lambda E: E.activation(out=c.e_junk[:, :], in_=xt[s2][:, :], func=AF.Copy, accum_out=s_[:, 0:1]))
                p.op('dve', [('ms_st', s2, 0)], [('ms_st', s2, 1)],
                     lambda E: E.tensor_scalar(out=s_[:, 1:2], in0=s_[:, 0:1], scalar1=-1.0 / D, scalar2=None, op0=ALU.mult))
                p.op('act', [('ms_xt', s2), ('ms_st', s2, 1)], ['e_junk', ('ms_st', s2, 2)],
                     lambda E: E.activation(out=c.e_junk[:, :], in_=xt[s2][:, :], func=AF.Square, bias=s_[:, 1:2], scale=1.0,
                                            accum_out=s_[:, 2:3]))
                p.op('act', [('ms_st', s2, 2)], [('ms_st', s2, 3)],
                     lambda E: E.activation(out=s_[:, 3:4], in_=s_[:, 2:3], func=AF.Ln, scale=1.0 / D, bias=LN_EPS))
                p.op('act', [('ms_st', s2, 3)], [('ms_st', s2, 4)],
                     lambda E: E.activation(out=s_[:, 4:5], in_=s_[:, 3:4], func=AF.Exp, scale=-0.5))
                p.op('dve', [('ms_xt', s2), ('ms_st', s2, 1), ('ms_st', s2, 4)], ['ms_xh'],
                     lambda E: E.tensor_scalar(out=xh[:, :], in0=xt[s2][:, :], scalar1=s_[:, 1:2], scalar2=s_[:, 4:5],
                                               op0=ALU.add, op1=ALU.mult))
                p.op('pool', ['ms_xh', 'ms_modbc'], [('ms_xt', s2)],
                     lambda E: E.tensor_tensor(out=xt[s2][:, :], in0=xh[:, :], in1=modbc[:, r, 0, :], op=ALU.mult))
                p.op('pool', [('ms_xt', s2), 'ms_modbc'], [('ms_hb', s2)],
                     lambda E: E.tensor_tensor(out=hb[s2][:, :], in0=xt[s2][:, :], in1=modbc[:, r, 1, :], op=ALU.add))
                p.dma('sp', 'ms_hb%d' % s2, [('ms_hb', s2)], [('HB', t)], [(HB[tok, :], hb[s2][:, :])])
                for hh in range(2):
                    def tr32(E):
                        ins = None
                        for k in range(4):
                            kk = hh * 4 + k
                            ins = E.transpose(B[hh][:, k * 128:(k + 1) * 128], xh[:, kk * 128:(kk + 1) * 128], ident32)
                        return ins
                    p.op('pe', ['ms_xh', 'cst'], [bk(hh)], tr32)
                    for k in range(4):
                        kk = hh * 4 + k
                        p.op('dve', [bk(hh), 'modT'], [('ms_h32', kk)],
                             lambda E: E.tensor_scalar(out=h32[:, kk, :], in0=B[hh][:, k * 128:(k + 1) * 128],
                                                       scalar1=c.modT[:, r, 4, kk:kk + 1], scalar2=c.modT[:, r, 3, kk:kk + 1],
                                                       op0=ALU.mult, op1=ALU.add))
                hk = [('ms_h32', kk) for kk in range(8)]

                def mml(E):
                    for kk in range(8):
                        E.matmul(B[2][:, 0:32], lhsT=h32[:, kk, :], rhs=rw[:, kk, :], start=(kk == 0), stop=False)
                    return E.matmul(B[2][:, 0:32], lhsT=ones[0:1, :], rhs=rb[0:1, :], start=False, stop=True)
                p.op('pe', hk + ['ms_c', 'cst'], [bk(2)], mml)
                lgj, mxj, Mj, Gj = Lall[:, t, :], mxall[:, t, :], Mall[:, t, :], Gall[:, t, :]
                p.op('dve', [bk(2)], [('ms_L', t)], lambda E: E.tensor_copy(out=lgj, in_=B[2][:, 0:32]))
                p.op('dve', [('ms_L', t)], [('ms_mx', t)], lambda E: E.max(out=mxj, in_=lgj))
                p.op('dve', [('ms_mx', t)], [('ms_sm', s2, 0)],
                     lambda E: E.tensor_scalar(out=sm[s2][:, 0:1], in0=mxall[:, t, 0:1], scalar1=-1.0, scalar2=None, op0=ALU.mult))
                p.op('act', [('ms_L', t), ('ms_sm', s2, 0)], [('ms_ex', s2)],
                     lambda E: E.activation(out=ex[s2][:, :], in_=lgj, func=AF.Exp, bias=sm[s2][:, 0:1], scale=1.0))
                p.op('dve', [('ms_L', t), ('ms_mx', t)], [('ms_M', t)],
                     lambda E: E.tensor_scalar(out=Mj, in0=lgj, scalar1=mxall[:, t, 3:4], scalar2=None, op0=ALU.is_ge))
                p.op('dve', [('ms_M', t), ('ms_ex', s2)], [('ms_ex', s2)],
                     lambda E: E.tensor_tensor(out=ex[s2][:, :], in0=ex[s2][:, :], in1=Mj, op=ALU.mult))
                p.op('dve', [('ms_ex', s2)], [('ms_sm', s2, 1)],
                     lambda E: E.tensor_reduce(out=sm[s2][:, 1:2], in_=ex[s2][:, :], axis=AX.X, op=ALU.add))
                p.op('dve', [('ms_sm', s2, 1)], [('ms_sm', s2, 1)], lambda E: E.reciprocal(out=sm[s2][:, 1:2], in_=sm[s2][:, 1:2]))
                p.op('dve', [('ms_ex', s2), ('ms_sm', s2, 1)], [('ms_G', t)],
                     lambda E: E.tensor_scalar(out=Gj, in0=ex[s2][:, :], scalar1=sm[s2][:, 1:2], scalar2=None, op0=ALU.mult))
                p.op('pe', [('ms_M', t), 'cst'], [bk(7)],
                     lambda E: E.matmul(B[7][0:1, 0:32], lhsT=ones[:, 0:1], rhs=Mj, start=(jj == 0), stop=(jj == ntl - 1)))
            p.op('dve', [bk(7)], ['ms_cnt'], lambda E: E.tensor_scalar(out=cnt[:, :], in0=B[7][0:1, 0:32], scalar1=1.0 / 512, scalar2=255.5 / 512,
                                                                       op0=ALU.mult, op1=ALU.add))
            p.op('dve', ['ms_cnt'], ['ms_cnti'], lambda E: E.tensor_copy(out=cnti[:, :], in_=cnt[:, :]))
            p.op('dve', ['ms_cnti'], ['ms_cnt'], lambda E: E.tensor_copy(out=cnt[:, :], in_=cnti[:, :]))
            p.op('dve', ['ms_cnt'], ['ms_cnt'], lambda E: E.tensor_scalar(out=cnt[:, :], in0=cnt[:, :], scalar1=512.0, scalar2=None, op0=ALU.mult))
            p.op('pe', ['ms_cnt', 'cst'], [bk(0)], lambda E: E.transpose(B[0][0:32, 0:1], cnt[0:1, :], ident32[0:1, 0:1]))
            p.op('dve', [bk(0)], ['ms_pcol'], lambda E: E.tensor_copy(out=pcol[:, 0:1], in_=B[0][0:32, 0:1]))
            p.op('pe', ['ms_pcol', 'ms_Lst'], [bk(1)],
                 lambda E: E.matmul(B[1][0:1, 0:32], lhsT=pcol[:, 0:1], rhs=Lst[0:32, 0:32], start=True, stop=True))
            p.op('dve', [bk(1)], ['ms_cnt'], lambda E: E.tensor_copy(out=cnt[:, :], in_=B[1][0:1, 0:32]))
            p.op('pe', ['ms_cnt', 'cst'], [bk(1)],
                 lambda E: E.matmul(B[1][:, 0:32], lhsT=ones[0:1, :], rhs=cnt[0:1, :], start=True, stop=True))
            p.op('dve', [bk(1)], ['ms_psbc'], lambda E: E.tensor_copy(out=psbc[:, :], in_=B[1][:, 0:32]))
            p.op('pe', ['ms_pcol', 'cst'], [bk(0)],
                 lambda E: E.matmul(B[0][0:32, 0:1], lhsT=cst[0:32, 384:416], rhs=pcol[:, 0:1], start=True, stop=True))
            p.op('dve', [bk(0)], ['ms_pcol2'], lambda E: E.tensor_copy(out=pcol[:, 1:2], in_=B[0][0:32, 0:1]))
            p.op('dve', ['ms_pcol2', 'cst'], ['ms_cmp'],
                 lambda E: E.tensor_scalar(out=cmp_[:, :], in0=blk512[0:32, :], scalar1=pcol[:, 1:2], scalar2=None, op0=ALU.is_ge))
            p.op('pe', ['ms_cmp', 'cst'], [bk(0)],
                 lambda E: E.matmul(B[0][0:1, 0:128], lhsT=ones[0:32, 0:1], rhs=cmp_[:, :], start=True, stop=True))
            p.op('dve', [bk(0)], ['ms_berow'], lambda E: E.tensor_copy(out=berow[:, :], in_=B[0][0:1, 0:128]))
            p.op('pe', ['ms_berow', 'cst'], [bk(0)],
                 lambda E: E.matmul(B[0][:, 0:128], lhsT=ones[0:1, :], rhs=berow[0:1, :], start=True, stop=True))
            p.op('dve', [bk(0)], ['ms_oob'],
                 lambda E: E.tensor_scalar(out=tmpf[:, 0:NB], in0=B[0][:, 0:NB], scalar1=31.5, scalar2=0.0, op0=ALU.is_ge, op1=ALU.mult))
            p.op('dve', [bk(0)], ['ms_bebc'],
                 lambda E: E.tensor_scalar(out=be_bc[:, :], in0=B[0][:, 0:NB], scalar1=31.0, scalar2=None, op0=ALU.min))
            p.op('dve', ['ms_bebc', 'cst'], ['ms_oh'],
                 lambda E: E.tensor_scalar(out=OHall[:, :], in0=be_bc[0:32, :], scalar1=iota_p[0:32, :], scalar2=None, op0=ALU.is_equal))
            p.op('dve', ['ms_bebc', 'cst', 'ms_oob'], ['ms_oob'],
                 lambda E: E.tensor_scalar(out=tmpf[:, 0:NB], in0=tmpf[:, 0:NB], scalar1=iota_p[:, :], scalar2=None, op0=ALU.add))
            p.op('dve', ['ms_bebc', 'ms_oob'], ['ms_tmpf'],
                 lambda E: E.scalar_tensor_tensor(out=tmpf[:, 0:NB], in0=be_bc[:, :], scalar=128.0, in1=tmpf[:, 0:NB], op0=ALU.mult, op1=ALU.add))
            p.op('dve', ['ms_tmpf'], ['ms_idxw'], lambda E: E.tensor_copy(out=idxw[:, :, 0], in_=tmpf[:, 0:NB]))
            p.op('dve', [], ['ms_Msum'], lambda E: E.memset(Msum[:, :], 0.0))
            for jj, t in enumerate(tiles):
                s2 = jj % 2
                tok = slice(t * 128, (t + 1) * 128)
                lgj, Mj, Gj = Lall[:, t, :], Mall[:, t, :], Gall[:, t, :]

                def mmp(E):
                    E.matmul(B[3 + s2][:, 0:32], lhsT=Lst[:, :], rhs=Mj, start=True, stop=False)
                    return E.matmul(B[3 + s2][:, 0:32], lhsT=ones[:, :], rhs=Msum[:, :], start=False, stop=True)
                p.op('pe', [('ms_M', t), 'ms_Msum', 'ms_Lst', 'cst'], [bk(3 + s2)], mmp)
                p.op('dve', [bk(3 + s2), 'ms_psbc'], [('ms_pos', s2)],
                     lambda E: E.tensor_tensor(out=pos[s2][:, :], in0=B[3 + s2][:, 0:32], in1=psbc[:, :], op=ALU.add))
                p.op('pool', [('ms_M', t), 'ms_Msum'], ['ms_Msum'],
                     lambda E: E.tensor_tensor(out=Msum[:, :], in0=Msum[:, :], in1=Mj, op=ALU.add))
                for k in range(4):
                    p.op('dve', [('ms_L', t), ('ms_mx', t)], [('ms_ohk', s2)],
                         lambda E: E.tensor_scalar(out=oh[s2][:, :], in0=lgj, scalar1=mxall[:, t, k:k + 1], scalar2=None, op0=ALU.is_equal))
                    p.op('dve', [('ms_ohk', s2), ('ms_pos', s2)], [('ms_tq', s2)],
                         lambda E: E.tensor_tensor(out=tq[s2][:, :], in0=oh[s2][:, :], in1=pos[s2][:, :], op=ALU.mult))
                    p.op('dve', [('ms_tq', s2)], [('ms_sl', s2, k)],
                         lambda E: E.tensor_reduce(out=sl[s2][:, k:k + 1], in_=tq[s2][:, :], axis=AX.X, op=ALU.add))
                    p.op('dve', [('ms_ohk', s2), ('ms_G', t)], [('ms_tq', s2)],
                         lambda E: E.tensor_tensor(out=tq[s2][:, :], in0=oh[s2][:, :], in1=Gj, op=ALU.mult))
                    p.op('dve', [('ms_tq', s2)], [('ms_gk', t, k)],
                         lambda E: E.tensor_reduce(out=gk[:, t, k:k + 1], in_=tq[s2][:, :], axis=AX.X, op=ALU.add))
                p.op('dve', [('ms_sl', s2, k) for k in range(4)], [('ms_slot', t)],
                     lambda E: E.tensor_copy(out=slot_i[:, t, :], in_=sl[s2][:, :]))
                p.dma('sp', 'ms_hbl%d' % s2, [('HB', t)], [('ms_hb', s2)], [(hb[s2][:, :], HB[tok, :])])
                p.idma('scat', [('ms_hb', s2), ('ms_slot', t), 'XBZ'], [('XB', t)],
                       [dict(out=XB[:, :], out_offset=bass.IndirectOffsetOnAxis(ap=slot_i[:, t, k:k + 1], axis=0),
                             in_=hb[s2][:, :], in_offset=None) for k in range(4)])
            p.barrier()
        if c.cfg.get('moe_phases', 3) < 2:
            return
        with ExitStack() as es:
            c.es = es
            w1 = [sb(c, 'me_w1%d' % i, [128, 8, 2048], BF16) for i in range(2)]
            w2 = [sb(c, 'me_w2%d' % i, [128, 8, 1024], BF16) for i in range(2)]
            xb = [sb(c, 'me_xb%d' % i, [128, 4, 1024], BF16) for i in range(2)]
            xT = [sb(c, 'me_xT%d' % i, [128, 8, 512], BF16) for i in range(2)]
            aT = sb(c, 'me_aT', [128, 8, 512], BF16)
            yb = [sb(c, 'me_yb%d' % i, [128, 1024], BF16) for i in range(2)]
            b1 = sb(c, 'me_b1', [128, 32, 16], F32)
            b2 = sb(c, 'me_b2', [32, 1024], BF16)
            b2f = sb(c, 'me_b2f', [32, 1024], F32)
            ohr = [sb(c, 'me_ohr%d' % i, [128, 32], F32) for i in range(2)]
            b1t = sb(c, 'me_b1t', [128, 32, 16], F32)
            b1s = [sb(c, 'me_b1s%d' % i, [128, 16], F32) for i in range(2)]
            ohb = [sb(c, 'me_ohb%d' % i, [32, 128], BF16) for i in range(2)]
            gg = [sb(c, 'me_gg%d' % i, [128, 512], F32) for i in range(2)]
            sg = [sb(c, 'me_sg%d' % i, [128, 512], F32) for i in range(2)]
            ll = [sb(c, 'me_ll%d' % i, [128, 512], F32) for i in range(2)]
            p.dma('sp', 'me_b2', [], ['me_b2f'], [(b2f[:, :], I['moe_b2'][l])])
            p.op('dve', ['me_b2f'], ['me_b2'], lambda E: E.tensor_copy(out=b2[:, :], in_=b2f[:, :]))
            p.dma('sp', 'me_b1', [], ['me_b1'],
                  [(b1[:, e, :], I['moe_b1'][l, e, :].rearrange("(j p) -> p j", p=128)) for e in range(32)], slow=True)
            wbk = [('WB', l, e) for e in range(32)]
            ei = 0
            def gather_w(i):
                ws = i % 2
                wdeps = (wbk if i < 2 else []) + ['ms_idxw']
                p.idma('gw1%d' % ws, wdeps, [('me_w1', ws)],
                       [dict(out=w1[ws][:, :, :].rearrange("p k n -> p (k n)"), out_offset=None, in_=W1f[:, :],
                             in_offset=bass.IndirectOffsetOnAxis(ap=idxw[:, i, 0:1], axis=0))])
                p.idma('gw2%d' % ws, wdeps, [('me_w2', ws)],
                       [dict(out=w2[ws][:, :, :].rearrange("p k n -> p (k n)"), out_offset=None, in_=W2f[:, :],
                             in_offset=bass.IndirectOffsetOnAxis(ap=idxw[:, i, 0:1], axis=0))])

            gather_w(0)
            for i in range(NB):
                ws = i % 2
                if i + 1 < NB:
                    gather_w(i + 1)
                xbk = [('XB', t) for t in tiles] if i == 0 else []
                p.dma('sp', 'me_xb%d' % ws, xbk, [('me_xb', ws)],
                      [(xb[ws][:, :, :], XB[i * 512:(i + 1) * 512, :].rearrange("(a p) f -> p a f", p=128))])
                for a in range(4):
                    tb = 4 + (i * 4 + a) % 2
                    tpv = bank16(c, tb)

                    def trx(E):
                        ins = None
                        for k in range(8):
                            ins = E.transpose(tpv[:, k, :], xb[ws][:, a, k * 128:(k + 1) * 128], c.ident[:, :])
                        return ins
                    p.op('pe', [('me_xb', ws), 'ident'], [bk(tb)], trx)
                    if a % 2 == 0:
                        p.op('act', [bk(tb)], [('me_xT', ws, a)],
                             lambda E: E.activation(out=xT[ws][:, :, a * 128:(a + 1) * 128], in_=tpv[:, :, :], func=AF.Copy))
                    else:
                        p.op('dve', [bk(tb)], [('me_xT', ws, a)],
                             lambda E: E.tensor_copy(out=xT[ws][:, :, a * 128:(a + 1) * 128], in_=tpv[:, :, :]))
                xTk = [('me_xT', ws, a) for a in range(4)]
                p.op('dve', ['ms_bebc', 'cst'], [('me_ohr', ws)],
                     lambda E: E.tensor_scalar(out=ohr[ws][:, :], in0=iota_f, scalar1=be_bc[:, i:i + 1], scalar2=None, op0=ALU.is_equal))
                p.op('dve', [('me_ohr', ws), 'me_b1'], ['me_b1t'],
                     lambda E: E.tensor_tensor(out=b1t[:, :, :], in0=b1[:, :, :], in1=ohr[ws][:, :].unsqueeze(2).to_broadcast([128, 32, 16]),
                                               op=ALU.mult))
                p.op('dve', ['me_b1t'], [('me_b1s', ws)],
                     lambda E: E.tensor_reduce(out=b1s[ws][:, :], in_=b1t[:, :, :].rearrange("p e j -> p j e"), axis=AX.X, op=ALU.add))
                p.op('dve', ['ms_oh', 'cst'], [('me_ohb', ws)],
                     lambda E: E.tensor_scalar(out=ohb[ws][:, :], in0=ones[0:32, :], scalar1=OHall[:, i:i + 1], scalar2=None, op0=ALU.mult))
                for fc in range(8):
                    s2 = ei % 2
                    ei += 1
                    bg, bl = s2 * 2, s2 * 2 + 1

                    def mm1(E):
                        ins = None
                        for k in range(8):
                            E.matmul(B[bg][:, :], lhsT=w1[ws][:, k, fc * 128:(fc + 1) * 128], rhs=xT[ws][:, k, :], start=(k == 0), stop=(k == 7))
                        for k in range(8):
                            ins = E.matmul(B[bl][:, :], lhsT=w1[ws][:, k, 1024 + fc * 128:1024 + (fc + 1) * 128], rhs=xT[ws][:, k, :],
                                           start=(k == 0), stop=(k == 7))
                        return ins
                    p.op('pe', xTk + [('me_w1', ws)], [bk(bg), bk(bl)], mm1)
                    p.op('dve', [bk(bg), ('me_b1s', ws)], [('me_gg', s2)],
                         lambda E: E.tensor_scalar(out=gg[s2][:, :], in0=B[bg][:, :], scalar1=b1s[ws][:, fc:fc + 1], scalar2=7.0,
                                                   op0=ALU.add, op1=ALU.min))
                    p.op('act', [('me_gg', s2)], [('me_sg', s2)],
                         lambda E: E.activation(out=sg[s2][:, :], in_=gg[s2][:, :], func=AF.Silu, scale=1.702))
                    p.op('dve', [bk(bl), ('me_b1s', ws)], [('me_ll', s2)],
                         lambda E: E.tensor_scalar(out=ll[s2][:, :], in0=B[bl][:, :], scalar1=b1s[ws][:, 8 + fc:9 + fc], scalar2=7.0,
                                                   op0=ALU.add, op1=ALU.min))
                    p.op('dve', [('me_ll', s2)], [('me_ll', s2)],
                         lambda E: E.tensor_scalar(out=ll[s2][:, :], in0=ll[s2][:, :], scalar1=-7.0, scalar2=1.0, op0=ALU.max, op1=ALU.add))
                    p.op('dve', [('me_sg', s2), ('me_ll', s2)], [('me_aT', fc)],
                         lambda E: E.scalar_tensor_tensor(out=aT[:, fc, :], in0=sg[s2][:, :], scalar=1.0 / 1.702, in1=ll[s2][:, :],
                                                          op0=ALU.mult, op1=ALU.mult))
                aTk = [('me_aT', fc) for fc in range(8)]
                for a in range(4):
                    y2 = (i * 4 + a) % 2
                    for half in range(2):
                        bb = 6 + half

                        def mm2(E):
                            for k in range(8):
                                E.matmul(B[bb][:, :], lhsT=aT[:, k, a * 128:(a + 1) * 128], rhs=w2[ws][:, k, half * 512:(half + 1) * 512],
                                         start=(k == 0), stop=False)
                            return E.matmul(B[bb][:, :], lhsT=ohb[ws][:, :], rhs=b2[:, half * 512:(half + 1) * 512], start=False, stop=True)
                        p.op('pe', aTk + [('me_w2', ws), ('me_ohb', ws), 'me_b2'], [bk(bb)], mm2)
                        if half == 0:
                            p.op('act', [bk(bb)], [('me_yb', y2, half)],
                                 lambda E: E.activation(out=yb[y2][:, 0:512], in_=B[bb][:, :], func=AF.Copy))
                        else:
                            p.op('dve', [bk(bb)], [('me_yb', y2, half)], lambda E: E.tensor_copy(out=yb[y2][:, 512:1024], in_=B[bb][:, :]))
                    r0 = i * 512 + a * 128
                    p.dma('sp', 'me_yb%d' % y2, [('me_yb', y2, 0), ('me_yb', y2, 1)], [('YB', i, a)], [(YB[r0:r0 + 128, :], yb[y2][:, :])])
            p.barrier()
        if c.cfg.get('moe_phases', 3) < 3:
            return
        with ExitStack() as es:
            c.es = es
            yg = [[sb(c, 'mc_yg%d_%d' % (i, k), [128, 1024], BF16) for k in range(4)] for i in range(2)]
            xt = [sb(c, 'mc_xt%d' % i, [128, 1024], F32) for i in range(2)]
            ff = [sb(c, 'mc_ff%d' % i, [128, 1024], F32) for i in range(2)]
            for jj, t in enumerate(tiles):
                s2 = jj % 2
                r = 1 if t < 2 else 0
                tok = slice(t * 128, (t + 1) * 128)
                p.dma('sp', 'mc_xt%d' % s2, ['XOUT'], [('mc_xt', s2)], [(xt[s2][:, :], c.X1[tok, :])])
                ybk = [('YB', i, a) for i in range(NB) for a in range(4)] if jj < 2 else []
                p.idma('gy%d' % s2, [('ms_slot', t)] + ybk, [('mc_yg', s2)],
                       [dict(out=yg[s2][k][:, :], out_offset=None, in_=YB[:, :],
                             in_offset=bass.IndirectOffsetOnAxis(ap=slot_i[:, t, k:k + 1], axis=0)) for k in range(4)])
                p.op('dve', [('mc_yg', s2), ('ms_gk', t, 0)], [('mc_ff', s2)],
                     lambda E: E.tensor_scalar(out=ff[s2][:, :], in0=yg[s2][0][:, :], scalar1=gk[:, t, 0:1], scalar2=None, op0=ALU.mult))
                for k in range(1, 4):
                    p.op('dve', [('mc_yg', s2), ('ms_gk', t, k), ('mc_ff', s2)], [('mc_ff', s2)],
                         lambda E: E.scalar_tensor_tensor(out=ff[s2][:, :], in0=yg[s2][k][:, :], scalar=gk[:, t, k:k + 1], in1=ff[s2][:, :],
                                                          op0=ALU.mult, op1=ALU.add))
                p.op('pool', [('mc_ff', s2), 'e_bc'], [('mc_ff', s2)],
                     lambda E: E.tensor_tensor(out=ff[s2][:, :], in0=ff[s2][:, :], in1=c.e_ag[:, r, :], op=ALU.mult))
                p.op('dve', [('mc_ff', s2), ('mc_xt', s2)], [('mc_xt', s2)],
                     lambda E: E.scalar_tensor_tensor(out=xt[s2][:, :], in0=xt[s2][:, :], scalar=ALPHA, in1=ff[s2][:, :],
                                                      op0=ALU.mult, op1=ALU.add))
                ln_affine_store(c, xt[s2], ('mc_xt', s2), c.e_gb, 'e_bc', dst_fn(t), s2)
            p.barrier()


def make_rope():
    rows = NLAT // 64
    row = np.repeat(np.arange(rows, dtype=np.float32), 64)
    col = np.tile(np.arange(64, dtype=np.float32), rows)
    inv = (np.float32(10000.0) ** (-np.arange(8, dtype=np.float32) / np.float32(8))).astype(np.float32)
    ang = np.concatenate([row[:, None] * inv, col[:, None] * inv], -1).astype(np.float32)
    return np.concatenate([np.cos(ang), np.sin(ang)], -1).astype(np.float32)


def make_hyena_consts():
    import ml_dtypes
    bf = ml_dtypes.bfloat16
    N = 16384
    out = {}
    a = np.arange(128, dtype=np.float64)
    f = np.arange(128, dtype=np.float64)
    ang = 2 * np.pi * np.outer(a, f) / 128
    d1 = np.zeros((128, 2, 2, 128), np.float64)
    d1[:, 0, 0, :] = np.cos(ang)
    d1[:, 0, 1, :] = -np.sin(ang)
    for aa in range(4):
        for ff_ in range(4):
            d1[aa, 1, 0, ff_] = np.cos(2 * np.pi * aa * ff_ / 4)
            d1[aa, 1, 1, ff_] = -np.sin(2 * np.pi * aa * ff_ / 4)
    out['hy_dft1'] = d1.astype(np.float32).astype(bf)
    e3 = np.zeros((128, 3, 128), np.float64)
    e3[:, 0, :] = np.cos(ang)
    e3[:, 1, :] = np.sin(ang)
    e3[:, 2, :] = -np.sin(ang)
    out['hy_e3'] = e3.astype(np.float32).astype(bf)
    f1 = np.arange(128)[:, None, None]
    b = np.arange(128)[None, :, None]
    f2 = np.arange(128)[None, None, :]
    th = 2 * np.pi * ((b * (f1 + 128 * f2)) % N) / N
    tw2 = np.stack([np.cos(th), -np.sin(th), np.sin(th)], axis=2)
    out['hy_tw2'] = tw2.astype(np.float32).astype(bf)
    bb = np.arange(128)[:, None, None]
    ff = np.arange(128)[None, :, None]
    aa = np.arange(64)[None, None, :]
    ps_ = 2 * np.pi * (((128 * aa + bb) * ff) % N) / N
    twf = np.stack([np.cos(ps_), -np.sin(ps_)], axis=2)
    out['hy_twf'] = twf.astype(np.float32).astype(bf)
    f1c = np.arange(4)[:, None, None]
    thc = 2 * np.pi * ((b * (f1c + 4 * f2)) % 512) / 512
    out['hy_tw2c'] = np.stack([np.cos(thc), -np.sin(thc), np.sin(thc)], axis=2).astype(np.float32).astype(bf)
    ffc = np.arange(4)[None, :, None]
    aac = np.arange(2)[None, None, :]
    psc = 2 * np.pi * (((128 * aac + bb) * ffc) % 512) / 512
    out['hy_twfc'] = np.stack([np.cos(psc), -np.sin(psc)], axis=2).astype(np.float32).astype(bf)
    deltas = np.abs(np.linspace(math.log(1e-2) / 1.5, math.log(1e-2) / 0.3, 512, dtype=np.float32))
    out['hy_negdelta'] = (-deltas).astype(np.float32)[None, :]

    def feats(L, s):
        s = np.asarray(s)
        t = np.linspace(0.0, 1.0, L, dtype=np.float32)[s][:, None]
        w = (np.float32(2 * math.pi) * np.arange(L, dtype=np.float32) / np.float32(L))[s][:, None]
        fq = np.linspace(1e-4, 15, 16, dtype=np.float32)
        z = np.concatenate([t, np.cos(fq * w), -np.sin(fq * w)], -1).astype(np.float32)
        return z, t[:, 0]
    BIG = 1e4
    L = 8192
    tau = np.arange(N)
    s = np.where(tau < L, tau, N - tau)
    s[L] = 0
    z, t = feats(L, s)
    out['hy_feat0'] = np.ascontiguousarray(z.T)
    out['hy_tvec0'] = t[None, :].astype(np.float32)
    L = 256
    tau = np.arange(512)
    s = np.where(tau < L, tau, 512 - tau)
    s[256] = 0
    z, t = feats(L, s)
    out['hy_feat1'] = np.ascontiguousarray(z.T)
    out['hy_tvec1'] = t[None, :].astype(np.float32)
    return out


def make_consts():
    cst = np.zeros((128, 1024), np.float32)
    cst[:, 0:128] = np.eye(128)
    U = (np.arange(128)[:, None] <= np.arange(128)[None, :]).astype(np.float32)
    cst[:, 128:256] = U / 16.0
    cst[:, 256:384] = U.T / 16.0
    cst[:, 384:512] = U
    cst[:, 512:640] = U.T
    cst[:, 640:768] = 1.0
    cst[:, 768:800] = np.arange(32, dtype=np.float32)[None, :]
    cst[:, 800] = np.arange(128, dtype=np.float32)
    cst[:, 896:1024] = 512.0 * np.arange(128, dtype=np.float32)[None, :]
    return cst


def build_program(cfg):
    nc = bass.Bass("TRN2", target_bir_lowering=False)
    es = ExitStack()
    c = Ctx()
    c.nc, c.es, c.cfg = nc, es, cfg
    c.dbg = set(cfg.get('dbg', []))
    c.p = Prog(nc, es)
    p = c.p
    layers = cfg.get('layers', [0, 1])
    stages = cfg.get('stages', ['adaln', 'proj'])

    def ext(name, shape, dt=F32):
        return nc.dram_tensor(name, list(shape), dt, kind="ExternalInput").ap()

    I = {}
    I['x'] = ext('x', [NLAT, D])
    I['ctx'] = ext('ctx', [NCTX, D])
    I['cc'] = ext('cc', [2, D])
    I['ada_w'] = ext('ada_w', [DEPTH, D, 6 * D])
    I['ada_b'] = ext('ada_b', [DEPTH, 6 * D])
    I['w_in'] = ext('w_in', [DEPTH, D, IN_TOTAL])
    I['cst'] = ext('cst', [128, 1024])
    for nm, shp in [('gla_wa2_f', [DEPTH, 16, 256]), ('gla_ba_f', [DEPTH, 256]), ('gla_wa2_b', [DEPTH, 16, 256]),
                    ('gla_ba_b', [DEPTH, 256]), ('gla_norm', [DEPTH, 128]),
                    ('mla_q_norm', [DEPTH, 384]), ('mla_w_uq', [DEPTH, 384, 768]), ('mla_kv_norm', [DEPTH, 256]),
                    ('mla_w_ukv', [DEPTH, 256, 1024]), ('rope', [NLAT, 32]),
                    ('hy_conv_w', [DEPTH, 3, 1536]), ('hy_conv_b', [DEPTH, 1536]), ('hy_w1', [DEPTH, 33, 64]),
                    ('hy_b1', [DEPTH, 64]), ('hy_w2', [DEPTH, 64, 64]), ('hy_b2', [DEPTH, 64]), ('hy_w3', [DEPTH, 64, 2048]),
                    ('hy_freq', [DEPTH, 64]), ('hy_bias', [DEPTH, 2, 512]),
                    ('hy_feat0', [33, 16384]), ('hy_tvec0', [1, 16384]), ('hy_feat1', [33, 512]), ('hy_tvec1', [1, 512]),
                    ('hy_negdelta', [1, 512]),
                    ('w_br_gla', [DEPTH, 512, D]), ('w_br_mla', [DEPTH, 512, D]), ('w_br_hy', [DEPTH, 512, D]),
                    ('w_out', [DEPTH, D, D]), ('ln1_g', [DEPTH, D]), ('ln1_b', [DEPTH, D]), ('ln2_g', [DEPTH, D]),
                    ('ln2_b', [DEPTH, D]), ('router_w', [DEPTH, D, 32]), ('router_b', [DEPTH, 32]),
                    ('moe_b1', [DEPTH, 32, 2048]), ('moe_b2', [DEPTH, 32, D])]:
        I[nm] = ext(nm, shp)
    for nm, shp in [('hy_dft1', [128, 2, 2, 128]), ('hy_e3', [128, 3, 128]), ('hy_tw2', [128, 128, 3, 128]),
                    ('hy_twf', [128, 128, 2, 64]), ('hy_tw2c', [4, 128, 3, 128]), ('hy_twfc', [128, 4, 2, 2])]:
        I[nm] = ext(nm, shp, BF16)
    if 'moe' in stages:
        I['moe_w1'] = ext('moe_w1', [DEPTH, 32, D, 2048])
        I['moe_w2'] = ext('moe_w2', [DEPTH, 32, D, D])
        c.WB1 = [nc.dram_tensor('WB1_%d' % l, [32, D, 2048], BF16).ap() for l in range(DEPTH)]
        c.WB2 = [nc.dram_tensor('WB2_%d' % l, [32, D, D], BF16).ap() for l in range(DEPTH)]
        c.XB = nc.dram_tensor('XB', [98 * 512, D], BF16).ap()
        c.YB = nc.dram_tensor('YB', [98 * 512, D], BF16).ap()
        c.HB = nc.dram_tensor('HB', [NT, D], BF16).ap()
    c.inp = I
    c.out = nc.dram_tensor('out', [NLAT, D], F32, kind="ExternalOutput").ap()

    c.modv = [dram(c, 'modv%d' % l, [2, 6 * D], F32) for l in range(DEPTH)]
    c.scr = []
    for l in range(DEPTH):
        S = {}
        for name, off, ncols in FM_GROUPS:
            S[name] = dram(c, '%s%d' % (name, l), [ncols, NT], F32 if name in ('AFT', 'ABT') else BF16)
        for name, off, ncols in TM_GROUPS:
            S[name] = dram(c, '%s%d' % (name, l), [NT, ncols], F32 if name == 'MKR' else BF16)
        for name in ('OGT', 'OMT', 'OHT'):
            S[name] = dram(c, '%s%d' % (name, l), [512, NT], BF16)
        c.scr.append(S)
    c.X1 = dram(c, 'X1', [NT, D], F32)
    c.X2 = dram(c, 'X2', [NT, D], F32)
    c.KpT = dram(c, 'KpT', [8, 97, NT], BF16)
    c.QpT = dram(c, 'QpT', [8, 97, NT], BF16)
    c.VpD = dram(c, 'VpD', [8, NT, 65], BF16)
    c.HYC = dram(c, 'HYC', [NT, 1536], BF16)
    c.Z2 = dram(c, 'Z2', [NT, 512], BF16)
    c.OH = dram(c, 'OH', [NT, 512], BF16)
    c.KTD = [dram(c, 'KTD0', [16384, 1024], BF16), dram(c, 'KTD1', [512, 1024], BF16)]
    c.KS = [dram(c, 'KS%d' % o, [128, 128, 2, 512], BF16) for o in range(2)]
    c.X1D = dram(c, 'X1D', [128, 128, 2, 512], BF16)
    c.QD = dram(c, 'QD', [128, 128, 2, 512], BF16)
    c.SCL = [dram(c, 'SCL%d' % j, [1, 1024], F32) for j in range(2)]

    c.cst = sb(c, 'cst_sb', [128, 1024], F32)
    c.ident = sb(c, 'ident16', [128, 128], BF16)
    c.cT = sb(c, 'cTsb', [128, 8, 2], F32)
    c.modT = sb(c, 'modT', [128, 2, 6, 8], F32)
    c.bank = [ps(c, 'bank%d' % i, [128, 512], F32) for i in range(8)]

    p.dma('sp', 'cst', [], ['cst'], [(c.cst[:, :], I['cst'][:, :])])
    p.op('dve', ['cst'], ['ident'], lambda E: E.tensor_copy(out=c.ident[:, :], in_=c.cst[:, 0:128]))
    p.dma('sp', 'cT', [], ['cT'],
          [(c.cT[:, :, r], I['cc'][r, :].rearrange("(k p) -> p k", p=128)) for r in range(2)], slow=True)
    p.op('act', ['cT'], ['cT'], lambda E: E.activation(out=c.cT[:, :, :], in_=c.cT[:, :, :], func=AF.Silu))

    for l in layers:
        if 'adaln' in stages:
            stage_adaln(c, l)
    if 'moe' in stages:
        for l in layers:
            moe_precast(c, l)
        if cfg.get('moe_mode', 'sparse') == 'sparse':
            c.zt = sb(c, 'zero_t', [128, 2048], BF16)
            p.op('pool', [], ['zero_t'], lambda E: E.memset(c.zt[:, :], 0.0))
            p.dma('sp', 'xbzero', ['zero_t'], ['XBZ'],
                  [(c.XB[i * 256:(i + 1) * 256, :].rearrange("(p a) f -> p (a f)", p=128), c.zt[:, :]) for i in range(196)])
    for l in layers:
        last = (l == DEPTH - 1)

        def xsrc(t, l=l):
            if l == 0:
                if t < 2:
                    return I['ctx'][t * 128:(t + 1) * 128, :]
                return I['x'][(t - 2) * 128:(t - 1) * 128, :]
            return c.X2[t * 128:(t + 1) * 128, :]
        if 'proj' in stages:
            stage_proj(c, l, xsrc)
        if 'gla' in stages:
            stage_gla(c, l)
        if 'mla' in stages:
            stage_mla(c, l, ctx_q=not last)
        if 'hyena' in stages:
            stage_hyena(c, l, with_ctx=not last)
        tiles = list(range(2, NTILE)) if last else list(range(NTILE))
        if 'merge' in stages:
            load_mod(c, l)
            stage_merge(c, l, xsrc, tiles)
        if 'moe' in stages:
            load_mod(c, l)

            def dst_fn(t, last=last):
                if last:
                    return [c.out[(t - 2) * 128:(t - 1) * 128, :]]
                return [c.X2[t * 128:(t + 1) * 128, :]]
            if cfg.get('moe_mode', 'sparse') == 'sparse':
                stage_moe2(c, l, tiles, dst_fn)
            else:
                stage_moe(c, l, tiles, dst_fn)
    p.finish('sp')
    return nc, c


ALL_STAGES = ['adaln', 'proj', 'gla', 'mla', 'hyena', 'merge', 'moe']
WEIGHT_KEYS = ['ada_w', 'ada_b', 'w_in', 'gla_wa2_f', 'gla_ba_f', 'gla_wa2_b', 'gla_ba_b', 'gla_norm', 'mla_q_norm', 'mla_w_uq',
               'mla_kv_norm', 'mla_w_ukv', 'hy_conv_w', 'hy_conv_b', 'hy_w1', 'hy_b1', 'hy_w2', 'hy_b2', 'hy_w3', 'hy_freq',
               'hy_bias', 'w_br_gla', 'w_br_mla', 'w_br_hy', 'w_out', 'ln1_g', 'ln1_b', 'ln2_g', 'ln2_b', 'router_w', 'router_b',
               'moe_w1', 'moe_b1', 'moe_w2', 'moe_b2']


def make_in_map(inputs, b, with_moe=True):
    f32 = lambda a: np.ascontiguousarray(np.asarray(a, dtype=np.float32))
    im = dict(x=f32(inputs['x'][b]), ctx=f32(inputs['ctx'][b]),
              cc=f32(np.stack([np.asarray(inputs['c'])[b], np.asarray(inputs['c_ctx'])])),
              cst=make_consts(), rope=make_rope())
    im.update(make_hyena_consts())
    for k in WEIGHT_KEYS:
        if not with_moe and k in ('moe_w1', 'moe_w2'):
            continue
        im[k] = f32(inputs[k])
    return im


def kernel(**inputs):
    nc, c = build_program(dict(layers=[0, 1], stages=ALL_STAGES))
    nb = np.asarray(inputs['x']).shape[0]
    in_maps = [make_in_map(inputs, b) for b in range(nb)]
    res = run_bass_kernel_spmd(nc, in_maps, core_ids=list(range(nb)))
    out = np.stack([np.asarray(res.results[b]['out']) for b in range(nb)], 0)
    return out.astype(np.float32)
```

```python
import math
from contextlib import ExitStack

import numpy as np
import concourse.bass as bass
import concourse.mybir as mybir
from concourse.bass_utils import run_bass_kernel_spmd

F32 = mybir.dt.float32
BF16 = mybir.dt.bfloat16
AF = mybir.ActivationFunctionType
ALU = mybir.AluOpType
AX = mybir.AxisListType

D = 1024
NCTX = 256
NLAT = 8192
NT = NCTX + NLAT
NTILE = NT // 128
DEPTH = 2
IN_TOTAL = 6848
LN_EPS = 1e-5
RMS_EPS = 1e-6
ALPHA = (2 * DEPTH) ** 0.25
O_GK, O_GV, O_GAF, O_GAB, O_MKVA, O_MKR, O_GQ, O_GR, O_MQA, O_HY, O_GATES = (
    0, 256, 768, 784, 800, 1056, 1088, 1344, 1856, 2240, 3776)


class Prog:
    def __init__(self, nc, es):
        self.nc = nc
        self.es = es
        self.E = dict(pe=nc.tensor, act=nc.scalar, dve=nc.vector, pool=nc.gpsimd, sp=nc.sync)
        self.sems = {}
        self.cnt = {}
        self.seen = {e: {} for e in self.E}
        self.st = {}
        self.n_ops = 0
        self.alias = {}
        self.free_slots = []
        self.n_slots = 0
        self.persistent = set(['wcast', 'xbzero'])

    def sem(self, key):
        if key not in self.sems:
            self.sems[key] = self.es.enter_context(self.nc.semaphore("s%d" % len(self.sems)))
            self.cnt[key] = 0
        return self.sems[key]

    def _deps(self, reads, writes):
        deps = {}

        def add(k, v):
            if deps.get(k, 0) < v:
                deps[k] = v

        for r in reads:
            s = self.st.get(r)
            if s and s[0]:
                add(*s[0])
        for w in writes:
            s = self.st.get(w)
            if s:
                if s[0]:
                    add(*s[0])
                for k, v in s[1].items():
                    add(k, v)
        return deps

    def _wait(self, eng, deps):
        E = self.E[eng]
        seen = self.seen[eng]
        for k, v in deps.items():
            if k == 'pe' and eng == 'pe':
                continue
            if seen.get(k, 0) < v:
                E.wait_ge(self.sems[k], v)
                seen[k] = v

    def _commit(self, ev, reads, writes):
        k, v = ev
        for r in reads:
            s = self.st.setdefault(r, [None, {}])
            if s[1].get(k, 0) < v:
                s[1][k] = v
        for w in writes:
            self.st[w] = [ev, {}]

    def op(self, eng, reads, writes, fn):
        reads = list(reads)
        writes = list(writes)
        for r in reads:
            if isinstance(r, tuple) and r[0] == 'bank' and r not in writes:
                writes.append(r)
        self._wait(eng, self._deps(reads, writes))
        sem = self.sem(eng)
        ins = fn(self.E[eng])
        self.cnt[eng] += 1
        ins.then_inc(sem, 1)
        self._commit((eng, self.cnt[eng]), reads, writes)
        self.n_ops += 1

    def _slot(self, semkey):
        if semkey in self.persistent:
            return semkey
        if semkey not in self.alias:
            if self.free_slots:
                self.alias[semkey] = self.free_slots.pop()
            else:
                self.alias[semkey] = ('dsem', self.n_slots)
                self.n_slots += 1
        return self.alias[semkey]

    def idma(self, semkey, reads, writes, calls):
        reads = list(reads)
        writes = list(writes)
        self._wait('pool', self._deps(reads, writes))
        key = self._slot(semkey)
        sem = self.sem(key)
        for kw in calls:
            self.nc.gpsimd.indirect_dma_start(**kw).then_inc(sem, 16)
            self.cnt[key] += 16
        self._commit((key, self.cnt[key]), reads, writes)
        self.n_ops += len(calls)

    def dma(self, eng, semkey, reads, writes, pairs, slow=False):
        reads = list(reads)
        writes = list(writes)
        self._wait(eng, self._deps(reads, writes))
        semkey = self._slot(semkey)
        sem = self.sem(semkey)
        for o, i in pairs:
            if slow:
                self.E[eng].dma_start(out=o, in_=i, allow_slow_non_contiguous=True).then_inc(sem, 16)
            else:
                self.E[eng].dma_start(out=o, in_=i).then_inc(sem, 16)
            self.cnt[semkey] += 16
        self._commit((semkey, self.cnt[semkey]), reads, writes)
        self.n_ops += len(pairs)

    def pe_fence(self, ins):
        sem = self.sem('pe')
        self.cnt['pe'] += 1
        ins.then_inc(sem, 1)
        self.E['pe'].wait_ge(sem, self.cnt['pe'])
        self.seen['pe']['pe'] = self.cnt['pe']

    def barrier(self):
        deps = {k: v for k, v in self.cnt.items() if v > 0 and k not in self.persistent}
        for eng in self.E:
            self._wait(eng, dict(deps))
        self.free_slots = [('dsem', i) for i in range(self.n_slots)]
        self.alias = {}

    def finish(self, eng='sp'):
        deps = {}
        for k, c in self.cnt.items():
            if c > 0:
                deps[k] = c
        self._wait(eng, deps)


class Ctx:
    pass


_uid = [0]


def sb(c, name, shape, dt):
    _uid[0] += 1
    return c.es.enter_context(c.nc.sbuf_tensor("%s_u%d" % (name, _uid[0]), list(shape), dt))


def ps(c, name, shape, dt):
    return c.es.enter_context(c.nc.psum_tensor(name, list(shape), dt))


def bank16(c, i):
    return c.bank[i][:, :].bitcast(BF16).rearrange("p (a b) -> p a b", a=8)


def dram(c, name, shape, dt, out=False):
    kind = "ExternalOutput" if (out or name in c.dbg) else "Internal"
    return c.nc.dram_tensor(name, list(shape), dt, kind=kind).ap()


def stage_adaln(c, l):
    with ExitStack() as es:
        c.es = es
        c.adaw = [sb(c, 'adaw%d' % i, [128, 8, 512], F32) for i in range(2)]
        c.adab = [sb(c, 'adab%d' % i, [2, 512], F32) for i in range(2)]
        c.modrow = [sb(c, 'modrow%d' % i, [2, 512], F32) for i in range(2)]
        c.psA = [c.bank[0], c.bank[1]]
        _stage_adaln(c, l)
        c.p.barrier()


def _stage_adaln(c, l):
    p, nc = c.p, c.nc
    I = c.inp
    cT = c.cT
    modv = c.modv[l]
    for cb in range(12):
        slot = cb % 2
        wt = c.adaw[slot]
        p.dma('sp', 'adaw%d' % slot, [], [('adaw', slot)],
              [(wt[:, :, :], I['ada_w'][l, :, cb * 512:(cb + 1) * 512].rearrange("(k p) n -> p k n", p=128))])
        bt = c.adab[slot]
        p.dma('sp', 'adab%d' % slot, [], [('adab', slot)],
              [(bt[0:1, :], I['ada_b'][l:l + 1, cb * 512:(cb + 1) * 512]),
               (bt[1:2, :], I['ada_b'][l:l + 1, cb * 512:(cb + 1) * 512])])
        pt = c.psA[cb % 2]

        def mm(E, wt=wt, pt=pt):
            ins = None
            for k in range(8):
                ins = E.matmul(pt[0:2, :], lhsT=cT[:, k, :], rhs=wt[:, k, :], start=(k == 0), stop=(k == 7))
            return ins
        p.op('pe', [('adaw', slot), 'cT'], [('bank', cb % 2)], mm)
        mt = c.modrow[slot]
        p.op('dve', [('bank', cb % 2), ('adab', slot)], [('modrow', slot)],
             lambda E, mt=mt, pt=pt, bt=bt: E.tensor_tensor(out=mt[0:2, :], in0=pt[0:2, :], in1=bt[0:2, :], op=ALU.add))
        p.dma('sp', 'modrow%d' % slot, [('modrow', slot)], [('modv', l)],
              [(modv[0:2, cb * 512:(cb + 1) * 512], mt[0:2, :])])


def load_mod(c, l):
    p = c.p
    modv = c.modv[l]
    pairs = []
    for r in range(2):
        for g in range(6):
            pairs.append((c.modT[:, r, g, :], modv[r, g * 1024:(g + 1) * 1024].rearrange("(k p) -> p k", p=128)))
    p.dma('sp', 'modT', [('modv', l)], ['modT'], pairs, slow=True)
    p.op('dve', ['modT'], ['modT'],
         lambda E: E.tensor_scalar(out=c.modT[:, :, 1, :], in0=c.modT[:, :, 1, :], scalar1=1.0, scalar2=None, op0=ALU.add))
    p.op('dve', ['modT'], ['modT'],
         lambda E: E.tensor_scalar(out=c.modT[:, :, 4, :], in0=c.modT[:, :, 4, :], scalar1=1.0, scalar2=None, op0=ALU.add))


def ln_mod_tile(c, src_ap, r, gsh, gsc, hT, col0, hkey):
    p = c.p
    i = c.ln_i
    c.ln_i += 1
    s = i % 3
    xt = c.xt[s]
    st = c.lnst[s]
    p.dma('pool', 'xt%d' % s, ['XOUT'], [('xt', s)], [(xt[:, :], src_ap)])
    junk = c.junk[i % 2]
    p.op('act', [('xt', s)], [('junk', i % 2), ('lnst', s, 0)],
         lambda E: E.activation(out=junk[:, :], in_=xt[:, :], func=AF.Copy, accum_out=st[:, 0:1]))
    p.op('dve', [('lnst', s, 0)], [('lnst', s, 1)],
         lambda E: E.tensor_scalar(out=st[:, 1:2], in0=st[:, 0:1], scalar1=-1.0 / D, scalar2=None, op0=ALU.mult))
    p.op('act', [('xt', s), ('lnst', s, 1)], [('junk', i % 2), ('lnst', s, 2)],
         lambda E: E.activation(out=junk[:, :], in_=xt[:, :], func=AF.Square, bias=st[:, 1:2], scale=1.0,
                                accum_out=st[:, 2:3]))
    p.op('act', [('lnst', s, 2)], [('lnst', s, 3)],
         lambda E: E.activation(out=st[:, 3:4], in_=st[:, 2:3], func=AF.Ln, scale=1.0 / D, bias=LN_EPS))
    p.op('act', [('lnst', s, 3)], [('lnst', s, 4)],
         lambda E: E.activation(out=st[:, 4:5], in_=st[:, 3:4], func=AF.Exp, scale=-0.5))
    if c.cfg.get('lnsteps', 9) < 2:
        return
    xh = c.xh[i % 2]
    p.op('dve', [('xt', s), ('lnst', s, 1), ('lnst', s, 4)], [('xh', i % 2)],
         lambda E: E.tensor_scalar(out=xh[:, :], in0=xt[:, :], scalar1=st[:, 1:2], scalar2=st[:, 4:5],
                                   op0=ALU.add, op1=ALU.mult))
    if c.cfg.get('lnsteps', 9) < 3:
        return
    tp = c.tp[i % 2]

    def tr(E):
        ins = None
        for k in range(8):
            ins = E.transpose(tp[:, k, :], xh[:, k * 128:(k + 1) * 128], c.ident[:, :])
        return ins
    p.op('pe', [('xh', i % 2), 'ident'], [c.tpk[i % 2]], tr)
    if c.cfg.get('lnsteps', 9) < 4:
        return
    for k in range(8):
        eng = c.cfg.get('evac_eng') or ('act' if i % 2 == 0 else 'dve')
        if eng == 'act':
            p.op('act', [c.tpk[i % 2], 'modT'], [hkey + (k,)],
                 lambda E, k=k: E.activation(out=hT[:, k, col0:col0 + 128], in_=tp[:, k, :], func=AF.Identity,
                                             bias=c.modT[:, r, gsh, k:k + 1], scale=c.modT[:, r, gsc, k:k + 1]))
        else:
            p.op('dve', [c.tpk[i % 2], 'modT'], [hkey + (k,)],
                 lambda E, k=k: E.tensor_scalar(out=hT[:, k, col0:col0 + 128], in0=tp[:, k, :],
                                                scalar1=c.modT[:, r, gsc, k:k + 1],
                                                scalar2=c.modT[:, r, gsh, k:k + 1],
                                                op0=ALU.mult, op1=ALU.add))


FM_GROUPS = [
    ('KT', O_GK, 256), ('QT', O_GQ, 256), ('AFT', O_GAF, 16), ('ABT', O_GAB, 16),
    ('MKVAT', O_MKVA, 256), ('MQAT', O_MQA, 384),
]
TM_GROUPS = [
    ('V', O_GV, 512), ('MKR', O_MKR, 32), ('GR', O_GR, 512), ('HY', O_HY, 1536), ('G3', O_GATES, 3072),
]


def stage_proj(c, l, xsrc):
    with ExitStack() as es:
        c.es = es
        c.win = sb(c, 'win', [128, 8, IN_TOTAL], BF16)
        c.hT = [sb(c, 'hT%d' % i, [128, 8, 512], BF16) for i in range(2)]
        alloc_ln(c)
        c.o16 = [sb(c, 'o16_%d' % i, [128, 512], BF16) for i in range(4)]
        c.o32 = [sb(c, 'o32_%d' % i, [128, 512], F32) for i in range(4)]
        c.psB = [c.bank[i] for i in range(4)]
        load_mod(c, l)
        _stage_proj(c, l, xsrc)
        c.p.barrier()


def alloc_ln(c):
    c.xt = [sb(c, 'xt%d' % i, [128, D], F32) for i in range(3)]
    c.lnst = [sb(c, 'lnst%d' % i, [128, 8], F32) for i in range(3)]
    c.junk = [sb(c, 'junk%d' % i, [128, D], BF16) for i in range(2)]
    c.xh = [sb(c, 'xh%d' % i, [128, D], BF16) for i in range(2)]
    c.tp = [bank16(c, 4), bank16(c, 5)]
    c.tpk = [('bank', 4), ('bank', 5)]
    c.ln_i = 0


def _stage_proj(c, l, xsrc):
    p, nc = c.p, c.nc
    I = c.inp
    win = c.win
    for k in range(8):
        p.dma('pool', 'win', [], [('win', k)],
              [(win[:, k, :], I['w_in'][l, k * 128:(k + 1) * 128, :])])
    S = c.scr[l]
    blocks = [(0, 2)] + [(2 + 4 * i, 4) for i in range(16)]
    blocks = blocks[:c.cfg.get('nblk', 17)]
    ev = 0
    for bi, (t0, ntl) in enumerate(blocks):
        T = ntl * 128
        tok0 = t0 * 128
        hb = bi % 2
        hT = c.hT[hb]
        r = 1 if bi == 0 else 0
        for j in range(ntl):
            ln_mod_tile(c, xsrc(t0 + j), r, 0, 1, hT, j * 128, ('hT', hb))
        hkeys = [('hT', hb, k) for k in range(8)]
        wkeys = [('win', k) for k in range(8)]
        if c.cfg.get('nomm'):
            continue
        for name, off, ncols in FM_GROUPS:
            for m0 in range(0, ncols, 128):
                M = min(128, ncols - m0)
                pb = ev % 4
                pt = c.psB[pb]

                def mm(E, pt=pt, off=off, m0=m0, M=M, T=T):
                    ins = None
                    for k in range(8):
                        ins = E.matmul(pt[0:M, 0:T], lhsT=win[:, k, off + m0:off + m0 + M], rhs=hT[:, k, 0:T],
                                       start=(k == 0), stop=(k == 7))
                    return ins
                p.op('pe', hkeys + wkeys, [('bank', pb)], mm)
                fp32 = name in ('AFT', 'ABT')
                ob = ev % 4
                ot = (c.o32 if fp32 else c.o16)[ob]
                okey = ('o32' if fp32 else 'o16', ob)
                eng = 'act' if ev % 2 == 0 else 'dve'
                if eng == 'act':
                    p.op('act', [('bank', pb)], [okey],
                         lambda E, ot=ot, pt=pt, M=M, T=T: E.activation(out=ot[0:M, 0:T], in_=pt[0:M, 0:T], func=AF.Copy))
                else:
                    p.op('dve', [('bank', pb)], [okey],
                         lambda E, ot=ot, pt=pt, M=M, T=T: E.tensor_copy(out=ot[0:M, 0:T], in_=pt[0:M, 0:T]))
                p.dma('sp', 'o%s%d' % ('32' if fp32 else '16', ob), [okey], [(name, l)],
                      [(S[name][m0:m0 + M, tok0:tok0 + T], ot[0:M, 0:T])])
                ev += 1
        for name, off, ncols in TM_GROUPS:
            for n0 in range(0, ncols, 512):
                N = min(512, ncols - n0)
                for j in range(ntl):
                    pb = ev % 4
                    pt = c.psB[pb]

                    def mm(E, pt=pt, off=off, n0=n0, N=N, j=j):
                        ins = None
                        for k in range(8):
                            ins = E.matmul(pt[:, 0:N], lhsT=hT[:, k, j * 128:(j + 1) * 128],
                                           rhs=win[:, k, off + n0:off + n0 + N], start=(k == 0), stop=(k == 7))
                        return ins
                    p.op('pe', hkeys + wkeys, [('bank', pb)], mm)
                    fp32 = name == 'MKR'
                    ob = ev % 4
                    ot = (c.o32 if fp32 else c.o16)[ob]
                    okey = ('o32' if fp32 else 'o16', ob)
                    if name == 'G3':
                        p.op('act', [('bank', pb)], [okey],
                             lambda E, ot=ot, pt=pt, N=N: E.activation(out=ot[:, 0:N], in_=pt[:, 0:N], func=AF.Sigmoid))
                    elif ev % 2 == 0:
                        p.op('act', [('bank', pb)], [okey],
                             lambda E, ot=ot, pt=pt, N=N: E.activation(out=ot[:, 0:N], in_=pt[:, 0:N], func=AF.Copy))
                    else:
                        p.op('dve', [('bank', pb)], [okey],
                             lambda E, ot=ot, pt=pt, N=N: E.tensor_copy(out=ot[:, 0:N], in_=pt[:, 0:N]))
                    p.dma('sp', 'o%s%d' % ('32' if fp32 else '16', ob), [okey], [(name, l)],
                          [(S[name][tok0 + j * 128:tok0 + (j + 1) * 128, n0:n0 + N], ot[:, 0:N])])
                    ev += 1


def stage_gla(c, l):
    with ExitStack() as es:
        c.es = es
        _stage_gla(c, l)
        c.p.barrier()


def _stage_gla(c, l):
    p, nc = c.p, c.nc
    I = c.inp
    S = c.scr[l]
    NCH = NTILE
    qT = sb(c, 'g_qT', [128, NT], BF16)
    kT = sb(c, 'g_kT', [128, NT], BF16)
    v = sb(c, 'g_v', [128, NCH, 256], BF16)
    oacc = sb(c, 'g_oacc', [128, NCH, 256], F32)
    wa2 = sb(c, 'g_wa2', [16, 2, 256], F32)
    ba = sb(c, 'g_ba', [1, 2, 256], F32)
    normw = sb(c, 'g_normw', [128, 128], F32)
    aft = [sb(c, 'g_aft%d' % i, [16, 128], F32) for i in range(3)]
    g1 = [sb(c, 'g_g1%d' % i, [128, 128], F32) for i in range(2)]
    g2 = [sb(c, 'g_g2%d' % i, [128, 128], F32) for i in range(2)]
    e1 = [sb(c, 'g_e1%d' % i, [128, 128], F32) for i in range(2)]
    e2 = [sb(c, 'g_e2%d' % i, [128, 128], F32) for i in range(2)]
    qb = [sb(c, 'g_qb%d' % i, [128, 128], BF16) for i in range(2)]
    kb = [sb(c, 'g_kb%d' % i, [128, 128], BF16) for i in range(2)]
    kd = [sb(c, 'g_kd%d' % i, [128, 128], BF16) for i in range(2)]
    kdt = [sb(c, 'g_kdt%d' % i, [128, 128], BF16) for i in range(2)]
    am = [sb(c, 'g_am%d' % i, [128, 256], BF16) for i in range(2)]
    St = sb(c, 'g_S', [128, 128], F32)
    Sb = sb(c, 'g_Sb', [128, 128], BF16)
    rst = sb(c, 'g_rst', [128, NCH * 2], F32)
    rst2 = sb(c, 'g_rst2', [128, NCH * 2], F32)
    junk = sb(c, 'g_junk', [128, 128], BF16)
    osb = [sb(c, 'g_osb%d' % i, [128, 512], BF16) for i in range(2)]
    psG = c.bank[0][:, 0:128]
    psBt = [c.bank[1][:, 0:128], c.bank[2][:, 0:128]]
    psA = c.bank[3][:, 0:256]
    psK = c.bank[4][:, :].bitcast(BF16)[:, 0:128]
    psO = [c.bank[5][:, 0:256], c.bank[6][:, 0:256]]
    psS = c.bank[7][:, 0:128]
    c.tp = [bank16(c, 1), bank16(c, 2)]
    cst = c.cst
    U16 = [cst[:, 128:256], cst[:, 256:384]]
    MSK = [cst[:, 384:512], cst[:, 512:640]]
    ones = cst[:, 640:768]

    p.dma('sp', 'g_w', [], ['g_w'],
          [(wa2[:, 0, :], I['gla_wa2_f'][l]), (wa2[:, 1, :], I['gla_wa2_b'][l]),
           (ba[0:1, 0, :], I['gla_ba_f'][l:l + 1, :]), (ba[0:1, 1, :], I['gla_ba_b'][l:l + 1, :]),
           (normw[:, :], I['gla_norm'][l, :].partition_broadcast(128))])
    gr = v
    step = 0
    for hp in range(2):
        p.dma('sp', 'g_q', [], ['g_qT'], [(qT[:, :], S['QT'][hp * 128:(hp + 1) * 128, :])])
        p.dma('sp', 'g_k', [], ['g_kT'], [(kT[:, :], S['KT'][hp * 128:(hp + 1) * 128, :])])
        p.dma('sp', 'g_v', [], ['g_v'],
              [(v[:, :, :], S['V'][:, hp * 256:(hp + 1) * 256].rearrange("(n p) c -> p n c", p=128))])
        for dirn in range(2):
            p.op('dve', [], ['g_S'], lambda E: E.memset(St[:, :], 0.0))
            p.op('dve', [], ['g_Sb'], lambda E: E.memset(Sb[:, :], 0.0))
            order = list(range(NCH)) if dirn == 0 else [1, 0] + list(range(NCH - 1, 1, -1))
            last = 127 if dirn == 0 else 0
            gsrc = S['AFT'] if dirn == 0 else S['ABT']
            for n in order[:c.cfg.get('gsteps', 999)]:
                t0 = n * 128
                s2 = step % 2
                s3 = step % 3
                step += 1
                a_t = aft[s3]
                p.dma('sp', 'g_aft%d' % s3, [], [('g_aft', s3)], [(a_t[:, :], gsrc[:, t0:t0 + 128])])

                def mmg(E, a_t=a_t, dirn=dirn, hp=hp):
                    E.matmul(psG[:, :], lhsT=a_t[0:16, :], rhs=wa2[0:16, dirn, hp * 128:(hp + 1) * 128],
                             start=True, stop=False)
                    return E.matmul(psG[:, :], lhsT=ones[0:1, :], rhs=ba[0:1, dirn, hp * 128:(hp + 1) * 128],
                                    start=False, stop=True)
                p.op('pe', [('g_aft', s3), 'g_w', 'cst'], [('bank', 0)], mmg)
                if c.cfg.get('gsub', 99) < 1:
                    continue
                p.op('act', [('bank', 0)], [('g_g1', s2)],
                     lambda E, s2=s2: E.activation(out=g1[s2][:, :], in_=psG[:, :], func=AF.Exp, scale=-1.0))
                p.op('act', [('g_g1', s2)], [('g_g2', s2)],
                     lambda E, s2=s2: E.activation(out=g2[s2][:, :], in_=g1[s2][:, :], func=AF.Ln, bias=1.0, scale=1.0))
                if c.cfg.get('gsub', 99) < 2:
                    continue
                pB = psBt[s2]
                p.op('pe', [('g_g2', s2), 'cst'], [('bank', 1 + s2)],
                     lambda E, s2=s2, pB=pB, dirn=dirn: E.matmul(pB[:, :], lhsT=g2[s2][:, :], rhs=U16[dirn],
                                                                 start=True, stop=True))
                p.op('act', [('bank', 1 + s2)], [('g_e1', s2)],
                     lambda E, s2=s2, pB=pB: E.activation(out=e1[s2][:, :], in_=pB[:, :], func=AF.Exp, scale=-1.0))
                p.op('act', [('bank', 1 + s2)], [('g_e2', s2)],
                     lambda E, s2=s2, pB=pB: E.activation(out=e2[s2][:, :], in_=pB[:, :], func=AF.Exp, scale=1.0))
                if c.cfg.get('gsub', 99) < 3:
                    continue
                p.op('dve', ['g_qT', ('g_e1', s2)], [('g_qb', s2)],
                     lambda E, s2=s2, t0=t0: E.scalar_tensor_tensor(out=qb[s2][:, :], in0=qT[:, t0:t0 + 128], scalar=0.125,
                                                                    in1=e1[s2][:, :], op0=ALU.mult, op1=ALU.mult))
                p.op('dve', ['g_kT', ('g_e2', s2)], [('g_kb', s2)],
                     lambda E, s2=s2, t0=t0: E.tensor_tensor(out=kb[s2][:, :], in0=kT[:, t0:t0 + 128], in1=e2[s2][:, :],
                                                             op=ALU.mult))
                p.op('dve', [('g_kb', s2), ('g_e1', s2)], [('g_kd', s2)],
                     lambda E, s2=s2, last=last: E.tensor_scalar(out=kd[s2][:, :], in0=kb[s2][:, :],
                                                                 scalar1=e1[s2][:, last:last + 1], scalar2=None,
                                                                 op0=ALU.mult))

                if c.cfg.get('gsub', 99) < 4:
                    continue
                def mma(E, s2=s2):
                    ins = None
                    for h in range(2):
                        if h == 1:
                            p.pe_fence(ins)
                        ins = E.matmul(psA[:, h * 128:(h + 1) * 128], lhsT=kb[s2][h * 64:(h + 1) * 64, :],
                                       rhs=qb[s2][h * 64:(h + 1) * 64, :], start=True, stop=True)
                    return ins
                p.op('pe', [('g_kb', s2), ('g_qb', s2)], [('bank', 3)], mma)
                if c.cfg.get('gsub', 99) < 5:
                    continue
                msk = MSK[dirn]
                p.op('dve', [('bank', 3), 'cst'], [('g_am', s2)],
                     lambda E, s2=s2, msk=msk: E.tensor_tensor(
                         out=am[s2][:, :].rearrange("p (h i) -> p h i", h=2),
                         in0=psA[:, :].rearrange("p (h i) -> p h i", h=2),
                         in1=msk.unsqueeze(1).to_broadcast([128, 2, 128]), op=ALU.mult))
                if c.cfg.get('gsub', 99) < 6:
                    continue
                p.op('pe', [('g_kd', s2), 'ident'], [('bank', 4)],
                     lambda E, s2=s2: E.transpose(psK[:, :], kd[s2][:, :], c.ident[:, :]))
                p.op('act', [('bank', 4)], [('g_kdt', s2)],
                     lambda E, s2=s2: E.activation(out=kdt[s2][:, :], in_=psK[:, :], func=AF.Copy))
                if c.cfg.get('gsub', 99) < 7:
                    continue
                pO = psO[s2]

                def mmo(E, s2=s2, pO=pO, n=n):
                    ins = None
                    for h in range(2):
                        if h == 1:
                            p.pe_fence(ins)
                        E.matmul(pO[:, h * 128:(h + 1) * 128], lhsT=am[s2][:, h * 128:(h + 1) * 128],
                                 rhs=v[:, n, h * 128:(h + 1) * 128], start=True, stop=False)
                        ins = E.matmul(pO[:, h * 128:(h + 1) * 128], lhsT=qb[s2][h * 64:(h + 1) * 64, :],
                                       rhs=Sb[h * 64:(h + 1) * 64, :], start=False, stop=True)
                    return ins
                p.op('pe', [('g_am', s2), 'g_v', ('g_qb', s2), 'g_Sb'], [('bank', 5 + s2)], mmo)
                if dirn == 0:
                    p.op('act', [('bank', 5 + s2)], [('g_oacc', n)],
                         lambda E, pO=pO, n=n: E.activation(out=oacc[:, n, :], in_=pO[:, :], func=AF.Copy))
                else:
                    p.op('dve', [('bank', 5 + s2), ('g_oacc', n)], [('g_oacc', n)],
                         lambda E, pO=pO, n=n: E.tensor_tensor(out=oacc[:, n, :], in0=pO[:, :], in1=oacc[:, n, :],
                                                               op=ALU.add))

                if c.cfg.get('gsub', 99) < 8:
                    continue
                def mms(E, s2=s2, n=n):
                    ins = None
                    for h in range(2):
                        if h == 1:
                            p.pe_fence(ins)
                        ins = E.matmul(psS[h * 64:(h + 1) * 64, :], lhsT=kdt[s2][:, h * 64:(h + 1) * 64],
                                       rhs=v[:, n, h * 128:(h + 1) * 128], start=True, stop=True)
                    return ins
                p.op('pe', [('g_kdt', s2), 'g_v'], [('bank', 7)], mms)
                if c.cfg.get('gsub', 99) < 9:
                    continue
                p.op('dve', [('bank', 7), ('g_e1', s2), 'g_S'], ['g_S'],
                     lambda E, s2=s2, last=last: E.scalar_tensor_tensor(out=St[:, :], in0=St[:, :],
                                                                        scalar=e1[s2][:, last:last + 1], in1=psS[:, :],
                                                                        op0=ALU.mult, op1=ALU.add))
                p.op('act', ['g_S'], ['g_Sb'], lambda E: E.activation(out=Sb[:, :], in_=St[:, :], func=AF.Copy))
        if c.cfg.get('gnofin'):
            continue
        okeys = [('g_oacc', n) for n in range(NCH)]
        for n in range(NCH):
            for h in range(2):
                p.op('act', [('g_oacc', n)], ['g_junk', ('g_rst', n, h)],
                     lambda E, n=n, h=h: E.activation(out=junk[:, :], in_=oacc[:, n, h * 128:(h + 1) * 128],
                                                      func=AF.Square, accum_out=rst[:, n * 2 + h:n * 2 + h + 1]))
        rkeys = [('g_rst', n, h) for n in range(NCH) for h in range(2)]
        p.op('act', rkeys, ['g_rst2'],
             lambda E: E.activation(out=rst2[:, :], in_=rst[:, :], func=AF.Ln, scale=1.0 / 128, bias=RMS_EPS))
        p.op('act', ['g_rst2'], ['g_rst2'],
             lambda E: E.activation(out=rst2[:, :], in_=rst2[:, :], func=AF.Exp, scale=-0.5))
        p.dma('sp', 'g_v', [], ['g_v'],
              [(gr[:, :, :], S['GR'][:, hp * 256:(hp + 1) * 256].rearrange("(n p) c -> p n c", p=128))])
        p.op('act', ['g_v'], ['g_v'], lambda E: E.activation(out=gr[:, :, :], in_=gr[:, :, :], func=AF.Silu))
        o4 = oacc[:, :, :].rearrange("p n (h d) -> p (n h) d", h=2)
        p.op('dve', okeys + ['g_rst2'], okeys,
             lambda E: E.tensor_tensor(out=o4, in0=o4, in1=rst2[:, :].unsqueeze(2).to_broadcast([128, NCH * 2, 128]),
                                       op=ALU.mult))
        p.op('dve', okeys + ['g_w'], okeys,
             lambda E: E.tensor_tensor(out=o4, in0=o4, in1=normw[:, :].unsqueeze(1).to_broadcast([128, NCH * 2, 128]),
                                       op=ALU.mult))
        p.op('dve', okeys + ['g_v'], ['g_v'],
             lambda E: E.tensor_tensor(out=gr[:, :, :], in0=oacc[:, :, :], in1=gr[:, :, :], op=ALU.mult))
        groups = [(0, 2)] + [(2 + 4 * i, 4) for i in range(16)]
        for gi, (n0, cnt) in enumerate(groups):
            for h in range(2):
                tb = (gi * 2 + h) % 2
                tpt = c.tp[tb]

                def tr(E, n0=n0, cnt=cnt, h=h, tpt=tpt):
                    ins = None
                    for j in range(cnt):
                        ins = E.transpose(tpt[:, j, :], gr[:, n0 + j, h * 128:(h + 1) * 128], c.ident[:, :])
                    return ins
                p.op('pe', ['g_v', 'ident'], [('bank', 1 + tb)], tr)
                ot = osb[tb]
                T = cnt * 128
                if tb == 0:
                    p.op('act', [('bank', 1 + tb)], [('g_osb', tb)],
                         lambda E, ot=ot, tpt=tpt, T=T: E.activation(out=ot[:, 0:T], in_=tpt[:, :, :].rearrange("p a b -> p (a b)")[:, 0:T], func=AF.Copy))
                else:
                    p.op('dve', [('bank', 1 + tb)], [('g_osb', tb)],
                         lambda E, ot=ot, tpt=tpt, T=T: E.tensor_copy(out=ot[:, 0:T], in_=tpt[:, :, :].rearrange("p a b -> p (a b)")[:, 0:T]))
                row0 = (hp * 2 + h) * 128
                p.dma('sp', 'g_osb%d' % tb, [('g_osb', tb)], [('OGT', l)],
                      [(S['OGT'][row0:row0 + 128, n0 * 128:n0 * 128 + T], ot[:, 0:T])])


MLA_SCALE = 96 ** -0.5


def stage_mla(c, l, ctx_q):
    with ExitStack() as es:
        c.es = es
        _stage_mla(c, l, ctx_q)
        c.p.barrier()


def _rms_rstd(c, psq, rs, nfeat, key_ps, key_rs):
    p = c.p
    p.op('act', [key_ps], [key_rs],
         lambda E: E.activation(out=rs, in_=psq, func=AF.Ln, scale=1.0 / nfeat, bias=RMS_EPS))
    p.op('act', [key_rs], [key_rs], lambda E: E.activation(out=rs, in_=rs, func=AF.Exp, scale=-0.5))


def _stage_mla(c, l, ctx_q):
    p, nc = c.p, c.nc
    I = c.inp
    S = c.scr[l]
    KpT, VpD, QpT = c.KpT, c.VpD, c.QpT
    cst = c.cst
    ones32 = cst[:, 640:768]
    wkv32 = sb(c, 'm_wkv32', [128, 2, 1024], F32)
    wq32 = sb(c, 'm_wq32', [128, 3, 768], F32)
    wkv = sb(c, 'm_wkv', [128, 2, 1024], BF16)
    wq = sb(c, 'm_wq', [128, 3, 768], BF16)
    gn = sb(c, 'm_gn', [128, 5], F32)
    onesb = sb(c, 'm_onesb', [128, 8], BF16)
    p.dma('sp', 'm_w', [], ['m_w32'],
          [(wkv32[:, :, :], I['mla_w_ukv'][l].rearrange("(k p) n -> p k n", p=128)),
           (wq32[:, :, :], I['mla_w_uq'][l].rearrange("(k p) n -> p k n", p=128))])
    p.dma('sp', 'm_g', [], ['m_gn'],
          [(gn[:, 0:2], I['mla_kv_norm'][l, :].rearrange("(k p) -> p k", p=128)),
           (gn[:, 2:5], I['mla_q_norm'][l, :].rearrange("(k p) -> p k", p=128))], slow=True)
    p.op('dve', [], ['m_onesb'], lambda E: E.memset(onesb[:, :], 1.0))
    for k in range(2):
        p.op('dve', ['m_w32', 'm_gn'], [('m_wkv', k)],
             lambda E, k=k: E.tensor_scalar(out=wkv[:, k, :], in0=wkv32[:, k, :], scalar1=gn[:, k:k + 1], scalar2=None,
                                            op0=ALU.mult))
    for k in range(3):
        p.op('dve', ['m_w32', 'm_gn'], [('m_wq', k)],
             lambda E, k=k: E.tensor_scalar(out=wq[:, k, :], in0=wq32[:, k, :], scalar1=gn[:, 2 + k:3 + k], scalar2=None,
                                            op0=ALU.mult))
    wkvk = [('m_wkv', k) for k in range(2)]
    wqk = [('m_wq', k) for k in range(3)]
    src = [sb(c, 'm_src%d' % i, [128, 3, 512], BF16) for i in range(2)]
    sq = [sb(c, 'm_sq%d' % i, [128, 3, 512], BF16) for i in range(2)]
    rs = [sb(c, 'm_rs%d' % i, [128, 2], F32) for i in range(2)]
    kr = [sb(c, 'm_kr%d' % i, [128, 32], F32) for i in range(2)]
    krr = [sb(c, 'm_krr%d' % i, [128, 32], F32) for i in range(2)]
    rtab = [sb(c, 'm_rtab%d' % i, [128, 32], F32) for i in range(2)]
    tmp16 = [sb(c, 'm_tmp%d' % i, [128, 16], F32) for i in range(2)]
    kp = [sb(c, 'm_kp%d' % i, [128, 8, 97], BF16) for i in range(2)]
    vp = [sb(c, 'm_vp%d' % i, [128, 8, 65], BF16) for i in range(2)]
    kpt = [sb(c, 'm_kpt%d' % i, [97, 8, 128], BF16) for i in range(2)]
    qf = [sb(c, 'm_qf%d' % i, [128, 8, 96], F32) for i in range(2)]
    qsq = sb(c, 'm_qsq', [128, 8, 96], F32)
    ks = sb(c, 'm_ks', [128, 8], F32)
    kmx = sb(c, 'm_kmx', [128, 8], F32)
    kmT = sb(c, 'm_kmT', [8, 1], F32)
    kdiag = sb(c, 'm_kdiag', [8, 8], F32)
    kbc = sb(c, 'm_kbc', [128, 8], F32)
    qn = [sb(c, 'm_qn%d' % i, [128, 8], F32) for i in range(2)]
    B = c.bank
    bk = lambda i: ('bank', i)
    for i in range(2):
        p.op('dve', [], [('m_kp', i)], lambda E, i=i: E.memset(kp[i][:, :, :], 1.0))
        p.op('dve', [], [('m_vp', i)], lambda E, i=i: E.memset(vp[i][:, :, :], 1.0))
    p.op('dve', [], ['m_kmx'], lambda E: E.memset(kmx[:, :], 0.0))

    blocks = [(0, 2)] + [(2 + 4 * i, 4) for i in range(16)]
    it = 0

    def load_src(name, nk, t0, ntl, sslot):
        T = ntl * 128
        p.dma('sp', 'm_src%d' % sslot, [], [('m_src', sslot)],
              [(src[sslot][:, 0:nk, 0:T], S[name][:, t0 * 128:t0 * 128 + T].rearrange("(k p) t -> p k t", p=128))])
        p.op('act', [('m_src', sslot)], [('m_sq', sslot)],
             lambda E: E.activation(out=sq[sslot][:, 0:nk, 0:T], in_=src[sslot][:, 0:nk, 0:T], func=AF.Square))

    def rope_rows(t, s2):
        p.dma('sp', 'm_rtab%d' % s2, [], [('m_rtab', s2)], [(rtab[s2][:, :], I['rope'][t * 128:(t + 1) * 128, :])])

    for bi, (tb0, ntl) in enumerate(blocks):
        sslot = bi % 2
        load_src('MKVAT', 2, tb0, ntl, sslot)
        for j in range(ntl):
            t = tb0 + j
            s2 = it % 2
            it += 1
            cols = slice(j * 128, (j + 1) * 128)

            def mmq(E, sslot=sslot, cols=cols):
                ins = None
                for k in range(2):
                    ins = E.matmul(B[0][:, 0:1], lhsT=sq[sslot][:, k, cols], rhs=onesb[:, 0:1], start=(k == 0), stop=(k == 1))
                return ins
            p.op('pe', [('m_sq', sslot), 'm_onesb'], [bk(0)], mmq)
            _rms_rstd(c, B[0][:, 0:1], rs[s2][:, 0:1], 256, bk(0), ('m_rs', s2))
            for half in range(2):
                def mmkv(E, sslot=sslot, cols=cols, half=half):
                    ins = None
                    for k in range(2):
                        ins = E.matmul(B[1 + half][:, :], lhsT=src[sslot][:, k, cols],
                                       rhs=wkv[:, k, half * 512:(half + 1) * 512], start=(k == 0), stop=(k == 1))
                    return ins
                p.op('pe', [('m_src', sslot)] + wkvk, [bk(1 + half)], mmkv)
            for half in range(2):
                pv = B[1 + half][:, :].rearrange("p (h e) -> p h e", h=4)
                p.op('dve', [bk(1 + half), ('m_rs', s2)], [('m_kp', s2)],
                     lambda E, pv=pv, half=half, s2=s2: E.tensor_scalar(out=kp[s2][:, half * 4:(half + 1) * 4, 0:64], in0=pv[:, :, 0:64],
                                                                        scalar1=rs[s2][:, 0:1], scalar2=None, op0=ALU.mult))
                p.op('act', [bk(1 + half), ('m_rs', s2)], [('m_vp', s2)],
                     lambda E, pv=pv, half=half, s2=s2: E.activation(out=vp[s2][:, half * 4:(half + 1) * 4, 0:64], in_=pv[:, :, 64:128],
                                                                     func=AF.Copy, scale=rs[s2][:, 0:1]))
            p.dma('sp', 'm_kr%d' % s2, [], [('m_kr', s2)], [(kr[s2][:, :], S['MKR'][t * 128:(t + 1) * 128, :])])
            if t >= 2:
                rope_rows(t - 2, s2)
                x1, x2 = kr[s2][:, 0:16], kr[s2][:, 16:32]
                cs, sn = rtab[s2][:, 0:16], rtab[s2][:, 16:32]
                o1, o2 = krr[s2][:, 0:16], krr[s2][:, 16:32]
                tm = tmp16[s2]
                rk = [('m_kr', s2), ('m_rtab', s2)]
                p.op('dve', rk, [('m_krr', s2)], lambda E, o1=o1, x1=x1, cs=cs: E.tensor_tensor(out=o1, in0=x1, in1=cs, op=ALU.mult))
                p.op('dve', rk, [('m_tmp', s2)], lambda E, tm=tm, x2=x2, sn=sn: E.tensor_tensor(out=tm[:, :], in0=x2, in1=sn, op=ALU.mult))
                p.op('dve', [('m_krr', s2), ('m_tmp', s2)], [('m_krr', s2)],
                     lambda E, o1=o1, tm=tm: E.tensor_tensor(out=o1, in0=o1, in1=tm[:, :], op=ALU.subtract))
                p.op('dve', rk + [('m_krr', s2)], [('m_krr', s2)], lambda E, o2=o2, x2=x2, cs=cs: E.tensor_tensor(out=o2, in0=x2, in1=cs, op=ALU.mult))
                p.op('dve', rk + [('m_tmp', s2)], [('m_tmp', s2)], lambda E, tm=tm, x1=x1, sn=sn: E.tensor_tensor(out=tm[:, :], in0=x1, in1=sn, op=ALU.mult))
                p.op('dve', [('m_krr', s2), ('m_tmp', s2)], [('m_krr', s2)],
                     lambda E, o2=o2, tm=tm: E.tensor_tensor(out=o2, in0=o2, in1=tm[:, :], op=ALU.add))
                rsrc, rkey = krr[s2], ('m_krr', s2)
            else:
                rsrc, rkey = kr[s2], ('m_kr', s2)
            p.op('dve', [rkey, ('m_kp', s2)], [('m_kp', s2)],
                 lambda E, rsrc=rsrc, s2=s2: E.tensor_copy(out=kp[s2][:, :, 64:96],
                                                           in_=rsrc[:, :].unsqueeze(1).to_broadcast([128, 8, 32])))
            p.op('dve', [('m_kp', s2)], ['m_qsq'],
                 lambda E, s2=s2: E.tensor_tensor(out=qsq[:, :, :], in0=kp[s2][:, :, 0:96], in1=kp[s2][:, :, 0:96], op=ALU.mult))
            p.op('dve', ['m_qsq'], ['m_ks'], lambda E: E.tensor_reduce(out=ks[:, :], in_=qsq[:, :, :], axis=AX.X, op=ALU.add))
            p.op('dve', ['m_ks', 'm_kmx'], ['m_kmx'], lambda E: E.tensor_tensor(out=kmx[:, :], in0=kmx[:, :], in1=ks[:, :], op=ALU.max))
            tpb = 3 + s2
            tpv = B[tpb][:, :].bitcast(BF16).rearrange("p (h t) -> p h t", h=8)

            def trk(E, s2=s2, tpv=tpv):
                ins = None
                for h in range(8):
                    ins = E.transpose(tpv[0:97, h, :], kp[s2][:, h, :], c.ident[:, :])
                return ins
            p.op('pe', [('m_kp', s2), 'ident'], [bk(tpb)], trk)
            p.op('act', [bk(tpb)], [('m_kpt', s2)],
                 lambda E, s2=s2, tpv=tpv: E.activation(out=kpt[s2][:, :, :], in_=tpv[0:97, :, :], func=AF.Copy))
            p.dma('sp', 'm_kpt%d' % s2, [('m_kpt', s2)], ['KpT'],
                  [(KpT[:, :, t * 128:(t + 1) * 128].rearrange("h d t -> d h t"), kpt[s2][:, :, :])])
            p.dma('sp', 'm_vp%d' % s2, [('m_vp', s2)], ['VpD'],
                  [(VpD[:, t * 128:(t + 1) * 128, :].rearrange("h p e -> p h e"), vp[s2][:, :, :])])
    p.op('pe', ['m_kmx', 'cst'], [bk(0)], lambda E: E.transpose(B[0][0:8, 0:128], kmx[:, :], cst[:, 0:128]))
    p.op('dve', [bk(0)], ['m_kmT'], lambda E: E.tensor_reduce(out=kmT[:, :], in_=B[0][0:8, 0:128], axis=AX.X, op=ALU.max))
    p.op('dve', ['m_kmT', 'cst'], ['m_kdiag'],
         lambda E: E.tensor_scalar(out=kdiag[:, :], in0=cst[0:8, 0:8], scalar1=kmT[:, 0:1], scalar2=None, op0=ALU.mult))
    p.op('pe', ['m_kdiag', 'cst'], [bk(0)],
         lambda E: E.matmul(B[0][:, 0:8], lhsT=ones32[0:8, :], rhs=kdiag[:, :], start=True, stop=True))
    p.op('act', [bk(0)], ['m_kbc'], lambda E: E.activation(out=kbc[:, :], in_=B[0][:, 0:8], func=AF.Copy))
    qblocks = ([(0, 2)] if ctx_q else []) + [(2 + 4 * i, 4) for i in range(16)]
    for bi, (tb0, ntl) in enumerate(qblocks):
        sslot = bi % 2
        load_src('MQAT', 3, tb0, ntl, sslot)
        for j in range(ntl):
            t = tb0 + j
            s2 = it % 2
            it += 1
            cols = slice(j * 128, (j + 1) * 128)

            def mmq(E, sslot=sslot, cols=cols):
                ins = None
                for k in range(3):
                    ins = E.matmul(B[0][:, 0:1], lhsT=sq[sslot][:, k, cols], rhs=onesb[:, 0:1], start=(k == 0), stop=(k == 2))
                return ins
            p.op('pe', [('m_sq', sslot), 'm_onesb'], [bk(0)], mmq)
            _rms_rstd(c, B[0][:, 0:1], rs[s2][:, 0:1], 384, bk(0), ('m_rs', s2))
            p.op('dve', [('m_rs', s2)], [('m_rs', s2)],
                 lambda E, s2=s2: E.tensor_scalar(out=rs[s2][:, 0:1], in0=rs[s2][:, 0:1], scalar1=MLA_SCALE, scalar2=None, op0=ALU.mult))
            for half, (n0, nn) in enumerate([(0, 512), (512, 256)]):
                def mmqq(E, sslot=sslot, cols=cols, half=half, n0=n0, nn=nn):
                    ins = None
                    for k in range(3):
                        ins = E.matmul(B[1 + half][:, 0:nn], lhsT=src[sslot][:, k, cols], rhs=wq[:, k, n0:n0 + nn],
                                       start=(k == 0), stop=(k == 2))
                    return ins
                p.op('pe', [('m_src', sslot)] + wqk, [bk(1 + half)], mmqq)
            qv = qf[s2][:, :, :].rearrange("p h e -> p (h e)")
            p.op('dve', [bk(1), ('m_rs', s2)], [('m_qf', s2, 0)],
                 lambda E, qv=qv, s2=s2: E.tensor_scalar(out=qv[:, 0:512], in0=B[1][:, 0:512], scalar1=rs[s2][:, 0:1], scalar2=None, op0=ALU.mult))
            p.op('act', [bk(2), ('m_rs', s2)], [('m_qf', s2, 1)],
                 lambda E, qv=qv, s2=s2: E.activation(out=qv[:, 512:768], in_=B[2][:, 0:256], func=AF.Copy, scale=rs[s2][:, 0:1]))
            qk = [('m_qf', s2, 0), ('m_qf', s2, 1)]
            kpq = kp[s2]
            if t >= 2:
                rope_rows(t - 2, s2)
                x1, x2 = qf[s2][:, :, 64:80], qf[s2][:, :, 80:96]
                cs = rtab[s2][:, 0:16].unsqueeze(1).to_broadcast([128, 8, 16])
                sn = rtab[s2][:, 16:32].unsqueeze(1).to_broadcast([128, 8, 16])
                ta, tb_ = qsq[:, :, 0:16], qsq[:, :, 16:32]
                tc_, td = qsq[:, :, 32:48], qsq[:, :, 48:64]
                rk = qk + [('m_rtab', s2)]
                p.op('dve', rk, ['m_qsq'], lambda E, ta=ta, x1=x1, cs=cs: E.tensor_tensor(out=ta, in0=x1, in1=cs, op=ALU.mult))
                p.op('dve', rk + ['m_qsq'], ['m_qsq'], lambda E, tb_=tb_, x2=x2, sn=sn: E.tensor_tensor(out=tb_, in0=x2, in1=sn, op=ALU.mult))
                p.op('dve', rk + ['m_qsq'], ['m_qsq'], lambda E, tc_=tc_, x2=x2, cs=cs: E.tensor_tensor(out=tc_, in0=x2, in1=cs, op=ALU.mult))
                p.op('dve', rk + ['m_qsq'], ['m_qsq'], lambda E, td=td, x1=x1, sn=sn: E.tensor_tensor(out=td, in0=x1, in1=sn, op=ALU.mult))
                p.op('dve', ['m_qsq'] + qk, qk, lambda E, x1=x1, ta=ta, tb_=tb_: E.tensor_tensor(out=x1, in0=ta, in1=tb_, op=ALU.subtract))
                p.op('dve', ['m_qsq'] + qk, qk, lambda E, x2=x2, tc_=tc_, td=td: E.tensor_tensor(out=x2, in0=tc_, in1=td, op=ALU.add))
            p.op('dve', qk, ['m_qsq'],
                 lambda E, s2=s2: E.tensor_tensor(out=qsq[:, :, :], in0=qf[s2][:, :, :], in1=qf[s2][:, :, :], op=ALU.mult))
            p.op('dve', ['m_qsq'], [('m_qn', s2)], lambda E, s2=s2: E.tensor_reduce(out=qn[s2][:, :], in_=qsq[:, :, :], axis=AX.X, op=ALU.add))
            p.op('dve', [('m_qn', s2), 'm_kbc'], [('m_qn', s2)],
                 lambda E, s2=s2: E.tensor_tensor(out=qn[s2][:, :], in0=qn[s2][:, :], in1=kbc[:, :], op=ALU.mult))
            p.op('act', [('m_qn', s2)], [('m_qn', s2)], lambda E, s2=s2: E.activation(out=qn[s2][:, :], in_=qn[s2][:, :], func=AF.Sqrt))
            p.op('dve', qk + [('m_kp', s2)], [('m_kp', s2)],
                 lambda E, s2=s2: E.tensor_copy(out=kpq[:, :, 0:96], in_=qf[s2][:, :, :]))
            p.op('dve', [('m_qn', s2), ('m_kp', s2)], [('m_kp', s2)],
                 lambda E, s2=s2: E.tensor_scalar(out=kpq[:, :, 96:97], in0=qn[s2][:, :].unsqueeze(2), scalar1=-1.0, scalar2=None, op0=ALU.mult))
            tpb = 3 + s2
            tpv = B[tpb][:, :].bitcast(BF16).rearrange("p (h t) -> p h t", h=8)

            def trq(E, s2=s2, tpv=tpv):
                ins = None
                for h in range(8):
                    ins = E.transpose(tpv[0:97, h, :], kp[s2][:, h, :], c.ident[:, :])
                return ins
            p.op('pe', [('m_kp', s2), 'ident'], [bk(tpb)], trq)
            p.op('act', [bk(tpb)], [('m_kpt', s2)],
                 lambda E, s2=s2, tpv=tpv: E.activation(out=kpt[s2][:, :, :], in_=tpv[0:97, :, :], func=AF.Copy))
            p.dma('sp', 'm_kpt%d' % s2, [('m_kpt', s2)], ['QpT'],
                  [(QpT[:, :, t * 128:(t + 1) * 128].rearrange("h d t -> d h t"), kpt[s2][:, :, :])])
    p.barrier()
    kh = [sb(c, 'm_kh%d' % i, [97, NT], BF16) for i in range(2)]
    qh = [sb(c, 'm_qh%d' % i, [97, NT], BF16) for i in range(2)]
    vh = [sb(c, 'm_vh%d' % i, [128, NTILE, 65], BF16) for i in range(2)]
    pt = [sb(c, 'm_pt%d' % i, [128, 512], BF16) for i in range(3)]
    osb = [sb(c, 'm_osb%d' % i, [65, 512], F32) for i in range(2)]
    on = [sb(c, 'm_on%d' % i, [64, 512], BF16) for i in range(2)]
    ei = 0
    ci = 0
    for h in range(8):
        hs = h % 2
        p.dma('sp', 'm_kh%d' % hs, ['KpT'], [('m_kh', hs)], [(kh[hs][:, :], KpT[h, :, :])])
        p.dma('sp', 'm_qh%d' % hs, ['QpT'], [('m_qh', hs)], [(qh[hs][:, :], QpT[h, :, :])])
        p.dma('sp', 'm_vh%d' % hs, ['VpD'], [('m_vh', hs)],
              [(vh[hs][:, :, :], VpD[h, :, :].rearrange("(n p) e -> p n e", p=128))])
        chunks = ([(0, 256, 2)] if ctx_q else []) + [(256 + 512 * i, 512, NTILE) for i in range(16)]
        for (q0, nq, nkt) in chunks:
            ob = 6 + ci % 2
            cs2 = ci % 2
            ci += 1
            def emit_qk(kt):
                sbk = (ei0 + kt) % 3
                p.op('pe', [('m_kh', hs), ('m_qh', hs)], [bk(sbk)],
                     lambda E: E.matmul(B[sbk][:, 0:nq], lhsT=kh[hs][:, kt * 128:(kt + 1) * 128],
                                        rhs=qh[hs][:, q0:q0 + nq], start=True, stop=True))
            ei0 = ei
            ei += nkt
            for kt in range(min(2, nkt)):
                emit_qk(kt)
            for kt in range(nkt):
                sbk = (ei0 + kt) % 3
                p.op('act', [bk(sbk)], [('m_pt', sbk)],
                     lambda E: E.activation(out=pt[sbk][:, 0:nq], in_=B[sbk][:, 0:nq], func=AF.Exp))
                if kt + 2 < nkt:
                    emit_qk(kt + 2)
                p.op('pe', [('m_vh', hs), ('m_pt', sbk)], [bk(ob)],
                     lambda E: E.matmul(B[ob][0:65, 0:nq], lhsT=vh[hs][:, kt, :], rhs=pt[sbk][:, 0:nq],
                                        start=(kt == 0), stop=(kt == nkt - 1)))
            o_t = osb[cs2]
            p.op('dve', [bk(ob)], [('m_osb', cs2)], lambda E, o_t=o_t, ob=ob, nq=nq: E.tensor_copy(out=o_t[:, 0:nq], in_=B[ob][0:65, 0:nq]))
            p.op('dve', [('m_osb', cs2)], [('m_osb', cs2)],
                 lambda E, o_t=o_t, nq=nq: E.reciprocal(out=o_t[64:65, 0:nq], in_=o_t[64:65, 0:nq]))
            p.op('pe', [('m_osb', cs2), 'cst'], [bk(5)],
                 lambda E, o_t=o_t, nq=nq: E.matmul(B[5][0:64, 0:nq], lhsT=ones32[64:65, 0:64], rhs=o_t[64:65, 0:nq], start=True, stop=True))
            p.op('dve', [bk(5), ('m_osb', cs2)], [('m_on', cs2)],
                 lambda E, o_t=o_t, nq=nq, cs2=cs2: E.tensor_tensor(out=on[cs2][:, 0:nq], in0=o_t[0:64, 0:nq], in1=B[5][0:64, 0:nq], op=ALU.mult))
            p.dma('sp', 'm_on%d' % cs2, [('m_on', cs2)], [('OMT', l)],
                  [(S['OMT'][h * 64:(h + 1) * 64, q0:q0 + nq], on[cs2][:, 0:nq])])


NFFT = 16384


def hy_conv3(c, l):
    p = c.p
    I = c.inp
    S = c.scr[l]
    with ExitStack() as es:
        c.es = es
        wb32 = sb(c, 'h3_w32', [64, 4, 1536], F32)
        wb = sb(c, 'h3_w', [64, 4, 1536], BF16)
        zin = [sb(c, 'h3_zin%d' % i, [64, 10, 512], BF16) for i in range(2)]
        t0_ = [sb(c, 'h3_t0%d' % i, [64, 8, 512], BF16) for i in range(2)]
        t1_ = [sb(c, 'h3_t1%d' % i, [64, 8, 512], BF16) for i in range(2)]
        zo = [sb(c, 'h3_zo%d' % i, [64, 8, 512], BF16) for i in range(2)]
        p.dma('sp', 'h3_w', [], ['h3_w32'],
              [(wb32[:, k, :], I['hy_conv_w'][l, k, :].partition_broadcast(64)) for k in range(3)] +
              [(wb32[:, 3, :], I['hy_conv_b'][l, :].partition_broadcast(64))])
        p.op('dve', ['h3_w32'], ['h3_w'], lambda E: E.tensor_copy(out=wb[:, :, :], in_=wb32[:, :, :]))
        it = 0
        for (tok0, na) in ((0, 2), (NCTX, 64)):
            src = S['HY'][tok0:tok0 + na * 128, :].rearrange("(a b) c -> a b c", b=128)
            dst = c.HYC[tok0:tok0 + na * 128, :].rearrange("(a b) c -> a b c", b=128)
            for cs in range(3):
                c0 = cs * 512
                for bc in range(16):
                    b0 = bc * 8
                    s2 = it % 2
                    it += 1
                    z = zin[s2]
                    pairs = []
                    pre = []
                    lo, hi = b0 - 1, b0 + 9
                    if bc == 0:
                        pre.append(lambda E, z=z: E.memset(z[0:1, 0:1, :], 0.0))
                        if na > 1:
                            pairs.append((z[1:na, 0:1, :], src[0:na - 1, 127:128, c0:c0 + 512]))
                        pairs.append((z[0:na, 1:10, :], src[0:na, 0:9, c0:c0 + 512]))
                    elif bc == 15:
                        pre.append(lambda E, z=z, na=na: E.memset(z[0:na, 9:10, :], 0.0))
                        if na > 1:
                            pairs.append((z[0:na - 1, 9:10, :], src[1:na, 0:1, c0:c0 + 512]))
                        pairs.append((z[0:na, 0:9, :], src[0:na, lo:128, c0:c0 + 512]))
                    else:
                        pairs.append((z[0:na, 0:10, :], src[0:na, lo:hi, c0:c0 + 512]))
                    for f in pre:
                        p.op('pool', [], [('h3_zin', s2)], f)
                    p.dma('sp', 'h3_zin%d' % s2, [('HY', l)], [('h3_zin', s2)], pairs)
                    w = lambda k, c0=c0, na=na: wb[0:na, k, c0:c0 + 512].unsqueeze(1).to_broadcast([na, 8, 512])
                    a0, a1, oz = t0_[s2], t1_[s2], zo[s2]
                    zk = ('h3_zin', s2)
                    p.op('dve', [zk, 'h3_w'], [('h3_t0', s2)],
                         lambda E, z=z, a0=a0, w=w, na=na: E.tensor_tensor(out=a0[0:na], in0=z[0:na, 0:8, :], in1=w(0), op=ALU.mult))
                    p.op('pool', [zk, 'h3_w'], [('h3_t1', s2)],
                         lambda E, z=z, a1=a1, w=w, na=na: E.tensor_tensor(out=a1[0:na], in0=z[0:na, 1:9, :], in1=w(1), op=ALU.mult))
                    p.op('dve', [zk, 'h3_w'], [('h3_zo', s2)],
                         lambda E, z=z, oz=oz, w=w, na=na: E.tensor_tensor(out=oz[0:na], in0=z[0:na, 2:10, :], in1=w(2), op=ALU.mult))
                    p.op('dve', [('h3_t0', s2), 'h3_w'], [('h3_t0', s2)],
                         lambda E, a0=a0, w=w, na=na: E.tensor_tensor(out=a0[0:na], in0=a0[0:na], in1=w(3), op=ALU.add))
                    p.op('dve', [('h3_t0', s2), ('h3_zo', s2)], [('h3_zo', s2)],
                         lambda E, a0=a0, oz=oz, na=na: E.tensor_tensor(out=oz[0:na], in0=a0[0:na], in1=oz[0:na], op=ALU.add))
                    p.op('dve', [('h3_t1', s2), ('h3_zo', s2)], [('h3_zo', s2)],
                         lambda E, a1=a1, oz=oz, na=na: E.tensor_tensor(out=oz[0:na], in0=a1[0:na], in1=oz[0:na], op=ALU.add))
                    p.dma('sp', 'h3_zo%d' % s2, [('h3_zo', s2)], ['HYC'],
                          [(dst[0:na, b0:b0 + 8, c0:c0 + 512], oz[0:na, :, :])])
        p.barrier()


def hy_filters(c, l, job):
    p = c.p
    I = c.inp
    nt = 128 if job == 0 else 4
    feat = I['hy_feat%d' % job]
    tvec = I['hy_tvec%d' % job]
    KTD = c.KTD[job]
    B = c.bank
    bk = lambda i: ('bank', i)
    with ExitStack() as es:
        c.es = es
        w1 = sb(c, 'hf_w1', [33, 64], F32)
        w2 = sb(c, 'hf_w2', [64, 64], F32)
        w3 = sb(c, 'hf_w3', [64, 2048], F32)
        pb = sb(c, 'hf_pb', [64, 8], F32)
        ft = [sb(c, 'hf_ft%d' % i, [33, 512], F32) for i in range(2)]
        tv = [sb(c, 'hf_tv%d' % i, [1, 512], F32) for i in range(2)]
        u = [sb(c, 'hf_u%d' % i, [64, 512], F32) for i in range(2)]
        ui = [sb(c, 'hf_ui%d' % i, [64, 512], mybir.dt.int32) for i in range(2)]
        uf = [sb(c, 'hf_uf%d' % i, [64, 512], F32) for i in range(2)]
        h1 = [sb(c, 'hf_h1%d' % i, [64, 512], F32) for i in range(2)]
        h2 = [sb(c, 'hf_h2%d' % i, [64, 512], F32) for i in range(2)]
        dec = [sb(c, 'hf_dec%d' % i, [128, 512], F32) for i in range(2)]
        hd = [sb(c, 'hf_hd%d' % i, [128, 2, 512], F32) for i in range(2)]
        ha = [sb(c, 'hf_ha%d' % i, [128, 2, 512], F32) for i in range(2)]
        hb = [sb(c, 'hf_hb%d' % i, [128, 2, 512], BF16) for i in range(2)]
        nd = sb(c, 'hf_nd', [1, 512], F32)
        l1 = sb(c, 'hf_l1', [1, 1024], F32)
        cst = c.cst
        ones = cst[:, 640:768]
        p.dma('sp', 'hf_w', [], ['hf_w'],
              [(w1[:, :], I['hy_w1'][l]), (w2[:, :], I['hy_w2'][l]), (w3[:, :], I['hy_w3'][l]),
               (nd[:, :], I['hy_negdelta'][0:1, :])])
        p.dma('sp', 'hf_pb', [], ['hf_pb'],
              [(pb[:, 0:1], I['hy_b1'][l, :].rearrange("(p o) -> p o", o=1)),
               (pb[:, 1:2], I['hy_b2'][l, :].rearrange("(p o) -> p o", o=1)),
               (pb[:, 2:3], I['hy_freq'][l, :].rearrange("(p o) -> p o", o=1))], slow=True)
        p.op('dve', ['hf_pb'], ['hf_pb2'],
             lambda E: E.tensor_scalar(out=pb[:, 3:4], in0=pb[:, 2:3], scalar1=1.0 / (2 * math.pi), scalar2=None, op0=ALU.mult))
        p.op('dve', ['hf_pb', 'hf_pb2'], ['hf_pb3'],
             lambda E: E.tensor_scalar(out=pb[:, 4:6], in0=pb[:, 0:2], scalar1=pb[:, 3:4], scalar2=None, op0=ALU.mult))
        pbk = ['hf_pb', 'hf_pb2', 'hf_pb3']

        def sin_layer(src_ps, srck, bcol, dst, dstk, s2):
            p.op('dve', [srck] + pbk, [('hf_u', s2)],
                 lambda E: E.tensor_scalar(out=u[s2][:, :], in0=src_ps, scalar1=pb[:, 3:4], scalar2=pb[:, bcol:bcol + 1],
                                           op0=ALU.mult, op1=ALU.add))
            p.op('dve', [('hf_u', s2)], [('hf_ui', s2)], lambda E: E.tensor_copy(out=ui[s2][:, :], in_=u[s2][:, :]))
            p.op('dve', [('hf_ui', s2)], [('hf_uf', s2)], lambda E: E.tensor_copy(out=uf[s2][:, :], in_=ui[s2][:, :]))
            p.op('dve', [('hf_u', s2), ('hf_uf', s2)], [('hf_u', s2)],
                 lambda E: E.tensor_tensor(out=u[s2][:, :], in0=u[s2][:, :], in1=uf[s2][:, :], op=ALU.subtract))
            p.op('act', [('hf_u', s2)], [dstk], lambda E: E.activation(out=dst, in_=u[s2][:, :], func=AF.Sin, scale=2 * math.pi))

        nchunk = nt // 4
        first_bwd_tile = 64 if job == 0 else 2
        for ch in range(nchunk):
            s2 = ch % 2
            p.dma('sp', 'hf_ft%d' % s2, [], [('hf_ft', s2)],
                  [(ft[s2][:, :], feat[:, ch * 512:(ch + 1) * 512]), (tv[s2][:, :], tvec[:, ch * 512:(ch + 1) * 512])])
            p.op('pe', [('hf_ft', s2), 'hf_w'], [bk(0)],
                 lambda E, s2=s2: E.matmul(B[0][0:64, :], lhsT=w1[:, :], rhs=ft[s2][:, :], start=True, stop=True))
            sin_layer(B[0][0:64, :], bk(0), 4, h1[s2][:, :], ('hf_h1', s2), s2)
            p.op('pe', [('hf_h1', s2), 'hf_w'], [bk(1)],
                 lambda E, s2=s2: E.matmul(B[1][0:64, :], lhsT=w2[:, :], rhs=h1[s2][:, :], start=True, stop=True))
            sin_layer(B[1][0:64, :], bk(1), 5, h2[s2][:, :], ('hf_h2', s2), s2)
            for j in range(4):
                tile = ch * 4 + j
                d = 0 if tile < first_bwd_tile else 1
                j2 = tile % 2
                cols = slice(j * 128, (j + 1) * 128)
                p.op('pe', [('hf_ft', s2), 'hf_w'], [bk(2)],
                     lambda E, s2=s2, cols=cols: E.matmul(B[2][:, :], lhsT=tv[s2][0:1, cols], rhs=nd[0:1, :], start=True, stop=True))
                p.op('act', [bk(2)], [('hf_dec', j2)], lambda E, j2=j2: E.activation(out=dec[j2][:, :], in_=B[2][:, :], func=AF.Exp))
                for o in range(2):
                    c0 = o * 1024 + d * 512
                    p.op('pe', [('hf_h2', s2), 'hf_w'], [bk(3 + o)],
                         lambda E, s2=s2, cols=cols, c0=c0, o=o: E.matmul(B[3 + o][:, :], lhsT=h2[s2][:, cols], rhs=w3[:, c0:c0 + 512],
                                                                        start=True, stop=True))
                    p.op('dve', [bk(3 + o), ('hf_dec', j2)], [('hf_hd', j2, o)],
                         lambda E, j2=j2, o=o: E.tensor_tensor(out=hd[j2][:, o, :], in0=B[3 + o][:, :], in1=dec[j2][:, :], op=ALU.mult))
                p.op('act', [('hf_hd', j2, 0), ('hf_hd', j2, 1)], [('hf_ha', j2)],
                     lambda E, j2=j2: E.activation(out=ha[j2][:, :, :], in_=hd[j2][:, :, :], func=AF.Abs))
                for o in range(2):
                    p.op('pe', [('hf_ha', j2), 'cst'], [bk(5 + o)],
                         lambda E, j2=j2, o=o, tile=tile: E.matmul(B[5 + o][0:1, :], lhsT=ones[:, 0:1], rhs=ha[j2][:, o, :],
                                                                   start=(tile == 0), stop=(tile == nt - 1)))
                p.op('pool', [('hf_hd', j2, 0), ('hf_hd', j2, 1)], [('hf_hb', j2)],
                     lambda E, j2=j2: E.tensor_copy(out=hb[j2][:, :, :], in_=hd[j2][:, :, :]))
                if tile == first_bwd_tile:
                    p.op('pool', [('hf_hb', j2)], [('hf_hb', j2)], lambda E, j2=j2: E.memset(hb[j2][0:1, :, :], 0.0))
                p.dma('sp', 'hf_hb%d' % j2, [('hf_hb', j2)], [('KTD', job)],
                      [(KTD[tile * 128:(tile + 1) * 128, :].rearrange("p (o c) -> p o c", o=2), hb[j2][:, :, :])])
        for o in range(2):
            p.op('dve', [bk(5 + o)], ['hf_l1'],
                 lambda E, o=o: E.tensor_scalar(out=l1[0:1, o * 512:(o + 1) * 512], in0=B[5 + o][0:1, :], scalar1=float(NFFT if job == 0 else 512), scalar2=None,
                                                op0=ALU.mult))
        p.op('dve', ['hf_l1'], ['hf_l1'], lambda E: E.reciprocal(out=l1[:, :], in_=l1[:, :]))
        p.dma('sp', 'hf_l1', ['hf_l1'], [('SCL', job)], [(c.SCL[job][:, :], l1[:, :])])
        p.barrier()


def hy_fwd1(c, src, K, tab, X1D, NF1):
    p = c.p
    B = c.bank
    bk = lambda i: ('bank', i)
    zt = c.hy_zt
    xo = c.hy_xo
    for bc in range(16):
        s2 = bc % 2
        p.dma('sp', 'hy_zt%d' % s2, ['HYC', 'Z2', ('KTD', 0), ('KTD', 1)], [('hy_zt', s2)], [(zt[s2][0:K, :, :], src(bc * 8, 8))])
        for j in range(8):
            b = bc * 8 + j
            e2 = b % 2
            for ri in range(2):
                p.op('pe', [('hy_zt', s2), 'hy_tab'], [bk(e2 * 2 + ri)],
                     lambda E, s2=s2, j=j, ri=ri, e2=e2: E.matmul(B[e2 * 2 + ri][0:NF1, :], lhsT=tab[0:K, ri, 0:NF1], rhs=zt[s2][0:K, j, :],
                                                                  start=True, stop=True))
            p.op('act', [bk(e2 * 2)], [('hy_xo', e2, 0)],
                 lambda E, e2=e2: E.activation(out=xo[e2][0:NF1, 0, :], in_=B[e2 * 2][0:NF1, :], func=AF.Copy))
            p.op('dve', [bk(e2 * 2 + 1)], [('hy_xo', e2, 1)],
                 lambda E, e2=e2: E.tensor_copy(out=xo[e2][0:NF1, 1, :], in_=B[e2 * 2 + 1][0:NF1, :]))
            p.dma('sp', 'hy_xo%d' % e2, [('hy_xo', e2, 0), ('hy_xo', e2, 1)], ['X1D'],
                  [(X1D[b, 0:NF1, :, :], xo[e2][0:NF1, :, :])])


def hy_stage2(c, X1D, mode, KS, QD, NF1=128, tw2name='hy_tw2', twres=None):
    p = c.p
    I = c.inp
    B = c.bank
    bk = lambda i: ('bank', i)
    xin, tw, ksb, pr, t4, qo = c.hy_xin, c.hy_tw, c.hy_ksb, c.hy_pr, c.hy_t4, c.hy_qo
    E3 = c.hy_E3

    def front(f1):
        s2 = f1 % 2
        p.dma('sp', 'hy_xin%d' % s2, ['X1D'], [('hy_xin', s2)],
              [(xin[s2][:, :, :], X1D[:, f1, :, :])])
        if twres is None:
            p.dma('sp', 'hy_tw%d' % s2, [], [('hy_tw', s2)], [(tw[s2][:, :, :], I[tw2name][f1])])
            twv, twk = tw[s2], ('hy_tw', s2)
        else:
            twv, twk = twres[:, f1, :, :], 'hy_twres'
        if mode != 'filter':
            p.dma('sp', 'hy_ksb%d' % s2, ['KS'], [('hy_ksb', s2)], [(ksb[s2][:, :, :], KS[:, f1, :, :])])
        zr, zi = s2 * 2, s2 * 2 + 1

        def mmz(E):
            E.matmul(B[zr][:, :], lhsT=twv[:, 0, :], rhs=xin[s2][:, 0, :], start=True, stop=False)
            E.matmul(B[zr][:, :], lhsT=twv[:, 2, :], rhs=xin[s2][:, 1, :], start=False, stop=True)
            E.matmul(B[zi][:, :], lhsT=twv[:, 0, :], rhs=xin[s2][:, 1, :], start=True, stop=False)
            return E.matmul(B[zi][:, :], lhsT=twv[:, 1, :], rhs=xin[s2][:, 0, :], start=False, stop=True)
        p.op('pe', [('hy_xin', s2), twk], [bk(zr), bk(zi)], mmz)

    front(0)
    for f1 in range(NF1):
        s2 = f1 % 2
        zr, zi = s2 * 2, s2 * 2 + 1
        if mode == 'filter':
            p.op('act', [bk(zr)], [('hy_pr', s2, 0)], lambda E: E.activation(out=pr[s2][:, 0, :], in_=B[zr][:, :], func=AF.Copy))
            p.op('dve', [bk(zi)], [('hy_pr', s2, 1)], lambda E: E.tensor_copy(out=pr[s2][:, 1, :], in_=B[zi][:, :]))
            if f1 + 1 < NF1:
                front(f1 + 1)
            p.dma('sp', 'hy_pr%d' % s2, [('hy_pr', s2, 0), ('hy_pr', s2, 1)], ['KS'],
                  [(KS[:, f1, :, :], pr[s2][:, :, :])])
            continue
        kk = ('hy_ksb', s2)
        tt = t4[s2]
        p.op('dve', [bk(zr), kk], [('hy_t4', s2, 0)], lambda E: E.tensor_tensor(out=tt[:, 0, :], in0=B[zr][:, :], in1=ksb[s2][:, 0, :], op=ALU.mult))
        p.op('dve', [bk(zi), kk], [('hy_t4', s2, 1)], lambda E: E.tensor_tensor(out=tt[:, 1, :], in0=B[zi][:, :], in1=ksb[s2][:, 1, :], op=ALU.mult))
        p.op('dve', [bk(zr), kk], [('hy_t4', s2, 2)], lambda E: E.tensor_tensor(out=tt[:, 2, :], in0=B[zr][:, :], in1=ksb[s2][:, 1, :], op=ALU.mult))
        p.op('dve', [bk(zi), kk], [('hy_t4', s2, 3)], lambda E: E.tensor_tensor(out=tt[:, 3, :], in0=B[zi][:, :], in1=ksb[s2][:, 0, :], op=ALU.mult))
        if f1 + 1 < NF1:
            front(f1 + 1)
        p.op('pool', [('hy_t4', s2, 0), ('hy_t4', s2, 1)], [('hy_pr', s2, 0)],
             lambda E: E.tensor_tensor(out=pr[s2][:, 0, :], in0=tt[:, 0, :], in1=tt[:, 1, :], op=ALU.subtract))
        p.op('pool', [('hy_t4', s2, 2), ('hy_t4', s2, 3)], [('hy_pr', s2, 1)],
             lambda E: E.tensor_tensor(out=pr[s2][:, 1, :], in0=tt[:, 2, :], in1=tt[:, 3, :], op=ALU.add))

        def mmq(E):
            E.matmul(B[4][:, :], lhsT=E3[:, 0, :], rhs=pr[s2][:, 0, :], start=True, stop=False)
            E.matmul(B[4][:, :], lhsT=E3[:, 2, :], rhs=pr[s2][:, 1, :], start=False, stop=True)
            E.matmul(B[5][:, :], lhsT=E3[:, 1, :], rhs=pr[s2][:, 0, :], start=True, stop=False)
            return E.matmul(B[5][:, :], lhsT=E3[:, 0, :], rhs=pr[s2][:, 1, :], start=False, stop=True)
        p.op('pe', [('hy_pr', s2, 0), ('hy_pr', s2, 1), 'hy_tab'], [bk(4), bk(5)], mmq)
        p.op('act', [bk(4)], [('hy_qo', s2, 0)], lambda E: E.activation(out=qo[s2][:, 0, :], in_=B[4][:, :], func=AF.Copy))
        p.op('act', [bk(5)], [('hy_qo', s2, 1)], lambda E: E.activation(out=qo[s2][:, 1, :], in_=B[5][:, :], func=AF.Copy))
        p.dma('sp', 'hy_qo%d' % s2, [('hy_qo', s2, 0), ('hy_qo', s2, 1)], ['QD'],
              [(QD[:, f1, :, :], qo[s2][:, :, :])])


def hy_final(c, QD, na, scl, bias, zsrc, gsrc, dst, NF1=128, twfname='hy_twf', Mm=64):
    p = c.p
    I = c.inp
    B = c.bank
    bk = lambda i: ('bank', i)
    qin, twf, zg, ya, yb, yo = c.hy_qin, c.hy_twf, c.hy_zg, c.hy_ya, c.hy_yb, c.hy_yo
    def loads(b):
        s2 = b % 2
        p.dma('sp', 'hy_qin%d' % s2, ['QD'], [('hy_qin', s2)], [(qin[s2][0:NF1, :, :], QD[b, 0:NF1, :, :])])
        p.dma('sp', 'hy_twf%d' % s2, [], [('hy_twf', s2)], [(twf[s2][0:NF1, :, 0:Mm], I[twfname][b])])
        p.dma('sp', 'hy_zg%d' % s2, ['HYC', 'Z2'], [('hy_zg', s2)],
              [(zg[s2][0:na, 0, :], zsrc[:, b, :]), (zg[s2][0:na, 1, :], gsrc[:, b, :])])

    loads(0)
    for b in range(128):
        s2 = b % 2
        if b + 1 < 128:
            loads(b + 1)

        def mmy(E, s2=s2):
            E.matmul(B[s2][0:Mm, :], lhsT=twf[s2][0:NF1, 0, 0:Mm], rhs=qin[s2][0:NF1, 0, :], start=True, stop=False)
            return E.matmul(B[s2][0:Mm, :], lhsT=twf[s2][0:NF1, 1, 0:Mm], rhs=qin[s2][0:NF1, 1, :], start=False, stop=True)
        p.op('pe', [('hy_qin', s2), ('hy_twf', s2)], [bk(s2)], mmy)
        p.op('dve', [bk(s2), 'hy_scl'], [('hy_ya', s2)],
             lambda E, s2=s2: E.tensor_tensor(out=ya[s2][0:na, :], in0=B[s2][0:na, :], in1=scl[0:na, :], op=ALU.mult))
        p.op('pool', [('hy_zg', s2), 'hy_scl'], [('hy_yb', s2)],
             lambda E, s2=s2: E.tensor_tensor(out=yb[s2][0:na, :], in0=zg[s2][0:na, 0, :], in1=bias[0:na, :], op=ALU.mult))
        p.op('pool', [('hy_ya', s2), ('hy_yb', s2)], [('hy_ya', s2)],
             lambda E, s2=s2: E.tensor_tensor(out=ya[s2][0:na, :], in0=ya[s2][0:na, :], in1=yb[s2][0:na, :], op=ALU.add))
        p.op('dve', [('hy_ya', s2), ('hy_zg', s2)], [('hy_yo', s2)],
             lambda E, s2=s2: E.tensor_tensor(out=yo[s2][0:na, :], in0=ya[s2][0:na, :], in1=zg[s2][0:na, 1, :], op=ALU.mult))
        p.dma('sp', 'hy_yo%d' % s2, [('hy_yo', s2)], ['Z2', 'OH'], [(dst[:, b, :], yo[s2][0:na, :])])


def stage_hyena(c, l, with_ctx):
    p = c.p
    I = c.inp
    hy_conv3(c, l)
    jobs = [0, 1] if with_ctx else [0]
    hp = c.cfg.get('hy_parts', 9)
    if hp < 1:
        return
    for job in jobs:
        hy_filters(c, l, job)
    if hp < 2:
        return
    with ExitStack() as es:
        c.es = es
        c.hy_zt = [sb(c, 'hy_zt%d' % i, [128, 8, 512], BF16) for i in range(2)]
        c.hy_xo = [sb(c, 'hy_xo%d' % i, [128, 2, 512], BF16) for i in range(2)]
        c.hy_xin = [sb(c, 'hy_xin%d' % i, [128, 2, 512], BF16) for i in range(2)]
        c.hy_tw = [sb(c, 'hy_tw%d' % i, [128, 3, 128], BF16) for i in range(2)]
        c.hy_ksb = [sb(c, 'hy_ksb%d' % i, [128, 2, 512], BF16) for i in range(2)]
        c.hy_pr = [sb(c, 'hy_pr%d' % i, [128, 2, 512], BF16) for i in range(2)]
        c.hy_t4 = [sb(c, 'hy_t4%d' % i, [128, 4, 512], F32) for i in range(2)]
        c.hy_qo = [sb(c, 'hy_qo%d' % i, [128, 2, 512], BF16) for i in range(2)]
        c.hy_qin = [sb(c, 'hy_qin%d' % i, [128, 2, 512], BF16) for i in range(2)]
        c.hy_twf = [sb(c, 'hy_twf%d' % i, [128, 2, 64], BF16) for i in range(2)]
        c.hy_zg = [sb(c, 'hy_zg%d' % i, [64, 2, 512], BF16) for i in range(2)]
        c.hy_ya = [sb(c, 'hy_ya%d' % i, [64, 512], F32) for i in range(2)]
        c.hy_yb = [sb(c, 'hy_yb%d' % i, [64, 512], F32) for i in range(2)]
        c.hy_yo = [sb(c, 'hy_yo%d' % i, [64, 512], BF16) for i in range(2)]
        tabs = sb(c, 'hy_tabs', [128, 2, 2, 128], BF16)
        c.hy_E3 = sb(c, 'hy_E3', [128, 3, 128], BF16)
        scl = sb(c, 'hy_scl', [64, 2, 512], F32)
        bias = sb(c, 'hy_bias', [64, 2, 512], F32)
        p.dma('sp', 'hy_tab', [], ['hy_tab'],
              [(tabs[:, :, :, :], I['hy_dft1'][:, :, :, :]), (c.hy_E3[:, :, :], I['hy_e3'][:, :, :])])
        twres = sb(c, 'hy_twres', [128, 128, 3, 128], BF16)
        p.dma('sp', 'hy_twres', [], ['hy_twres'],
              [(twres[:, g * 16:(g + 1) * 16, :, :], I['hy_tw2'][g * 16:(g + 1) * 16].rearrange("f b k g -> b f k g")) for g in range(8)])
        for job in jobs:
            na = 64 if job == 0 else 2
            tok0 = NCTX if job == 0 else 0
            nt = 128 if job == 0 else 4
            ftab = tabs[:, job, :, :]
            NF1 = 128 if job == 0 else 4
            tw2n = 'hy_tw2' if job == 0 else 'hy_tw2c'
            twfn = 'hy_twf' if job == 0 else 'hy_twfc'
            Mm = 64 if job == 0 else 2
            KTD, KS, X1D, QD = c.KTD[job], c.KS, c.X1D, c.QD
            for o in range(2):
                ksrc = KTD[:, o * 512:(o + 1) * 512].rearrange("(a b) c -> a b c", b=128)
                hy_fwd1(c, lambda b0, nb, ksrc=ksrc: ksrc[:, b0:b0 + nb, :], nt, ftab, X1D, NF1)
                if hp >= 4:
                    hy_stage2(c, X1D, 'filter', KS[o], None, NF1, tw2n, twres if job == 0 else None)
            if hp < 5:
                continue
            p.dma('sp', 'hy_scl', [('SCL', job)], ['hy_scl'],
                  [(scl[:, o, :], c.SCL[job][0, o * 512:(o + 1) * 512].partition_broadcast(64)) for o in range(2)] +
                  [(bias[:, o, :], I['hy_bias'][l, o, :].partition_broadcast(64)) for o in range(2)])
            hyc = c.HYC[tok0:tok0 + na * 128, :].rearrange("(a b) c -> a b c", b=128)
            z2 = c.Z2[tok0:tok0 + na * 128, :].rearrange("(a b) c -> a b c", b=128)
            oh = c.OH[tok0:tok0 + na * 128, :].rearrange("(a b) c -> a b c", b=128)
            vsrc = hyc[:, :, 0:512]
            hy_fwd1(c, lambda b0, nb: vsrc[:, b0:b0 + nb, :], na, ftab, X1D, NF1)
            hy_stage2(c, X1D, 'conv', KS[0], QD, NF1, tw2n, twres if job == 0 else None)
            if hp < 6:
                continue
            hy_final(c, QD, na, scl[:, 0, :], bias[:, 0, :], vsrc, hyc[:, :, 512:1024], z2, NF1, twfn, Mm)
            if hp < 7:
                continue
            hy_fwd1(c, lambda b0, nb: z2[:, b0:b0 + nb, :], na, ftab, X1D, NF1)
            hy_stage2(c, X1D, 'conv', KS[1], QD, NF1, tw2n, twres if job == 0 else None)
            hy_final(c, QD, na, scl[:, 1, :], bias[:, 1, :], z2, hyc[:, :, 1024:1536], oh, NF1, twfn, Mm)
        p.barrier()


def ln_affine_store(c, r, rkey, gb, gbkey, dsts, slot):
    p = c.p
    st = c.e_st[slot]
    junk = c.e_junk
    p.op('act', [rkey], ['e_junk', ('e_st', slot, 0)],
         lambda E: E.activation(out=junk[:, :], in_=r[:, :], func=AF.Copy, accum_out=st[:, 0:1]))
    p.op('dve', [('e_st', slot, 0)], [('e_st', slot, 1)],
         lambda E: E.tensor_scalar(out=st[:, 1:2], in0=st[:, 0:1], scalar1=-1.0 / D, scalar2=None, op0=ALU.mult))
    p.op('act', [rkey, ('e_st', slot, 1)], ['e_junk', ('e_st', slot, 2)],
         lambda E: E.activation(out=junk[:, :], in_=r[:, :], func=AF.Square, bias=st[:, 1:2], scale=1.0, accum_out=st[:, 2:3]))
    p.op('act', [('e_st', slot, 2)], [('e_st', slot, 3)],
         lambda E: E.activation(out=st[:, 3:4], in_=st[:, 2:3], func=AF.Ln, scale=1.0 / D, bias=LN_EPS))
    p.op('act', [('e_st', slot, 3)], [('e_st', slot, 4)],
         lambda E: E.activation(out=st[:, 4:5], in_=st[:, 3:4], func=AF.Exp, scale=-0.5))
    p.op('dve', [rkey, ('e_st', slot, 1), ('e_st', slot, 4)], [rkey],
         lambda E: E.tensor_scalar(out=r[:, :], in0=r[:, :], scalar1=st[:, 1:2], scalar2=st[:, 4:5], op0=ALU.add, op1=ALU.mult))
    p.op('pool', [rkey, gbkey], [rkey], lambda E: E.tensor_tensor(out=r[:, :], in0=r[:, :], in1=gb[:, 0, :], op=ALU.mult))
    p.op('pool', [rkey, gbkey], [rkey], lambda E: E.tensor_tensor(out=r[:, :], in0=r[:, :], in1=gb[:, 1, :], op=ALU.add))
    p.dma('sp', 'e_r%d' % slot, [rkey], ['XOUT'], [(d, r[:, :]) for d in dsts])


def load_bc_rows(c, l, which):
    p = c.p
    I = c.inp
    g = 2 if which == 1 else 5
    lg, lb = ('ln1_g', 'ln1_b') if which == 1 else ('ln2_g', 'ln2_b')
    p.dma('sp', 'e_bc', [('modv', l)], ['e_bc'],
          [(c.e_ag[:, r, :], c.modv[l][r, g * 1024:(g + 1) * 1024].partition_broadcast(128)) for r in range(2)] +
          [(c.e_gb[:, 0, :], I[lg][l, :].partition_broadcast(128)), (c.e_gb[:, 1, :], I[lb][l, :].partition_broadcast(128))])


def stage_merge(c, l, xsrc, tiles):
    p = c.p
    I = c.inp
    S = c.scr[l]
    B = c.bank
    bk = lambda i: ('bank', i)
    with ExitStack() as es:
        c.es = es
        wbr = sb(c, 'mg_wbr', [128, 3, 4, 1024], BF16)
        wout = sb(c, 'mg_wout', [128, 8, 1024], BF16)
        c.e_ag = sb(c, 'e_ag', [128, 2, 1024], F32)
        c.e_gb = sb(c, 'e_gb', [128, 2, 1024], F32)
        c.e_st = [sb(c, 'e_st%d' % i, [128, 8], F32) for i in range(2)]
        c.e_junk = sb(c, 'e_junk', [128, 1024], BF16)
        oT = [sb(c, 'mg_oT%d' % i, [128, 3, 4, 128], BF16) for i in range(2)]
        oh = [sb(c, 'mg_oh%d' % i, [128, 512], BF16) for i in range(2)]
        g3 = [sb(c, 'mg_g3%d' % i, [128, 3072], BF16) for i in range(2)]
        y = [sb(c, 'mg_y%d' % i, [128, 1024], F32) for i in range(2)]
        tt = [sb(c, 'mg_t%d' % i, [128, 1024], F32) for i in range(2)]
        yb = [sb(c, 'mg_yb%d' % i, [128, 1024], BF16) for i in range(2)]
        yT = [sb(c, 'mg_yT%d' % i, [128, 8, 128], BF16) for i in range(2)]
        xt = [sb(c, 'mg_xt%d' % i, [128, 1024], F32) for i in range(2)]
        for br, nm in enumerate(['w_br_gla', 'w_br_mla', 'w_br_hy']):
            p.dma('pool', 'mg_w', [], ['mg_w'], [(wbr[:, br, :, :], I[nm][l].rearrange("(k p) n -> p k n", p=128))])
        p.dma('pool', 'mg_w', [], ['mg_w'], [(wout[:, :, :], I['w_out'][l].rearrange("(k p) n -> p k n", p=128))])
        load_bc_rows(c, l, 1)
        def mloads(i):
            t = tiles[i]
            s2 = i % 2
            tok = slice(t * 128, (t + 1) * 128)
            p.dma('sp', 'mg_oT%d' % s2, [('OGT', l), ('OMT', l)], [('mg_oT', s2)],
                  [(oT[s2][:, 0, :, :], S['OGT'][:, tok].rearrange("(k p) t -> p k t", p=128)),
                   (oT[s2][:, 1, :, :], S['OMT'][:, tok].rearrange("(k p) t -> p k t", p=128))])
            p.dma('sp', 'mg_oh%d' % s2, ['OH'], [('mg_oh', s2)], [(oh[s2][:, :], c.OH[tok, :])])
            p.dma('sp', 'mg_g3%d' % s2, [('G3', l)], [('mg_g3', s2)], [(g3[s2][:, :], S['G3'][tok, :])])
            p.dma('sp', 'mg_xt%d' % s2, ['XOUT'], [('mg_xt', s2)], [(xt[s2][:, :], xsrc(t))])

        mloads(0)
        for i, t in enumerate(tiles):
            s2 = i % 2
            r = 1 if t < 2 else 0
            tok = slice(t * 128, (t + 1) * 128)
            if i + 1 < len(tiles):
                mloads(i + 1)
            tpv = bank16(c, 6)

            def tro(E):
                ins = None
                for k in range(4):
                    ins = E.transpose(tpv[:, k, :], oh[s2][:, k * 128:(k + 1) * 128], c.ident[:, :])
                return ins
            p.op('pe', [('mg_oh', s2), 'ident'], [bk(6)], tro)
            p.op('act', [bk(6)], [('mg_oT', s2)], lambda E: E.activation(out=oT[s2][:, 2, :, :], in_=tpv[:, 0:4, :], func=AF.Copy))
            for br in range(3):
                for half in range(2):
                    bb = (br * 2 + half) % 4

                    def mmb(E):
                        ins = None
                        for k in range(4):
                            ins = E.matmul(B[bb][:, :], lhsT=oT[s2][:, br, k, :], rhs=wbr[:, br, k, half * 512:(half + 1) * 512],
                                           start=(k == 0), stop=(k == 3))
                        return ins
                    p.op('pe', [('mg_oT', s2), 'mg_w'], [bk(bb)], mmb)
                    hs = slice(half * 512, (half + 1) * 512)
                    gs = slice(br * 1024 + half * 512, br * 1024 + (half + 1) * 512)
                    if br == 0:
                        p.op('dve', [bk(bb), ('mg_g3', s2)], [('mg_y', s2, half)],
                             lambda E: E.tensor_tensor(out=y[s2][:, hs], in0=B[bb][:, :], in1=g3[s2][:, gs], op=ALU.mult))
                    else:
                        p.op('dve', [bk(bb), ('mg_g3', s2)], [('mg_t', s2, half)],
                             lambda E: E.tensor_tensor(out=tt[s2][:, hs], in0=B[bb][:, :], in1=g3[s2][:, gs], op=ALU.mult))
                        p.op('pool', [('mg_t', s2, half), ('mg_y', s2, half)], [('mg_y', s2, half)],
                             lambda E: E.tensor_tensor(out=y[s2][:, hs], in0=y[s2][:, hs], in1=tt[s2][:, hs], op=ALU.add))
            p.op('act', [('mg_y', s2, 0), ('mg_y', s2, 1)], [('mg_yb', s2)],
                 lambda E: E.activation(out=yb[s2][:, :], in_=y[s2][:, :], func=AF.Copy))
            tp7 = bank16(c, 7)

            def try_(E):
                ins = None
                for k in range(8):
                    ins = E.transpose(tp7[:, k, :], yb[s2][:, k * 128:(k + 1) * 128], c.ident[:, :])
                return ins
            p.op('pe', [('mg_yb', s2), 'ident'], [bk(7)], try_)
            p.op('act', [bk(7)], [('mg_yT', s2)], lambda E: E.activation(out=yT[s2][:, :, :], in_=tp7[:, :, :], func=AF.Copy))
            for half in range(2):
                bb = 4 + half

                def mmo(E):
                    ins = None
                    for k in range(8):
                        ins = E.matmul(B[bb][:, :], lhsT=yT[s2][:, k, :], rhs=wout[:, k, half * 512:(half + 1) * 512],
                                       start=(k == 0), stop=(k == 7))
                    return ins
                p.op('pe', [('mg_yT', s2), 'mg_w'], [bk(bb)], mmo)
                hs = slice(half * 512, (half + 1) * 512)
                p.op('dve', [bk(bb), 'e_bc'], [('mg_t', s2, half)],
                     lambda E: E.tensor_tensor(out=tt[s2][:, hs], in0=B[bb][:, :], in1=c.e_ag[:, r, hs], op=ALU.mult))
                p.op('dve', [('mg_t', s2, half), ('mg_xt', s2)], [('mg_xt', s2)],
                     lambda E: E.scalar_tensor_tensor(out=xt[s2][:, hs], in0=xt[s2][:, hs], scalar=ALPHA, in1=tt[s2][:, hs],
                                                      op0=ALU.mult, op1=ALU.add))
            ln_affine_store(c, xt[s2], ('mg_xt', s2), c.e_gb, 'e_bc', [c.X1[tok, :]], s2)
        p.barrier()


def moe_precast(c, l):
    p = c.p
    I = c.inp
    for e in range(32):
        if c.cfg.get('moe_mode', 'sparse') == 'sparse':
            p.dma('pool', 'wcast', [], [('WB', l, e)],
                  [(c.WB1[l][e].rearrange("(p k) n -> p k n", k=8), I['moe_w1'][l, e].rearrange("(k p) n -> p k n", p=128)),
                   (c.WB2[l][e].rearrange("(p k) n -> p k n", k=8), I['moe_w2'][l, e].rearrange("(k p) n -> p k n", p=128))])
        else:
            p.dma('pool', 'wcast', [], [('WB', l, e)],
                  [(c.WB1[l][e], I['moe_w1'][l, e]), (c.WB2[l][e], I['moe_w2'][l, e])])


def stage_moe(c, l, tiles, dst_fn):
    p = c.p
    I = c.inp
    B = c.bank
    bk = lambda i: ('bank', i)
    cst = c.cst
    with ExitStack() as es:
        c.es = es
        c.e_ag = sb(c, 'e_ag', [128, 2, 1024], F32)
        c.e_gb = sb(c, 'e_gb', [128, 2, 1024], F32)
        c.e_st = [sb(c, 'e_st%d' % i, [128, 8], F32) for i in range(2)]
        c.e_junk = sb(c, 'e_junk', [128, 1024], BF16)
        w1 = [sb(c, 'mo_w1%d' % i, [128, 8, 2048], BF16) for i in range(2)]
        w2 = [sb(c, 'mo_w2%d' % i, [128, 8, 1024], BF16) for i in range(1)] * 2
        hT = sb(c, 'mo_hT', [128, 8, 1024], BF16)
        aT = sb(c, 'mo_aT', [128, 8, 1024], BF16)
        yacc = sb(c, 'mo_yacc', [128, 8, 1024], F32)
        rw = sb(c, 'mo_rw', [128, 8, 32], F32)
        rb = sb(c, 'mo_rb', [1, 32], F32)
        b1 = sb(c, 'mo_b1', [128, 32, 16], F32)
        b2 = sb(c, 'mo_b2', [32, 1024], F32)
        G = sb(c, 'mo_G', [128, 8, 32], F32)
        GT = sb(c, 'mo_GT', [32, 8, 128], F32)
        xt = [sb(c, 'mo_xt%d' % i, [128, 1024], F32) for i in range(2)]
        xh = [sb(c, 'mo_xh%d' % i, [128, 1024], F32) for i in range(1)] * 2
        h32 = [sb(c, 'mo_h32%d' % i, [128, 8, 128], F32) for i in range(1)] * 2
        lg = [sb(c, 'mo_lg%d' % i, [128, 32], F32) for i in range(2)]
        mx = [sb(c, 'mo_mx%d' % i, [128, 8], F32) for i in range(2)]
        ex = [sb(c, 'mo_ex%d' % i, [128, 32], F32) for i in range(2)]
        sm = [sb(c, 'mo_sm%d' % i, [128, 2], F32) for i in range(2)]
        gg = [sb(c, 'mo_gg%d' % i, [128, 512], F32) for i in range(2)]
        sg = [sb(c, 'mo_sg%d' % i, [128, 512], F32) for i in range(2)]
        ll = [sb(c, 'mo_ll%d' % i, [128, 512], F32) for i in range(2)]
        st = [sb(c, 'mo_st%d' % i, [128, 8], F32) for i in range(2)]
        p.dma('sp', 'mo_c', [], ['mo_c'],
              [(rw[:, :, :], I['router_w'][l].rearrange("(k p) e -> p k e", p=128)),
               (rb[:, :], I['router_b'][l:l + 1, :]), (b2[:, :], I['moe_b2'][l])])
        p.dma('sp', 'mo_b1', [], ['mo_b1'],
              [(b1[:, e, :], I['moe_b1'][l, e, :].rearrange("(j p) -> p j", p=128)) for e in range(32)], slow=True)
        load_bc_rows(c, l, 2)
        ones = cst[:, 640:768]
        ident32 = cst[:, 0:128]
        groups = [tiles[i:i + 8] for i in range(0, len(tiles), 8)][:c.cfg.get('moe_groups', 99)]
        wi = 0
        for gi, gt in enumerate(groups):
            ng = len(gt)
            T = ng * 128
            for j, t in enumerate(gt):
                s2 = j % 2
                r = 1 if t < 2 else 0
                tok = slice(t * 128, (t + 1) * 128)
                p.dma('sp', 'mo_xt%d' % s2, ['XOUT'], [('mo_xt', s2)], [(xt[s2][:, :], c.X1[tok, :])])
                s_ = st[s2]
                p.op('act', [('mo_xt', s2)], ['e_junk', ('mo_st', s2, 0)],
                     lambda E: E.activation(out=c.e_junk[:, :], in_=xt[s2][:, :], func=AF.Copy, accum_out=s_[:, 0:1]))
                p.op('dve', [('mo_st', s2, 0)], [('mo_st', s2, 1)],
                     lambda E: E.tensor_scalar(out=s_[:, 1:2], in0=s_[:, 0:1], scalar1=-1.0 / D, scalar2=None, op0=ALU.mult))
                p.op('act', [('mo_xt', s2), ('mo_st', s2, 1)], ['e_junk', ('mo_st', s2, 2)],
                     lambda E: E.activation(out=c.e_junk[:, :], in_=xt[s2][:, :], func=AF.Square, bias=s_[:, 1:2], scale=1.0,
                                            accum_out=s_[:, 2:3]))
                p.op('act', [('mo_st', s2, 2)], [('mo_st', s2, 3)],
                     lambda E: E.activation(out=s_[:, 3:4], in_=s_[:, 2:3], func=AF.Ln, scale=1.0 / D, bias=LN_EPS))
                p.op('act', [('mo_st', s2, 3)], [('mo_st', s2, 4)],
                     lambda E: E.activation(out=s_[:, 4:5], in_=s_[:, 3:4], func=AF.Exp, scale=-0.5))
                p.op('dve', [('mo_xt', s2), ('mo_st', s2, 1), ('mo_st', s2, 4)], [('mo_xh', 0)],
                     lambda E: E.tensor_scalar(out=xh[s2][:, :], in0=xt[s2][:, :], scalar1=s_[:, 1:2], scalar2=s_[:, 4:5],
                                               op0=ALU.add, op1=ALU.mult))
                for hh in range(2):
                    bb = hh
                    def tr32(E):
                        ins = None
                        for k in range(4):
                            kk = hh * 4 + k
                            ins = E.transpose(B[bb][:, k * 128:(k + 1) * 128], xh[s2][:, kk * 128:(kk + 1) * 128], ident32)
                        return ins
                    p.op('pe', [('mo_xh', 0), 'cst'], [bk(bb)], tr32)
                    for k in range(4):
                        kk = hh * 4 + k
                        p.op('dve', [bk(bb), 'modT'], [('mo_h32', 0, kk)],
                             lambda E: E.tensor_scalar(out=h32[s2][:, kk, :], in0=B[bb][:, k * 128:(k + 1) * 128],
                                                       scalar1=c.modT[:, r, 4, kk:kk + 1], scalar2=c.modT[:, r, 3, kk:kk + 1],
                                                       op0=ALU.mult, op1=ALU.add))
                hk = [('mo_h32', 0, kk) for kk in range(8)]
                p.op('pool', hk, [('mo_hT', j)], lambda E: E.tensor_copy(out=hT[:, :, j * 128:(j + 1) * 128], in_=h32[s2][:, :, :]))

                def mml(E):
                    for kk in range(8):
                        E.matmul(B[2][:, 0:32], lhsT=h32[s2][:, kk, :], rhs=rw[:, kk, :], start=(kk == 0), stop=False)
                    return E.matmul(B[2][:, 0:32], lhsT=ones[0:1, :], rhs=rb[0:1, :], start=False, stop=True)
                p.op('pe', hk + ['mo_c', 'cst'], [bk(2)], mml)
                p.op('dve', [bk(2)], [('mo_lg', s2)], lambda E: E.tensor_copy(out=lg[s2][:, :], in_=B[2][:, 0:32]))
                p.op('dve', [('mo_lg', s2)], [('mo_mx', s2)], lambda E: E.max(out=mx[s2][:, :], in_=lg[s2][:, :]))
                p.op('dve', [('mo_mx', s2)], [('mo_sm', s2, 0)],
                     lambda E: E.tensor_scalar(out=sm[s2][:, 0:1], in0=mx[s2][:, 0:1], scalar1=-1.0, scalar2=None, op0=ALU.mult))
                p.op('act', [('mo_lg', s2), ('mo_sm', s2, 0)], [('mo_ex', s2)],
                     lambda E: E.activation(out=ex[s2][:, :], in_=lg[s2][:, :], func=AF.Exp, bias=sm[s2][:, 0:1], scale=1.0))
                p.op('dve', [('mo_lg', s2), ('mo_mx', s2)], [('mo_lg', s2)],
                     lambda E: E.tensor_scalar(out=lg[s2][:, :], in0=lg[s2][:, :], scalar1=mx[s2][:, 3:4], scalar2=None, op0=ALU.is_ge))
                p.op('dve', [('mo_lg', s2), ('mo_ex', s2)], [('mo_ex', s2)],
                     lambda E: E.tensor_tensor(out=ex[s2][:, :], in0=ex[s2][:, :], in1=lg[s2][:, :], op=ALU.mult))
                p.op('dve', [('mo_ex', s2)], [('mo_sm', s2, 1)],
                     lambda E: E.tensor_reduce(out=sm[s2][:, 1:2], in_=ex[s2][:, :], axis=AX.X, op=ALU.add))
                p.op('dve', [('mo_sm', s2, 1)], [('mo_sm', s2, 1)], lambda E: E.reciprocal(out=sm[s2][:, 1:2], in_=sm[s2][:, 1:2]))
                p.op('dve', [('mo_ex', s2), ('mo_sm', s2, 1)], [('mo_G', j)],
                     lambda E: E.tensor_scalar(out=G[:, j, :], in0=ex[s2][:, :], scalar1=sm[s2][:, 1:2], scalar2=None, op0=ALU.mult))
                p.op('pe', [('mo_G', j), 'cst'], [bk(3)], lambda E: E.transpose(B[3][0:32, 0:128], G[:, j, :], ident32))
                p.op('act', [bk(3)], [('mo_GT', j)], lambda E: E.activation(out=GT[:, j, :], in_=B[3][0:32, 0:128], func=AF.Copy))
                for half in range(2):
                    p.op('pe', [('mo_GT', j), 'mo_c'], [bk(4 + half)],
                         lambda E: E.matmul(B[4 + half][:, :], lhsT=GT[:, j, :], rhs=b2[:, half * 512:(half + 1) * 512], start=True, stop=True))
                    p.op('act', [bk(4 + half)], [('mo_yacc', j, half)],
                         lambda E: E.activation(out=yacc[:, j, half * 512:(half + 1) * 512], in_=B[4 + half][:, :], func=AF.Copy))
            hTk = [('mo_hT', j) for j in range(ng)]
            ei = 0
            for e in range(32):
                ws = wi % 2
                wi += 1
                p.dma('sp', 'mo_w1%d' % ws, [('WB', l, e)], [('mo_w1', ws)],
                      [(w1[ws][:, :, :], c.WB1[l][e].rearrange("(k p) n -> p k n", p=128))])
                p.dma('sp', 'mo_w2', [('WB', l, e)], [('mo_w2', 0)],
                      [(w2[ws][:, :, :], c.WB2[l][e].rearrange("(k p) n -> p k n", p=128))])
                for th in range((T + 511) // 512):
                    c0 = th * 512
                    n = min(512, T - c0)
                    for fc in range(8):
                        s2 = ei % 2
                        ei += 1
                        bg, bl = s2 * 2, s2 * 2 + 1

                        def mm1(E):
                            ins = None
                            for k in range(8):
                                E.matmul(B[bg][:, 0:n], lhsT=w1[ws][:, k, fc * 128:(fc + 1) * 128], rhs=hT[:, k, c0:c0 + n],
                                         start=(k == 0), stop=(k == 7))
                            for k in range(8):
                                ins = E.matmul(B[bl][:, 0:n], lhsT=w1[ws][:, k, 1024 + fc * 128:1024 + (fc + 1) * 128],
                                               rhs=hT[:, k, c0:c0 + n], start=(k == 0), stop=(k == 7))
                            return ins
                        p.op('pe', hTk + [('mo_w1', ws)], [bk(bg), bk(bl)], mm1)
                        p.op('dve', [bk(bg), 'mo_b1'], [('mo_gg', s2)],
                             lambda E: E.tensor_scalar(out=gg[s2][:, 0:n], in0=B[bg][:, 0:n], scalar1=b1[:, e, fc:fc + 1], scalar2=7.0,
                                                       op0=ALU.add, op1=ALU.min))
                        p.op('act', [('mo_gg', s2)], [('mo_sg', s2)],
                             lambda E: E.activation(out=sg[s2][:, 0:n], in_=gg[s2][:, 0:n], func=AF.Sigmoid, scale=1.702))
                        p.op('dve', [bk(bl), 'mo_b1'], [('mo_ll', s2)],
                             lambda E: E.tensor_scalar(out=ll[s2][:, 0:n], in0=B[bl][:, 0:n], scalar1=b1[:, e, 8 + fc:9 + fc], scalar2=7.0,
                                                       op0=ALU.add, op1=ALU.min))
                        p.op('pool', [('mo_ll', s2)], [('mo_ll', s2)],
                             lambda E: E.tensor_scalar(out=ll[s2][:, 0:n], in0=ll[s2][:, 0:n], scalar1=-7.0, scalar2=1.0,
                                                       op0=ALU.max, op1=ALU.add))
                        p.op('pool', [('mo_gg', s2), ('mo_sg', s2)], [('mo_gg', s2)],
                             lambda E: E.tensor_tensor(out=gg[s2][:, 0:n], in0=gg[s2][:, 0:n], in1=sg[s2][:, 0:n], op=ALU.mult))
                        p.op('dve', [('mo_gg', s2), ('mo_ll', s2)], [('mo_aT', fc, th)],
                             lambda E: E.tensor_tensor(out=aT[:, fc, c0:c0 + n], in0=gg[s2][:, 0:n], in1=ll[s2][:, 0:n], op=ALU.mult))
                aTk = [('mo_aT', fc, th) for fc in range(8) for th in range((T + 511) // 512)]
                for j in range(ng):
                    for half in range(2):
                        bb = 4 + (j * 2 + half) % 4

                        def mm2(E):
                            ins = None
                            for k in range(8):
                                ins = E.matmul(B[bb][:, :], lhsT=aT[:, k, j * 128:(j + 1) * 128], rhs=w2[ws][:, k, half * 512:(half + 1) * 512],
                                               start=(k == 0), stop=(k == 7))
                            return ins
                        p.op('pe', aTk + [('mo_w2', 0)], [bk(bb)], mm2)
                        hs = slice(half * 512, (half + 1) * 512)
                        p.op('dve', [bk(bb), ('mo_G', j), ('mo_yacc', j, half)], [('mo_yacc', j, half)],
                             lambda E: E.scalar_tensor_tensor(out=yacc[:, j, hs], in0=B[bb][:, :], scalar=G[:, j, e:e + 1], in1=yacc[:, j, hs],
                                                              op0=ALU.mult, op1=ALU.add))
            for j, t in enumerate(gt):
                s2 = j % 2
                r = 1 if t < 2 else 0
                tok = slice(t * 128, (t + 1) * 128)
                p.dma('sp', 'mo_xt%d' % s2, ['XOUT'], [('mo_xt', s2)], [(xt[s2][:, :], c.X1[tok, :])])
                yk = [('mo_yacc', j, 0), ('mo_yacc', j, 1)]
                p.op('pool', yk + ['e_bc'], yk, lambda E: E.tensor_tensor(out=yacc[:, j, :], in0=yacc[:, j, :], in1=c.e_ag[:, r, :], op=ALU.mult))
                p.op('dve', yk + [('mo_xt', s2)], [('mo_xt', s2)],
                     lambda E: E.scalar_tensor_tensor(out=xt[s2][:, :], in0=xt[s2][:, :], scalar=ALPHA, in1=yacc[:, j, :],
                                                      op0=ALU.mult, op1=ALU.add))
                ln_affine_store(c, xt[s2], ('mo_xt', s2), c.e_gb, 'e_bc', dst_fn(t), s2)
        p.barrier()


def stage_moe2(c, l, tiles, dst_fn):
    p = c.p
    I = c.inp
    B = c.bank
    bk = lambda i: ('bank', i)
    cst = c.cst
    ones = cst[:, 640:768]
    ident32 = cst[:, 0:128]
    iota_f = cst[:, 768:800]
    iota_p = cst[:, 800:801]
    blk512 = cst[:, 896:1024]
    ntl = len(tiles)
    NB = (ntl * 128 * 4) // 512 + 32
    XB, YB, HB = c.XB, c.YB, c.HB
    W1f = c.WB1[l].rearrange("e (p k) n -> (e p) (k n)", k=8)
    W2f = c.WB2[l].rearrange("e (p k) n -> (e p) (k n)", k=8)
    I32 = mybir.dt.int32
    with ExitStack() as es_outer:
        c.es = es_outer
        slot_i = sb(c, 'ms_slot', [128, NTILE, 4], I32)
        gk = sb(c, 'ms_gk', [128, NTILE, 4], F32)
        idxw = sb(c, 'ms_idxw', [128, NB, 8], I32)
        be_bc = sb(c, 'ms_bebc', [128, NB], F32)
        OHall = sb(c, 'ms_oh', [32, NB], F32)
        c.e_ag = sb(c, 'e_ag', [128, 2, 1024], F32)
        c.e_gb = sb(c, 'e_gb', [128, 2, 1024], F32)
        c.e_st = [sb(c, 'e_st%d' % i, [128, 8], F32) for i in range(2)]
        c.e_junk = sb(c, 'e_junk', [128, 1024], BF16)
        load_bc_rows(c, l, 2)
        with ExitStack() as es:
            c.es = es
            Mall = sb(c, 'ms_M', [128, NTILE, 32], F32)
            Gall = sb(c, 'ms_G', [128, NTILE, 32], F32)
            Lall = sb(c, 'ms_L', [128, NTILE, 32], F32)
            mxall = sb(c, 'ms_mx', [128, NTILE, 8], F32)
            rw = sb(c, 'ms_rw', [128, 8, 32], F32)
            rb = sb(c, 'ms_rb', [1, 32], F32)
            modbc = sb(c, 'ms_modbc', [128, 2, 2, 1024], F32)
            xt = [sb(c, 'ms_xt%d' % i, [128, 1024], F32) for i in range(2)]
            xh = sb(c, 'ms_xh', [128, 1024], F32)
            hb = [sb(c, 'ms_hb%d' % i, [128, 1024], BF16) for i in range(2)]
            h32 = sb(c, 'ms_h32', [128, 8, 128], F32)
            ex = [sb(c, 'ms_ex%d' % i, [128, 32], F32) for i in range(2)]
            sm = [sb(c, 'ms_sm%d' % i, [128, 2], F32) for i in range(2)]
            st = [sb(c, 'ms_st%d' % i, [128, 8], F32) for i in range(2)]
            Lst = sb(c, 'ms_Lst', [128, 128], F32)
            Msum = sb(c, 'ms_Msum', [128, 32], F32)
            cnt = sb(c, 'ms_cnt', [1, 32], F32)
            cnti = sb(c, 'ms_cnti', [1, 32], I32)
            pcol = sb(c, 'ms_pcol', [32, 4], F32)
            psbc = sb(c, 'ms_psbc', [128, 32], F32)
            cmp_ = sb(c, 'ms_cmp', [32, 128], F32)
            berow = sb(c, 'ms_berow', [1, 128], F32)
            tmpf = sb(c, 'ms_tmpf', [128, 128], F32)
            pos = [sb(c, 'ms_pos%d' % i, [128, 32], F32) for i in range(2)]
            oh = [sb(c, 'ms_ohk%d' % i, [128, 32], F32) for i in range(2)]
            tq = [sb(c, 'ms_tq%d' % i, [128, 32], F32) for i in range(2)]
            sl = [sb(c, 'ms_sl%d' % i, [128, 4], F32) for i in range(2)]
            p.dma('sp', 'ms_c', [], ['ms_c'],
                  [(rw[:, :, :], I['router_w'][l].rearrange("(k p) e -> p k e", p=128)), (rb[:, :], I['router_b'][l:l + 1, :])])
            p.dma('sp', 'ms_modbc', [('modv', l)], ['ms_modbc'],
                  [(modbc[:, r, q, :], c.modv[l][r, (4 - q) * 1024:(5 - q) * 1024].partition_broadcast(128))
                   for r in range(2) for q in range(2)])
            for r in range(2):
                p.op('dve', ['ms_modbc'], ['ms_modbc'],
                     lambda E: E.tensor_scalar(out=modbc[:, r, 0, :], in0=modbc[:, r, 0, :], scalar1=1.0, scalar2=None, op0=ALU.add))
            p.op('dve', ['cst'], ['ms_Lst'], lambda E: E.tensor_tensor(out=Lst[:, :], in0=cst[:, 384:512], in1=ident32, op=ALU.subtract))
            for jj, t in enumerate(tiles):
                s2 = jj % 2
                r = 1 if t < 2 else 0
                tok = slice(t * 128, (t + 1) * 128)
                p.dma('sp', 'ms_xt%d' % s2, ['XOUT'], [('ms_xt', s2)], [(xt[s2][:, :], c.X1[tok, :])])
                s_ = st[s2]
                p.op('act', [('ms_xt', s2)], ['e_junk', ('ms_st', s2, 0)],
                     lambda E: E.activation(out=c.e_junk[:, :], in_=xt[s2][:, :], func=AF.Copy, accum_out=s_[:, 0:1]))
                p.op('dve', [('ms_st', s2, 0)], [('ms_st', s2, 1)],
                     lambda E: E.tensor_scalar(out=s_[:, 1:2], in0=s_[:, 0:1], scalar1=-1.0 / D, scalar2=None, op0=ALU.mult))
                p.op('act', [('ms_xt', s2), ('ms_st', s2, 1)], ['e_junk', ('ms_st', s2, 2)],
                     lambda E: E.activation(out=c.e_junk[:, :], in_=xt[s2][:, :], func=AF.Square, bias=s_[:, 1:2], scale=1.0,
                                            accum_out=s_[:, 2:3]))
                p.op('act', [('ms_st', s2, 2)], [('ms_st', s2, 3)],
                     lambda E: E.activation(out=s_[:, 3:4], in_=s_[:, 2:3], func=AF.Ln, scale=1.0 / D, bias=LN_EPS))
                p.op('act', [('ms_st', s2, 3)], [('ms_st', s2, 4)],
                     lambda E: E.activation(out=s_[:, 4:5], in_=s_[:, 3:4], func=AF.Exp, scale=-0.5))
                p.op('dve', [('ms_xt', s2), ('ms_st', s2, 1), ('ms_st', s2, 4)], ['ms_xh'],
                     lambda E: E.tensor_scalar(out=xh[:, :], in0=xt[s2][:, :], scalar1=s_[:, 1:2], scalar2=s_[:, 4:5],
                                               op0=ALU.add, op1=ALU.mult))
                p.op('pool', ['ms_xh', 'ms_modbc'], [('ms_xt', s2)],
                     lambda E: E.tensor_tensor(out=xt[s2][:, :], in0=xh[:, :], in1=modbc[:, r, 0, :], op=ALU.mult))
                p.op('pool', [('ms_xt', s2), 'ms_modbc'], [('ms_hb', s2)],
                     lambda E: E.tensor_tensor(out=hb[s2][:, :], in0=xt[s2][:, :], in1=modbc[:, r, 1, :], op=ALU.add))
                p.dma('sp', 'ms_hb%d' % s2, [('ms_hb', s2)], [('HB', t)], [(HB[tok, :], hb[s2][:, :])])
                for hh in range(2):
                    def tr32(E):
                        ins = None
                        for k in range(4):
                            kk = hh * 4 + k
                            ins = E.transpose(B[hh][:, k * 128:(k + 1) * 128], xh[:, kk * 128:(kk + 1) * 128], ident32)
                        return ins
                    p.op('pe', ['ms_xh', 'cst'], [bk(hh)], tr32)
                    for k in range(4):
                        kk = hh * 4 + k
                        p.op('dve', [bk(hh), 'modT'], [('ms_h32', kk)],
                             lambda E: E.tensor_scalar(out=h32[:, kk, :], in0=B[hh][:, k * 128:(k + 1) * 128],
                                                       scalar1=c.modT[:, r, 4, kk:kk + 1], scalar2=c.modT[:, r, 3, kk:kk + 1],
                                                       op0=ALU.mult, op1=ALU.add))
                hk = [('ms_h32', kk) for kk in range(8)]

                def mml(E):
                    for kk in range(8):
                        E.matmul(B[2][:, 0:32], lhsT=h32[:, kk, :], rhs=rw[:, kk, :], start=(kk == 0), stop=False)
                    return E.matmul(B[2][:, 0:32], lhsT=ones[0:1, :], rhs=rb[0:1, :], start=False, stop=True)
                p.op('pe', hk + ['ms_c', 'cst'], [bk(2)], mml)
                lgj, mxj, Mj, Gj = Lall[:, t, :], mxall[:, t, :], Mall[:, t, :], Gall[:, t, :]
                p.op('dve', [bk(2)], [('ms_L', t)], lambda E: E.tensor_copy(out=lgj, in_=B[2][:, 0:32]))
                p.op('dve', [('ms_L', t)], [('ms_mx', t)], lambda E: E.max(out=mxj, in_=lgj))
                p.op('dve', [('ms_mx', t)], [('ms_sm', s2, 0)],
                     lambda E: E.tensor_scalar(out=sm[s2][:, 0:1], in0=mxall[:, t, 0:1], scalar1=-1.0, scalar2=None, op0=ALU.mult))
                p.op('act', [('ms_L', t), ('ms_sm', s2, 0)], [('ms_ex', s2)],
                     lambda E: E.activation(out=ex[s2][:, :], in_=lgj, func=AF.Exp, bias=sm[s2][:, 0:1], scale=1.0))
                p.op('dve', [('ms_L', t), ('ms_mx', t)], [('ms_M', t)],
                     lambda E: E.tensor_scalar(out=Mj, in0=lgj, scalar1=mxall[:, t, 3:4], scalar2=None, op0=ALU.is_ge))
                p.op('dve', [('ms_M', t), ('ms_ex', s2)], [('ms_ex', s2)],
                     lambda E: E.tensor_tensor(out=ex[s2][:, :], in0=ex[s2][:, :], in1=Mj, op=ALU.mult))
                p.op('dve', [('ms_ex', s2)], [('ms_sm', s2, 1)],
                     lambda E: E.tensor_reduce(out=sm[s2][:, 1:2], in_=ex[s2][:, :], axis=AX.X, op=ALU.add))
                p.op('dve', [('ms_sm', s2, 1)], [('ms_sm', s2, 1)], lambda E: E.reciprocal(out=sm[s2][:, 1:2], in_=sm[s2][:, 1:2]))
                p.op('dve', [('ms_ex', s2), ('ms_sm', s2, 1)], [('ms_G', t)],
                     lambda E: E.tensor_scalar(out=Gj, in0=ex[s2][:, :], scalar1=sm[s2][:, 1:2], scalar2=None, op0=ALU.mult))
                p.op('pe', [('ms_M', t), 'cst'], [bk(7)],
                     lambda E: E.matmul(B[7][0:1, 0:32], lhsT=ones[:, 0:1], rhs=Mj, start=(jj == 0), stop=(jj == ntl - 1)))
            p.op('dve', [bk(7)], ['ms_cnt'], lambda E: E.tensor_scalar(out=cnt[:, :], in0=B[7][0:1, 0:32], scalar1=1.0 / 512, scalar2=255.5 / 512,
                                                                       op0=ALU.mult, op1=ALU.add))
            p.op('dve', ['ms_cnt'], ['ms_cnti'], lambda E: E.tensor_copy(out=cnti[:, :], in_=cnt[:, :]))
            p.op('dve', ['ms_cnti'], ['ms_cnt'], lambda E: E.tensor_copy(out=cnt[:, :], in_=cnti[:, :]))
            p.op('dve', ['ms_cnt'], ['ms_cnt'], lambda E: E.tensor_scalar(out=cnt[:, :], in0=cnt[:, :], scalar1=512.0, scalar2=None, op0=ALU.mult))
            p.op('pe', ['ms_cnt', 'cst'], [bk(0)], lambda E: E.transpose(B[0][0:32, 0:1], cnt[0:1, :], ident32[0:1, 0:1]))
            p.op('dve', [bk(0)], ['ms_pcol'], lambda E: E.tensor_copy(out=pcol[:, 0:1], in_=B[0][0:32, 0:1]))
            p.op('pe', ['ms_pcol', 'ms_Lst'], [bk(1)],
                 lambda E: E.matmul(B[1][0:1, 0:32], lhsT=pcol[:, 0:1], rhs=Lst[0:32, 0:32], start=True, stop=True))
            p.op('dve', [bk(1)], ['ms_cnt'], lambda E: E.tensor_copy(out=cnt[:, :], in_=B[1][0:1, 0:32]))
            p.op('pe', ['ms_cnt', 'cst'], [bk(1)],
                 lambda E: E.matmul(B[1][:, 0:32], lhsT=ones[0:1, :], rhs=cnt[0:1, :], start=True, stop=True))
            p.op('dve', [bk(1)], ['ms_psbc'], lambda E: E.tensor_copy(out=psbc[:, :], in_=B[1][:, 0:32]))
            p.op('pe', ['ms_pcol', 'cst'], [bk(0)],
                 lambda E: E.matmul(B[0][0:32, 0:1], lhsT=cst[0:32, 384:416], rhs=pcol[:, 0:1], start=True, stop=True))
            p.op('dve', [bk(0)], ['ms_pcol2'], lambda E: E.tensor_copy(out=pcol[:, 1:2], in_=B[0][0:32, 0:1]))
            p.op('dve', ['ms_pcol2', 'cst'], ['ms_cmp'],
                 lambda E: E.tensor_scalar(out=cmp_[:, :], in0=blk512[0:32, :], scalar1=pcol[:, 1:2], scalar2=None, op0=ALU.is_ge))
            p.op('pe', ['ms_cmp', 'cst'], [bk(0)],
                 lambda E: E.matmul(B[0][0:1, 0:128], lhsT=ones[0:32, 0:1], rhs=cmp_[:, :], start=True, stop=True))
            p.op('dve', [bk(0)], ['ms_berow'], lambda E: E.tensor_copy(out=berow[:, :], in_=B[0][0:1, 0:128]))
            p.op('pe', ['ms_berow', 'cst'], [bk(0)],
                 lambda E: E.matmul(B[0][:, 0:128], lhsT=ones[0:1, :], rhs=berow[0:1, :], start=True, stop=True))
            p.op('dve', [bk(0)], ['ms_oob'],
                 lambda E: E.tensor_scalar(out=tmpf[:, 0:NB], in0=B[0][:, 0:NB], scalar1=31.5, scalar2=0.0, op0=ALU.is_ge, op1=ALU.mult))
            p.op('dve', [bk(0)], ['ms_bebc'],
                 lambda E: E.tensor_scalar(out=be_bc[:, :], in0=B[0][:, 0:NB], scalar1=31.0, scalar2=None, op0=ALU.min))
            p.op('dve', ['ms_bebc', 'cst'], ['ms_oh'],
                 lambda E: E.tensor_scalar(out=OHall[:, :], in0=be_bc[0:32, :], scalar1=iota_p[0:32, :], scalar2=None, op0=ALU.is_equal))
            p.op('dve', ['ms_bebc', 'cst', 'ms_oob'], ['ms_oob'],
                 lambda E: E.tensor_scalar(out=tmpf[:, 0:NB], in0=tmpf[:, 0:NB], scalar1=iota_p[:, :], scalar2=None, op0=ALU.add))
            p.op('dve', ['ms_bebc', 'ms_oob'], ['ms_tmpf'],
                 lambda E: E.scalar_tensor_tensor(out=tmpf[:, 0:NB], in0=be_bc[:, :], scalar=128.0, in1=tmpf[:, 0:NB], op0=ALU.mult, op1=ALU.add))
            p.op('dve', ['ms_tmpf'], ['ms_idxw'], lambda E: E.tensor_copy(out=idxw[:, :, 0], in_=tmpf[:, 0:NB]))
            p.op('dve', [], ['ms_Msum'], lambda E: E.memset(Msum[:, :], 0.0))
            for jj, t in enumerate(tiles):
                s2 = jj % 2
                tok = slice(t * 128, (t + 1) * 128)
                lgj, Mj, Gj = Lall[:, t, :], Mall[:, t, :], Gall[:, t, :]

                def mmp(E):
                    E.matmul(B[3 + s2][:, 0:32], lhsT=Lst[:, :], rhs=Mj, start=True, stop=False)
                    return E.matmul(B[3 + s2][:, 0:32], lhsT=ones[:, :], rhs=Msum[:, :], start=False, stop=True)
                p.op('pe', [('ms_M', t), 'ms_Msum', 'ms_Lst', 'cst'], [bk(3 + s2)], mmp)
                p.op('dve', [bk(3 + s2), 'ms_psbc'], [('ms_pos', s2)],
                     lambda E: E.tensor_tensor(out=pos[s2][:, :], in0=B[3 + s2][:, 0:32], in1=psbc[:, :], op=ALU.add))
                p.op('pool', [('ms_M', t), 'ms_Msum'], ['ms_Msum'],
                     lambda E: E.tensor_tensor(out=Msum[:, :], in0=Msum[:, :], in1=Mj, op=ALU.add))
                for k in range(4):
                    p.op('dve', [('ms_L', t), ('ms_mx', t)], [('ms_ohk', s2)],
                         lambda E: E.tensor_scalar(out=oh[s2][:, :], in0=lgj, scalar1=mxall[:, t, k:k + 1], scalar2=None, op0=ALU.is_equal))
                    p.op('dve', [('ms_ohk', s2), ('ms_pos', s2)], [('ms_tq', s2)],
                         lambda E: E.tensor_tensor(out=tq[s2][:, :], in0=oh[s2][:, :], in1=pos[s2][:, :], op=ALU.mult))
                    p.op('dve', [('ms_tq', s2)], [('ms_sl', s2, k)],
                         lambda E: E.tensor_reduce(out=sl[s2][:, k:k + 1], in_=tq[s2][:, :], axis=AX.X, op=ALU.add))
                    p.op('dve', [('ms_ohk', s2), ('ms_G', t)], [('ms_tq', s2)],
                         lambda E: E.tensor_tensor(out=tq[s2][:, :], in0=oh[s2][:, :], in1=Gj, op=ALU.mult))
                    p.op('dve', [('ms_tq', s2)], [('ms_gk', t, k)],
                         lambda E: E.tensor_reduce(out=gk[:, t, k:k + 1], in_=tq[s2][:, :], axis=AX.X, op=ALU.add))
                p.op('dve', [('ms_sl', s2, k) for k in range(4)], [('ms_slot', t)],
                     lambda E: E.tensor_copy(out=slot_i[:, t, :], in_=sl[s2][:, :]))
                p.dma('sp', 'ms_hbl%d' % s2, [('HB', t)], [('ms_hb', s2)], [(hb[s2][:, :], HB[tok, :])])
                p.idma('scat', [('ms_hb', s2), ('ms_slot', t), 'XBZ'], [('XB', t)],
                       [dict(out=XB[:, :], out_offset=bass.IndirectOffsetOnAxis(ap=slot_i[:, t, k:k + 1], axis=0),
                             in_=hb[s2][:, :], in_offset=None) for k in range(4)])
            p.barrier()
        if c.cfg.get('moe_phases', 3) < 2:
            return
        with ExitStack() as es:
            c.es = es
            w1 = [sb(c, 'me_w1%d' % i, [128, 8, 2048], BF16) for i in range(2)]
            w2 = [sb(c, 'me_w2%d' % i, [128, 8, 1024], BF16) for i in range(2)]
            xb = [sb(c, 'me_xb%d' % i, [128, 4, 1024], BF16) for i in range(2)]
            xT = [sb(c, 'me_xT%d' % i, [128, 8, 512], BF16) for i in range(2)]
            aT = sb(c, 'me_aT', [128, 8, 512], BF16)
            yb = [sb(c, 'me_yb%d' % i, [128, 1024], BF16) for i in range(2)]
            b1 = sb(c, 'me_b1', [128, 32, 16], F32)
            b2 = sb(c, 'me_b2', [32, 1024], BF16)
            b2f = sb(c, 'me_b2f', [32, 1024], F32)
            ohr = [sb(c, 'me_ohr%d' % i, [128, 32], F32) for i in range(2)]
            b1t = sb(c, 'me_b1t', [128, 32, 16], F32)
            b1s = [sb(c, 'me_b1s%d' % i, [128, 16], F32) for i in range(2)]
            ohb = [sb(c, 'me_ohb%d' % i, [32, 128], BF16) for i in range(2)]
            gg = [sb(c, 'me_gg%d' % i, [128, 512], F32) for i in range(2)]
            sg = [sb(c, 'me_sg%d' % i, [128, 512], F32) for i in range(2)]
            ll = [sb(c, 'me_ll%d' % i, [128, 512], F32) for i in range(2)]
            p.dma('sp', 'me_b2', [], ['me_b2f'], [(b2f[:, :], I['moe_b2'][l])])
            p.op('dve', ['me_b2f'], ['me_b2'], lambda E: E.tensor_copy(out=b2[:, :], in_=b2f[:, :]))
            p.dma('sp', 'me_b1', [], ['me_b1'],
                  [(b1[:, e, :], I['moe_b1'][l, e, :].rearrange("(j p) -> p j", p=128)) for e in range(32)], slow=True)
            wbk = [('WB', l, e) for e in range(32)]
            ei = 0
            def gather_w(i):
                ws = i % 2
                wdeps = (wbk if i < 2 else []) + ['ms_idxw']
                p.idma('gw1%d' % ws, wdeps, [('me_w1', ws)],
                       [dict(out=w1[ws][:, :, :].rearrange("p k n -> p (k n)"), out_offset=None, in_=W1f[:, :],
                             in_offset=bass.IndirectOffsetOnAxis(ap=idxw[:, i, 0:1], axis=0))])
                p.idma('gw2%d' % ws, wdeps, [('me_w2', ws)],
                       [dict(out=w2[ws][:, :, :].rearrange("p k n -> p (k n)"), out_offset=None, in_=W2f[:, :],
                             in_offset=bass.IndirectOffsetOnAxis(ap=idxw[:, i, 0:1], axis=0))])

            def load_xb(i):
                ws = i % 2
                xbk = [('XB', t) for t in tiles] if i < 2 else []
                p.dma('sp', 'me_xb%d' % ws, xbk, [('me_xb', ws)],
                      [(xb[ws][:, :, :], XB[i * 512:(i + 1) * 512, :].rearrange("(a p) f -> p a f", p=128))])

            gather_w(0)
            for i in range(NB):
                ws = i % 2
                if i + 1 < NB:
                    gather_w(i + 1)
                if i == 0:
                    load_xb(0)
                if i + 1 < NB:
                    load_xb(i + 1)
                for a in range(4):
                    tb = 4 + (i * 4 + a) % 2
                    tpv = bank16(c, tb)

                    def trx(E):
                        ins = None
                        for k in range(8):
                            ins = E.transpose(tpv[:, k, :], xb[ws][:, a, k * 128:(k + 1) * 128], c.ident[:, :])
                        return ins
                    p.op('pe', [('me_xb', ws), 'ident'], [bk(tb)], trx)
                    if a % 2 == 0:
                        p.op('act', [bk(tb)], [('me_xT', ws, a)],
                             lambda E: E.activation(out=xT[ws][:, :, a * 128:(a + 1) * 128], in_=tpv[:, :, :], func=AF.Copy))
                    else:
                        p.op('dve', [bk(tb)], [('me_xT', ws, a)],
                             lambda E: E.tensor_copy(out=xT[ws][:, :, a * 128:(a + 1) * 128], in_=tpv[:, :, :]))
                xTk = [('me_xT', ws, a) for a in range(4)]
                p.op('dve', ['ms_bebc', 'cst'], [('me_ohr', ws)],
                     lambda E: E.tensor_scalar(out=ohr[ws][:, :], in0=iota_f, scalar1=be_bc[:, i:i + 1], scalar2=None, op0=ALU.is_equal))
                p.op('dve', [('me_ohr', ws), 'me_b1'], ['me_b1t'],
                     lambda E: E.tensor_tensor(out=b1t[:, :, :], in0=b1[:, :, :], in1=ohr[ws][:, :].unsqueeze(2).to_broadcast([128, 32, 16]),
                                               op=ALU.mult))
                p.op('dve', ['me_b1t'], [('me_b1s', ws)],
                     lambda E: E.tensor_reduce(out=b1s[ws][:, :], in_=b1t[:, :, :].rearrange("p e j -> p j e"), axis=AX.X, op=ALU.add))
                p.op('dve', ['ms_oh', 'cst'], [('me_ohb', ws)],
                     lambda E: E.tensor_scalar(out=ohb[ws][:, :], in0=ones[0:32, :], scalar1=OHall[:, i:i + 1], scalar2=None, op0=ALU.mult))
                for fc in range(8):
                    s2 = ei % 2
                    ei += 1
                    bg, bl = s2 * 2, s2 * 2 + 1

                    def mm1(E):
                        ins = None
                        for k in range(8):
                            E.matmul(B[bg][:, :], lhsT=w1[ws][:, k, fc * 128:(fc + 1) * 128], rhs=xT[ws][:, k, :], start=(k == 0), stop=(k == 7))
                        for k in range(8):
                            ins = E.matmul(B[bl][:, :], lhsT=w1[ws][:, k, 1024 + fc * 128:1024 + (fc + 1) * 128], rhs=xT[ws][:, k, :],
                                           start=(k == 0), stop=(k == 7))
                        return ins
                    p.op('pe', xTk + [('me_w1', ws)], [bk(bg), bk(bl)], mm1)
                    p.op('dve', [bk(bg), ('me_b1s', ws)], [('me_gg', s2)],
                         lambda E: E.tensor_scalar(out=gg[s2][:, :], in0=B[bg][:, :], scalar1=b1s[ws][:, fc:fc + 1], scalar2=7.0,
                                                   op0=ALU.add, op1=ALU.min))
                    p.op('act', [('me_gg', s2)], [('me_sg', s2)],
                         lambda E: E.activation(out=sg[s2][:, :], in_=gg[s2][:, :], func=AF.Silu, scale=1.702))
                    p.op('dve', [bk(bl), ('me_b1s', ws)], [('me_ll', s2)],
                         lambda E: E.tensor_scalar(out=ll[s2][:, :], in0=B[bl][:, :], scalar1=b1s[ws][:, 8 + fc:9 + fc], scalar2=7.0,
                                                   op0=ALU.add, op1=ALU.min))
                    p.op('dve', [('me_ll', s2)], [('me_ll', s2)],
                         lambda E: E.tensor_scalar(out=ll[s2][:, :], in0=ll[s2][:, :], scalar1=-7.0, scalar2=1.0, op0=ALU.max, op1=ALU.add))
                    p.op('dve', [('me_sg', s2), ('me_ll', s2)], [('me_aT', fc)],
                         lambda E: E.scalar_tensor_tensor(out=aT[:, fc, :], in0=sg[s2][:, :], scalar=1.0 / 1.702, in1=ll[s2][:, :],
                                                          op0=ALU.mult, op1=ALU.mult))
                aTk = [('me_aT', fc) for fc in range(8)]
                for a in range(4):
                    y2 = (i * 4 + a) % 2
                    for half in range(2):
                        bb = 6 + half

                        def mm2(E):
                            for k in range(8):
                                E.matmul(B[bb][:, :], lhsT=aT[:, k, a * 128:(a + 1) * 128], rhs=w2[ws][:, k, half * 512:(half + 1) * 512],
                                         start=(k == 0), stop=False)
                            return E.matmul(B[bb][:, :], lhsT=ohb[ws][:, :], rhs=b2[:, half * 512:(half + 1) * 512], start=False, stop=True)
                        p.op('pe', aTk + [('me_w2', ws), ('me_ohb', ws), 'me_b2'], [bk(bb)], mm2)
                        if half == 0:
                            p.op('act', [bk(bb)], [('me_yb', y2, half)],
                                 lambda E: E.activation(out=yb[y2][:, 0:512], in_=B[bb][:, :], func=AF.Copy))
                        else:
                            p.op('dve', [bk(bb)], [('me_yb', y2, half)], lambda E: E.tensor_copy(out=yb[y2][:, 512:1024], in_=B[bb][:, :]))
                    r0 = i * 512 + a * 128
                    p.dma('sp', 'me_yb%d' % y2, [('me_yb', y2, 0), ('me_yb', y2, 1)], [('YB', i, a)], [(YB[r0:r0 + 128, :], yb[y2][:, :])])
            p.barrier()
        if c.cfg.get('moe_phases', 3) < 3:
            return
        with ExitStack() as es:
            c.es = es
            yg = [[sb(c, 'mc_yg%d_%d' % (i, k), [128, 1024], BF16) for k in range(4)] for i in range(2)]
            xt = [sb(c, 'mc_xt%d' % i, [128, 1024], F32) for i in range(2)]
            ff = [sb(c, 'mc_ff%d' % i, [128, 1024], F32) for i in range(2)]
            for jj, t in enumerate(tiles):
                s2 = jj % 2
                r = 1 if t < 2 else 0
                tok = slice(t * 128, (t + 1) * 128)
                p.dma('sp', 'mc_xt%d' % s2, ['XOUT'], [('mc_xt', s2)], [(xt[s2][:, :], c.X1[tok, :])])
                ybk = [('YB', i, a) for i in range(NB) for a in range(4)] if jj < 2 else []
                p.idma('gy%d' % s2, [('ms_slot', t)] + ybk, [('mc_yg', s2)],
                       [dict(out=yg[s2][k][:, :], out_offset=None, in_=YB[:, :],
                             in_offset=bass.IndirectOffsetOnAxis(ap=slot_i[:, t, k:k + 1], axis=0)) for k in range(4)])
                p.op('dve', [('mc_yg', s2), ('ms_gk', t, 0)], [('mc_ff', s2)],
                     lambda E: E.tensor_scalar(out=ff[s2][:, :], in0=yg[s2][0][:, :], scalar1=gk[:, t, 0:1], scalar2=None, op0=ALU.mult))
                for k in range(1, 4):
                    p.op('dve', [('mc_yg', s2), ('ms_gk', t, k), ('mc_ff', s2)], [('mc_ff', s2)],
                         lambda E: E.scalar_tensor_tensor(out=ff[s2][:, :], in0=yg[s2][k][:, :], scalar=gk[:, t, k:k + 1], in1=ff[s2][:, :],
                                                          op0=ALU.mult, op1=ALU.add))
                p.op('pool', [('mc_ff', s2), 'e_bc'], [('mc_ff', s2)],
                     lambda E: E.tensor_tensor(out=ff[s2][:, :], in0=ff[s2][:, :], in1=c.e_ag[:, r, :], op=ALU.mult))
                p.op('dve', [('mc_ff', s2), ('mc_xt', s2)], [('mc_xt', s2)],
                     lambda E: E.scalar_tensor_tensor(out=xt[s2][:, :], in0=xt[s2][:, :], scalar=ALPHA, in1=ff[s2][:, :],
                                                      op0=ALU.mult, op1=ALU.add))
                ln_affine_store(c, xt[s2], ('mc_xt', s2), c.e_gb, 'e_bc', dst_fn(t), s2)
            p.barrier()


def make_rope():
    rows = NLAT // 64
    row = np.repeat(np.arange(rows, dtype=np.float32), 64)
    col = np.tile(np.arange(64, dtype=np.float32), rows)
    inv = (np.float32(10000.0) ** (-np.arange(8, dtype=np.float32) / np.float32(8))).astype(np.float32)
    ang = np.concatenate([row[:, None] * inv, col[:, None] * inv], -1).astype(np.float32)
    return np.concatenate([np.cos(ang), np.sin(ang)], -1).astype(np.float32)


def make_hyena_consts():
    import ml_dtypes
    bf = ml_dtypes.bfloat16
    N = 16384
    out = {}
    a = np.arange(128, dtype=np.float64)
    f = np.arange(128, dtype=np.float64)
    ang = 2 * np.pi * np.outer(a, f) / 128
    d1 = np.zeros((128, 2, 2, 128), np.float64)
    d1[:, 0, 0, :] = np.cos(ang)
    d1[:, 0, 1, :] = -np.sin(ang)
    for aa in range(4):
        for ff_ in range(4):
            d1[aa, 1, 0, ff_] = np.cos(2 * np.pi * aa * ff_ / 4)
            d1[aa, 1, 1, ff_] = -np.sin(2 * np.pi * aa * ff_ / 4)
    out['hy_dft1'] = d1.astype(np.float32).astype(bf)
    e3 = np.zeros((128, 3, 128), np.float64)
    e3[:, 0, :] = np.cos(ang)
    e3[:, 1, :] = np.sin(ang)
    e3[:, 2, :] = -np.sin(ang)
    out['hy_e3'] = e3.astype(np.float32).astype(bf)
    f1 = np.arange(128)[:, None, None]
    b = np.arange(128)[None, :, None]
    f2 = np.arange(128)[None, None, :]
    th = 2 * np.pi * ((b * (f1 + 128 * f2)) % N) / N
    tw2 = np.stack([np.cos(th), -np.sin(th), np.sin(th)], axis=2)
    out['hy_tw2'] = tw2.astype(np.float32).astype(bf)
    bb = np.arange(128)[:, None, None]
    ff = np.arange(128)[None, :, None]
    aa = np.arange(64)[None, None, :]
    ps_ = 2 * np.pi * (((128 * aa + bb) * ff) % N) / N
    twf = np.stack([np.cos(ps_), -np.sin(ps_)], axis=2)
    out['hy_twf'] = twf.astype(np.float32).astype(bf)
    f1c = np.arange(4)[:, None, None]
    thc = 2 * np.pi * ((b * (f1c + 4 * f2)) % 512) / 512
    out['hy_tw2c'] = np.stack([np.cos(thc), -np.sin(thc), np.sin(thc)], axis=2).astype(np.float32).astype(bf)
    ffc = np.arange(4)[None, :, None]
    aac = np.arange(2)[None, None, :]
    psc = 2 * np.pi * (((128 * aac + bb) * ffc) % 512) / 512
    out['hy_twfc'] = np.stack([np.cos(psc), -np.sin(psc)], axis=2).astype(np.float32).astype(bf)
    deltas = np.abs(np.linspace(math.log(1e-2) / 1.5, math.log(1e-2) / 0.3, 512, dtype=np.float32))
    out['hy_negdelta'] = (-deltas).astype(np.float32)[None, :]

    def feats(L, s):
        s = np.asarray(s)
        t = np.linspace(0.0, 1.0, L, dtype=np.float32)[s][:, None]
        w = (np.float32(2 * math.pi) * np.arange(L, dtype=np.float32) / np.float32(L))[s][:, None]
        fq = np.linspace(1e-4, 15, 16, dtype=np.float32)
        z = np.concatenate([t, np.cos(fq * w), -np.sin(fq * w)], -1).astype(np.float32)
        return z, t[:, 0]
    BIG = 1e4
    L = 8192
    tau = np.arange(N)
    s = np.where(tau < L, tau, N - tau)
    s[L] = 0
    z, t = feats(L, s)
    out['hy_feat0'] = np.ascontiguousarray(z.T)
    out['hy_tvec0'] = t[None, :].astype(np.float32)
    L = 256
    tau = np.arange(512)
    s = np.where(tau < L, tau, 512 - tau)
    s[256] = 0
    z, t = feats(L, s)
    out['hy_feat1'] = np.ascontiguousarray(z.T)
    out['hy_tvec1'] = t[None, :].astype(np.float32)
    return out


def make_consts():
    cst = np.zeros((128, 1024), np.float32)
    cst[:, 0:128] = np.eye(128)
    U = (np.arange(128)[:, None] <= np.arange(128)[None, :]).astype(np.float32)
    cst[:, 128:256] = U / 16.0
    cst[:, 256:384] = U.T / 16.0
    cst[:, 384:512] = U
    cst[:, 512:640] = U.T
    cst[:, 640:768] = 1.0
    cst[:, 768:800] = np.arange(32, dtype=np.float32)[None, :]
    cst[:, 800] = np.arange(128, dtype=np.float32)
    cst[:, 896:1024] = 512.0 * np.arange(128, dtype=np.float32)[None, :]
    return cst


def build_program(cfg):
    nc = bass.Bass("TRN2", target_bir_lowering=False)
    es = ExitStack()
    c = Ctx()
    c.nc, c.es, c.cfg = nc, es, cfg
    c.dbg = set(cfg.get('dbg', []))
    c.p = Prog(nc, es)
    p = c.p
    layers = cfg.get('layers', [0, 1])
    stages = cfg.get('stages', ['adaln', 'proj'])

    def ext(name, shape, dt=F32):
        return nc.dram_tensor(name, list(shape), dt, kind="ExternalInput").ap()

    I = {}
    I['x'] = ext('x', [NLAT, D])
    I['ctx'] = ext('ctx', [NCTX, D])
    I['cc'] = ext('cc', [2, D])
    I['ada_w'] = ext('ada_w', [DEPTH, D, 6 * D])
    I['ada_b'] = ext('ada_b', [DEPTH, 6 * D])
    I['w_in'] = ext('w_in', [DEPTH, D, IN_TOTAL])
    I['cst'] = ext('cst', [128, 1024])
    for nm, shp in [('gla_wa2_f', [DEPTH, 16, 256]), ('gla_ba_f', [DEPTH, 256]), ('gla_wa2_b', [DEPTH, 16, 256]),
                    ('gla_ba_b', [DEPTH, 256]), ('gla_norm', [DEPTH, 128]),
                    ('mla_q_norm', [DEPTH, 384]), ('mla_w_uq', [DEPTH, 384, 768]), ('mla_kv_norm', [DEPTH, 256]),
                    ('mla_w_ukv', [DEPTH, 256, 1024]), ('rope', [NLAT, 32]),
                    ('hy_conv_w', [DEPTH, 3, 1536]), ('hy_conv_b', [DEPTH, 1536]), ('hy_w1', [DEPTH, 33, 64]),
                    ('hy_b1', [DEPTH, 64]), ('hy_w2', [DEPTH, 64, 64]), ('hy_b2', [DEPTH, 64]), ('hy_w3', [DEPTH, 64, 2048]),
                    ('hy_freq', [DEPTH, 64]), ('hy_bias', [DEPTH, 2, 512]),
                    ('hy_feat0', [33, 16384]), ('hy_tvec0', [1, 16384]), ('hy_feat1', [33, 512]), ('hy_tvec1', [1, 512]),
                    ('hy_negdelta', [1, 512]),
                    ('w_br_gla', [DEPTH, 512, D]), ('w_br_mla', [DEPTH, 512, D]), ('w_br_hy', [DEPTH, 512, D]),
                    ('w_out', [DEPTH, D, D]), ('ln1_g', [DEPTH, D]), ('ln1_b', [DEPTH, D]), ('ln2_g', [DEPTH, D]),
                    ('ln2_b', [DEPTH, D]), ('router_w', [DEPTH, D, 32]), ('router_b', [DEPTH, 32]),
                    ('moe_b1', [DEPTH, 32, 2048]), ('moe_b2', [DEPTH, 32, D])]:
        I[nm] = ext(nm, shp)
    for nm, shp in [('hy_dft1', [128, 2, 2, 128]), ('hy_e3', [128, 3, 128]), ('hy_tw2', [128, 128, 3, 128]),
                    ('hy_twf', [128, 128, 2, 64]), ('hy_tw2c', [4, 128, 3, 128]), ('hy_twfc', [128, 4, 2, 2])]:
        I[nm] = ext(nm, shp, BF16)
    if 'moe' in stages:
        I['moe_w1'] = ext('moe_w1', [DEPTH, 32, D, 2048])
        I['moe_w2'] = ext('moe_w2', [DEPTH, 32, D, D])
        c.WB1 = [nc.dram_tensor('WB1_%d' % l, [32, D, 2048], BF16).ap() for l in range(DEPTH)]
        c.WB2 = [nc.dram_tensor('WB2_%d' % l, [32, D, D], BF16).ap() for l in range(DEPTH)]
        c.XB = nc.dram_tensor('XB', [98 * 512, D], BF16).ap()
        c.YB = nc.dram_tensor('YB', [98 * 512, D], BF16).ap()
        c.HB = nc.dram_tensor('HB', [NT, D], BF16).ap()
    c.inp = I
    c.out = nc.dram_tensor('out', [NLAT, D], F32, kind="ExternalOutput").ap()

    c.modv = [dram(c, 'modv%d' % l, [2, 6 * D], F32) for l in range(DEPTH)]
    c.scr = []
    for l in range(DEPTH):
        S = {}
        for name, off, ncols in FM_GROUPS:
            S[name] = dram(c, '%s%d' % (name, l), [ncols, NT], F32 if name in ('AFT', 'ABT') else BF16)
        for name, off, ncols in TM_GROUPS:
            S[name] = dram(c, '%s%d' % (name, l), [NT, ncols], F32 if name == 'MKR' else BF16)
        for name in ('OGT', 'OMT', 'OHT'):
            S[name] = dram(c, '%s%d' % (name, l), [512, NT], BF16)
        c.scr.append(S)
    c.X1 = dram(c, 'X1', [NT, D], F32)
    c.X2 = dram(c, 'X2', [NT, D], F32)
    c.KpT = dram(c, 'KpT', [8, 97, NT], BF16)
    c.QpT = dram(c, 'QpT', [8, 97, NT], BF16)
    c.VpD = dram(c, 'VpD', [8, NT, 65], BF16)
    c.HYC = dram(c, 'HYC', [NT, 1536], BF16)
    c.Z2 = dram(c, 'Z2', [NT, 512], BF16)
    c.OH = dram(c, 'OH', [NT, 512], BF16)
    c.KTD = [dram(c, 'KTD0', [16384, 1024], BF16), dram(c, 'KTD1', [512, 1024], BF16)]
    c.KS = [dram(c, 'KS%d' % o, [128, 128, 2, 512], BF16) for o in range(2)]
    c.X1D = dram(c, 'X1D', [128, 128, 2, 512], BF16)
    c.QD = dram(c, 'QD', [128, 128, 2, 512], BF16)
    c.SCL = [dram(c, 'SCL%d' % j, [1, 1024], F32) for j in range(2)]

    c.cst = sb(c, 'cst_sb', [128, 1024], F32)
    c.ident = sb(c, 'ident16', [128, 128], BF16)
    c.cT = sb(c, 'cTsb', [128, 8, 2], F32)
    c.modT = sb(c, 'modT', [128, 2, 6, 8], F32)
    c.bank = [ps(c, 'bank%d' % i, [128, 512], F32) for i in range(8)]

    p.dma('sp', 'cst', [], ['cst'], [(c.cst[:, :], I['cst'][:, :])])
    p.op('dve', ['cst'], ['ident'], lambda E: E.tensor_copy(out=c.ident[:, :], in_=c.cst[:, 0:128]))
    p.dma('sp', 'cT', [], ['cT'],
          [(c.cT[:, :, r], I['cc'][r, :].rearrange("(k p) -> p k", p=128)) for r in range(2)], slow=True)
    p.op('act', ['cT'], ['cT'], lambda E: E.activation(out=c.cT[:, :, :], in_=c.cT[:, :, :], func=AF.Silu))

    for l in layers:
        if 'adaln' in stages:
            stage_adaln(c, l)
    if 'moe' in stages:
        for l in layers:
            moe_precast(c, l)
        if cfg.get('moe_mode', 'sparse') == 'sparse':
            c.zt = sb(c, 'zero_t', [128, 2048], BF16)
            p.op('pool', [], ['zero_t'], lambda E: E.memset(c.zt[:, :], 0.0))
            p.dma('sp', 'xbzero', ['zero_t'], ['XBZ'],
                  [(c.XB[i * 256:(i + 1) * 256, :].rearrange("(p a) f -> p (a f)", p=128), c.zt[:, :]) for i in range(196)])
    for l in layers:
        last = (l == DEPTH - 1)

        def xsrc(t, l=l):
            if l == 0:
                if t < 2:
                    return I['ctx'][t * 128:(t + 1) * 128, :]
                return I['x'][(t - 2) * 128:(t - 1) * 128, :]
            return c.X2[t * 128:(t + 1) * 128, :]
        if 'proj' in stages:
            stage_proj(c, l, xsrc)
        if 'gla' in stages:
            stage_gla(c, l)
        if 'mla' in stages:
            stage_mla(c, l, ctx_q=not last)
        if 'hyena' in stages:
            stage_hyena(c, l, with_ctx=not last)
        tiles = list(range(2, NTILE)) if last else list(range(NTILE))
        if 'merge' in stages:
            load_mod(c, l)
            stage_merge(c, l, xsrc, tiles)
        if 'moe' in stages:
            load_mod(c, l)

            def dst_fn(t, last=last):
                if last:
                    return [c.out[(t - 2) * 128:(t - 1) * 128, :]]
                return [c.X2[t * 128:(t + 1) * 128, :]]
            if cfg.get('moe_mode', 'sparse') == 'sparse':
                stage_moe2(c, l, tiles, dst_fn)
            else:
                stage_moe(c, l, tiles, dst_fn)
    p.finish('sp')
    return nc, c


ALL_STAGES = ['adaln', 'proj', 'gla', 'mla', 'hyena', 'merge', 'moe']
WEIGHT_KEYS = ['ada_w', 'ada_b', 'w_in', 'gla_wa2_f', 'gla_ba_f', 'gla_wa2_b', 'gla_ba_b', 'gla_norm', 'mla_q_norm', 'mla_w_uq',
               'mla_kv_norm', 'mla_w_ukv', 'hy_conv_w', 'hy_conv_b', 'hy_w1', 'hy_b1', 'hy_w2', 'hy_b2', 'hy_w3', 'hy_freq',
               'hy_bias', 'w_br_gla', 'w_br_mla', 'w_br_hy', 'w_out', 'ln1_g', 'ln1_b', 'ln2_g', 'ln2_b', 'router_w', 'router_b',
               'moe_w1', 'moe_b1', 'moe_w2', 'moe_b2']


def make_in_map(inputs, b, with_moe=True):
    f32 = lambda a: np.ascontiguousarray(np.asarray(a, dtype=np.float32))
    im = dict(x=f32(inputs['x'][b]), ctx=f32(inputs['ctx'][b]),
              cc=f32(np.stack([np.asarray(inputs['c'])[b], np.asarray(inputs['c_ctx'])])),
              cst=make_consts(), rope=make_rope())
    im.update(make_hyena_consts())
    for k in WEIGHT_KEYS:
        if not with_moe and k in ('moe_w1', 'moe_w2'):
            continue
        im[k] = f32(inputs[k])
    return im


def kernel(**inputs):
    nc, c = build_program(dict(layers=[0, 1], stages=ALL_STAGES))
    nb = np.asarray(inputs['x']).shape[0]
    in_maps = [make_in_map(inputs, b) for b in range(nb)]
    res = run_bass_kernel_spmd(nc, in_maps, core_ids=list(range(nb)))
    out = np.stack([np.asarray(res.results[b]['out']) for b in range(nb)], 0)
    return out.astype(np.float32)
```

```python
import math
from contextlib import ExitStack

import numpy as np
import concourse.bass as bass
import concourse.mybir as mybir
from concourse.bass_utils import run_bass_kernel_spmd

F32 = mybir.dt.float32
BF16 = mybir.dt.bfloat16
AF = mybir.ActivationFunctionType
ALU = mybir.AluOpType
AX = mybir.AxisListType

D = 1024
NCTX = 256
NLAT = 8192
NT = NCTX + NLAT
NTILE = NT // 128
DEPTH = 2
IN_TOTAL = 6848
LN_EPS = 1e-5
RMS_EPS = 1e-6
ALPHA = (2 * DEPTH) ** 0.25
O_GK, O_GV, O_GAF, O_GAB, O_MKVA, O_MKR, O_GQ, O_GR, O_MQA, O_HY, O_GATES = (
    0, 256, 768, 784, 800, 1056, 1088, 1344, 1856, 2240, 3776)


class Prog:
    def __init__(self, nc, es):
        self.nc = nc
        self.es = es
        self.E = dict(pe=nc.tensor, act=nc.scalar, dve=nc.vector, pool=nc.gpsimd, sp=nc.sync)
        self.sems = {}
        self.cnt = {}
        self.seen = {e: {} for e in self.E}
        self.st = {}
        self.n_ops = 0
        self.alias = {}
        self.free_slots = []
        self.n_slots = 0
        self.persistent = set(['wcast', 'xbzero'])

    def sem(self, key):
        if key not in self.sems:
            self.sems[key] = self.es.enter_context(self.nc.semaphore("s%d" % len(self.sems)))
            self.cnt[key] = 0
        return self.sems[key]

    def _deps(self, reads, writes):
        deps = {}

        def add(k, v):
            if deps.get(k, 0) < v:
                deps[k] = v

        for r in reads:
            s = self.st.get(r)
            if s and s[0]:
                add(*s[0])
        for w in writes:
            s = self.st.get(w)
            if s:
                if s[0]:
                    add(*s[0])
                for k, v in s[1].items():
                    add(k, v)
        return deps

    def _wait(self, eng, deps):
        E = self.E[eng]
        seen = self.seen[eng]
        for k, v in deps.items():
            if k == 'pe' and eng == 'pe':
                continue
            if seen.get(k, 0) < v:
                E.wait_ge(self.sems[k], v)
                seen[k] = v

    def _commit(self, ev, reads, writes):
        k, v = ev
        for r in reads:
            s = self.st.setdefault(r, [None, {}])
            if s[1].get(k, 0) < v:
                s[1][k] = v
        for w in writes:
            self.st[w] = [ev, {}]

    def op(self, eng, reads, writes, fn):
        reads = list(reads)
        writes = list(writes)
        for r in reads:
            if isinstance(r, tuple) and r[0] == 'bank' and r not in writes:
                writes.append(r)
        self._wait(eng, self._deps(reads, writes))
        sem = self.sem(eng)
        ins = fn(self.E[eng])
        self.cnt[eng] += 1
        ins.then_inc(sem, 1)
        self._commit((eng, self.cnt[eng]), reads, writes)
        self.n_ops += 1

    def _slot(self, semkey):
        if semkey in self.persistent:
            return semkey
        if semkey not in self.alias:
            if self.free_slots:
                self.alias[semkey] = self.free_slots.pop()
            else:
                self.alias[semkey] = ('dsem', self.n_slots)
                self.n_slots += 1
        return self.alias[semkey]

    def idma(self, semkey, reads, writes, calls):
        reads = list(reads)
        writes = list(writes)
        self._wait('pool', self._deps(reads, writes))
        key = self._slot(semkey)
        sem = self.sem(key)
        for kw in calls:
            self.nc.gpsimd.indirect_dma_start(**kw).then_inc(sem, 16)
            self.cnt[key] += 16
        self._commit((key, self.cnt[key]), reads, writes)
        self.n_ops += len(calls)

    def dma(self, eng, semkey, reads, writes, pairs, slow=False):
        reads = list(reads)
        writes = list(writes)
        self._wait(eng, self._deps(reads, writes))
        semkey = self._slot(semkey)
        sem = self.sem(semkey)
        for o, i in pairs:
            if slow:
                self.E[eng].dma_start(out=o, in_=i, allow_slow_non_contiguous=True).then_inc(sem, 16)
            else:
                self.E[eng].dma_start(out=o, in_=i).then_inc(sem, 16)
            self.cnt[semkey] += 16
        self._commit((semkey, self.cnt[semkey]), reads, writes)
        self.n_ops += len(pairs)

    def pe_fence(self, ins):
        sem = self.sem('pe')
        self.cnt['pe'] += 1
        ins.then_inc(sem, 1)
        self.E['pe'].wait_ge(sem, self.cnt['pe'])
        self.seen['pe']['pe'] = self.cnt['pe']

    def barrier(self):
        deps = {k: v for k, v in self.cnt.items() if v > 0 and k not in self.persistent}
        for eng in self.E:
            self._wait(eng, dict(deps))
        self.free_slots = [('dsem', i) for i in range(self.n_slots)]
        self.alias = {}

    def finish(self, eng='sp'):
        deps = {}
        for k, c in self.cnt.items():
            if c > 0:
                deps[k] = c
        self._wait(eng, deps)


class Ctx:
    pass


_uid = [0]


def sb(c, name, shape, dt):
    _uid[0] += 1
    return c.es.enter_context(c.nc.sbuf_tensor("%s_u%d" % (name, _uid[0]), list(shape), dt))


def ps(c, name, shape, dt):
    return c.es.enter_context(c.nc.psum_tensor(name, list(shape), dt))


def bank16(c, i):
    return c.bank[i][:, :].bitcast(BF16).rearrange("p (a b) -> p a b", a=8)


def dram(c, name, shape, dt, out=False):
    kind = "ExternalOutput" if (out or name in c.dbg) else "Internal"
    return c.nc.dram_tensor(name, list(shape), dt, kind=kind).ap()


def stage_adaln(c, l):
    with ExitStack() as es:
        c.es = es
        c.adaw = [sb(c, 'adaw%d' % i, [128, 8, 512], F32) for i in range(2)]
        c.adab = [sb(c, 'adab%d' % i, [2, 512], F32) for i in range(2)]
        c.modrow = [sb(c, 'modrow%d' % i, [2, 512], F32) for i in range(2)]
        c.psA = [c.bank[0], c.bank[1]]
        _stage_adaln(c, l)
        c.p.barrier()


def _stage_adaln(c, l):
    p, nc = c.p, c.nc
    I = c.inp
    cT = c.cT
    modv = c.modv[l]
    for cb in range(12):
        slot = cb % 2
        wt = c.adaw[slot]
        p.dma('sp', 'adaw%d' % slot, [], [('adaw', slot)],
              [(wt[:, :, :], I['ada_w'][l, :, cb * 512:(cb + 1) * 512].rearrange("(k p) n -> p k n", p=128))])
        bt = c.adab[slot]
        p.dma('sp', 'adab%d' % slot, [], [('adab', slot)],
              [(bt[0:1, :], I['ada_b'][l:l + 1, cb * 512:(cb + 1) * 512]),
               (bt[1:2, :], I['ada_b'][l:l + 1, cb * 512:(cb + 1) * 512])])
        pt = c.psA[cb % 2]

        def mm(E, wt=wt, pt=pt):
            ins = None
            for k in range(8):
                ins = E.matmul(pt[0:2, :], lhsT=cT[:, k, :], rhs=wt[:, k, :], start=(k == 0), stop=(k == 7))
            return ins
        p.op('pe', [('adaw', slot), 'cT'], [('bank', cb % 2)], mm)
        mt = c.modrow[slot]
        p.op('dve', [('bank', cb % 2), ('adab', slot)], [('modrow', slot)],
             lambda E, mt=mt, pt=pt, bt=bt: E.tensor_tensor(out=mt[0:2, :], in0=pt[0:2, :], in1=bt[0:2, :], op=ALU.add))
        p.dma('sp', 'modrow%d' % slot, [('modrow', slot)], [('modv', l)],
              [(modv[0:2, cb * 512:(cb + 1) * 512], mt[0:2, :])])


def load_mod(c, l):
    p = c.p
    modv = c.modv[l]
    pairs = []
    for r in range(2):
        for g in range(6):
            pairs.append((c.modT[:, r, g, :], modv[r, g * 1024:(g + 1) * 1024].rearrange("(k p) -> p k", p=128)))
    p.dma('sp', 'modT', [('modv', l)], ['modT'], pairs, slow=True)
    p.op('dve', ['modT'], ['modT'],
         lambda E: E.tensor_scalar(out=c.modT[:, :, 1, :], in0=c.modT[:, :, 1, :], scalar1=1.0, scalar2=None, op0=ALU.add))
    p.op('dve', ['modT'], ['modT'],
         lambda E: E.tensor_scalar(out=c.modT[:, :, 4, :], in0=c.modT[:, :, 4, :], scalar1=1.0, scalar2=None, op0=ALU.add))


def ln_mod_tile(c, src_ap, r, gsh, gsc, hT, col0, hkey):
    p = c.p
    i = c.ln_i
    c.ln_i += 1
    s = i % 3
    xt = c.xt[s]
    st = c.lnst[s]
    p.dma('pool', 'xt%d' % s, ['XOUT'], [('xt', s)], [(xt[:, :], src_ap)])
    junk = c.junk[i % 2]
    p.op('act', [('xt', s)], [('junk', i % 2), ('lnst', s, 0)],
         lambda E: E.activation(out=junk[:, :], in_=xt[:, :], func=AF.Copy, accum_out=st[:, 0:1]))
    p.op('dve', [('lnst', s, 0)], [('lnst', s, 1)],
         lambda E: E.tensor_scalar(out=st[:, 1:2], in0=st[:, 0:1], scalar1=-1.0 / D, scalar2=None, op0=ALU.mult))
    p.op('act', [('xt', s), ('lnst', s, 1)], [('junk', i % 2), ('lnst', s, 2)],
         lambda E: E.activation(out=junk[:, :], in_=xt[:, :], func=AF.Square, bias=st[:, 1:2], scale=1.0,
                                accum_out=st[:, 2:3]))
    p.op('act', [('lnst', s, 2)], [('lnst', s, 3)],
         lambda E: E.activation(out=st[:, 3:4], in_=st[:, 2:3], func=AF.Ln, scale=1.0 / D, bias=LN_EPS))
    p.op('act', [('lnst', s, 3)], [('lnst', s, 4)],
         lambda E: E.activation(out=st[:, 4:5], in_=st[:, 3:4], func=AF.Exp, scale=-0.5))
    if c.cfg.get('lnsteps', 9) < 2:
        return
    xh = c.xh[i % 2]
    p.op('dve', [('xt', s), ('lnst', s, 1), ('lnst', s, 4)], [('xh', i % 2)],
         lambda E: E.tensor_scalar(out=xh[:, :], in0=xt[:, :], scalar1=st[:, 1:2], scalar2=st[:, 4:5],
                                   op0=ALU.add, op1=ALU.mult))
    if c.cfg.get('lnsteps', 9) < 3:
        return
    tp = c.tp[i % 2]

    def tr(E):
        ins = None
        for k in range(8):
            ins = E.transpose(tp[:, k, :], xh[:, k * 128:(k + 1) * 128], c.ident[:, :])
        return ins
    p.op('pe', [('xh', i % 2), 'ident'], [c.tpk[i % 2]], tr)
    if c.cfg.get('lnsteps', 9) < 4:
        return
    for k in range(8):
        eng = c.cfg.get('evac_eng') or ('act' if i % 2 == 0 else 'dve')
        if eng == 'act':
            p.op('act', [c.tpk[i % 2], 'modT'], [hkey + (k,)],
                 lambda E, k=k: E.activation(out=hT[:, k, col0:col0 + 128], in_=tp[:, k, :], func=AF.Identity,
                                             bias=c.modT[:, r, gsh, k:k + 1], scale=c.modT[:, r, gsc, k:k + 1]))
        else:
            p.op('dve', [c.tpk[i % 2], 'modT'], [hkey + (k,)],
                 lambda E, k=k: E.tensor_scalar(out=hT[:, k, col0:col0 + 128], in0=tp[:, k, :],
                                                scalar1=c.modT[:, r, gsc, k:k + 1],
                                                scalar2=c.modT[:, r, gsh, k:k + 1],
                                                op0=ALU.mult, op1=ALU.add))


FM_GROUPS = [
    ('KT', O_GK, 256), ('QT', O_GQ, 256), ('AFT', O_GAF, 16), ('ABT', O_GAB, 16),
    ('MKVAT', O_MKVA, 256), ('MQAT', O_MQA, 384),
]
TM_GROUPS = [
    ('V', O_GV, 512), ('MKR', O_MKR, 32), ('GR', O_GR, 512), ('HY', O_HY, 1536), ('G3', O_GATES, 3072),
]


def stage_proj(c, l, xsrc):
    with ExitStack() as es:
        c.es = es
        c.win = sb(c, 'win', [128, 8, IN_TOTAL], BF16)
        c.hT = [sb(c, 'hT%d' % i, [128, 8, 512], BF16) for i in range(2)]
        alloc_ln(c)
        c.o16 = [sb(c, 'o16_%d' % i, [128, 512], BF16) for i in range(4)]
        c.o32 = [sb(c, 'o32_%d' % i, [128, 512], F32) for i in range(4)]
        c.psB = [c.bank[i] for i in range(4)]
        load_mod(c, l)
        _stage_proj(c, l, xsrc)
        c.p.barrier()


def alloc_ln(c):
    c.xt = [sb(c, 'xt%d' % i, [128, D], F32) for i in range(3)]
    c.lnst = [sb(c, 'lnst%d' % i, [128, 8], F32) for i in range(3)]
    c.junk = [sb(c, 'junk%d' % i, [128, D], BF16) for i in range(2)]
    c.xh = [sb(c, 'xh%d' % i, [128, D], BF16) for i in range(2)]
    c.tp = [bank16(c, 4), bank16(c, 5)]
    c.tpk = [('bank', 4), ('bank', 5)]
    c.ln_i = 0


def _stage_proj(c, l, xsrc):
    p, nc = c.p, c.nc
    I = c.inp
    win = c.win
    for k in range(8):
        p.dma('pool', 'win', [], [('win', k)],
              [(win[:, k, :], I['w_in'][l, k * 128:(k + 1) * 128, :])])
    S = c.scr[l]
    blocks = [(0, 2)] + [(2 + 4 * i, 4) for i in range(16)]
    blocks = blocks[:c.cfg.get('nblk', 17)]
    ev = 0
    for bi, (t0, ntl) in enumerate(blocks):
        T = ntl * 128
        tok0 = t0 * 128
        hb = bi % 2
        hT = c.hT[hb]
        r = 1 if bi == 0 else 0
        for j in range(ntl):
            ln_mod_tile(c, xsrc(t0 + j), r, 0, 1, hT, j * 128, ('hT', hb))
        hkeys = [('hT', hb, k) for k in range(8)]
        wkeys = [('win', k) for k in range(8)]
        if c.cfg.get('nomm'):
            continue
        for name, off, ncols in FM_GROUPS:
            for m0 in range(0, ncols, 128):
                M = min(128, ncols - m0)
                pb = ev % 4
                pt = c.psB[pb]

                def mm(E, pt=pt, off=off, m0=m0, M=M, T=T):
                    ins = None
                    for k in range(8):
                        ins = E.matmul(pt[0:M, 0:T], lhsT=win[:, k, off + m0:off + m0 + M], rhs=hT[:, k, 0:T],
                                       start=(k == 0), stop=(k == 7))
                    return ins
                p.op('pe', hkeys + wkeys, [('bank', pb)], mm)
                fp32 = name in ('AFT', 'ABT')
                ob = ev % 4
                ot = (c.o32 if fp32 else c.o16)[ob]
                okey = ('o32' if fp32 else 'o16', ob)
                eng = 'act' if ev % 2 == 0 else 'dve'
                if eng == 'act':
                    p.op('act', [('bank', pb)], [okey],
                         lambda E, ot=ot, pt=pt, M=M, T=T: E.activation(out=ot[0:M, 0:T], in_=pt[0:M, 0:T], func=AF.Copy))
                else:
                    p.op('dve', [('bank', pb)], [okey],
                         lambda E, ot=ot, pt=pt, M=M, T=T: E.tensor_copy(out=ot[0:M, 0:T], in_=pt[0:M, 0:T]))
                p.dma('sp', 'o%s%d' % ('32' if fp32 else '16', ob), [okey], [(name, l)],
                      [(S[name][m0:m0 + M, tok0:tok0 + T], ot[0:M, 0:T])])
                ev += 1
        for name, off, ncols in TM_GROUPS:
            for n0 in range(0, ncols, 512):
                N = min(512, ncols - n0)
                for j in range(ntl):
                    pb = ev % 4
                    pt = c.psB[pb]

                    def mm(E, pt=pt, off=off, n0=n0, N=N, j=j):
                        ins = None
                        for k in range(8):
                            ins = E.matmul(pt[:, 0:N], lhsT=hT[:, k, j * 128:(j + 1) * 128],
                                           rhs=win[:, k, off + n0:off + n0 + N], start=(k == 0), stop=(k == 7))
                        return ins
                    p.op('pe', hkeys + wkeys, [('bank', pb)], mm)
                    fp32 = name == 'MKR'
                    ob = ev % 4
                    ot = (c.o32 if fp32 else c.o16)[ob]
                    okey = ('o32' if fp32 else 'o16', ob)
                    if name == 'G3':
                        p.op('act', [('bank', pb)], [okey],
                             lambda E, ot=ot, pt=pt, N=N: E.activation(out=ot[:, 0:N], in_=pt[:, 0:N], func=AF.Sigmoid))
                    elif ev % 2 == 0:
                        p.op('act', [('bank', pb)], [okey],
                             lambda E, ot=ot, pt=pt, N=N: E.activation(out=ot[:, 0:N], in_=pt[:, 0:N], func=AF.Copy))
                    else:
                        p.op('dve', [('bank', pb)], [okey],
                             lambda E, ot=ot, pt=pt, N=N: E.tensor_copy(out=ot[:, 0:N], in_=pt[:, 0:N]))
                    p.dma('sp', 'o%s%d' % ('32' if fp32 else '16', ob), [okey], [(name, l)],
                          [(S[name][tok0 + j * 128:tok0 + (j + 1) * 128, n0:n0 + N], ot[:, 0:N])])
                    ev += 1


def stage_gla(c, l):
    with ExitStack() as es:
        c.es = es
        _stage_gla(c, l)
        c.p.barrier()


def _stage_gla(c, l):
    p, nc = c.p, c.nc
    I = c.inp
    S = c.scr[l]
    NCH = NTILE
    qT = sb(c, 'g_qT', [128, NT], BF16)
    kT = sb(c, 'g_kT', [128, NT], BF16)
    v = sb(c, 'g_v', [128, NCH, 256], BF16)
    oacc = sb(c, 'g_oacc', [128, NCH, 256], F32)
    wa2 = sb(c, 'g_wa2', [16, 2, 256], F32)
    ba = sb(c, 'g_ba', [1, 2, 256], F32)
    normw = sb(c, 'g_normw', [128, 128], F32)
    aft = [sb(c, 'g_aft%d' % i, [16, 128], F32) for i in range(3)]
    g1 = [sb(c, 'g_g1%d' % i, [128, 128], F32) for i in range(2)]
    g2 = [sb(c, 'g_g2%d' % i, [128, 128], F32) for i in range(2)]
    e1 = [sb(c, 'g_e1%d' % i, [128, 128], F32) for i in range(2)]
    e2 = [sb(c, 'g_e2%d' % i, [128, 128], F32) for i in range(2)]
    qb = [sb(c, 'g_qb%d' % i, [128, 128], BF16) for i in range(2)]
    kb = [sb(c, 'g_kb%d' % i, [128, 128], BF16) for i in range(2)]
    kd = [sb(c, 'g_kd%d' % i, [128, 128], BF16) for i in range(2)]
    kdt = [sb(c, 'g_kdt%d' % i, [128, 128], BF16) for i in range(2)]
    am = [sb(c, 'g_am%d' % i, [128, 256], BF16) for i in range(2)]
    St = sb(c, 'g_S', [128, 128], F32)
    Sb = sb(c, 'g_Sb', [128, 128], BF16)
    rst = sb(c, 'g_rst', [128, NCH * 2], F32)
    rst2 = sb(c, 'g_rst2', [128, NCH * 2], F32)
    junk = sb(c, 'g_junk', [128, 128], BF16)
    osb = [sb(c, 'g_osb%d' % i, [128, 512], BF16) for i in range(2)]
    psG = c.bank[0][:, 0:128]
    psBt = [c.bank[1][:, 0:128], c.bank[2][:, 0:128]]
    psA = c.bank[3][:, 0:256]
    psK = c.bank[4][:, :].bitcast(BF16)[:, 0:128]
    psO = [c.bank[5][:, 0:256], c.bank[6][:, 0:256]]
    psS = c.bank[7][:, 0:128]
    c.tp = [bank16(c, 1), bank16(c, 2)]
    cst = c.cst
    U16 = [cst[:, 128:256], cst[:, 256:384]]
    MSK = [cst[:, 384:512], cst[:, 512:640]]
    ones = cst[:, 640:768]

    p.dma('sp', 'g_w', [], ['g_w'],
          [(wa2[:, 0, :], I['gla_wa2_f'][l]), (wa2[:, 1, :], I['gla_wa2_b'][l]),
           (ba[0:1, 0, :], I['gla_ba_f'][l:l + 1, :]), (ba[0:1, 1, :], I['gla_ba_b'][l:l + 1, :]),
           (normw[:, :], I['gla_norm'][l, :].partition_broadcast(128))])
    gr = v
    step = 0
    for hp in range(2):
        p.dma('sp', 'g_q', [], ['g_qT'], [(qT[:, :], S['QT'][hp * 128:(hp + 1) * 128, :])])
        p.dma('sp', 'g_k', [], ['g_kT'], [(kT[:, :], S['KT'][hp * 128:(hp + 1) * 128, :])])
        p.dma('sp', 'g_v', [], ['g_v'],
              [(v[:, :, :], S['V'][:, hp * 256:(hp + 1) * 256].rearrange("(n p) c -> p n c", p=128))])
        for dirn in range(2):
            p.op('dve', [], ['g_S'], lambda E: E.memset(St[:, :], 0.0))
            p.op('dve', [], ['g_Sb'], lambda E: E.memset(Sb[:, :], 0.0))
            order = list(range(NCH)) if dirn == 0 else [1, 0] + list(range(NCH - 1, 1, -1))
            last = 127 if dirn == 0 else 0
            gsrc = S['AFT'] if dirn == 0 else S['ABT']
            for n in order[:c.cfg.get('gsteps', 999)]:
                t0 = n * 128
                s2 = step % 2
                s3 = step % 3
                step += 1
                a_t = aft[s3]
                p.dma('sp', 'g_aft%d' % s3, [], [('g_aft', s3)], [(a_t[:, :], gsrc[:, t0:t0 + 128])])

                def mmg(E, a_t=a_t, dirn=dirn, hp=hp):
                    E.matmul(psG[:, :], lhsT=a_t[0:16, :], rhs=wa2[0:16, dirn, hp * 128:(hp + 1) * 128],
                             start=True, stop=False)
                    return E.matmul(psG[:, :], lhsT=ones[0:1, :], rhs=ba[0:1, dirn, hp * 128:(hp + 1) * 128],
                                    start=False, stop=True)
                p.op('pe', [('g_aft', s3), 'g_w', 'cst'], [('bank', 0)], mmg)
                if c.cfg.get('gsub', 99) < 1:
                    continue
                p.op('act', [('bank', 0)], [('g_g1', s2)],
                     lambda E, s2=s2: E.activation(out=g1[s2][:, :], in_=psG[:, :], func=AF.Exp, scale=-1.0))
                p.op('act', [('g_g1', s2)], [('g_g2', s2)],
                     lambda E, s2=s2: E.activation(out=g2[s2][:, :], in_=g1[s2][:, :], func=AF.Ln, bias=1.0, scale=1.0))
                if c.cfg.get('gsub', 99) < 2:
                    continue
                pB = psBt[s2]
                p.op('pe', [('g_g2', s2), 'cst'], [('bank', 1 + s2)],
                     lambda E, s2=s2, pB=pB, dirn=dirn: E.matmul(pB[:, :], lhsT=g2[s2][:, :], rhs=U16[dirn],
                                                                 start=True, stop=True))
                p.op('act', [('bank', 1 + s2)], [('g_e1', s2)],
                     lambda E, s2=s2, pB=pB: E.activation(out=e1[s2][:, :], in_=pB[:, :], func=AF.Exp, scale=-1.0))
                p.op('act', [('bank', 1 + s2)], [('g_e2', s2)],
                     lambda E, s2=s2, pB=pB: E.activation(out=e2[s2][:, :], in_=pB[:, :], func=AF.Exp, scale=1.0))
                if c.cfg.get('gsub', 99) < 3:
                    continue
                p.op('dve', ['g_qT', ('g_e1', s2)], [('g_qb', s2)],
                     lambda E, s2=s2, t0=t0: E.scalar_tensor_tensor(out=qb[s2][:, :], in0=qT[:, t0:t0 + 128], scalar=0.125,
                                                                    in1=e1[s2][:, :], op0=ALU.mult, op1=ALU.mult))
                p.op('dve', ['g_kT', ('g_e2', s2)], [('g_kb', s2)],
                     lambda E, s2=s2, t0=t0: E.tensor_tensor(out=kb[s2][:, :], in0=kT[:, t0:t0 + 128], in1=e2[s2][:, :],
                                                             op=ALU.mult))
                p.op('dve', [('g_kb', s2), ('g_e1', s2)], [('g_kd', s2)],
                     lambda E, s2=s2, last=last: E.tensor_scalar(out=kd[s2][:, :], in0=kb[s2][:, :],
                                                                 scalar1=e1[s2][:, last:last + 1], scalar2=None,
                                                                 op0=ALU.mult))

                if c.cfg.get('gsub', 99) < 4:
                    continue
                def mma(E, s2=s2):
                    ins = None
                    for h in range(2):
                        if h == 1:
                            p.pe_fence(ins)
                        ins = E.matmul(psA[:, h * 128:(h + 1) * 128], lhsT=kb[s2][h * 64:(h + 1) * 64, :],
                                       rhs=qb[s2][h * 64:(h + 1) * 64, :], start=True, stop=True)
                    return ins
                p.op('pe', [('g_kb', s2), ('g_qb', s2)], [('bank', 3)], mma)
                if c.cfg.get('gsub', 99) < 5:
                    continue
                msk = MSK[dirn]
                p.op('dve', [('bank', 3), 'cst'], [('g_am', s2)],
                     lambda E, s2=s2, msk=msk: E.tensor_tensor(
                         out=am[s2][:, :].rearrange("p (h i) -> p h i", h=2),
                         in0=psA[:, :].rearrange("p (h i) -> p h i", h=2),
                         in1=msk.unsqueeze(1).to_broadcast([128, 2, 128]), op=ALU.mult))
                if c.cfg.get('gsub', 99) < 6:
                    continue
                p.op('pe', [('g_kd', s2), 'ident'], [('bank', 4)],
                     lambda E, s2=s2: E.transpose(psK[:, :], kd[s2][:, :], c.ident[:, :]))
                p.op('act', [('bank', 4)], [('g_kdt', s2)],
                     lambda E, s2=s2: E.activation(out=kdt[s2][:, :], in_=psK[:, :], func=AF.Copy))
                if c.cfg.get('gsub', 99) < 7:
                    continue
                pO = psO[s2]

                def mmo(E, s2=s2, pO=pO, n=n):
                    ins = None
                    for h in range(2):
                        if h == 1:
                            p.pe_fence(ins)
                        E.matmul(pO[:, h * 128:(h + 1) * 128], lhsT=am[s2][:, h * 128:(h + 1) * 128],
                                 rhs=v[:, n, h * 128:(h + 1) * 128], start=True, stop=False)
                        ins = E.matmul(pO[:, h * 128:(h + 1) * 128], lhsT=qb[s2][h * 64:(h + 1) * 64, :],
                                       rhs=Sb[h * 64:(h + 1) * 64, :], start=False, stop=True)
                    return ins
                p.op('pe', [('g_am', s2), 'g_v', ('g_qb', s2), 'g_Sb'], [('bank', 5 + s2)], mmo)
                if dirn == 0:
                    p.op('act', [('bank', 5 + s2)], [('g_oacc', n)],
                         lambda E, pO=pO, n=n: E.activation(out=oacc[:, n, :], in_=pO[:, :], func=AF.Copy))
                else:
                    p.op('dve', [('bank', 5 + s2), ('g_oacc', n)], [('g_oacc', n)],
                         lambda E, pO=pO, n=n: E.tensor_tensor(out=oacc[:, n, :], in0=pO[:, :], in1=oacc[:, n, :],
                                                               op=ALU.add))

                if c.cfg.get('gsub', 99) < 8:
                    continue
                def mms(E, s2=s2, n=n):
                    ins = None
                    for h in range(2):
                        if h == 1:
                            p.pe_fence(ins)
                        ins = E.matmul(psS[h * 64:(h + 1) * 64, :], lhsT=kdt[s2][:, h * 64:(h + 1) * 64],
                                       rhs=v[:, n, h * 128:(h + 1) * 128], start=True, stop=True)
                    return ins
                p.op('pe', [('g_kdt', s2), 'g_v'], [('bank', 7)], mms)
                if c.cfg.get('gsub', 99) < 9:
                    continue
                p.op('dve', [('bank', 7), ('g_e1', s2), 'g_S'], ['g_S'],
                     lambda E, s2=s2, last=last: E.scalar_tensor_tensor(out=St[:, :], in0=St[:, :],
                                                                        scalar=e1[s2][:, last:last + 1], in1=psS[:, :],
                                                                        op0=ALU.mult, op1=ALU.add))
                p.op('act', ['g_S'], ['g_Sb'], lambda E: E.activation(out=Sb[:, :], in_=St[:, :], func=AF.Copy))
        if c.cfg.get('gnofin'):
            continue
        okeys = [('g_oacc', n) for n in range(NCH)]
        for n in range(NCH):
            for h in range(2):
                p.op('act', [('g_oacc', n)], ['g_junk', ('g_rst', n, h)],
                     lambda E, n=n, h=h: E.activation(out=junk[:, :], in_=oacc[:, n, h * 128:(h + 1) * 128],
                                                      func=AF.Square, accum_out=rst[:, n * 2 + h:n * 2 + h + 1]))
        rkeys = [('g_rst', n, h) for n in range(NCH) for h in range(2)]
        p.op('act', rkeys, ['g_rst2'],
             lambda E: E.activation(out=rst2[:, :], in_=rst[:, :], func=AF.Ln, scale=1.0 / 128, bias=RMS_EPS))
        p.op('act', ['g_rst2'], ['g_rst2'],
             lambda E: E.activation(out=rst2[:, :], in_=rst2[:, :], func=AF.Exp, scale=-0.5))
        p.dma('sp', 'g_v', [], ['g_v'],
              [(gr[:, :, :], S['GR'][:, hp * 256:(hp + 1) * 256].rearrange("(n p) c -> p n c", p=128))])
        p.op('act', ['g_v'], ['g_v'], lambda E: E.activation(out=gr[:, :, :], in_=gr[:, :, :], func=AF.Silu))
        o4 = oacc[:, :, :].rearrange("p n (h d) -> p (n h) d", h=2)
        p.op('dve', okeys + ['g_rst2'], okeys,
             lambda E: E.tensor_tensor(out=o4, in0=o4, in1=rst2[:, :].unsqueeze(2).to_broadcast([128, NCH * 2, 128]),
                                       op=ALU.mult))
        p.op('dve', okeys + ['g_w'], okeys,
             lambda E: E.tensor_tensor(out=o4, in0=o4, in1=normw[:, :].unsqueeze(1).to_broadcast([128, NCH * 2, 128]),
                                       op=ALU.mult))
        p.op('dve', okeys + ['g_v'], ['g_v'],
             lambda E: E.tensor_tensor(out=gr[:, :, :], in0=oacc[:, :, :], in1=gr[:, :, :], op=ALU.mult))
        groups = [(0, 2)] + [(2 + 4 * i, 4) for i in range(16)]
        for gi, (n0, cnt) in enumerate(groups):
            for h in range(2):
                tb = (gi * 2 + h) % 2
                tpt = c.tp[tb]

                def tr(E, n0=n0, cnt=cnt, h=h, tpt=tpt):
                    ins = None
                    for j in range(cnt):
                        ins = E.transpose(tpt[:, j, :], gr[:, n0 + j, h * 128:(h + 1) * 128], c.ident[:, :])
                    return ins
                p.op('pe', ['g_v', 'ident'], [('bank', 1 + tb)], tr)
                ot = osb[tb]
                T = cnt * 128
                if tb == 0:
                    p.op('act', [('bank', 1 + tb)], [('g_osb', tb)],
                         lambda E, ot=ot, tpt=tpt, T=T: E.activation(out=ot[:, 0:T], in_=tpt[:, :, :].rearrange("p a b -> p (a b)")[:, 0:T], func=AF.Copy))
                else:
                    p.op('dve', [('bank', 1 + tb)], [('g_osb', tb)],
                         lambda E, ot=ot, tpt=tpt, T=T: E.tensor_copy(out=ot[:, 0:T], in_=tpt[:, :, :].rearrange("p a b -> p (a b)")[:, 0:T]))
                row0 = (hp * 2 + h) * 128
                p.dma('sp', 'g_osb%d' % tb, [('g_osb', tb)], [('OGT', l)],
                      [(S['OGT'][row0:row0 + 128, n0 * 128:n0 * 128 + T], ot[:, 0:T])])


MLA_SCALE = 96 ** -0.5


def stage_mla(c, l, ctx_q):
    with ExitStack() as es:
        c.es = es
        _stage_mla(c, l, ctx_q)
        c.p.barrier()


def _rms_rstd(c, psq, rs, nfeat, key_ps, key_rs):
    p = c.p
    p.op('act', [key_ps], [key_rs],
         lambda E: E.activation(out=rs, in_=psq, func=AF.Ln, scale=1.0 / nfeat, bias=RMS_EPS))
    p.op('act', [key_rs], [key_rs], lambda E: E.activation(out=rs, in_=rs, func=AF.Exp, scale=-0.5))


def _stage_mla(c, l, ctx_q):
    p, nc = c.p, c.nc
    I = c.inp
    S = c.scr[l]
    KpT, VpD, QpT = c.KpT, c.VpD, c.QpT
    cst = c.cst
    ones32 = cst[:, 640:768]
    wkv32 = sb(c, 'm_wkv32', [128, 2, 1024], F32)
    wq32 = sb(c, 'm_wq32', [128, 3, 768], F32)
    wkv = sb(c, 'm_wkv', [128, 2, 1024], BF16)
    wq = sb(c, 'm_wq', [128, 3, 768], BF16)
    gn = sb(c, 'm_gn', [128, 5], F32)
    onesb = sb(c, 'm_onesb', [128, 8], BF16)
    p.dma('sp', 'm_w', [], ['m_w32'],
          [(wkv32[:, :, :], I['mla_w_ukv'][l].rearrange("(k p) n -> p k n", p=128)),
           (wq32[:, :, :], I['mla_w_uq'][l].rearrange("(k p) n -> p k n", p=128))])
    p.dma('sp', 'm_g', [], ['m_gn'],
          [(gn[:, 0:2], I['mla_kv_norm'][l, :].rearrange("(k p) -> p k", p=128)),
           (gn[:, 2:5], I['mla_q_norm'][l, :].rearrange("(k p) -> p k", p=128))], slow=True)
    p.op('dve', [], ['m_onesb'], lambda E: E.memset(onesb[:, :], 1.0))
    for k in range(2):
        p.op('dve', ['m_w32', 'm_gn'], [('m_wkv', k)],
             lambda E, k=k: E.tensor_scalar(out=wkv[:, k, :], in0=wkv32[:, k, :], scalar1=gn[:, k:k + 1], scalar2=None,
                                            op0=ALU.mult))
    for k in range(3):
        p.op('dve', ['m_w32', 'm_gn'], [('m_wq', k)],
             lambda E, k=k: E.tensor_scalar(out=wq[:, k, :], in0=wq32[:, k, :], scalar1=gn[:, 2 + k:3 + k], scalar2=None,
                                            op0=ALU.mult))
    wkvk = [('m_wkv', k) for k in range(2)]
    wqk = [('m_wq', k) for k in range(3)]
    src = [sb(c, 'm_src%d' % i, [128, 3, 512], BF16) for i in range(2)]
    sq = [sb(c, 'm_sq%d' % i, [128, 3, 512], BF16) for i in range(2)]
    rs = [sb(c, 'm_rs%d' % i, [128, 2], F32) for i in range(2)]
    kr = [sb(c, 'm_kr%d' % i, [128, 32], F32) for i in range(2)]
    krr = [sb(c, 'm_krr%d' % i, [128, 32], F32) for i in range(2)]
    rtab = [sb(c, 'm_rtab%d' % i, [128, 32], F32) for i in range(2)]
    tmp16 = [sb(c, 'm_tmp%d' % i, [128, 16], F32) for i in range(2)]
    kp = [sb(c, 'm_kp%d' % i, [128, 8, 97], BF16) for i in range(2)]
    vp = [sb(c, 'm_vp%d' % i, [128, 8, 65], BF16) for i in range(2)]
    kpt = [sb(c, 'm_kpt%d' % i, [97, 8, 128], BF16) for i in range(2)]
    qf = [sb(c, 'm_qf%d' % i, [128, 8, 96], F32) for i in range(2)]
    qsq = sb(c, 'm_qsq', [128, 8, 96], F32)
    ks = sb(c, 'm_ks', [128, 8], F32)
    kmx = sb(c, 'm_kmx', [128, 8], F32)
    kmT = sb(c, 'm_kmT', [8, 1], F32)
    kdiag = sb(c, 'm_kdiag', [8, 8], F32)
    kbc = sb(c, 'm_kbc', [128, 8], F32)
    qn = [sb(c, 'm_qn%d' % i, [128, 8], F32) for i in range(2)]
    B = c.bank
    bk = lambda i: ('bank', i)
    for i in range(2):
        p.op('dve', [], [('m_kp', i)], lambda E, i=i: E.memset(kp[i][:, :, :], 1.0))
        p.op('dve', [], [('m_vp', i)], lambda E, i=i: E.memset(vp[i][:, :, :], 1.0))
    p.op('dve', [], ['m_kmx'], lambda E: E.memset(kmx[:, :], 0.0))

    blocks = [(0, 2)] + [(2 + 4 * i, 4) for i in range(16)]
    it = 0

    def load_src(name, nk, t0, ntl, sslot):
        T = ntl * 128
        p.dma('pool', 'm_src%d' % sslot, [], [('m_src', sslot)],
              [(src[sslot][:, 0:nk, 0:T], S[name][:, t0 * 128:t0 * 128 + T].rearrange("(k p) t -> p k t", p=128))])
        p.op('act', [('m_src', sslot)], [('m_sq', sslot)],
             lambda E: E.activation(out=sq[sslot][:, 0:nk, 0:T], in_=src[sslot][:, 0:nk, 0:T], func=AF.Square))

    def rope_rows(t, s2):
        p.dma('pool', 'm_rtab%d' % s2, [], [('m_rtab', s2)], [(rtab[s2][:, :], I['rope'][t * 128:(t + 1) * 128, :])])

    for bi, (tb0, ntl) in enumerate(blocks):
        sslot = bi % 2
        load_src('MKVAT', 2, tb0, ntl, sslot)
        for j in range(ntl):
            t = tb0 + j
            s2 = it % 2
            it += 1
            cols = slice(j * 128, (j + 1) * 128)

            def mmq(E, sslot=sslot, cols=cols):
                ins = None
                for k in range(2):
                    ins = E.matmul(B[0][:, 0:1], lhsT=sq[sslot][:, k, cols], rhs=onesb[:, 0:1], start=(k == 0), stop=(k == 1))
                return ins
            p.op('pe', [('m_sq', sslot), 'm_onesb'], [bk(0)], mmq)
            _rms_rstd(c, B[0][:, 0:1], rs[s2][:, 0:1], 256, bk(0), ('m_rs', s2))
            for half in range(2):
                def mmkv(E, sslot=sslot, cols=cols, half=half):
                    ins = None
                    for k in range(2):
                        ins = E.matmul(B[1 + half][:, :], lhsT=src[sslot][:, k, cols],
                                       rhs=wkv[:, k, half * 512:(half + 1) * 512], start=(k == 0), stop=(k == 1))
                    return ins
                p.op('pe', [('m_src', sslot)] + wkvk, [bk(1 + half)], mmkv)
            for half in range(2):
                pv = B[1 + half][:, :].rearrange("p (h e) -> p h e", h=4)
                p.op('dve', [bk(1 + half), ('m_rs', s2)], [('m_kp', s2)],
                     lambda E, pv=pv, half=half, s2=s2: E.tensor_scalar(out=kp[s2][:, half * 4:(half + 1) * 4, 0:64], in0=pv[:, :, 0:64],
                                                                        scalar1=rs[s2][:, 0:1], scalar2=None, op0=ALU.mult))
                p.op('act', [bk(1 + half), ('m_rs', s2)], [('m_vp', s2)],
                     lambda E, pv=pv, half=half, s2=s2: E.activation(out=vp[s2][:, half * 4:(half + 1) * 4, 0:64], in_=pv[:, :, 64:128],
                                                                     func=AF.Copy, scale=rs[s2][:, 0:1]))
            p.dma('pool', 'm_kr%d' % s2, [], [('m_kr', s2)], [(kr[s2][:, :], S['MKR'][t * 128:(t + 1) * 128, :])])
            if t >= 2:
                rope_rows(t - 2, s2)
                x1, x2 = kr[s2][:, 0:16], kr[s2][:, 16:32]
                cs, sn = rtab[s2][:, 0:16], rtab[s2][:, 16:32]
                o1, o2 = krr[s2][:, 0:16], krr[s2][:, 16:32]
                tm = tmp16[s2]
                rk = [('m_kr', s2), ('m_rtab', s2)]
                p.op('dve', rk, [('m_krr', s2)], lambda E, o1=o1, x1=x1, cs=cs: E.tensor_tensor(out=o1, in0=x1, in1=cs, op=ALU.mult))
                p.op('dve', rk, [('m_tmp', s2)], lambda E, tm=tm, x2=x2, sn=sn: E.tensor_tensor(out=tm[:, :], in0=x2, in1=sn, op=ALU.mult))
                p.op('dve', [('m_krr', s2), ('m_tmp', s2)], [('m_krr', s2)],
                     lambda E, o1=o1, tm=tm: E.tensor_tensor(out=o1, in0=o1, in1=tm[:, :], op=ALU.subtract))
                p.op('dve', rk + [('m_krr', s2)], [('m_krr', s2)], lambda E, o2=o2, x2=x2, cs=cs: E.tensor_tensor(out=o2, in0=x2, in1=cs, op=ALU.mult))
                p.op('dve', rk + [('m_tmp', s2)], [('m_tmp', s2)], lambda E, tm=tm, x1=x1, sn=sn: E.tensor_tensor(out=tm[:, :], in0=x1, in1=sn, op=ALU.mult))
                p.op('dve', [('m_krr', s2), ('m_tmp', s2)], [('m_krr', s2)],
                     lambda E, o2=o2, tm=tm: E.tensor_tensor(out=o2, in0=o2, in1=tm[:, :], op=ALU.add))
                rsrc, rkey = krr[s2], ('m_krr', s2)
            else:
                rsrc, rkey = kr[s2], ('m_kr', s2)
            p.op('dve', [rkey, ('m_kp', s2)], [('m_kp', s2)],
                 lambda E, rsrc=rsrc, s2=s2: E.tensor_copy(out=kp[s2][:, :, 64:96],
                                                           in_=rsrc[:, :].unsqueeze(1).to_broadcast([128, 8, 32])))
            p.op('dve', [('m_kp', s2)], ['m_qsq'],
                 lambda E, s2=s2: E.tensor_tensor(out=qsq[:, :, :], in0=kp[s2][:, :, 0:96], in1=kp[s2][:, :, 0:96], op=ALU.mult))
            p.op('dve', ['m_qsq'], ['m_ks'], lambda E: E.tensor_reduce(out=ks[:, :], in_=qsq[:, :, :], axis=AX.X, op=ALU.add))
            p.op('dve', ['m_ks', 'm_kmx'], ['m_kmx'], lambda E: E.tensor_tensor(out=kmx[:, :], in0=kmx[:, :], in1=ks[:, :], op=ALU.max))
            tpb = 3 + s2
            tpv = B[tpb][:, :].bitcast(BF16).rearrange("p (h t) -> p h t", h=8)

            def trk(E, s2=s2, tpv=tpv):
                ins = None
                for h in range(8):
                    ins = E.transpose(tpv[0:97, h, :], kp[s2][:, h, :], c.ident[:, :])
                return ins
            p.op('pe', [('m_kp', s2), 'ident'], [bk(tpb)], trk)
            p.op('act', [bk(tpb)], [('m_kpt', s2)],
                 lambda E, s2=s2, tpv=tpv: E.activation(out=kpt[s2][:, :, :], in_=tpv[0:97, :, :], func=AF.Copy))
            p.dma('sp', 'm_kpt%d' % s2, [('m_kpt', s2)], ['KpT'],
                  [(KpT[:, :, t * 128:(t + 1) * 128].rearrange("h d t -> d h t"), kpt[s2][:, :, :])])
            p.dma('sp', 'm_vp%d' % s2, [('m_vp', s2)], ['VpD'],
                  [(VpD[:, t * 128:(t + 1) * 128, :].rearrange("h p e -> p h e"), vp[s2][:, :, :])])
    p.op('pe', ['m_kmx', 'cst'], [bk(0)], lambda E: E.transpose(B[0][0:8, 0:128], kmx[:, :], cst[:, 0:128]))
    p.op('dve', [bk(0)], ['m_kmT'], lambda E: E.tensor_reduce(out=kmT[:, :], in_=B[0][0:8, 0:128], axis=AX.X, op=ALU.max))
    p.op('dve', ['m_kmT', 'cst'], ['m_kdiag'],
         lambda E: E.tensor_scalar(out=kdiag[:, :], in0=cst[0:8, 0:8], scalar1=kmT[:, 0:1], scalar2=None, op0=ALU.mult))
    p.op('pe', ['m_kdiag', 'cst'], [bk(0)],
         lambda E: E.matmul(B[0][:, 0:8], lhsT=ones32[0:8, :], rhs=kdiag[:, :], start=True, stop=True))
    p.op('act', [bk(0)], ['m_kbc'], lambda E: E.activation(out=kbc[:, :], in_=B[0][:, 0:8], func=AF.Copy))
    qblocks = ([(0, 2)] if ctx_q else []) + [(2 + 4 * i, 4) for i in range(16)]
    for bi, (tb0, ntl) in enumerate(qblocks):
        sslot = bi % 2
        load_src('MQAT', 3, tb0, ntl, sslot)
        for j in range(ntl):
            t = tb0 + j
            s2 = it % 2
            it += 1
            cols = slice(j * 128, (j + 1) * 128)

            def mmq(E, sslot=sslot, cols=cols):
                ins = None
                for k in range(3):
                    ins = E.matmul(B[0][:, 0:1], lhsT=sq[sslot][:, k, cols], rhs=onesb[:, 0:1], start=(k == 0), stop=(k == 2))
                return ins
            p.op('pe', [('m_sq', sslot), 'm_onesb'], [bk(0)], mmq)
            _rms_rstd(c, B[0][:, 0:1], rs[s2][:, 0:1], 384, bk(0), ('m_rs', s2))
            p.op('dve', [('m_rs', s2)], [('m_rs', s2)],
                 lambda E, s2=s2: E.tensor_scalar(out=rs[s2][:, 0:1], in0=rs[s2][:, 0:1], scalar1=MLA_SCALE, scalar2=None, op0=ALU.mult))
            for half, (n0, nn) in enumerate([(0, 512), (512, 256)]):
                def mmqq(E, sslot=sslot, cols=cols, half=half, n0=n0, nn=nn):
                    ins = None
                    for k in range(3):
                        ins = E.matmul(B[1 + half][:, 0:nn], lhsT=src[sslot][:, k, cols], rhs=wq[:, k, n0:n0 + nn],
                                       start=(k == 0), stop=(k == 2))
                    return ins
                p.op('pe', [('m_src', sslot)] + wqk, [bk(1 + half)], mmqq)
            qv = qf[s2][:, :, :].rearrange("p h e -> p (h e)")
            p.op('dve', [bk(1), ('m_rs', s2)], [('m_qf', s2, 0)],
                 lambda E, qv=qv, s2=s2: E.tensor_scalar(out=qv[:, 0:512], in0=B[1][:, 0:512], scalar1=rs[s2][:, 0:1], scalar2=None, op0=ALU.mult))
            p.op('act', [bk(2), ('m_rs', s2)], [('m_qf', s2, 1)],
                 lambda E, qv=qv, s2=s2: E.activation(out=qv[:, 512:768], in_=B[2][:, 0:256], func=AF.Copy, scale=rs[s2][:, 0:1]))
            qk = [('m_qf', s2, 0), ('m_qf', s2, 1)]
            kpq = kp[s2]
            if t >= 2:
                rope_rows(t - 2, s2)
                x1, x2 = qf[s2][:, :, 64:80], qf[s2][:, :, 80:96]
                cs = rtab[s2][:, 0:16].unsqueeze(1).to_broadcast([128, 8, 16])
                sn = rtab[s2][:, 16:32].unsqueeze(1).to_broadcast([128, 8, 16])
                ta, tb_ = qsq[:, :, 0:16], qsq[:, :, 16:32]
                tc_, td = qsq[:, :, 32:48], qsq[:, :, 48:64]
                rk = qk + [('m_rtab', s2)]
                p.op('dve', rk, ['m_qsq'], lambda E, ta=ta, x1=x1, cs=cs: E.tensor_tensor(out=ta, in0=x1, in1=cs, op=ALU.mult))
                p.op('dve', rk + ['m_qsq'], ['m_qsq'], lambda E, tb_=tb_, x2=x2, sn=sn: E.tensor_tensor(out=tb_, in0=x2, in1=sn, op=ALU.mult))
                p.op('dve', rk + ['m_qsq'], ['m_qsq'], lambda E, tc_=tc_, x2=x2, cs=cs: E.tensor_tensor(out=tc_, in0=x2, in1=cs, op=ALU.mult))
                p.op('dve', rk + ['m_qsq'], ['m_qsq'], lambda E, td=td, x1=x1, sn=sn: E.tensor_tensor(out=td, in0=x1, in1=sn, op=ALU.mult))
                p.op('dve', ['m_qsq'] + qk, qk, lambda E, x1=x1, ta=ta, tb_=tb_: E.tensor_tensor(out=x1, in0=ta, in1=tb_, op=ALU.subtract))
                p.op('dve', ['m_qsq'] + qk, qk, lambda E, x2=x2, tc_=tc_, td=td: E.tensor_tensor(out=x2, in0=tc_, in1=td, op=ALU.add))
            p.op('dve', qk, ['m_qsq'],
                 lambda E, s2=s2: E.tensor_tensor(out=qsq[:, :, :], in0=qf[s2][:, :, :], in1=qf[s2][:, :, :], op=ALU.mult))
            p.op('dve', ['m_qsq'], [('m_qn', s2)], lambda E, s2=s2: E.tensor_reduce(out=qn[s2][:, :], in_=qsq[:, :, :], axis=AX.X, op=ALU.add))
            p.op('dve', [('m_qn', s2), 'm_kbc'], [('m_qn', s2)],
                 lambda E, s2=s2: E.tensor_tensor(out=qn[s2][:, :], in0=qn[s2][:, :], in1=kbc[:, :], op=ALU.mult))
            p.op('act', [('m_qn', s2)], [('m_qn', s2)], lambda E, s2=s2: E.activation(out=qn[s2][:, :], in_=qn[s2][:, :], func=AF.Sqrt))
            p.op('dve', qk + [('m_kp', s2)], [('m_kp', s2)],
                 lambda E, s2=s2: E.tensor_copy(out=kpq[:, :, 0:96], in_=qf[s2][:, :, :]))
            p.op('dve', [('m_qn', s2), ('m_kp', s2)], [('m_kp', s2)],
                 lambda E, s2=s2: E.tensor_scalar(out=kpq[:, :, 96:97], in0=qn[s2][:, :].unsqueeze(2), scalar1=-1.0, scalar2=None, op0=ALU.mult))
            tpb = 3 + s2
            tpv = B[tpb][:, :].bitcast(BF16).rearrange("p (h t) -> p h t", h=8)

            def trq(E, s2=s2, tpv=tpv):
                ins = None
                for h in range(8):
                    ins = E.transpose(tpv[0:97, h, :], kp[s2][:, h, :], c.ident[:, :])
                return ins
            p.op('pe', [('m_kp', s2), 'ident'], [bk(tpb)], trq)
            p.op('act', [bk(tpb)], [('m_kpt', s2)],
                 lambda E, s2=s2, tpv=tpv: E.activation(out=kpt[s2][:, :, :], in_=tpv[0:97, :, :], func=AF.Copy))
            p.dma('sp', 'm_kpt%d' % s2, [('m_kpt', s2)], ['QpT'],
                  [(QpT[:, :, t * 128:(t + 1) * 128].rearrange("h d t -> d h t"), kpt[s2][:, :, :])])
    p.barrier()
    kh = [sb(c, 'm_kh%d' % i, [97, NT], BF16) for i in range(2)]
    qh = [sb(c, 'm_qh%d' % i, [97, NT], BF16) for i in range(2)]
    vh = [sb(c, 'm_vh%d' % i, [128, NTILE, 65], BF16) for i in range(2)]
    pt = [sb(c, 'm_pt%d' % i, [128, 512], BF16) for i in range(3)]
    osb = [sb(c, 'm_osb%d' % i, [65, 512], F32) for i in range(2)]
    on = [sb(c, 'm_on%d' % i, [64, 512], BF16) for i in range(2)]
    ei = 0
    ci = 0
    for h in range(8):
        hs = h % 2
        p.dma('sp', 'm_kh%d' % hs, ['KpT'], [('m_kh', hs)], [(kh[hs][:, :], KpT[h, :, :])])
        p.dma('sp', 'm_qh%d' % hs, ['QpT'], [('m_qh', hs)], [(qh[hs][:, :], QpT[h, :, :])])
        p.dma('sp', 'm_vh%d' % hs, ['VpD'], [('m_vh', hs)],
              [(vh[hs][:, :, :], VpD[h, :, :].rearrange("(n p) e -> p n e", p=128))])
        chunks = ([(0, 256, 2)] if ctx_q else []) + [(256 + 512 * i, 512, NTILE) for i in range(16)]
        for (q0, nq, nkt) in chunks:
            ob = 6 + ci % 2
            cs2 = ci % 2
            ci += 1
            def emit_qk(kt):
                sbk = (ei0 + kt) % 3
                p.op('pe', [('m_kh', hs), ('m_qh', hs)], [bk(sbk)],
                     lambda E: E.matmul(B[sbk][:, 0:nq], lhsT=kh[hs][:, kt * 128:(kt + 1) * 128],
                                        rhs=qh[hs][:, q0:q0 + nq], start=True, stop=True))
            ei0 = ei
            ei += nkt
            for kt in range(min(2, nkt)):
                emit_qk(kt)
            for kt in range(nkt):
                sbk = (ei0 + kt) % 3
                p.op('act', [bk(sbk)], [('m_pt', sbk)],
                     lambda E: E.activation(out=pt[sbk][:, 0:nq], in_=B[sbk][:, 0:nq], func=AF.Exp))
                if kt + 2 < nkt:
                    emit_qk(kt + 2)
                p.op('pe', [('m_vh', hs), ('m_pt', sbk)], [bk(ob)],
                     lambda E: E.matmul(B[ob][0:65, 0:nq], lhsT=vh[hs][:, kt, :], rhs=pt[sbk][:, 0:nq],
                                        start=(kt == 0), stop=(kt == nkt - 1)))
            o_t = osb[cs2]
            p.op('dve', [bk(ob)], [('m_osb', cs2)], lambda E, o_t=o_t, ob=ob, nq=nq: E.tensor_copy(out=o_t[:, 0:nq], in_=B[ob][0:65, 0:nq]))
            p.op('dve', [('m_osb', cs2)], [('m_osb', cs2)],
                 lambda E, o_t=o_t, nq=nq: E.reciprocal(out=o_t[64:65, 0:nq], in_=o_t[64:65, 0:nq]))
            p.op('pe', [('m_osb', cs2), 'cst'], [bk(5)],
                 lambda E, o_t=o_t, nq=nq: E.matmul(B[5][0:64, 0:nq], lhsT=ones32[64:65, 0:64], rhs=o_t[64:65, 0:nq], start=True, stop=True))
            p.op('dve', [bk(5), ('m_osb', cs2)], [('m_on', cs2)],
                 lambda E, o_t=o_t, nq=nq, cs2=cs2: E.tensor_tensor(out=on[cs2][:, 0:nq], in0=o_t[0:64, 0:nq], in1=B[5][0:64, 0:nq], op=ALU.mult))
            p.dma('sp', 'm_on%d' % cs2, [('m_on', cs2)], [('OMT', l)],
                  [(S['OMT'][h * 64:(h + 1) * 64, q0:q0 + nq], on[cs2][:, 0:nq])])


NFFT = 16384


def hy_conv3(c, l):
    p = c.p
    I = c.inp
    S = c.scr[l]
    with ExitStack() as es:
        c.es = es
        wb32 = sb(c, 'h3_w32', [64, 4, 1536], F32)
        wb = sb(c, 'h3_w', [64, 4, 1536], BF16)
        zin = [sb(c, 'h3_zin%d' % i, [64, 10, 512], BF16) for i in range(2)]
        t0_ = [sb(c, 'h3_t0%d' % i, [64, 8, 512], BF16) for i in range(2)]
        t1_ = [sb(c, 'h3_t1%d' % i, [64, 8, 512], BF16) for i in range(2)]
        zo = [sb(c, 'h3_zo%d' % i, [64, 8, 512], BF16) for i in range(2)]
        p.dma('sp', 'h3_w', [], ['h3_w32'],
              [(wb32[:, k, :], I['hy_conv_w'][l, k, :].partition_broadcast(64)) for k in range(3)] +
              [(wb32[:, 3, :], I['hy_conv_b'][l, :].partition_broadcast(64))])
        p.op('dve', ['h3_w32'], ['h3_w'], lambda E: E.tensor_copy(out=wb[:, :, :], in_=wb32[:, :, :]))
        its = [(tok0, na, cs, bc) for (tok0, na) in ((0, 2), (NCTX, 64)) for cs in range(3) for bc in range(16)]

        def c3_load(it):
            tok0, na, cs, bc = its[it]
            src = S['HY'][tok0:tok0 + na * 128, :].rearrange("(a b) c -> a b c", b=128)
            c0 = cs * 512
            b0 = bc * 8
            s2 = it % 2
            z = zin[s2]
            pairs = []
            pre = []
            lo, hi = b0 - 1, b0 + 9
            if bc == 0:
                pre.append(lambda E, z=z: E.memset(z[0:1, 0:1, :], 0.0))
                if na > 1:
                    pairs.append((z[1:na, 0:1, :], src[0:na - 1, 127:128, c0:c0 + 512]))
                pairs.append((z[0:na, 1:10, :], src[0:na, 0:9, c0:c0 + 512]))
            elif bc == 15:
                pre.append(lambda E, z=z, na=na: E.memset(z[0:na, 9:10, :], 0.0))
                if na > 1:
                    pairs.append((z[0:na - 1, 9:10, :], src[1:na, 0:1, c0:c0 + 512]))
                pairs.append((z[0:na, 0:9, :], src[0:na, lo:128, c0:c0 + 512]))
            else:
                pairs.append((z[0:na, 0:10, :], src[0:na, lo:hi, c0:c0 + 512]))
            for f in pre:
                p.op('pool', [], [('h3_zin', s2)], f)
            p.dma('sp', 'h3_zin%d' % s2, [('HY', l)], [('h3_zin', s2)], pairs)

        c3_load(0)
        for it in range(len(its)):
            tok0, na, cs, bc = its[it]
            dst = c.HYC[tok0:tok0 + na * 128, :].rearrange("(a b) c -> a b c", b=128)
            c0 = cs * 512
            b0 = bc * 8
            s2 = it % 2
            z = zin[s2]
            if it + 1 < len(its):
                c3_load(it + 1)
            w = lambda k, c0=c0, na=na: wb[0:na, k, c0:c0 + 512].unsqueeze(1).to_broadcast([na, 8, 512])
            a0, a1, oz = t0_[s2], t1_[s2], zo[s2]
            zk = ('h3_zin', s2)
            p.op('dve', [zk, 'h3_w'], [('h3_t0', s2)],
                 lambda E: E.tensor_tensor(out=a0[0:na], in0=z[0:na, 0:8, :], in1=w(0), op=ALU.mult))
            p.op('pool', [zk, 'h3_w'], [('h3_t1', s2)],
                 lambda E: E.tensor_tensor(out=a1[0:na], in0=z[0:na, 1:9, :], in1=w(1), op=ALU.mult))
            p.op('dve', [zk, 'h3_w'], [('h3_zo', s2)],
                 lambda E: E.tensor_tensor(out=oz[0:na], in0=z[0:na, 2:10, :], in1=w(2), op=ALU.mult))
            p.op('dve', [('h3_t0', s2), 'h3_w'], [('h3_t0', s2)],
                 lambda E: E.tensor_tensor(out=a0[0:na], in0=a0[0:na], in1=w(3), op=ALU.add))
            p.op('dve', [('h3_t0', s2), ('h3_zo', s2)], [('h3_zo', s2)],
                 lambda E: E.tensor_tensor(out=oz[0:na], in0=a0[0:na], in1=oz[0:na], op=ALU.add))
            p.op('dve', [('h3_t1', s2), ('h3_zo', s2)], [('h3_zo', s2)],
                 lambda E: E.tensor_tensor(out=oz[0:na], in0=a1[0:na], in1=oz[0:na], op=ALU.add))
            p.dma('sp', 'h3_zo%d' % s2, [('h3_zo', s2)], ['HYC'],
                  [(dst[0:na, b0:b0 + 8, c0:c0 + 512], oz[0:na, :, :])])
        p.barrier()


def hy_filters(c, l, job):
    p = c.p
    I = c.inp
    nt = 128 if job == 0 else 4
    feat = I['hy_feat%d' % job]
    tvec = I['hy_tvec%d' % job]
    KTD = c.KTD[job]
    B = c.bank
    bk = lambda i: ('bank', i)
    with ExitStack() as es:
        c.es = es
        w1 = sb(c, 'hf_w1', [33, 64], F32)
        w2 = sb(c, 'hf_w2', [64, 64], F32)
        w3 = sb(c, 'hf_w3', [64, 2048], F32)
        pb = sb(c, 'hf_pb', [64, 8], F32)
        ft = [sb(c, 'hf_ft%d' % i, [33, 512], F32) for i in range(2)]
        tv = [sb(c, 'hf_tv%d' % i, [1, 512], F32) for i in range(2)]
        u = [sb(c, 'hf_u%d' % i, [64, 512], F32) for i in range(2)]
        ui = [sb(c, 'hf_ui%d' % i, [64, 512], mybir.dt.int32) for i in range(2)]
        uf = [sb(c, 'hf_uf%d' % i, [64, 512], F32) for i in range(2)]
        h1 = [sb(c, 'hf_h1%d' % i, [64, 512], F32) for i in range(2)]
        h2 = [sb(c, 'hf_h2%d' % i, [64, 512], F32) for i in range(2)]
        dec = [sb(c, 'hf_dec%d' % i, [128, 512], F32) for i in range(2)]
        hd = [sb(c, 'hf_hd%d' % i, [128, 2, 512], F32) for i in range(2)]
        ha = [sb(c, 'hf_ha%d' % i, [128, 2, 512], F32) for i in range(2)]
        hb = [sb(c, 'hf_hb%d' % i, [128, 2, 512], BF16) for i in range(2)]
        nd = sb(c, 'hf_nd', [1, 512], F32)
        l1 = sb(c, 'hf_l1', [1, 1024], F32)
        cst = c.cst
        ones = cst[:, 640:768]
        p.dma('sp', 'hf_w', [], ['hf_w'],
              [(w1[:, :], I['hy_w1'][l]), (w2[:, :], I['hy_w2'][l]), (w3[:, :], I['hy_w3'][l]),
               (nd[:, :], I['hy_negdelta'][0:1, :])])
        p.dma('sp', 'hf_pb', [], ['hf_pb'],
              [(pb[:, 0:1], I['hy_b1'][l, :].rearrange("(p o) -> p o", o=1)),
               (pb[:, 1:2], I['hy_b2'][l, :].rearrange("(p o) -> p o", o=1)),
               (pb[:, 2:3], I['hy_freq'][l, :].rearrange("(p o) -> p o", o=1))], slow=True)
        p.op('dve', ['hf_pb'], ['hf_pb2'],
             lambda E: E.tensor_scalar(out=pb[:, 3:4], in0=pb[:, 2:3], scalar1=1.0 / (2 * math.pi), scalar2=None, op0=ALU.mult))
        p.op('dve', ['hf_pb', 'hf_pb2'], ['hf_pb3'],
             lambda E: E.tensor_scalar(out=pb[:, 4:6], in0=pb[:, 0:2], scalar1=pb[:, 3:4], scalar2=None, op0=ALU.mult))
        pbk = ['hf_pb', 'hf_pb2', 'hf_pb3']

        def sin_layer(src_ps, srck, bcol, dst, dstk, s2):
            p.op('dve', [srck] + pbk, [('hf_u', s2)],
                 lambda E: E.tensor_scalar(out=u[s2][:, :], in0=src_ps, scalar1=pb[:, 3:4], scalar2=pb[:, bcol:bcol + 1],
                                           op0=ALU.mult, op1=ALU.add))
            p.op('dve', [('hf_u', s2)], [('hf_ui', s2)], lambda E: E.tensor_copy(out=ui[s2][:, :], in_=u[s2][:, :]))
            p.op('dve', [('hf_ui', s2)], [('hf_uf', s2)], lambda E: E.tensor_copy(out=uf[s2][:, :], in_=ui[s2][:, :]))
            p.op('dve', [('hf_u', s2), ('hf_uf', s2)], [('hf_u', s2)],
                 lambda E: E.tensor_tensor(out=u[s2][:, :], in0=u[s2][:, :], in1=uf[s2][:, :], op=ALU.subtract))
            p.op('act', [('hf_u', s2)], [dstk], lambda E: E.activation(out=dst, in_=u[s2][:, :], func=AF.Sin, scale=2 * math.pi))

        nchunk = nt // 4
        first_bwd_tile = 64 if job == 0 else 2
        for ch in range(nchunk):
            s2 = ch % 2
            p.dma('sp', 'hf_ft%d' % s2, [], [('hf_ft', s2)],
                  [(ft[s2][:, :], feat[:, ch * 512:(ch + 1) * 512]), (tv[s2][:, :], tvec[:, ch * 512:(ch + 1) * 512])])
            p.op('pe', [('hf_ft', s2), 'hf_w'], [bk(0)],
                 lambda E, s2=s2: E.matmul(B[0][0:64, :], lhsT=w1[:, :], rhs=ft[s2][:, :], start=True, stop=True))
            sin_layer(B[0][0:64, :], bk(0), 4, h1[s2][:, :], ('hf_h1', s2), s2)
            p.op('pe', [('hf_h1', s2), 'hf_w'], [bk(1)],
                 lambda E, s2=s2: E.matmul(B[1][0:64, :], lhsT=w2[:, :], rhs=h1[s2][:, :], start=True, stop=True))
            sin_layer(B[1][0:64, :], bk(1), 5, h2[s2][:, :], ('hf_h2', s2), s2)
            for j in range(4):
                tile = ch * 4 + j
                d = 0 if tile < first_bwd_tile else 1
                j2 = tile % 2
                cols = slice(j * 128, (j + 1) * 128)
                p.op('pe', [('hf_ft', s2), 'hf_w'], [bk(2)],
                     lambda E, s2=s2, cols=cols: E.matmul(B[2][:, :], lhsT=tv[s2][0:1, cols], rhs=nd[0:1, :], start=True, stop=True))
                p.op('act', [bk(2)], [('hf_dec', j2)], lambda E, j2=j2: E.activation(out=dec[j2][:, :], in_=B[2][:, :], func=AF.Exp))
                for o in range(2):
                    c0 = o * 1024 + d * 512
                    p.op('pe', [('hf_h2', s2), 'hf_w'], [bk(3 + o)],
                         lambda E, s2=s2, cols=cols, c0=c0, o=o: E.matmul(B[3 + o][:, :], lhsT=h2[s2][:, cols], rhs=w3[:, c0:c0 + 512],
                                                                        start=True, stop=True))
                    p.op('dve', [bk(3 + o), ('hf_dec', j2)], [('hf_hd', j2, o)],
                         lambda E, j2=j2, o=o: E.tensor_tensor(out=hd[j2][:, o, :], in0=B[3 + o][:, :], in1=dec[j2][:, :], op=ALU.mult))
                p.op('act', [('hf_hd', j2, 0), ('hf_hd', j2, 1)], [('hf_ha', j2)],
                     lambda E, j2=j2: E.activation(out=ha[j2][:, :, :], in_=hd[j2][:, :, :], func=AF.Abs))
                for o in range(2):
                    p.op('pe', [('hf_ha', j2), 'cst'], [bk(5 + o)],
                         lambda E, j2=j2, o=o, tile=tile: E.matmul(B[5 + o][0:1, :], lhsT=ones[:, 0:1], rhs=ha[j2][:, o, :],
                                                                   start=(tile == 0), stop=(tile == nt - 1)))
                p.op('pool', [('hf_hd', j2, 0), ('hf_hd', j2, 1)], [('hf_hb', j2)],
                     lambda E, j2=j2: E.tensor_copy(out=hb[j2][:, :, :], in_=hd[j2][:, :, :]))
                if tile == first_bwd_tile:
                    p.op('pool', [('hf_hb', j2)], [('hf_hb', j2)], lambda E, j2=j2: E.memset(hb[j2][0:1, :, :], 0.0))
                p.dma('sp', 'hf_hb%d' % j2, [('hf_hb', j2)], [('KTD', job)],
                      [(KTD[tile * 128:(tile + 1) * 128, :].rearrange("p (o c) -> p o c", o=2), hb[j2][:, :, :])])
        for o in range(2):
            p.op('dve', [bk(5 + o)], ['hf_l1'],
                 lambda E, o=o: E.tensor_scalar(out=l1[0:1, o * 512:(o + 1) * 512], in0=B[5 + o][0:1, :], scalar1=float(NFFT if job == 0 else 512), scalar2=None,
                                                op0=ALU.mult))
        p.op('dve', ['hf_l1'], ['hf_l1'], lambda E: E.reciprocal(out=l1[:, :], in_=l1[:, :]))
        p.dma('sp', 'hf_l1', ['hf_l1'], [('SCL', job)], [(c.SCL[job][:, :], l1[:, :])])
        p.barrier()


def hy_fwd1(c, src, K, tab, X1D, NF1):
    p = c.p
    B = c.bank
    bk = lambda i: ('bank', i)
    zt = c.hy_zt
    xo = c.hy_xo
    for bc in range(16):
        s2 = bc % 2
        p.dma('pool', 'hy_zt%d' % s2, ['HYC', 'Z2', ('KTD', 0), ('KTD', 1)], [('hy_zt', s2)], [(zt[s2][0:K, :, :], src(bc * 8, 8))])
        for j in range(8):
            b = bc * 8 + j
            e2 = b % 2
            for ri in range(2):
                p.op('pe', [('hy_zt', s2), 'hy_tab'], [bk(e2 * 2 + ri)],
                     lambda E, s2=s2, j=j, ri=ri, e2=e2: E.matmul(B[e2 * 2 + ri][0:NF1, :], lhsT=tab[0:K, ri, 0:NF1], rhs=zt[s2][0:K, j, :],
                                                                  start=True, stop=True))
            p.op('act', [bk(e2 * 2)], [('hy_xo', e2, 0)],
                 lambda E, e2=e2: E.activation(out=xo[e2][0:NF1, 0, :], in_=B[e2 * 2][0:NF1, :], func=AF.Copy))
            p.op('dve', [bk(e2 * 2 + 1)], [('hy_xo', e2, 1)],
                 lambda E, e2=e2: E.tensor_copy(out=xo[e2][0:NF1, 1, :], in_=B[e2 * 2 + 1][0:NF1, :]))
            p.dma('sp', 'hy_xo%d' % e2, [('hy_xo', e2, 0), ('hy_xo', e2, 1)], ['X1D'],
                  [(X1D[b, 0:NF1, :, :], xo[e2][0:NF1, :, :])])


def hy_stage2(c, X1D, mode, KS, QD, NF1=128, tw2name='hy_tw2', twres=None):
    p = c.p
    I = c.inp
    B = c.bank
    bk = lambda i: ('bank', i)
    xin, tw, ksb, pr, t4, qo = c.hy_xin, c.hy_tw, c.hy_ksb, c.hy_pr, c.hy_t4, c.hy_qo
    E3 = c.hy_E3

    def front(f1):
        s2 = f1 % 2
        p.dma('sp', 'hy_xin%d' % s2, ['X1D'], [('hy_xin', s2)],
              [(xin[s2][:, :, :], X1D[:, f1, :, :])])
        if twres is None:
            p.dma('sp', 'hy_tw%d' % s2, [], [('hy_tw', s2)], [(tw[s2][:, :, :], I[tw2name][f1])])
            twv, twk = tw[s2], ('hy_tw', s2)
        else:
            twv, twk = twres[:, f1, :, :], 'hy_twres'
        if mode != 'filter':
            p.dma('sp', 'hy_ksb%d' % s2, ['KS'], [('hy_ksb', s2)], [(ksb[s2][:, :, :], KS[:, f1, :, :])])
        zr, zi = s2 * 2, s2 * 2 + 1

        def mmz(E):
            E.matmul(B[zr][:, :], lhsT=twv[:, 0, :], rhs=xin[s2][:, 0, :], start=True, stop=False)
            E.matmul(B[zr][:, :], lhsT=twv[:, 2, :], rhs=xin[s2][:, 1, :], start=False, stop=True)
            E.matmul(B[zi][:, :], lhsT=twv[:, 0, :], rhs=xin[s2][:, 1, :], start=True, stop=False)
            return E.matmul(B[zi][:, :], lhsT=twv[:, 1, :], rhs=xin[s2][:, 0, :], start=False, stop=True)
        p.op('pe', [('hy_xin', s2), twk], [bk(zr), bk(zi)], mmz)

    front(0)
    for f1 in range(NF1):
        s2 = f1 % 2
        zr, zi = s2 * 2, s2 * 2 + 1
        if mode == 'filter':
            p.op('act', [bk(zr)], [('hy_pr', s2, 0)], lambda E: E.activation(out=pr[s2][:, 0, :], in_=B[zr][:, :], func=AF.Copy))
            p.op('dve', [bk(zi)], [('hy_pr', s2, 1)], lambda E: E.tensor_copy(out=pr[s2][:, 1, :], in_=B[zi][:, :]))
            if f1 + 1 < NF1:
                front(f1 + 1)
            p.dma('sp', 'hy_pr%d' % s2, [('hy_pr', s2, 0), ('hy_pr', s2, 1)], ['KS'],
                  [(KS[:, f1, :, :], pr[s2][:, :, :])])
            continue
        kk = ('hy_ksb', s2)
        tt = t4[s2]
        p.op('dve', [bk(zr), kk], [('hy_t4', s2, 0)], lambda E: E.tensor_tensor(out=tt[:, 0, :], in0=B[zr][:, :], in1=ksb[s2][:, 0, :], op=ALU.mult))
        p.op('dve', [bk(zi), kk], [('hy_t4', s2, 1)], lambda E: E.tensor_tensor(out=tt[:, 1, :], in0=B[zi][:, :], in1=ksb[s2][:, 1, :], op=ALU.mult))
        p.op('dve', [bk(zr), kk], [('hy_t4', s2, 2)], lambda E: E.tensor_tensor(out=tt[:, 2, :], in0=B[zr][:, :], in1=ksb[s2][:, 1, :], op=ALU.mult))
        p.op('dve', [bk(zi), kk], [('hy_t4', s2, 3)], lambda E: E.tensor_tensor(out=tt[:, 3, :], in0=B[zi][:, :], in1=ksb[s2][:, 0, :], op=ALU.mult))
        if f1 + 1 < NF1:
            front(f1 + 1)
        p.op('pool', [('hy_t4', s2, 0), ('hy_t4', s2, 1)], [('hy_pr', s2, 0)],
             lambda E: E.tensor_tensor(out=pr[s2][:, 0, :], in0=tt[:, 0, :], in1=tt[:, 1, :], op=ALU.subtract))
        p.op('pool', [('hy_t4', s2, 2), ('hy_t4', s2, 3)], [('hy_pr', s2, 1)],
             lambda E: E.tensor_tensor(out=pr[s2][:, 1, :], in0=tt[:, 2, :], in1=tt[:, 3, :], op=ALU.add))

        def mmq(E):
            E.matmul(B[4][:, :], lhsT=E3[:, 0, :], rhs=pr[s2][:, 0, :], start=True, stop=False)
            E.matmul(B[4][:, :], lhsT=E3[:, 2, :], rhs=pr[s2][:, 1, :], start=False, stop=True)
            E.matmul(B[5][:, :], lhsT=E3[:, 1, :], rhs=pr[s2][:, 0, :], start=True, stop=False)
            return E.matmul(B[5][:, :], lhsT=E3[:, 0, :], rhs=pr[s2][:, 1, :], start=False, stop=True)
        p.op('pe', [('hy_pr', s2, 0), ('hy_pr', s2, 1), 'hy_tab'], [bk(4), bk(5)], mmq)
        p.op('act', [bk(4)], [('hy_qo', s2, 0)], lambda E: E.activation(out=qo[s2][:, 0, :], in_=B[4][:, :], func=AF.Copy))
        p.op('act', [bk(5)], [('hy_qo', s2, 1)], lambda E: E.activation(out=qo[s2][:, 1, :], in_=B[5][:, :], func=AF.Copy))
        p.dma('sp', 'hy_qo%d' % s2, [('hy_qo', s2, 0), ('hy_qo', s2, 1)], ['QD'],
              [(QD[:, f1, :, :], qo[s2][:, :, :])])


def hy_final(c, QD, na, scl, bias, zsrc, gsrc, dst, NF1=128, twfname='hy_twf', Mm=64):
    p = c.p
    I = c.inp
    B = c.bank
    bk = lambda i: ('bank', i)
    qin, twf, zg, ya, yb, yo = c.hy_qin, c.hy_twf, c.hy_zg, c.hy_ya, c.hy_yb, c.hy_yo
    def loads(b):
        s2 = b % 2
        p.dma('sp', 'hy_qin%d' % s2, ['QD'], [('hy_qin', s2)], [(qin[s2][0:NF1, :, :], QD[b, 0:NF1, :, :])])
        p.dma('sp', 'hy_twf%d' % s2, [], [('hy_twf', s2)], [(twf[s2][0:NF1, :, 0:Mm], I[twfname][b])])
        p.dma('sp', 'hy_zg%d' % s2, ['HYC', 'Z2'], [('hy_zg', s2)],
              [(zg[s2][0:na, 0, :], zsrc[:, b, :]), (zg[s2][0:na, 1, :], gsrc[:, b, :])])

    loads(0)
    for b in range(128):
        s2 = b % 2
        if b + 1 < 128:
            loads(b + 1)

        def mmy(E, s2=s2):
            E.matmul(B[s2][0:Mm, :], lhsT=twf[s2][0:NF1, 0, 0:Mm], rhs=qin[s2][0:NF1, 0, :], start=True, stop=False)
            return E.matmul(B[s2][0:Mm, :], lhsT=twf[s2][0:NF1, 1, 0:Mm], rhs=qin[s2][0:NF1, 1, :], start=False, stop=True)
        p.op('pe', [('hy_qin', s2), ('hy_twf', s2)], [bk(s2)], mmy)
        p.op('dve', [bk(s2), 'hy_scl'], [('hy_ya', s2)],
             lambda E, s2=s2: E.tensor_tensor(out=ya[s2][0:na, :], in0=B[s2][0:na, :], in1=scl[0:na, :], op=ALU.mult))
        p.op('pool', [('hy_zg', s2), 'hy_scl'], [('hy_yb', s2)],
             lambda E, s2=s2: E.tensor_tensor(out=yb[s2][0:na, :], in0=zg[s2][0:na, 0, :], in1=bias[0:na, :], op=ALU.mult))
        p.op('pool', [('hy_ya', s2), ('hy_yb', s2)], [('hy_ya', s2)],
             lambda E, s2=s2: E.tensor_tensor(out=ya[s2][0:na, :], in0=ya[s2][0:na, :], in1=yb[s2][0:na, :], op=ALU.add))
        p.op('dve', [('hy_ya', s2), ('hy_zg', s2)], [('hy_yo', s2)],
             lambda E, s2=s2: E.tensor_tensor(out=yo[s2][0:na, :], in0=ya[s2][0:na, :], in1=zg[s2][0:na, 1, :], op=ALU.mult))
        p.dma('sp', 'hy_yo%d' % s2, [('hy_yo', s2)], ['Z2', 'OH'], [(dst[:, b, :], yo[s2][0:na, :])])


def stage_hyena(c, l, with_ctx):
    p = c.p
    I = c.inp
    hy_conv3(c, l)
    jobs = [0, 1] if with_ctx else [0]
    hp = c.cfg.get('hy_parts', 9)
    if hp < 1:
        return
    for job in jobs:
        hy_filters(c, l, job)
    if hp < 2:
        return
    with ExitStack() as es:
        c.es = es
        c.hy_zt = [sb(c, 'hy_zt%d' % i, [128, 8, 512], BF16) for i in range(2)]
        c.hy_xo = [sb(c, 'hy_xo%d' % i, [128, 2, 512], BF16) for i in range(2)]
        c.hy_xin = [sb(c, 'hy_xin%d' % i, [128, 2, 512], BF16) for i in range(2)]
        c.hy_tw = [sb(c, 'hy_tw%d' % i, [128, 3, 128], BF16) for i in range(2)]
        c.hy_ksb = [sb(c, 'hy_ksb%d' % i, [128, 2, 512], BF16) for i in range(2)]
        c.hy_pr = [sb(c, 'hy_pr%d' % i, [128, 2, 512], BF16) for i in range(2)]
        c.hy_t4 = [sb(c, 'hy_t4%d' % i, [128, 4, 512], F32) for i in range(2)]
        c.hy_qo = [sb(c, 'hy_qo%d' % i, [128, 2, 512], BF16) for i in range(2)]
        c.hy_qin = [sb(c, 'hy_qin%d' % i, [128, 2, 512], BF16) for i in range(2)]
        c.hy_twf = [sb(c, 'hy_twf%d' % i, [128, 2, 64], BF16) for i in range(2)]
        c.hy_zg = [sb(c, 'hy_zg%d' % i, [64, 2, 512], BF16) for i in range(2)]
        c.hy_ya = [sb(c, 'hy_ya%d' % i, [64, 512], F32) for i in range(2)]
        c.hy_yb = [sb(c, 'hy_yb%d' % i, [64, 512], F32) for i in range(2)]
        c.hy_yo = [sb(c, 'hy_yo%d' % i, [64, 512], BF16) for i in range(2)]
        tabs = sb(c, 'hy_tabs', [128, 2, 2, 128], BF16)
        c.hy_E3 = sb(c, 'hy_E3', [128, 3, 128], BF16)
        scl = sb(c, 'hy_scl', [64, 2, 512], F32)
        bias = sb(c, 'hy_bias', [64, 2, 512], F32)
        p.dma('sp', 'hy_tab', [], ['hy_tab'],
              [(tabs[:, :, :, :], I['hy_dft1'][:, :, :, :]), (c.hy_E3[:, :, :], I['hy_e3'][:, :, :])])
        twres = sb(c, 'hy_twres', [128, 128, 3, 128], BF16)
        p.dma('sp', 'hy_twres', [], ['hy_twres'],
              [(twres[:, g * 16:(g + 1) * 16, :, :], I['hy_tw2'][g * 16:(g + 1) * 16].rearrange("f b k g -> b f k g")) for g in range(8)])
        for job in jobs:
            na = 64 if job == 0 else 2
            tok0 = NCTX if job == 0 else 0
            nt = 128 if job == 0 else 4
            ftab = tabs[:, job, :, :]
            NF1 = 128 if job == 0 else 4
            tw2n = 'hy_tw2' if job == 0 else 'hy_tw2c'
            twfn = 'hy_twf' if job == 0 else 'hy_twfc'
            Mm = 64 if job == 0 else 2
            KTD, KS, X1D, QD = c.KTD[job], c.KS, c.X1D, c.QD
            for o in range(2):
                ksrc = KTD[:, o * 512:(o + 1) * 512].rearrange("(a b) c -> a b c", b=128)
                hy_fwd1(c, lambda b0, nb, ksrc=ksrc: ksrc[:, b0:b0 + nb, :], nt, ftab, X1D, NF1)
                if hp >= 4:
                    hy_stage2(c, X1D, 'filter', KS[o], None, NF1, tw2n, twres if job == 0 else None)
            if hp < 5:
                continue
            p.dma('sp', 'hy_scl', [('SCL', job)], ['hy_scl'],
                  [(scl[:, o, :], c.SCL[job][0, o * 512:(o + 1) * 512].partition_broadcast(64)) for o in range(2)] +
                  [(bias[:, o, :], I['hy_bias'][l, o, :].partition_broadcast(64)) for o in range(2)])
            hyc = c.HYC[tok0:tok0 + na * 128, :].rearrange("(a b) c -> a b c", b=128)
            z2 = c.Z2[tok0:tok0 + na * 128, :].rearrange("(a b) c -> a b c", b=128)
            oh = c.OH[tok0:tok0 + na * 128, :].rearrange("(a b) c -> a b c", b=128)
            vsrc = hyc[:, :, 0:512]
            hy_fwd1(c, lambda b0, nb: vsrc[:, b0:b0 + nb, :], na, ftab, X1D, NF1)
            hy_stage2(c, X1D, 'conv', KS[0], QD, NF1, tw2n, twres if job == 0 else None)
            if hp < 6:
                continue
            hy_final(c, QD, na, scl[:, 0, :], bias[:, 0, :], vsrc, hyc[:, :, 512:1024], z2, NF1, twfn, Mm)
            if hp < 7:
                continue
            hy_fwd1(c, lambda b0, nb: z2[:, b0:b0 + nb, :], na, ftab, X1D, NF1)
            hy_stage2(c, X1D, 'conv', KS[1], QD, NF1, tw2n, twres if job == 0 else None)
            hy_final(c, QD, na, scl[:, 1, :], bias[:, 1, :], z2, hyc[:, :, 1024:1536], oh, NF1, twfn, Mm)
        p.barrier()


def ln_affine_store(c, r, rkey, gb, gbkey, dsts, slot):
    p = c.p
    st = c.e_st[slot]
    junk = c.e_junk
    p.op('act', [rkey], ['e_junk', ('e_st', slot, 0)],
         lambda E: E.activation(out=junk[:, :], in_=r[:, :], func=AF.Copy, accum_out=st[:, 0:1]))
    p.op('dve', [('e_st', slot, 0)], [('e_st', slot, 1)],
         lambda E: E.tensor_scalar(out=st[:, 1:2], in0=st[:, 0:1], scalar1=-1.0 / D, scalar2=None, op0=ALU.mult))
    p.op('act', [rkey, ('e_st', slot, 1)], ['e_junk', ('e_st', slot, 2)],
         lambda E: E.activation(out=junk[:, :], in_=r[:, :], func=AF.Square, bias=st[:, 1:2], scale=1.0, accum_out=st[:, 2:3]))
    p.op('act', [('e_st', slot, 2)], [('e_st', slot, 3)],
         lambda E: E.activation(out=st[:, 3:4], in_=st[:, 2:3], func=AF.Ln, scale=1.0 / D, bias=LN_EPS))
    p.op('act', [('e_st', slot, 3)], [('e_st', slot, 4)],
         lambda E: E.activation(out=st[:, 4:5], in_=st[:, 3:4], func=AF.Exp, scale=-0.5))
    p.op('dve', [rkey, ('e_st', slot, 1), ('e_st', slot, 4)], [rkey],
         lambda E: E.tensor_scalar(out=r[:, :], in0=r[:, :], scalar1=st[:, 1:2], scalar2=st[:, 4:5], op0=ALU.add, op1=ALU.mult))
    p.op('pool', [rkey, gbkey], [rkey], lambda E: E.tensor_tensor(out=r[:, :], in0=r[:, :], in1=gb[:, 0, :], op=ALU.mult))
    p.op('pool', [rkey, gbkey], [rkey], lambda E: E.tensor_tensor(out=r[:, :], in0=r[:, :], in1=gb[:, 1, :], op=ALU.add))
    p.dma('sp', 'e_r%d' % slot, [rkey], ['XOUT'], [(d, r[:, :]) for d in dsts])


def load_bc_rows(c, l, which):
    p = c.p
    I = c.inp
    g = 2 if which == 1 else 5
    lg, lb = ('ln1_g', 'ln1_b') if which == 1 else ('ln2_g', 'ln2_b')
    p.dma('sp', 'e_bc', [('modv', l)], ['e_bc'],
          [(c.e_ag[:, r, :], c.modv[l][r, g * 1024:(g + 1) * 1024].partition_broadcast(128)) for r in range(2)] +
          [(c.e_gb[:, 0, :], I[lg][l, :].partition_broadcast(128)), (c.e_gb[:, 1, :], I[lb][l, :].partition_broadcast(128))])


def stage_merge(c, l, xsrc, tiles):
    p = c.p
    I = c.inp
    S = c.scr[l]
    B = c.bank
    bk = lambda i: ('bank', i)
    with ExitStack() as es:
        c.es = es
        wbr = sb(c, 'mg_wbr', [128, 3, 4, 1024], BF16)
        wout = sb(c, 'mg_wout', [128, 8, 1024], BF16)
        c.e_ag = sb(c, 'e_ag', [128, 2, 1024], F32)
        c.e_gb = sb(c, 'e_gb', [128, 2, 1024], F32)
        c.e_st = [sb(c, 'e_st%d' % i, [128, 8], F32) for i in range(2)]
        c.e_junk = sb(c, 'e_junk', [128, 1024], BF16)
        oT = [sb(c, 'mg_oT%d' % i, [128, 3, 4, 128], BF16) for i in range(2)]
        oh = [sb(c, 'mg_oh%d' % i, [128, 512], BF16) for i in range(2)]
        g3 = [sb(c, 'mg_g3%d' % i, [128, 3072], BF16) for i in range(2)]
        y = [sb(c, 'mg_y%d' % i, [128, 1024], F32) for i in range(2)]
        tt = [sb(c, 'mg_t%d' % i, [128, 1024], F32) for i in range(2)]
        yb = [sb(c, 'mg_yb%d' % i, [128, 1024], BF16) for i in range(2)]
        yT = [sb(c, 'mg_yT%d' % i, [128, 8, 128], BF16) for i in range(2)]
        xt = [sb(c, 'mg_xt%d' % i, [128, 1024], F32) for i in range(2)]
        for br, nm in enumerate(['w_br_gla', 'w_br_mla', 'w_br_hy']):
            p.dma('pool', 'mg_w', [], ['mg_w'], [(wbr[:, br, :, :], I[nm][l].rearrange("(k p) n -> p k n", p=128))])
        p.dma('pool', 'mg_w', [], ['mg_w'], [(wout[:, :, :], I['w_out'][l].rearrange("(k p) n -> p k n", p=128))])
        load_bc_rows(c, l, 1)
        def mloads(i):
            t = tiles[i]
            s2 = i % 2
            tok = slice(t * 128, (t + 1) * 128)
            p.dma('sp', 'mg_oT%d' % s2, [('OGT', l), ('OMT', l)], [('mg_oT', s2)],
                  [(oT[s2][:, 0, :, :], S['OGT'][:, tok].rearrange("(k p) t -> p k t", p=128)),
                   (oT[s2][:, 1, :, :], S['OMT'][:, tok].rearrange("(k p) t -> p k t", p=128))])
            p.dma('sp', 'mg_oh%d' % s2, ['OH'], [('mg_oh', s2)], [(oh[s2][:, :], c.OH[tok, :])])
            p.dma('sp', 'mg_g3%d' % s2, [('G3', l)], [('mg_g3', s2)], [(g3[s2][:, :], S['G3'][tok, :])])
            p.dma('sp', 'mg_xt%d' % s2, ['XOUT'], [('mg_xt', s2)], [(xt[s2][:, :], xsrc(t))])

        mloads(0)
        for i, t in enumerate(tiles):
            s2 = i % 2
            r = 1 if t < 2 else 0
            tok = slice(t * 128, (t + 1) * 128)
            if i + 1 < len(tiles):
                mloads(i + 1)
            tpv = bank16(c, 6)

            def tro(E):
                ins = None
                for k in range(4):
                    ins = E.transpose(tpv[:, k, :], oh[s2][:, k * 128:(k + 1) * 128], c.ident[:, :])
                return ins
            p.op('pe', [('mg_oh', s2), 'ident'], [bk(6)], tro)
            p.op('act', [bk(6)], [('mg_oT', s2)], lambda E: E.activation(out=oT[s2][:, 2, :, :], in_=tpv[:, 0:4, :], func=AF.Copy))
            for br in range(3):
                for half in range(2):
                    bb = (br * 2 + half) % 4

                    def mmb(E):
                        ins = None
                        for k in range(4):
                            ins = E.matmul(B[bb][:, :], lhsT=oT[s2][:, br, k, :], rhs=wbr[:, br, k, half * 512:(half + 1) * 512],
                                           start=(k == 0), stop=(k == 3))
                        return ins
                    p.op('pe', [('mg_oT', s2), 'mg_w'], [bk(bb)], mmb)
                    hs = slice(half * 512, (half + 1) * 512)
                    gs = slice(br * 1024 + half * 512, br * 1024 + (half + 1) * 512)
                    if br == 0:
                        p.op('dve', [bk(bb), ('mg_g3', s2)], [('mg_y', s2, half)],
                             lambda E: E.tensor_tensor(out=y[s2][:, hs], in0=B[bb][:, :], in1=g3[s2][:, gs], op=ALU.mult))
                    else:
                        p.op('dve', [bk(bb), ('mg_g3', s2)], [('mg_t', s2, half)],
                             lambda E: E.tensor_tensor(out=tt[s2][:, hs], in0=B[bb][:, :], in1=g3[s2][:, gs], op=ALU.mult))
                        p.op('pool', [('mg_t', s2, half), ('mg_y', s2, half)], [('mg_y', s2, half)],
                             lambda E: E.tensor_tensor(out=y[s2][:, hs], in0=y[s2][:, hs], in1=tt[s2][:, hs], op=ALU.add))
            p.op('act', [('mg_y', s2, 0), ('mg_y', s2, 1)], [('mg_yb', s2)],
                 lambda E: E.activation(out=yb[s2][:, :], in_=y[s2][:, :], func=AF.Copy))
            tp7 = bank16(c, 7)

            def try_(E):
                ins = None
                for k in range(8):
                    ins = E.transpose(tp7[:, k, :], yb[s2][:, k * 128:(k + 1) * 128], c.ident[:, :])
                return ins
            p.op('pe', [('mg_yb', s2), 'ident'], [bk(7)], try_)
            p.op('act', [bk(7)], [('mg_yT', s2)], lambda E: E.activation(out=yT[s2][:, :, :], in_=tp7[:, :, :], func=AF.Copy))
            for half in range(2):
                bb = 4 + half

                def mmo(E):
                    ins = None
                    for k in range(8):
                        ins = E.matmul(B[bb][:, :], lhsT=yT[s2][:, k, :], rhs=wout[:, k, half * 512:(half + 1) * 512],
                                       start=(k == 0), stop=(k == 7))
                    return ins
                p.op('pe', [('mg_yT', s2), 'mg_w'], [bk(bb)], mmo)
                hs = slice(half * 512, (half + 1) * 512)
                p.op('dve', [bk(bb), 'e_bc'], [('mg_t', s2, half)],
                     lambda E: E.tensor_tensor(out=tt[s2][:, hs], in0=B[bb][:, :], in1=c.e_ag[:, r, hs], op=ALU.mult))
                p.op('dve', [('mg_t', s2, half), ('mg_xt', s2)], [('mg_xt', s2)],
                     lambda E: E.scalar_tensor_tensor(out=xt[s2][:, hs], in0=xt[s2][:, hs], scalar=ALPHA, in1=tt[s2][:, hs],
                                                      op0=ALU.mult, op1=ALU.add))
            ln_affine_store(c, xt[s2], ('mg_xt', s2), c.e_gb, 'e_bc', [c.X1[tok, :]], s2)
        p.barrier()


def moe_precast(c, l):
    p = c.p
    I = c.inp
    for e in range(32):
        if c.cfg.get('moe_mode', 'sparse') == 'sparse':
            p.dma('pool', 'wcast', [], [('WB', l, e)],
                  [(c.WB1[l][e].rearrange("(p k) n -> p k n", k=8), I['moe_w1'][l, e].rearrange("(k p) n -> p k n", p=128)),
                   (c.WB2[l][e].rearrange("(p k) n -> p k n", k=8), I['moe_w2'][l, e].rearrange("(k p) n -> p k n", p=128))])
        else:
            p.dma('pool', 'wcast', [], [('WB', l, e)],
                  [(c.WB1[l][e], I['moe_w1'][l, e]), (c.WB2[l][e], I['moe_w2'][l, e])])


def stage_moe(c, l, tiles, dst_fn):
    p = c.p
    I = c.inp
    B = c.bank
    bk = lambda i: ('bank', i)
    cst = c.cst
    with ExitStack() as es:
        c.es = es
        c.e_ag = sb(c, 'e_ag', [128, 2, 1024], F32)
        c.e_gb = sb(c, 'e_gb', [128, 2, 1024], F32)
        c.e_st = [sb(c, 'e_st%d' % i, [128, 8], F32) for i in range(2)]
        c.e_junk = sb(c, 'e_junk', [128, 1024], BF16)
        w1 = [sb(c, 'mo_w1%d' % i, [128, 8, 2048], BF16) for i in range(2)]
        w2 = [sb(c, 'mo_w2%d' % i, [128, 8, 1024], BF16) for i in range(1)] * 2
        hT = sb(c, 'mo_hT', [128, 8, 1024], BF16)
        aT = sb(c, 'mo_aT', [128, 8, 1024], BF16)
        yacc = sb(c, 'mo_yacc', [128, 8, 1024], F32)
        rw = sb(c, 'mo_rw', [128, 8, 32], F32)
        rb = sb(c, 'mo_rb', [1, 32], F32)
        b1 = sb(c, 'mo_b1', [128, 32, 16], F32)
        b2 = sb(c, 'mo_b2', [32, 1024], F32)
        G = sb(c, 'mo_G', [128, 8, 32], F32)
        GT = sb(c, 'mo_GT', [32, 8, 128], F32)
        xt = [sb(c, 'mo_xt%d' % i, [128, 1024], F32) for i in range(2)]
        xh = [sb(c, 'mo_xh%d' % i, [128, 1024], F32) for i in range(1)] * 2
        h32 = [sb(c, 'mo_h32%d' % i, [128, 8, 128], F32) for i in range(1)] * 2
        lg = [sb(c, 'mo_lg%d' % i, [128, 32], F32) for i in range(2)]
        mx = [sb(c, 'mo_mx%d' % i, [128, 8], F32) for i in range(2)]
        ex = [sb(c, 'mo_ex%d' % i, [128, 32], F32) for i in range(2)]
        sm = [sb(c, 'mo_sm%d' % i, [128, 2], F32) for i in range(2)]
        gg = [sb(c, 'mo_gg%d' % i, [128, 512], F32) for i in range(2)]
        sg = [sb(c, 'mo_sg%d' % i, [128, 512], F32) for i in range(2)]
        ll = [sb(c, 'mo_ll%d' % i, [128, 512], F32) for i in range(2)]
        st = [sb(c, 'mo_st%d' % i, [128, 8], F32) for i in range(2)]
        p.dma('sp', 'mo_c', [], ['mo_c'],
              [(rw[:, :, :], I['router_w'][l].rearrange("(k p) e -> p k e", p=128)),
               (rb[:, :], I['router_b'][l:l + 1, :]), (b2[:, :], I['moe_b2'][l])])
        p.dma('sp', 'mo_b1', [], ['mo_b1'],
              [(b1[:, e, :], I['moe_b1'][l, e, :].rearrange("(j p) -> p j", p=128)) for e in range(32)], slow=True)
        load_bc_rows(c, l, 2)
        ones = cst[:, 640:768]
        ident32 = cst[:, 0:128]
        groups = [tiles[i:i + 8] for i in range(0, len(tiles), 8)][:c.cfg.get('moe_groups', 99)]
        wi = 0
        for gi, gt in enumerate(groups):
            ng = len(gt)
            T = ng * 128
            for j, t in enumerate(gt):
                s2 = j % 2
                r = 1 if t < 2 else 0
                tok = slice(t * 128, (t + 1) * 128)
                p.dma('sp', 'mo_xt%d' % s2, ['XOUT'], [('mo_xt', s2)], [(xt[s2][:, :], c.X1[tok, :])])
                s_ = st[s2]
                p.op('act', [('mo_xt', s2)], ['e_junk', ('mo_st', s2, 0)],
                     lambda E: E.activation(out=c.e_junk[:, :], in_=xt[s2][:, :], func=AF.Copy, accum_out=s_[:, 0:1]))
                p.op('dve', [('mo_st', s2, 0)], [('mo_st', s2, 1)],
                     lambda E: E.tensor_scalar(out=s_[:, 1:2], in0=s_[:, 0:1], scalar1=-1.0 / D, scalar2=None, op0=ALU.mult))
                p.op('act', [('mo_xt', s2), ('mo_st', s2, 1)], ['e_junk', ('mo_st', s2, 2)],
                     lambda E: E.activation(out=c.e_junk[:, :], in_=xt[s2][:, :], func=AF.Square, bias=s_[:, 1:2], scale=1.0,
                                            accum_out=s_[:, 2:3]))
                p.op('act', [('mo_st', s2, 2)], [('mo_st', s2, 3)],
                     lambda E: E.activation(out=s_[:, 3:4], in_=s_[:, 2:3], func=AF.Ln, scale=1.0 / D, bias=LN_EPS))
                p.op('act', [('mo_st', s2, 3)], [('mo_st', s2, 4)],
                     lambda E: E.activation(out=s_[:, 4:5], in_=s_[:, 3:4], func=AF.Exp, scale=-0.5))
                p.op('dve', [('mo_xt', s2), ('mo_st', s2, 1), ('mo_st', s2, 4)], [('mo_xh', 0)],
                     lambda E: E.tensor_scalar(out=xh[s2][:, :], in0=xt[s2][:, :], scalar1=s_[:, 1:2], scalar2=s_[:, 4:5],
                                               op0=ALU.add, op1=ALU.mult))
                for hh in range(2):
                    bb = hh
                    def tr32(E):
                        ins = None
                        for k in range(4):
                            kk = hh * 4 + k
                            ins = E.transpose(B[bb][:, k * 128:(k + 1) * 128], xh[s2][:, kk * 128:(kk + 1) * 128], ident32)
                        return ins
                    p.op('pe', [('mo_xh', 0), 'cst'], [bk(bb)], tr32)
                    for k in range(4):
                        kk = hh * 4 + k
                        p.op('dve', [bk(bb), 'modT'], [('mo_h32', 0, kk)],
                             lambda E: E.tensor_scalar(out=h32[s2][:, kk, :], in0=B[bb][:, k * 128:(k + 1) * 128],
                                                       scalar1=c.modT[:, r, 4, kk:kk + 1], scalar2=c.modT[:, r, 3, kk:kk + 1],
                                                       op0=ALU.mult, op1=ALU.add))
                hk = [('mo_h32', 0, kk) for kk in range(8)]
                p.op('pool', hk, [('mo_hT', j)], lambda E: E.tensor_copy(out=hT[:, :, j * 128:(j + 1) * 128], in_=h32[s2][:, :, :]))

                def mml(E):
                    for kk in range(8):
                        E.matmul(B[2][:, 0:32], lhsT=h32[s2][:, kk, :], rhs=rw[:, kk, :], start=(kk == 0), stop=False)
                    return E.matmul(B[2][:, 0:32], lhsT=ones[0:1, :], rhs=rb[0:1, :], start=False, stop=True)
                p.op('pe', hk + ['mo_c', 'cst'], [bk(2)], mml)
                p.op('dve', [bk(2)], [('mo_lg', s2)], lambda E: E.tensor_copy(out=lg[s2][:, :], in_=B[2][:, 0:32]))
                p.op('dve', [('mo_lg', s2)], [('mo_mx', s2)], lambda E: E.max(out=mx[s2][:, :], in_=lg[s2][:, :]))
                p.op('dve', [('mo_mx', s2)], [('mo_sm', s2, 0)],
                     lambda E: E.tensor_scalar(out=sm[s2][:, 0:1], in0=mx[s2][:, 0:1], scalar1=-1.0, scalar2=None, op0=ALU.mult))
                p.op('act', [('mo_lg', s2), ('mo_sm', s2, 0)], [('mo_ex', s2)],
                     lambda E: E.activation(out=ex[s2][:, :], in_=lg[s2][:, :], func=AF.Exp, bias=sm[s2][:, 0:1], scale=1.0))
                p.op('dve', [('mo_lg', s2), ('mo_mx', s2)], [('mo_lg', s2)],
                     lambda E: E.tensor_scalar(out=lg[s2][:, :], in0=lg[s2][:, :], scalar1=mx[s2][:, 3:4], scalar2=None, op0=ALU.is_ge))
                p.op('dve', [('mo_lg', s2), ('mo_ex', s2)], [('mo_ex', s2)],
                     lambda E: E.tensor_tensor(out=ex[s2][:, :], in0=ex[s2][:, :], in1=lg[s2][:, :], op=ALU.mult))
                p.op('dve', [('mo_ex', s2)], [('mo_sm', s2, 1)],
                     lambda E: E.tensor_reduce(out=sm[s2][:, 1:2], in_=ex[s2][:, :], axis=AX.X, op=ALU.add))
                p.op('dve', [('mo_sm', s2, 1)], [('mo_sm', s2, 1)], lambda E: E.reciprocal(out=sm[s2][:, 1:2], in_=sm[s2][:, 1:2]))
                p.op('dve', [('mo_ex', s2), ('mo_sm', s2, 1)], [('mo_G', j)],
                     lambda E: E.tensor_scalar(out=G[:, j, :], in0=ex[s2][:, :], scalar1=sm[s2][:, 1:2], scalar2=None, op0=ALU.mult))
                p.op('pe', [('mo_G', j), 'cst'], [bk(3)], lambda E: E.transpose(B[3][0:32, 0:128], G[:, j, :], ident32))
                p.op('act', [bk(3)], [('mo_GT', j)], lambda E: E.activation(out=GT[:, j, :], in_=B[3][0:32, 0:128], func=AF.Copy))
                for half in range(2):
                    p.op('pe', [('mo_GT', j), 'mo_c'], [bk(4 + half)],
                         lambda E: E.matmul(B[4 + half][:, :], lhsT=GT[:, j, :], rhs=b2[:, half * 512:(half + 1) * 512], start=True, stop=True))
                    p.op('act', [bk(4 + half)], [('mo_yacc', j, half)],
                         lambda E: E.activation(out=yacc[:, j, half * 512:(half + 1) * 512], in_=B[4 + half][:, :], func=AF.Copy))
            hTk = [('mo_hT', j) for j in range(ng)]
            ei = 0
            for e in range(32):
                ws = wi % 2
                wi += 1
                p.dma('sp', 'mo_w1%d' % ws, [('WB', l, e)], [('mo_w1', ws)],
                      [(w1[ws][:, :, :], c.WB1[l][e].rearrange("(k p) n -> p k n", p=128))])
                p.dma('sp', 'mo_w2', [('WB', l, e)], [('mo_w2', 0)],
                      [(w2[ws][:, :, :], c.WB2[l][e].rearrange("(k p) n -> p k n", p=128))])
                for th in range((T + 511) // 512):
                    c0 = th * 512
                    n = min(512, T - c0)
                    for fc in range(8):
                        s2 = ei % 2
                        ei += 1
                        bg, bl = s2 * 2, s2 * 2 + 1

                        def mm1(E):
                            ins = None
                            for k in range(8):
                                E.matmul(B[bg][:, 0:n], lhsT=w1[ws][:, k, fc * 128:(fc + 1) * 128], rhs=hT[:, k, c0:c0 + n],
                                         start=(k == 0), stop=(k == 7))
                            for k in range(8):
                                ins = E.matmul(B[bl][:, 0:n], lhsT=w1[ws][:, k, 1024 + fc * 128:1024 + (fc + 1) * 128],
                                               rhs=hT[:, k, c0:c0 + n], start=(k == 0), stop=(k == 7))
                            return ins
                        p.op('pe', hTk + [('mo_w1', ws)], [bk(bg), bk(bl)], mm1)
                        p.op('dve', [bk(bg), 'mo_b1'], [('mo_gg', s2)],
                             lambda E: E.tensor_scalar(out=gg[s2][:, 0:n], in0=B[bg][:, 0:n], scalar1=b1[:, e, fc:fc + 1], scalar2=7.0,
                                                       op0=ALU.add, op1=ALU.min))
                        p.op('act', [('mo_gg', s2)], [('mo_sg', s2)],
                             lambda E: E.activation(out=sg[s2][:, 0:n], in_=gg[s2][:, 0:n], func=AF.Sigmoid, scale=1.702))
                        p.op('dve', [bk(bl), 'mo_b1'], [('mo_ll', s2)],
                             lambda E: E.tensor_scalar(out=ll[s2][:, 0:n], in0=B[bl][:, 0:n], scalar1=b1[:, e, 8 + fc:9 + fc], scalar2=7.0,
                                                       op0=ALU.add, op1=ALU.min))
                        p.op('pool', [('mo_ll', s2)], [('mo_ll', s2)],
                             lambda E: E.tensor_scalar(out=ll[s2][:, 0:n], in0=ll[s2][:, 0:n], scalar1=-7.0, scalar2=1.0,
                                                       op0=ALU.max, op1=ALU.add))
                        p.op('pool', [('mo_gg', s2), ('mo_sg', s2)], [('mo_gg', s2)],
                             lambda E: E.tensor_tensor(out=gg[s2][:, 0:n], in0=gg[s2][:, 0:n], in1=sg[s2][:, 0:n], op=ALU.mult))
                        p.op('dve', [('mo_gg', s2), ('mo_ll', s2)], [('mo_aT', fc, th)],
                             lambda E: E.tensor_tensor(out=aT[:, fc, c0:c0 + n], in0=gg[s2][:, 0:n], in1=ll[s2][:, 0:n], op=ALU.mult))
                aTk = [('mo_aT', fc, th) for fc in range(8) for th in range((T + 511) // 512)]
                for j in range(ng):
                    for half in range(2):
                        bb = 4 + (j * 2 + half) % 4

                        def mm2(E):
                            ins = None
                            for k in range(8):
                                ins = E.matmul(B[bb][:, :], lhsT=aT[:, k, j * 128:(j + 1) * 128], rhs=w2[ws][:, k, half * 512:(half + 1) * 512],
                                               start=(k == 0), stop=(k == 7))
                            return ins
                        p.op('pe', aTk + [('mo_w2', 0)], [bk(bb)], mm2)
                        hs = slice(half * 512, (half + 1) * 512)
                        p.op('dve', [bk(bb), ('mo_G', j), ('mo_yacc', j, half)], [('mo_yacc', j, half)],
                             lambda E: E.scalar_tensor_tensor(out=yacc[:, j, hs], in0=B[bb][:, :], scalar=G[:, j, e:e + 1], in1=yacc[:, j, hs],
                                                              op0=ALU.mult, op1=ALU.add))
            for j, t in enumerate(gt):
                s2 = j % 2
                r = 1 if t < 2 else 0
                tok = slice(t * 128, (t + 1) * 128)
                p.dma('sp', 'mo_xt%d' % s2, ['XOUT'], [('mo_xt', s2)], [(xt[s2][:, :], c.X1[tok, :])])
                yk = [('mo_yacc', j, 0), ('mo_yacc', j, 1)]
                p.op('pool', yk + ['e_bc'], yk, lambda E: E.tensor_tensor(out=yacc[:, j, :], in0=yacc[:, j, :], in1=c.e_ag[:, r, :], op=ALU.mult))
                p.op('dve', yk + [('mo_xt', s2)], [('mo_xt', s2)],
                     lambda E: E.scalar_tensor_tensor(out=xt[s2][:, :], in0=xt[s2][:, :], scalar=ALPHA, in1=yacc[:, j, :],
                                                      op0=ALU.mult, op1=ALU.add))
                ln_affine_store(c, xt[s2], ('mo_xt', s2), c.e_gb, 'e_bc', dst_fn(t), s2)
        p.barrier()


def stage_moe2(c, l, tiles, dst_fn):
    p = c.p
    I = c.inp
    B = c.bank
    bk = lambda i: ('bank', i)
    cst = c.cst
    ones = cst[:, 640:768]
    ident32 = cst[:, 0:128]
    iota_f = cst[:, 768:800]
    iota_p = cst[:, 800:801]
    blk512 = cst[:, 896:1024]
    ntl = len(tiles)
    NB = (ntl * 128 * 4) // 512 + 32
    XB, YB, HB = c.XB, c.YB, c.HB
    W1f = c.WB1[l].rearrange("e (p k) n -> (e p) (k n)", k=8)
    W2f = c.WB2[l].rearrange("e (p k) n -> (e p) (k n)", k=8)
    I32 = mybir.dt.int32
    with ExitStack() as es_outer:
        c.es = es_outer
        slot_i = sb(c, 'ms_slot', [128, NTILE, 4], I32)
        gk = sb(c, 'ms_gk', [128, NTILE, 4], F32)
        idxw = sb(c, 'ms_idxw', [128, NB, 8], I32)
        be_bc = sb(c, 'ms_bebc', [128, NB], F32)
        OHall = sb(c, 'ms_oh', [32, NB], F32)
        c.e_ag = sb(c, 'e_ag', [128, 2, 1024], F32)
        c.e_gb = sb(c, 'e_gb', [128, 2, 1024], F32)
        c.e_st = [sb(c, 'e_st%d' % i, [128, 8], F32) for i in range(2)]
        c.e_junk = sb(c, 'e_junk', [128, 1024], BF16)
        load_bc_rows(c, l, 2)
        with ExitStack() as es:
            c.es = es
            Mall = sb(c, 'ms_M', [128, NTILE, 32], F32)
            Gall = sb(c, 'ms_G', [128, NTILE, 32], F32)
            Lall = sb(c, 'ms_L', [128, NTILE, 32], F32)
            mxall = sb(c, 'ms_mx', [128, NTILE, 8], F32)
            rw = sb(c, 'ms_rw', [128, 8, 32], F32)
            rb = sb(c, 'ms_rb', [1, 32], F32)
            modbc = sb(c, 'ms_modbc', [128, 2, 2, 1024], F32)
            xt = [sb(c, 'ms_xt%d' % i, [128, 1024], F32) for i in range(2)]
            xh = sb(c, 'ms_xh', [128, 1024], F32)
            hb = [sb(c, 'ms_hb%d' % i, [128, 1024], BF16) for i in range(2)]
            h32 = sb(c, 'ms_h32', [128, 8, 128], F32)
            ex = [sb(c, 'ms_ex%d' % i, [128, 32], F32) for i in range(2)]
            sm = [sb(c, 'ms_sm%d' % i, [128, 2], F32) for i in range(2)]
            st = [sb(c, 'ms_st%d' % i, [128, 8], F32) for i in range(2)]
            Lst = sb(c, 'ms_Lst', [128, 128], F32)
            Msum = sb(c, 'ms_Msum', [128, 32], F32)
            cnt = sb(c, 'ms_cnt', [1, 32], F32)
            cnti = sb(c, 'ms_cnti', [1, 32], I32)
            pcol = sb(c, 'ms_pcol', [32, 4], F32)
            psbc = sb(c, 'ms_psbc', [128, 32], F32)
            cmp_ = sb(c, 'ms_cmp', [32, 128], F32)
            berow = sb(c, 'ms_berow', [1, 128], F32)
            tmpf = sb(c, 'ms_tmpf', [128, 128], F32)
            pos = [sb(c, 'ms_pos%d' % i, [128, 32], F32) for i in range(2)]
            oh = [sb(c, 'ms_ohk%d' % i, [128, 32], F32) for i in range(2)]
            tq = [sb(c, 'ms_tq%d' % i, [128, 32], F32) for i in range(2)]
            sl = [sb(c, 'ms_sl%d' % i, [128, 4], F32) for i in range(2)]
            p.dma('sp', 'ms_c', [], ['ms_c'],
                  [(rw[:, :, :], I['router_w'][l].rearrange("(k p) e -> p k e", p=128)), (rb[:, :], I['router_b'][l:l + 1, :])])
            p.dma('sp', 'ms_modbc', [('modv', l)], ['ms_modbc'],
                  [(modbc[:, r, q, :], c.modv[l][r, (4 - q) * 1024:(5 - q) * 1024].partition_broadcast(128))
                   for r in range(2) for q in range(2)])
            for r in range(2):
                p.op('dve', ['ms_modbc'], ['ms_modbc'],
                     lambda E: E.tensor_scalar(out=modbc[:, r, 0, :], in0=modbc[:, r, 0, :], scalar1=1.0, scalar2=None, op0=ALU.add))
            p.op('dve', ['cst'], ['ms_Lst'], lambda E: E.tensor_tensor(out=Lst[:, :], in0=cst[:, 384:512], in1=ident32, op=ALU.subtract))
            def rload(jj):
                t = tiles[jj]
                p.dma('sp', 'ms_xt%d' % (jj % 2), ['XOUT'], [('ms_xt', jj % 2)], [(xt[jj % 2][:, :], c.X1[t * 128:(t + 1) * 128, :])])

            rload(0)
            for jj, t in enumerate(tiles):
                s2 = jj % 2
                r = 1 if t < 2 else 0
                tok = slice(t * 128, (t + 1) * 128)
                if jj + 1 < len(tiles):
                    rload(jj + 1)
                s_ = st[s2]
                p.op('act', [('ms_xt', s2)], ['e_junk', ('ms_st', s2, 0)],
                     lambda E: E.activation(out=c.e_junk[:, :], in_=xt[s2][:, :], func=AF.Copy, accum_out=s_[:, 0:1]))
                p.op('dve', [('ms_st', s2, 0)], [('ms_st', s2, 1)],
                     lambda E: E.tensor_scalar(out=s_[:, 1:2], in0=s_[:, 0:1], scalar1=-1.0 / D, scalar2=None, op0=ALU.mult))
                p.op('act', [('ms_xt', s2), ('ms_st', s2, 1)], ['e_junk', ('ms_st', s2, 2)],
                     lambda E: E.activation(out=c.e_junk[:, :], in_=xt[s2][:, :], func=AF.Square, bias=s_[:, 1:2], scale=1.0,
                                            accum_out=s_[:, 2:3]))
                p.op('act', [('ms_st', s2, 2)], [('ms_st', s2, 3)],
                     lambda E: E.activation(out=s_[:, 3:4], in_=s_[:, 2:3], func=AF.Ln, scale=1.0 / D, bias=LN_EPS))
                p.op('act', [('ms_st', s2, 3)], [('ms_st', s2, 4)],
                     lambda E: E.activation(out=s_[:, 4:5], in_=s_[:, 3:4], func=AF.Exp, scale=-0.5))
                p.op('dve', [('ms_xt', s2), ('ms_st', s2, 1), ('ms_st', s2, 4)], ['ms_xh'],
                     lambda E: E.tensor_scalar(out=xh[:, :], in0=xt[s2][:, :], scalar1=s_[:, 1:2], scalar2=s_[:, 4:5],
                                               op0=ALU.add, op1=ALU.mult))
                p.op('pool', ['ms_xh', 'ms_modbc'], [('ms_xt', s2)],
                     lambda E: E.tensor_tensor(out=xt[s2][:, :], in0=xh[:, :], in1=modbc[:, r, 0, :], op=ALU.mult))
                p.op('pool', [('ms_xt', s2), 'ms_modbc'], [('ms_hb', s2)],
                     lambda E: E.tensor_tensor(out=hb[s2][:, :], in0=xt[s2][:, :], in1=modbc[:, r, 1, :], op=ALU.add))
                p.dma('sp', 'ms_hb%d' % s2, [('ms_hb', s2)], [('HB', t)], [(HB[tok, :], hb[s2][:, :])])
                for hh in range(2):
                    def tr32(E):
                        ins = None
                        for k in range(4):
                            kk = hh * 4 + k
                            ins = E.transpose(B[hh][:, k * 128:(k + 1) * 128], xh[:, kk * 128:(kk + 1) * 128], ident32)
                        return ins
                    p.op('pe', ['ms_xh', 'cst'], [bk(hh)], tr32)
                    for k in range(4):
                        kk = hh * 4 + k
                        p.op('dve', [bk(hh), 'modT'], [('ms_h32', kk)],
                             lambda E: E.tensor_scalar(out=h32[:, kk, :], in0=B[hh][:, k * 128:(k + 1) * 128],
                                                       scalar1=c.modT[:, r, 4, kk:kk + 1], scalar2=c.modT[:, r, 3, kk:kk + 1],
                                                       op0=ALU.mult, op1=ALU.add))
                hk = [('ms_h32', kk) for kk in range(8)]

                def mml(E):
                    for kk in range(8):
                        E.matmul(B[2][:, 0:32], lhsT=h32[:, kk, :], rhs=rw[:, kk, :], start=(kk == 0), stop=False)
                    return E.matmul(B[2][:, 0:32], lhsT=ones[0:1, :], rhs=rb[0:1, :], start=False, stop=True)
                p.op('pe', hk + ['ms_c', 'cst'], [bk(2)], mml)
                lgj, mxj, Mj, Gj = Lall[:, t, :], mxall[:, t, :], Mall[:, t, :], Gall[:, t, :]
                p.op('dve', [bk(2)], [('ms_L', t)], lambda E: E.tensor_copy(out=lgj, in_=B[2][:, 0:32]))
                p.op('dve', [('ms_L', t)], [('ms_mx', t)], lambda E: E.max(out=mxj, in_=lgj))
                p.op('dve', [('ms_mx', t)], [('ms_sm', s2, 0)],
                     lambda E: E.tensor_scalar(out=sm[s2][:, 0:1], in0=mxall[:, t, 0:1], scalar1=-1.0, scalar2=None, op0=ALU.mult))
                p.op('act', [('ms_L', t), ('ms_sm', s2, 0)], [('ms_ex', s2)],
                     lambda E: E.activation(out=ex[s2][:, :], in_=lgj, func=AF.Exp, bias=sm[s2][:, 0:1], scale=1.0))
                p.op('dve', [('ms_L', t), ('ms_mx', t)], [('ms_M', t)],
                     lambda E: E.tensor_scalar(out=Mj, in0=lgj, scalar1=mxall[:, t, 3:4], scalar2=None, op0=ALU.is_ge))
                p.op('dve', [('ms_M', t), ('ms_ex', s2)], [('ms_ex', s2)],
                     lambda E: E.tensor_tensor(out=ex[s2][:, :], in0=ex[s2][:, :], in1=Mj, op=ALU.mult))
                p.op('dve', [('ms_ex', s2)], [('ms_sm', s2, 1)],
                     lambda E: E.tensor_reduce(out=sm[s2][:, 1:2], in_=ex[s2][:, :], axis=AX.X, op=ALU.add))
                p.op('dve', [('ms_sm', s2, 1)], [('ms_sm', s2, 1)], lambda E: E.reciprocal(out=sm[s2][:, 1:2], in_=sm[s2][:, 1:2]))
                p.op('dve', [('ms_ex', s2), ('ms_sm', s2, 1)], [('ms_G', t)],
                     lambda E: E.tensor_scalar(out=Gj, in0=ex[s2][:, :], scalar1=sm[s2][:, 1:2], scalar2=None, op0=ALU.mult))
                p.op('pe', [('ms_M', t), 'cst'], [bk(7)],
                     lambda E: E.matmul(B[7][0:1, 0:32], lhsT=ones[:, 0:1], rhs=Mj, start=(jj == 0), stop=(jj == ntl - 1)))
            p.op('dve', [bk(7)], ['ms_cnt'], lambda E: E.tensor_scalar(out=cnt[:, :], in0=B[7][0:1, 0:32], scalar1=1.0 / 512, scalar2=255.5 / 512,
                                                                       op0=ALU.mult, op1=ALU.add))
            p.op('dve', ['ms_cnt'], ['ms_cnti'], lambda E: E.tensor_copy(out=cnti[:, :], in_=cnt[:, :]))
            p.op('dve', ['ms_cnti'], ['ms_cnt'], lambda E: E.tensor_copy(out=cnt[:, :], in_=cnti[:, :]))
            p.op('dve', ['ms_cnt'], ['ms_cnt'], lambda E: E.tensor_scalar(out=cnt[:, :], in0=cnt[:, :], scalar1=512.0, scalar2=None, op0=ALU.mult))
            p.op('pe', ['ms_cnt', 'cst'], [bk(0)], lambda E: E.transpose(B[0][0:32, 0:1], cnt[0:1, :], ident32[0:1, 0:1]))
            p.op('dve', [bk(0)], ['ms_pcol'], lambda E: E.tensor_copy(out=pcol[:, 0:1], in_=B[0][0:32, 0:1]))
            p.op('pe', ['ms_pcol', 'ms_Lst'], [bk(1)],
                 lambda E: E.matmul(B[1][0:1, 0:32], lhsT=pcol[:, 0:1], rhs=Lst[0:32, 0:32], start=True, stop=True))
            p.op('dve', [bk(1)], ['ms_cnt'], lambda E: E.tensor_copy(out=cnt[:, :], in_=B[1][0:1, 0:32]))
            p.op('pe', ['ms_cnt', 'cst'], [bk(1)],
                 lambda E: E.matmul(B[1][:, 0:32], lhsT=ones[0:1, :], rhs=cnt[0:1, :], start=True, stop=True))
            p.op('dve', [bk(1)], ['ms_psbc'], lambda E: E.tensor_copy(out=psbc[:, :], in_=B[1][:, 0:32]))
            p.op('pe', ['ms_pcol', 'cst'], [bk(0)],
                 lambda E: E.matmul(B[0][0:32, 0:1], lhsT=cst[0:32, 384:416], rhs=pcol[:, 0:1], start=True, stop=True))
            p.op('dve', [bk(0)], ['ms_pcol2'], lambda E: E.tensor_copy(out=pcol[:, 1:2], in_=B[0][0:32, 0:1]))
            p.op('dve', ['ms_pcol2', 'cst'], ['ms_cmp'],
                 lambda E: E.tensor_scalar(out=cmp_[:, :], in0=blk512[0:32, :], scalar1=pcol[:, 1:2], scalar2=None, op0=ALU.is_ge))
            p.op('pe', ['ms_cmp', 'cst'], [bk(0)],
                 lambda E: E.matmul(B[0][0:1, 0:128], lhsT=ones[0:32, 0:1], rhs=cmp_[:, :], start=True, stop=True))
            p.op('dve', [bk(0)], ['ms_berow'], lambda E: E.tensor_copy(out=berow[:, :], in_=B[0][0:1, 0:128]))
            p.op('pe', ['ms_berow', 'cst'], [bk(0)],
                 lambda E: E.matmul(B[0][:, 0:128], lhsT=ones[0:1, :], rhs=berow[0:1, :], start=True, stop=True))
            p.op('dve', [bk(0)], ['ms_oob'],
                 lambda E: E.tensor_scalar(out=tmpf[:, 0:NB], in0=B[0][:, 0:NB], scalar1=31.5, scalar2=0.0, op0=ALU.is_ge, op1=ALU.mult))
            p.op('dve', [bk(0)], ['ms_bebc'],
                 lambda E: E.tensor_scalar(out=be_bc[:, :], in0=B[0][:, 0:NB], scalar1=31.0, scalar2=None, op0=ALU.min))
            p.op('dve', ['ms_bebc', 'cst'], ['ms_oh'],
                 lambda E: E.tensor_scalar(out=OHall[:, :], in0=be_bc[0:32, :], scalar1=iota_p[0:32, :], scalar2=None, op0=ALU.is_equal))
            p.op('dve', ['ms_bebc', 'cst', 'ms_oob'], ['ms_oob'],
                 lambda E: E.tensor_scalar(out=tmpf[:, 0:NB], in0=tmpf[:, 0:NB], scalar1=iota_p[:, :], scalar2=None, op0=ALU.add))
            p.op('dve', ['ms_bebc', 'ms_oob'], ['ms_tmpf'],
                 lambda E: E.scalar_tensor_tensor(out=tmpf[:, 0:NB], in0=be_bc[:, :], scalar=128.0, in1=tmpf[:, 0:NB], op0=ALU.mult, op1=ALU.add))
            p.op('dve', ['ms_tmpf'], ['ms_idxw'], lambda E: E.tensor_copy(out=idxw[:, :, 0], in_=tmpf[:, 0:NB]))
            p.op('dve', [], ['ms_Msum'], lambda E: E.memset(Msum[:, :], 0.0))
            for jj, t in enumerate(tiles):
                s2 = jj % 2
                tok = slice(t * 128, (t + 1) * 128)
                lgj, Mj, Gj = Lall[:, t, :], Mall[:, t, :], Gall[:, t, :]

                def mmp(E):
                    E.matmul(B[3 + s2][:, 0:32], lhsT=Lst[:, :], rhs=Mj, start=True, stop=False)
                    return E.matmul(B[3 + s2][:, 0:32], lhsT=ones[:, :], rhs=Msum[:, :], start=False, stop=True)
                p.op('pe', [('ms_M', t), 'ms_Msum', 'ms_Lst', 'cst'], [bk(3 + s2)], mmp)
                p.op('dve', [bk(3 + s2), 'ms_psbc'], [('ms_pos', s2)],
                     lambda E: E.tensor_tensor(out=pos[s2][:, :], in0=B[3 + s2][:, 0:32], in1=psbc[:, :], op=ALU.add))
                p.op('pool', [('ms_M', t), 'ms_Msum'], ['ms_Msum'],
                     lambda E: E.tensor_tensor(out=Msum[:, :], in0=Msum[:, :], in1=Mj, op=ALU.add))
                for k in range(4):
                    p.op('dve', [('ms_L', t), ('ms_mx', t)], [('ms_ohk', s2)],
                         lambda E: E.tensor_scalar(out=oh[s2][:, :], in0=lgj, scalar1=mxall[:, t, k:k + 1], scalar2=None, op0=ALU.is_equal))
                    p.op('dve', [('ms_ohk', s2), ('ms_pos', s2)], [('ms_tq', s2)],
                         lambda E: E.tensor_tensor(out=tq[s2][:, :], in0=oh[s2][:, :], in1=pos[s2][:, :], op=ALU.mult))
                    p.op('dve', [('ms_tq', s2)], [('ms_sl', s2, k)],
                         lambda E: E.tensor_reduce(out=sl[s2][:, k:k + 1], in_=tq[s2][:, :], axis=AX.X, op=ALU.add))
                    p.op('dve', [('ms_ohk', s2), ('ms_G', t)], [('ms_tq', s2)],
                         lambda E: E.tensor_tensor(out=tq[s2][:, :], in0=oh[s2][:, :], in1=Gj, op=ALU.mult))
                    p.op('dve', [('ms_tq', s2)], [('ms_gk', t, k)],
                         lambda E: E.tensor_reduce(out=gk[:, t, k:k + 1], in_=tq[s2][:, :], axis=AX.X, op=ALU.add))
                p.op('dve', [('ms_sl', s2, k) for k in range(4)], [('ms_slot', t)],
                     lambda E: E.tensor_copy(out=slot_i[:, t, :], in_=sl[s2][:, :]))
                p.dma('sp', 'ms_hbl%d' % s2, [('HB', t)], [('ms_hb', s2)], [(hb[s2][:, :], HB[tok, :])])
                p.idma('scat', [('ms_hb', s2), ('ms_slot', t), 'XBZ'], [('XB', t)],
                       [dict(out=XB[:, :], out_offset=bass.IndirectOffsetOnAxis(ap=slot_i[:, t, k:k + 1], axis=0),
                             in_=hb[s2][:, :], in_offset=None) for k in range(4)])
            p.barrier()
        if c.cfg.get('moe_phases', 3) < 2:
            return
        with ExitStack() as es:
            c.es = es
            w1 = [sb(c, 'me_w1%d' % i, [128, 8, 2048], BF16) for i in range(2)]
            w2 = [sb(c, 'me_w2%d' % i, [128, 8, 1024], BF16) for i in range(2)]
            xb = [sb(c, 'me_xb%d' % i, [128, 4, 1024], BF16) for i in range(2)]
            xT = [sb(c, 'me_xT%d' % i, [128, 8, 512], BF16) for i in range(2)]
            aT = sb(c, 'me_aT', [128, 8, 512], BF16)
            yb = [sb(c, 'me_yb%d' % i, [128, 1024], BF16) for i in range(2)]
            b1 = sb(c, 'me_b1', [128, 32, 16], F32)
            b2 = sb(c, 'me_b2', [32, 1024], BF16)
            b2f = sb(c, 'me_b2f', [32, 1024], F32)
            ohr = [sb(c, 'me_ohr%d' % i, [128, 32], F32) for i in range(2)]
            b1t = sb(c, 'me_b1t', [128, 32, 16], F32)
            b1s = [sb(c, 'me_b1s%d' % i, [128, 16], F32) for i in range(2)]
            ohb = [sb(c, 'me_ohb%d' % i, [32, 128], BF16) for i in range(2)]
            gg = [sb(c, 'me_gg%d' % i, [128, 512], F32) for i in range(2)]
            sg = [sb(c, 'me_sg%d' % i, [128, 512], F32) for i in range(2)]
            ll = [sb(c, 'me_ll%d' % i, [128, 512], F32) for i in range(2)]
            p.dma('sp', 'me_b2', [], ['me_b2f'], [(b2f[:, :], I['moe_b2'][l])])
            p.op('dve', ['me_b2f'], ['me_b2'], lambda E: E.tensor_copy(out=b2[:, :], in_=b2f[:, :]))
            p.dma('sp', 'me_b1', [], ['me_b1'],
                  [(b1[:, e, :], I['moe_b1'][l, e, :].rearrange("(j p) -> p j", p=128)) for e in range(32)], slow=True)
            wbk = [('WB', l, e) for e in range(32)]
            ei = 0
            def gather_w(i):
                ws = i % 2
                wdeps = (wbk if i < 2 else []) + ['ms_idxw']
                p.idma('gw1%d' % ws, wdeps, [('me_w1', ws)],
                       [dict(out=w1[ws][:, :, :].rearrange("p k n -> p (k n)"), out_offset=None, in_=W1f[:, :],
                             in_offset=bass.IndirectOffsetOnAxis(ap=idxw[:, i, 0:1], axis=0))])
                p.idma('gw2%d' % ws, wdeps, [('me_w2', ws)],
                       [dict(out=w2[ws][:, :, :].rearrange("p k n -> p (k n)"), out_offset=None, in_=W2f[:, :],
                             in_offset=bass.IndirectOffsetOnAxis(ap=idxw[:, i, 0:1], axis=0))])

            def load_xb(i):
                ws = i % 2
                xbk = [('XB', t) for t in tiles] if i < 2 else []
                p.dma('sp', 'me_xb%d' % ws, xbk, [('me_xb', ws)],
                      [(xb[ws][:, :, :], XB[i * 512:(i + 1) * 512, :].rearrange("(a p) f -> p a f", p=128))])

            gather_w(0)
            for i in range(NB):
                ws = i % 2
                if i + 1 < NB:
                    gather_w(i + 1)
                if i == 0:
                    load_xb(0)
                if i + 1 < NB:
                    load_xb(i + 1)
                for a in range(4):
                    tb = 4 + (i * 4 + a) % 2
                    tpv = bank16(c, tb)

                    def trx(E):
                        ins = None
                        for k in range(8):
                            ins = E.transpose(tpv[:, k, :], xb[ws][:, a, k * 128:(k + 1) * 128], c.ident[:, :])
                        return ins
                    p.op('pe', [('me_xb', ws), 'ident'], [bk(tb)], trx)
                    if a % 2 == 0:
                        p.op('act', [bk(tb)], [('me_xT', ws, a)],
                             lambda E: E.activation(out=xT[ws][:, :, a * 128:(a + 1) * 128], in_=tpv[:, :, :], func=AF.Copy))
                    else:
                        p.op('dve', [bk(tb)], [('me_xT', ws, a)],
                             lambda E: E.tensor_copy(out=xT[ws][:, :, a * 128:(a + 1) * 128], in_=tpv[:, :, :]))
                xTk = [('me_xT', ws, a) for a in range(4)]
                p.op('dve', ['ms_bebc', 'cst'], [('me_ohr', ws)],
                     lambda E: E.tensor_scalar(out=ohr[ws][:, :], in0=iota_f, scalar1=be_bc[:, i:i + 1], scalar2=None, op0=ALU.is_equal))
                p.op('dve', [('me_ohr', ws), 'me_b1'], ['me_b1t'],
                     lambda E: E.tensor_tensor(out=b1t[:, :, :], in0=b1[:, :, :], in1=ohr[ws][:, :].unsqueeze(2).to_broadcast([128, 32, 16]),
                                               op=ALU.mult))
                p.op('dve', ['me_b1t'], [('me_b1s', ws)],
                     lambda E: E.tensor_reduce(out=b1s[ws][:, :], in_=b1t[:, :, :].rearrange("p e j -> p j e"), axis=AX.X, op=ALU.add))
                p.op('dve', ['ms_oh', 'cst'], [('me_ohb', ws)],
                     lambda E: E.tensor_scalar(out=ohb[ws][:, :], in0=ones[0:32, :], scalar1=OHall[:, i:i + 1], scalar2=None, op0=ALU.mult))
                for fc in range(8):
                    s2 = ei % 2
                    ei += 1
                    bg, bl = s2 * 2, s2 * 2 + 1

                    def mm1(E):
                        ins = None
                        for k in range(8):
                            E.matmul(B[bg][:, :], lhsT=w1[ws][:, k, fc * 128:(fc + 1) * 128], rhs=xT[ws][:, k, :], start=(k == 0), stop=(k == 7))
                        for k in range(8):
                            ins = E.matmul(B[bl][:, :], lhsT=w1[ws][:, k, 1024 + fc * 128:1024 + (fc + 1) * 128], rhs=xT[ws][:, k, :],
                                           start=(k == 0), stop=(k == 7))
                        return ins
                    p.op('pe', xTk + [('me_w1', ws)], [bk(bg), bk(bl)], mm1)
                    p.op('dve', [bk(bg), ('me_b1s', ws)], [('me_gg', s2)],
                         lambda E: E.tensor_scalar(out=gg[s2][:, :], in0=B[bg][:, :], scalar1=b1s[ws][:, fc:fc + 1], scalar2=7.0,
                                                   op0=ALU.add, op1=ALU.min))
                    p.op('act', [('me_gg', s2)], [('me_sg', s2)],
                         lambda E: E.activation(out=sg[s2][:, :], in_=gg[s2][:, :], func=AF.Silu, scale=1.702))
                    p.op('dve', [bk(bl), ('me_b1s', ws)], [('me_ll', s2)],
                         lambda E: E.tensor_scalar(out=ll[s2][:, :], in0=B[bl][:, :], scalar1=b1s[ws][:, 8 + fc:9 + fc], scalar2=7.0,
                                                   op0=ALU.add, op1=ALU.min))
                    p.op('dve', [('me_ll', s2)], [('me_ll', s2)],
                         lambda E: E.tensor_scalar(out=ll[s2][:, :], in0=ll[s2][:, :], scalar1=-7.0, scalar2=1.0, op0=ALU.max, op1=ALU.add))
                    p.op('dve', [('me_sg', s2), ('me_ll', s2)], [('me_aT', fc)],
                         lambda E: E.scalar_tensor_tensor(out=aT[:, fc, :], in0=sg[s2][:, :], scalar=1.0 / 1.702, in1=ll[s2][:, :],
                                                          op0=ALU.mult, op1=ALU.mult))
                aTk = [('me_aT', fc) for fc in range(8)]
                for a in range(4):
                    y2 = (i * 4 + a) % 2
                    for half in range(2):
                        bb = 6 + half

                        def mm2(E):
                            for k in range(8):
                                E.matmul(B[bb][:, :], lhsT=aT[:, k, a * 128:(a + 1) * 128], rhs=w2[ws][:, k, half * 512:(half + 1) * 512],
                                         start=(k == 0), stop=False)
                            return E.matmul(B[bb][:, :], lhsT=ohb[ws][:, :], rhs=b2[:, half * 512:(half + 1) * 512], start=False, stop=True)
                        p.op('pe', aTk + [('me_w2', ws), ('me_ohb', ws), 'me_b2'], [bk(bb)], mm2)
                        if half == 0:
                            p.op('act', [bk(bb)], [('me_yb', y2, half)],
                                 lambda E: E.activation(out=yb[y2][:, 0:512], in_=B[bb][:, :], func=AF.Copy))
                        else:
                            p.op('dve', [bk(bb)], [('me_yb', y2, half)], lambda E: E.tensor_copy(out=yb[y2][:, 512:1024], in_=B[bb][:, :]))
                    r0 = i * 512 + a * 128
                    p.dma('sp', 'me_yb%d' % y2, [('me_yb', y2, 0), ('me_yb', y2, 1)], [('YB', i, a)], [(YB[r0:r0 + 128, :], yb[y2][:, :])])
            p.barrier()
        if c.cfg.get('moe_phases', 3) < 3:
            return
        with ExitStack() as es:
            c.es = es
            yg = [[sb(c, 'mc_yg%d_%d' % (i, k), [128, 1024], BF16) for k in range(4)] for i in range(2)]
            xt = [sb(c, 'mc_xt%d' % i, [128, 1024], F32) for i in range(2)]
            ff = [sb(c, 'mc_ff%d' % i, [128, 1024], F32) for i in range(2)]
            def cloads(jj):
                t = tiles[jj]
                s2 = jj % 2
                tok = slice(t * 128, (t + 1) * 128)
                p.dma('sp', 'mc_xt%d' % s2, ['XOUT'], [('mc_xt', s2)], [(xt[s2][:, :], c.X1[tok, :])])
                ybk = [('YB', i, a) for i in range(NB) for a in range(4)] if jj < 2 else []
                p.idma('gy%d' % s2, [('ms_slot', t)] + ybk, [('mc_yg', s2)],
                       [dict(out=yg[s2][k][:, :], out_offset=None, in_=YB[:, :],
                             in_offset=bass.IndirectOffsetOnAxis(ap=slot_i[:, t, k:k + 1], axis=0)) for k in range(4)])

            cloads(0)
            for jj, t in enumerate(tiles):
                s2 = jj % 2
                r = 1 if t < 2 else 0
                tok = slice(t * 128, (t + 1) * 128)
                if jj + 1 < len(tiles):
                    cloads(jj + 1)
                p.op('dve', [('mc_yg', s2), ('ms_gk', t, 0)], [('mc_ff', s2)],
                     lambda E: E.tensor_scalar(out=ff[s2][:, :], in0=yg[s2][0][:, :], scalar1=gk[:, t, 0:1], scalar2=None, op0=ALU.mult))
                for k in range(1, 4):
                    p.op('dve', [('mc_yg', s2), ('ms_gk', t, k), ('mc_ff', s2)], [('mc_ff', s2)],
                         lambda E: E.scalar_tensor_tensor(out=ff[s2][:, :], in0=yg[s2][k][:, :], scalar=gk[:, t, k:k + 1], in1=ff[s2][:, :],
                                                          op0=ALU.mult, op1=ALU.add))
                p.op('pool', [('mc_ff', s2), 'e_bc'], [('mc_ff', s2)],
                     lambda E: E.tensor_tensor(out=ff[s2][:, :], in0=ff[s2][:, :], in1=c.e_ag[:, r, :], op=ALU.mult))
                p.op('dve', [('mc_ff', s2), ('mc_xt', s2)], [('mc_xt', s2)],
                     lambda E: E.scalar_tensor_tensor(out=xt[s2][:, :], in0=xt[s2][:, :], scalar=ALPHA, in1=ff[s2][:, :],
                                                      op0=ALU.mult, op1=ALU.add))
                ln_affine_store(c, xt[s2], ('mc_xt', s2), c.e_gb, 'e_bc', dst_fn(t), s2)
            p.barrier()


def make_rope():
    rows = NLAT // 64
    row = np.repeat(np.arange(rows, dtype=np.float32), 64)
    col = np.tile(np.arange(64, dtype=np.float32), rows)
    inv = (np.float32(10000.0) ** (-np.arange(8, dtype=np.float32) / np.float32(8))).astype(np.float32)
    ang = np.concatenate([row[:, None] * inv, col[:, None] * inv], -1).astype(np.float32)
    return np.concatenate([np.cos(ang), np.sin(ang)], -1).astype(np.float32)


def make_hyena_consts():
    import ml_dtypes
    bf = ml_dtypes.bfloat16
    N = 16384
    out = {}
    a = np.arange(128, dtype=np.float64)
    f = np.arange(128, dtype=np.float64)
    ang = 2 * np.pi * np.outer(a, f) / 128
    d1 = np.zeros((128, 2, 2, 128), np.float64)
    d1[:, 0, 0, :] = np.cos(ang)
    d1[:, 0, 1, :] = -np.sin(ang)
    for aa in range(4):
        for ff_ in range(4):
            d1[aa, 1, 0, ff_] = np.cos(2 * np.pi * aa * ff_ / 4)
            d1[aa, 1, 1, ff_] = -np.sin(2 * np.pi * aa * ff_ / 4)
    out['hy_dft1'] = d1.astype(np.float32).astype(bf)
    e3 = np.zeros((128, 3, 128), np.float64)
    e3[:, 0, :] = np.cos(ang)
    e3[:, 1, :] = np.sin(ang)
    e3[:, 2, :] = -np.sin(ang)
    out['hy_e3'] = e3.astype(np.float32).astype(bf)
    f1 = np.arange(128)[:, None, None]
    b = np.arange(128)[None, :, None]
    f2 = np.arange(128)[None, None, :]
    th = 2 * np.pi * ((b * (f1 + 128 * f2)) % N) / N
    tw2 = np.stack([np.cos(th), -np.sin(th), np.sin(th)], axis=2)
    out['hy_tw2'] = tw2.astype(np.float32).astype(bf)
    bb = np.arange(128)[:, None, None]
    ff = np.arange(128)[None, :, None]
    aa = np.arange(64)[None, None, :]
    ps_ = 2 * np.pi * (((128 * aa + bb) * ff) % N) / N
    twf = np.stack([np.cos(ps_), -np.sin(ps_)], axis=2)
    out['hy_twf'] = twf.astype(np.float32).astype(bf)
    f1c = np.arange(4)[:, None, None]
    thc = 2 * np.pi * ((b * (f1c + 4 * f2)) % 512) / 512
    out['hy_tw2c'] = np.stack([np.cos(thc), -np.sin(thc), np.sin(thc)], axis=2).astype(np.float32).astype(bf)
    ffc = np.arange(4)[None, :, None]
    aac = np.arange(2)[None, None, :]
    psc = 2 * np.pi * (((128 * aac + bb) * ffc) % 512) / 512
    out['hy_twfc'] = np.stack([np.cos(psc), -np.sin(psc)], axis=2).astype(np.float32).astype(bf)
    deltas = np.abs(np.linspace(math.log(1e-2) / 1.5, math.log(1e-2) / 0.3, 512, dtype=np.float32))
    out['hy_negdelta'] = (-deltas).astype(np.float32)[None, :]

    def feats(L, s):
        s = np.asarray(s)
        t = np.linspace(0.0, 1.0, L, dtype=np.float32)[s][:, None]
        w = (np.float32(2 * math.pi) * np.arange(L, dtype=np.float32) / np.float32(L))[s][:, None]
        fq = np.linspace(1e-4, 15, 16, dtype=np.float32)
        z = np.concatenate([t, np.cos(fq * w), -np.sin(fq * w)], -1).astype(np.float32)
        return z, t[:, 0]
    BIG = 1e4
    L = 8192
    tau = np.arange(N)
    s = np.where(tau < L, tau, N - tau)
    s[L] = 0
    z, t = feats(L, s)
    out['hy_feat0'] = np.ascontiguousarray(z.T)
    out['hy_tvec0'] = t[None, :].astype(np.float32)
    L = 256
    tau = np.arange(512)
    s = np.where(tau < L, tau, 512 - tau)
    s[256] = 0
    z, t = feats(L, s)
    out['hy_feat1'] = np.ascontiguousarray(z.T)
    out['hy_tvec1'] = t[None, :].astype(np.float32)
    return out


def make_consts():
    cst = np.zeros((128, 1024), np.float32)
    cst[:, 0:128] = np.eye(128)
    U = (np.arange(128)[:, None] <= np.arange(128)[None, :]).astype(np.float32)
    cst[:, 128:256] = U / 16.0
    cst[:, 256:384] = U.T / 16.0
    cst[:, 384:512] = U
    cst[:, 512:640] = U.T
    cst[:, 640:768] = 1.0
    cst[:, 768:800] = np.arange(32, dtype=np.float32)[None, :]
    cst[:, 800] = np.arange(128, dtype=np.float32)
    cst[:, 896:1024] = 512.0 * np.arange(128, dtype=np.float32)[None, :]
    return cst


def build_program(cfg):
    nc = bass.Bass("TRN2", target_bir_lowering=False)
    es = ExitStack()
    c = Ctx()
    c.nc, c.es, c.cfg = nc, es, cfg
    c.dbg = set(cfg.get('dbg', []))
    c.p = Prog(nc, es)
    p = c.p
    layers = cfg.get('layers', [0, 1])
    stages = cfg.get('stages', ['adaln', 'proj'])

    def ext(name, shape, dt=F32):
        return nc.dram_tensor(name, list(shape), dt, kind="ExternalInput").ap()

    I = {}
    I['x'] = ext('x', [NLAT, D])
    I['ctx'] = ext('ctx', [NCTX, D])
    I['cc'] = ext('cc', [2, D])
    I['ada_w'] = ext('ada_w', [DEPTH, D, 6 * D])
    I['ada_b'] = ext('ada_b', [DEPTH, 6 * D])
    I['w_in'] = ext('w_in', [DEPTH, D, IN_TOTAL])
    I['cst'] = ext('cst', [128, 1024])
    for nm, shp in [('gla_wa2_f', [DEPTH, 16, 256]), ('gla_ba_f', [DEPTH, 256]), ('gla_wa2_b', [DEPTH, 16, 256]),
                    ('gla_ba_b', [DEPTH, 256]), ('gla_norm', [DEPTH, 128]),
                    ('mla_q_norm', [DEPTH, 384]), ('mla_w_uq', [DEPTH, 384, 768]), ('mla_kv_norm', [DEPTH, 256]),
                    ('mla_w_ukv', [DEPTH, 256, 1024]), ('rope', [NLAT, 32]),
                    ('hy_conv_w', [DEPTH, 3, 1536]), ('hy_conv_b', [DEPTH, 1536]), ('hy_w1', [DEPTH, 33, 64]),
                    ('hy_b1', [DEPTH, 64]), ('hy_w2', [DEPTH, 64, 64]), ('hy_b2', [DEPTH, 64]), ('hy_w3', [DEPTH, 64, 2048]),
                    ('hy_freq', [DEPTH, 64]), ('hy_bias', [DEPTH, 2, 512]),
                    ('hy_feat0', [33, 16384]), ('hy_tvec0', [1, 16384]), ('hy_feat1', [33, 512]), ('hy_tvec1', [1, 512]),
                    ('hy_negdelta', [1, 512]),
                    ('w_br_gla', [DEPTH, 512, D]), ('w_br_mla', [DEPTH, 512, D]), ('w_br_hy', [DEPTH, 512, D]),
                    ('w_out', [DEPTH, D, D]), ('ln1_g', [DEPTH, D]), ('ln1_b', [DEPTH, D]), ('ln2_g', [DEPTH, D]),
                    ('ln2_b', [DEPTH, D]), ('router_w', [DEPTH, D, 32]), ('router_b', [DEPTH, 32]),
                    ('moe_b1', [DEPTH, 32, 2048]), ('moe_b2', [DEPTH, 32, D])]:
        I[nm] = ext(nm, shp)
    for nm, shp in [('hy_dft1', [128, 2, 2, 128]), ('hy_e3', [128, 3, 128]), ('hy_tw2', [128, 128, 3, 128]),
                    ('hy_twf', [128, 128, 2, 64]), ('hy_tw2c', [4, 128, 3, 128]), ('hy_twfc', [128, 4, 2, 2])]:
        I[nm] = ext(nm, shp, BF16)
    if 'moe' in stages:
        I['moe_w1'] = ext('moe_w1', [DEPTH, 32, D, 2048])
        I['moe_w2'] = ext('moe_w2', [DEPTH, 32, D, D])
        c.WB1 = [nc.dram_tensor('WB1_%d' % l, [32, D, 2048], BF16).ap() for l in range(DEPTH)]
        c.WB2 = [nc.dram_tensor('WB2_%d' % l, [32, D, D], BF16).ap() for l in range(DEPTH)]
        c.XB = nc.dram_tensor('XB', [98 * 512, D], BF16).ap()
        c.YB = nc.dram_tensor('YB', [98 * 512, D], BF16).ap()
        c.HB = nc.dram_tensor('HB', [NT, D], BF16).ap()
    c.inp = I
    c.out = nc.dram_tensor('out', [NLAT, D], F32, kind="ExternalOutput").ap()

    c.modv = [dram(c, 'modv%d' % l, [2, 6 * D], F32) for l in range(DEPTH)]
    c.scr = []
    for l in range(DEPTH):
        S = {}
        for name, off, ncols in FM_GROUPS:
            S[name] = dram(c, '%s%d' % (name, l), [ncols, NT], F32 if name in ('AFT', 'ABT') else BF16)
        for name, off, ncols in TM_GROUPS:
            S[name] = dram(c, '%s%d' % (name, l), [NT, ncols], F32 if name == 'MKR' else BF16)
        for name in ('OGT', 'OMT', 'OHT'):
            S[name] = dram(c, '%s%d' % (name, l), [512, NT], BF16)
        c.scr.append(S)
    c.X1 = dram(c, 'X1', [NT, D], F32)
    c.X2 = dram(c, 'X2', [NT, D], F32)
    c.KpT = dram(c, 'KpT', [8, 97, NT], BF16)
    c.QpT = dram(c, 'QpT', [8, 97, NT], BF16)
    c.VpD = dram(c, 'VpD', [8, NT, 65], BF16)
    c.HYC = dram(c, 'HYC', [NT, 1536], BF16)
    c.Z2 = dram(c, 'Z2', [NT, 512], BF16)
    c.OH = dram(c, 'OH', [NT, 512], BF16)
    c.KTD = [dram(c, 'KTD0', [16384, 1024], BF16), dram(c, 'KTD1', [512, 1024], BF16)]
    c.KS = [dram(c, 'KS%d' % o, [128, 128, 2, 512], BF16) for o in range(2)]
    c.X1D = dram(c, 'X1D', [128, 128, 2, 512], BF16)
    c.QD = dram(c, 'QD', [128, 128, 2, 512], BF16)
    c.SCL = [dram(c, 'SCL%d' % j, [1, 1024], F32) for j in range(2)]

    c.cst = sb(c, 'cst_sb', [128, 1024], F32)
    c.ident = sb(c, 'ident16', [128, 128], BF16)
    c.cT = sb(c, 'cTsb', [128, 8, 2], F32)
    c.modT = sb(c, 'modT', [128, 2, 6, 8], F32)
    c.bank = [ps(c, 'bank%d' % i, [128, 512], F32) for i in range(8)]

    p.dma('sp', 'cst', [], ['cst'], [(c.cst[:, :], I['cst'][:, :])])
    p.op('dve', ['cst'], ['ident'], lambda E: E.tensor_copy(out=c.ident[:, :], in_=c.cst[:, 0:128]))
    p.dma('sp', 'cT', [], ['cT'],
          [(c.cT[:, :, r], I['cc'][r, :].rearrange("(k p) -> p k", p=128)) for r in range(2)], slow=True)
    p.op('act', ['cT'], ['cT'], lambda E: E.activation(out=c.cT[:, :, :], in_=c.cT[:, :, :], func=AF.Silu))

    for l in layers:
        if 'adaln' in stages:
            stage_adaln(c, l)
    if 'moe' in stages:
        for l in layers:
            moe_precast(c, l)
        if cfg.get('moe_mode', 'sparse') == 'sparse':
            c.zt = sb(c, 'zero_t', [128, 2048], BF16)
            p.op('pool', [], ['zero_t'], lambda E: E.memset(c.zt[:, :], 0.0))
            p.dma('sp', 'xbzero', ['zero_t'], ['XBZ'],
                  [(c.XB[i * 256:(i + 1) * 256, :].rearrange("(p a) f -> p (a f)", p=128), c.zt[:, :]) for i in range(196)])
    for l in layers:
        last = (l == DEPTH - 1)

        def xsrc(t, l=l):
            if l == 0:
                if t < 2:
                    return I['ctx'][t * 128:(t + 1) * 128, :]
                return I['x'][(t - 2) * 128:(t - 1) * 128, :]
            return c.X2[t * 128:(t + 1) * 128, :]
        if 'proj' in stages:
            stage_proj(c, l, xsrc)
        if 'gla' in stages:
            stage_gla(c, l)
        if 'mla' in stages:
            stage_mla(c, l, ctx_q=not last)
        if 'hyena' in stages:
            stage_hyena(c, l, with_ctx=not last)
        tiles = list(range(2, NTILE)) if last else list(range(NTILE))
        if 'merge' in stages:
            load_mod(c, l)
            stage_merge(c, l, xsrc, tiles)
        if 'moe' in stages:
            load_mod(c, l)

            def dst_fn(t, last=last):
                if last:
                    return [c.out[(t - 2) * 128:(t - 1) * 128, :]]
                return [c.X2[t * 128:(t + 1) * 128, :]]
            if cfg.get('moe_mode', 'sparse') == 'sparse':
                stage_moe2(c, l, tiles, dst_fn)
            else:
                stage_moe(c, l, tiles, dst_fn)
    p.finish('sp')
    return nc, c


ALL_STAGES = ['adaln', 'proj', 'gla', 'mla', 'hyena', 'merge', 'moe']
WEIGHT_KEYS = ['ada_w', 'ada_b', 'w_in', 'gla_wa2_f', 'gla_ba_f', 'gla_wa2_b', 'gla_ba_b', 'gla_norm', 'mla_q_norm', 'mla_w_uq',
               'mla_kv_norm', 'mla_w_ukv', 'hy_conv_w', 'hy_conv_b', 'hy_w1', 'hy_b1', 'hy_w2', 'hy_b2', 'hy_w3', 'hy_freq',
               'hy_bias', 'w_br_gla', 'w_br_mla', 'w_br_hy', 'w_out', 'ln1_g', 'ln1_b', 'ln2_g', 'ln2_b', 'router_w', 'router_b',
               'moe_w1', 'moe_b1', 'moe_w2', 'moe_b2']


def make_in_map(inputs, b, with_moe=True):
    f32 = lambda a: np.ascontiguousarray(np.asarray(a, dtype=np.float32))
    im = dict(x=f32(inputs['x'][b]), ctx=f32(inputs['ctx'][b]),
              cc=f32(np.stack([np.asarray(inputs['c'])[b], np.asarray(inputs['c_ctx'])])),
              cst=make_consts(), rope=make_rope())
    im.update(make_hyena_consts())
    for k in WEIGHT_KEYS:
        if not with_moe and k in ('moe_w1', 'moe_w2'):
            continue
        im[k] = f32(inputs[k])
    return im


def kernel(**inputs):
    nc, c = build_program(dict(layers=[0, 1], stages=ALL_STAGES))
    nb = np.asarray(inputs['x']).shape[0]
    in_maps = [make_in_map(inputs, b) for b in range(nb)]
    res = run_bass_kernel_spmd(nc, in_maps, core_ids=list(range(nb)))
    out = np.stack([np.asarray(res.results[b]['out']) for b in range(nb)], 0)
    return out.astype(np.float32)
```

```python
import math
from contextlib import ExitStack

import numpy as np
import concourse.bass as bass
import concourse.mybir as mybir
from concourse.bass_utils import run_bass_kernel_spmd

F32 = mybir.dt.float32
BF16 = mybir.dt.bfloat16
AF = mybir.ActivationFunctionType
ALU = mybir.AluOpType
AX = mybir.AxisListType

D = 1024
NCTX = 256
NLAT = 8192
NT = NCTX + NLAT
NTILE = NT // 128
DEPTH = 2
IN_TOTAL = 6848
LN_EPS = 1e-5
RMS_EPS = 1e-6
ALPHA = (2 * DEPTH) ** 0.25
O_GK, O_GV, O_GAF, O_GAB, O_MKVA, O_MKR, O_GQ, O_GR, O_MQA, O_HY, O_GATES = (
    0, 256, 768, 784, 800, 1056, 1088, 1344, 1856, 2240, 3776)


class Prog:
    def __init__(self, nc, es):
        self.nc = nc
        self.es = es
        self.E = dict(pe=nc.tensor, act=nc.scalar, dve=nc.vector, pool=nc.gpsimd, sp=nc.sync)
        self.sems = {}
        self.cnt = {}
        self.seen = {e: {} for e in self.E}
        self.st = {}
        self.n_ops = 0
        self.alias = {}
        self.free_slots = []
        self.n_slots = 0
        self.persistent = set(['wcast', 'xbzero'])

    def sem(self, key):
        if key not in self.sems:
            self.sems[key] = self.es.enter_context(self.nc.semaphore("s%d" % len(self.sems)))
            self.cnt[key] = 0
        return self.sems[key]

    def _deps(self, reads, writes):
        deps = {}

        def add(k, v):
            if deps.get(k, 0) < v:
                deps[k] = v

        for r in reads:
            s = self.st.get(r)
            if s and s[0]:
                add(*s[0])
        for w in writes:
            s = self.st.get(w)
            if s:
                if s[0]:
                    add(*s[0])
                for k, v in s[1].items():
                    add(k, v)
        return deps

    def _wait(self, eng, deps):
        E = self.E[eng]
        seen = self.seen[eng]
        for k, v in deps.items():
            if k == 'pe' and eng == 'pe':
                continue
            if seen.get(k, 0) < v:
                E.wait_ge(self.sems[k], v)
                seen[k] = v

    def _commit(self, ev, reads, writes):
        k, v = ev
        for r in reads:
            s = self.st.setdefault(r, [None, {}])
            if s[1].get(k, 0) < v:
                s[1][k] = v
        for w in writes:
            self.st[w] = [ev, {}]

    def op(self, eng, reads, writes, fn):
        reads = list(reads)
        writes = list(writes)
        for r in reads:
            if isinstance(r, tuple) and r[0] == 'bank' and r not in writes:
                writes.append(r)
        self._wait(eng, self._deps(reads, writes))
        sem = self.sem(eng)
        ins = fn(self.E[eng])
        self.cnt[eng] += 1
        ins.then_inc(sem, 1)
        self._commit((eng, self.cnt[eng]), reads, writes)
        self.n_ops += 1

    def _slot(self, semkey):
        if semkey in self.persistent:
            return semkey
        if semkey not in self.alias:
            if self.free_slots:
                self.alias[semkey] = self.free_slots.pop()
            else:
                self.alias[semkey] = ('dsem', self.n_slots)
                self.n_slots += 1
        return self.alias[semkey]

    def idma(self, semkey, reads, writes, calls):
        reads = list(reads)
        writes = list(writes)
        self._wait('pool', self._deps(reads, writes))
        key = self._slot(semkey)
        sem = self.sem(key)
        for kw in calls:
            self.nc.gpsimd.indirect_dma_start(**kw).then_inc(sem, 16)
            self.cnt[key] += 16
        self._commit((key, self.cnt[key]), reads, writes)
        self.n_ops += len(calls)

    def dma(self, eng, semkey, reads, writes, pairs, slow=False):
        reads = list(reads)
        writes = list(writes)
        self._wait(eng, self._deps(reads, writes))
        semkey = self._slot(semkey)
        sem = self.sem(semkey)
        for o, i in pairs:
            if slow:
                self.E[eng].dma_start(out=o, in_=i, allow_slow_non_contiguous=True).then_inc(sem, 16)
            else:
                self.E[eng].dma_start(out=o, in_=i).then_inc(sem, 16)
            self.cnt[semkey] += 16
        self._commit((semkey, self.cnt[semkey]), reads, writes)
        self.n_ops += len(pairs)

    def pe_fence(self, ins):
        sem = self.sem('pe')
        self.cnt['pe'] += 1
        ins.then_inc(sem, 1)
        self.E['pe'].wait_ge(sem, self.cnt['pe'])
        self.seen['pe']['pe'] = self.cnt['pe']

    def barrier(self):
        deps = {k: v for k, v in self.cnt.items() if v > 0 and k not in self.persistent}
        for eng in self.E:
            self._wait(eng, dict(deps))
        self.free_slots = [('dsem', i) for i in range(self.n_slots)]
        self.alias = {}

    def finish(self, eng='sp'):
        deps = {}
        for k, c in self.cnt.items():
            if c > 0:
                deps[k] = c
        self._wait(eng, deps)


class Ctx:
    pass


_uid = [0]


def sb(c, name, shape, dt):
    _uid[0] += 1
    return c.es.enter_context(c.nc.sbuf_tensor("%s_u%d" % (name, _uid[0]), list(shape), dt))


def ps(c, name, shape, dt):
    return c.es.enter_context(c.nc.psum_tensor(name, list(shape), dt))


def bank16(c, i):
    return c.bank[i][:, :].bitcast(BF16).rearrange("p (a b) -> p a b", a=8)


def dram(c, name, shape, dt, out=False):
    kind = "ExternalOutput" if (out or name in c.dbg) else "Internal"
    return c.nc.dram_tensor(name, list(shape), dt, kind=kind).ap()


def stage_adaln(c, l):
    with ExitStack() as es:
        c.es = es
        c.adaw = [sb(c, 'adaw%d' % i, [128, 8, 512], F32) for i in range(2)]
        c.adab = [sb(c, 'adab%d' % i, [2, 512], F32) for i in range(2)]
        c.modrow = [sb(c, 'modrow%d' % i, [2, 512], F32) for i in range(2)]
        c.psA = [c.bank[0], c.bank[1]]
        _stage_adaln(c, l)
        c.p.barrier()


def _stage_adaln(c, l):
    p, nc = c.p, c.nc
    I = c.inp
    cT = c.cT
    modv = c.modv[l]
    for cb in range(12):
        slot = cb % 2
        wt = c.adaw[slot]
        p.dma('sp', 'adaw%d' % slot, [], [('adaw', slot)],
              [(wt[:, :, :], I['ada_w'][l, :, cb * 512:(cb + 1) * 512].rearrange("(k p) n -> p k n", p=128))])
        bt = c.adab[slot]
        p.dma('sp', 'adab%d' % slot, [], [('adab', slot)],
              [(bt[0:1, :], I['ada_b'][l:l + 1, cb * 512:(cb + 1) * 512]),
               (bt[1:2, :], I['ada_b'][l:l + 1, cb * 512:(cb + 1) * 512])])
        pt = c.psA[cb % 2]

        def mm(E, wt=wt, pt=pt):
            ins = None
            for k in range(8):
                ins = E.matmul(pt[0:2, :], lhsT=cT[:, k, :], rhs=wt[:, k, :], start=(k == 0), stop=(k == 7))
            return ins
        p.op('pe', [('adaw', slot), 'cT'], [('bank', cb % 2)], mm)
        mt = c.modrow[slot]
        p.op('dve', [('bank', cb % 2), ('adab', slot)], [('modrow', slot)],
             lambda E, mt=mt, pt=pt, bt=bt: E.tensor_tensor(out=mt[0:2, :], in0=pt[0:2, :], in1=bt[0:2, :], op=ALU.add))
        p.dma('sp', 'modrow%d' % slot, [('modrow', slot)], [('modv', l)],
              [(modv[0:2, cb * 512:(cb + 1) * 512], mt[0:2, :])])


def load_mod(c, l):
    p = c.p
    modv = c.modv[l]
    pairs = []
    for r in range(2):
        for g in range(6):
            pairs.append((c.modT[:, r, g, :], modv[r, g * 1024:(g + 1) * 1024].rearrange("(k p) -> p k", p=128)))
    p.dma('sp', 'modT', [('modv', l)], ['modT'], pairs, slow=True)
    p.op('dve', ['modT'], ['modT'],
         lambda E: E.tensor_scalar(out=c.modT[:, :, 1, :], in0=c.modT[:, :, 1, :], scalar1=1.0, scalar2=None, op0=ALU.add))
    p.op('dve', ['modT'], ['modT'],
         lambda E: E.tensor_scalar(out=c.modT[:, :, 4, :], in0=c.modT[:, :, 4, :], scalar1=1.0, scalar2=None, op0=ALU.add))


def ln_mod_tile(c, src_ap, r, gsh, gsc, hT, col0, hkey):
    p = c.p
    i = c.ln_i
    c.ln_i += 1
    s = i % 3
    xt = c.xt[s]
    st = c.lnst[s]
    p.dma('pool', 'xt%d' % s, ['XOUT'], [('xt', s)], [(xt[:, :], src_ap)])
    junk = c.junk[i % 2]
    p.op('act', [('xt', s)], [('junk', i % 2), ('lnst', s, 0)],
         lambda E: E.activation(out=junk[:, :], in_=xt[:, :], func=AF.Copy, accum_out=st[:, 0:1]))
    p.op('dve', [('lnst', s, 0)], [('lnst', s, 1)],
         lambda E: E.tensor_scalar(out=st[:, 1:2], in0=st[:, 0:1], scalar1=-1.0 / D, scalar2=None, op0=ALU.mult))
    p.op('act', [('xt', s), ('lnst', s, 1)], [('junk', i % 2), ('lnst', s, 2)],
         lambda E: E.activation(out=junk[:, :], in_=xt[:, :], func=AF.Square, bias=st[:, 1:2], scale=1.0,
                                accum_out=st[:, 2:3]))
    p.op('act', [('lnst', s, 2)], [('lnst', s, 3)],
         lambda E: E.activation(out=st[:, 3:4], in_=st[:, 2:3], func=AF.Ln, scale=1.0 / D, bias=LN_EPS))
    p.op('act', [('lnst', s, 3)], [('lnst', s, 4)],
         lambda E: E.activation(out=st[:, 4:5], in_=st[:, 3:4], func=AF.Exp, scale=-0.5))
    if c.cfg.get('lnsteps', 9) < 2:
        return
    xh = c.xh[i % 2]
    p.op('dve', [('xt', s), ('lnst', s, 1), ('lnst', s, 4)], [('xh', i % 2)],
         lambda E: E.tensor_scalar(out=xh[:, :], in0=xt[:, :], scalar1=st[:, 1:2], scalar2=st[:, 4:5],
                                   op0=ALU.add, op1=ALU.mult))
    if c.cfg.get('lnsteps', 9) < 3:
        return
    tp = c.tp[i % 2]

    def tr(E):
        ins = None
        for k in range(8):
            ins = E.transpose(tp[:, k, :], xh[:, k * 128:(k + 1) * 128], c.ident[:, :])
        return ins
    p.op('pe', [('xh', i % 2), 'ident'], [c.tpk[i % 2]], tr)
    if c.cfg.get('lnsteps', 9) < 4:
        return
    for k in range(8):
        eng = c.cfg.get('evac_eng') or ('act' if i % 2 == 0 else 'dve')
        if eng == 'act':
            p.op('act', [c.tpk[i % 2], 'modT'], [hkey + (k,)],
                 lambda E, k=k: E.activation(out=hT[:, k, col0:col0 + 128], in_=tp[:, k, :], func=AF.Identity,
                                             bias=c.modT[:, r, gsh, k:k + 1], scale=c.modT[:, r, gsc, k:k + 1]))
        else:
            p.op('dve', [c.tpk[i % 2], 'modT'], [hkey + (k,)],
                 lambda E, k=k: E.tensor_scalar(out=hT[:, k, col0:col0 + 128], in0=tp[:, k, :],
                                                scalar1=c.modT[:, r, gsc, k:k + 1],
                                                scalar2=c.modT[:, r, gsh, k:k + 1],
                                                op0=ALU.mult, op1=ALU.add))


FM_GROUPS = [
    ('KT', O_GK, 256), ('QT', O_GQ, 256), ('AFT', O_GAF, 16), ('ABT', O_GAB, 16),
    ('MKVAT', O_MKVA, 256), ('MQAT', O_MQA, 384),
]
TM_GROUPS = [
    ('V', O_GV, 512), ('MKR', O_MKR, 32), ('GR', O_GR, 512), ('HY', O_HY, 1536), ('G3', O_GATES, 3072),
]


def stage_proj(c, l, xsrc):
    with ExitStack() as es:
        c.es = es
        c.win = sb(c, 'win', [128, 8, IN_TOTAL], BF16)
        c.hT = [sb(c, 'hT%d' % i, [128, 8, 512], BF16) for i in range(2)]
        alloc_ln(c)
        c.o16 = [sb(c, 'o16_%d' % i, [128, 512], BF16) for i in range(4)]
        c.o32 = [sb(c, 'o32_%d' % i, [128, 512], F32) for i in range(4)]
        c.psB = [c.bank[i] for i in range(4)]
        load_mod(c, l)
        _stage_proj(c, l, xsrc)
        c.p.barrier()


def alloc_ln(c):
    c.xt = [sb(c, 'xt%d' % i, [128, D], F32) for i in range(3)]
    c.lnst = [sb(c, 'lnst%d' % i, [128, 8], F32) for i in range(3)]
    c.junk = [sb(c, 'junk%d' % i, [128, D], BF16) for i in range(2)]
    c.xh = [sb(c, 'xh%d' % i, [128, D], BF16) for i in range(2)]
    c.tp = [bank16(c, 4), bank16(c, 5)]
    c.tpk = [('bank', 4), ('bank', 5)]
    c.ln_i = 0


def _stage_proj(c, l, xsrc):
    p, nc = c.p, c.nc
    I = c.inp
    win = c.win
    for k in range(8):
        p.dma('pool', 'win', [], [('win', k)],
              [(win[:, k, :], I['w_in'][l, k * 128:(k + 1) * 128, :])])
    S = c.scr[l]
    blocks = [(0, 2)] + [(2 + 4 * i, 4) for i in range(16)]
    blocks = blocks[:c.cfg.get('nblk', 17)]
    ev = 0
    for bi, (t0, ntl) in enumerate(blocks):
        T = ntl * 128
        tok0 = t0 * 128
        hb = bi % 2
        hT = c.hT[hb]
        r = 1 if bi == 0 else 0
        for j in range(ntl):
            ln_mod_tile(c, xsrc(t0 + j), r, 0, 1, hT, j * 128, ('hT', hb))
        hkeys = [('hT', hb, k) for k in range(8)]
        wkeys = [('win', k) for k in range(8)]
        if c.cfg.get('nomm'):
            continue
        for name, off, ncols in FM_GROUPS:
            for m0 in range(0, ncols, 128):
                M = min(128, ncols - m0)
                pb = ev % 4
                pt = c.psB[pb]

                def mm(E, pt=pt, off=off, m0=m0, M=M, T=T):
                    ins = None
                    for k in range(8):
                        ins = E.matmul(pt[0:M, 0:T], lhsT=win[:, k, off + m0:off + m0 + M], rhs=hT[:, k, 0:T],
                                       start=(k == 0), stop=(k == 7))
                    return ins
                p.op('pe', hkeys + wkeys, [('bank', pb)], mm)
                fp32 = name in ('AFT', 'ABT')
                ob = ev % 4
                ot = (c.o32 if fp32 else c.o16)[ob]
                okey = ('o32' if fp32 else 'o16', ob)
                eng = 'act' if ev % 2 == 0 else 'dve'
                if eng == 'act':
                    p.op('act', [('bank', pb)], [okey],
                         lambda E, ot=ot, pt=pt, M=M, T=T: E.activation(out=ot[0:M, 0:T], in_=pt[0:M, 0:T], func=AF.Copy))
                else:
                    p.op('dve', [('bank', pb)], [okey],
                         lambda E, ot=ot, pt=pt, M=M, T=T: E.tensor_copy(out=ot[0:M, 0:T], in_=pt[0:M, 0:T]))
                p.dma('sp', 'o%s%d' % ('32' if fp32 else '16', ob), [okey], [(name, l)],
                      [(S[name][m0:m0 + M, tok0:tok0 + T], ot[0:M, 0:T])])
                ev += 1
        for name, off, ncols in TM_GROUPS:
            for n0 in range(0, ncols, 512):
                N = min(512, ncols - n0)
                for j in range(ntl):
                    pb = ev % 4
                    pt = c.psB[pb]

                    def mm(E, pt=pt, off=off, n0=n0, N=N, j=j):
                        ins = None
                        for k in range(8):
                            ins = E.matmul(pt[:, 0:N], lhsT=hT[:, k, j * 128:(j + 1) * 128],
                                           rhs=win[:, k, off + n0:off + n0 + N], start=(k == 0), stop=(k == 7))
                        return ins
                    p.op('pe', hkeys + wkeys, [('bank', pb)], mm)
                    fp32 = name == 'MKR'
                    ob = ev % 4
                    ot = (c.o32 if fp32 else c.o16)[ob]
                    okey = ('o32' if fp32 else 'o16', ob)
                    if name == 'G3':
                        p.op('act', [('bank', pb)], [okey],
                             lambda E, ot=ot, pt=pt, N=N: E.activation(out=ot[:, 0:N], in_=pt[:, 0:N], func=AF.Sigmoid))
                    elif ev % 2 == 0:
                        p.op('act', [('bank', pb)], [okey],
                             lambda E, ot=ot, pt=pt, N=N: E.activation(out=ot[:, 0:N], in_=pt[:, 0:N], func=AF.Copy))
                    else:
                        p.op('dve', [('bank', pb)], [okey],
                             lambda E, ot=ot, pt=pt, N=N: E.tensor_copy(out=ot[:, 0:N], in_=pt[:, 0:N]))
                    p.dma('sp', 'o%s%d' % ('32' if fp32 else '16', ob), [okey], [(name, l)],
                          [(S[name][tok0 + j * 128:tok0 + (j + 1) * 128, n0:n0 + N], ot[:, 0:N])])
                    ev += 1


def stage_gla(c, l):
    with ExitStack() as es:
        c.es = es
        _stage_gla(c, l)
        c.p.barrier()


def _stage_gla(c, l):
    p, nc = c.p, c.nc
    I = c.inp
    S = c.scr[l]
    NCH = NTILE
    qT = sb(c, 'g_qT', [128, NT], BF16)
    kT = sb(c, 'g_kT', [128, NT], BF16)
    v = sb(c, 'g_v', [128, NCH, 256], BF16)
    oacc = sb(c, 'g_oacc', [128, NCH, 256], F32)
    wa2 = sb(c, 'g_wa2', [16, 2, 256], F32)
    ba = sb(c, 'g_ba', [1, 2, 256], F32)
    normw = sb(c, 'g_normw', [128, 128], F32)
    aft = [sb(c, 'g_aft%d' % i, [16, 128], F32) for i in range(3)]
    g1 = [sb(c, 'g_g1%d' % i, [128, 128], F32) for i in range(2)]
    g2 = [sb(c, 'g_g2%d' % i, [128, 128], F32) for i in range(2)]
    e1 = [sb(c, 'g_e1%d' % i, [128, 128], F32) for i in range(2)]
    e2 = [sb(c, 'g_e2%d' % i, [128, 128], F32) for i in range(2)]
    qb = [sb(c, 'g_qb%d' % i, [128, 128], BF16) for i in range(2)]
    kb = [sb(c, 'g_kb%d' % i, [128, 128], BF16) for i in range(2)]
    kd = [sb(c, 'g_kd%d' % i, [128, 128], BF16) for i in range(2)]
    kdt = [sb(c, 'g_kdt%d' % i, [128, 128], BF16) for i in range(2)]
    am = [sb(c, 'g_am%d' % i, [128, 256], BF16) for i in range(2)]
    St = sb(c, 'g_S', [128, 128], F32)
    Sb = sb(c, 'g_Sb', [128, 128], BF16)
    rst = sb(c, 'g_rst', [128, NCH * 2], F32)
    rst2 = sb(c, 'g_rst2', [128, NCH * 2], F32)
    junk = sb(c, 'g_junk', [128, 128], BF16)
    osb = [sb(c, 'g_osb%d' % i, [128, 512], BF16) for i in range(2)]
    psG = c.bank[0][:, 0:128]
    psBt = [c.bank[1][:, 0:128], c.bank[2][:, 0:128]]
    psA = c.bank[3][:, 0:256]
    psK = c.bank[4][:, :].bitcast(BF16)[:, 0:128]
    psO = [c.bank[5][:, 0:256], c.bank[6][:, 0:256]]
    psS = c.bank[7][:, 0:128]
    c.tp = [bank16(c, 1), bank16(c, 2)]
    cst = c.cst
    U16 = [cst[:, 128:256], cst[:, 256:384]]
    MSK = [cst[:, 384:512], cst[:, 512:640]]
    ones = cst[:, 640:768]

    p.dma('sp', 'g_w', [], ['g_w'],
          [(wa2[:, 0, :], I['gla_wa2_f'][l]), (wa2[:, 1, :], I['gla_wa2_b'][l]),
           (ba[0:1, 0, :], I['gla_ba_f'][l:l + 1, :]), (ba[0:1, 1, :], I['gla_ba_b'][l:l + 1, :]),
           (normw[:, :], I['gla_norm'][l, :].partition_broadcast(128))])
    gr = v
    step = 0
    for hp in range(2):
        p.dma('sp', 'g_q', [], ['g_qT'], [(qT[:, :], S['QT'][hp * 128:(hp + 1) * 128, :])])
        p.dma('sp', 'g_k', [], ['g_kT'], [(kT[:, :], S['KT'][hp * 128:(hp + 1) * 128, :])])
        p.dma('sp', 'g_v', [], ['g_v'],
              [(v[:, :, :], S['V'][:, hp * 256:(hp + 1) * 256].rearrange("(n p) c -> p n c", p=128))])
        for dirn in range(2):
            p.op('dve', [], ['g_S'], lambda E: E.memset(St[:, :], 0.0))
            p.op('dve', [], ['g_Sb'], lambda E: E.memset(Sb[:, :], 0.0))
            order = list(range(NCH)) if dirn == 0 else [1, 0] + list(range(NCH - 1, 1, -1))
            last = 127 if dirn == 0 else 0
            gsrc = S['AFT'] if dirn == 0 else S['ABT']
            for n in order[:c.cfg.get('gsteps', 999)]:
                t0 = n * 128
                s2 = step % 2
                s3 = step % 3
                step += 1
                a_t = aft[s3]
                p.dma('sp', 'g_aft%d' % s3, [], [('g_aft', s3)], [(a_t[:, :], gsrc[:, t0:t0 + 128])])

                def mmg(E, a_t=a_t, dirn=dirn, hp=hp):
                    E.matmul(psG[:, :], lhsT=a_t[0:16, :], rhs=wa2[0:16, dirn, hp * 128:(hp + 1) * 128],
                             start=True, stop=False)
                    return E.matmul(psG[:, :], lhsT=ones[0:1, :], rhs=ba[0:1, dirn, hp * 128:(hp + 1) * 128],
                                    start=False, stop=True)
                p.op('pe', [('g_aft', s3), 'g_w', 'cst'], [('bank', 0)], mmg)
                if c.cfg.get('gsub', 99) < 1:
                    continue
                p.op('act', [('bank', 0)], [('g_g1', s2)],
                     lambda E, s2=s2: E.activation(out=g1[s2][:, :], in_=psG[:, :], func=AF.Exp, scale=-1.0))
                p.op('act', [('g_g1', s2)], [('g_g2', s2)],
                     lambda E, s2=s2: E.activation(out=g2[s2][:, :], in_=g1[s2][:, :], func=AF.Ln, bias=1.0, scale=1.0))
                if c.cfg.get('gsub', 99) < 2:
                    continue
                pB = psBt[s2]
                p.op('pe', [('g_g2', s2), 'cst'], [('bank', 1 + s2)],
                     lambda E, s2=s2, pB=pB, dirn=dirn: E.matmul(pB[:, :], lhsT=g2[s2][:, :], rhs=U16[dirn],
                                                                 start=True, stop=True))
                p.op('act', [('bank', 1 + s2)], [('g_e1', s2)],
                     lambda E, s2=s2, pB=pB: E.activation(out=e1[s2][:, :], in_=pB[:, :], func=AF.Exp, scale=-1.0))
                p.op('act', [('bank', 1 + s2)], [('g_e2', s2)],
                     lambda E, s2=s2, pB=pB: E.activation(out=e2[s2][:, :], in_=pB[:, :], func=AF.Exp, scale=1.0))
                if c.cfg.get('gsub', 99) < 3:
                    continue
                p.op('dve', ['g_qT', ('g_e1', s2)], [('g_qb', s2)],
                     lambda E, s2=s2, t0=t0: E.scalar_tensor_tensor(out=qb[s2][:, :], in0=qT[:, t0:t0 + 128], scalar=0.125,
                                                                    in1=e1[s2][:, :], op0=ALU.mult, op1=ALU.mult))
                p.op('dve', ['g_kT', ('g_e2', s2)], [('g_kb', s2)],
                     lambda E, s2=s2, t0=t0: E.tensor_tensor(out=kb[s2][:, :], in0=kT[:, t0:t0 + 128], in1=e2[s2][:, :],
                                                             op=ALU.mult))
                p.op('dve', [('g_kb', s2), ('g_e1', s2)], [('g_kd', s2)],
                     lambda E, s2=s2, last=last: E.tensor_scalar(out=kd[s2][:, :], in0=kb[s2][:, :],
                                                                 scalar1=e1[s2][:, last:last + 1], scalar2=None,
                                                                 op0=ALU.mult))

                if c.cfg.get('gsub', 99) < 4:
                    continue
                def mma(E, s2=s2):
                    ins = None
                    for h in range(2):
                        if h == 1:
                            p.pe_fence(ins)
                        ins = E.matmul(psA[:, h * 128:(h + 1) * 128], lhsT=kb[s2][h * 64:(h + 1) * 64, :],
                                       rhs=qb[s2][h * 64:(h + 1) * 64, :], start=True, stop=True)
                    return ins
                p.op('pe', [('g_kb', s2), ('g_qb', s2)], [('bank', 3)], mma)
                if c.cfg.get('gsub', 99) < 5:
                    continue
                msk = MSK[dirn]
                p.op('dve', [('bank', 3), 'cst'], [('g_am', s2)],
                     lambda E, s2=s2, msk=msk: E.tensor_tensor(
                         out=am[s2][:, :].rearrange("p (h i) -> p h i", h=2),
                         in0=psA[:, :].rearrange("p (h i) -> p h i", h=2),
                         in1=msk.unsqueeze(1).to_broadcast([128, 2, 128]), op=ALU.mult))
                if c.cfg.get('gsub', 99) < 6:
                    continue
                p.op('pe', [('g_kd', s2), 'ident'], [('bank', 4)],
                     lambda E, s2=s2: E.transpose(psK[:, :], kd[s2][:, :], c.ident[:, :]))
                p.op('act', [('bank', 4)], [('g_kdt', s2)],
                     lambda E, s2=s2: E.activation(out=kdt[s2][:, :], in_=psK[:, :], func=AF.Copy))
                if c.cfg.get('gsub', 99) < 7:
                    continue
                pO = psO[s2]

                def mmo(E, s2=s2, pO=pO, n=n):
                    ins = None
                    for h in range(2):
                        if h == 1:
                            p.pe_fence(ins)
                        E.matmul(pO[:, h * 128:(h + 1) * 128], lhsT=am[s2][:, h * 128:(h + 1) * 128],
                                 rhs=v[:, n, h * 128:(h + 1) * 128], start=True, stop=False)
                        ins = E.matmul(pO[:, h * 128:(h + 1) * 128], lhsT=qb[s2][h * 64:(h + 1) * 64, :],
                                       rhs=Sb[h * 64:(h + 1) * 64, :], start=False, stop=True)
                    return ins
                p.op('pe', [('g_am', s2), 'g_v', ('g_qb', s2), 'g_Sb'], [('bank', 5 + s2)], mmo)
                if dirn == 0:
                    p.op('act', [('bank', 5 + s2)], [('g_oacc', n)],
                         lambda E, pO=pO, n=n: E.activation(out=oacc[:, n, :], in_=pO[:, :], func=AF.Copy))
                else:
                    p.op('dve', [('bank', 5 + s2), ('g_oacc', n)], [('g_oacc', n)],
                         lambda E, pO=pO, n=n: E.tensor_tensor(out=oacc[:, n, :], in0=pO[:, :], in1=oacc[:, n, :],
                                                               op=ALU.add))

                if c.cfg.get('gsub', 99) < 8:
                    continue
                def mms(E, s2=s2, n=n):
                    ins = None
                    for h in range(2):
                        if h == 1:
                            p.pe_fence(ins)
                        ins = E.matmul(psS[h * 64:(h + 1) * 64, :], lhsT=kdt[s2][:, h * 64:(h + 1) * 64],
                                       rhs=v[:, n, h * 128:(h + 1) * 128], start=True, stop=True)
                    return ins
                p.op('pe', [('g_kdt', s2), 'g_v'], [('bank', 7)], mms)
                if c.cfg.get('gsub', 99) < 9:
                    continue
                p.op('dve', [('bank', 7), ('g_e1', s2), 'g_S'], ['g_S'],
                     lambda E, s2=s2, last=last: E.scalar_tensor_tensor(out=St[:, :], in0=St[:, :],
                                                                        scalar=e1[s2][:, last:last + 1], in1=psS[:, :],
                                                                        op0=ALU.mult, op1=ALU.add))
                p.op('act', ['g_S'], ['g_Sb'], lambda E: E.activation(out=Sb[:, :], in_=St[:, :], func=AF.Copy))
        if c.cfg.get('gnofin'):
            continue
        okeys = [('g_oacc', n) for n in range(NCH)]
        for n in range(NCH):
            for h in range(2):
                p.op('act', [('g_oacc', n)], ['g_junk', ('g_rst', n, h)],
                     lambda E, n=n, h=h: E.activation(out=junk[:, :], in_=oacc[:, n, h * 128:(h + 1) * 128],
                                                      func=AF.Square, accum_out=rst[:, n * 2 + h:n * 2 + h + 1]))
        rkeys = [('g_rst', n, h) for n in range(NCH) for h in range(2)]
        p.op('act', rkeys, ['g_rst2'],
             lambda E: E.activation(out=rst2[:, :], in_=rst[:, :], func=AF.Ln, scale=1.0 / 128, bias=RMS_EPS))
        p.op('act', ['g_rst2'], ['g_rst2'],
             lambda E: E.activation(out=rst2[:, :], in_=rst2[:, :], func=AF.Exp, scale=-0.5))
        p.dma('sp', 'g_v', [], ['g_v'],
              [(gr[:, :, :], S['GR'][:, hp * 256:(hp + 1) * 256].rearrange("(n p) c -> p n c", p=128))])
        p.op('act', ['g_v'], ['g_v'], lambda E: E.activation(out=gr[:, :, :], in_=gr[:, :, :], func=AF.Silu))
        o4 = oacc[:, :, :].rearrange("p n (h d) -> p (n h) d", h=2)
        p.op('dve', okeys + ['g_rst2'], okeys,
             lambda E: E.tensor_tensor(out=o4, in0=o4, in1=rst2[:, :].unsqueeze(2).to_broadcast([128, NCH * 2, 128]),
                                       op=ALU.mult))
        p.op('dve', okeys + ['g_w'], okeys,
             lambda E: E.tensor_tensor(out=o4, in0=o4, in1=normw[:, :].unsqueeze(1).to_broadcast([128, NCH * 2, 128]),
                                       op=ALU.mult))
        p.op('dve', okeys + ['g_v'], ['g_v'],
             lambda E: E.tensor_tensor(out=gr[:, :, :], in0=oacc[:, :, :], in1=gr[:, :, :], op=ALU.mult))
        groups = [(0, 2)] + [(2 + 4 * i, 4) for i in range(16)]
        for gi, (n0, cnt) in enumerate(groups):
            for h in range(2):
                tb = (gi * 2 + h) % 2
                tpt = c.tp[tb]

                def tr(E, n0=n0, cnt=cnt, h=h, tpt=tpt):
                    ins = None
                    for j in range(cnt):
                        ins = E.transpose(tpt[:, j, :], gr[:, n0 + j, h * 128:(h + 1) * 128], c.ident[:, :])
                    return ins
                p.op('pe', ['g_v', 'ident'], [('bank', 1 + tb)], tr)
                ot = osb[tb]
                T = cnt * 128
                if tb == 0:
                    p.op('act', [('bank', 1 + tb)], [('g_osb', tb)],
                         lambda E, ot=ot, tpt=tpt, T=T: E.activation(out=ot[:, 0:T], in_=tpt[:, :, :].rearrange("p a b -> p (a b)")[:, 0:T], func=AF.Copy))
                else:
                    p.op('dve', [('bank', 1 + tb)], [('g_osb', tb)],
                         lambda E, ot=ot, tpt=tpt, T=T: E.tensor_copy(out=ot[:, 0:T], in_=tpt[:, :, :].rearrange("p a b -> p (a b)")[:, 0:T]))
                row0 = (hp * 2 + h) * 128
                p.dma('sp', 'g_osb%d' % tb, [('g_osb', tb)], [('OGT', l)],
                      [(S['OGT'][row0:row0 + 128, n0 * 128:n0 * 128 + T], ot[:, 0:T])])


MLA_SCALE = 96 ** -0.5


def stage_mla(c, l, ctx_q):
    with ExitStack() as es:
        c.es = es
        _stage_mla(c, l, ctx_q)
        c.p.barrier()


def _rms_rstd(c, psq, rs, nfeat, key_ps, key_rs):
    p = c.p
    p.op('act', [key_ps], [key_rs],
         lambda E: E.activation(out=rs, in_=psq, func=AF.Ln, scale=1.0 / nfeat, bias=RMS_EPS))
    p.op('act', [key_rs], [key_rs], lambda E: E.activation(out=rs, in_=rs, func=AF.Exp, scale=-0.5))


def _stage_mla(c, l, ctx_q):
    p, nc = c.p, c.nc
    I = c.inp
    S = c.scr[l]
    KpT, VpD, QpT = c.KpT, c.VpD, c.QpT
    cst = c.cst
    ones32 = cst[:, 640:768]
    wkv32 = sb(c, 'm_wkv32', [128, 2, 1024], F32)
    wq32 = sb(c, 'm_wq32', [128, 3, 768], F32)
    wkv = sb(c, 'm_wkv', [128, 2, 1024], BF16)
    wq = sb(c, 'm_wq', [128, 3, 768], BF16)
    gn = sb(c, 'm_gn', [128, 5], F32)
    onesb = sb(c, 'm_onesb', [128, 8], BF16)
    p.dma('sp', 'm_w', [], ['m_w32'],
          [(wkv32[:, :, :], I['mla_w_ukv'][l].rearrange("(k p) n -> p k n", p=128)),
           (wq32[:, :, :], I['mla_w_uq'][l].rearrange("(k p) n -> p k n", p=128))])
    p.dma('sp', 'm_g', [], ['m_gn'],
          [(gn[:, 0:2], I['mla_kv_norm'][l, :].rearrange("(k p) -> p k", p=128)),
           (gn[:, 2:5], I['mla_q_norm'][l, :].rearrange("(k p) -> p k", p=128))], slow=True)
    p.op('dve', [], ['m_onesb'], lambda E: E.memset(onesb[:, :], 1.0))
    for k in range(2):
        p.op('dve', ['m_w32', 'm_gn'], [('m_wkv', k)],
             lambda E, k=k: E.tensor_scalar(out=wkv[:, k, :], in0=wkv32[:, k, :], scalar1=gn[:, k:k + 1], scalar2=None,
                                            op0=ALU.mult))
    for k in range(3):
        p.op('dve', ['m_w32', 'm_gn'], [('m_wq', k)],
             lambda E, k=k: E.tensor_scalar(out=wq[:, k, :], in0=wq32[:, k, :], scalar1=gn[:, 2 + k:3 + k], scalar2=None,
                                            op0=ALU.mult))
    wkvk = [('m_wkv', k) for k in range(2)]
    wqk = [('m_wq', k) for k in range(3)]
    src = [sb(c, 'm_src%d' % i, [128, 3, 512], BF16) for i in range(2)]
    sq = [sb(c, 'm_sq%d' % i, [128, 3, 512], BF16) for i in range(2)]
    rs = [sb(c, 'm_rs%d' % i, [128, 2], F32) for i in range(2)]
    kr = [sb(c, 'm_kr%d' % i, [128, 32], F32) for i in range(2)]
    krr = [sb(c, 'm_krr%d' % i, [128, 32], F32) for i in range(2)]
    rtab = [sb(c, 'm_rtab%d' % i, [128, 32], F32) for i in range(2)]
    tmp16 = [sb(c, 'm_tmp%d' % i, [128, 16], F32) for i in range(2)]
    kp = [sb(c, 'm_kp%d' % i, [128, 8, 97], BF16) for i in range(2)]
    vp = [sb(c, 'm_vp%d' % i, [128, 8, 65], BF16) for i in range(2)]
    kpt = [sb(c, 'm_kpt%d' % i, [97, 8, 128], BF16) for i in range(2)]
    qf = [sb(c, 'm_qf%d' % i, [128, 8, 96], F32) for i in range(2)]
    qsq = sb(c, 'm_qsq', [128, 8, 96], F32)
    ks = sb(c, 'm_ks', [128, 8], F32)
    kmx = sb(c, 'm_kmx', [128, 8], F32)
    kmT = sb(c, 'm_kmT', [8, 1], F32)
    kdiag = sb(c, 'm_kdiag', [8, 8], F32)
    kbc = sb(c, 'm_kbc', [128, 8], F32)
    qn = [sb(c, 'm_qn%d' % i, [128, 8], F32) for i in range(2)]
    B = c.bank
    bk = lambda i: ('bank', i)
    for i in range(2):
        p.op('dve', [], [('m_kp', i)], lambda E, i=i: E.memset(kp[i][:, :, :], 1.0))
        p.op('dve', [], [('m_vp', i)], lambda E, i=i: E.memset(vp[i][:, :, :], 1.0))
    p.op('dve', [], ['m_kmx'], lambda E: E.memset(kmx[:, :], 0.0))

    blocks = [(0, 2)] + [(2 + 4 * i, 4) for i in range(16)]
    it = 0

    def load_src(name, nk, t0, ntl, sslot):
        T = ntl * 128
        p.dma('pool', 'm_src%d' % sslot, [], [('m_src', sslot)],
              [(src[sslot][:, 0:nk, 0:T], S[name][:, t0 * 128:t0 * 128 + T].rearrange("(k p) t -> p k t", p=128))])
        p.op('act', [('m_src', sslot)], [('m_sq', sslot)],
             lambda E: E.activation(out=sq[sslot][:, 0:nk, 0:T], in_=src[sslot][:, 0:nk, 0:T], func=AF.Square))

    def rope_rows(t, s2):
        p.dma('pool', 'm_rtab%d' % s2, [], [('m_rtab', s2)], [(rtab[s2][:, :], I['rope'][t * 128:(t + 1) * 128, :])])

    for bi, (tb0, ntl) in enumerate(blocks):
        sslot = bi % 2
        load_src('MKVAT', 2, tb0, ntl, sslot)
        for j in range(ntl):
            t = tb0 + j
            s2 = it % 2
            it += 1
            cols = slice(j * 128, (j + 1) * 128)

            def mmq(E, sslot=sslot, cols=cols):
                ins = None
                for k in range(2):
                    ins = E.matmul(B[0][:, 0:1], lhsT=sq[sslot][:, k, cols], rhs=onesb[:, 0:1], start=(k == 0), stop=(k == 1))
                return ins
            p.op('pe', [('m_sq', sslot), 'm_onesb'], [bk(0)], mmq)
            _rms_rstd(c, B[0][:, 0:1], rs[s2][:, 0:1], 256, bk(0), ('m_rs', s2))
            for half in range(2):
                def mmkv(E, sslot=sslot, cols=cols, half=half):
                    ins = None
                    for k in range(2):
                        ins = E.matmul(B[1 + half][:, :], lhsT=src[sslot][:, k, cols],
                                       rhs=wkv[:, k, half * 512:(half + 1) * 512], start=(k == 0), stop=(k == 1))
                    return ins
                p.op('pe', [('m_src', sslot)] + wkvk, [bk(1 + half)], mmkv)
            for half in range(2):
                pv = B[1 + half][:, :].rearrange("p (h e) -> p h e", h=4)
                p.op('dve', [bk(1 + half), ('m_rs', s2)], [('m_kp', s2)],
                     lambda E, pv=pv, half=half, s2=s2: E.tensor_scalar(out=kp[s2][:, half * 4:(half + 1) * 4, 0:64], in0=pv[:, :, 0:64],
                                                                        scalar1=rs[s2][:, 0:1], scalar2=None, op0=ALU.mult))
                p.op('act', [bk(1 + half), ('m_rs', s2)], [('m_vp', s2)],
                     lambda E, pv=pv, half=half, s2=s2: E.activation(out=vp[s2][:, half * 4:(half + 1) * 4, 0:64], in_=pv[:, :, 64:128],
                                                                     func=AF.Copy, scale=rs[s2][:, 0:1]))
            p.dma('pool', 'm_kr%d' % s2, [], [('m_kr', s2)], [(kr[s2][:, :], S['MKR'][t * 128:(t + 1) * 128, :])])
            if t >= 2:
                rope_rows(t - 2, s2)
                x1, x2 = kr[s2][:, 0:16], kr[s2][:, 16:32]
                cs, sn = rtab[s2][:, 0:16], rtab[s2][:, 16:32]
                o1, o2 = krr[s2][:, 0:16], krr[s2][:, 16:32]
                tm = tmp16[s2]
                rk = [('m_kr', s2), ('m_rtab', s2)]
                p.op('dve', rk, [('m_krr', s2)], lambda E, o1=o1, x1=x1, cs=cs: E.tensor_tensor(out=o1, in0=x1, in1=cs, op=ALU.mult))
                p.op('dve', rk, [('m_tmp', s2)], lambda E, tm=tm, x2=x2, sn=sn: E.tensor_tensor(out=tm[:, :], in0=x2, in1=sn, op=ALU.mult))
                p.op('dve', [('m_krr', s2), ('m_tmp', s2)], [('m_krr', s2)],
                     lambda E, o1=o1, tm=tm: E.tensor_tensor(out=o1, in0=o1, in1=tm[:, :], op=ALU.subtract))
                p.op('dve', rk + [('m_krr', s2)], [('m_krr', s2)], lambda E, o2=o2, x2=x2, cs=cs: E.tensor_tensor(out=o2, in0=x2, in1=cs, op=ALU.mult))
                p.op('dve', rk + [('m_tmp', s2)], [('m_tmp', s2)], lambda E, tm=tm, x1=x1, sn=sn: E.tensor_tensor(out=tm[:, :], in0=x1, in1=sn, op=ALU.mult))
                p.op('dve', [('m_krr', s2), ('m_tmp', s2)], [('m_krr', s2)],
                     lambda E, o2=o2, tm=tm: E.tensor_tensor(out=o2, in0=o2, in1=tm[:, :], op=ALU.add))
                rsrc, rkey = krr[s2], ('m_krr', s2)
            else:
                rsrc, rkey = kr[s2], ('m_kr', s2)
            p.op('dve', [rkey, ('m_kp', s2)], [('m_kp', s2)],
                 lambda E, rsrc=rsrc, s2=s2: E.tensor_copy(out=kp[s2][:, :, 64:96],
                                                           in_=rsrc[:, :].unsqueeze(1).to_broadcast([128, 8, 32])))
            p.op('dve', [('m_kp', s2)], ['m_qsq'],
                 lambda E, s2=s2: E.tensor_tensor(out=qsq[:, :, :], in0=kp[s2][:, :, 0:96], in1=kp[s2][:, :, 0:96], op=ALU.mult))
            p.op('dve', ['m_qsq'], ['m_ks'], lambda E: E.tensor_reduce(out=ks[:, :], in_=qsq[:, :, :], axis=AX.X, op=ALU.add))
            p.op('dve', ['m_ks', 'm_kmx'], ['m_kmx'], lambda E: E.tensor_tensor(out=kmx[:, :], in0=kmx[:, :], in1=ks[:, :], op=ALU.max))
            tpb = 3 + s2
            tpv = B[tpb][:, :].bitcast(BF16).rearrange("p (h t) -> p h t", h=8)

            def trk(E, s2=s2, tpv=tpv):
                ins = None
                for h in range(8):
                    ins = E.transpose(tpv[0:97, h, :], kp[s2][:, h, :], c.ident[:, :])
                return ins
            p.op('pe', [('m_kp', s2), 'ident'], [bk(tpb)], trk)
            p.op('act', [bk(tpb)], [('m_kpt', s2)],
                 lambda E, s2=s2, tpv=tpv: E.activation(out=kpt[s2][:, :, :], in_=tpv[0:97, :, :], func=AF.Copy))
            p.dma('sp', 'm_kpt%d' % s2, [('m_kpt', s2)], ['KpT'],
                  [(KpT[:, :, t * 128:(t + 1) * 128].rearrange("h d t -> d h t"), kpt[s2][:, :, :])])
            p.dma('sp', 'm_vp%d' % s2, [('m_vp', s2)], ['VpD'],
                  [(VpD[:, t * 128:(t + 1) * 128, :].rearrange("h p e -> p h e"), vp[s2][:, :, :])])
    p.op('pe', ['m_kmx', 'cst'], [bk(0)], lambda E: E.transpose(B[0][0:8, 0:128], kmx[:, :], cst[:, 0:128]))
    p.op('dve', [bk(0)], ['m_kmT'], lambda E: E.tensor_reduce(out=kmT[:, :], in_=B[0][0:8, 0:128], axis=AX.X, op=ALU.max))
    p.op('dve', ['m_kmT', 'cst'], ['m_kdiag'],
         lambda E: E.tensor_scalar(out=kdiag[:, :], in0=cst[0:8, 0:8], scalar1=kmT[:, 0:1], scalar2=None, op0=ALU.mult))
    p.op('pe', ['m_kdiag', 'cst'], [bk(0)],
         lambda E: E.matmul(B[0][:, 0:8], lhsT=ones32[0:8, :], rhs=kdiag[:, :], start=True, stop=True))
    p.op('act', [bk(0)], ['m_kbc'], lambda E: E.activation(out=kbc[:, :], in_=B[0][:, 0:8], func=AF.Copy))
    qblocks = ([(0, 2)] if ctx_q else []) + [(2 + 4 * i, 4) for i in range(16)]
    for bi, (tb0, ntl) in enumerate(qblocks):
        sslot = bi % 2
        load_src('MQAT', 3, tb0, ntl, sslot)
        for j in range(ntl):
            t = tb0 + j
            s2 = it % 2
            it += 1
            cols = slice(j * 128, (j + 1) * 128)

            def mmq(E, sslot=sslot, cols=cols):
                ins = None
                for k in range(3):
                    ins = E.matmul(B[0][:, 0:1], lhsT=sq[sslot][:, k, cols], rhs=onesb[:, 0:1], start=(k == 0), stop=(k == 2))
                return ins
            p.op('pe', [('m_sq', sslot), 'm_onesb'], [bk(0)], mmq)
            _rms_rstd(c, B[0][:, 0:1], rs[s2][:, 0:1], 384, bk(0), ('m_rs', s2))
            p.op('dve', [('m_rs', s2)], [('m_rs', s2)],
                 lambda E, s2=s2: E.tensor_scalar(out=rs[s2][:, 0:1], in0=rs[s2][:, 0:1], scalar1=MLA_SCALE, scalar2=None, op0=ALU.mult))
            for half, (n0, nn) in enumerate([(0, 512), (512, 256)]):
                def mmqq(E, sslot=sslot, cols=cols, half=half, n0=n0, nn=nn):
                    ins = None
                    for k in range(3):
                        ins = E.matmul(B[1 + half][:, 0:nn], lhsT=src[sslot][:, k, cols], rhs=wq[:, k, n0:n0 + nn],
                                       start=(k == 0), stop=(k == 2))
                    return ins
                p.op('pe', [('m_src', sslot)] + wqk, [bk(1 + half)], mmqq)
            qv = qf[s2][:, :, :].rearrange("p h e -> p (h e)")
            p.op('dve', [bk(1), ('m_rs', s2)], [('m_qf', s2, 0)],
                 lambda E, qv=qv, s2=s2: E.tensor_scalar(out=qv[:, 0:512], in0=B[1][:, 0:512], scalar1=rs[s2][:, 0:1], scalar2=None, op0=ALU.mult))
            p.op('act', [bk(2), ('m_rs', s2)], [('m_qf', s2, 1)],
                 lambda E, qv=qv, s2=s2: E.activation(out=qv[:, 512:768], in_=B[2][:, 0:256], func=AF.Copy, scale=rs[s2][:, 0:1]))
            qk = [('m_qf', s2, 0), ('m_qf', s2, 1)]
            kpq = kp[s2]
            if t >= 2:
                rope_rows(t - 2, s2)
                x1, x2 = qf[s2][:, :, 64:80], qf[s2][:, :, 80:96]
                cs = rtab[s2][:, 0:16].unsqueeze(1).to_broadcast([128, 8, 16])
                sn = rtab[s2][:, 16:32].unsqueeze(1).to_broadcast([128, 8, 16])
                ta, tb_ = qsq[:, :, 0:16], qsq[:, :, 16:32]
                tc_, td = qsq[:, :, 32:48], qsq[:, :, 48:64]
                rk = qk + [('m_rtab', s2)]
                p.op('dve', rk, ['m_qsq'], lambda E, ta=ta, x1=x1, cs=cs: E.tensor_tensor(out=ta, in0=x1, in1=cs, op=ALU.mult))
                p.op('dve', rk + ['m_qsq'], ['m_qsq'], lambda E, tb_=tb_, x2=x2, sn=sn: E.tensor_tensor(out=tb_, in0=x2, in1=sn, op=ALU.mult))
                p.op('dve', rk + ['m_qsq'], ['m_qsq'], lambda E, tc_=tc_, x2=x2, cs=cs: E.tensor_tensor(out=tc_, in0=x2, in1=cs, op=ALU.mult))
                p.op('dve', rk + ['m_qsq'], ['m_qsq'], lambda E, td=td, x1=x1, sn=sn: E.tensor_tensor(out=td, in0=x1, in1=sn, op=ALU.mult))
                p.op('dve', ['m_qsq'] + qk, qk, lambda E, x1=x1, ta=ta, tb_=tb_: E.tensor_tensor(out=x1, in0=ta, in1=tb_, op=ALU.subtract))
                p.op('dve', ['m_qsq'] + qk, qk, lambda E, x2=x2, tc_=tc_, td=td: E.tensor_tensor(out=x2, in0=tc_, in1=td, op=ALU.add))
            p.op('dve', qk, ['m_qsq'],
                 lambda E, s2=s2: E.tensor_tensor(out=qsq[:, :, :], in0=qf[s2][:, :, :], in1=qf[s2][:, :, :], op=ALU.mult))
            p.op('dve', ['m_qsq'], [('m_qn', s2)], lambda E, s2=s2: E.tensor_reduce(out=qn[s2][:, :], in_=qsq[:, :, :], axis=AX.X, op=ALU.add))
            p.op('dve', [('m_qn', s2), 'm_kbc'], [('m_qn', s2)],
                 lambda E, s2=s2: E.tensor_tensor(out=qn[s2][:, :], in0=qn[s2][:, :], in1=kbc[:, :], op=ALU.mult))
            p.op('act', [('m_qn', s2)], [('m_qn', s2)], lambda E, s2=s2: E.activation(out=qn[s2][:, :], in_=qn[s2][:, :], func=AF.Sqrt))
            p.op('dve', qk + [('m_kp', s2)], [('m_kp', s2)],
                 lambda E, s2=s2: E.tensor_copy(out=kpq[:, :, 0:96], in_=qf[s2][:, :, :]))
            p.op('dve', [('m_qn', s2), ('m_kp', s2)], [('m_kp', s2)],
                 lambda E, s2=s2: E.tensor_scalar(out=kpq[:, :, 96:97], in0=qn[s2][:, :].unsqueeze(2), scalar1=-1.0, scalar2=None, op0=ALU.mult))
            tpb = 3 + s2
            tpv = B[tpb][:, :].bitcast(BF16).rearrange("p (h t) -> p h t", h=8)

            def trq(E, s2=s2, tpv=tpv):
                ins = None
                for h in range(8):
                    ins = E.transpose(tpv[0:97, h, :], kp[s2][:, h, :], c.ident[:, :])
                return ins
            p.op('pe', [('m_kp', s2), 'ident'], [bk(tpb)], trq)
            p.op('act', [bk(tpb)], [('m_kpt', s2)],
                 lambda E, s2=s2, tpv=tpv: E.activation(out=kpt[s2][:, :, :], in_=tpv[0:97, :, :], func=AF.Copy))
            p.dma('sp', 'm_kpt%d' % s2, [('m_kpt', s2)], ['QpT'],
                  [(QpT[:, :, t * 128:(t + 1) * 128].rearrange("h d t -> d h t"), kpt[s2][:, :, :])])
    p.barrier()
    kh = [sb(c, 'm_kh%d' % i, [97, NT], BF16) for i in range(2)]
    qh = [sb(c, 'm_qh%d' % i, [97, NT], BF16) for i in range(2)]
    vh = [sb(c, 'm_vh%d' % i, [128, NTILE, 65], BF16) for i in range(2)]
    pt = [sb(c, 'm_pt%d' % i, [128, 512], BF16) for i in range(3)]
    osb = [sb(c, 'm_osb%d' % i, [65, 512], F32) for i in range(2)]
    on = [sb(c, 'm_on%d' % i, [64, 512], BF16) for i in range(2)]
    ei = 0
    ci = 0
    for h in range(8):
        hs = h % 2
        p.dma('pool', 'm_kh%d' % hs, ['KpT'], [('m_kh', hs)], [(kh[hs][:, :], KpT[h, :, :])])
        p.dma('pool', 'm_qh%d' % hs, ['QpT'], [('m_qh', hs)], [(qh[hs][:, :], QpT[h, :, :])])
        p.dma('sp', 'm_vh%d' % hs, ['VpD'], [('m_vh', hs)],
              [(vh[hs][:, :, :], VpD[h, :, :].rearrange("(n p) e -> p n e", p=128))])
        chunks = ([(0, 256, 2)] if ctx_q else []) + [(256 + 512 * i, 512, NTILE) for i in range(16)]
        for (q0, nq, nkt) in chunks:
            ob = 6 + ci % 2
            cs2 = ci % 2
            ci += 1
            def emit_qk(kt):
                sbk = (ei0 + kt) % 3
                p.op('pe', [('m_kh', hs), ('m_qh', hs)], [bk(sbk)],
                     lambda E: E.matmul(B[sbk][:, 0:nq], lhsT=kh[hs][:, kt * 128:(kt + 1) * 128],
                                        rhs=qh[hs][:, q0:q0 + nq], start=True, stop=True))
            ei0 = ei
            ei += nkt
            for kt in range(min(2, nkt)):
                emit_qk(kt)
            for kt in range(nkt):
                sbk = (ei0 + kt) % 3
                p.op('act', [bk(sbk)], [('m_pt', sbk)],
                     lambda E: E.activation(out=pt[sbk][:, 0:nq], in_=B[sbk][:, 0:nq], func=AF.Exp))
                if kt + 2 < nkt:
                    emit_qk(kt + 2)
                p.op('pe', [('m_vh', hs), ('m_pt', sbk)], [bk(ob)],
                     lambda E: E.matmul(B[ob][0:65, 0:nq], lhsT=vh[hs][:, kt, :], rhs=pt[sbk][:, 0:nq],
                                        start=(kt == 0), stop=(kt == nkt - 1)))
            o_t = osb[cs2]
            p.op('dve', [bk(ob)], [('m_osb', cs2)], lambda E, o_t=o_t, ob=ob, nq=nq: E.tensor_copy(out=o_t[:, 0:nq], in_=B[ob][0:65, 0:nq]))
            p.op('dve', [('m_osb', cs2)], [('m_osb', cs2)],
                 lambda E, o_t=o_t, nq=nq: E.reciprocal(out=o_t[64:65, 0:nq], in_=o_t[64:65, 0:nq]))
            p.op('pe', [('m_osb', cs2), 'cst'], [bk(5)],
                 lambda E, o_t=o_t, nq=nq: E.matmul(B[5][0:64, 0:nq], lhsT=ones32[64:65, 0:64], rhs=o_t[64:65, 0:nq], start=True, stop=True))
            p.op('dve', [bk(5), ('m_osb', cs2)], [('m_on', cs2)],
                 lambda E, o_t=o_t, nq=nq, cs2=cs2: E.tensor_tensor(out=on[cs2][:, 0:nq], in0=o_t[0:64, 0:nq], in1=B[5][0:64, 0:nq], op=ALU.mult))
            p.dma('sp', 'm_on%d' % cs2, [('m_on', cs2)], [('OMT', l)],
                  [(S['OMT'][h * 64:(h + 1) * 64, q0:q0 + nq], on[cs2][:, 0:nq])])


NFFT = 16384


def hy_conv3(c, l):
    p = c.p
    I = c.inp
    S = c.scr[l]
    with ExitStack() as es:
        c.es = es
        wb32 = sb(c, 'h3_w32', [64, 4, 1536], F32)
        wb = sb(c, 'h3_w', [64, 4, 1536], BF16)
        zin = [sb(c, 'h3_zin%d' % i, [64, 10, 512], BF16) for i in range(2)]
        t0_ = [sb(c, 'h3_t0%d' % i, [64, 8, 512], BF16) for i in range(2)]
        t1_ = [sb(c, 'h3_t1%d' % i, [64, 8, 512], BF16) for i in range(2)]
        zo = [sb(c, 'h3_zo%d' % i, [64, 8, 512], BF16) for i in range(2)]
        p.dma('sp', 'h3_w', [], ['h3_w32'],
              [(wb32[:, k, :], I['hy_conv_w'][l, k, :].partition_broadcast(64)) for k in range(3)] +
              [(wb32[:, 3, :], I['hy_conv_b'][l, :].partition_broadcast(64))])
        p.op('dve', ['h3_w32'], ['h3_w'], lambda E: E.tensor_copy(out=wb[:, :, :], in_=wb32[:, :, :]))
        its = [(tok0, na, cs, bc) for (tok0, na) in ((0, 2), (NCTX, 64)) for cs in range(3) for bc in range(16)]

        def c3_load(it):
            tok0, na, cs, bc = its[it]
            src = S['HY'][tok0:tok0 + na * 128, :].rearrange("(a b) c -> a b c", b=128)
            c0 = cs * 512
            b0 = bc * 8
            s2 = it % 2
            z = zin[s2]
            pairs = []
            pre = []
            lo, hi = b0 - 1, b0 + 9
            if bc == 0:
                pre.append(lambda E, z=z: E.memset(z[0:1, 0:1, :], 0.0))
                if na > 1:
                    pairs.append((z[1:na, 0:1, :], src[0:na - 1, 127:128, c0:c0 + 512]))
                pairs.append((z[0:na, 1:10, :], src[0:na, 0:9, c0:c0 + 512]))
            elif bc == 15:
                pre.append(lambda E, z=z, na=na: E.memset(z[0:na, 9:10, :], 0.0))
                if na > 1:
                    pairs.append((z[0:na - 1, 9:10, :], src[1:na, 0:1, c0:c0 + 512]))
                pairs.append((z[0:na, 0:9, :], src[0:na, lo:128, c0:c0 + 512]))
            else:
                pairs.append((z[0:na, 0:10, :], src[0:na, lo:hi, c0:c0 + 512]))
            for f in pre:
                p.op('pool', [], [('h3_zin', s2)], f)
            p.dma('sp', 'h3_zin%d' % s2, [('HY', l)], [('h3_zin', s2)], pairs)

        c3_load(0)
        for it in range(len(its)):
            tok0, na, cs, bc = its[it]
            dst = c.HYC[tok0:tok0 + na * 128, :].rearrange("(a b) c -> a b c", b=128)
            c0 = cs * 512
            b0 = bc * 8
            s2 = it % 2
            z = zin[s2]
            if it + 1 < len(its):
                c3_load(it + 1)
            w = lambda k, c0=c0, na=na: wb[0:na, k, c0:c0 + 512].unsqueeze(1).to_broadcast([na, 8, 512])
            a0, a1, oz = t0_[s2], t1_[s2], zo[s2]
            zk = ('h3_zin', s2)
            p.op('dve', [zk, 'h3_w'], [('h3_t0', s2)],
                 lambda E: E.tensor_tensor(out=a0[0:na], in0=z[0:na, 0:8, :], in1=w(0), op=ALU.mult))
            p.op('pool', [zk, 'h3_w'], [('h3_t1', s2)],
                 lambda E: E.tensor_tensor(out=a1[0:na], in0=z[0:na, 1:9, :], in1=w(1), op=ALU.mult))
            p.op('dve', [zk, 'h3_w'], [('h3_zo', s2)],
                 lambda E: E.tensor_tensor(out=oz[0:na], in0=z[0:na, 2:10, :], in1=w(2), op=ALU.mult))
            p.op('dve', [('h3_t0', s2), 'h3_w'], [('h3_t0', s2)],
                 lambda E: E.tensor_tensor(out=a0[0:na], in0=a0[0:na], in1=w(3), op=ALU.add))
            p.op('dve', [('h3_t0', s2), ('h3_zo', s2)], [('h3_zo', s2)],
                 lambda E: E.tensor_tensor(out=oz[0:na], in0=a0[0:na], in1=oz[0:na], op=ALU.add))
            p.op('dve', [('h3_t1', s2), ('h3_zo', s2)], [('h3_zo', s2)],
                 lambda E: E.tensor_tensor(out=oz[0:na], in0=a1[0:na], in1=oz[0:na], op=ALU.add))
            p.dma('sp', 'h3_zo%d' % s2, [('h3_zo', s2)], ['HYC'],
                  [(dst[0:na, b0:b0 + 8, c0:c0 + 512], oz[0:na, :, :])])
        p.barrier()


def hy_filters(c, l, job):
    p = c.p
    I = c.inp
    nt = 128 if job == 0 else 4
    feat = I['hy_feat%d' % job]
    tvec = I['hy_tvec%d' % job]
    KTD = c.KTD[job]
    B = c.bank
    bk = lambda i: ('bank', i)
    with ExitStack() as es:
        c.es = es
        w1 = sb(c, 'hf_w1', [33, 64], F32)
        w2 = sb(c, 'hf_w2', [64, 64], F32)
        w3 = sb(c, 'hf_w3', [64, 2048], F32)
        pb = sb(c, 'hf_pb', [64, 8], F32)
        ft = [sb(c, 'hf_ft%d' % i, [33, 512], F32) for i in range(2)]
        tv = [sb(c, 'hf_tv%d' % i, [1, 512], F32) for i in range(2)]
        u = [sb(c, 'hf_u%d' % i, [64, 512], F32) for i in range(2)]
        ui = [sb(c, 'hf_ui%d' % i, [64, 512], mybir.dt.int32) for i in range(2)]
        uf = [sb(c, 'hf_uf%d' % i, [64, 512], F32) for i in range(2)]
        h1 = [sb(c, 'hf_h1%d' % i, [64, 512], F32) for i in range(2)]
        h2 = [sb(c, 'hf_h2%d' % i, [64, 512], F32) for i in range(2)]
        dec = [sb(c, 'hf_dec%d' % i, [128, 512], F32) for i in range(2)]
        hd = [sb(c, 'hf_hd%d' % i, [128, 2, 512], F32) for i in range(2)]
        ha = [sb(c, 'hf_ha%d' % i, [128, 2, 512], F32) for i in range(2)]
        hb = [sb(c, 'hf_hb%d' % i, [128, 2, 512], BF16) for i in range(2)]
        nd = sb(c, 'hf_nd', [1, 512], F32)
        l1 = sb(c, 'hf_l1', [1, 1024], F32)
        cst = c.cst
        ones = cst[:, 640:768]
        p.dma('sp', 'hf_w', [], ['hf_w'],
              [(w1[:, :], I['hy_w1'][l]), (w2[:, :], I['hy_w2'][l]), (w3[:, :], I['hy_w3'][l]),
               (nd[:, :], I['hy_negdelta'][0:1, :])])
        p.dma('sp', 'hf_pb', [], ['hf_pb'],
              [(pb[:, 0:1], I['hy_b1'][l, :].rearrange("(p o) -> p o", o=1)),
               (pb[:, 1:2], I['hy_b2'][l, :].rearrange("(p o) -> p o", o=1)),
               (pb[:, 2:3], I['hy_freq'][l, :].rearrange("(p o) -> p o", o=1))], slow=True)
        p.op('dve', ['hf_pb'], ['hf_pb2'],
             lambda E: E.tensor_scalar(out=pb[:, 3:4], in0=pb[:, 2:3], scalar1=1.0 / (2 * math.pi), scalar2=None, op0=ALU.mult))
        p.op('dve', ['hf_pb', 'hf_pb2'], ['hf_pb3'],
             lambda E: E.tensor_scalar(out=pb[:, 4:6], in0=pb[:, 0:2], scalar1=pb[:, 3:4], scalar2=None, op0=ALU.mult))
        pbk = ['hf_pb', 'hf_pb2', 'hf_pb3']

        def sin_layer(src_ps, srck, bcol, dst, dstk, s2):
            p.op('dve', [srck] + pbk, [('hf_u', s2)],
                 lambda E: E.tensor_scalar(out=u[s2][:, :], in0=src_ps, scalar1=pb[:, 3:4], scalar2=pb[:, bcol:bcol + 1],
                                           op0=ALU.mult, op1=ALU.add))
            p.op('dve', [('hf_u', s2)], [('hf_ui', s2)], lambda E: E.tensor_copy(out=ui[s2][:, :], in_=u[s2][:, :]))
            p.op('dve', [('hf_ui', s2)], [('hf_uf', s2)], lambda E: E.tensor_copy(out=uf[s2][:, :], in_=ui[s2][:, :]))
            p.op('dve', [('hf_u', s2), ('hf_uf', s2)], [('hf_u', s2)],
                 lambda E: E.tensor_tensor(out=u[s2][:, :], in0=u[s2][:, :], in1=uf[s2][:, :], op=ALU.subtract))
            p.op('act', [('hf_u', s2)], [dstk], lambda E: E.activation(out=dst, in_=u[s2][:, :], func=AF.Sin, scale=2 * math.pi))

        nchunk = nt // 4
        first_bwd_tile = 64 if job == 0 else 2
        for ch in range(nchunk):
            s2 = ch % 2
            p.dma('sp', 'hf_ft%d' % s2, [], [('hf_ft', s2)],
                  [(ft[s2][:, :], feat[:, ch * 512:(ch + 1) * 512]), (tv[s2][:, :], tvec[:, ch * 512:(ch + 1) * 512])])
            p.op('pe', [('hf_ft', s2), 'hf_w'], [bk(0)],
                 lambda E, s2=s2: E.matmul(B[0][0:64, :], lhsT=w1[:, :], rhs=ft[s2][:, :], start=True, stop=True))
            sin_layer(B[0][0:64, :], bk(0), 4, h1[s2][:, :], ('hf_h1', s2), s2)
            p.op('pe', [('hf_h1', s2), 'hf_w'], [bk(1)],
                 lambda E, s2=s2: E.matmul(B[1][0:64, :], lhsT=w2[:, :], rhs=h1[s2][:, :], start=True, stop=True))
            sin_layer(B[1][0:64, :], bk(1), 5, h2[s2][:, :], ('hf_h2', s2), s2)
            for j in range(4):
                tile = ch * 4 + j
                d = 0 if tile < first_bwd_tile else 1
                j2 = tile % 2
                cols = slice(j * 128, (j + 1) * 128)
                p.op('pe', [('hf_ft', s2), 'hf_w'], [bk(2)],
                     lambda E, s2=s2, cols=cols: E.matmul(B[2][:, :], lhsT=tv[s2][0:1, cols], rhs=nd[0:1, :], start=True, stop=True))
                p.op('act', [bk(2)], [('hf_dec', j2)], lambda E, j2=j2: E.activation(out=dec[j2][:, :], in_=B[2][:, :], func=AF.Exp))
                for o in range(2):
                    c0 = o * 1024 + d * 512
                    p.op('pe', [('hf_h2', s2), 'hf_w'], [bk(3 + o)],
                         lambda E, s2=s2, cols=cols, c0=c0, o=o: E.matmul(B[3 + o][:, :], lhsT=h2[s2][:, cols], rhs=w3[:, c0:c0 + 512],
                                                                        start=True, stop=True))
                    p.op('dve', [bk(3 + o), ('hf_dec', j2)], [('hf_hd', j2, o)],
                         lambda E, j2=j2, o=o: E.tensor_tensor(out=hd[j2][:, o, :], in0=B[3 + o][:, :], in1=dec[j2][:, :], op=ALU.mult))
                p.op('act', [('hf_hd', j2, 0), ('hf_hd', j2, 1)], [('hf_ha', j2)],
                     lambda E, j2=j2: E.activation(out=ha[j2][:, :, :], in_=hd[j2][:, :, :], func=AF.Abs))
                for o in range(2):
                    p.op('pe', [('hf_ha', j2), 'cst'], [bk(5 + o)],
                         lambda E, j2=j2, o=o, tile=tile: E.matmul(B[5 + o][0:1, :], lhsT=ones[:, 0:1], rhs=ha[j2][:, o, :],
                                                                   start=(tile == 0), stop=(tile == nt - 1)))
                p.op('pool', [('hf_hd', j2, 0), ('hf_hd', j2, 1)], [('hf_hb', j2)],
                     lambda E, j2=j2: E.tensor_copy(out=hb[j2][:, :, :], in_=hd[j2][:, :, :]))
                if tile == first_bwd_tile:
                    p.op('pool', [('hf_hb', j2)], [('hf_hb', j2)], lambda E, j2=j2: E.memset(hb[j2][0:1, :, :], 0.0))
                p.dma('sp', 'hf_hb%d' % j2, [('hf_hb', j2)], [('KTD', job)],
                      [(KTD[tile * 128:(tile + 1) * 128, :].rearrange("p (o c) -> p o c", o=2), hb[j2][:, :, :])])
        for o in range(2):
            p.op('dve', [bk(5 + o)], ['hf_l1'],
                 lambda E, o=o: E.tensor_scalar(out=l1[0:1, o * 512:(o + 1) * 512], in0=B[5 + o][0:1, :], scalar1=float(NFFT if job == 0 else 512), scalar2=None,
                                                op0=ALU.mult))
        p.op('dve', ['hf_l1'], ['hf_l1'], lambda E: E.reciprocal(out=l1[:, :], in_=l1[:, :]))
        p.dma('sp', 'hf_l1', ['hf_l1'], [('SCL', job)], [(c.SCL[job][:, :], l1[:, :])])
        p.barrier()


def hy_fwd1(c, src, K, tab, X1D, NF1):
    p = c.p
    B = c.bank
    bk = lambda i: ('bank', i)
    zt = c.hy_zt
    xo = c.hy_xo
    for bc in range(16):
        s2 = bc % 2
        p.dma('pool', 'hy_zt%d' % s2, ['HYC', 'Z2', ('KTD', 0), ('KTD', 1)], [('hy_zt', s2)], [(zt[s2][0:K, :, :], src(bc * 8, 8))])
        for j in range(8):
            b = bc * 8 + j
            e2 = b % 2
            for ri in range(2):
                p.op('pe', [('hy_zt', s2), 'hy_tab'], [bk(e2 * 2 + ri)],
                     lambda E, s2=s2, j=j, ri=ri, e2=e2: E.matmul(B[e2 * 2 + ri][0:NF1, :], lhsT=tab[0:K, ri, 0:NF1], rhs=zt[s2][0:K, j, :],
                                                                  start=True, stop=True))
            p.op('act', [bk(e2 * 2)], [('hy_xo', e2, 0)],
                 lambda E, e2=e2: E.activation(out=xo[e2][0:NF1, 0, :], in_=B[e2 * 2][0:NF1, :], func=AF.Copy))
            p.op('dve', [bk(e2 * 2 + 1)], [('hy_xo', e2, 1)],
                 lambda E, e2=e2: E.tensor_copy(out=xo[e2][0:NF1, 1, :], in_=B[e2 * 2 + 1][0:NF1, :]))
            p.dma('sp', 'hy_xo%d' % e2, [('hy_xo', e2, 0), ('hy_xo', e2, 1)], ['X1D'],
                  [(X1D[b, 0:NF1, :, :], xo[e2][0:NF1, :, :])])


def hy_stage2(c, X1D, mode, KS, QD, NF1=128, tw2name='hy_tw2', twres=None):
    p = c.p
    I = c.inp
    B = c.bank
    bk = lambda i: ('bank', i)
    xin, tw, ksb, pr, t4, qo = c.hy_xin, c.hy_tw, c.hy_ksb, c.hy_pr, c.hy_t4, c.hy_qo
    E3 = c.hy_E3

    def front(f1):
        s2 = f1 % 2
        p.dma('sp', 'hy_xin%d' % s2, ['X1D'], [('hy_xin', s2)],
              [(xin[s2][:, :, :], X1D[:, f1, :, :])])
        if twres is None:
            p.dma('sp', 'hy_tw%d' % s2, [], [('hy_tw', s2)], [(tw[s2][:, :, :], I[tw2name][f1])])
            twv, twk = tw[s2], ('hy_tw', s2)
        else:
            twv, twk = twres[:, f1, :, :], 'hy_twres'
        if mode != 'filter':
            p.dma('sp', 'hy_ksb%d' % s2, ['KS'], [('hy_ksb', s2)], [(ksb[s2][:, :, :], KS[:, f1, :, :])])
        zr, zi = s2 * 2, s2 * 2 + 1

        def mmz(E):
            E.matmul(B[zr][:, :], lhsT=twv[:, 0, :], rhs=xin[s2][:, 0, :], start=True, stop=False)
            E.matmul(B[zr][:, :], lhsT=twv[:, 2, :], rhs=xin[s2][:, 1, :], start=False, stop=True)
            E.matmul(B[zi][:, :], lhsT=twv[:, 0, :], rhs=xin[s2][:, 1, :], start=True, stop=False)
            return E.matmul(B[zi][:, :], lhsT=twv[:, 1, :], rhs=xin[s2][:, 0, :], start=False, stop=True)
        p.op('pe', [('hy_xin', s2), twk], [bk(zr), bk(zi)], mmz)

    front(0)
    for f1 in range(NF1):
        s2 = f1 % 2
        zr, zi = s2 * 2, s2 * 2 + 1
        if mode == 'filter':
            p.op('act', [bk(zr)], [('hy_pr', s2, 0)], lambda E: E.activation(out=pr[s2][:, 0, :], in_=B[zr][:, :], func=AF.Copy))
            p.op('dve', [bk(zi)], [('hy_pr', s2, 1)], lambda E: E.tensor_copy(out=pr[s2][:, 1, :], in_=B[zi][:, :]))
            if f1 + 1 < NF1:
                front(f1 + 1)
            p.dma('sp', 'hy_pr%d' % s2, [('hy_pr', s2, 0), ('hy_pr', s2, 1)], ['KS'],
                  [(KS[:, f1, :, :], pr[s2][:, :, :])])
            continue
        kk = ('hy_ksb', s2)
        tt = t4[s2]
        p.op('dve', [bk(zr), kk], [('hy_t4', s2, 0)], lambda E: E.tensor_tensor(out=tt[:, 0, :], in0=B[zr][:, :], in1=ksb[s2][:, 0, :], op=ALU.mult))
        p.op('dve', [bk(zi), kk], [('hy_t4', s2, 1)], lambda E: E.tensor_tensor(out=tt[:, 1, :], in0=B[zi][:, :], in1=ksb[s2][:, 1, :], op=ALU.mult))
        p.op('dve', [bk(zr), kk], [('hy_t4', s2, 2)], lambda E: E.tensor_tensor(out=tt[:, 2, :], in0=B[zr][:, :], in1=ksb[s2][:, 1, :], op=ALU.mult))
        p.op('dve', [bk(zi), kk], [('hy_t4', s2, 3)], lambda E: E.tensor_tensor(out=tt[:, 3, :], in0=B[zi][:, :], in1=ksb[s2][:, 0, :], op=ALU.mult))
        if f1 + 1 < NF1:
            front(f1 + 1)
        p.op('pool', [('hy_t4', s2, 0), ('hy_t4', s2, 1)], [('hy_pr', s2, 0)],
             lambda E: E.tensor_tensor(out=pr[s2][:, 0, :], in0=tt[:, 0, :], in1=tt[:, 1, :], op=ALU.subtract))
        p.op('pool', [('hy_t4', s2, 2), ('hy_t4', s2, 3)], [('hy_pr', s2, 1)],
             lambda E: E.tensor_tensor(out=pr[s2][:, 1, :], in0=tt[:, 2, :], in1=tt[:, 3, :], op=ALU.add))

        def mmq(E):
            E.matmul(B[4][:, :], lhsT=E3[:, 0, :], rhs=pr[s2][:, 0, :], start=True, stop=False)
            E.matmul(B[4][:, :], lhsT=E3[:, 2, :], rhs=pr[s2][:, 1, :], start=False, stop=True)
            E.matmul(B[5][:, :], lhsT=E3[:, 1, :], rhs=pr[s2][:, 0, :], start=True, stop=False)
            return E.matmul(B[5][:, :], lhsT=E3[:, 0, :], rhs=pr[s2][:, 1, :], start=False, stop=True)
        p.op('pe', [('hy_pr', s2, 0), ('hy_pr', s2, 1), 'hy_tab'], [bk(4), bk(5)], mmq)
        p.op('act', [bk(4)], [('hy_qo', s2, 0)], lambda E: E.activation(out=qo[s2][:, 0, :], in_=B[4][:, :], func=AF.Copy))
        p.op('act', [bk(5)], [('hy_qo', s2, 1)], lambda E: E.activation(out=qo[s2][:, 1, :], in_=B[5][:, :], func=AF.Copy))
        p.dma('sp', 'hy_qo%d' % s2, [('hy_qo', s2, 0), ('hy_qo', s2, 1)], ['QD'],
              [(QD[:, f1, :, :], qo[s2][:, :, :])])


def hy_final(c, QD, na, scl, bias, zsrc, gsrc, dst, NF1=128, twfname='hy_twf', Mm=64):
    p = c.p
    I = c.inp
    B = c.bank
    bk = lambda i: ('bank', i)
    qin, twf, zg, ya, yb, yo = c.hy_qin, c.hy_twf, c.hy_zg, c.hy_ya, c.hy_yb, c.hy_yo
    def loads(b):
        s2 = b % 2
        p.dma('sp', 'hy_qin%d' % s2, ['QD'], [('hy_qin', s2)], [(qin[s2][0:NF1, :, :], QD[b, 0:NF1, :, :])])
        p.dma('sp', 'hy_twf%d' % s2, [], [('hy_twf', s2)], [(twf[s2][0:NF1, :, 0:Mm], I[twfname][b])])
        p.dma('sp', 'hy_zg%d' % s2, ['HYC', 'Z2'], [('hy_zg', s2)],
              [(zg[s2][0:na, 0, :], zsrc[:, b, :]), (zg[s2][0:na, 1, :], gsrc[:, b, :])])

    loads(0)
    for b in range(128):
        s2 = b % 2
        if b + 1 < 128:
            loads(b + 1)

        def mmy(E, s2=s2):
            E.matmul(B[s2][0:Mm, :], lhsT=twf[s2][0:NF1, 0, 0:Mm], rhs=qin[s2][0:NF1, 0, :], start=True, stop=False)
            return E.matmul(B[s2][0:Mm, :], lhsT=twf[s2][0:NF1, 1, 0:Mm], rhs=qin[s2][0:NF1, 1, :], start=False, stop=True)
        p.op('pe', [('hy_qin', s2), ('hy_twf', s2)], [bk(s2)], mmy)
        p.op('dve', [bk(s2), 'hy_scl'], [('hy_ya', s2)],
             lambda E, s2=s2: E.tensor_tensor(out=ya[s2][0:na, :], in0=B[s2][0:na, :], in1=scl[0:na, :], op=ALU.mult))
        p.op('pool', [('hy_zg', s2), 'hy_scl'], [('hy_yb', s2)],
             lambda E, s2=s2: E.tensor_tensor(out=yb[s2][0:na, :], in0=zg[s2][0:na, 0, :], in1=bias[0:na, :], op=ALU.mult))
        p.op('pool', [('hy_ya', s2), ('hy_yb', s2)], [('hy_ya', s2)],
             lambda E, s2=s2: E.tensor_tensor(out=ya[s2][0:na, :], in0=ya[s2][0:na, :], in1=yb[s2][0:na, :], op=ALU.add))
        p.op('dve', [('hy_ya', s2), ('hy_zg', s2)], [('hy_yo', s2)],
             lambda E, s2=s2: E.tensor_tensor(out=yo[s2][0:na, :], in0=ya[s2][0:na, :], in1=zg[s2][0:na, 1, :], op=ALU.mult))
        p.dma('sp', 'hy_yo%d' % s2, [('hy_yo', s2)], ['Z2', 'OH'], [(dst[:, b, :], yo[s2][0:na, :])])


def stage_hyena(c, l, with_ctx):
    p = c.p
    I = c.inp
    hy_conv3(c, l)
    jobs = [0, 1] if with_ctx else [0]
    hp = c.cfg.get('hy_parts', 9)
    if hp < 1:
        return
    for job in jobs:
        hy_filters(c, l, job)
    if hp < 2:
        return
    with ExitStack() as es:
        c.es = es
        c.hy_zt = [sb(c, 'hy_zt%d' % i, [128, 8, 512], BF16) for i in range(2)]
        c.hy_xo = [sb(c, 'hy_xo%d' % i, [128, 2, 512], BF16) for i in range(2)]
        c.hy_xin = [sb(c, 'hy_xin%d' % i, [128, 2, 512], BF16) for i in range(2)]
        c.hy_tw = [sb(c, 'hy_tw%d' % i, [128, 3, 128], BF16) for i in range(2)]
        c.hy_ksb = [sb(c, 'hy_ksb%d' % i, [128, 2, 512], BF16) for i in range(2)]
        c.hy_pr = [sb(c, 'hy_pr%d' % i, [128, 2, 512], BF16) for i in range(2)]
        c.hy_t4 = [sb(c, 'hy_t4%d' % i, [128, 4, 512], F32) for i in range(2)]
        c.hy_qo = [sb(c, 'hy_qo%d' % i, [128, 2, 512], BF16) for i in range(2)]
        c.hy_qin = [sb(c, 'hy_qin%d' % i, [128, 2, 512], BF16) for i in range(2)]
        c.hy_twf = [sb(c, 'hy_twf%d' % i, [128, 2, 64], BF16) for i in range(2)]
        c.hy_zg = [sb(c, 'hy_zg%d' % i, [64, 2, 512], BF16) for i in range(2)]
        c.hy_ya = [sb(c, 'hy_ya%d' % i, [64, 512], F32) for i in range(2)]
        c.hy_yb = [sb(c, 'hy_yb%d' % i, [64, 512], F32) for i in range(2)]
        c.hy_yo = [sb(c, 'hy_yo%d' % i, [64, 512], BF16) for i in range(2)]
        tabs = sb(c, 'hy_tabs', [128, 2, 2, 128], BF16)
        c.hy_E3 = sb(c, 'hy_E3', [128, 3, 128], BF16)
        scl = sb(c, 'hy_scl', [64, 2, 512], F32)
        bias = sb(c, 'hy_bias', [64, 2, 512], F32)
        p.dma('sp', 'hy_tab', [], ['hy_tab'],
              [(tabs[:, :, :, :], I['hy_dft1'][:, :, :, :]), (c.hy_E3[:, :, :], I['hy_e3'][:, :, :])])
        twres = sb(c, 'hy_twres', [128, 128, 3, 128], BF16)
        p.dma('sp', 'hy_twres', [], ['hy_twres'],
              [(twres[:, g * 16:(g + 1) * 16, :, :], I['hy_tw2'][g * 16:(g + 1) * 16].rearrange("f b k g -> b f k g")) for g in range(8)])
        for job in jobs:
            na = 64 if job == 0 else 2
            tok0 = NCTX if job == 0 else 0
            nt = 128 if job == 0 else 4
            ftab = tabs[:, job, :, :]
            NF1 = 128 if job == 0 else 4
            tw2n = 'hy_tw2' if job == 0 else 'hy_tw2c'
            twfn = 'hy_twf' if job == 0 else 'hy_twfc'
            Mm = 64 if job == 0 else 2
            KTD, KS, X1D, QD = c.KTD[job], c.KS, c.X1D, c.QD
            for o in range(2):
                ksrc = KTD[:, o * 512:(o + 1) * 512].rearrange("(a b) c -> a b c", b=128)
                hy_fwd1(c, lambda b0, nb, ksrc=ksrc: ksrc[:, b0:b0 + nb, :], nt, ftab, X1D, NF1)
                if hp >= 4:
                    hy_stage2(c, X1D, 'filter', KS[o], None, NF1, tw2n, twres if job == 0 else None)
            if hp < 5:
                continue
            p.dma('sp', 'hy_scl', [('SCL', job)], ['hy_scl'],
                  [(scl[:, o, :], c.SCL[job][0, o * 512:(o + 1) * 512].partition_broadcast(64)) for o in range(2)] +
                  [(bias[:, o, :], I['hy_bias'][l, o, :].partition_broadcast(64)) for o in range(2)])
            hyc = c.HYC[tok0:tok0 + na * 128, :].rearrange("(a b) c -> a b c", b=128)
            z2 = c.Z2[tok0:tok0 + na * 128, :].rearrange("(a b) c -> a b c", b=128)
            oh = c.OH[tok0:tok0 + na * 128, :].rearrange("(a b) c -> a b c", b=128)
            vsrc = hyc[:, :, 0:512]
            hy_fwd1(c, lambda b0, nb: vsrc[:, b0:b0 + nb, :], na, ftab, X1D, NF1)
            hy_stage2(c, X1D, 'conv', KS[0], QD, NF1, tw2n, twres if job == 0 else None)
            if hp < 6:
                continue
            hy_final(c, QD, na, scl[:, 0, :], bias[:, 0, :], vsrc, hyc[:, :, 512:1024], z2, NF1, twfn, Mm)
            if hp < 7:
                continue
            hy_fwd1(c, lambda b0, nb: z2[:, b0:b0 + nb, :], na, ftab, X1D, NF1)
            hy_stage2(c, X1D, 'conv', KS[1], QD, NF1, tw2n, twres if job == 0 else None)
            hy_final(c, QD, na, scl[:, 1, :], bias[:, 1, :], z2, hyc[:, :, 1024:1536], oh, NF1, twfn, Mm)
        p.barrier()


def ln_affine_store(c, r, rkey, gb, gbkey, dsts, slot):
    p = c.p
    st = c.e_st[slot]
    junk = c.e_junk
    p.op('act', [rkey], ['e_junk', ('e_st', slot, 0)],
         lambda E: E.activation(out=junk[:, :], in_=r[:, :], func=AF.Copy, accum_out=st[:, 0:1]))
    p.op('dve', [('e_st', slot, 0)], [('e_st', slot, 1)],
         lambda E: E.tensor_scalar(out=st[:, 1:2], in0=st[:, 0:1], scalar1=-1.0 / D, scalar2=None, op0=ALU.mult))
    p.op('act', [rkey, ('e_st', slot, 1)], ['e_junk', ('e_st', slot, 2)],
         lambda E: E.activation(out=junk[:, :], in_=r[:, :], func=AF.Square, bias=st[:, 1:2], scale=1.0, accum_out=st[:, 2:3]))
    p.op('act', [('e_st', slot, 2)], [('e_st', slot, 3)],
         lambda E: E.activation(out=st[:, 3:4], in_=st[:, 2:3], func=AF.Ln, scale=1.0 / D, bias=LN_EPS))
    p.op('act', [('e_st', slot, 3)], [('e_st', slot, 4)],
         lambda E: E.activation(out=st[:, 4:5], in_=st[:, 3:4], func=AF.Exp, scale=-0.5))
    p.op('dve', [rkey, ('e_st', slot, 1), ('e_st', slot, 4)], [rkey],
         lambda E: E.tensor_scalar(out=r[:, :], in0=r[:, :], scalar1=st[:, 1:2], scalar2=st[:, 4:5], op0=ALU.add, op1=ALU.mult))
    p.op('pool', [rkey, gbkey], [rkey], lambda E: E.tensor_tensor(out=r[:, :], in0=r[:, :], in1=gb[:, 0, :], op=ALU.mult))
    p.op('pool', [rkey, gbkey], [rkey], lambda E: E.tensor_tensor(out=r[:, :], in0=r[:, :], in1=gb[:, 1, :], op=ALU.add))
    p.dma('sp', 'e_r%d' % slot, [rkey], ['XOUT'], [(d, r[:, :]) for d in dsts])


def load_bc_rows(c, l, which):
    p = c.p
    I = c.inp
    g = 2 if which == 1 else 5
    lg, lb = ('ln1_g', 'ln1_b') if which == 1 else ('ln2_g', 'ln2_b')
    p.dma('sp', 'e_bc', [('modv', l)], ['e_bc'],
          [(c.e_ag[:, r, :], c.modv[l][r, g * 1024:(g + 1) * 1024].partition_broadcast(128)) for r in range(2)] +
          [(c.e_gb[:, 0, :], I[lg][l, :].partition_broadcast(128)), (c.e_gb[:, 1, :], I[lb][l, :].partition_broadcast(128))])


def stage_merge(c, l, xsrc, tiles):
    p = c.p
    I = c.inp
    S = c.scr[l]
    B = c.bank
    bk = lambda i: ('bank', i)
    with ExitStack() as es:
        c.es = es
        wbr = sb(c, 'mg_wbr', [128, 3, 4, 1024], BF16)
        wout = sb(c, 'mg_wout', [128, 8, 1024], BF16)
        c.e_ag = sb(c, 'e_ag', [128, 2, 1024], F32)
        c.e_gb = sb(c, 'e_gb', [128, 2, 1024], F32)
        c.e_st = [sb(c, 'e_st%d' % i, [128, 8], F32) for i in range(2)]
        c.e_junk = sb(c, 'e_junk', [128, 1024], BF16)
        oT = [sb(c, 'mg_oT%d' % i, [128, 3, 4, 128], BF16) for i in range(2)]
        oh = [sb(c, 'mg_oh%d' % i, [128, 512], BF16) for i in range(2)]
        g3 = [sb(c, 'mg_g3%d' % i, [128, 3072], BF16) for i in range(2)]
        y = [sb(c, 'mg_y%d' % i, [128, 1024], F32) for i in range(2)]
        tt = [sb(c, 'mg_t%d' % i, [128, 1024], F32) for i in range(2)]
        yb = [sb(c, 'mg_yb%d' % i, [128, 1024], BF16) for i in range(2)]
        yT = [sb(c, 'mg_yT%d' % i, [128, 8, 128], BF16) for i in range(2)]
        xt = [sb(c, 'mg_xt%d' % i, [128, 1024], F32) for i in range(2)]
        for br, nm in enumerate(['w_br_gla', 'w_br_mla', 'w_br_hy']):
            p.dma('pool', 'mg_w', [], ['mg_w'], [(wbr[:, br, :, :], I[nm][l].rearrange("(k p) n -> p k n", p=128))])
        p.dma('pool', 'mg_w', [], ['mg_w'], [(wout[:, :, :], I['w_out'][l].rearrange("(k p) n -> p k n", p=128))])
        load_bc_rows(c, l, 1)
        def mloads(i):
            t = tiles[i]
            s2 = i % 2
            tok = slice(t * 128, (t + 1) * 128)
            p.dma('sp', 'mg_oT%d' % s2, [('OGT', l), ('OMT', l)], [('mg_oT', s2)],
                  [(oT[s2][:, 0, :, :], S['OGT'][:, tok].rearrange("(k p) t -> p k t", p=128)),
                   (oT[s2][:, 1, :, :], S['OMT'][:, tok].rearrange("(k p) t -> p k t", p=128))])
            p.dma('sp', 'mg_oh%d' % s2, ['OH'], [('mg_oh', s2)], [(oh[s2][:, :], c.OH[tok, :])])
            p.dma('sp', 'mg_g3%d' % s2, [('G3', l)], [('mg_g3', s2)], [(g3[s2][:, :], S['G3'][tok, :])])
            p.dma('sp', 'mg_xt%d' % s2, ['XOUT'], [('mg_xt', s2)], [(xt[s2][:, :], xsrc(t))])

        mloads(0)
        for i, t in enumerate(tiles):
            s2 = i % 2
            r = 1 if t < 2 else 0
            tok = slice(t * 128, (t + 1) * 128)
            if i + 1 < len(tiles):
                mloads(i + 1)
            tpv = bank16(c, 6)

            def tro(E):
                ins = None
                for k in range(4):
                    ins = E.transpose(tpv[:, k, :], oh[s2][:, k * 128:(k + 1) * 128], c.ident[:, :])
                return ins
            p.op('pe', [('mg_oh', s2), 'ident'], [bk(6)], tro)
            p.op('act', [bk(6)], [('mg_oT', s2)], lambda E: E.activation(out=oT[s2][:, 2, :, :], in_=tpv[:, 0:4, :], func=AF.Copy))
            for br in range(3):
                for half in range(2):
                    bb = (br * 2 + half) % 4

                    def mmb(E):
                        ins = None
                        for k in range(4):
                            ins = E.matmul(B[bb][:, :], lhsT=oT[s2][:, br, k, :], rhs=wbr[:, br, k, half * 512:(half + 1) * 512],
                                           start=(k == 0), stop=(k == 3))
                        return ins
                    p.op('pe', [('mg_oT', s2), 'mg_w'], [bk(bb)], mmb)
                    hs = slice(half * 512, (half + 1) * 512)
                    gs = slice(br * 1024 + half * 512, br * 1024 + (half + 1) * 512)
                    if br == 0:
                        p.op('dve', [bk(bb), ('mg_g3', s2)], [('mg_y', s2, half)],
                             lambda E: E.tensor_tensor(out=y[s2][:, hs], in0=B[bb][:, :], in1=g3[s2][:, gs], op=ALU.mult))
                    else:
                        p.op('dve', [bk(bb), ('mg_g3', s2)], [('mg_t', s2, half)],
                             lambda E: E.tensor_tensor(out=tt[s2][:, hs], in0=B[bb][:, :], in1=g3[s2][:, gs], op=ALU.mult))
                        p.op('pool', [('mg_t', s2, half), ('mg_y', s2, half)], [('mg_y', s2, half)],
                             lambda E: E.tensor_tensor(out=y[s2][:, hs], in0=y[s2][:, hs], in1=tt[s2][:, hs], op=ALU.add))
            p.op('act', [('mg_y', s2, 0), ('mg_y', s2, 1)], [('mg_yb', s2)],
                 lambda E: E.activation(out=yb[s2][:, :], in_=y[s2][:, :], func=AF.Copy))
            tp7 = bank16(c, 7)

            def try_(E):
                ins = None
                for k in range(8):
                    ins = E.transpose(tp7[:, k, :], yb[s2][:, k * 128:(k + 1) * 128], c.ident[:, :])
                return ins
            p.op('pe', [('mg_yb', s2), 'ident'], [bk(7)], try_)
            p.op('act', [bk(7)], [('mg_yT', s2)], lambda E: E.activation(out=yT[s2][:, :, :], in_=tp7[:, :, :], func=AF.Copy))
            for half in range(2):
                bb = 4 + half

                def mmo(E):
                    ins = None
                    for k in range(8):
                        ins = E.matmul(B[bb][:, :], lhsT=yT[s2][:, k, :], rhs=wout[:, k, half * 512:(half + 1) * 512],
                                       start=(k == 0), stop=(k == 7))
                    return ins
                p.op('pe', [('mg_yT', s2), 'mg_w'], [bk(bb)], mmo)
                hs = slice(half * 512, (half + 1) * 512)
                p.op('dve', [bk(bb), 'e_bc'], [('mg_t', s2, half)],
                     lambda E: E.tensor_tensor(out=tt[s2][:, hs], in0=B[bb][:, :], in1=c.e_ag[:, r, hs], op=ALU.mult))
                p.op('dve', [('mg_t', s2, half), ('mg_xt', s2)], [('mg_xt', s2)],
                     lambda E: E.scalar_tensor_tensor(out=xt[s2][:, hs], in0=xt[s2][:, hs], scalar=ALPHA, in1=tt[s2][:, hs],
                                                      op0=ALU.mult, op1=ALU.add))
            ln_affine_store(c, xt[s2], ('mg_xt', s2), c.e_gb, 'e_bc', [c.X1[tok, :]], s2)
        p.barrier()


def moe_precast(c, l):
    p = c.p
    I = c.inp
    for e in range(32):
        if c.cfg.get('moe_mode', 'sparse') == 'sparse':
            p.dma('pool', 'wcast', [], [('WB', l, e)],
                  [(c.WB1[l][e].rearrange("(p k) n -> p k n", k=8), I['moe_w1'][l, e].rearrange("(k p) n -> p k n", p=128)),
                   (c.WB2[l][e].rearrange("(p k) n -> p k n", k=8), I['moe_w2'][l, e].rearrange("(k p) n -> p k n", p=128))])
        else:
            p.dma('pool', 'wcast', [], [('WB', l, e)],
                  [(c.WB1[l][e], I['moe_w1'][l, e]), (c.WB2[l][e], I['moe_w2'][l, e])])


def stage_moe(c, l, tiles, dst_fn):
    p = c.p
    I = c.inp
    B = c.bank
    bk = lambda i: ('bank', i)
    cst = c.cst
    with ExitStack() as es:
        c.es = es
        c.e_ag = sb(c, 'e_ag', [128, 2, 1024], F32)
        c.e_gb = sb(c, 'e_gb', [128, 2, 1024], F32)
        c.e_st = [sb(c, 'e_st%d' % i, [128, 8], F32) for i in range(2)]
        c.e_junk = sb(c, 'e_junk', [128, 1024], BF16)
        w1 = [sb(c, 'mo_w1%d' % i, [128, 8, 2048], BF16) for i in range(2)]
        w2 = [sb(c, 'mo_w2%d' % i, [128, 8, 1024], BF16) for i in range(1)] * 2
        hT = sb(c, 'mo_hT', [128, 8, 1024], BF16)
        aT = sb(c, 'mo_aT', [128, 8, 1024], BF16)
        yacc = sb(c, 'mo_yacc', [128, 8, 1024], F32)
        rw = sb(c, 'mo_rw', [128, 8, 32], F32)
        rb = sb(c, 'mo_rb', [1, 32], F32)
        b1 = sb(c, 'mo_b1', [128, 32, 16], F32)
        b2 = sb(c, 'mo_b2', [32, 1024], F32)
        G = sb(c, 'mo_G', [128, 8, 32], F32)
        GT = sb(c, 'mo_GT', [32, 8, 128], F32)
        xt = [sb(c, 'mo_xt%d' % i, [128, 1024], F32) for i in range(2)]
        xh = [sb(c, 'mo_xh%d' % i, [128, 1024], F32) for i in range(1)] * 2
        h32 = [sb(c, 'mo_h32%d' % i, [128, 8, 128], F32) for i in range(1)] * 2
        lg = [sb(c, 'mo_lg%d' % i, [128, 32], F32) for i in range(2)]
        mx = [sb(c, 'mo_mx%d' % i, [128, 8], F32) for i in range(2)]
        ex = [sb(c, 'mo_ex%d' % i, [128, 32], F32) for i in range(2)]
        sm = [sb(c, 'mo_sm%d' % i, [128, 2], F32) for i in range(2)]
        gg = [sb(c, 'mo_gg%d' % i, [128, 512], F32) for i in range(2)]
        sg = [sb(c, 'mo_sg%d' % i, [128, 512], F32) for i in range(2)]
        ll = [sb(c, 'mo_ll%d' % i, [128, 512], F32) for i in range(2)]
        st = [sb(c, 'mo_st%d' % i, [128, 8], F32) for i in range(2)]
        p.dma('sp', 'mo_c', [], ['mo_c'],
              [(rw[:, :, :], I['router_w'][l].rearrange("(k p) e -> p k e", p=128)),
               (rb[:, :], I['router_b'][l:l + 1, :]), (b2[:, :], I['moe_b2'][l])])
        p.dma('sp', 'mo_b1', [], ['mo_b1'],
              [(b1[:, e, :], I['moe_b1'][l, e, :].rearrange("(j p) -> p j", p=128)) for e in range(32)], slow=True)
        load_bc_rows(c, l, 2)
        ones = cst[:, 640:768]
        ident32 = cst[:, 0:128]
        groups = [tiles[i:i + 8] for i in range(0, len(tiles), 8)][:c.cfg.get('moe_groups', 99)]
        wi = 0
        for gi, gt in enumerate(groups):
            ng = len(gt)
            T = ng * 128
            for j, t in enumerate(gt):
                s2 = j % 2
                r = 1 if t < 2 else 0
                tok = slice(t * 128, (t + 1) * 128)
                p.dma('sp', 'mo_xt%d' % s2, ['XOUT'], [('mo_xt', s2)], [(xt[s2][:, :], c.X1[tok, :])])
                s_ = st[s2]
                p.op('act', [('mo_xt', s2)], ['e_junk', ('mo_st', s2, 0)],
                     lambda E: E.activation(out=c.e_junk[:, :], in_=xt[s2][:, :], func=AF.Copy, accum_out=s_[:, 0:1]))
                p.op('dve', [('mo_st', s2, 0)], [('mo_st', s2, 1)],
                     lambda E: E.tensor_scalar(out=s_[:, 1:2], in0=s_[:, 0:1], scalar1=-1.0 / D, scalar2=None, op0=ALU.mult))
                p.op('act', [('mo_xt', s2), ('mo_st', s2, 1)], ['e_junk', ('mo_st', s2, 2)],
                     lambda E: E.activation(out=c.e_junk[:, :], in_=xt[s2][:, :], func=AF.Square, bias=s_[:, 1:2], scale=1.0,
                                            accum_out=s_[:, 2:3]))
                p.op('act', [('mo_st', s2, 2)], [('mo_st', s2, 3)],
                     lambda E: E.activation(out=s_[:, 3:4], in_=s_[:, 2:3], func=AF.Ln, scale=1.0 / D, bias=LN_EPS))
                p.op('act', [('mo_st', s2, 3)], [('mo_st', s2, 4)],
                     lambda E: E.activation(out=s_[:, 4:5], in_=s_[:, 3:4], func=AF.Exp, scale=-0.5))
                p.op('dve', [('mo_xt', s2), ('mo_st', s2, 1), ('mo_st', s2, 4)], [('mo_xh', 0)],
                     lambda E: E.tensor_scalar(out=xh[s2][:, :], in0=xt[s2][:, :], scalar1=s_[:, 1:2], scalar2=s_[:, 4:5],
                                               op0=ALU.add, op1=ALU.mult))
                for hh in range(2):
                    bb = hh
                    def tr32(E):
                        ins = None
                        for k in range(4):
                            kk = hh * 4 + k
                            ins = E.transpose(B[bb][:, k * 128:(k + 1) * 128], xh[s2][:, kk * 128:(kk + 1) * 128], ident32)
                        return ins
                    p.op('pe', [('mo_xh', 0), 'cst'], [bk(bb)], tr32)
                    for k in range(4):
                        kk = hh * 4 + k
                        p.op('dve', [bk(bb), 'modT'], [('mo_h32', 0, kk)],
                             lambda E: E.tensor_scalar(out=h32[s2][:, kk, :], in0=B[bb][:, k * 128:(k + 1) * 128],
                                                       scalar1=c.modT[:, r, 4, kk:kk + 1], scalar2=c.modT[:, r, 3, kk:kk + 1],
                                                       op0=ALU.mult, op1=ALU.add))
                hk = [('mo_h32', 0, kk) for kk in range(8)]
                p.op('pool', hk, [('mo_hT', j)], lambda E: E.tensor_copy(out=hT[:, :, j * 128:(j + 1) * 128], in_=h32[s2][:, :, :]))

                def mml(E):
                    for kk in range(8):
                        E.matmul(B[2][:, 0:32], lhsT=h32[s2][:, kk, :], rhs=rw[:, kk, :], start=(kk == 0), stop=False)
                    return E.matmul(B[2][:, 0:32], lhsT=ones[0:1, :], rhs=rb[0:1, :], start=False, stop=True)
                p.op('pe', hk + ['mo_c', 'cst'], [bk(2)], mml)
                p.op('dve', [bk(2)], [('mo_lg', s2)], lambda E: E.tensor_copy(out=lg[s2][:, :], in_=B[2][:, 0:32]))
                p.op('dve', [('mo_lg', s2)], [('mo_mx', s2)], lambda E: E.max(out=mx[s2][:, :], in_=lg[s2][:, :]))
                p.op('dve', [('mo_mx', s2)], [('mo_sm', s2, 0)],
                     lambda E: E.tensor_scalar(out=sm[s2][:, 0:1], in0=mx[s2][:, 0:1], scalar1=-1.0, scalar2=None, op0=ALU.mult))
                p.op('act', [('mo_lg', s2), ('mo_sm', s2, 0)], [('mo_ex', s2)],
                     lambda E: E.activation(out=ex[s2][:, :], in_=lg[s2][:, :], func=AF.Exp, bias=sm[s2][:, 0:1], scale=1.0))
                p.op('dve', [('mo_lg', s2), ('mo_mx', s2)], [('mo_lg', s2)],
                     lambda E: E.tensor_scalar(out=lg[s2][:, :], in0=lg[s2][:, :], scalar1=mx[s2][:, 3:4], scalar2=None, op0=ALU.is_ge))
                p.op('dve', [('mo_lg', s2), ('mo_ex', s2)], [('mo_ex', s2)],
                     lambda E: E.tensor_tensor(out=ex[s2][:, :], in0=ex[s2][:, :], in1=lg[s2][:, :], op=ALU.mult))
                p.op('dve', [('mo_ex', s2)], [('mo_sm', s2, 1)],
                     lambda E: E.tensor_reduce(out=sm[s2][:, 1:2], in_=ex[s2][:, :], axis=AX.X, op=ALU.add))
                p.op('dve', [('mo_sm', s2, 1)], [('mo_sm', s2, 1)], lambda E: E.reciprocal(out=sm[s2][:, 1:2], in_=sm[s2][:, 1:2]))
                p.op('dve', [('mo_ex', s2), ('mo_sm', s2, 1)], [('mo_G', j)],
                     lambda E: E.tensor_scalar(out=G[:, j, :], in0=ex[s2][:, :], scalar1=sm[s2][:, 1:2], scalar2=None, op0=ALU.mult))
                p.op('pe', [('mo_G', j), 'cst'], [bk(3)], lambda E: E.transpose(B[3][0:32, 0:128], G[:, j, :], ident32))
                p.op('act', [bk(3)], [('mo_GT', j)], lambda E: E.activation(out=GT[:, j, :], in_=B[3][0:32, 0:128], func=AF.Copy))
                for half in range(2):
                    p.op('pe', [('mo_GT', j), 'mo_c'], [bk(4 + half)],
                         lambda E: E.matmul(B[4 + half][:, :], lhsT=GT[:, j, :], rhs=b2[:, half * 512:(half + 1) * 512], start=True, stop=True))
                    p.op('act', [bk(4 + half)], [('mo_yacc', j, half)],
                         lambda E: E.activation(out=yacc[:, j, half * 512:(half + 1) * 512], in_=B[4 + half][:, :], func=AF.Copy))
            hTk = [('mo_hT', j) for j in range(ng)]
            ei = 0
            for e in range(32):
                ws = wi % 2
                wi += 1
                p.dma('sp', 'mo_w1%d' % ws, [('WB', l, e)], [('mo_w1', ws)],
                      [(w1[ws][:, :, :], c.WB1[l][e].rearrange("(k p) n -> p k n", p=128))])
                p.dma('sp', 'mo_w2', [('WB', l, e)], [('mo_w2', 0)],
                      [(w2[ws][:, :, :], c.WB2[l][e].rearrange("(k p) n -> p k n", p=128))])
                for th in range((T + 511) // 512):
                    c0 = th * 512
                    n = min(512, T - c0)
                    for fc in range(8):
                        s2 = ei % 2
                        ei += 1
                        bg, bl = s2 * 2, s2 * 2 + 1

                        def mm1(E):
                            ins = None
                            for k in range(8):
                                E.matmul(B[bg][:, 0:n], lhsT=w1[ws][:, k, fc * 128:(fc + 1) * 128], rhs=hT[:, k, c0:c0 + n],
                                         start=(k == 0), stop=(k == 7))
                            for k in range(8):
                                ins = E.matmul(B[bl][:, 0:n], lhsT=w1[ws][:, k, 1024 + fc * 128:1024 + (fc + 1) * 128],
                                               rhs=hT[:, k, c0:c0 + n], start=(k == 0), stop=(k == 7))
                            return ins
                        p.op('pe', hTk + [('mo_w1', ws)], [bk(bg), bk(bl)], mm1)
                        p.op('dve', [bk(bg), 'mo_b1'], [('mo_gg', s2)],
                             lambda E: E.tensor_scalar(out=gg[s2][:, 0:n], in0=B[bg][:, 0:n], scalar1=b1[:, e, fc:fc + 1], scalar2=7.0,
                                                       op0=ALU.add, op1=ALU.min))
                        p.op('act', [('mo_gg', s2)], [('mo_sg', s2)],
                             lambda E: E.activation(out=sg[s2][:, 0:n], in_=gg[s2][:, 0:n], func=AF.Sigmoid, scale=1.702))
                        p.op('dve', [bk(bl), 'mo_b1'], [('mo_ll', s2)],
                             lambda E: E.tensor_scalar(out=ll[s2][:, 0:n], in0=B[bl][:, 0:n], scalar1=b1[:, e, 8 + fc:9 + fc], scalar2=7.0,
                                                       op0=ALU.add, op1=ALU.min))
                        p.op('pool', [('mo_ll', s2)], [('mo_ll', s2)],
                             lambda E: E.tensor_scalar(out=ll[s2][:, 0:n], in0=ll[s2][:, 0:n], scalar1=-7.0, scalar2=1.0,
                                                       op0=ALU.max, op1=ALU.add))
                        p.op('pool', [('mo_gg', s2), ('mo_sg', s2)], [('mo_gg', s2)],
                             lambda E: E.tensor_tensor(out=gg[s2][:, 0:n], in0=gg[s2][:, 0:n], in1=sg[s2][:, 0:n], op=ALU.mult))
                        p.op('dve', [('mo_gg', s2), ('mo_ll', s2)], [('mo_aT', fc, th)],
                             lambda E: E.tensor_tensor(out=aT[:, fc, c0:c0 + n], in0=gg[s2][:, 0:n], in1=ll[s2][:, 0:n], op=ALU.mult))
                aTk = [('mo_aT', fc, th) for fc in range(8) for th in range((T + 511) // 512)]
                for j in range(ng):
                    for half in range(2):
                        bb = 4 + (j * 2 + half) % 4

                        def mm2(E):
                            ins = None
                            for k in range(8):
                                ins = E.matmul(B[bb][:, :], lhsT=aT[:, k, j * 128:(j + 1) * 128], rhs=w2[ws][:, k, half * 512:(half + 1) * 512],
                                               start=(k == 0), stop=(k == 7))
                            return ins
                        p.op('pe', aTk + [('mo_w2', 0)], [bk(bb)], mm2)
                        hs = slice(half * 512, (half + 1) * 512)
                        p.op('dve', [bk(bb), ('mo_G', j), ('mo_yacc', j, half)], [('mo_yacc', j, half)],
                             lambda E: E.scalar_tensor_tensor(out=yacc[:, j, hs], in0=B[bb][:, :], scalar=G[:, j, e:e + 1], in1=yacc[:, j, hs],
                                                              op0=ALU.mult, op1=ALU.add))
            for j, t in enumerate(gt):
                s2 = j % 2
                r = 1 if t < 2 else 0
                tok = slice(t * 128, (t + 1) * 128)
                p.dma('sp', 'mo_xt%d' % s2, ['XOUT'], [('mo_xt', s2)], [(xt[s2][:, :], c.X1[tok, :])])
                yk = [('mo_yacc', j, 0), ('mo_yacc', j, 1)]
                p.op('pool', yk + ['e_bc'], yk, lambda E: E.tensor_tensor(out=yacc[:, j, :], in0=yacc[:, j, :], in1=c.e_ag[:, r, :], op=ALU.mult))
                p.op('dve', yk + [('mo_xt', s2)], [('mo_xt', s2)],
                     lambda E: E.scalar_tensor_tensor(out=xt[s2][:, :], in0=xt[s2][:, :], scalar=ALPHA, in1=yacc[:, j, :],
                                                      op0=ALU.mult, op1=ALU.add))
                ln_affine_store(c, xt[s2], ('mo_xt', s2), c.e_gb, 'e_bc', dst_fn(t), s2)
        p.barrier()


def stage_moe2(c, l, tiles, dst_fn):
    p = c.p
    I = c.inp
    B = c.bank
    bk = lambda i: ('bank', i)
    cst = c.cst
    ones = cst[:, 640:768]
    ident32 = cst[:, 0:128]
    iota_f = cst[:, 768:800]
    iota_p = cst[:, 800:801]
    blk512 = cst[:, 896:1024]
    ntl = len(tiles)
    NB = (ntl * 128 * 4) // 512 + 32
    XB, YB, HB = c.XB, c.YB, c.HB
    W1f = c.WB1[l].rearrange("e (p k) n -> (e p) (k n)", k=8)
    W2f = c.WB2[l].rearrange("e (p k) n -> (e p) (k n)", k=8)
    I32 = mybir.dt.int32
    with ExitStack() as es_outer:
        c.es = es_outer
        slot_i = sb(c, 'ms_slot', [128, NTILE, 4], I32)
        gk = sb(c, 'ms_gk', [128, NTILE, 4], F32)
        idxw = sb(c, 'ms_idxw', [128, NB, 8], I32)
        be_bc = sb(c, 'ms_bebc', [128, NB], F32)
        OHall = sb(c, 'ms_oh', [32, NB], F32)
        c.e_ag = sb(c, 'e_ag', [128, 2, 1024], F32)
        c.e_gb = sb(c, 'e_gb', [128, 2, 1024], F32)
        c.e_st = [sb(c, 'e_st%d' % i, [128, 8], F32) for i in range(2)]
        c.e_junk = sb(c, 'e_junk', [128, 1024], BF16)
        load_bc_rows(c, l, 2)
        with ExitStack() as es:
            c.es = es
            Mall = sb(c, 'ms_M', [128, NTILE, 32], F32)
            Gall = sb(c, 'ms_G', [128, NTILE, 32], F32)
            Lall = sb(c, 'ms_L', [128, NTILE, 32], F32)
            mxall = sb(c, 'ms_mx', [128, NTILE, 8], F32)
            rw = sb(c, 'ms_rw', [128, 8, 32], F32)
            rb = sb(c, 'ms_rb', [1, 32], F32)
            modbc = sb(c, 'ms_modbc', [128, 2, 2, 1024], F32)
            xt = [sb(c, 'ms_xt%d' % i, [128, 1024], F32) for i in range(2)]
            xh = sb(c, 'ms_xh', [128, 1024], F32)
            hb = [sb(c, 'ms_hb%d' % i, [128, 1024], BF16) for i in range(2)]
            h32 = sb(c, 'ms_h32', [128, 8, 128], F32)
            ex = [sb(c, 'ms_ex%d' % i, [128, 32], F32) for i in range(2)]
            sm = [sb(c, 'ms_sm%d' % i, [128, 2], F32) for i in range(2)]
            st = [sb(c, 'ms_st%d' % i, [128, 8], F32) for i in range(2)]
            Lst = sb(c, 'ms_Lst', [128, 128], F32)
            Msum = sb(c, 'ms_Msum', [128, 32], F32)
            cnt = sb(c, 'ms_cnt', [1, 32], F32)
            cnti = sb(c, 'ms_cnti', [1, 32], I32)
            pcol = sb(c, 'ms_pcol', [32, 4], F32)
            psbc = sb(c, 'ms_psbc', [128, 32], F32)
            cmp_ = sb(c, 'ms_cmp', [32, 128], F32)
            berow = sb(c, 'ms_berow', [1, 128], F32)
            tmpf = sb(c, 'ms_tmpf', [128, 128], F32)
            pos = [sb(c, 'ms_pos%d' % i, [128, 32], F32) for i in range(2)]
            oh = [sb(c, 'ms_ohk%d' % i, [128, 32], F32) for i in range(2)]
            tq = [sb(c, 'ms_tq%d' % i, [128, 32], F32) for i in range(2)]
            sl = [sb(c, 'ms_sl%d' % i, [128, 4], F32) for i in range(2)]
            p.dma('sp', 'ms_c', [], ['ms_c'],
                  [(rw[:, :, :], I['router_w'][l].rearrange("(k p) e -> p k e", p=128)), (rb[:, :], I['router_b'][l:l + 1, :])])
            p.dma('sp', 'ms_modbc', [('modv', l)], ['ms_modbc'],
                  [(modbc[:, r, q, :], c.modv[l][r, (4 - q) * 1024:(5 - q) * 1024].partition_broadcast(128))
                   for r in range(2) for q in range(2)])
            for r in range(2):
                p.op('dve', ['ms_modbc'], ['ms_modbc'],
                     lambda E: E.tensor_scalar(out=modbc[:, r, 0, :], in0=modbc[:, r, 0, :], scalar1=1.0, scalar2=None, op0=ALU.add))
            p.op('dve', ['cst'], ['ms_Lst'], lambda E: E.tensor_tensor(out=Lst[:, :], in0=cst[:, 384:512], in1=ident32, op=ALU.subtract))
            def rload(jj):
                t = tiles[jj]
                p.dma('sp', 'ms_xt%d' % (jj % 2), ['XOUT'], [('ms_xt', jj % 2)], [(xt[jj % 2][:, :], c.X1[t * 128:(t + 1) * 128, :])])

            rload(0)
            for jj, t in enumerate(tiles):
                s2 = jj % 2
                r = 1 if t < 2 else 0
                tok = slice(t * 128, (t + 1) * 128)
                if jj + 1 < len(tiles):
                    rload(jj + 1)
                s_ = st[s2]
                p.op('act', [('ms_xt', s2)], ['e_junk', ('ms_st', s2, 0)],
                     lambda E: E.activation(out=c.e_junk[:, :], in_=xt[s2][:, :], func=AF.Copy, accum_out=s_[:, 0:1]))
                p.op('dve', [('ms_st', s2, 0)], [('ms_st', s2, 1)],
                     lambda E: E.tensor_scalar(out=s_[:, 1:2], in0=s_[:, 0:1], scalar1=-1.0 / D, scalar2=None, op0=ALU.mult))
                p.op('act', [('ms_xt', s2), ('ms_st', s2, 1)], ['e_junk', ('ms_st', s2, 2)],
                     lambda E: E.activation(out=c.e_junk[:, :], in_=xt[s2][:, :], func=AF.Square, bias=s_[:, 1:2], scale=1.0,
                                            accum_out=s_[:, 2:3]))
                p.op('act', [('ms_st', s2, 2)], [('ms_st', s2, 3)],
                     lambda E: E.activation(out=s_[:, 3:4], in_=s_[:, 2:3], func=AF.Ln, scale=1.0 / D, bias=LN_EPS))
                p.op('act', [('ms_st', s2, 3)], [('ms_st', s2, 4)],
                     lambda E: E.activation(out=s_[:, 4:5], in_=s_[:, 3:4], func=AF.Exp, scale=-0.5))
                p.op('dve', [('ms_xt', s2), ('ms_st', s2, 1), ('ms_st', s2, 4)], ['ms_xh'],
                     lambda E: E.tensor_scalar(out=xh[:, :], in0=xt[s2][:, :], scalar1=s_[:, 1:2], scalar2=s_[:, 4:5],
                                               op0=ALU.add, op1=ALU.mult))
                p.op('pool', ['ms_xh', 'ms_modbc'], [('ms_xt', s2)],
                     lambda E: E.tensor_tensor(out=xt[s2][:, :], in0=xh[:, :], in1=modbc[:, r, 0, :], op=ALU.mult))
                p.op('pool', [('ms_xt', s2), 'ms_modbc'], [('ms_hb', s2)],
                     lambda E: E.tensor_tensor(out=hb[s2][:, :], in0=xt[s2][:, :], in1=modbc[:, r, 1, :], op=ALU.add))
                p.dma('sp', 'ms_hb%d' % s2, [('ms_hb', s2)], [('HB', t)], [(HB[tok, :], hb[s2][:, :])])
                for hh in range(2):
                    def tr32(E):
                        ins = None
                        for k in range(4):
                            kk = hh * 4 + k
                            ins = E.transpose(B[hh][:, k * 128:(k + 1) * 128], xh[:, kk * 128:(kk + 1) * 128], ident32)
                        return ins
                    p.op('pe', ['ms_xh', 'cst'], [bk(hh)], tr32)
                    for k in range(4):
                        kk = hh * 4 + k
                        p.op('dve', [bk(hh), 'modT'], [('ms_h32', kk)],
                             lambda E: E.tensor_scalar(out=h32[:, kk, :], in0=B[hh][:, k * 128:(k + 1) * 128],
                                                       scalar1=c.modT[:, r, 4, kk:kk + 1], scalar2=c.modT[:, r, 3, kk:kk + 1],
                                                       op0=ALU.mult, op1=ALU.add))
                hk = [('ms_h32', kk) for kk in range(8)]

                def mml(E):
                    for kk in range(8):
                        E.matmul(B[2][:, 0:32], lhsT=h32[:, kk, :], rhs=rw[:, kk, :], start=(kk == 0), stop=False)
                    return E.matmul(B[2][:, 0:32], lhsT=ones[0:1, :], rhs=rb[0:1, :], start=False, stop=True)
                p.op('pe', hk + ['ms_c', 'cst'], [bk(2)], mml)
                lgj, mxj, Mj, Gj = Lall[:, t, :], mxall[:, t, :], Mall[:, t, :], Gall[:, t, :]
                p.op('dve', [bk(2)], [('ms_L', t)], lambda E: E.tensor_copy(out=lgj, in_=B[2][:, 0:32]))
                p.op('dve', [('ms_L', t)], [('ms_mx', t)], lambda E: E.max(out=mxj, in_=lgj))
                p.op('dve', [('ms_mx', t)], [('ms_sm', s2, 0)],
                     lambda E: E.tensor_scalar(out=sm[s2][:, 0:1], in0=mxall[:, t, 0:1], scalar1=-1.0, scalar2=None, op0=ALU.mult))
                p.op('act', [('ms_L', t), ('ms_sm', s2, 0)], [('ms_ex', s2)],
                     lambda E: E.activation(out=ex[s2][:, :], in_=lgj, func=AF.Exp, bias=sm[s2][:, 0:1], scale=1.0))
                p.op('dve', [('ms_L', t), ('ms_mx', t)], [('ms_M', t)],
                     lambda E: E.tensor_scalar(out=Mj, in0=lgj, scalar1=mxall[:, t, 3:4], scalar2=None, op0=ALU.is_ge))
                p.op('dve', [('ms_M', t), ('ms_ex', s2)], [('ms_ex', s2)],
                     lambda E: E.tensor_tensor(out=ex[s2][:, :], in0=ex[s2][:, :], in1=Mj, op=ALU.mult))
                p.op('dve', [('ms_ex', s2)], [('ms_sm', s2, 1)],
                     lambda E: E.tensor_reduce(out=sm[s2][:, 1:2], in_=ex[s2][:, :], axis=AX.X, op=ALU.add))
                p.op('dve', [('ms_sm', s2, 1)], [('ms_sm', s2, 1)], lambda E: E.reciprocal(out=sm[s2][:, 1:2], in_=sm[s2][:, 1:2]))
                p.op('dve', [('ms_ex', s2), ('ms_sm', s2, 1)], [('ms_G', t)],
                     lambda E: E.tensor_scalar(out=Gj, in0=ex[s2][:, :], scalar1=sm[s2][:, 1:2], scalar2=None, op0=ALU.mult))
                p.op('pe', [('ms_M', t), 'cst'], [bk(7)],
                     lambda E: E.matmul(B[7][0:1, 0:32], lhsT=ones[:, 0:1], rhs=Mj, start=(jj == 0), stop=(jj == ntl - 1)))
            p.op('dve', [bk(7)], ['ms_cnt'], lambda E: E.tensor_scalar(out=cnt[:, :], in0=B[7][0:1, 0:32], scalar1=1.0 / 512, scalar2=255.5 / 512,
                                                                       op0=ALU.mult, op1=ALU.add))
            p.op('dve', ['ms_cnt'], ['ms_cnti'], lambda E: E.tensor_copy(out=cnti[:, :], in_=cnt[:, :]))
            p.op('dve', ['ms_cnti'], ['ms_cnt'], lambda E: E.tensor_copy(out=cnt[:, :], in_=cnti[:, :]))
            p.op('dve', ['ms_cnt'], ['ms_cnt'], lambda E: E.tensor_scalar(out=cnt[:, :], in0=cnt[:, :], scalar1=512.0, scalar2=None, op0=ALU.mult))
            p.op('pe', ['ms_cnt', 'cst'], [bk(0)], lambda E: E.transpose(B[0][0:32, 0:1], cnt[0:1, :], ident32[0:1, 0:1]))
            p.op('dve', [bk(0)], ['ms_pcol'], lambda E: E.tensor_copy(out=pcol[:, 0:1], in_=B[0][0:32, 0:1]))
            p.op('pe', ['ms_pcol', 'ms_Lst'], [bk(1)],
                 lambda E: E.matmul(B[1][0:1, 0:32], lhsT=pcol[:, 0:1], rhs=Lst[0:32, 0:32], start=True, stop=True))
            p.op('dve', [bk(1)], ['ms_cnt'], lambda E: E.tensor_copy(out=cnt[:, :], in_=B[1][0:1, 0:32]))
            p.op('pe', ['ms_cnt', 'cst'], [bk(1)],
                 lambda E: E.matmul(B[1][:, 0:32], lhsT=ones[0:1, :], rhs=cnt[0:1, :], start=True, stop=True))
            p.op('dve', [bk(1)], ['ms_psbc'], lambda E: E.tensor_copy(out=psbc[:, :], in_=B[1][:, 0:32]))
            p.op('pe', ['ms_pcol', 'cst'], [bk(0)],
                 lambda E: E.matmul(B[0][0:32, 0:1], lhsT=cst[0:32, 384:416], rhs=pcol[:, 0:1], start=True, stop=True))
            p.op('dve', [bk(0)], ['ms_pcol2'], lambda E: E.tensor_copy(out=pcol[:, 1:2], in_=B[0][0:32, 0:1]))
            p.op('dve', ['ms_pcol2', 'cst'], ['ms_cmp'],
                 lambda E: E.tensor_scalar(out=cmp_[:, :], in0=blk512[0:32, :], scalar1=pcol[:, 1:2], scalar2=None, op0=ALU.is_ge))
            p.op('pe', ['ms_cmp', 'cst'], [bk(0)],
                 lambda E: E.matmul(B[0][0:1, 0:128], lhsT=ones[0:32, 0:1], rhs=cmp_[:, :], start=True, stop=True))
            p.op('dve', [bk(0)], ['ms_berow'], lambda E: E.tensor_copy(out=berow[:, :], in_=B[0][0:1, 0:128]))
            p.op('pe', ['ms_berow', 'cst'], [bk(0)],
                 lambda E: E.matmul(B[0][:, 0:128], lhsT=ones[0:1, :], rhs=berow[0:1, :], start=True, stop=True))
            p.op('dve', [bk(0)], ['ms_oob'],
                 lambda E: E.tensor_scalar(out=tmpf[:, 0:NB], in0=B[0][:, 0:NB], scalar1=31.5, scalar2=0.0, op0=ALU.is_ge, op1=ALU.mult))
            p.op('dve', [bk(0)], ['ms_bebc'],
                 lambda E: E.tensor_scalar(out=be_bc[:, :], in0=B[0][:, 0:NB], scalar1=31.0, scalar2=None, op0=ALU.min))
            p.op('dve', ['ms_bebc', 'cst'], ['ms_oh'],
                 lambda E: E.tensor_scalar(out=OHall[:, :], in0=be_bc[0:32, :], scalar1=iota_p[0:32, :], scalar2=None, op0=ALU.is_equal))
            p.op('dve', ['ms_bebc', 'cst', 'ms_oob'], ['ms_oob'],
                 lambda E: E.tensor_scalar(out=tmpf[:, 0:NB], in0=tmpf[:, 0:NB], scalar1=iota_p[:, :], scalar2=None, op0=ALU.add))
            p.op('dve', ['ms_bebc', 'ms_oob'], ['ms_tmpf'],
                 lambda E: E.scalar_tensor_tensor(out=tmpf[:, 0:NB], in0=be_bc[:, :], scalar=128.0, in1=tmpf[:, 0:NB], op0=ALU.mult, op1=ALU.add))
            p.op('dve', ['ms_tmpf'], ['ms_idxw'], lambda E: E.tensor_copy(out=idxw[:, :, 0], in_=tmpf[:, 0:NB]))
            p.op('dve', [], ['ms_Msum'], lambda E: E.memset(Msum[:, :], 0.0))
            for jj, t in enumerate(tiles):
                s2 = jj % 2
                tok = slice(t * 128, (t + 1) * 128)
                lgj, Mj, Gj = Lall[:, t, :], Mall[:, t, :], Gall[:, t, :]

                def mmp(E):
                    E.matmul(B[3 + s2][:, 0:32], lhsT=Lst[:, :], rhs=Mj, start=True, stop=False)
                    return E.matmul(B[3 + s2][:, 0:32], lhsT=ones[:, :], rhs=Msum[:, :], start=False, stop=True)
                p.op('pe', [('ms_M', t), 'ms_Msum', 'ms_Lst', 'cst'], [bk(3 + s2)], mmp)
                p.op('dve', [bk(3 + s2), 'ms_psbc'], [('ms_pos', s2)],
                     lambda E: E.tensor_tensor(out=pos[s2][:, :], in0=B[3 + s2][:, 0:32], in1=psbc[:, :], op=ALU.add))
                p.op('pool', [('ms_M', t), 'ms_Msum'], ['ms_Msum'],
                     lambda E: E.tensor_tensor(out=Msum[:, :], in0=Msum[:, :], in1=Mj, op=ALU.add))
                for k in range(4):
                    p.op('dve', [('ms_L', t), ('ms_mx', t)], [('ms_ohk', s2)],
                         lambda E: E.tensor_scalar(out=oh[s2][:, :], in0=lgj, scalar1=mxall[:, t, k:k + 1], scalar2=None, op0=ALU.is_equal))
                    p.op('dve', [('ms_ohk', s2), ('ms_pos', s2)], [('ms_tq', s2)],
                         lambda E: E.tensor_tensor(out=tq[s2][:, :], in0=oh[s2][:, :], in1=pos[s2][:, :], op=ALU.mult))
                    p.op('dve', [('ms_tq', s2)], [('ms_sl', s2, k)],
                         lambda E: E.tensor_reduce(out=sl[s2][:, k:k + 1], in_=tq[s2][:, :], axis=AX.X, op=ALU.add))
                    p.op('dve', [('ms_ohk', s2), ('ms_G', t)], [('ms_tq', s2)],
                         lambda E: E.tensor_tensor(out=tq[s2][:, :], in0=oh[s2][:, :], in1=Gj, op=ALU.mult))
                    p.op('dve', [('ms_tq', s2)], [('ms_gk', t, k)],
                         lambda E: E.tensor_reduce(out=gk[:, t, k:k + 1], in_=tq[s2][:, :], axis=AX.X, op=ALU.add))
                p.op('dve', [('ms_sl', s2, k) for k in range(4)], [('ms_slot', t)],
                     lambda E: E.tensor_copy(out=slot_i[:, t, :], in_=sl[s2][:, :]))
                p.dma('sp', 'ms_hbl%d' % s2, [('HB', t)], [('ms_hb', s2)], [(hb[s2][:, :], HB[tok, :])])
                p.idma('scat', [('ms_hb', s2), ('ms_slot', t), 'XBZ'], [('XB', t)],
                       [dict(out=XB[:, :], out_offset=bass.IndirectOffsetOnAxis(ap=slot_i[:, t, k:k + 1], axis=0),
                             in_=hb[s2][:, :], in_offset=None) for k in range(4)])
            p.barrier()
        if c.cfg.get('moe_phases', 3) < 2:
            return
        with ExitStack() as es:
            c.es = es
            w1 = [sb(c, 'me_w1%d' % i, [128, 8, 2048], BF16) for i in range(2)]
            w2 = [sb(c, 'me_w2%d' % i, [128, 8, 1024], BF16) for i in range(2)]
            xb = [sb(c, 'me_xb%d' % i, [128, 4, 1024], BF16) for i in range(2)]
            xT = [sb(c, 'me_xT%d' % i, [128, 8, 512], BF16) for i in range(2)]
            aT = sb(c, 'me_aT', [128, 8, 512], BF16)
            yb = [sb(c, 'me_yb%d' % i, [128, 1024], BF16) for i in range(2)]
            b1 = sb(c, 'me_b1', [128, 32, 16], F32)
            b2 = sb(c, 'me_b2', [32, 1024], BF16)
            b2f = sb(c, 'me_b2f', [32, 1024], F32)
            ohr = [sb(c, 'me_ohr%d' % i, [128, 32], F32) for i in range(2)]
            b1t = sb(c, 'me_b1t', [128, 32, 16], F32)
            b1s = [sb(c, 'me_b1s%d' % i, [128, 16], F32) for i in range(2)]
            ohb = [sb(c, 'me_ohb%d' % i, [32, 128], BF16) for i in range(2)]
            gg = [sb(c, 'me_gg%d' % i, [128, 512], F32) for i in range(2)]
            sg = [sb(c, 'me_sg%d' % i, [128, 512], F32) for i in range(2)]
            ll = [sb(c, 'me_ll%d' % i, [128, 512], F32) for i in range(2)]
            p.dma('sp', 'me_b2', [], ['me_b2f'], [(b2f[:, :], I['moe_b2'][l])])
            p.op('dve', ['me_b2f'], ['me_b2'], lambda E: E.tensor_copy(out=b2[:, :], in_=b2f[:, :]))
            p.dma('sp', 'me_b1', [], ['me_b1'],
                  [(b1[:, e, :], I['moe_b1'][l, e, :].rearrange("(j p) -> p j", p=128)) for e in range(32)], slow=True)
            wbk = [('WB', l, e) for e in range(32)]
            ei = 0
            def gather_w(i):
                ws = i % 2
                wdeps = (wbk if i < 2 else []) + ['ms_idxw']
                p.idma('gw1%d' % ws, wdeps, [('me_w1', ws)],
                       [dict(out=w1[ws][:, :, :].rearrange("p k n -> p (k n)"), out_offset=None, in_=W1f[:, :],
                             in_offset=bass.IndirectOffsetOnAxis(ap=idxw[:, i, 0:1], axis=0))])
                p.idma('gw2%d' % ws, wdeps, [('me_w2', ws)],
                       [dict(out=w2[ws][:, :, :].rearrange("p k n -> p (k n)"), out_offset=None, in_=W2f[:, :],
                             in_offset=bass.IndirectOffsetOnAxis(ap=idxw[:, i, 0:1], axis=0))])

            def load_xb(i):
                ws = i % 2
                xbk = [('XB', t) for t in tiles] if i < 2 else []
                p.dma('sp', 'me_xb%d' % ws, xbk, [('me_xb', ws)],
                      [(xb[ws][:, :, :], XB[i * 512:(i + 1) * 512, :].rearrange("(a p) f -> p a f", p=128))])

            gather_w(0)
            for i in range(NB):
                ws = i % 2
                if i + 1 < NB:
                    gather_w(i + 1)
                if i == 0:
                    load_xb(0)
                if i + 1 < NB:
                    load_xb(i + 1)
                for a in range(4):
                    tb = 4 + (i * 4 + a) % 2
                    tpv = bank16(c, tb)

                    def trx(E):
                        ins = None
                        for k in range(8):
                            ins = E.transpose(tpv[:, k, :], xb[ws][:, a, k * 128:(k + 1) * 128], c.ident[:, :])
                        return ins
                    p.op('pe', [('me_xb', ws), 'ident'], [bk(tb)], trx)
                    if a % 2 == 0:
                        p.op('act', [bk(tb)], [('me_xT', ws, a)],
                             lambda E: E.activation(out=xT[ws][:, :, a * 128:(a + 1) * 128], in_=tpv[:, :, :], func=AF.Copy))
                    else:
                        p.op('dve', [bk(tb)], [('me_xT', ws, a)],
                             lambda E: E.tensor_copy(out=xT[ws][:, :, a * 128:(a + 1) * 128], in_=tpv[:, :, :]))
                xTk = [('me_xT', ws, a) for a in range(4)]
                p.op('dve', ['ms_bebc', 'cst'], [('me_ohr', ws)],
                     lambda E: E.tensor_scalar(out=ohr[ws][:, :], in0=iota_f, scalar1=be_bc[:, i:i + 1], scalar2=None, op0=ALU.is_equal))
                p.op('dve', [('me_ohr', ws), 'me_b1'], ['me_b1t'],
                     lambda E: E.tensor_tensor(out=b1t[:, :, :], in0=b1[:, :, :], in1=ohr[ws][:, :].unsqueeze(2).to_broadcast([128, 32, 16]),
                                               op=ALU.mult))
                p.op('dve', ['me_b1t'], [('me_b1s', ws)],
                     lambda E: E.tensor_reduce(out=b1s[ws][:, :], in_=b1t[:, :, :].rearrange("p e j -> p j e"), axis=AX.X, op=ALU.add))
                p.op('dve', ['ms_oh', 'cst'], [('me_ohb', ws)],
                     lambda E: E.tensor_scalar(out=ohb[ws][:, :], in0=ones[0:32, :], scalar1=OHall[:, i:i + 1], scalar2=None, op0=ALU.mult))
                for fc in range(8):
                    s2 = ei % 2
                    ei += 1
                    bg, bl = s2 * 2, s2 * 2 + 1

                    def mm1(E):
                        ins = None
                        for k in range(8):
                            E.matmul(B[bg][:, :], lhsT=w1[ws][:, k, fc * 128:(fc + 1) * 128], rhs=xT[ws][:, k, :], start=(k == 0), stop=(k == 7))
                        for k in range(8):
                            ins = E.matmul(B[bl][:, :], lhsT=w1[ws][:, k, 1024 + fc * 128:1024 + (fc + 1) * 128], rhs=xT[ws][:, k, :],
                                           start=(k == 0), stop=(k == 7))
                        return ins
                    p.op('pe', xTk + [('me_w1', ws)], [bk(bg), bk(bl)], mm1)
                    p.op('dve', [bk(bg), ('me_b1s', ws)], [('me_gg', s2)],
                         lambda E: E.tensor_scalar(out=gg[s2][:, :], in0=B[bg][:, :], scalar1=b1s[ws][:, fc:fc + 1], scalar2=7.0,
                                                   op0=ALU.add, op1=ALU.min))
                    p.op('act', [('me_gg', s2)], [('me_sg', s2)],
                         lambda E: E.activation(out=sg[s2][:, :], in_=gg[s2][:, :], func=AF.Silu, scale=1.702))
                    p.op('dve', [bk(bl), ('me_b1s', ws)], [('me_ll', s2)],
                         lambda E: E.tensor_scalar(out=ll[s2][:, :], in0=B[bl][:, :], scalar1=b1s[ws][:, 8 + fc:9 + fc], scalar2=7.0,
                                                   op0=ALU.add, op1=ALU.min))
                    p.op('dve', [('me_ll', s2)], [('me_ll', s2)],
                         lambda E: E.tensor_scalar(out=ll[s2][:, :], in0=ll[s2][:, :], scalar1=-7.0, scalar2=1.0, op0=ALU.max, op1=ALU.add))
                    p.op('dve', [('me_sg', s2), ('me_ll', s2)], [('me_aT', fc)],
                         lambda E: E.scalar_tensor_tensor(out=aT[:, fc, :], in0=sg[s2][:, :], scalar=1.0 / 1.702, in1=ll[s2][:, :],
                                                          op0=ALU.mult, op1=ALU.mult))
                aTk = [('me_aT', fc) for fc in range(8)]
                for a in range(4):
                    y2 = (i * 4 + a) % 2
                    for half in range(2):
                        bb = 6 + half

                        def mm2(E):
                            for k in range(8):
                                E.matmul(B[bb][:, :], lhsT=aT[:, k, a * 128:(a + 1) * 128], rhs=w2[ws][:, k, half * 512:(half + 1) * 512],
                                         start=(k == 0), stop=False)
                            return E.matmul(B[bb][:, :], lhsT=ohb[ws][:, :], rhs=b2[:, half * 512:(half + 1) * 512], start=False, stop=True)
                        p.op('pe', aTk + [('me_w2', ws), ('me_ohb', ws), 'me_b2'], [bk(bb)], mm2)
                        if half == 0:
                            p.op('act', [bk(bb)], [('me_yb', y2, half)],
                                 lambda E: E.activation(out=yb[y2][:, 0:512], in_=B[bb][:, :], func=AF.Copy))
                        else:
                            p.op('dve', [bk(bb)], [('me_yb', y2, half)], lambda E: E.tensor_copy(out=yb[y2][:, 512:1024], in_=B[bb][:, :]))
                    r0 = i * 512 + a * 128
                    p.dma('sp', 'me_yb%d' % y2, [('me_yb', y2, 0), ('me_yb', y2, 1)], [('YB', i, a)], [(YB[r0:r0 + 128, :], yb[y2][:, :])])
            p.barrier()
        if c.cfg.get('moe_phases', 3) < 3:
            return
        with ExitStack() as es:
            c.es = es
            yg = [[sb(c, 'mc_yg%d_%d' % (i, k), [128, 1024], BF16) for k in range(4)] for i in range(2)]
            xt = [sb(c, 'mc_xt%d' % i, [128, 1024], F32) for i in range(2)]
            ff = [sb(c, 'mc_ff%d' % i, [128, 1024], F32) for i in range(2)]
            def cloads(jj):
                t = tiles[jj]
                s2 = jj % 2
                tok = slice(t * 128, (t + 1) * 128)
                p.dma('sp', 'mc_xt%d' % s2, ['XOUT'], [('mc_xt', s2)], [(xt[s2][:, :], c.X1[tok, :])])
                ybk = [('YB', i, a) for i in range(NB) for a in range(4)] if jj < 2 else []
                p.idma('gy%d' % s2, [('ms_slot', t)] + ybk, [('mc_yg', s2)],
                       [dict(out=yg[s2][k][:, :], out_offset=None, in_=YB[:, :],
                             in_offset=bass.IndirectOffsetOnAxis(ap=slot_i[:, t, k:k + 1], axis=0)) for k in range(4)])

            cloads(0)
            for jj, t in enumerate(tiles):
                s2 = jj % 2
                r = 1 if t < 2 else 0
                tok = slice(t * 128, (t + 1) * 128)
                if jj + 1 < len(tiles):
                    cloads(jj + 1)
                p.op('dve', [('mc_yg', s2), ('ms_gk', t, 0)], [('mc_ff', s2)],
                     lambda E: E.tensor_scalar(out=ff[s2][:, :], in0=yg[s2][0][:, :], scalar1=gk[:, t, 0:1], scalar2=None, op0=ALU.mult))
                for k in range(1, 4):
                    p.op('dve', [('mc_yg', s2), ('ms_gk', t, k), ('mc_ff', s2)], [('mc_ff', s2)],
                         lambda E: E.scalar_tensor_tensor(out=ff[s2][:, :], in0=yg[s2][k][:, :], scalar=gk[:, t, k:k + 1], in1=ff[s2][:, :],
                                                          op0=ALU.mult, op1=ALU.add))
                p.op('pool', [('mc_ff', s2), 'e_bc'], [('mc_ff', s2)],
                     lambda E: E.tensor_tensor(out=ff[s2][:, :], in0=ff[s2][:, :], in1=c.e_ag[:, r, :], op=ALU.mult))
                p.op('dve', [('mc_ff', s2), ('mc_xt', s2)], [('mc_xt', s2)],
                     lambda E: E.scalar_tensor_tensor(out=xt[s2][:, :], in0=xt[s2][:, :], scalar=ALPHA, in1=ff[s2][:, :],
                                                      op0=ALU.mult, op1=ALU.add))
                ln_affine_store(c, xt[s2], ('mc_xt', s2), c.e_gb, 'e_bc', dst_fn(t), s2)
            p.barrier()


def make_rope():
    rows = NLAT // 64
    row = np.repeat(np.arange(rows, dtype=np.float32), 64)
    col = np.tile(np.arange(64, dtype=np.float32), rows)
    inv = (np.float32(10000.0) ** (-np.arange(8, dtype=np.float32) / np.float32(8))).astype(np.float32)
    ang = np.concatenate([row[:, None] * inv, col[:, None] * inv], -1).astype(np.float32)
    return np.concatenate([np.cos(ang), np.sin(ang)], -1).astype(np.float32)


def make_hyena_consts():
    import ml_dtypes
    bf = ml_dtypes.bfloat16
    N = 16384
    out = {}
    a = np.arange(128, dtype=np.float64)
    f = np.arange(128, dtype=np.float64)
    ang = 2 * np.pi * np.outer(a, f) / 128
    d1 = np.zeros((128, 2, 2, 128), np.float64)
    d1[:, 0, 0, :] = np.cos(ang)
    d1[:, 0, 1, :] = -np.sin(ang)
    for aa in range(4):
        for ff_ in range(4):
            d1[aa, 1, 0, ff_] = np.cos(2 * np.pi * aa * ff_ / 4)
            d1[aa, 1, 1, ff_] = -np.sin(2 * np.pi * aa * ff_ / 4)
    out['hy_dft1'] = d1.astype(np.float32).astype(bf)
    e3 = np.zeros((128, 3, 128), np.float64)
    e3[:, 0, :] = np.cos(ang)
    e3[:, 1, :] = np.sin(ang)
    e3[:, 2, :] = -np.sin(ang)
    out['hy_e3'] = e3.astype(np.float32).astype(bf)
    f1 = np.arange(128)[:, None, None]
    b = np.arange(128)[None, :, None]
    f2 = np.arange(128)[None, None, :]
    th = 2 * np.pi * ((b * (f1 + 128 * f2)) % N) / N
    tw2 = np.stack([np.cos(th), -np.sin(th), np.sin(th)], axis=2)
    out['hy_tw2'] = tw2.astype(np.float32).astype(bf)
    bb = np.arange(128)[:, None, None]
    ff = np.arange(128)[None, :, None]
    aa = np.arange(64)[None, None, :]
    ps_ = 2 * np.pi * (((128 * aa + bb) * ff) % N) / N
    twf = np.stack([np.cos(ps_), -np.sin(ps_)], axis=2)
    out['hy_twf'] = twf.astype(np.float32).astype(bf)
    f1c = np.arange(4)[:, None, None]
    thc = 2 * np.pi * ((b * (f1c + 4 * f2)) % 512) / 512
    out['hy_tw2c'] = np.stack([np.cos(thc), -np.sin(thc), np.sin(thc)], axis=2).astype(np.float32).astype(bf)
    ffc = np.arange(4)[None, :, None]
    aac = np.arange(2)[None, None, :]
    psc = 2 * np.pi * (((128 * aac + bb) * ffc) % 512) / 512
    out['hy_twfc'] = np.stack([np.cos(psc), -np.sin(psc)], axis=2).astype(np.float32).astype(bf)
    deltas = np.abs(np.linspace(math.log(1e-2) / 1.5, math.log(1e-2) / 0.3, 512, dtype=np.float32))
    out['hy_negdelta'] = (-deltas).astype(np.float32)[None, :]

    def feats(L, s):
        s = np.asarray(s)
        t = np.linspace(0.0, 1.0, L, dtype=np.float32)[s][:, None]
        w = (np.float32(2 * math.pi) * np.arange(L, dtype=np.float32) / np.float32(L))[s][:, None]
        fq = np.linspace(1e-4, 15, 16, dtype=np.float32)
        z = np.concatenate([t, np.cos(fq * w), -np.sin(fq * w)], -1).astype(np.float32)
        return z, t[:, 0]
    BIG = 1e4
    L = 8192
    tau = np.arange(N)
    s = np.where(tau < L, tau, N - tau)
    s[L] = 0
    z, t = feats(L, s)
    out['hy_feat0'] = np.ascontiguousarray(z.T)
    out['hy_tvec0'] = t[None, :].astype(np.float32)
    L = 256
    tau = np.arange(512)
    s = np.where(tau < L, tau, 512 - tau)
    s[256] = 0
    z, t = feats(L, s)
    out['hy_feat1'] = np.ascontiguousarray(z.T)
    out['hy_tvec1'] = t[None, :].astype(np.float32)
    return out


def make_consts():
    cst = np.zeros((128, 1024), np.float32)
    cst[:, 0:128] = np.eye(128)
    U = (np.arange(128)[:, None] <= np.arange(128)[None, :]).astype(np.float32)
    cst[:, 128:256] = U / 16.0
    cst[:, 256:384] = U.T / 16.0
    cst[:, 384:512] = U
    cst[:, 512:640] = U.T
    cst[:, 640:768] = 1.0
    cst[:, 768:800] = np.arange(32, dtype=np.float32)[None, :]
    cst[:, 800] = np.arange(128, dtype=np.float32)
    cst[:, 896:1024] = 512.0 * np.arange(128, dtype=np.float32)[None, :]
    return cst


def build_program(cfg):
    nc = bass.Bass("TRN2", target_bir_lowering=False)
    es = ExitStack()
    c = Ctx()
    c.nc, c.es, c.cfg = nc, es, cfg
    c.dbg = set(cfg.get('dbg', []))
    c.p = Prog(nc, es)
    p = c.p
    layers = cfg.get('layers', [0, 1])
    stages = cfg.get('stages', ['adaln', 'proj'])

    def ext(name, shape, dt=F32):
        return nc.dram_tensor(name, list(shape), dt, kind="ExternalInput").ap()

    I = {}
    I['x'] = ext('x', [NLAT, D])
    I['ctx'] = ext('ctx', [NCTX, D])
    I['cc'] = ext('cc', [2, D])
    I['ada_w'] = ext('ada_w', [DEPTH, D, 6 * D])
    I['ada_b'] = ext('ada_b', [DEPTH, 6 * D])
    I['w_in'] = ext('w_in', [DEPTH, D, IN_TOTAL])
    I['cst'] = ext('cst', [128, 1024])
    for nm, shp in [('gla_wa2_f', [DEPTH, 16, 256]), ('gla_ba_f', [DEPTH, 256]), ('gla_wa2_b', [DEPTH, 16, 256]),
                    ('gla_ba_b', [DEPTH, 256]), ('gla_norm', [DEPTH, 128]),
                    ('mla_q_norm', [DEPTH, 384]), ('mla_w_uq', [DEPTH, 384, 768]), ('mla_kv_norm', [DEPTH, 256]),
                    ('mla_w_ukv', [DEPTH, 256, 1024]), ('rope', [NLAT, 32]),
                    ('hy_conv_w', [DEPTH, 3, 1536]), ('hy_conv_b', [DEPTH, 1536]), ('hy_w1', [DEPTH, 33, 64]),
                    ('hy_b1', [DEPTH, 64]), ('hy_w2', [DEPTH, 64, 64]), ('hy_b2', [DEPTH, 64]), ('hy_w3', [DEPTH, 64, 2048]),
                    ('hy_freq', [DEPTH, 64]), ('hy_bias', [DEPTH, 2, 512]),
                    ('hy_feat0', [33, 16384]), ('hy_tvec0', [1, 16384]), ('hy_feat1', [33, 512]), ('hy_tvec1', [1, 512]),
                    ('hy_negdelta', [1, 512]),
                    ('w_br_gla', [DEPTH, 512, D]), ('w_br_mla', [DEPTH, 512, D]), ('w_br_hy', [DEPTH, 512, D]),
                    ('w_out', [DEPTH, D, D]), ('ln1_g', [DEPTH, D]), ('ln1_b', [DEPTH, D]), ('ln2_g', [DEPTH, D]),
                    ('ln2_b', [DEPTH, D]), ('router_w', [DEPTH, D, 32]), ('router_b', [DEPTH, 32]),
                    ('moe_b1', [DEPTH, 32, 2048]), ('moe_b2', [DEPTH, 32, D])]:
        I[nm] = ext(nm, shp)
    for nm, shp in [('hy_dft1', [128, 2, 2, 128]), ('hy_e3', [128, 3, 128]), ('hy_tw2', [128, 128, 3, 128]),
                    ('hy_twf', [128, 128, 2, 64]), ('hy_tw2c', [4, 128, 3, 128]), ('hy_twfc', [128, 4, 2, 2])]:
        I[nm] = ext(nm, shp, BF16)
    if 'moe' in stages:
        I['moe_w1'] = ext('moe_w1', [DEPTH, 32, D, 2048])
        I['moe_w2'] = ext('moe_w2', [DEPTH, 32, D, D])
        c.WB1 = [nc.dram_tensor('WB1_%d' % l, [32, D, 2048], BF16).ap() for l in range(DEPTH)]
        c.WB2 = [nc.dram_tensor('WB2_%d' % l, [32, D, D], BF16).ap() for l in range(DEPTH)]
        c.XB = nc.dram_tensor('XB', [98 * 512, D], BF16).ap()
        c.YB = nc.dram_tensor('YB', [98 * 512, D], BF16).ap()
        c.HB = nc.dram_tensor('HB', [NT, D], BF16).ap()
    c.inp = I
    c.out = nc.dram_tensor('out', [NLAT, D], F32, kind="ExternalOutput").ap()

    c.modv = [dram(c, 'modv%d' % l, [2, 6 * D], F32) for l in range(DEPTH)]
    c.scr = []
    for l in range(DEPTH):
        S = {}
        for name, off, ncols in FM_GROUPS:
            S[name] = dram(c, '%s%d' % (name, l), [ncols, NT], F32 if name in ('AFT', 'ABT') else BF16)
        for name, off, ncols in TM_GROUPS:
            S[name] = dram(c, '%s%d' % (name, l), [NT, ncols], F32 if name == 'MKR' else BF16)
        for name in ('OGT', 'OMT', 'OHT'):
            S[name] = dram(c, '%s%d' % (name, l), [512, NT], BF16)
        c.scr.append(S)
    c.X1 = dram(c, 'X1', [NT, D], F32)
    c.X2 = dram(c, 'X2', [NT, D], F32)
    c.KpT = dram(c, 'KpT', [8, 97, NT], BF16)
    c.QpT = dram(c, 'QpT', [8, 97, NT], BF16)
    c.VpD = dram(c, 'VpD', [8, NT, 65], BF16)
    c.HYC = dram(c, 'HYC', [NT, 1536], BF16)
    c.Z2 = dram(c, 'Z2', [NT, 512], BF16)
    c.OH = dram(c, 'OH', [NT, 512], BF16)
    c.KTD = [dram(c, 'KTD0', [16384, 1024], BF16), dram(c, 'KTD1', [512, 1024], BF16)]
    c.KS = [dram(c, 'KS%d' % o, [128, 128, 2, 512], BF16) for o in range(2)]
    c.X1D = dram(c, 'X1D', [128, 128, 2, 512], BF16)
    c.QD = dram(c, 'QD', [128, 128, 2, 512], BF16)
    c.SCL = [dram(c, 'SCL%d' % j, [1, 1024], F32) for j in range(2)]

    c.cst = sb(c, 'cst_sb', [128, 1024], F32)
    c.ident = sb(c, 'ident16', [128, 128], BF16)
    c.cT = sb(c, 'cTsb', [128, 8, 2], F32)
    c.modT = sb(c, 'modT', [128, 2, 6, 8], F32)
    c.bank = [ps(c, 'bank%d' % i, [128, 512], F32) for i in range(8)]

    p.dma('sp', 'cst', [], ['cst'], [(c.cst[:, :], I['cst'][:, :])])
    p.op('dve', ['cst'], ['ident'], lambda E: E.tensor_copy(out=c.ident[:, :], in_=c.cst[:, 0:128]))
    p.dma('sp', 'cT', [], ['cT'],
          [(c.cT[:, :, r], I['cc'][r, :].rearrange("(k p) -> p k", p=128)) for r in range(2)], slow=True)
    p.op('act', ['cT'], ['cT'], lambda E: E.activation(out=c.cT[:, :, :], in_=c.cT[:, :, :], func=AF.Silu))

    for l in layers:
        if 'adaln' in stages:
            stage_adaln(c, l)
    if 'moe' in stages:
        for l in layers:
            moe_precast(c, l)
        if cfg.get('moe_mode', 'sparse') == 'sparse':
            c.zt = sb(c, 'zero_t', [128, 2048], BF16)
            p.op('pool', [], ['zero_t'], lambda E: E.memset(c.zt[:, :], 0.0))
            p.dma('sp', 'xbzero', ['zero_t'], ['XBZ'],
                  [(c.XB[i * 256:(i + 1) * 256, :].rearrange("(p a) f -> p (a f)", p=128), c.zt[:, :]) for i in range(196)])
    for l in layers:
        last = (l == DEPTH - 1)

        def xsrc(t, l=l):
            if l == 0:
                if t < 2:
                    return I['ctx'][t * 128:(t + 1) * 128, :]
                return I['x'][(t - 2) * 128:(t - 1) * 128, :]
            return c.X2[t * 128:(t + 1) * 128, :]
        if 'proj' in stages:
            stage_proj(c, l, xsrc)
        if 'gla' in stages:
            stage_gla(c, l)
        if 'mla' in stages:
            stage_mla(c, l, ctx_q=not last)
        if 'hyena' in stages:
            stage_hyena(c, l, with_ctx=not last)
        tiles = list(range(2, NTILE)) if last else list(range(NTILE))
        if 'merge' in stages:
            load_mod(c, l)
            stage_merge(c, l, xsrc, tiles)
        if 'moe' in stages:
            load_mod(c, l)

            def dst_fn(t, last=last):
                if last:
                    return [c.out[(t - 2) * 128:(t - 1) * 128, :]]
                return [c.X2[t * 128:(t + 1) * 128, :]]
            if cfg.get('moe_mode', 'sparse') == 'sparse':
                stage_moe2(c, l, tiles, dst_fn)
            else:
                stage_moe(c, l, tiles, dst_fn)
    p.finish('sp')
    return nc, c


ALL_STAGES = ['adaln', 'proj', 'gla', 'mla', 'hyena', 'merge', 'moe']
WEIGHT_KEYS = ['ada_w', 'ada_b', 'w_in', 'gla_wa2_f', 'gla_ba_f', 'gla_wa2_b', 'gla_ba_b', 'gla_norm', 'mla_q_norm', 'mla_w_uq',
               'mla_kv_norm', 'mla_w_ukv', 'hy_conv_w', 'hy_conv_b', 'hy_w1', 'hy_b1', 'hy_w2', 'hy_b2', 'hy_w3', 'hy_freq',
               'hy_bias', 'w_br_gla', 'w_br_mla', 'w_br_hy', 'w_out', 'ln1_g', 'ln1_b', 'ln2_g', 'ln2_b', 'router_w', 'router_b',
               'moe_w1', 'moe_b1', 'moe_w2', 'moe_b2']


def make_in_map(inputs, b, with_moe=True):
    f32 = lambda a: np.ascontiguousarray(np.asarray(a, dtype=np.float32))
    im = dict(x=f32(inputs['x'][b]), ctx=f32(inputs['ctx'][b]),
              cc=f32(np.stack([np.asarray(inputs['c'])[b], np.asarray(inputs['c_ctx'])])),
              cst=make_consts(), rope=make_rope())
    im.update(make_hyena_consts())
    for k in WEIGHT_KEYS:
        if not with_moe and k in ('moe_w1', 'moe_w2'):
            continue
        im[k] = f32(inputs[k])
    return im


def kernel(**inputs):
    nc, c = build_program(dict(layers=[0, 1], stages=ALL_STAGES))
    nb = np.asarray(inputs['x']).shape[0]
    in_maps = [make_in_map(inputs, b) for b in range(nb)]
    res = run_bass_kernel_spmd(nc, in_maps, core_ids=list(range(nb)))
    out = np.stack([np.asarray(res.results[b]['out']) for b in range(nb)], 0)
    return out.astype(np.float32)
```
